# Optimizing a Trainium2 kernel written in Bass

```python
import math
import jax
import jax.numpy as jnp
from jax import lax
import numpy as np

D_MODEL = 1024
BATCH = 8
SEQ = 4096
DEPTH = 4

D_MIX = D_MODEL
EPS = 1e-6
CONV_WIDTH = 4
CHUNK = 64
S5_WIDTH = D_MIX // 4
S5_CH = 16
S5_GROUPS = S5_WIDTH // S5_CH
S5_STATE = 64
GDN_WIDTH = (3 * D_MIX) // 8
GDN_HEAD_DIM = 64
GDN_HEADS = GDN_WIDTH // GDN_HEAD_DIM
GDN_CONV_DIM = 3 * GDN_WIDTH
SSD_WIDTH = D_MIX - S5_WIDTH - GDN_WIDTH
SSD_HEAD_DIM = 64
SSD_HEADS = SSD_WIDTH // SSD_HEAD_DIM
SSD_GROUPS = 2
SSD_STATE = 128
SSD_CONV_DIM = SSD_WIDTH + 2 * SSD_GROUPS * SSD_STATE
PROJ_SIZES = (S5_WIDTH, GDN_CONV_DIM, GDN_WIDTH, GDN_HEADS, GDN_HEADS,
              SSD_WIDTH, SSD_CONV_DIM, SSD_HEADS)
D_IN_PROJ = sum(PROJ_SIZES)
MOE_GROUPS = 4
EXPERTS_PER_GROUP = 8
N_EXPERTS = MOE_GROUPS * EXPERTS_PER_GROUP
TOP_K = 2
D_EXPERT = 256
MOE_BLOCK = 128

kernel_name = "hymba_s5_gdn_ssd_hiermoe_adaln"


def rms_norm(x, w):
    xf = x.astype(jnp.float32)
    xf = xf * lax.rsqrt(jnp.mean(xf * xf, axis=-1, keepdims=True) + EPS)
    return (xf * w.astype(jnp.float32)).astype(x.dtype)


def l2norm(x):
    return x * lax.rsqrt(jnp.sum(x * x, axis=-1, keepdims=True) + EPS)


def causal_conv(x, w, b=None):
    K, S = w.shape[0], x.shape[1]
    xp = jnp.pad(x, ((0, 0), (K - 1, 0), (0, 0)))
    y = sum(xp[:, j:j + S] * w[j] for j in range(K))
    if b is not None:
        y = y + b
    return jax.nn.silu(y)


def _linear_recurrence(e1, e2):
    a1, b1 = e1
    a2, b2 = e2
    return a1 * a2, a2 * b1 + b2


def s5_mixer(u, a_re, a_im, b_re, b_im, c_re, c_im, d_skip, log_dt, w_glu, norm_w):
    Bsz, S, _ = u.shape
    f32 = jnp.float32
    uf = u.astype(f32).reshape(Bsz, S, S5_GROUPS, S5_CH)
    lam = lax.complex(a_re.astype(f32), a_im.astype(f32))
    step = jnp.exp(log_dt.astype(f32))[:, None]
    lam_bar = jnp.exp(lam * step)
    b_bar = ((lam_bar - 1.0) / lam)[..., None] * lax.complex(b_re.astype(f32), b_im.astype(f32))
    bu = jnp.einsum('gpc,bsgc->bsgp', b_bar, uf.astype(jnp.complex64))
    lam_seq = jnp.broadcast_to(lam_bar, bu.shape)
    _, states = lax.associative_scan(_linear_recurrence, (lam_seq, bu), axis=1)
    c_mat = lax.complex(c_re.astype(f32), c_im.astype(f32))
    y = jnp.einsum('gcp,bsgp->bsgc', c_mat, states).real + d_skip.astype(f32).reshape(S5_GROUPS, S5_CH) * uf
    y = jax.nn.gelu(y.reshape(Bsz, S, S5_WIDTH))
    y = y * jax.nn.sigmoid(y @ w_glu.astype(f32))
    return rms_norm(y, norm_w).astype(u.dtype)


def gdn_mixer(qkv, z, a, b, conv_w, a_log, dt_bias, norm_w):
    Bsz, S, _ = qkv.shape
    H, Dh, C = GDN_HEADS, GDN_HEAD_DIM, CHUNK
    NC = S // C
    f32 = jnp.float32
    qkv_c = causal_conv(qkv, conv_w).astype(f32)

    def chunks(t):
        return t.reshape(Bsz, NC, C, H, Dh).transpose(0, 3, 1, 2, 4)

    q, k, v = (chunks(t) for t in jnp.split(qkv_c, 3, axis=-1))
    q = l2norm(q) * Dh ** -0.5
    k = l2norm(k)
    beta = jax.nn.sigmoid(b.astype(f32)).reshape(Bsz, NC, C, H).transpose(0, 3, 1, 2)
    g = -jnp.exp(a_log.astype(f32)) * jax.nn.softplus(a.astype(f32) + dt_bias.astype(f32))
    gc = jnp.cumsum(g.reshape(Bsz, NC, C, H).transpose(0, 3, 1, 2), axis=-1)
    causal = jnp.tril(jnp.ones((C, C), bool))
    strict = jnp.tril(jnp.ones((C, C), bool), -1)
    decay = jnp.exp(jnp.where(causal, gc[..., :, None] - gc[..., None, :], -jnp.inf))
    k_beta = k * beta[..., None]
    lower = jnp.where(strict, jnp.einsum('bhncd,bhnmd->bhncm', k_beta, k) * decay, 0.0)
    rhs = jnp.concatenate([v * beta[..., None], k_beta * jnp.exp(gc)[..., None]], axis=-1)
    sol = lax.linalg.triangular_solve(lower, rhs, left_side=True, lower=True, unit_diagonal=True)
    u_val, w_dec = sol[..., :Dh], sol[..., Dh:]
    attn = jnp.einsum('bhncd,bhnmd->bhncm', q, k) * decay
    q_dec = q * jnp.exp(gc)[..., None]
    k_dec = k * jnp.exp(gc[..., -1:] - gc)[..., None]
    g_tot = jnp.exp(gc[..., -1])

    def step(state, inp):
        attn_c, q_c, w_c, u_c, k_c, gt_c = inp
        v_new = u_c - jnp.einsum('bhcd,bhde->bhce', w_c, state)
        o = jnp.einsum('bhcd,bhde->bhce', q_c, state) + jnp.einsum('bhcm,bhme->bhce', attn_c, v_new)
        state = state * gt_c[..., None, None] + jnp.einsum('bhcd,bhce->bhde', k_c, v_new)
        return state, o

    xs = tuple(jnp.moveaxis(t, 2, 0) for t in (attn, q_dec, w_dec, u_val, k_dec, g_tot))
    _, o = lax.scan(step, jnp.zeros((Bsz, H, Dh, Dh), f32), xs)
    o = o.transpose(1, 0, 3, 2, 4).reshape(Bsz, S, H, Dh)
    o = rms_norm(o, norm_w) * jax.nn.silu(z.astype(f32).reshape(Bsz, S, H, Dh))
    return o.reshape(Bsz, S, GDN_WIDTH).astype(qkv.dtype)


def ssd_mixer(z, xbc, dt_raw, conv_w, conv_b, a_log, dt_bias, d_skip, norm_w):
    Bsz, S, _ = z.shape
    H, P, G, N, C = SSD_HEADS, SSD_HEAD_DIM, SSD_GROUPS, SSD_STATE, CHUNK
    R, NC = H // G, S // C
    f32 = jnp.float32
    xbc_c = causal_conv(xbc, conv_w, conv_b).astype(f32)
    xs, bm, cm = jnp.split(xbc_c, [SSD_WIDTH, SSD_WIDTH + G * N], axis=-1)
    xs = xs.reshape(Bsz, NC, C, G, R, P)
    bm = bm.reshape(Bsz, NC, C, G, N)
    cm = cm.reshape(Bsz, NC, C, G, N)
    dt = jax.nn.softplus(dt_raw.astype(f32) + dt_bias.astype(f32)).reshape(Bsz, NC, C, G, R)
    a = -jnp.exp(a_log.astype(f32)).reshape(G, R)
    x_dt = xs * dt[..., None]
    acs = jnp.cumsum((dt * a).transpose(0, 3, 4, 1, 2), axis=-1)
    causal = jnp.tril(jnp.ones((C, C), bool))
    seg = jnp.exp(jnp.where(causal, acs[..., :, None] - acs[..., None, :], -jnp.inf))
    cb = jnp.einsum('bnlgd,bnmgd->bgnlm', cm, bm)
    y_diag = jnp.einsum('bgnlm,bgrnlm,bnmgrp->bnlgrp', cb, seg, x_dt)
    decay_in = jnp.exp(acs[..., -1:] - acs)
    chunk_states = jnp.einsum('bnmgd,bgrnm,bnmgrp->bngrpd', bm, decay_in, x_dt)
    chunk_decay = jnp.exp(acs[..., -1])
    decay_out = jnp.exp(acs)

    def step(state, inp):
        c_n, dout_n, cs_n, cd_n = inp
        y_off = jnp.einsum('blgd,bgrpd,bgrl->blgrp', c_n, state, dout_n)
        state = state * cd_n[..., None, None] + cs_n
        return state, y_off

    scan_in = (jnp.moveaxis(cm, 1, 0), jnp.moveaxis(decay_out, 3, 0),
               jnp.moveaxis(chunk_states, 1, 0), jnp.moveaxis(chunk_decay, 3, 0))
    _, y_off = lax.scan(step, jnp.zeros((Bsz, G, R, P, N), f32), scan_in)
    y = y_diag + jnp.moveaxis(y_off, 0, 1) + d_skip.astype(f32).reshape(G, R, 1) * xs
    y = y.reshape(Bsz, S, SSD_WIDTH) * jax.nn.silu(z.astype(f32))
    y = rms_norm(y.reshape(Bsz, S, G, SSD_WIDTH // G), norm_w.reshape(G, SSD_WIDTH // G))
    return y.reshape(Bsz, S, SSD_WIDTH).astype(z.dtype)


def hier_moe(h, w_grp, b_grp, w_rt, b_rt, w_gate, w_up, w_down):
    Bsz, S, D = h.shape
    N = Bsz * S
    NK = N * TOP_K
    f32 = jnp.float32
    xt = h.reshape(N, D)
    lg = (xt @ w_grp).astype(f32) + b_grp.astype(f32)
    g_idx = jnp.argmax(lg, axis=-1)
    g_w = jnp.take_along_axis(jax.nn.softmax(lg, axis=-1), g_idx[:, None], axis=1)[:, 0]
    le = ((xt @ w_rt).astype(f32) + b_rt.astype(f32)).reshape(N, MOE_GROUPS, EXPERTS_PER_GROUP)
    le_sel = jnp.take_along_axis(le, g_idx[:, None, None], axis=1)[:, 0]
    top_v, top_i = lax.top_k(le_sel, TOP_K)
    w_e = jax.nn.softmax(top_v, axis=-1)
    eid = (g_idx[:, None] * EXPERTS_PER_GROUP + top_i).reshape(NK).astype(jnp.int32)
    wt = (g_w[:, None] * w_e).reshape(NK)
    tok = jnp.repeat(jnp.arange(N, dtype=jnp.int32), TOP_K)
    order = jnp.argsort(eid)
    eid_s, tok_s, wt_s = eid[order], tok[order], wt[order]
    counts = jnp.bincount(eid, length=N_EXPERTS)
    padded = ((counts + MOE_BLOCK - 1) // MOE_BLOCK) * MOE_BLOCK
    start = jnp.cumsum(counts) - counts
    pend = jnp.cumsum(padded)
    pstart = pend - padded
    dest = pstart[eid_s] + jnp.arange(NK, dtype=jnp.int32) - start[eid_s]
    L_pad = NK + N_EXPERTS * MOE_BLOCK
    n_blk = L_pad // MOE_BLOCK
    buf_tok = jnp.full((L_pad,), N, jnp.int32).at[dest].set(tok_s)
    buf_wt = jnp.zeros((L_pad,), f32).at[dest].set(wt_s)
    blk_e = jnp.minimum(jnp.searchsorted(pend, jnp.arange(n_blk) * MOE_BLOCK, side='right'), N_EXPERTS - 1)
    x_pad = jnp.concatenate([xt, jnp.zeros((1, D), xt.dtype)], axis=0)
    xb = x_pad[buf_tok].reshape(n_blk, MOE_BLOCK, D)

    def expert_block(args):
        xblk, e = args
        hid = jax.nn.silu(xblk @ w_gate[e]) * (xblk @ w_up[e])
        return hid @ w_down[e]

    yb = lax.map(expert_block, (xb, blk_e)).reshape(L_pad, D)
    y = yb * buf_wt[:, None].astype(yb.dtype)
    out = jax.ops.segment_sum(y, buf_tok, num_segments=N + 1)[:N]
    return out.reshape(Bsz, S, D)


def setup_inputs(seed: int = 0) -> dict:
    key = jax.random.key(seed)
    k = jax.random.split(key, 36)
    f32 = jnp.float32
    L, D = DEPTH, D_MODEL

    def nrm(i, shape, scale):
        return scale * jax.random.normal(k[i], shape, f32)

    def unif(i, shape, lo, hi):
        return jax.random.uniform(k[i], shape, f32, lo, hi)

    def dt_bias(i, shape):
        dt = jnp.exp(unif(i, shape, math.log(1e-3), math.log(1e-1)))
        return dt + jnp.log(-jnp.expm1(-dt))

    s5_n = jnp.arange(S5_STATE, dtype=f32)
    return {
        "x": nrm(0, (BATCH, SEQ, D), 1.0),
        "c": nrm(1, (BATCH, D), 1.0),
        "w_ada": nrm(2, (L, D, 6 * D), 0.5 * D ** -0.5),
        "b_ada": nrm(3, (L, 6 * D), 0.02),
        "norm_mix": 1.0 + nrm(4, (L, D), 0.02),
        "norm_ffn": 1.0 + nrm(5, (L, D), 0.02),
        "w_in": nrm(6, (L, D, D_IN_PROJ), D ** -0.5),
        "w_out": nrm(7, (L, D_MIX, D), D_MIX ** -0.5),
        "s5_a_re": -0.5 + nrm(8, (L, S5_GROUPS, S5_STATE), 0.01),
        "s5_a_im": math.pi * s5_n + nrm(9, (L, S5_GROUPS, S5_STATE), 0.01),
        "s5_b_re": nrm(10, (L, S5_GROUPS, S5_STATE, S5_CH), (2 * S5_CH) ** -0.5),
        "s5_b_im": nrm(11, (L, S5_GROUPS, S5_STATE, S5_CH), (2 * S5_CH) ** -0.5),
        "s5_c_re": nrm(12, (L, S5_GROUPS, S5_CH, S5_STATE), (2 * S5_STATE) ** -0.5),
        "s5_c_im": nrm(13, (L, S5_GROUPS, S5_CH, S5_STATE), (2 * S5_STATE) ** -0.5),
        "s5_d": nrm(14, (L, S5_WIDTH), 1.0),
        "s5_log_dt": unif(15, (L, S5_GROUPS), math.log(1e-3), math.log(1e-1)),
        "s5_w_glu": nrm(16, (L, S5_WIDTH, S5_WIDTH), S5_WIDTH ** -0.5),
        "s5_norm": 1.0 + nrm(17, (L, S5_WIDTH), 0.02),
        "gdn_conv_w": nrm(18, (L, CONV_WIDTH, GDN_CONV_DIM), CONV_WIDTH ** -0.5),
        "gdn_a_log": jnp.log(unif(19, (L, GDN_HEADS), 1.0, 16.0)),
        "gdn_dt_bias": dt_bias(20, (L, GDN_HEADS)),
        "gdn_norm": 1.0 + nrm(21, (L, GDN_HEAD_DIM), 0.02),
        "ssd_conv_w": nrm(22, (L, CONV_WIDTH, SSD_CONV_DIM), CONV_WIDTH ** -0.5),
        "ssd_conv_b": nrm(23, (L, SSD_CONV_DIM), 0.02),
        "ssd_a_log": jnp.log(unif(24, (L, SSD_HEADS), 1.0, 16.0)),
        "ssd_dt_bias": dt_bias(25, (L, SSD_HEADS)),
        "ssd_d": 1.0 + nrm(26, (L, SSD_HEADS), 0.02),
        "ssd_norm": 1.0 + nrm(27, (L, SSD_WIDTH), 0.02),
        "moe_w_grp": nrm(28, (L, D, MOE_GROUPS), D ** -0.5),
        "moe_b_grp": nrm(29, (L, MOE_GROUPS), 0.01),
        "moe_w_rt": nrm(30, (L, D, N_EXPERTS), D ** -0.5),
        "moe_b_rt": nrm(31, (L, N_EXPERTS), 0.01),
        "moe_w_gate": nrm(32, (L, N_EXPERTS, D, D_EXPERT), D ** -0.5),
        "moe_w_up": nrm(33, (L, N_EXPERTS, D, D_EXPERT), D ** -0.5),
        "moe_w_down": nrm(34, (L, N_EXPERTS, D_EXPERT, D), D_EXPERT ** -0.5),
        "norm_final": 1.0 + nrm(35, (D,), 0.02),
    }


def reference(x, c, w_ada, b_ada, norm_mix, norm_ffn, w_in, w_out,
              s5_a_re, s5_a_im, s5_b_re, s5_b_im, s5_c_re, s5_c_im, s5_d, s5_log_dt, s5_w_glu, s5_norm,
              gdn_conv_w, gdn_a_log, gdn_dt_bias, gdn_norm,
              ssd_conv_w, ssd_conv_b, ssd_a_log, ssd_dt_bias, ssd_d, ssd_norm,
              moe_w_grp, moe_b_grp, moe_w_rt, moe_b_rt, moe_w_gate, moe_w_up, moe_w_down,
              norm_final):
    Bsz = x.shape[0]
    splits = np.cumsum(PROJ_SIZES)[:-1].tolist()
    cond = jax.nn.silu(c)
    for l in range(DEPTH):
        mod = (cond @ w_ada[l] + b_ada[l]).reshape(Bsz, 6, 1, D_MODEL)
        h = rms_norm(x, norm_mix[l]) * (1.0 + mod[:, 1]) + mod[:, 0]
        proj = h @ w_in[l]
        s5_u, g_qkv, g_z, g_a, g_b, s_z, s_xbc, s_dt = jnp.split(proj, splits, axis=-1)
        y_s5 = s5_mixer(s5_u, s5_a_re[l], s5_a_im[l], s5_b_re[l], s5_b_im[l], s5_c_re[l], s5_c_im[l],
                        s5_d[l], s5_log_dt[l], s5_w_glu[l], s5_norm[l])
        y_gdn = gdn_mixer(g_qkv, g_z, g_a, g_b, gdn_conv_w[l], gdn_a_log[l], gdn_dt_bias[l], gdn_norm[l])
        y_ssd = ssd_mixer(s_z, s_xbc, s_dt, ssd_conv_w[l], ssd_conv_b[l], ssd_a_log[l], ssd_dt_bias[l],
                          ssd_d[l], ssd_norm[l])
        y = jnp.concatenate([y_s5, y_gdn, y_ssd], axis=-1) @ w_out[l]
        x = x + mod[:, 2] * y
        h = rms_norm(x, norm_ffn[l]) * (1.0 + mod[:, 4]) + mod[:, 3]
        x = x + mod[:, 5] * hier_moe(h, moe_w_grp[l], moe_b_grp[l], moe_w_rt[l], moe_b_rt[l],
                                     moe_w_gate[l], moe_w_up[l], moe_w_down[l])
    return rms_norm(x, norm_final)
```

```python
import contextlib
import math
import numpy as np
import concourse.bass as bass
import concourse.mybir as mybir
from concourse.bass_utils import run_bass_kernel_spmd

F32 = mybir.dt.float32
BF16 = mybir.dt.bfloat16
ALU = mybir.AluOpType
AF = mybir.ActivationFunctionType
AX = mybir.AxisListType

D = 1024
NEXP = 32
DEXP = 256
EPS = 1e-6
ENGS = ("pe", "act", "dve", "pool", "sp")


class Buf:
    def __init__(self, t, name, multi=False):
        self.t = t
        self.name = name
        self.w = {}
        self.r = {}
        self.sem = None
        self.dcnt = 0
        self.multi = multi

    def __getitem__(self, k):
        return self.t[k]


class Prog:
    def __init__(self, nc, es):
        self.nc = nc
        self.es = es
        self.q = {e: [] for e in ENGS}
        self.sems = []
        self.esem = {}
        self.ecnt = {e: 0 for e in ENGS}
        self.waited = {e: {} for e in ENGS}
        for e in ENGS:
            if e != "sp":
                self.esem[e] = self.newsem("e_" + e)
        self.uid = 0
        self.dsem_pool = []
        self.dbufs = []
        self.phase_bufs = []

    def newsem(self, name):
        s = self.es.enter_context(self.nc.semaphore(name))
        self.sems.append(s)
        return len(self.sems) - 1

    def sb(self, es, name, shape, dtype):
        self.uid += 1
        name = f"{name}_{self.uid}"
        t = es.enter_context(self.nc.sbuf_tensor(name, list(shape), dtype))
        b = Buf(t, name)
        self.phase_bufs.append(b)
        return b

    def fence(self):
        toks = {}
        for e, s in self.esem.items():
            if self.ecnt[e] > 0:
                toks[s] = self.ecnt[e]
        for b in self.dbufs:
            toks[b.sem] = max(toks.get(b.sem, 0), b.dcnt)
        for e in ENGS:
            wd = self.waited[e]
            waits = []
            for s, v in toks.items():
                if wd.get(s, 0) < v:
                    waits.append((s, v))
                    wd[s] = v
            if waits:
                self.q[e].append((waits, None, None))

    def end_phase(self):
        self.fence()
        for b in self.phase_bufs:
            if b.sem is not None:
                self.dsem_pool.append((b.sem, b.dcnt))
                self.dbufs.remove(b)
                b.sem = None
        self.phase_bufs = []
        self.emit()

    def ps(self, es, name, shape, dtype):
        self.uid += 1
        name = f"{name}_{self.uid}"
        t = es.enter_context(self.nc.psum_tensor(name, list(shape), dtype))
        return Buf(t, name)

    def dram(self, name, shape, dtype, kind="Internal"):
        t = self.nc.dram_tensor(name, list(shape), dtype, kind=kind).ap()
        return Buf(t, name)

    def region(self, name):
        return Buf(None, name, multi=True)

    def op(self, eng, fn, reads=(), writes=(), dma=None):
        deps = {}
        for b in reads:
            for s, v in b.w.items():
                if deps.get(s, 0) < v:
                    deps[s] = v
        for b in writes:
            for d in ((b.r,) if b.multi else (b.w, b.r)):
                for s, v in d.items():
                    if deps.get(s, 0) < v:
                        deps[s] = v
        wd = self.waited[eng]
        waits = []
        for s, v in deps.items():
            if wd.get(s, 0) < v:
                waits.append((s, v))
                wd[s] = v
        if dma is None:
            s = self.esem[eng]
            self.ecnt[eng] += 1
            tok = (s, self.ecnt[eng])
            inc = (s, 1)
        else:
            if dma.sem is None:
                if self.dsem_pool:
                    dma.sem, dma.dcnt = self.dsem_pool.pop()
                else:
                    dma.sem = self.newsem("d_" + dma.name)
                self.dbufs.append(dma)
            dma.dcnt += 16
            tok = (dma.sem, dma.dcnt)
            inc = (dma.sem, 16)
        self.q[eng].append((waits, fn, inc))
        for b in reads:
            if b.r.get(tok[0], 0) < tok[1]:
                b.r[tok[0]] = tok[1]
        for b in writes:
            if b.multi:
                if b.w.get(tok[0], 0) < tok[1]:
                    b.w[tok[0]] = tok[1]
            else:
                b.w = {tok[0]: tok[1]}
                b.r = {}

    def final_wait(self, eng, bufs):
        deps = {}
        for b in bufs:
            for d in (b.w, b.r):
                for s, v in d.items():
                    deps[s] = max(deps.get(s, 0), v)
        self.q[eng].append((list(deps.items()), None, None))

    def emit(self):
        nc = self.nc
        sems = self.sems
        qs = self.q
        self.q = {e: [] for e in ENGS}

        def mk(e):
            def f(eng):
                for waits, fn, inc in qs[e]:
                    for s, v in waits:
                        eng.wait_ge(sems[s], v)
                    if fn is None:
                        continue
                    ins = fn(eng)
                    ins.then_inc(sems[inc[0]], inc[1])
            return f

        with nc.Block() as block:
            block.sync(mk("sp"))
            block.scalar(mk("act"))
            block.vector(mk("dve"))
            block.gpsimd(mk("pool"))
            block.tensor(mk("pe"))


class H:
    def __init__(self, P):
        self.P = P

    def dma(self, eng, out, in_, R, W, buf, slow=False):
        if slow:
            self.P.op(eng, lambda e: e.dma_start(out=out, in_=in_, allow_slow_non_contiguous=True), reads=R, writes=W, dma=buf)
        else:
            self.P.op(eng, lambda e: e.dma_start(out=out, in_=in_), reads=R, writes=W, dma=buf)

    def tt(self, eng, out, in0, in1, op, R, W):
        self.P.op(eng, lambda e: e.tensor_tensor(out=out, in0=in0, in1=in1, op=op), reads=R, writes=W)

    def ts(self, eng, out, in0, s1, s2, op0, op1, R, W):
        if s2 is None:
            self.P.op(eng, lambda e: e.tensor_scalar(out=out, in0=in0, scalar1=s1, scalar2=None, op0=op0), reads=R, writes=W)
        else:
            self.P.op(eng, lambda e: e.tensor_scalar(out=out, in0=in0, scalar1=s1, scalar2=s2, op0=op0, op1=op1), reads=R, writes=W)

    def stt(self, eng, out, in0, sc, in1, op0, op1, R, W):
        eng = "dve"
        self.P.op(eng, lambda e: e.scalar_tensor_tensor(out=out, in0=in0, scalar=sc, in1=in1, op0=op0, op1=op1), reads=R, writes=W)

    def act(self, out, in_, func, R, W, bias=None, scale=None, accum=None):
        kw = {}
        if bias is not None:
            kw["bias"] = bias
        if scale is not None:
            kw["scale"] = scale
        if accum is not None:
            kw["accum_out"] = accum
        self.P.op("act", lambda e: e.activation(out=out, in_=in_, func=func, **kw), reads=R, writes=W)

    def cp(self, eng, out, in_, R, W):
        if eng == "act":
            self.P.op("act", lambda e: e.copy(out=out, in_=in_), reads=R, writes=W)
        else:
            self.P.op(eng, lambda e: e.tensor_copy(out=out, in_=in_), reads=R, writes=W)

    def memset(self, eng, ap, val, W):
        self.P.op(eng, lambda e: e.memset(ap, val), writes=W)

    def recip(self, out, in_, R, W):
        self.P.op("dve", lambda e: e.reciprocal(out=out, in_=in_), reads=R, writes=W)

    def absv(self, eng, out, in_, R, W):
        self.P.op(eng, lambda e: e.tensor_single_scalar(out=out, in_=in_, scalar=0.0, op=ALU.abs_max), reads=R, writes=W)

    def reduce(self, out, in_, op, R, W):
        self.P.op("dve", lambda e: e.tensor_reduce(out=out, in_=in_, axis=AX.X, op=op), reads=R, writes=W)

    def mm(self, items, R, W):
        def f(e):
            r = None
            for (o, l, rh, st, sp) in items:
                r = e.matmul(o, lhsT=l, rhs=rh, start=st, stop=sp)
            return r
        self.P.op("pe", f, reads=R, writes=W)

    def tr(self, items, ident, R, W):
        def f(e):
            r = None
            for (o, i) in items:
                r = e.transpose(out=o, in_=i, identity=ident)
            return r
        self.P.op("pe", f, reads=R, writes=W)

    def select(self, out, in_, cmp, fill, base, cm, pattern, R, W):
        self.P.op("pool", lambda e: e.affine_select(out=out, in_=in_, pattern=pattern, compare_op=cmp, fill=fill,
                                                    base=base, channel_multiplier=cm), reads=R, writes=W)


def b3(ap, n, m):
    return ap.unsqueeze(1).to_broadcast([128, n, m])


def s3(ap, n, m):
    return ap.unsqueeze(2).to_broadcast([128, n, m])


def s4(ap):
    return ap.rearrange("p (b h) -> p b h", b=2).unsqueeze(3).to_broadcast([128, 2, 3, 128])


def v4(ap):
    return ap.rearrange("p (b h) l -> p b h l", b=2)


def w4(ps):
    return ps[:, :, 0:384].rearrange("p b (h l) -> p b h l", h=3)


def softplus12(h, P, es, tagp):
    xa = P.sb(es, tagp + "xa", [128, 12], F32)
    ax = P.sb(es, tagp + "ax", [128, 12], F32)
    ex = P.sb(es, tagp + "ex", [128, 12], F32)
    ln = P.sb(es, tagp + "ln", [128, 12], F32)
    one = P.sb(es, tagp + "one", [128, 1], F32)
    h.memset("pool", one[:], 1.0, [one])

    def f(xin, xin_buf, bias, out):
        h.tt("dve", xa[:], xin, bias[:], ALU.add, [xin_buf, bias], [xa])
        h.act(ax[:], xa[:], AF.Abs, [xa], [ax])
        h.act(ex[:], ax[:], AF.Exp, [ax], [ex], scale=-1.0)
        h.act(ln[:], ex[:], AF.Ln, [ex, one], [ln], bias=one[:, 0:1])
        h.ts("dve", xa[:], xa[:], 0.0, None, ALU.max, None, [xa], [xa])
        h.tt("dve", out[:], xa[:], ln[:], ALU.add, [xa, ln], [out])
    return f


def make_masks(h, P, es):
    m = {}
    ones = P.sb(es, "m_ones", [128, 128], F32)
    h.memset("pool", ones[:], 1.0, [ones])
    m["ones"] = ones
    for name, cmp, cm, st in (("U", ALU.is_ge, -1, 1), ("L", ALU.is_ge, 1, -1), ("Ls", ALU.is_gt, 1, -1)):
        t = P.sb(es, "m_" + name, [128, 128], F32)
        h.select(t[:], ones[:], cmp, 0.0, 0, cm, [[st, 128]], [ones], [t])
        m[name] = t
    sel = P.sb(es, "m_sel", [128, 128], F32)
    zer = P.sb(es, "m_zero", [128, 128], F32)
    h.memset("pool", zer[:], 0.0, [zer])
    h.select(sel[:], zer[:], ALU.not_equal, 1.0, -127, 1, [[0, 128]], [zer], [sel])
    m["sel"] = sel
    bd = P.sb(es, "m_bd", [128, 128], F32)
    h.memset("pool", bd[:], 0.0, [bd])
    h.memset("pool", bd[0:64, 0:64], 1.0, [bd])
    h.memset("pool", bd[64:128, 64:128], 1.0, [bd])
    m["bd"] = bd
    return m


def mixer_layer(k, l, cur, dstt):
    P, T, flags, h = k.P, k.T, k.flags, k.h
    inp = k.inp
    src, rsrc = cur
    dst, rdst = dstt
    NMT = T // 512
    MT = 512
    identf, identb = k.identf, k.identb
    u_d, qn_d, kn_d, v_d, xs_d, B_d, C_d, pt_d, y_d = k.u_d, k.qn_d, k.kn_d, k.v_d, k.xs_d, k.B_d, k.C_d, k.pt_d, k.y_d
    r_pre, r_y = k.r_pre, k.r_y

    with contextlib.ExitStack() as es:
        A, B = k.norm_consts(es, l, inp["norm_mix"].t[l:l + 1, :], 1, 0, "m")
        winf = P.sb(es, "winf", [128, 8, 2304], BF16)
        wint = P.sb(es, "wint", [128, 8, 786], BF16)
        for (c0, c1) in ((0, 1152), (1152, 2304)):
            h.dma("pool", winf[:, :, c0:c1], inp["w_in_f"].t[l, :, c0:c1].rearrange("(k p) n -> p k n", p=128), [], [winf], winf)
        h.dma("pool", wint[:], inp["w_in_t"].t[l].rearrange("(k p) n -> p k n", p=128), [], [wint], wint)
        cwt = P.sb(es, "cwt", [128, 16, 4], F32)
        cbt = P.sb(es, "cbt", [128, 16], F32)
        h.dma("sp", cwt[:], inp["conv_w"].t[l], [], [cwt], cwt)
        h.dma("sp", cbt[:], inp["conv_b"].t[l], [], [cbt], cbt)
        carry = P.sb(es, "carry", [128, 16, 3], F32)
        h.memset("pool", carry[:], 0.0, [carry])
        epsb = P.sb(es, "epsb", [128, 1], F32)
        h.memset("pool", epsb[:], EPS, [epsb])
        bones = P.sb(es, "bones", [128, 128], F32)
        h.memset("pool", bones[:], 0.0, [bones])
        h.memset("pool", bones[0:64, 0:64], 1.0, [bones])
        h.memset("pool", bones[64:128, 64:128], 1.0, [bones])
        xt = [P.sb(es, f"m1x{i}", [128, D], F32) for i in range(2)]
        tmp = P.sb(es, "m1tmp", [128, D], F32)
        hb = P.sb(es, "m1hb", [128, D], BF16)
        hT = P.sb(es, "m1hT", [128, 8, MT], BF16)
        cin = [P.sb(es, f"cin{i}", [128, MT + 3], F32) for i in range(2)]
        acc = [P.sb(es, f"acc{i}", [128, MT], F32) for i in range(2)]
        so = [P.sb(es, f"so{i}", [128, MT], F32) for i in range(2)]
        sob = [P.sb(es, f"sob{i}", [128, MT], BF16) for i in range(2)]
        sq = P.sb(es, "m1sq", [128, MT], F32)
        rinv = P.sb(es, "m1rinv", [128, MT], F32)
        ptst = [P.sb(es, f"ptst{i}", [128, 786], F32) for i in range(2)]
        ssq = P.sb(es, "m1ssq", [128, 1], F32)
        rstd = P.sb(es, "m1rstd", [128, 1], F32)
        ptr = P.ps(es, "m1ptr", [128, 8, 128], BF16)
        pp = [P.ps(es, f"m1pp{i}", [128, MT], F32) for i in range(2)]
        pt = P.ps(es, "m1pt", [128, 2, 512], F32)
        pq = P.ps(es, "m1pq", [128, MT], F32)
        for mt in range(NMT):
            cols = slice(mt * MT, (mt + 1) * MT)
            for ti in range(4):
                t = mt * 4 + ti
                xb = xt[t % 2]
                h.dma("sp", xb[:], src.t[t * 128:(t + 1) * 128, :], [rsrc[t]], [xb], xb)
                k.rms_mod(xb, A, B, hb, ssq, rstd, tmp, tmp)
                h.tr([(ptr[:, kk, :], hb[:, kk * 128:(kk + 1) * 128]) for kk in range(8)], identb[:], [hb, identb], [ptr])
                h.cp("act", hT[:, :, ti * 128:(ti + 1) * 128], ptr[:], [ptr], [hT])
            for c in range(18):
                pb = pp[c % 2]
                h.mm([(pb[:], winf[:, kk, c * 128:(c + 1) * 128], hT[:, kk, :], kk == 0, kk == 7) for kk in range(8)], [winf, hT], [pb])
                if c < 2:
                    sb_ = so[c % 2]
                    h.cp("act", sb_[:], pb[:], [pb], [sb_])
                    h.dma("sp", u_d.t[c * 128:(c + 1) * 128, cols], sb_[:], [sb_], [r_pre[mt]], sb_)
                    continue
                ci = c - 2
                cb_ = cin[ci % 2]
                h.cp("pool", cb_[:, 0:3], carry[:, ci, :], [carry], [cb_])
                h.cp("act", cb_[:, 3:MT + 3], pb[:], [pb], [cb_])
                h.cp("pool", carry[:, ci, :], cb_[:, MT:MT + 3], [cb_], [carry])
                ab = acc[ci % 2]
                h.ts("dve", ab[:], cb_[:, 0:MT], cwt[:, ci, 0:1], None, ALU.mult, None, [cb_, cwt], [ab])
                for j in range(1, 4):
                    h.stt("dve", ab[:], cb_[:, j:j + MT], cwt[:, ci, j:j + 1], ab[:], ALU.mult, ALU.add, [cb_, cwt, ab], [ab])
                sb_ = so[ci % 2]
                h.act(sb_[:], ab[:], AF.Silu, [ab, cbt], [sb_], bias=cbt[:, ci:ci + 1])
                if ci < 6:
                    h.tt("pool", sq[:], sb_[:], sb_[:], ALU.mult, [sb_], [sq])
                    h.mm([(pq[:], bones[:], sq[:], True, True)], [bones, sq], [pq])
                    h.act(rinv[:], pq[:], AF.Sqrt, [pq, epsb], [rinv], bias=epsb[:, 0:1])
                    h.recip(rinv[:], rinv[:], [rinv], [rinv])
                    ob = sob[ci % 2]
                    if ci < 3:
                        h.stt("dve", ob[:], sb_[:], 0.125, rinv[:], ALU.mult, ALU.mult, [sb_, rinv], [ob])
                    else:
                        h.tt("dve", ob[:], sb_[:], rinv[:], ALU.mult, [sb_, rinv], [ob])
                    dd = qn_d if ci < 3 else kn_d
                    j = ci % 3
                    h.dma("sp", dd.t[j * 128:(j + 1) * 128, cols], ob[:], [ob], [r_pre[mt]], ob)
                elif ci < 12:
                    dd = v_d if ci < 9 else xs_d
                    j = (ci - 6) % 3
                    h.dma("sp", dd.t[j * 128:(j + 1) * 128, cols], sb_[:], [sb_], [r_pre[mt]], sb_)
                else:
                    ob = sob[ci % 2]
                    h.cp("pool", ob[:], sb_[:], [sb_], [ob])
                    dd = B_d if ci < 14 else C_d
                    j = (ci - 12) % 2
                    h.dma("sp", dd.t[j * 128:(j + 1) * 128, cols], ob[:], [ob], [r_pre[mt]], ob)
            for ti in range(4):
                t = mt * 4 + ti
                tc = slice(ti * 128, (ti + 1) * 128)
                h.mm([(pt[:, 0, :], hT[:, kk, tc], wint[:, kk, 0:512], kk == 0, kk == 7) for kk in range(8)]
                     + [(pt[:, 1, 0:274], hT[:, kk, tc], wint[:, kk, 512:786], kk == 0, kk == 7) for kk in range(8)],
                     [hT, wint], [pt])
                stg = ptst[t % 2]
                h.cp("act", stg[:, 0:512], pt[:, 0, :], [pt], [stg])
                h.cp("dve", stg[:, 512:786], pt[:, 1, 0:274], [pt], [stg])
                h.dma("sp", pt_d.t[t * 128:(t + 1) * 128, :], stg[:], [stg], [r_pre[mt]], stg)
        P.end_phase()

    if flags.get("s5", True):
        s5_phase(k, l)
    if flags.get("gdn", True):
        gdn_phase(k, l)
    if flags.get("ssd", True):
        ssd_phase(k, l)

    with contextlib.ExitStack() as es:
        wout = P.sb(es, "wout", [128, 8, D], BF16)
        h.dma("pool", wout[:], inp["w_out"].t[l].rearrange("(k p) n -> p k n", p=128), [], [wout], wout)
        G = P.sb(es, "Gm", [128, D], F32)
        k.load_row(G, k.modv.t[l:l + 1, 2 * D:3 * D], [k.r_modv])
        yT = [P.sb(es, f"m5y{i}", [128, 8, MT], BF16) for i in range(2)]
        xt = [P.sb(es, f"m5x{i}", [128, D], F32) for i in range(2)]
        tm = [P.sb(es, f"m5t{i}", [128, D], F32) for i in range(2)]
        po = [P.ps(es, f"m5p{i}", [128, 512], F32) for i in range(4)]
        for mt in range(NMT):
            cols = slice(mt * MT, (mt + 1) * MT)
            yb = yT[mt % 2]
            h.dma("sp", yb[:], y_d.t[:, cols].rearrange("(k p) t -> p k t", p=128), [r_y[0][mt], r_y[1][mt], r_y[2][mt]], [yb], yb)
            if not flags.get("s5", True):
                h.memset("pool", yb[:, 0:2, :], 0.0, [yb])
            if not flags.get("gdn", True):
                h.memset("pool", yb[:, 2:5, :], 0.0, [yb])
            if not flags.get("ssd", True):
                h.memset("pool", yb[:, 5:8, :], 0.0, [yb])
            for ti in range(4):
                t = mt * 4 + ti
                tc = slice(ti * 128, (ti + 1) * 128)
                xb = xt[t % 2]
                tb = tm[t % 2]
                h.dma("sp", xb[:], src.t[t * 128:(t + 1) * 128, :], [rsrc[t]], [xb], xb)
                for n2 in range(2):
                    pb = po[(t % 2) * 2 + n2]
                    nc_ = slice(n2 * 512, (n2 + 1) * 512)
                    h.mm([(pb[:], yb[:, kk, tc], wout[:, kk, nc_], kk == 0, kk == 7) for kk in range(8)], [yb, wout], [pb])
                    h.tt("dve", tb[:, nc_], pb[:], G[:, nc_], ALU.mult, [pb, G], [tb])
                h.tt("pool", tb[:], tb[:], xb[:], ALU.add, [tb, xb], [tb])
                h.dma("sp", dst.t[t * 128:(t + 1) * 128, :], tb[:], [tb], [rdst[t]], tb)
        P.end_phase()


def gate_consts(k, es, l, tag):
    P, h, inp = k.P, k.h, k.inp
    b12 = P.sb(es, tag + "b12", [128, 12], F32)
    na12 = P.sb(es, tag + "na12", [128, 12], F32)
    k.load_row(b12, inp["bias12"].t[l:l + 1, :])
    k.load_row(na12, inp["alog12"].t[l:l + 1, :])
    h.act(na12[:], na12[:], AF.Exp, [na12], [na12])
    h.ts("dve", na12[:], na12[:], -1.0, None, ALU.mult, None, [na12], [na12])
    return b12, na12


def gate_tile(k, sp_fn, pj_ap, pj_buf, b12, na12, masks, sp12, gda, cs12, cl12, pA):
    h = k.h
    sp_fn(pj_ap, pj_buf, b12, sp12)
    h.tt("dve", gda[:], sp12[:], na12[:], ALU.mult, [sp12, na12], [gda])
    h.mm([(pA[:, 0:12], masks["U"][:], gda[:], True, True)], [masks["U"], gda], [pA])
    h.cp("dve", cs12[:], pA[:, 0:12], [pA], [cs12])
    h.mm([(pA[:, 16:28], masks["sel"][:], cs12[:], True, True)], [masks["sel"], cs12], [pA])
    h.cp("dve", cl12[:], pA[:, 16:28], [pA], [cl12])


def ssd_phase(k, l):
    P, T, h, inp = k.P, k.T, k.h, k.inp
    NMT = T // 512
    MT = 512
    identf, identb = k.identf, k.identb
    with contextlib.ExitStack() as es:
        masks = make_masks(h, P, es)
        b12, na12 = gate_consts(k, es, l, "sd")
        sp_fn = softplus12(h, P, es, "sd")
        dsk = P.sb(es, "sd_dsk", [128, 6], F32)
        k.load_row(dsk, inp["ssd_d"].t[l:l + 1, :])
        nws = P.sb(es, "sd_nws", [128, 384], F32)
        k.load_row(nws, inp["ssd_norm"].t[l:l + 1, :])
        stT = P.sb(es, "sd_stT", [128, 384], F32)
        stTb = P.sb(es, "sd_stTb", [128, 384], BF16)
        h.memset("pool", stT[:], 0.0, [stT])
        h.memset("pool", stTb[:], 0.0, [stTb])
        xsT = [P.sb(es, f"sd_xsT{i}", [128, 3, MT], F32) for i in range(2)]
        BTt = [P.sb(es, f"sd_BT{i}", [128, 2, MT], BF16) for i in range(2)]
        CTt = [P.sb(es, f"sd_CT{i}", [128, 2, MT], BF16) for i in range(2)]
        pj = [P.sb(es, f"sd_pj{i}", [128, 4, 786], F32) for i in range(2)]
        yo = [P.sb(es, f"sd_yo{i}", [128, 3, MT], BF16) for i in range(2)]
        sp12 = P.sb(es, "sd_sp12", [128, 12], F32)
        gda = P.sb(es, "sd_gda", [128, 12], F32)
        cs12 = P.sb(es, "sd_cs12", [128, 12], F32)
        cl12 = P.sb(es, "sd_cl12", [128, 12], F32)
        t6 = P.sb(es, "sd_t6", [128, 6], F32)
        din = P.sb(es, "sd_din", [128, 6], F32)
        eacs = P.sb(es, "sd_eacs", [128, 6], F32)
        cd = P.sb(es, "sd_cd", [128, 6], F32)
        dg = P.sb(es, "sd_dg", [128, 6, 128], F32)
        arg = P.sb(es, "sd_arg", [128, 6, 128], F32)
        seg = P.sb(es, "sd_seg", [128, 6, 128], F32)
        WTb = P.sb(es, "sd_WTb", [128, 6, 128], BF16)
        xs_tm = P.sb(es, "sd_xstm", [128, 384], F32)
        xdtf = P.sb(es, "sd_xdtf", [128, 384], F32)
        xdtb = P.sb(es, "sd_xdtb", [128, 384], BF16)
        xddb = P.sb(es, "sd_xddb", [128, 384], BF16)
        Btm = P.sb(es, "sd_Btm", [128, 256], BF16)
        t1 = P.sb(es, "sd_t1", [128, 384], F32)
        t2 = P.sb(es, "sd_t2", [128, 384], F32)
        y = P.sb(es, "sd_y", [128, 384], F32)
        zs = P.sb(es, "sd_zs", [128, 384], F32)
        junk = P.sb(es, "sd_junk", [128, 192], F32)
        yb = P.sb(es, "sd_yb", [128, 384], BF16)
        ss2 = P.sb(es, "sd_ss2", [128, 2], F32)
        rs2 = P.sb(es, "sd_rs2", [128, 2], F32)
        W0 = P.ps(es, "sd_W0", [128, 2, 512], F32)
        S0 = P.ps(es, "sd_S0", [128, 512], F32)
        S1 = P.ps(es, "sd_S1", [128, 512], F32)
        S2 = P.ps(es, "sd_S2", [128, 512], F32)
        PB = P.ps(es, "sd_PB", [128, 512], BF16)
        pA = P.ps(es, "sd_pA", [128, 32], F32)
        for mt in range(NMT):
            cols = slice(mt * MT, (mt + 1) * MT)
            i2 = mt % 2
            rp = [k.r_pre[mt]]
            h.dma("sp", xsT[i2][:], k.xs_d.t[:, cols].rearrange("(j p) t -> p j t", p=128), rp, [xsT[i2]], xsT[i2])
            h.dma("sp", BTt[i2][:], k.B_d.t[:, cols].rearrange("(j p) t -> p j t", p=128), rp, [BTt[i2]], BTt[i2])
            h.dma("sp", CTt[i2][:], k.C_d.t[:, cols].rearrange("(j p) t -> p j t", p=128), rp, [CTt[i2]], CTt[i2])
            h.dma("sp", pj[i2][:], k.pt_d.t[mt * MT:(mt + 1) * MT, :].rearrange("(a p) n -> p a n", p=128), rp, [pj[i2]], pj[i2])
            xs_, B_, C_, pj_, yo_ = xsT[i2], BTt[i2], CTt[i2], pj[i2], yo[i2]
            for ti in range(4):
                tc = slice(ti * 128, (ti + 1) * 128)
                gate_tile(k, sp_fn, pj_[:, ti, 768:780], pj_, b12, na12, masks, sp12, gda, cs12, cl12, pA)
                acs = cs12[:, 6:12]
                h.tt("pool", dg[:], b3(identf[:], 6, 128), s3(acs, 6, 128), ALU.mult, [identf, cs12], [dg])
                h.mm([(W0[:, 0, 0:384], masks["ones"][:], dg[:, 0:3, :].rearrange("p a l -> p (a l)"), True, True),
                      (W0[:, 1, 0:384], masks["ones"][:], dg[:, 3:6, :].rearrange("p a l -> p (a l)"), True, True)],
                     [masks["ones"], dg], [W0])
                h.tt("dve", v4(arg[:]), w4(W0), s4(acs), ALU.subtract, [W0, cs12], [arg])
                h.ts("pool", arg[:], arg[:], 0.0, None, ALU.min, None, [arg], [arg])
                h.act(seg[:], arg[:], AF.Exp, [arg], [seg])
                h.tt("pool", seg[:], seg[:], b3(masks["U"][:], 6, 128), ALU.mult, [seg, masks["U"]], [seg])
                h.mm([(S0[:, g * 128:(g + 1) * 128], B_[:, g, tc], C_[:, g, tc], True, True) for g in range(2)], [B_, C_], [S0])
                h.tt("dve", v4(WTb[:]), v4(seg[:]),
                     S0[:, 0:256].rearrange("p (g l) -> p g l", g=2).unsqueeze(2).to_broadcast([128, 2, 3, 128]),
                     ALU.mult, [seg, S0], [WTb])
                h.tr([(S1[:, j * 128:(j + 1) * 128], xs_[:, j, tc]) for j in range(3)], identf[:], [xs_, identf], [S1])
                h.cp("act", xs_tm[:], S1[:, 0:384], [S1], [xs_tm])
                h.tr([(PB[:, g * 128:(g + 1) * 128], B_[:, g, tc]) for g in range(2)], identb[:], [B_, identb], [PB])
                h.cp("act", Btm[:], PB[:, 0:256], [PB], [Btm])
                x3 = lambda ap: ap.rearrange("p (h d) -> p h d", h=6)
                h.tt("dve", x3(xdtf[:]), x3(xs_tm[:]), s3(sp12[:, 6:12], 6, 64), ALU.mult, [xs_tm, sp12], [xdtf])
                h.cp("pool", xdtb[:], xdtf[:], [xdtf], [xdtb])
                h.tt("dve", t6[:], cl12[:, 6:12], acs, ALU.subtract, [cl12, cs12], [t6])
                h.act(din[:], t6[:], AF.Exp, [t6], [din])
                h.tt("pool", x3(xddb[:]), x3(xdtf[:]), s3(din[:], 6, 64), ALU.mult, [xdtf, din], [xddb])
                h.mm([(S2[:, hd * 64:(hd + 1) * 64], WTb[:, hd, :], xdtb[:, hd * 64:(hd + 1) * 64], True, True) for hd in range(6)],
                     [WTb, xdtb], [S2])
                h.mm([(S0[:, g * 192:(g + 1) * 192], C_[:, g, tc], stTb[:, g * 192:(g + 1) * 192], True, True) for g in range(2)],
                     [C_, stTb], [S0])
                h.act(eacs[:], acs, AF.Exp, [cs12], [eacs])
                h.tt("dve", x3(t2[:]), x3(S0[:, 0:384]), s3(eacs[:], 6, 64), ALU.mult, [S0, eacs], [t2])
                h.tt("pool", x3(t1[:]), x3(xs_tm[:]), s3(dsk[:], 6, 64), ALU.mult, [xs_tm, dsk], [t1])
                h.tt("pool", t2[:], t2[:], t1[:], ALU.add, [t2, t1], [t2])
                h.tt("dve", y[:], S2[:, 0:384], t2[:], ALU.add, [S2, t2], [y])
                h.act(zs[:], pj_[:, ti, 384:768], AF.Silu, [pj_], [zs])
                h.tt("pool", y[:], y[:], zs[:], ALU.mult, [y, zs], [y])
                for g in range(2):
                    h.act(junk[:], y[:, g * 192:(g + 1) * 192], AF.Square, [y], [junk, ss2], accum=ss2[:, g:g + 1])
                h.ts("dve", rs2[:], ss2[:], 1.0 / 192, EPS, ALU.mult, ALU.add, [ss2], [rs2])
                h.act(rs2[:], rs2[:], AF.Sqrt, [rs2], [rs2])
                h.recip(rs2[:], rs2[:], [rs2], [rs2])
                y3 = lambda ap: ap.rearrange("p (g c) -> p g c", g=2)
                h.tt("dve", y3(y[:]), y3(y[:]), s3(rs2[:], 2, 192), ALU.mult, [y, rs2], [y])
                h.tt("pool", yb[:], y[:], nws[:], ALU.mult, [y, nws], [yb])
                h.tr([(PB[:, j * 128:(j + 1) * 128], yb[:, j * 128:(j + 1) * 128]) for j in range(3)], identb[:], [yb, identb], [PB])
                h.cp("act", yo_[:, :, tc], PB[:, 0:384].rearrange("p (j t) -> p j t", j=3), [PB], [yo_])
                h.mm([(S1[:, g * 192:(g + 1) * 192], Btm[:, g * 128:(g + 1) * 128], xddb[:, g * 192:(g + 1) * 192], True, True) for g in range(2)],
                     [Btm, xddb], [S1])
                h.act(cd[:], cl12[:, 6:12], AF.Exp, [cl12], [cd])
                h.tt("pool", x3(stT[:]), x3(stT[:]), s3(cd[:], 6, 64), ALU.mult, [stT, cd], [stT])
                h.tt("dve", stT[:], stT[:], S1[:, 0:384], ALU.add, [stT, S1], [stT])
                h.cp("act", stTb[:], stT[:], [stT], [stTb])
            h.dma("sp", k.y_d.t[640:1024, cols].rearrange("(j p) t -> p j t", p=128), yo_[:], [yo_], [k.r_y[2][mt]], yo_)
        P.end_phase()


C1_2PI = 6.28125
C2_2PI = 2.0 * math.pi - 6.28125


def sincos(h, eng, x, out, b, ki, c, negpi, R, W, bufs, is_cos):
    bb, kb, cb_ = bufs
    off = 16.5 + (0.25 if is_cos else 0.0)
    add = 33.0 * math.pi + (0.5 * math.pi if is_cos else 0.0)
    h.ts(eng, b, x, 1.0 / (2.0 * math.pi), off, ALU.mult, ALU.add, R, [bb])
    h.cp(eng, ki, b, [bb], [kb])
    h.cp(eng, c, ki, [kb], [cb_])
    h.stt(eng, b, c, -C1_2PI, x, ALU.mult, ALU.add, R + [cb_], [bb])
    h.stt(eng, b, c, -C2_2PI, b, ALU.mult, ALU.add, [cb_, bb], [bb])
    h.ts(eng, b, b, add, None, ALU.add, None, [bb], [bb])
    h.ts(eng, c, b, 2.0 * math.pi, -2.0 * math.pi, ALU.is_gt, ALU.mult, [bb], [cb_])
    h.tt(eng, b, b, c, ALU.add, [bb, cb_], [bb])
    h.ts(eng, c, b, 0.0, 2.0 * math.pi, ALU.is_lt, ALU.mult, [bb], [cb_])
    h.tt(eng, b, b, c, ALU.add, [bb, cb_], [bb])
    h.act(out, b, AF.Sin, [bb, negpi], W, bias=negpi[:, 0:1])


def s5_phase(k, l):
    P, T, h, inp = k.P, k.T, k.h, k.inp
    NMT = T // 512
    SEG = 512
    with contextlib.ExitStack() as es:
        I32 = mybir.dt.int32
        are = P.sb(es, "s5are", [128, 8], F32)
        aim = P.sb(es, "s5aim", [128, 8], F32)
        stp = P.sb(es, "s5stp", [128, 8], F32)
        h.dma("sp", are[:], inp["s5_are"].t[l], [], [are], are)
        h.dma("sp", aim[:], inp["s5_aim"].t[l], [], [aim], aim)
        h.dma("sp", stp[:], inp["s5_ldt"].t[l], [], [stp], stp)
        dsk = P.sb(es, "s5dsk", [128, 2], F32)
        nw5 = P.sb(es, "s5nw", [128, 2], F32)
        h.dma("sp", dsk[:], inp["s5_dcol"].t[l], [], [dsk], dsk)
        h.dma("sp", nw5[:], inp["s5_ncol"].t[l], [], [nw5], nw5)
        bTre = P.sb(es, "s5bTre", [128, 8, 128], BF16)
        bTim = P.sb(es, "s5bTim", [128, 8, 128], BF16)
        cTre = P.sb(es, "s5cTre", [128, 8, 128], BF16)
        cTim = P.sb(es, "s5cTim", [128, 8, 128], BF16)
        for dstb, nm in ((bTre, "s5_bT_re"), (bTim, "s5_bT_im"), (cTre, "s5_cT_re"), (cTim, "s5_cT_im")):
            h.dma("pool", dstb[:], inp[nm].t[l].rearrange("s r m -> r s m"), [], [dstb], dstb)
        wglu = P.sb(es, "s5wglu", [128, 2, 256], BF16)
        h.dma("pool", wglu[:], inp["s5_w_glu"].t[l].rearrange("(k p) n -> p k n", p=128), [], [wglu], wglu)
        negpi = P.sb(es, "s5negpi", [128, 1], F32)
        h.memset("pool", negpi[:], -math.pi, [negpi])
        epsb = P.sb(es, "s5eps", [128, 1], F32)
        h.memset("pool", epsb[:], EPS, [epsb])
        onesf = P.sb(es, "s5ones", [128, 128], F32)
        h.memset("pool", onesf[:], 1.0, [onesf])
        jrow = P.sb(es, "s5jrow", [128, SEG], F32)
        P.op("pool", lambda e: e.iota(jrow[:], pattern=[[1, SEG]], base=0, channel_multiplier=0,
                                      allow_small_or_imprecise_dtypes=True), writes=[jrow])
        th = P.sb(es, "s5th", [128, 8], F32)
        rr = P.sb(es, "s5r", [128, 8], F32)
        sth = P.sb(es, "s5sth", [128, 8], F32)
        cth = P.sb(es, "s5cth", [128, 8], F32)
        thS = P.sb(es, "s5thS", [128, 8], F32)
        sS = P.sb(es, "s5sS", [128, 8], F32)
        cS = P.sb(es, "s5cS", [128, 8], F32)
        nsS = P.sb(es, "s5nsS", [128, 8], F32)
        cr = P.sb(es, "s5cr", [128, 8], F32)
        ci = P.sb(es, "s5ci", [128, 8], F32)
        ncr = P.sb(es, "s5ncr", [128, 8], F32)
        q1 = P.sb(es, "s5q1", [128, 8], F32)
        q2 = P.sb(es, "s5q2", [128, 8], F32)
        q3 = P.sb(es, "s5q3", [128, 8], F32)
        sb8 = P.sb(es, "s5sb8", [128, 8], F32)
        si8 = P.sb(es, "s5si8", [128, 8], I32)
        sc8 = P.sb(es, "s5sc8", [128, 8], F32)
        h.act(stp[:], stp[:], AF.Exp, [stp], [stp])
        h.tt("dve", th[:], aim[:], stp[:], ALU.mult, [aim, stp], [th])
        h.tt("dve", rr[:], are[:], stp[:], ALU.mult, [are, stp], [rr])
        h.act(rr[:], rr[:], AF.Exp, [rr], [rr])
        sm = (sb8, si8, sc8)
        sincos(h, "dve", th[:], sth[:], sb8[:], si8[:], sc8[:], negpi, [th], [sth], sm, False)
        sincos(h, "dve", th[:], cth[:], sb8[:], si8[:], sc8[:], negpi, [th], [cth], sm, True)
        h.ts("dve", thS[:], th[:], float(SEG), None, ALU.mult, None, [th], [thS])
        sincos(h, "dve", thS[:], sS[:], sb8[:], si8[:], sc8[:], negpi, [thS], [sS], sm, False)
        sincos(h, "dve", thS[:], cS[:], sb8[:], si8[:], sc8[:], negpi, [thS], [cS], sm, True)
        h.ts("dve", nsS[:], sS[:], -1.0, None, ALU.mult, None, [sS], [nsS])
        h.tt("dve", q1[:], rr[:], cth[:], ALU.mult, [rr, cth], [q1])
        h.ts("dve", q1[:], q1[:], -1.0, None, ALU.add, None, [q1], [q1])
        h.tt("dve", q2[:], rr[:], sth[:], ALU.mult, [rr, sth], [q2])
        h.tt("dve", q3[:], are[:], are[:], ALU.mult, [are], [q3])
        h.tt("dve", sc8[:], aim[:], aim[:], ALU.mult, [aim], [sc8])
        h.tt("dve", q3[:], q3[:], sc8[:], ALU.add, [q3, sc8], [q3])
        h.recip(q3[:], q3[:], [q3], [q3])
        h.tt("dve", cr[:], q1[:], are[:], ALU.mult, [q1, are], [cr])
        h.tt("dve", sc8[:], q2[:], aim[:], ALU.mult, [q2, aim], [sc8])
        h.tt("dve", cr[:], cr[:], sc8[:], ALU.add, [cr, sc8], [cr])
        h.tt("dve", cr[:], cr[:], q3[:], ALU.mult, [cr, q3], [cr])
        h.tt("dve", ci[:], q2[:], are[:], ALU.mult, [q2, are], [ci])
        h.tt("dve", sc8[:], q1[:], aim[:], ALU.mult, [q1, aim], [sc8])
        h.tt("dve", ci[:], ci[:], sc8[:], ALU.subtract, [ci, sc8], [ci])
        h.tt("dve", ci[:], ci[:], q3[:], ALU.mult, [ci, q3], [ci])
        h.ts("dve", ncr[:], cr[:], -1.0, None, ALU.mult, None, [cr], [ncr])
        cosT = P.sb(es, "s5cosT", [128, 8, SEG], F32)
        sinT = P.sb(es, "s5sinT", [128, 8, SEG], F32)
        tabr = P.sb(es, "s5tabr", [128, 8, SEG], F32)
        tabi = P.sb(es, "s5tabi", [128, 8, SEG], F32)
        ang = [P.sb(es, f"s5ang{i}", [128, SEG], F32) for i in range(2)]
        tb = [P.sb(es, f"s5tb{i}", [128, SEG], F32) for i in range(2)]
        tki = [P.sb(es, f"s5tki{i}", [128, SEG], I32) for i in range(2)]
        tcc = [P.sb(es, f"s5tc{i}", [128, SEG], F32) for i in range(2)]
        for sc in range(8):
            i = sc % 2
            eng = "dve" if i == 0 else "pool"
            h.ts(eng, ang[i][:], jrow[:], th[:, sc:sc + 1], None, ALU.mult, None, [jrow, th], [ang[i]])
            bufs = (tb[i], tki[i], tcc[i])
            sincos(h, eng, ang[i][:], sinT[:, sc, :], tb[i][:], tki[i][:], tcc[i][:], negpi, [ang[i]], [sinT], bufs, False)
            sincos(h, eng, ang[i][:], cosT[:, sc, :], tb[i][:], tki[i][:], tcc[i][:], negpi, [ang[i]], [cosT], bufs, True)
            h.ts(eng, tabr[:, sc, :], cosT[:, sc, :], cr[:, sc:sc + 1], None, ALU.mult, None, [cosT, cr], [tabr])
            h.stt(eng, tabr[:, sc, :], sinT[:, sc, :], ci[:, sc:sc + 1], tabr[:, sc, :], ALU.mult, ALU.add, [sinT, ci, tabr], [tabr])
            h.ts(eng, tabi[:, sc, :], cosT[:, sc, :], ci[:, sc:sc + 1], None, ALU.mult, None, [cosT, ci], [tabi])
            h.stt(eng, tabi[:, sc, :], sinT[:, sc, :], ncr[:, sc:sc + 1], tabi[:, sc, :], ALU.mult, ALU.add, [sinT, ncr, tabi], [tabi])
        ire = P.sb(es, "s5ire", [128, 8], F32)
        iim = P.sb(es, "s5iim", [128, 8], F32)
        gre_e = P.sb(es, "s5gree", [128, 8], F32)
        gim_e = P.sb(es, "s5gime", [128, 8], F32)
        h.memset("pool", ire[:], 0.0, [ire])
        h.memset("pool", iim[:], 0.0, [iim])
        uTf = [P.sb(es, f"s5uTf{i}", [128, 2, SEG], F32) for i in range(2)]
        uTb = [P.sb(es, f"s5uTb{i}", [128, 2, SEG], BF16) for i in range(2)]
        m1 = [P.sb(es, f"s5m1{i}", [128, SEG], F32) for i in range(2)]
        m2 = [P.sb(es, f"s5m2{i}", [128, SEG], F32) for i in range(2)]
        m3 = [P.sb(es, f"s5m3{i}", [128, SEG], F32) for i in range(2)]
        m4 = [P.sb(es, f"s5m4{i}", [128, SEG], F32) for i in range(2)]
        dre = [P.sb(es, f"s5dre{i}", [128, SEG], F32) for i in range(2)]
        dim = [P.sb(es, f"s5dim{i}", [128, SEG], F32) for i in range(2)]
        gre = [P.sb(es, f"s5gre{i}", [128, SEG], F32) for i in range(2)]
        gim = [P.sb(es, f"s5gim{i}", [128, SEG], F32) for i in range(2)]
        hre = [P.sb(es, f"s5hre{i}", [128, SEG], BF16) for i in range(2)]
        him = [P.sb(es, f"s5him{i}", [128, SEG], BF16) for i in range(2)]
        y1 = P.sb(es, "s5y1", [128, 2, SEG], F32)
        yt = P.sb(es, "s5yt", [128, 2, SEG], F32)
        yg = P.sb(es, "s5yg", [128, 2, SEG], F32)
        ygb = P.sb(es, "s5ygb", [128, 2, SEG], BF16)
        sg = P.sb(es, "s5sg", [128, 2, SEG], F32)
        y2 = P.sb(es, "s5y2", [128, 2, SEG], F32)
        rstd = P.sb(es, "s5rstd", [128, SEG], F32)
        yo = [P.sb(es, f"s5yo{i}", [128, 2, SEG], BF16) for i in range(2)]
        Pre = [P.ps(es, f"s5Pre{i}", [128, SEG], F32) for i in range(2)]
        Pim = [P.ps(es, f"s5Pim{i}", [128, SEG], F32) for i in range(2)]
        Y = [P.ps(es, f"s5Y{i}", [128, SEG], F32) for i in range(2)]
        Pg = P.ps(es, "s5Pg", [128, SEG], F32)
        Pt = P.ps(es, "s5Pt", [128, SEG], F32)
        GK = 2.0 * math.sqrt(2.0 / math.pi)
        for mt in range(NMT):
            cols = slice(mt * SEG, (mt + 1) * SEG)
            i2 = mt % 2
            uf, ub, yo_ = uTf[i2], uTb[i2], yo[i2]
            h.dma("sp", uf[:], k.u_d.t[:, cols].rearrange("(j p) t -> p j t", p=128), [k.r_pre[mt]], [uf], uf)
            h.cp("pool", ub[:], uf[:], [uf], [ub])
            for sc in range(8):
                i = sc % 2
                cc = sc // 4
                h.mm([(Pre[i][:], bTre[:, sc, :], ub[:, cc, :], True, True)], [bTre, ub], [Pre[i]])
                h.mm([(Pim[i][:], bTim[:, sc, :], ub[:, cc, :], True, True)], [bTim, ub], [Pim[i]])
                h.tt("dve", m1[i][:], Pre[i][:], tabr[:, sc, :], ALU.mult, [Pre[i], tabr], [m1[i]])
                h.tt("dve", m2[i][:], Pim[i][:], tabi[:, sc, :], ALU.mult, [Pim[i], tabi], [m2[i]])
                h.tt("pool", dre[i][:], m1[i][:], m2[i][:], ALU.subtract, [m1[i], m2[i]], [dre[i]])
                h.tt("dve", m3[i][:], Pre[i][:], tabi[:, sc, :], ALU.mult, [Pre[i], tabi], [m3[i]])
                h.tt("dve", m4[i][:], Pim[i][:], tabr[:, sc, :], ALU.mult, [Pim[i], tabr], [m4[i]])
                h.tt("pool", dim[i][:], m3[i][:], m4[i][:], ALU.add, [m3[i], m4[i]], [dim[i]])
                for (go, di, ini) in ((gre[i], dre[i], ire), (gim[i], dim[i], iim)):
                    P.op("dve", (lambda go, di, ini, sc: (lambda e: e.tensor_tensor_scan(
                        out=go[:], data0=rr[:, sc:sc + 1].to_broadcast([128, SEG]), data1=di[:],
                        initial=ini[:, sc:sc + 1], op0=ALU.mult, op1=ALU.add)))(go, di, ini, sc),
                        reads=[rr, di, ini], writes=[go])
                h.cp("act", gre_e[:, sc:sc + 1], gre[i][:, SEG - 1:SEG], [gre[i]], [gre_e])
                h.cp("act", gim_e[:, sc:sc + 1], gim[i][:, SEG - 1:SEG], [gim[i]], [gim_e])
                h.tt("pool", m1[i][:], gre[i][:], cosT[:, sc, :], ALU.mult, [gre[i], cosT], [m1[i]])
                h.tt("pool", m2[i][:], gim[i][:], sinT[:, sc, :], ALU.mult, [gim[i], sinT], [m2[i]])
                h.tt("pool", hre[i][:], m1[i][:], m2[i][:], ALU.subtract, [m1[i], m2[i]], [hre[i]])
                h.tt("pool", m3[i][:], gre[i][:], sinT[:, sc, :], ALU.mult, [gre[i], sinT], [m3[i]])
                h.tt("pool", m4[i][:], gim[i][:], cosT[:, sc, :], ALU.mult, [gim[i], cosT], [m4[i]])
                h.stt("pool", him[i][:], m3[i][:], -1.0, m4[i][:], ALU.mult, ALU.subtract, [m3[i], m4[i]], [him[i]])
                h.mm([(Y[cc][:], cTre[:, sc, :], hre[i][:], sc % 4 == 0, False),
                      (Y[cc][:], cTim[:, sc, :], him[i][:], False, sc % 4 == 3)], [cTre, cTim, hre[i], him[i]], [Y[cc]])
                if sc % 4 == 3:
                    h.stt("dve", y1[:, cc, :], uf[:, cc, :], dsk[:, cc:cc + 1], Y[cc][:], ALU.mult, ALU.add, [uf, dsk, Y[cc]], [y1])
                    h.tt("pool", yt[:, cc, :], y1[:, cc, :], y1[:, cc, :], ALU.mult, [y1], [yt])
                    h.ts("pool", yt[:, cc, :], yt[:, cc, :], 0.044715, 1.0, ALU.mult, ALU.add, [yt], [yt])
                    h.tt("pool", yt[:, cc, :], yt[:, cc, :], y1[:, cc, :], ALU.mult, [yt, y1], [yt])
                    h.act(yt[:, cc, :], yt[:, cc, :], AF.Sigmoid, [yt], [yt], scale=GK)
                    h.tt("pool", yg[:, cc, :], y1[:, cc, :], yt[:, cc, :], ALU.mult, [y1, yt], [yg])
                    h.cp("pool", ygb[:, cc, :], yg[:, cc, :], [yg], [ygb])
            h.tt("dve", q1[:], gre_e[:], cS[:], ALU.mult, [gre_e, cS], [q1])
            h.tt("dve", q2[:], gim_e[:], nsS[:], ALU.mult, [gim_e, nsS], [q2])
            h.tt("dve", ire[:], q1[:], q2[:], ALU.add, [q1, q2], [ire])
            h.tt("dve", q1[:], gre_e[:], sS[:], ALU.mult, [gre_e, sS], [q1])
            h.tt("dve", q2[:], gim_e[:], cS[:], ALU.mult, [gim_e, cS], [q2])
            h.tt("dve", iim[:], q1[:], q2[:], ALU.add, [q1, q2], [iim])
            for oc in range(2):
                h.mm([(Pg[:], wglu[:, kc, oc * 128:(oc + 1) * 128], ygb[:, kc, :], kc == 0, kc == 1) for kc in range(2)], [wglu, ygb], [Pg])
                h.act(sg[:, oc, :], Pg[:], AF.Sigmoid, [Pg], [sg])
                h.tt("pool", y2[:, oc, :], yg[:, oc, :], sg[:, oc, :], ALU.mult, [yg, sg], [y2])
                h.tt("pool", sg[:, oc, :], y2[:, oc, :], y2[:, oc, :], ALU.mult, [y2], [sg])
            h.mm([(Pt[:], onesf[:], sg[:, oc, :], oc == 0, oc == 1) for oc in range(2)], [onesf, sg], [Pt])
            h.act(rstd[:], Pt[:], AF.Sqrt, [Pt, epsb], [rstd], bias=epsb[:, 0:1], scale=1.0 / 256)
            h.recip(rstd[:], rstd[:], [rstd], [rstd])
            for oc in range(2):
                h.stt("dve", yo_[:, oc, :], y2[:, oc, :], nw5[:, oc:oc + 1], rstd[:], ALU.mult, ALU.mult, [y2, nw5, rstd], [yo_])
            h.dma("sp", k.y_d.t[0:256, cols].rearrange("(j p) t -> p j t", p=128), yo_[:], [yo_], [k.r_y[0][mt]], yo_)
        P.end_phase()


def gdn_phase(k, l):
    P, T, h, inp = k.P, k.T, k.h, k.inp
    NMT = T // 512
    MT = 512
    identf, identb = k.identf, k.identb
    with contextlib.ExitStack() as es:
        masks = make_masks(h, P, es)
        b12, na12 = gate_consts(k, es, l, "gd")
        sp_fn = softplus12(h, P, es, "gd")
        gnw = P.sb(es, "gd_gnw", [128, 64], F32)
        k.load_row(gnw, inp["gdn_norm"].t[l:l + 1, :])
        Sf = P.sb(es, "gd_Sf", [128, 3, 128], F32)
        Sb = P.sb(es, "gd_Sb", [128, 3, 128], BF16)
        h.memset("pool", Sf[:], 0.0, [Sf])
        h.memset("pool", Sb[:], 0.0, [Sb])
        qnT = [P.sb(es, f"gd_qn{i}", [128, 3, MT], BF16) for i in range(2)]
        knT = [P.sb(es, f"gd_kn{i}", [128, 3, MT], BF16) for i in range(2)]
        vT = [P.sb(es, f"gd_vT{i}", [128, 3, MT], F32) for i in range(2)]
        pj = [P.sb(es, f"gd_pj{i}", [128, 4, 786], F32) for i in range(2)]
        yo = [P.sb(es, f"gd_yo{i}", [128, 3, MT], BF16) for i in range(2)]
        sp12 = P.sb(es, "gd_sp12", [128, 12], F32)
        gda = P.sb(es, "gd_gda", [128, 12], F32)
        cs12 = P.sb(es, "gd_cs12", [128, 12], F32)
        cl12 = P.sb(es, "gd_cl12", [128, 12], F32)
        beta = P.sb(es, "gd_beta", [128, 6], F32)
        nbeta = P.sb(es, "gd_nbeta", [128, 6], F32)
        egc = P.sb(es, "gd_egc", [128, 6], F32)
        t6 = P.sb(es, "gd_t6", [128, 6], F32)
        dkk = P.sb(es, "gd_dkk", [128, 6], F32)
        gtot = P.sb(es, "gd_gtot", [128, 6], F32)
        gtc = P.sb(es, "gd_gtc", [128, 3], F32)
        dg = P.sb(es, "gd_dg", [128, 6, 128], F32)
        arg = P.sb(es, "gd_arg", [128, 6, 128], F32)
        E = P.sb(es, "gd_E", [128, 6, 128], F32)
        EU = P.sb(es, "gd_EU", [128, 6, 128], F32)
        ELn = P.sb(es, "gd_ELn", [128, 6, 128], F32)
        attnT = P.sb(es, "gd_attnT", [128, 6, 128], BF16)
        Pm = [P.sb(es, f"gd_Pm{i}", [128, 6, 128], BF16) for i in range(2)]
        Qm = [P.sb(es, f"gd_Qm{i}", [128, 6, 128], BF16) for i in range(2)]
        Xf = P.sb(es, "gd_Xf", [128, 6, 128], F32)
        Xb = P.sb(es, "gd_Xb", [128, 6, 128], BF16)
        kdec = P.sb(es, "gd_kdec", [128, 384], BF16)
        v_tm = P.sb(es, "gd_vtm", [128, 384], F32)
        rr_ = P.sb(es, "gd_rr", [128, 384], F32)
        rb = P.sb(es, "gd_rb", [128, 384], BF16)
        vnb = P.sb(es, "gd_vnb", [128, 384], BF16)
        oa = P.sb(es, "gd_oa", [128, 384], F32)
        o = P.sb(es, "gd_o", [128, 384], F32)
        sq = P.sb(es, "gd_sq", [128, 384], F32)
        ss6 = P.sb(es, "gd_ss6", [128, 6], F32)
        rs6 = P.sb(es, "gd_rs6", [128, 6], F32)
        zg = P.sb(es, "gd_zg", [128, 384], F32)
        yb = P.sb(es, "gd_yb", [128, 384], BF16)
        tmpS = P.sb(es, "gd_tmpS", [128, 3, 128], F32)
        W0 = P.ps(es, "gd_W0", [128, 2, 512], F32)
        W1 = P.ps(es, "gd_W1", [128, 2, 512], F32)
        W2 = P.ps(es, "gd_W2", [128, 2, 512], F32)
        PB = P.ps(es, "gd_PB", [128, 1024], BF16)
        pA = P.ps(es, "gd_pA", [128, 32], F32)
        x3 = lambda ap: ap.rearrange("p (h d) -> p h d", h=6)
        kz = [P.sb(es, f"gd_kz{i}", [128, 3, MT], BF16) for i in range(2)]
        Tb = P.sb(es, "gd_Tb", [128, 6, 128], BF16)
        M1b = P.sb(es, "gd_M1b", [128, 6, 128], BF16)
        M1pb = P.sb(es, "gd_M1pb", [128, 6, 128], BF16)
        cmask = P.sb(es, "gd_cmask", [128, 14, 128], BF16)
        h.dma("pool", cmask[:], inp["gdn_cmask"].t.rearrange("m p j -> p m j"), [], [cmask], cmask)
        rmask = P.sb(es, "gd_rmask", [128, 2], F32)
        h.memset("pool", rmask[:], 0.0, [rmask])
        h.memset("pool", rmask[0:64, 0:1], 1.0, [rmask])
        h.memset("pool", rmask[64:128, 1:2], 1.0, [rmask])

        def headmm(Wps, lh, rh, R):
            w = w4(Wps)
            h.mm([(w[:, hd // 3, hd % 3, :], lh[:, hd, :], rh[:, hd, :], True, True) for hd in range(6)], R, [Wps])

        stop = k.flags.get("gdn_stop", 99)
        for mt in range(NMT):
            cols = slice(mt * MT, (mt + 1) * MT)
            i2 = mt % 2
            rp = [k.r_pre[mt]]
            h.dma("sp", qnT[i2][:], k.qn_d.t[:, cols].rearrange("(j p) t -> p j t", p=128), rp, [qnT[i2]], qnT[i2])
            h.dma("sp", knT[i2][:], k.kn_d.t[:, cols].rearrange("(j p) t -> p j t", p=128), rp, [knT[i2]], knT[i2])
            h.dma("sp", vT[i2][:], k.v_d.t[:, cols].rearrange("(j p) t -> p j t", p=128), rp, [vT[i2]], vT[i2])
            h.dma("sp", pj[i2][:], k.pt_d.t[mt * MT:(mt + 1) * MT, :].rearrange("(a p) n -> p a n", p=128), rp, [pj[i2]], pj[i2])
            qn_, kn_, v_, pj_, yo_ = qnT[i2], knT[i2], vT[i2], pj[i2], yo[i2]
            for s_ in range(2):
                h.ts("dve", kz[s_][:], kn_[:], rmask[:, s_:s_ + 1], None, ALU.mult, None, [kn_, rmask], [kz[s_]])
            for ti in range(4):
                tc = slice(ti * 128, (ti + 1) * 128)
                gate_tile(k, sp_fn, pj_[:, ti, 768:780], pj_, b12, na12, masks, sp12, gda, cs12, cl12, pA)
                gc = cs12[:, 0:6]
                h.act(beta[:], pj_[:, ti, 780:786], AF.Sigmoid, [pj_], [beta])
                h.ts("dve", nbeta[:], beta[:], -1.0, None, ALU.mult, None, [beta], [nbeta])
                h.act(egc[:], gc, AF.Exp, [cs12], [egc])
                h.tt("dve", t6[:], cl12[:, 0:6], gc, ALU.subtract, [cl12, cs12], [t6])
                h.act(dkk[:], t6[:], AF.Exp, [t6], [dkk])
                h.act(gtot[:], cl12[:, 0:6], AF.Exp, [cl12], [gtot])
                g2 = gtot[:].rearrange("p (j s) -> p j s", s=2)
                h.cp("dve", gtc[0:64, :], g2[0:64, :, 0], [gtot], [gtc])
                h.cp("dve", gtc[64:128, :], g2[64:128, :, 1], [gtot], [gtc])
                if stop < 1:
                    h.memset("pool", yo_[:, :, tc], 0.0, [yo_])
                    continue
                h.tt("pool", dg[:], b3(identf[:], 6, 128), s3(gc, 6, 128), ALU.mult, [identf, cs12], [dg])
                h.mm([(W0[:, 0, 0:384], masks["ones"][:], dg[:, 0:3, :].rearrange("p a l -> p (a l)"), True, True),
                      (W0[:, 1, 0:384], masks["ones"][:], dg[:, 3:6, :].rearrange("p a l -> p (a l)"), True, True)],
                     [masks["ones"], dg], [W0])
                if stop < 1.2:
                    h.memset("pool", yo_[:, :, tc], 0.0, [yo_])
                    continue
                h.tt("dve", v4(arg[:]), w4(W0), s4(gc), ALU.subtract, [W0, cs12], [arg])
                if stop < 1.4:
                    h.memset("pool", yo_[:, :, tc], 0.0, [yo_])
                    continue
                h.act(arg[:], arg[:], AF.Abs, [arg], [arg])
                h.act(E[:], arg[:], AF.Exp, [arg], [E], scale=-1.0)
                if stop < 1.6:
                    h.memset("pool", yo_[:, :, tc], 0.0, [yo_])
                    continue
                h.tt("pool", EU[:], E[:], b3(masks["U"][:], 6, 128), ALU.mult, [E, masks["U"]], [EU])
                if stop < 1.8:
                    h.memset("pool", yo_[:, :, tc], 0.0, [yo_])
                    continue
                h.tt("pool", ELn[:], E[:], b3(masks["Ls"][:], 6, 128), ALU.mult, [E, masks["Ls"]], [ELn])
                if stop < 1.9:
                    h.memset("pool", yo_[:, :, tc], 0.0, [yo_])
                    continue
                h.tt("dve", ELn[:], ELn[:], s3(nbeta[:], 6, 128), ALU.mult, [ELn, nbeta], [ELn])
                if stop < 2:
                    h.memset("pool", yo_[:, :, tc], 0.0, [yo_])
                    continue
                w1 = w4(W1)
                w2 = w4(W2)
                h.mm([(w1[:, hd // 3, hd % 3, :], kz[hd % 2][:, hd // 2, tc], kn_[:, hd // 2, tc], True, True) for hd in range(6)],
                     [kz[0], kz[1], kn_], [W1])
                h.mm([(w2[:, hd // 3, hd % 3, :], kz[hd % 2][:, hd // 2, tc], qn_[:, hd // 2, tc], True, True) for hd in range(6)],
                     [kz[0], kz[1], qn_], [W2])
                h.tt("dve", v4(Pm[0][:]), w1, v4(ELn[:]), ALU.mult, [W1, ELn], [Pm[0]])
                h.tt("dve", v4(attnT[:]), w2, v4(EU[:]), ALU.mult, [W2, EU], [attnT])
                if stop < 3:
                    h.memset("pool", yo_[:, :, tc], 0.0, [yo_])
                    continue
                h.tr([(PB[:, hd * 128:(hd + 1) * 128], Pm[0][:, hd, :]) for hd in range(6)], identb[:], [Pm[0], identb], [PB])
                h.cp("act", Qm[0][:], PB[:, 0:768].rearrange("p (a l) -> p a l", a=6), [PB], [Qm[0]])
                Nn, NT_ = Pm[0], Qm[0]
                h.tt("pool", Pm[1][:], Nn[:], b3(cmask[:, 0, :], 6, 128), ALU.mult, [Nn, cmask], [Pm[1]])
                h.tt("pool", Qm[1][:], NT_[:], b3(cmask[:, 7, :], 6, 128), ALU.mult, [NT_, cmask], [Qm[1]])
                h.tt("dve", Tb[:], Pm[1][:], b3(identf[:], 6, 128), ALU.add, [Pm[1], identf], [Tb])
                h.tt("dve", Xb[:], Qm[1][:], b3(identf[:], 6, 128), ALU.add, [Qm[1], identf], [Xb])
                for lv in range(1, 7):
                    h.tt("pool", Pm[1][:], Nn[:], b3(cmask[:, lv, :], 6, 128), ALU.mult, [Nn, cmask], [Pm[1]])
                    h.tt("pool", Qm[1][:], NT_[:], b3(cmask[:, 7 + lv, :], 6, 128), ALU.mult, [NT_, cmask], [Qm[1]])
                    headmm(W1, Qm[1], Tb, [Qm[1], Tb])
                    headmm(W2, Pm[1], Xb, [Pm[1], Xb])
                    h.cp("act", v4(M1b[:]), w4(W1), [W1], [M1b])
                    h.cp("dve", v4(M1pb[:]), w4(W2), [W2], [M1pb])
                    headmm(W1, Xb, M1b, [Xb, M1b])
                    headmm(W2, Tb, M1pb, [Tb, M1pb])
                    h.tt("dve", v4(Tb[:]), v4(Tb[:]), w4(W1), ALU.add, [Tb, W1], [Tb])
                    h.tt("dve", v4(Xb[:]), v4(Xb[:]), w4(W2), ALU.add, [Xb, W2], [Xb])
                if stop < 4:
                    h.memset("pool", yo_[:, :, tc], 0.0, [yo_])
                    continue
                h.tr([(PB[:, j * 128:(j + 1) * 128], kn_[:, j, tc]) for j in range(3)], identb[:], [kn_, identb], [PB])
                h.tt("dve", x3(kdec[:]), x3(PB[:, 0:384]), s3(dkk[:], 6, 64), ALU.mult, [PB, dkk], [kdec])
                h.tr([(W0[:, 0, j * 128:(j + 1) * 128], v_[:, j, tc]) for j in range(3)], identf[:], [v_, identf], [W0])
                h.cp("act", v_tm[:], W0[:, 0, 0:384], [W0], [v_tm])
                if stop < 5:
                    h.memset("pool", yo_[:, :, tc], 0.0, [yo_])
                    continue
                h.mm([(W1[:, 0, j * 128:(j + 1) * 128], kn_[:, j, tc], Sb[:, j, :], True, True) for j in range(3)], [kn_, Sb], [W1])
                h.tt("dve", x3(rr_[:]), x3(W1[:, 0, 0:384]), s3(egc[:], 6, 64), ALU.mult, [W1, egc], [rr_])
                h.tt("pool", rr_[:], rr_[:], v_tm[:], ALU.subtract, [rr_, v_tm], [rr_])
                h.tt("dve", x3(rb[:]), x3(rr_[:]), s3(nbeta[:], 6, 64), ALU.mult, [rr_, nbeta], [rb])
                h.mm([(W2[:, 0, hd * 64:(hd + 1) * 64], Xb[:, hd, :], rb[:, hd * 64:(hd + 1) * 64], True, True) for hd in range(6)], [Xb, rb], [W2])
                h.cp("act", vnb[:], W2[:, 0, 0:384], [W2], [vnb])
                h.mm([(W1[:, 0, j * 128:(j + 1) * 128], qn_[:, j, tc], Sb[:, j, :], True, True) for j in range(3)], [qn_, Sb], [W1])
                h.tt("dve", x3(oa[:]), x3(W1[:, 0, 0:384]), s3(egc[:], 6, 64), ALU.mult, [W1, egc], [oa])
                h.mm([(W2[:, 0, hd * 64:(hd + 1) * 64], attnT[:, hd, :], vnb[:, hd * 64:(hd + 1) * 64], True, True) for hd in range(6)], [attnT, vnb], [W2])
                h.tt("dve", o[:], W2[:, 0, 0:384], oa[:], ALU.add, [W2, oa], [o])
                h.mm([(W0[:, 0, j * 128:(j + 1) * 128], kdec[:, j * 128:(j + 1) * 128], vnb[:, j * 128:(j + 1) * 128], True, True) for j in range(3)],
                     [kdec, vnb], [W0])
                h.tt("dve", tmpS[:], W0[:, 0, 0:384].rearrange("p (j c) -> p j c", j=3), b3(masks["bd"][:], 3, 128), ALU.mult, [W0, masks["bd"]], [tmpS])
                h.tt("dve", Sf[:], Sf[:], s3(gtc[:], 3, 128), ALU.mult, [Sf, gtc], [Sf])
                h.tt("pool", Sf[:], Sf[:], tmpS[:], ALU.add, [Sf, tmpS], [Sf])
                h.cp("act", Sb[:], Sf[:], [Sf], [Sb])
                if stop < 6:
                    h.memset("pool", yo_[:, :, tc], 0.0, [yo_])
                    continue
                h.tt("pool", sq[:], o[:], o[:], ALU.mult, [o], [sq])
                h.reduce(ss6[:], x3(sq[:]), ALU.add, [sq], [ss6])
                h.ts("dve", rs6[:], ss6[:], 1.0 / 64, EPS, ALU.mult, ALU.add, [ss6], [rs6])
                h.act(rs6[:], rs6[:], AF.Sqrt, [rs6], [rs6])
                h.recip(rs6[:], rs6[:], [rs6], [rs6])
                h.tt("dve", x3(o[:]), x3(o[:]), s3(rs6[:], 6, 64), ALU.mult, [o, rs6], [o])
                h.tt("dve", x3(o[:]), x3(o[:]), b3(gnw[:], 6, 64), ALU.mult, [o, gnw], [o])
                h.act(zg[:], pj_[:, ti, 0:384], AF.Silu, [pj_], [zg])
                h.tt("pool", yb[:], o[:], zg[:], ALU.mult, [o, zg], [yb])
                h.tr([(PB[:, j * 128:(j + 1) * 128], yb[:, j * 128:(j + 1) * 128]) for j in range(3)], identb[:], [yb, identb], [PB])
                h.cp("act", yo_[:, :, tc], PB[:, 0:384].rearrange("p (j t) -> p j t", j=3), [PB], [yo_])
            h.dma("sp", k.y_d.t[256:640, cols].rearrange("(j p) t -> p j t", p=128), yo_[:], [yo_], [k.r_y[1][mt]], yo_)
        P.end_phase()


class K:
    def __init__(self, T, L, flags):
        self.T = T
        self.L = L
        self.NT = T // 128
        self.flags = flags


def bcast(ap, shape):
    return ap.to_broadcast(list(shape))


def build(T, L, flags=None):
    flags = flags or {}
    nc = bass.Bass("TRN2", target_bir_lowering=False)
    k = K(T, L, flags)
    NT = T // 128
    with contextlib.ExitStack() as es0:
        P = Prog(nc, es0)
        k.P = P
        inp = {}

        def ein(name, shape):
            inp[name] = P.dram(name, shape, F32, "ExternalInput")
            return inp[name]

        x_in = ein("x", [T, D])
        c_in = ein("c", [1, D])
        w_ada = ein("w_ada", [L, D, 6 * D])
        b_ada = ein("b_ada", [L, 6 * D])
        norm_mix = ein("norm_mix", [L, D])
        norm_ffn = ein("norm_ffn", [L, D])
        norm_final = ein("norm_final", [1, D])
        w_rt = ein("w_rt", [L, D, 36])
        b_rt = ein("b_rt", [L, 36])
        w_gate = ein("moe_w_gate", [L, NEXP, D, DEXP])
        w_up = ein("moe_w_up", [L, NEXP, D, DEXP])
        w_down = ein("moe_w_down", [L, NEXP, DEXP, D])
        ein("w_in_f", [L, D, 2304])
        ein("w_in_t", [L, D, 786])
        ein("w_out", [L, D, D])
        ein("conv_w", [L, 128, 16, 4])
        ein("conv_b", [L, 128, 16])
        ein("bias12", [L, 12])
        ein("alog12", [L, 12])
        ein("ssd_d", [L, 6])
        ein("ssd_norm", [L, 384])
        ein("gdn_norm", [L, 64])
        ein("gdn_cmask", [14, 128, 128])
        ein("s5_are", [L, 128, 8])
        ein("s5_aim", [L, 128, 8])
        ein("s5_ldt", [L, 128, 8])
        ein("s5_dcol", [L, 128, 2])
        ein("s5_ncol", [L, 128, 2])
        ein("s5_bT_re", [L, 8, 128, 128])
        ein("s5_bT_im", [L, 8, 128, 128])
        ein("s5_cT_re", [L, 8, 128, 128])
        ein("s5_cT_im", [L, 8, 128, 128])
        ein("s5_w_glu", [L, 256, 256])
        out = P.dram("out", [T, D], F32, "ExternalOutput")
        k.u_d = P.dram("u_d", [256, T], F32)
        k.qn_d = P.dram("qn_d", [384, T], BF16)
        k.kn_d = P.dram("kn_d", [384, T], BF16)
        k.v_d = P.dram("v_d", [384, T], F32)
        k.xs_d = P.dram("xs_d", [384, T], F32)
        k.B_d = P.dram("B_d", [256, T], BF16)
        k.C_d = P.dram("C_d", [256, T], BF16)
        k.pt_d = P.dram("pt_d", [T, 786], F32)
        k.y_d = P.dram("y_d", [D, T], BF16)
        k.r_pre = [P.region(f"pre_{i}") for i in range(max(1, T // 512))]
        k.r_y = [[P.region(f"y{j}_{i}") for i in range(max(1, T // 512))] for j in range(3)]
        k.h = H(P)
        h = k.h
        modv = P.dram("modv", [L, 6 * D], F32)
        dbg = flags.get("dbg", False)
        if dbg:
            dbg_coef = P.dram("dbg_coef", [T, 32], F32, "ExternalOutput")
            dbg_y = P.dram("dbg_y", [T, D], F32, "ExternalOutput")
            dbg_h = P.dram("dbg_h", [T, D], F32, "ExternalOutput")
            r_dbg = P.region("dbg")
        scr = [P.dram("xs0", [T, D], F32), P.dram("xs1", [T, D], F32)]
        k.inp = inp

        def regs(name):
            return [P.region(f"{name}_{i}") for i in range(NT)]
        r_x = regs("x")
        r_scr = [regs("xs0"), regs("xs1")]
        r_out = regs("out")
        r_modv = P.region("modv")

        identf = P.sb(es0, "identf", [128, 128], F32)
        identb = P.sb(es0, "identb", [128, 128], BF16)
        P.op("pool", lambda e: e.memset(identf[:], 0.0), writes=[identf])
        P.op("pool", lambda e: e.affine_select(out=identf[:], in_=identf[:], pattern=[[-1, 128]],
                                               compare_op=ALU.not_equal, fill=1.0, base=0,
                                               channel_multiplier=1),
             reads=[identf], writes=[identf])
        P.op("dve", lambda e: e.tensor_copy(out=identb[:], in_=identf[:]), reads=[identf], writes=[identb])
        k.identf, k.identb = identf, identb

        with contextlib.ExitStack() as es:
            ccol = P.sb(es, "ccol", [128, 8], F32)
            cb = P.sb(es, "cb", [128, 8, 128], BF16)
            wa = [P.sb(es, f"wa{i}", [128, 8, 512], BF16) for i in range(2)]
            pm = [P.ps(es, f"pm{i}", [128, 512], F32) for i in range(2)]
            brow = [P.sb(es, f"brow{i}", [1, 512], F32) for i in range(2)]
            mrow = [P.sb(es, f"mrow{i}", [1, 512], F32) for i in range(2)]
            P.op("sp", lambda e: e.dma_start(out=ccol[:], in_=c_in.t.rearrange("o (k p) -> p (o k)", p=128),
                                             allow_slow_non_contiguous=True),
                 writes=[ccol], dma=ccol)
            P.op("act", lambda e: e.activation(out=ccol[:], in_=ccol[:], func=AF.Silu), reads=[ccol], writes=[ccol])
            P.op("dve", lambda e: e.tensor_copy(out=cb[:], in_=bcast(ccol[:].unsqueeze(2), [128, 8, 128])),
                 reads=[ccol], writes=[cb])
            it = 0
            for l in range(L):
                for n in range(12):
                    i = it % 2
                    it += 1
                    P.op("pool", lambda e, l=l, n=n, i=i: e.dma_start(
                        out=wa[i][:], in_=w_ada.t[l, :, n * 512:(n + 1) * 512].rearrange("(k p) n -> p k n", p=128)),
                        writes=[wa[i]], dma=wa[i])
                    P.op("sp", lambda e, l=l, n=n, i=i: e.dma_start(
                        out=brow[i][:], in_=b_ada.t[l:l + 1, n * 512:(n + 1) * 512]),
                        writes=[brow[i]], dma=brow[i])

                    def mm(e, i=i):
                        r = None
                        for kk in range(8):
                            r = e.matmul(pm[i][:], lhsT=cb[:, kk, :], rhs=wa[i][:, kk, :], start=(kk == 0), stop=(kk == 7))
                        return r
                    P.op("pe", mm, reads=[cb, wa[i]], writes=[pm[i]])
                    P.op("dve", lambda e, i=i: e.tensor_tensor(out=mrow[i][:], in0=pm[i][0:1, :], in1=brow[i][:], op=ALU.add),
                         reads=[pm[i], brow[i]], writes=[mrow[i]])
                    P.op("sp", lambda e, l=l, n=n, i=i: e.dma_start(
                        out=modv.t[l:l + 1, n * 512:(n + 1) * 512], in_=mrow[i][:]),
                        reads=[mrow[i]], writes=[r_modv], dma=mrow[i])
            P.end_phase()

        def load_row(dst, src_ap, extra_reads=()):
            P.op("sp", lambda e: e.dma_start(out=dst[:], in_=src_ap.partition_broadcast(128)),
                 reads=list(extra_reads), writes=[dst], dma=dst)

        def norm_consts(es, l, nw, i_scale, i_shift, tag):
            A = P.sb(es, f"A{tag}", [128, D], F32)
            B = P.sb(es, f"B{tag}", [128, D], F32)
            W = P.sb(es, f"W{tag}", [128, D], F32)
            load_row(A, modv.t[l:l + 1, i_scale * D:(i_scale + 1) * D], [r_modv])
            load_row(B, modv.t[l:l + 1, i_shift * D:(i_shift + 1) * D], [r_modv])
            load_row(W, nw)
            P.op("dve", lambda e: e.scalar_tensor_tensor(out=A[:], in0=A[:], scalar=1.0, in1=W[:], op0=ALU.add, op1=ALU.mult),
                 reads=[A, W], writes=[A])
            return A, B

        def rms_mod(xt, A, B, hout, ssq, rstd, junk, tmp):
            P.op("act", lambda e: e.activation(out=junk[:], in_=xt[:], func=AF.Square, accum_out=ssq[:]),
                 reads=[xt], writes=[junk, ssq])
            P.op("dve", lambda e: e.tensor_scalar(out=rstd[:], in0=ssq[:], scalar1=1.0 / D, scalar2=EPS, op0=ALU.mult, op1=ALU.add),
                 reads=[ssq], writes=[rstd])
            P.op("act", lambda e: e.activation(out=rstd[:], in_=rstd[:], func=AF.Sqrt), reads=[rstd], writes=[rstd])
            P.op("dve", lambda e: e.reciprocal(out=rstd[:], in_=rstd[:]), reads=[rstd], writes=[rstd])
            if B is None:
                P.op("dve", lambda e: e.scalar_tensor_tensor(out=hout[:], in0=xt[:], scalar=rstd[:, 0:1], in1=A[:], op0=ALU.mult, op1=ALU.mult),
                     reads=[xt, rstd, A], writes=[hout])
            else:
                P.op("dve", lambda e: e.scalar_tensor_tensor(out=tmp[:], in0=xt[:], scalar=rstd[:, 0:1], in1=A[:], op0=ALU.mult, op1=ALU.mult),
                     reads=[xt, rstd, A], writes=[tmp])
                P.op("pool", lambda e: e.tensor_tensor(out=hout[:], in0=tmp[:], in1=B[:], op=ALU.add),
                     reads=[tmp, B], writes=[hout])

        k.modv, k.r_modv = modv, r_modv
        k.load_row, k.norm_consts, k.rms_mod = load_row, norm_consts, rms_mod
        cur = (x_in, r_x)
        nxt_i = 0

        def next_dst():
            nonlocal nxt_i
            d = (scr[nxt_i], r_scr[nxt_i])
            nxt_i ^= 1
            return d

        for l in range(L):
            if flags.get("mixer", True):
                dstt = next_dst()
                mixer_layer(k, l, cur, dstt)
                cur = dstt
            if flags.get("moe", True):
                src, rsrc = cur
                dst, rdst = next_dst()
                SBT = min(16, NT)
                with contextlib.ExitStack() as es:
                    A, B = norm_consts(es, l, norm_ffn.t[l:l + 1, :], 4, 3, "f")
                    G = P.sb(es, "Gf", [128, D], F32)
                    load_row(G, modv.t[l:l + 1, 5 * D:6 * D], [r_modv])
                    brt = P.sb(es, "brt", [128, 36], F32)
                    load_row(brt, b_rt.t[l:l + 1, :])
                    wrt = P.sb(es, "wrt", [128, 8, 36], F32)
                    P.op("sp", lambda e: e.dma_start(out=wrt[:], in_=w_rt.t[l].rearrange("(k p) n -> p k n", p=128)),
                         writes=[wrt], dma=wrt)
                    hT = P.sb(es, "hT", [128, 8, SBT * 128], BF16)
                    yacc = [P.sb(es, f"yacc{i}", [128, D], F32) for i in range(SBT)]
                    coef = P.sb(es, "coef", [128, SBT, 32], F32)
                    xt = [P.sb(es, f"xt{i}", [128, D], F32) for i in range(2)]
                    hf = P.sb(es, "hf", [128, D], F32)
                    tmp = P.sb(es, "tmpf", [128, D], F32)
                    junk = P.sb(es, "junkf", [128, D], F32)
                    hTf = P.sb(es, "hTf", [128, 8, 128], F32)
                    ssq = P.sb(es, "ssq", [128, 1], F32)
                    rstd = P.sb(es, "rstd", [128, 1], F32)
                    lg = P.sb(es, "lg", [128, 36], F32)
                    sm = P.sb(es, "sm", [128, 16], F32)
                    gm = P.sb(es, "gm", [128, 4], F32)
                    gex = P.sb(es, "gex", [128, 4], F32)
                    le4 = P.sb(es, "le4", [128, 4, 8], F32)
                    les = P.sb(es, "les", [128, 8], F32)
                    le2 = P.sb(es, "le2", [128, 8], F32)
                    mk1 = P.sb(es, "mk1", [128, 8], F32)
                    mk2 = P.sb(es, "mk2", [128, 8], F32)
                    csel = P.sb(es, "csel", [128, 8], F32)
                    wg = [P.sb(es, f"wg{i}", [128, 8, DEXP], BF16) for i in range(2)]
                    wu = [P.sb(es, f"wu{i}", [128, 8, DEXP], BF16) for i in range(2)]
                    wd = [P.sb(es, f"wd{i}", [128, 2, D], BF16) for i in range(2)]
                    sg = [P.sb(es, f"sg{i}", [128, 512], F32) for i in range(2)]
                    hid = [P.sb(es, f"hid{i}", [128, 2, 512], BF16) for i in range(2)]
                    ptr = P.ps(es, "ptr", [128, 8, 128], F32)
                    pg = [P.ps(es, f"pg{i}", [128, 512], F32) for i in range(2)]
                    pu = [P.ps(es, f"pu{i}", [128, 512], F32) for i in range(2)]
                    py = [P.ps(es, f"py{i}", [128, 512], F32) for i in range(2)]

                    wcnt = 0
                    for sb0 in range(0, NT, SBT):
                        for ti in range(SBT):
                            t = sb0 + ti
                            xb = xt[t % 2]
                            P.op("sp", lambda e, t=t, xb=xb: e.dma_start(out=xb[:], in_=src.t[t * 128:(t + 1) * 128, :]),
                                 reads=[rsrc[t]], writes=[xb], dma=xb)
                            rms_mod(xb, A, B, hf, ssq, rstd, junk, tmp)

                            if dbg and l == 0:
                                P.op("sp", lambda e, t=t: e.dma_start(out=dbg_h.t[t * 128:(t + 1) * 128, :], in_=hf[:]),
                                     reads=[hf], writes=[r_dbg], dma=hf)

                            def trf(e):
                                r = None
                                for kk in range(8):
                                    r = e.transpose(out=ptr[:, kk, :], in_=hf[:, kk * 128:(kk + 1) * 128], identity=identf[:])
                                return r
                            P.op("pe", trf, reads=[hf, identf], writes=[ptr])
                            P.op("act", lambda e: e.copy(out=hTf[:], in_=ptr[:]), reads=[ptr], writes=[hTf])
                            P.op("dve", lambda e, ti=ti: e.tensor_copy(out=hT[:, :, ti * 128:(ti + 1) * 128], in_=hTf[:]),
                                 reads=[hTf], writes=[hT])

                            def mrt(e):
                                r = None
                                for kk in range(8):
                                    r = e.matmul(ptr[:, 0, 0:36], lhsT=hTf[:, kk, :], rhs=wrt[:, kk, :], start=(kk == 0), stop=(kk == 7))
                                return r
                            P.op("pe", mrt, reads=[hTf, wrt], writes=[ptr])
                            P.op("dve", lambda e: e.tensor_tensor(out=lg[:], in0=ptr[:, 0, 0:36], in1=brt[:], op=ALU.add),
                                 reads=[ptr, brt], writes=[lg])
                            P.op("dve", lambda e: e.tensor_reduce(out=sm[:, 0:1], in_=lg[:, 0:4], axis=AX.X, op=ALU.max),
                                 reads=[lg], writes=[sm])
                            P.op("dve", lambda e: e.tensor_scalar(out=gm[:], in0=lg[:, 0:4], scalar1=sm[:, 0:1], scalar2=None, op0=ALU.is_equal),
                                 reads=[lg, sm], writes=[gm])
                            P.op("dve", lambda e: e.tensor_scalar(out=sm[:, 1:2], in0=sm[:, 0:1], scalar1=-1.0, scalar2=None, op0=ALU.mult),
                                 reads=[sm], writes=[sm])
                            P.op("act", lambda e: e.activation(out=gex[:], in_=lg[:, 0:4], func=AF.Exp, bias=sm[:, 1:2], accum_out=sm[:, 2:3]),
                                 reads=[lg, sm], writes=[gex, sm])
                            P.op("dve", lambda e: e.reciprocal(out=sm[:, 3:4], in_=sm[:, 2:3]), reads=[sm], writes=[sm])
                            P.op("dve", lambda e: e.tensor_tensor(out=le4[:], in0=lg[:, 4:36].rearrange("p (g e) -> p g e", g=4),
                                                                  in1=bcast(gm[:].unsqueeze(2), [128, 4, 8]), op=ALU.mult),
                                 reads=[lg, gm], writes=[le4])
                            P.op("dve", lambda e: e.tensor_reduce(out=les[:], in_=le4[:].rearrange("p g e -> p e g"), axis=AX.X, op=ALU.add),
                                 reads=[le4], writes=[les])
                            P.op("dve", lambda e: e.tensor_reduce(out=sm[:, 4:5], in_=les[:], axis=AX.X, op=ALU.max), reads=[les], writes=[sm])
                            P.op("dve", lambda e: e.tensor_scalar(out=mk1[:], in0=les[:], scalar1=sm[:, 4:5], scalar2=None, op0=ALU.is_equal),
                                 reads=[les, sm], writes=[mk1])
                            P.op("dve", lambda e: e.scalar_tensor_tensor(out=le2[:], in0=mk1[:], scalar=-1e30, in1=les[:], op0=ALU.mult, op1=ALU.add),
                                 reads=[mk1, les], writes=[le2])
                            P.op("dve", lambda e: e.tensor_reduce(out=sm[:, 5:6], in_=le2[:], axis=AX.X, op=ALU.max), reads=[le2], writes=[sm])
                            P.op("dve", lambda e: e.tensor_scalar(out=mk2[:], in0=le2[:], scalar1=sm[:, 5:6], scalar2=None, op0=ALU.is_equal),
                                 reads=[le2, sm], writes=[mk2])
                            P.op("dve", lambda e: e.tensor_tensor(out=sm[:, 6:7], in0=sm[:, 4:5], in1=sm[:, 5:6], op=ALU.subtract),
                                 reads=[sm], writes=[sm])
                            P.op("act", lambda e: e.activation(out=sm[:, 7:8], in_=sm[:, 6:7], func=AF.Sigmoid), reads=[sm], writes=[sm])
                            P.op("act", lambda e: e.activation(out=sm[:, 8:9], in_=sm[:, 6:7], func=AF.Sigmoid, scale=-1.0), reads=[sm], writes=[sm])
                            P.op("dve", lambda e: e.tensor_scalar(out=sm[:, 7:9], in0=sm[:, 7:9], scalar1=sm[:, 3:4], scalar2=None, op0=ALU.mult),
                                 reads=[sm], writes=[sm])
                            P.op("dve", lambda e: e.tensor_scalar(out=csel[:], in0=mk1[:], scalar1=sm[:, 7:8], scalar2=None, op0=ALU.mult),
                                 reads=[mk1, sm], writes=[csel])
                            P.op("dve", lambda e: e.scalar_tensor_tensor(out=csel[:], in0=mk2[:], scalar=sm[:, 8:9], in1=csel[:], op0=ALU.mult, op1=ALU.add),
                                 reads=[mk2, sm, csel], writes=[csel])
                            P.op("dve", lambda e, ti=ti: e.tensor_tensor(out=coef[:, ti, :].rearrange("p (g e) -> p g e", g=4),
                                                                         in0=bcast(gm[:].unsqueeze(2), [128, 4, 8]),
                                                                         in1=bcast(csel[:].unsqueeze(1), [128, 4, 8]), op=ALU.mult),
                                 reads=[gm, csel], writes=[coef])
                        nblk = (SBT * 128 + 511) // 512
                        seq = [(ex, blk) for ex in range(NEXP) for blk in range(nblk)]

                        def load_w(ex):
                            wi = ex % 2
                            h.dma("pool", wg[wi][:], w_gate.t[l, ex].rearrange("(k p) n -> p k n", p=128), [], [wg[wi]], wg[wi])
                            h.dma("pool", wu[wi][:], w_up.t[l, ex].rearrange("(k p) n -> p k n", p=128), [], [wu[wi]], wu[wi])
                            h.dma("pool", wd[wi][:], w_down.t[l, ex].rearrange("(k p) n -> p k n", p=128), [], [wd[wi]], wd[wi])

                        def GU(i):
                            ex, blk = seq[i]
                            wi, bi = ex % 2, i % 2
                            c0 = blk * 512
                            cw = min(512, SBT * 128 - c0)
                            for fc in range(2):
                                fs = slice(fc * 128, (fc + 1) * 128)
                                h.mm([(pg[fc][:, 0:cw], wg[wi][:, kk, fs], hT[:, kk, c0:c0 + cw], kk == 0, kk == 7) for kk in range(8)],
                                     [wg[wi], hT], [pg[fc]])
                                h.act(sg[fc][:, 0:cw], pg[fc][:, 0:cw], AF.Silu, [pg[fc]], [sg[fc]])
                                yield
                                h.mm([(pu[fc][:, 0:cw], wu[wi][:, kk, fs], hT[:, kk, c0:c0 + cw], kk == 0, kk == 7) for kk in range(8)],
                                     [wu[wi], hT], [pu[fc]])
                                h.tt("dve", hid[bi][:, fc, 0:cw], sg[fc][:, 0:cw], pu[fc][:, 0:cw], ALU.mult, [sg[fc], pu[fc]], [hid[bi]])
                                yield

                        def DN(i):
                            ex, blk = seq[i]
                            wi, bi = ex % 2, i % 2
                            c0 = blk * 512
                            cw = min(512, SBT * 128 - c0)
                            for st in range(cw // 128):
                                ti = blk * 4 + st
                                for n2 in range(2):
                                    pi = n2
                                    ns = slice(n2 * 512, (n2 + 1) * 512)
                                    h.mm([(py[pi][:], hid[bi][:, fc, st * 128:(st + 1) * 128], wd[wi][:, fc, ns], fc == 0, fc == 1) for fc in range(2)],
                                         [hid[bi], wd[wi]], [py[pi]])
                                    if ex == 0:
                                        h.ts("dve", yacc[ti][:, ns], py[pi][:], coef[:, ti, ex:ex + 1], None, ALU.mult, None, [py[pi], coef], [yacc[ti]])
                                    else:
                                        h.stt("dve", yacc[ti][:, ns], py[pi][:], coef[:, ti, ex:ex + 1], yacc[ti][:, ns], ALU.mult, ALU.add,
                                              [py[pi], coef, yacc[ti]], [yacc[ti]])
                                    yield
                            if blk == nblk - 1 and ex + 2 < NEXP:
                                load_w(ex + 2)

                        def drain(g):
                            for _ in g:
                                pass

                        def step(g, n):
                            for _ in range(n):
                                if next(g, "END") == "END":
                                    return

                        load_w(0)
                        load_w(1)
                        drain(GU(0))
                        for i in range(len(seq)):
                            gd = DN(i)
                            if i + 1 < len(seq):
                                gg = GU(i + 1)
                                for _ in range(4):
                                    step(gg, 1)
                                    step(gd, 2)
                                drain(gg)
                            drain(gd)
                        for ti in range(SBT):
                            t = sb0 + ti
                            if dbg and l == 0:
                                P.op("sp", lambda e, t=t, ti=ti: e.dma_start(out=dbg_y.t[t * 128:(t + 1) * 128, :], in_=yacc[ti][:]),
                                     reads=[yacc[ti]], writes=[r_dbg], dma=yacc[ti])
                                P.op("sp", lambda e, t=t, ti=ti: e.dma_start(out=dbg_coef.t[t * 128:(t + 1) * 128, :], in_=coef[:, ti, :]),
                                     reads=[coef], writes=[r_dbg], dma=coef)
                            xb = xt[t % 2]
                            P.op("sp", lambda e, t=t, xb=xb: e.dma_start(out=xb[:], in_=src.t[t * 128:(t + 1) * 128, :]),
                                 reads=[rsrc[t]], writes=[xb], dma=xb)
                            P.op("pool", lambda e, ti=ti: e.tensor_tensor(out=yacc[ti][:], in0=yacc[ti][:], in1=G[:], op=ALU.mult),
                                 reads=[yacc[ti], G], writes=[yacc[ti]])
                            P.op("dve", lambda e, ti=ti, xb=xb: e.tensor_tensor(out=yacc[ti][:], in0=yacc[ti][:], in1=xb[:], op=ALU.add),
                                 reads=[yacc[ti], xb], writes=[yacc[ti]])
                            P.op("sp", lambda e, t=t, ti=ti: e.dma_start(out=dst.t[t * 128:(t + 1) * 128, :], in_=yacc[ti][:]),
                                 reads=[yacc[ti]], writes=[rdst[t]], dma=yacc[ti])
                    P.end_phase()
                cur = (dst, rdst)

        src, rsrc = cur
        with contextlib.ExitStack() as es:
            Wn = P.sb(es, "Wn", [128, D], F32)
            load_row(Wn, norm_final.t[0:1, :])
            xt = [P.sb(es, f"xtn{i}", [128, D], F32) for i in range(2)]
            ho = [P.sb(es, f"hon{i}", [128, D], F32) for i in range(2)]
            junk = P.sb(es, "junkn", [128, D], F32)
            ssq = P.sb(es, "ssqn", [128, 1], F32)
            rstd = P.sb(es, "rstdn", [128, 1], F32)
            for t in range(NT):
                xb = xt[t % 2]
                hb = ho[t % 2]
                P.op("sp", lambda e, t=t, xb=xb: e.dma_start(out=xb[:], in_=src.t[t * 128:(t + 1) * 128, :]),
                     reads=[rsrc[t]], writes=[xb], dma=xb)
                rms_mod(xb, Wn, None, hb, ssq, rstd, junk, None)
                P.op("sp", lambda e, t=t, hb=hb: e.dma_start(out=out.t[t * 128:(t + 1) * 128, :], in_=hb[:]),
                     reads=[hb], writes=[r_out[t]], dma=hb)
            P.final_wait("sp", r_out)
            P.end_phase()
    return nc


def host_inputs(inputs, L, T):
    f = lambda a: np.ascontiguousarray(np.asarray(a, dtype=np.float32))
    w_rt = f(np.concatenate([inputs["moe_w_grp"][:L], inputs["moe_w_rt"][:L]], axis=-1))
    b_rt = f(np.concatenate([inputs["moe_b_grp"][:L], inputs["moe_b_rt"][:L]], axis=-1))
    w_in = np.asarray(inputs["w_in"][:L], dtype=np.float32)
    w_in_f = np.concatenate([w_in[:, :, 0:1408], w_in[:, :, 2188:3084]], axis=-1)
    w_in_t = np.concatenate([w_in[:, :, 1408:1792], w_in[:, :, 1804:2188], w_in[:, :, 1792:1798],
                             w_in[:, :, 3084:3090], w_in[:, :, 1798:1804]], axis=-1)
    gcw = np.asarray(inputs["gdn_conv_w"][:L], dtype=np.float32)
    scw = np.asarray(inputs["ssd_conv_w"][:L], dtype=np.float32)
    cw = np.concatenate([gcw, scw], axis=-1)
    conv_w = cw.reshape(L, 4, 16, 128).transpose(0, 3, 2, 1)
    cb = np.concatenate([np.zeros((L, 1152), np.float32), np.asarray(inputs["ssd_conv_b"][:L], dtype=np.float32)], axis=-1)
    conv_b = cb.reshape(L, 16, 128).transpose(0, 2, 1)
    bias12 = np.concatenate([inputs["gdn_dt_bias"][:L], inputs["ssd_dt_bias"][:L]], axis=-1)
    alog12 = np.concatenate([inputs["gdn_a_log"][:L], inputs["ssd_a_log"][:L]], axis=-1)
    def st_layout(a):
        a = np.asarray(a[:L], dtype=np.float32)
        return a.reshape(L, 8, 2, 64).transpose(0, 2, 3, 1).reshape(L, 128, 8)
    ldt = np.repeat(np.asarray(inputs["s5_log_dt"][:L], dtype=np.float32)[:, :, None], 64, axis=2)
    def bT_layout(b):
        b = np.asarray(b[:L], dtype=np.float32)
        o = np.zeros((L, 8, 128, 128), np.float32)
        for sc in range(8):
            for gl in range(2):
                r0 = 32 * (sc % 4) + 16 * gl
                o[:, sc, r0:r0 + 16, gl * 64:(gl + 1) * 64] = b[:, 2 * sc + gl].transpose(0, 2, 1)
        return o
    def cT_layout(c):
        c = np.asarray(c[:L], dtype=np.float32)
        o = np.zeros((L, 8, 128, 128), np.float32)
        for sc in range(8):
            for gl in range(2):
                r0 = 32 * (sc % 4) + 16 * gl
                o[:, sc, gl * 64:(gl + 1) * 64, r0:r0 + 16] = c[:, 2 * sc + gl].transpose(0, 2, 1)
        return o
    ii = np.arange(128)[:, None]
    jj = np.arange(128)[None, :]
    cm = []
    for lv in range(7):
        s_ = 1 << lv
        cm.append(((ii // (2 * s_) == jj // (2 * s_)) & (ii % (2 * s_) >= s_) & (jj % (2 * s_) < s_)).astype(np.float32))
    cmask = np.stack(cm + [m.T for m in cm], axis=0)
    col2 = lambda a: np.asarray(a[:L], dtype=np.float32).reshape(L, 2, 128).transpose(0, 2, 1)
    shared = {
        "gdn_cmask": f(cmask),
        "s5_are": f(st_layout(inputs["s5_a_re"])), "s5_aim": f(st_layout(inputs["s5_a_im"])), "s5_ldt": f(st_layout(ldt)),
        "s5_dcol": f(col2(inputs["s5_d"])), "s5_ncol": f(col2(inputs["s5_norm"])),
        "s5_bT_re": f(bT_layout(inputs["s5_b_re"])), "s5_bT_im": f(bT_layout(inputs["s5_b_im"])),
        "s5_cT_re": f(cT_layout(inputs["s5_c_re"])), "s5_cT_im": f(cT_layout(inputs["s5_c_im"])),
        "s5_w_glu": f(inputs["s5_w_glu"][:L]),
        "w_in_f": f(w_in_f), "w_in_t": f(w_in_t), "w_out": f(inputs["w_out"][:L]),
        "conv_w": f(conv_w), "conv_b": f(conv_b), "bias12": f(bias12), "alog12": f(alog12),
        "ssd_d": f(inputs["ssd_d"][:L]), "ssd_norm": f(inputs["ssd_norm"][:L]), "gdn_norm": f(inputs["gdn_norm"][:L]),
        "w_ada": f(inputs["w_ada"][:L]), "b_ada": f(inputs["b_ada"][:L]),
        "norm_mix": f(inputs["norm_mix"][:L]), "norm_ffn": f(inputs["norm_ffn"][:L]),
        "norm_final": f(inputs["norm_final"]).reshape(1, D),
        "w_rt": w_rt, "b_rt": b_rt,
        "moe_w_gate": f(inputs["moe_w_gate"][:L]), "moe_w_up": f(inputs["moe_w_up"][:L]),
        "moe_w_down": f(inputs["moe_w_down"][:L]),
    }
    maps = []
    B = inputs["x"].shape[0]
    for b in range(B):
        m = dict(shared)
        m["x"] = f(inputs["x"][b, :T])
        m["c"] = f(inputs["c"][b]).reshape(1, D)
        maps.append(m)
    return maps


def run(inputs, L, T, flags=None, trace=False):
    nc = build(T, L, flags)
    maps = host_inputs(inputs, L, T)
    res = run_bass_kernel_spmd(nc, maps, core_ids=list(range(len(maps))))
    if flags and flags.get("dbg"):
        return res.results
    return np.stack([r["out"] for r in res.results], axis=0)


def kernel(**inputs):
    return run(inputs, 4, 4096).astype(np.float32)
```

```python
import contextlib
import math
import numpy as np
import concourse.bass as bass
import concourse.mybir as mybir
from concourse.bass_utils import run_bass_kernel_spmd

F32 = mybir.dt.float32
BF16 = mybir.dt.bfloat16
ALU = mybir.AluOpType
AF = mybir.ActivationFunctionType
AX = mybir.AxisListType

D = 1024
NEXP = 32
DEXP = 256
EPS = 1e-6
ENGS = ("pe", "act", "dve", "pool", "sp")


class Buf:
    def __init__(self, t, name, multi=False):
        self.t = t
        self.name = name
        self.w = {}
        self.r = {}
        self.sem = None
        self.dcnt = 0
        self.multi = multi

    def __getitem__(self, k):
        return self.t[k]


class Prog:
    SEM_LAT = 0.15

    def __init__(self, nc, es):
        self.nc = nc
        self.es = es
        self.ops = []
        self.sems = []
        self.esem = {}
        self.ecnt = {e: 0 for e in ENGS}
        self.waited = {e: {} for e in ENGS}
        for e in ENGS:
            if e != "sp":
                self.esem[e] = self.newsem("e_" + e)
        self.uid = 0
        self.dsem_pool = []
        self.dbufs = []
        self.phase_bufs = []

    def newsem(self, name):
        s = self.es.enter_context(self.nc.semaphore(name))
        self.sems.append(s)
        return len(self.sems) - 1

    def sb(self, es, name, shape, dtype):
        self.uid += 1
        name = f"{name}_{self.uid}"
        t = es.enter_context(self.nc.sbuf_tensor(name, list(shape), dtype))
        b = Buf(t, name)
        self.phase_bufs.append(b)
        return b

    def ps(self, es, name, shape, dtype):
        self.uid += 1
        name = f"{name}_{self.uid}"
        t = es.enter_context(self.nc.psum_tensor(name, list(shape), dtype))
        return Buf(t, name)

    def dram(self, name, shape, dtype, kind="Internal"):
        t = self.nc.dram_tensor(name, list(shape), dtype, kind=kind).ap()
        return Buf(t, name)

    def region(self, name):
        return Buf(None, name, multi=True)

    def op(self, eng, fn, reads=(), writes=(), dma=None, est=None):
        if est is None:
            est = {"pe": 1.0, "act": 0.5, "dve": 0.35, "pool": 0.45, "sp": 3.0}[eng] if dma is None else 3.0
        self.ops.append((eng, fn, tuple(reads), tuple(writes), dma, est))

    def final_wait(self, eng, bufs):
        pass

    def end_phase(self):
        ops = self.ops
        self.ops = []
        n = len(ops)
        lw, rd = {}, {}
        deps = [None] * n
        for i, (eng, fn, reads, writes, dma, est) in enumerate(ops):
            d = set()
            for b in reads:
                d.update(lw.get(id(b), ()))
            for b in writes:
                d.update(rd.get(id(b), ()))
                if not b.multi:
                    d.update(lw.get(id(b), ()))
            deps[i] = d
            for b in reads:
                rd.setdefault(id(b), []).append(i)
            for b in writes:
                if b.multi:
                    lw.setdefault(id(b), []).append(i)
                else:
                    lw[id(b)] = [i]
                    rd[id(b)] = []
        import heapq
        succ = [[] for _ in range(n)]
        indeg = [0] * n
        for i in range(n):
            indeg[i] = len(deps[i])
            for j in deps[i]:
                succ[j].append(i)
        ready_t = [0.0] * n
        fin = [0.0] * n
        start = [0.0] * n
        efree = {e: 0.0 for e in ENGS}
        heap = [(0.0, i) for i in range(n) if indeg[i] == 0]
        heapq.heapify(heap)
        order = {e: [] for e in ENGS}
        glob = []
        while heap:
            rt, i = heapq.heappop(heap)
            eng, fn, reads, writes, dma, est = ops[i]
            st = max(rt, efree[eng])
            start[i] = st
            if dma is not None:
                occ = 0.5 if eng == "pool" else 0.08
                efree[eng] = st + occ
                fin[i] = st + occ + est
            else:
                efree[eng] = st + est
                fin[i] = st + est
            order[eng].append(i)
            glob.append(i)
            for k2 in succ[i]:
                indeg[k2] -= 1
                if ready_t[k2] < fin[i] + self.SEM_LAT:
                    ready_t[k2] = fin[i] + self.SEM_LAT
                if indeg[k2] == 0:
                    heapq.heappush(heap, (ready_t[k2], k2))
        assert len(glob) == n, "dependency cycle"
        tok = [None] * n
        for e in ENGS:
            if e == "sp":
                continue
        waits_raw = [None] * n
        cnt = dict(self.ecnt)
        for i in glob:
            eng, fn, reads, writes, dma, est = ops[i]
            w = {}
            for j in deps[i]:
                dj = ops[j][4]
                if dj is None:
                    s_, v_ = tok[j]
                else:
                    s_, v_ = dj.sem, dj.dcnt
                if w.get(s_, 0) < v_:
                    w[s_] = v_
            waits_raw[i] = w
            if dma is None:
                cnt[eng] += 1
                tok[i] = (self.esem[eng], cnt[eng])
            else:
                if dma.sem is None:
                    if self.dsem_pool:
                        dma.sem, dma.dcnt = self.dsem_pool.pop()
                    else:
                        dma.sem = self.newsem("d_" + dma.name)
                        dma.dcnt = 0
                    self.dbufs.append(dma)
                dma.dcnt += 16
                tok[i] = (dma.sem, dma.dcnt)
        self.ecnt = cnt
        nc = self.nc
        sems = self.sems
        qs = {}
        for e in ENGS:
            wd = self.waited[e]
            q = []
            for i in order[e]:
                ws = []
                for s_, v_ in waits_raw[i].items():
                    if wd.get(s_, 0) < v_:
                        ws.append((s_, v_))
                        wd[s_] = v_
                inc = (tok[i][0], 16 if ops[i][4] is not None else 1)
                q.append((ws, ops[i][1], inc))
            qs[e] = q
        toks = {}
        for e, s_ in self.esem.items():
            if self.ecnt[e] > 0:
                toks[s_] = self.ecnt[e]
        for b in self.dbufs:
            toks[b.sem] = max(toks.get(b.sem, 0), b.dcnt)
        for e in ENGS:
            wd = self.waited[e]
            ws = []
            for s_, v_ in toks.items():
                if wd.get(s_, 0) < v_:
                    ws.append((s_, v_))
                    wd[s_] = v_
            if ws:
                qs[e].append((ws, None, None))
        for b in self.phase_bufs:
            if b.sem is not None:
                self.dsem_pool.append((b.sem, b.dcnt))
                self.dbufs.remove(b)
                b.sem = None
        self.phase_bufs = []

        def mk(e):
            def f(eng):
                for waits, fn, inc in qs[e]:
                    for s_, v_ in waits:
                        eng.wait_ge(sems[s_], v_)
                    if fn is None:
                        continue
                    ins = fn(eng)
                    ins.then_inc(sems[inc[0]], inc[1])
            return f

        with nc.Block() as block:
            block.sync(mk("sp"))
            block.scalar(mk("act"))
            block.vector(mk("dve"))
            block.gpsimd(mk("pool"))
            block.tensor(mk("pe"))
        self.last_makespan = max(efree.values()) if n else 0.0


def _fsz(ap):
    n = 1
    for d in ap.shape[1:]:
        n *= int(d)
    return n


def _est(eng, ap, psum=False):
    n = _fsz(ap)
    if eng == "dve":
        return (60 + n) / 960.0 + (0.06 if psum else 0.0)
    if eng == "act":
        return (220 + n) / 1400.0
    if eng == "pool":
        return (120 + n) / 900.0
    return 0.5


class H:
    def __init__(self, P):
        self.P = P

    def dma(self, eng, out, in_, R, W, buf, slow=False):
        nbytes = _fsz(out) * 128 * 4
        est = 2.0 + nbytes / 150000.0
        if slow:
            self.P.op(eng, lambda e: e.dma_start(out=out, in_=in_, allow_slow_non_contiguous=True), reads=R, writes=W, dma=buf, est=est)
        else:
            self.P.op(eng, lambda e: e.dma_start(out=out, in_=in_), reads=R, writes=W, dma=buf, est=est)

    def tt(self, eng, out, in0, in1, op, R, W):
        self.P.op(eng, lambda e: e.tensor_tensor(out=out, in0=in0, in1=in1, op=op), reads=R, writes=W, est=_est(eng, out))

    def ts(self, eng, out, in0, s1, s2, op0, op1, R, W):
        if s2 is None:
            self.P.op(eng, lambda e: e.tensor_scalar(out=out, in0=in0, scalar1=s1, scalar2=None, op0=op0), reads=R, writes=W, est=_est(eng, out))
        else:
            self.P.op(eng, lambda e: e.tensor_scalar(out=out, in0=in0, scalar1=s1, scalar2=s2, op0=op0, op1=op1), reads=R, writes=W, est=_est(eng, out))

    def stt(self, eng, out, in0, sc, in1, op0, op1, R, W):
        eng = "dve"
        self.P.op(eng, lambda e: e.scalar_tensor_tensor(out=out, in0=in0, scalar=sc, in1=in1, op0=op0, op1=op1), reads=R, writes=W, est=_est(eng, out))

    def act(self, out, in_, func, R, W, bias=None, scale=None, accum=None):
        kw = {}
        if bias is not None:
            kw["bias"] = bias
        if scale is not None:
            kw["scale"] = scale
        if accum is not None:
            kw["accum_out"] = accum
        self.P.op("act", lambda e: e.activation(out=out, in_=in_, func=func, **kw), reads=R, writes=W, est=_est("act", out))

    def cp(self, eng, out, in_, R, W):
        if eng == "act":
            self.P.op("act", lambda e: e.copy(out=out, in_=in_), reads=R, writes=W, est=_est("act", out))
        else:
            self.P.op(eng, lambda e: e.tensor_copy(out=out, in_=in_), reads=R, writes=W, est=_est(eng, out))

    def memset(self, eng, ap, val, W):
        self.P.op(eng, lambda e: e.memset(ap, val), writes=W, est=_est(eng, ap))

    def recip(self, out, in_, R, W):
        self.P.op("dve", lambda e: e.reciprocal(out=out, in_=in_), reads=R, writes=W, est=_est("dve", out))

    def reduce(self, out, in_, op, R, W):
        self.P.op("dve", lambda e: e.tensor_reduce(out=out, in_=in_, axis=AX.X, op=op), reads=R, writes=W, est=_est("dve", in_))

    def mm(self, items, R, W):
        est = 0.0
        for (o, l, rh, st, sp) in items:
            est += (max(64, _fsz(rh)) * (4 if l.dtype == F32 else 1)) / 2400.0 + 0.01
        est += 0.06

        def f(e):
            r = None
            for (o, l, rh, st, sp) in items:
                r = e.matmul(o, lhsT=l, rhs=rh, start=st, stop=sp)
            return r
        self.P.op("pe", f, reads=R, writes=W, est=est)

    def tr(self, items, ident, R, W):
        est = 0.06
        for (o, i) in items:
            est += (128 * (4 if i.dtype == F32 else 1)) / 2400.0 + 0.03

        def f(e):
            r = None
            for (o, i) in items:
                r = e.transpose(out=o, in_=i, identity=ident)
            return r
        self.P.op("pe", f, reads=R, writes=W, est=est)

    def select(self, out, in_, cmp, fill, base, cm, pattern, R, W):
        self.P.op("pool", lambda e: e.affine_select(out=out, in_=in_, pattern=pattern, compare_op=cmp, fill=fill,
                                                    base=base, channel_multiplier=cm), reads=R, writes=W, est=_est("pool", out))


class Rot:
    def __init__(self, P, es, name, shape, dtype, n=2):
        self.bufs = [P.sb(es, f"{name}r{i}", shape, dtype) for i in range(n)]

    def at(self, i):
        return self.bufs[i % len(self.bufs)]


def b3(ap, n, m):
    return ap.unsqueeze(1).to_broadcast([128, n, m])


def s3(ap, n, m):
    return ap.unsqueeze(2).to_broadcast([128, n, m])


def s4(ap):
    return ap.rearrange("p (b h) -> p b h", b=2).unsqueeze(3).to_broadcast([128, 2, 3, 128])


def v4(ap):
    return ap.rearrange("p (b h) l -> p b h l", b=2)


def w4(ps):
    return ps[:, :, 0:384].rearrange("p b (h l) -> p b h l", h=3)


def softplus12(h, P, es, tagp):
    xa = P.sb(es, tagp + "xa", [128, 12], F32)
    ax = P.sb(es, tagp + "ax", [128, 12], F32)
    ex = P.sb(es, tagp + "ex", [128, 12], F32)
    ln = P.sb(es, tagp + "ln", [128, 12], F32)
    one = P.sb(es, tagp + "one", [128, 1], F32)
    h.memset("pool", one[:], 1.0, [one])

    def f(xin, xin_buf, bias, out):
        h.tt("dve", xa[:], xin, bias[:], ALU.add, [xin_buf, bias], [xa])
        h.act(ax[:], xa[:], AF.Abs, [xa], [ax])
        h.act(ex[:], ax[:], AF.Exp, [ax], [ex], scale=-1.0)
        h.act(ln[:], ex[:], AF.Ln, [ex, one], [ln], bias=one[:, 0:1])
        h.ts("dve", xa[:], xa[:], 0.0, None, ALU.max, None, [xa], [xa])
        h.tt("dve", out[:], xa[:], ln[:], ALU.add, [xa, ln], [out])
    return f


def make_masks(h, P, es):
    m = {}
    ones = P.sb(es, "m_ones", [128, 128], F32)
    h.memset("pool", ones[:], 1.0, [ones])
    m["ones"] = ones
    for name, cmp, cm, st in (("U", ALU.is_ge, -1, 1), ("L", ALU.is_ge, 1, -1), ("Ls", ALU.is_gt, 1, -1)):
        t = P.sb(es, "m_" + name, [128, 128], F32)
        h.select(t[:], ones[:], cmp, 0.0, 0, cm, [[st, 128]], [ones], [t])
        m[name] = t
    sel = P.sb(es, "m_sel", [128, 128], F32)
    zer = P.sb(es, "m_zero", [128, 128], F32)
    h.memset("pool", zer[:], 0.0, [zer])
    h.select(sel[:], zer[:], ALU.not_equal, 1.0, -127, 1, [[0, 128]], [zer], [sel])
    m["sel"] = sel
    bd = P.sb(es, "m_bd", [128, 128], F32)
    h.memset("pool", bd[:], 0.0, [bd])
    h.memset("pool", bd[0:64, 0:64], 1.0, [bd])
    h.memset("pool", bd[64:128, 64:128], 1.0, [bd])
    m["bd"] = bd
    return m


def mixer_layer(k, l, cur, dstt):
    P, T, flags, h = k.P, k.T, k.flags, k.h
    inp = k.inp
    src, rsrc = cur
    dst, rdst = dstt
    NMT = T // 512
    MT = 512
    identf, identb = k.identf, k.identb
    u_d, qn_d, kn_d, v_d, xs_d, B_d, C_d, pt_d, y_d = k.u_d, k.qn_d, k.kn_d, k.v_d, k.xs_d, k.B_d, k.C_d, k.pt_d, k.y_d
    r_pre, r_y = k.r_pre, k.r_y

    with contextlib.ExitStack() as es:
        A, B = k.norm_consts(es, l, inp["norm_mix"].t[l:l + 1, :], 1, 0, "m")
        winf = P.sb(es, "winf", [128, 8, 2304], BF16)
        wint = P.sb(es, "wint", [128, 8, 786], BF16)
        for (c0, c1) in ((0, 1152), (1152, 2304)):
            h.dma("pool", winf[:, :, c0:c1], inp["w_in_f"].t[l, :, c0:c1].rearrange("(k p) n -> p k n", p=128), [], [winf], winf)
        h.dma("pool", wint[:], inp["w_in_t"].t[l].rearrange("(k p) n -> p k n", p=128), [], [wint], wint)
        cwt = P.sb(es, "cwt", [128, 16, 4], F32)
        cbt = P.sb(es, "cbt", [128, 16], F32)
        h.dma("sp", cwt[:], inp["conv_w"].t[l], [], [cwt], cwt)
        h.dma("sp", cbt[:], inp["conv_b"].t[l], [], [cbt], cbt)
        carry = P.sb(es, "carry", [128, 16, 3], F32)
        h.memset("pool", carry[:], 0.0, [carry])
        epsb = P.sb(es, "epsb", [128, 1], F32)
        h.memset("pool", epsb[:], EPS, [epsb])
        bones = P.sb(es, "bones", [128, 128], F32)
        h.memset("pool", bones[:], 0.0, [bones])
        h.memset("pool", bones[0:64, 0:64], 1.0, [bones])
        h.memset("pool", bones[64:128, 64:128], 1.0, [bones])
        xt = [P.sb(es, f"m1x{i}", [128, D], F32) for i in range(2)]
        tmp = P.sb(es, "m1tmp", [128, D], F32)
        hb = P.sb(es, "m1hb", [128, D], BF16)
        hT = P.sb(es, "m1hT", [128, 8, MT], BF16)
        cin = [P.sb(es, f"cin{i}", [128, MT + 3], F32) for i in range(2)]
        acc = [P.sb(es, f"acc{i}", [128, MT], F32) for i in range(2)]
        so = [P.sb(es, f"so{i}", [128, MT], F32) for i in range(2)]
        sob = [P.sb(es, f"sob{i}", [128, MT], BF16) for i in range(2)]
        sq = P.sb(es, "m1sq", [128, MT], F32)
        rinv = P.sb(es, "m1rinv", [128, MT], F32)
        ptst = [P.sb(es, f"ptst{i}", [128, 786], F32) for i in range(2)]
        ssq = P.sb(es, "m1ssq", [128, 1], F32)
        rstd = P.sb(es, "m1rstd", [128, 1], F32)
        ptr = P.ps(es, "m1ptr", [128, 8, 128], BF16)
        pp = [P.ps(es, f"m1pp{i}", [128, MT], F32) for i in range(2)]
        pt = P.ps(es, "m1pt", [128, 2, 512], F32)
        pq = P.ps(es, "m1pq", [128, MT], F32)
        for mt in range(NMT):
            cols = slice(mt * MT, (mt + 1) * MT)
            for ti in range(4):
                t = mt * 4 + ti
                xb = xt[t % 2]
                h.dma("sp", xb[:], src.t[t * 128:(t + 1) * 128, :], [rsrc[t]], [xb], xb)
                k.rms_mod(xb, A, B, hb, ssq, rstd, tmp, tmp)
                h.tr([(ptr[:, kk, :], hb[:, kk * 128:(kk + 1) * 128]) for kk in range(8)], identb[:], [hb, identb], [ptr])
                h.cp("act", hT[:, :, ti * 128:(ti + 1) * 128], ptr[:], [ptr], [hT])
            for c in range(18):
                pb = pp[c % 2]
                h.mm([(pb[:], winf[:, kk, c * 128:(c + 1) * 128], hT[:, kk, :], kk == 0, kk == 7) for kk in range(8)], [winf, hT], [pb])
                if c < 2:
                    sb_ = so[c % 2]
                    h.cp("act", sb_[:], pb[:], [pb], [sb_])
                    h.dma("sp", u_d.t[c * 128:(c + 1) * 128, cols], sb_[:], [sb_], [r_pre[mt]], sb_)
                    continue
                ci = c - 2
                cb_ = cin[ci % 2]
                h.cp("pool", cb_[:, 0:3], carry[:, ci, :], [carry], [cb_])
                h.cp("act", cb_[:, 3:MT + 3], pb[:], [pb], [cb_])
                h.cp("pool", carry[:, ci, :], cb_[:, MT:MT + 3], [cb_], [carry])
                ab = acc[ci % 2]
                h.ts("dve", ab[:], cb_[:, 0:MT], cwt[:, ci, 0:1], None, ALU.mult, None, [cb_, cwt], [ab])
                for j in range(1, 4):
                    h.stt("dve", ab[:], cb_[:, j:j + MT], cwt[:, ci, j:j + 1], ab[:], ALU.mult, ALU.add, [cb_, cwt, ab], [ab])
                sb_ = so[ci % 2]
                h.act(sb_[:], ab[:], AF.Silu, [ab, cbt], [sb_], bias=cbt[:, ci:ci + 1])
                if ci < 6:
                    h.tt("pool", sq[:], sb_[:], sb_[:], ALU.mult, [sb_], [sq])
                    h.mm([(pq[:], bones[:], sq[:], True, True)], [bones, sq], [pq])
                    h.act(rinv[:], pq[:], AF.Sqrt, [pq, epsb], [rinv], bias=epsb[:, 0:1])
                    h.recip(rinv[:], rinv[:], [rinv], [rinv])
                    ob = sob[ci % 2]
                    if ci < 3:
                        h.stt("dve", ob[:], sb_[:], 0.125, rinv[:], ALU.mult, ALU.mult, [sb_, rinv], [ob])
                    else:
                        h.tt("dve", ob[:], sb_[:], rinv[:], ALU.mult, [sb_, rinv], [ob])
                    dd = qn_d if ci < 3 else kn_d
                    j = ci % 3
                    h.dma("sp", dd.t[j * 128:(j + 1) * 128, cols], ob[:], [ob], [r_pre[mt]], ob)
                elif ci < 12:
                    dd = v_d if ci < 9 else xs_d
                    j = (ci - 6) % 3
                    h.dma("sp", dd.t[j * 128:(j + 1) * 128, cols], sb_[:], [sb_], [r_pre[mt]], sb_)
                else:
                    ob = sob[ci % 2]
                    h.cp("pool", ob[:], sb_[:], [sb_], [ob])
                    dd = B_d if ci < 14 else C_d
                    j = (ci - 12) % 2
                    h.dma("sp", dd.t[j * 128:(j + 1) * 128, cols], ob[:], [ob], [r_pre[mt]], ob)
            for ti in range(4):
                t = mt * 4 + ti
                tc = slice(ti * 128, (ti + 1) * 128)
                h.mm([(pt[:, 0, :], hT[:, kk, tc], wint[:, kk, 0:512], kk == 0, kk == 7) for kk in range(8)]
                     + [(pt[:, 1, 0:274], hT[:, kk, tc], wint[:, kk, 512:786], kk == 0, kk == 7) for kk in range(8)],
                     [hT, wint], [pt])
                stg = ptst[t % 2]
                h.cp("act", stg[:, 0:512], pt[:, 0, :], [pt], [stg])
                h.cp("dve", stg[:, 512:786], pt[:, 1, 0:274], [pt], [stg])
                h.dma("sp", pt_d.t[t * 128:(t + 1) * 128, :], stg[:], [stg], [r_pre[mt]], stg)
        P.end_phase()

    if flags.get("s5", True):
        s5_phase(k, l)
    if flags.get("gdn", True):
        gdn_phase(k, l)
    if flags.get("ssd", True):
        ssd_phase(k, l)

    with contextlib.ExitStack() as es:
        wout = P.sb(es, "wout", [128, 8, D], BF16)
        h.dma("pool", wout[:], inp["w_out"].t[l].rearrange("(k p) n -> p k n", p=128), [], [wout], wout)
        G = P.sb(es, "Gm", [128, D], F32)
        k.load_row(G, k.modv.t[l:l + 1, 2 * D:3 * D], [k.r_modv])
        yT = [P.sb(es, f"m5y{i}", [128, 8, MT], BF16) for i in range(2)]
        xt = [P.sb(es, f"m5x{i}", [128, D], F32) for i in range(2)]
        tm = [P.sb(es, f"m5t{i}", [128, D], F32) for i in range(2)]
        po = [P.ps(es, f"m5p{i}", [128, 512], F32) for i in range(4)]
        for mt in range(NMT):
            cols = slice(mt * MT, (mt + 1) * MT)
            yb = yT[mt % 2]
            h.dma("sp", yb[:], y_d.t[:, cols].rearrange("(k p) t -> p k t", p=128), [r_y[0][mt], r_y[1][mt], r_y[2][mt]], [yb], yb)
            if not flags.get("s5", True):
                h.memset("pool", yb[:, 0:2, :], 0.0, [yb])
            if not flags.get("gdn", True):
                h.memset("pool", yb[:, 2:5, :], 0.0, [yb])
            if not flags.get("ssd", True):
                h.memset("pool", yb[:, 5:8, :], 0.0, [yb])
            for ti in range(4):
                t = mt * 4 + ti
                tc = slice(ti * 128, (ti + 1) * 128)
                xb = xt[t % 2]
                tb = tm[t % 2]
                h.dma("sp", xb[:], src.t[t * 128:(t + 1) * 128, :], [rsrc[t]], [xb], xb)
                for n2 in range(2):
                    pb = po[(t % 2) * 2 + n2]
                    nc_ = slice(n2 * 512, (n2 + 1) * 512)
                    h.mm([(pb[:], yb[:, kk, tc], wout[:, kk, nc_], kk == 0, kk == 7) for kk in range(8)], [yb, wout], [pb])
                    h.tt("dve", tb[:, nc_], pb[:], G[:, nc_], ALU.mult, [pb, G], [tb])
                h.tt("pool", tb[:], tb[:], xb[:], ALU.add, [tb, xb], [tb])
                h.dma("sp", dst.t[t * 128:(t + 1) * 128, :], tb[:], [tb], [rdst[t]], tb)
        P.end_phase()


def gate_consts(k, es, l, tag):
    P, h, inp = k.P, k.h, k.inp
    b12 = P.sb(es, tag + "b12", [128, 12], F32)
    na12 = P.sb(es, tag + "na12", [128, 12], F32)
    k.load_row(b12, inp["bias12"].t[l:l + 1, :])
    k.load_row(na12, inp["alog12"].t[l:l + 1, :])
    h.act(na12[:], na12[:], AF.Exp, [na12], [na12])
    h.ts("dve", na12[:], na12[:], -1.0, None, ALU.mult, None, [na12], [na12])
    return b12, na12


def gate_tile(k, sp_fn, pj_ap, pj_buf, b12, na12, masks, sp12, gda, cs12, cl12, pA):
    h = k.h
    sp_fn(pj_ap, pj_buf, b12, sp12)
    h.tt("dve", gda[:], sp12[:], na12[:], ALU.mult, [sp12, na12], [gda])
    h.mm([(pA[:, 0:12], masks["U"][:], gda[:], True, True)], [masks["U"], gda], [pA])
    h.cp("dve", cs12[:], pA[:, 0:12], [pA], [cs12])
    h.mm([(pA[:, 16:28], masks["sel"][:], cs12[:], True, True)], [masks["sel"], cs12], [pA])
    h.cp("dve", cl12[:], pA[:, 16:28], [pA], [cl12])


def ssd_phase(k, l):
    P, T, h, inp = k.P, k.T, k.h, k.inp
    NMT = T // 512
    MT = 512
    identf, identb = k.identf, k.identb
    with contextlib.ExitStack() as es:
        masks = make_masks(h, P, es)
        b12, na12 = gate_consts(k, es, l, "sd")
        sp_fns = [softplus12(h, P, es, f"sd{i}") for i in range(2)]
        dsk = P.sb(es, "sd_dsk", [128, 6], F32)
        k.load_row(dsk, inp["ssd_d"].t[l:l + 1, :])
        nws = P.sb(es, "sd_nws", [128, 384], F32)
        k.load_row(nws, inp["ssd_norm"].t[l:l + 1, :])
        stT = P.sb(es, "sd_stT", [128, 384], F32)
        stTb = P.sb(es, "sd_stTb", [128, 384], BF16)
        h.memset("pool", stT[:], 0.0, [stT])
        h.memset("pool", stTb[:], 0.0, [stTb])
        xsT = [P.sb(es, f"sd_xsT{i}", [128, 3, MT], F32) for i in range(2)]
        BTt = [P.sb(es, f"sd_BT{i}", [128, 2, MT], BF16) for i in range(2)]
        CTt = [P.sb(es, f"sd_CT{i}", [128, 2, MT], BF16) for i in range(2)]
        pj = [P.sb(es, f"sd_pj{i}", [128, 4, 786], F32) for i in range(2)]
        yo = [P.sb(es, f"sd_yo{i}", [128, 3, MT], BF16) for i in range(2)]
        R_sp12 = Rot(P, es, "sd_sp12", [128, 12], F32)
        R_gda = Rot(P, es, "sd_gda", [128, 12], F32)
        R_cs12 = Rot(P, es, "sd_cs12", [128, 12], F32)
        R_cl12 = Rot(P, es, "sd_cl12", [128, 12], F32)
        R_t6 = Rot(P, es, "sd_t6", [128, 6], F32)
        R_din = Rot(P, es, "sd_din", [128, 6], F32)
        R_eacs = Rot(P, es, "sd_eacs", [128, 6], F32)
        R_cd = Rot(P, es, "sd_cd", [128, 6], F32)
        R_dg = Rot(P, es, "sd_dg", [128, 6, 128], F32)
        R_arg = Rot(P, es, "sd_arg", [128, 6, 128], F32)
        R_seg = Rot(P, es, "sd_seg", [128, 6, 128], F32)
        R_WTb = Rot(P, es, "sd_WTb", [128, 6, 128], BF16)
        R_xs_tm = Rot(P, es, "sd_xstm", [128, 384], F32)
        R_xdtf = Rot(P, es, "sd_xdtf", [128, 384], F32)
        R_xdtb = Rot(P, es, "sd_xdtb", [128, 384], BF16)
        R_xddb = Rot(P, es, "sd_xddb", [128, 384], BF16)
        R_Btm = Rot(P, es, "sd_Btm", [128, 256], BF16)
        R_t1 = Rot(P, es, "sd_t1", [128, 384], F32)
        R_t2 = Rot(P, es, "sd_t2", [128, 384], F32)
        R_y = Rot(P, es, "sd_y", [128, 384], F32)
        R_zs = Rot(P, es, "sd_zs", [128, 384], F32)
        R_junk = Rot(P, es, "sd_junk", [128, 192], F32)
        R_yb = Rot(P, es, "sd_yb", [128, 384], BF16)
        R_ss2 = Rot(P, es, "sd_ss2", [128, 2], F32)
        R_rs2 = Rot(P, es, "sd_rs2", [128, 2], F32)
        W0 = P.ps(es, "sd_W0", [128, 2, 512], F32)
        S0 = P.ps(es, "sd_S0", [128, 512], F32)
        S1 = P.ps(es, "sd_S1", [128, 512], F32)
        S2 = P.ps(es, "sd_S2", [128, 512], F32)
        PB = P.ps(es, "sd_PB", [128, 512], BF16)
        pA = P.ps(es, "sd_pA", [128, 32], F32)
        for mt in range(NMT):
            cols = slice(mt * MT, (mt + 1) * MT)
            i2 = mt % 2
            rp = [k.r_pre[mt]]
            h.dma("sp", xsT[i2][:], k.xs_d.t[:, cols].rearrange("(j p) t -> p j t", p=128), rp, [xsT[i2]], xsT[i2])
            h.dma("sp", BTt[i2][:], k.B_d.t[:, cols].rearrange("(j p) t -> p j t", p=128), rp, [BTt[i2]], BTt[i2])
            h.dma("sp", CTt[i2][:], k.C_d.t[:, cols].rearrange("(j p) t -> p j t", p=128), rp, [CTt[i2]], CTt[i2])
            h.dma("sp", pj[i2][:], k.pt_d.t[mt * MT:(mt + 1) * MT, :].rearrange("(a p) n -> p a n", p=128), rp, [pj[i2]], pj[i2])
            xs_, B_, C_, pj_, yo_ = xsT[i2], BTt[i2], CTt[i2], pj[i2], yo[i2]
            for ti in range(4):
                tc = slice(ti * 128, (ti + 1) * 128)
                tix = mt * 4 + ti
                sp12 = R_sp12.at(tix); gda = R_gda.at(tix); cs12 = R_cs12.at(tix); cl12 = R_cl12.at(tix); t6 = R_t6.at(tix); din = R_din.at(tix); eacs = R_eacs.at(tix); cd = R_cd.at(tix); dg = R_dg.at(tix); arg = R_arg.at(tix); seg = R_seg.at(tix); WTb = R_WTb.at(tix); xs_tm = R_xs_tm.at(tix); xdtf = R_xdtf.at(tix); xdtb = R_xdtb.at(tix); xddb = R_xddb.at(tix); Btm = R_Btm.at(tix); t1 = R_t1.at(tix); t2 = R_t2.at(tix); y = R_y.at(tix); zs = R_zs.at(tix); junk = R_junk.at(tix); yb = R_yb.at(tix); ss2 = R_ss2.at(tix); rs2 = R_rs2.at(tix)
                sp_fn = sp_fns[tix % 2]
                gate_tile(k, sp_fn, pj_[:, ti, 768:780], pj_, b12, na12, masks, sp12, gda, cs12, cl12, pA)
                acs = cs12[:, 6:12]
                h.tt("pool", dg[:], b3(identf[:], 6, 128), s3(acs, 6, 128), ALU.mult, [identf, cs12], [dg])
                h.mm([(W0[:, 0, 0:384], masks["ones"][:], dg[:, 0:3, :].rearrange("p a l -> p (a l)"), True, True),
                      (W0[:, 1, 0:384], masks["ones"][:], dg[:, 3:6, :].rearrange("p a l -> p (a l)"), True, True)],
                     [masks["ones"], dg], [W0])
                h.tt("dve", v4(arg[:]), w4(W0), s4(acs), ALU.subtract, [W0, cs12], [arg])
                h.ts("pool", arg[:], arg[:], 0.0, None, ALU.min, None, [arg], [arg])
                h.act(seg[:], arg[:], AF.Exp, [arg], [seg])
                h.tt("pool", seg[:], seg[:], b3(masks["U"][:], 6, 128), ALU.mult, [seg, masks["U"]], [seg])
                h.mm([(S0[:, g * 128:(g + 1) * 128], B_[:, g, tc], C_[:, g, tc], True, True) for g in range(2)], [B_, C_], [S0])
                h.tt("dve", v4(WTb[:]), v4(seg[:]),
                     S0[:, 0:256].rearrange("p (g l) -> p g l", g=2).unsqueeze(2).to_broadcast([128, 2, 3, 128]),
                     ALU.mult, [seg, S0], [WTb])
                h.tr([(S1[:, j * 128:(j + 1) * 128], xs_[:, j, tc]) for j in range(3)], identf[:], [xs_, identf], [S1])
                h.cp("act", xs_tm[:], S1[:, 0:384], [S1], [xs_tm])
                h.tr([(PB[:, g * 128:(g + 1) * 128], B_[:, g, tc]) for g in range(2)], identb[:], [B_, identb], [PB])
                h.cp("act", Btm[:], PB[:, 0:256], [PB], [Btm])
                x3 = lambda ap: ap.rearrange("p (h d) -> p h d", h=6)
                h.tt("dve", x3(xdtf[:]), x3(xs_tm[:]), s3(sp12[:, 6:12], 6, 64), ALU.mult, [xs_tm, sp12], [xdtf])
                h.cp("pool", xdtb[:], xdtf[:], [xdtf], [xdtb])
                h.tt("dve", t6[:], cl12[:, 6:12], acs, ALU.subtract, [cl12, cs12], [t6])
                h.act(din[:], t6[:], AF.Exp, [t6], [din])
                h.tt("pool", x3(xddb[:]), x3(xdtf[:]), s3(din[:], 6, 64), ALU.mult, [xdtf, din], [xddb])
                h.mm([(S2[:, hd * 64:(hd + 1) * 64], WTb[:, hd, :], xdtb[:, hd * 64:(hd + 1) * 64], True, True) for hd in range(6)],
                     [WTb, xdtb], [S2])
                h.mm([(S0[:, g * 192:(g + 1) * 192], C_[:, g, tc], stTb[:, g * 192:(g + 1) * 192], True, True) for g in range(2)],
                     [C_, stTb], [S0])
                h.act(eacs[:], acs, AF.Exp, [cs12], [eacs])
                h.tt("dve", x3(t2[:]), x3(S0[:, 0:384]), s3(eacs[:], 6, 64), ALU.mult, [S0, eacs], [t2])
                h.tt("pool", x3(t1[:]), x3(xs_tm[:]), s3(dsk[:], 6, 64), ALU.mult, [xs_tm, dsk], [t1])
                h.tt("pool", t2[:], t2[:], t1[:], ALU.add, [t2, t1], [t2])
                h.tt("dve", y[:], S2[:, 0:384], t2[:], ALU.add, [S2, t2], [y])
                h.act(zs[:], pj_[:, ti, 384:768], AF.Silu, [pj_], [zs])
                h.tt("pool", y[:], y[:], zs[:], ALU.mult, [y, zs], [y])
                for g in range(2):
                    h.act(junk[:], y[:, g * 192:(g + 1) * 192], AF.Square, [y], [junk, ss2], accum=ss2[:, g:g + 1])
                h.ts("dve", rs2[:], ss2[:], 1.0 / 192, EPS, ALU.mult, ALU.add, [ss2], [rs2])
                h.act(rs2[:], rs2[:], AF.Sqrt, [rs2], [rs2])
                h.recip(rs2[:], rs2[:], [rs2], [rs2])
                y3 = lambda ap: ap.rearrange("p (g c) -> p g c", g=2)
                h.tt("dve", y3(y[:]), y3(y[:]), s3(rs2[:], 2, 192), ALU.mult, [y, rs2], [y])
                h.tt("pool", yb[:], y[:], nws[:], ALU.mult, [y, nws], [yb])
                h.tr([(PB[:, j * 128:(j + 1) * 128], yb[:, j * 128:(j + 1) * 128]) for j in range(3)], identb[:], [yb, identb], [PB])
                h.cp("act", yo_[:, :, tc], PB[:, 0:384].rearrange("p (j t) -> p j t", j=3), [PB], [yo_])
                h.mm([(S1[:, g * 192:(g + 1) * 192], Btm[:, g * 128:(g + 1) * 128], xddb[:, g * 192:(g + 1) * 192], True, True) for g in range(2)],
                     [Btm, xddb], [S1])
                h.act(cd[:], cl12[:, 6:12], AF.Exp, [cl12], [cd])
                h.tt("pool", x3(stT[:]), x3(stT[:]), s3(cd[:], 6, 64), ALU.mult, [stT, cd], [stT])
                h.tt("dve", stT[:], stT[:], S1[:, 0:384], ALU.add, [stT, S1], [stT])
                h.cp("act", stTb[:], stT[:], [stT], [stTb])
            h.dma("sp", k.y_d.t[640:1024, cols].rearrange("(j p) t -> p j t", p=128), yo_[:], [yo_], [k.r_y[2][mt]], yo_)
        P.end_phase()


C1_2PI = 6.28125
C2_2PI = 2.0 * math.pi - 6.28125


def sincos(h, eng, x, out, b, ki, c, negpi, R, W, bufs, is_cos):
    bb, kb, cb_ = bufs
    off = 16.5 + (0.25 if is_cos else 0.0)
    add = 33.0 * math.pi + (0.5 * math.pi if is_cos else 0.0)
    h.ts(eng, b, x, 1.0 / (2.0 * math.pi), off, ALU.mult, ALU.add, R, [bb])
    h.cp(eng, ki, b, [bb], [kb])
    h.cp(eng, c, ki, [kb], [cb_])
    h.stt(eng, b, c, -C1_2PI, x, ALU.mult, ALU.add, R + [cb_], [bb])
    h.stt(eng, b, c, -C2_2PI, b, ALU.mult, ALU.add, [cb_, bb], [bb])
    h.ts(eng, b, b, add, None, ALU.add, None, [bb], [bb])
    h.ts(eng, c, b, 2.0 * math.pi, -2.0 * math.pi, ALU.is_gt, ALU.mult, [bb], [cb_])
    h.tt(eng, b, b, c, ALU.add, [bb, cb_], [bb])
    h.ts(eng, c, b, 0.0, 2.0 * math.pi, ALU.is_lt, ALU.mult, [bb], [cb_])
    h.tt(eng, b, b, c, ALU.add, [bb, cb_], [bb])
    h.act(out, b, AF.Sin, [bb, negpi], W, bias=negpi[:, 0:1])


def s5_phase(k, l):
    P, T, h, inp = k.P, k.T, k.h, k.inp
    NMT = T // 512
    SEG = 512
    with contextlib.ExitStack() as es:
        I32 = mybir.dt.int32
        are = P.sb(es, "s5are", [128, 8], F32)
        aim = P.sb(es, "s5aim", [128, 8], F32)
        stp = P.sb(es, "s5stp", [128, 8], F32)
        h.dma("sp", are[:], inp["s5_are"].t[l], [], [are], are)
        h.dma("sp", aim[:], inp["s5_aim"].t[l], [], [aim], aim)
        h.dma("sp", stp[:], inp["s5_ldt"].t[l], [], [stp], stp)
        dsk = P.sb(es, "s5dsk", [128, 2], F32)
        nw5 = P.sb(es, "s5nw", [128, 2], F32)
        h.dma("sp", dsk[:], inp["s5_dcol"].t[l], [], [dsk], dsk)
        h.dma("sp", nw5[:], inp["s5_ncol"].t[l], [], [nw5], nw5)
        bTre = P.sb(es, "s5bTre", [128, 8, 128], BF16)
        bTim = P.sb(es, "s5bTim", [128, 8, 128], BF16)
        cTre = P.sb(es, "s5cTre", [128, 8, 128], BF16)
        cTim = P.sb(es, "s5cTim", [128, 8, 128], BF16)
        for dstb, nm in ((bTre, "s5_bT_re"), (bTim, "s5_bT_im"), (cTre, "s5_cT_re"), (cTim, "s5_cT_im")):
            h.dma("pool", dstb[:], inp[nm].t[l].rearrange("s r m -> r s m"), [], [dstb], dstb)
        wglu = P.sb(es, "s5wglu", [128, 2, 256], BF16)
        h.dma("pool", wglu[:], inp["s5_w_glu"].t[l].rearrange("(k p) n -> p k n", p=128), [], [wglu], wglu)
        negpi = P.sb(es, "s5negpi", [128, 1], F32)
        h.memset("pool", negpi[:], -math.pi, [negpi])
        epsb = P.sb(es, "s5eps", [128, 1], F32)
        h.memset("pool", epsb[:], EPS, [epsb])
        onesf = P.sb(es, "s5ones", [128, 128], F32)
        h.memset("pool", onesf[:], 1.0, [onesf])
        jrow = P.sb(es, "s5jrow", [128, SEG], F32)
        P.op("pool", lambda e: e.iota(jrow[:], pattern=[[1, SEG]], base=0, channel_multiplier=0,
                                      allow_small_or_imprecise_dtypes=True), writes=[jrow])
        th = P.sb(es, "s5th", [128, 8], F32)
        rr = P.sb(es, "s5r", [128, 8], F32)
        sth = P.sb(es, "s5sth", [128, 8], F32)
        cth = P.sb(es, "s5cth", [128, 8], F32)
        thS = P.sb(es, "s5thS", [128, 8], F32)
        sS = P.sb(es, "s5sS", [128, 8], F32)
        cS = P.sb(es, "s5cS", [128, 8], F32)
        nsS = P.sb(es, "s5nsS", [128, 8], F32)
        cr = P.sb(es, "s5cr", [128, 8], F32)
        ci = P.sb(es, "s5ci", [128, 8], F32)
        ncr = P.sb(es, "s5ncr", [128, 8], F32)
        q1 = P.sb(es, "s5q1", [128, 8], F32)
        q2 = P.sb(es, "s5q2", [128, 8], F32)
        q3 = P.sb(es, "s5q3", [128, 8], F32)
        sb8 = P.sb(es, "s5sb8", [128, 8], F32)
        si8 = P.sb(es, "s5si8", [128, 8], I32)
        sc8 = P.sb(es, "s5sc8", [128, 8], F32)
        h.act(stp[:], stp[:], AF.Exp, [stp], [stp])
        h.tt("dve", th[:], aim[:], stp[:], ALU.mult, [aim, stp], [th])
        h.tt("dve", rr[:], are[:], stp[:], ALU.mult, [are, stp], [rr])
        h.act(rr[:], rr[:], AF.Exp, [rr], [rr])
        sm = (sb8, si8, sc8)
        sincos(h, "dve", th[:], sth[:], sb8[:], si8[:], sc8[:], negpi, [th], [sth], sm, False)
        sincos(h, "dve", th[:], cth[:], sb8[:], si8[:], sc8[:], negpi, [th], [cth], sm, True)
        h.ts("dve", thS[:], th[:], float(SEG), None, ALU.mult, None, [th], [thS])
        sincos(h, "dve", thS[:], sS[:], sb8[:], si8[:], sc8[:], negpi, [thS], [sS], sm, False)
        sincos(h, "dve", thS[:], cS[:], sb8[:], si8[:], sc8[:], negpi, [thS], [cS], sm, True)
        h.ts("dve", nsS[:], sS[:], -1.0, None, ALU.mult, None, [sS], [nsS])
        h.tt("dve", q1[:], rr[:], cth[:], ALU.mult, [rr, cth], [q1])
        h.ts("dve", q1[:], q1[:], -1.0, None, ALU.add, None, [q1], [q1])
        h.tt("dve", q2[:], rr[:], sth[:], ALU.mult, [rr, sth], [q2])
        h.tt("dve", q3[:], are[:], are[:], ALU.mult, [are], [q3])
        h.tt("dve", sc8[:], aim[:], aim[:], ALU.mult, [aim], [sc8])
        h.tt("dve", q3[:], q3[:], sc8[:], ALU.add, [q3, sc8], [q3])
        h.recip(q3[:], q3[:], [q3], [q3])
        h.tt("dve", cr[:], q1[:], are[:], ALU.mult, [q1, are], [cr])
        h.tt("dve", sc8[:], q2[:], aim[:], ALU.mult, [q2, aim], [sc8])
        h.tt("dve", cr[:], cr[:], sc8[:], ALU.add, [cr, sc8], [cr])
        h.tt("dve", cr[:], cr[:], q3[:], ALU.mult, [cr, q3], [cr])
        h.tt("dve", ci[:], q2[:], are[:], ALU.mult, [q2, are], [ci])
        h.tt("dve", sc8[:], q1[:], aim[:], ALU.mult, [q1, aim], [sc8])
        h.tt("dve", ci[:], ci[:], sc8[:], ALU.subtract, [ci, sc8], [ci])
        h.tt("dve", ci[:], ci[:], q3[:], ALU.mult, [ci, q3], [ci])
        h.ts("dve", ncr[:], cr[:], -1.0, None, ALU.mult, None, [cr], [ncr])
        cosT = P.sb(es, "s5cosT", [128, 8, SEG], F32)
        sinT = P.sb(es, "s5sinT", [128, 8, SEG], F32)
        tabr = P.sb(es, "s5tabr", [128, 8, SEG], F32)
        tabi = P.sb(es, "s5tabi", [128, 8, SEG], F32)
        ang = [P.sb(es, f"s5ang{i}", [128, SEG], F32) for i in range(2)]
        tb = [P.sb(es, f"s5tb{i}", [128, SEG], F32) for i in range(2)]
        tki = [P.sb(es, f"s5tki{i}", [128, SEG], I32) for i in range(2)]
        tcc = [P.sb(es, f"s5tc{i}", [128, SEG], F32) for i in range(2)]
        for sc in range(8):
            i = sc % 2
            eng = "dve" if i == 0 else "pool"
            h.ts(eng, ang[i][:], jrow[:], th[:, sc:sc + 1], None, ALU.mult, None, [jrow, th], [ang[i]])
            bufs = (tb[i], tki[i], tcc[i])
            sincos(h, eng, ang[i][:], sinT[:, sc, :], tb[i][:], tki[i][:], tcc[i][:], negpi, [ang[i]], [sinT], bufs, False)
            sincos(h, eng, ang[i][:], cosT[:, sc, :], tb[i][:], tki[i][:], tcc[i][:], negpi, [ang[i]], [cosT], bufs, True)
            h.ts(eng, tabr[:, sc, :], cosT[:, sc, :], cr[:, sc:sc + 1], None, ALU.mult, None, [cosT, cr], [tabr])
            h.stt(eng, tabr[:, sc, :], sinT[:, sc, :], ci[:, sc:sc + 1], tabr[:, sc, :], ALU.mult, ALU.add, [sinT, ci, tabr], [tabr])
            h.ts(eng, tabi[:, sc, :], cosT[:, sc, :], ci[:, sc:sc + 1], None, ALU.mult, None, [cosT, ci], [tabi])
            h.stt(eng, tabi[:, sc, :], sinT[:, sc, :], ncr[:, sc:sc + 1], tabi[:, sc, :], ALU.mult, ALU.add, [sinT, ncr, tabi], [tabi])
        ire = P.sb(es, "s5ire", [128, 8], F32)
        iim = P.sb(es, "s5iim", [128, 8], F32)
        gre_e = P.sb(es, "s5gree", [128, 8], F32)
        gim_e = P.sb(es, "s5gime", [128, 8], F32)
        h.memset("pool", ire[:], 0.0, [ire])
        h.memset("pool", iim[:], 0.0, [iim])
        uTf = [P.sb(es, f"s5uTf{i}", [128, 2, SEG], F32) for i in range(2)]
        uTb = [P.sb(es, f"s5uTb{i}", [128, 2, SEG], BF16) for i in range(2)]
        m1 = [P.sb(es, f"s5m1{i}", [128, SEG], F32) for i in range(2)]
        m2 = [P.sb(es, f"s5m2{i}", [128, SEG], F32) for i in range(2)]
        m3 = [P.sb(es, f"s5m3{i}", [128, SEG], F32) for i in range(2)]
        m4 = [P.sb(es, f"s5m4{i}", [128, SEG], F32) for i in range(2)]
        n1 = [P.sb(es, f"s5n1{i}", [128, SEG], F32) for i in range(2)]
        n2 = [P.sb(es, f"s5n2{i}", [128, SEG], F32) for i in range(2)]
        n3 = [P.sb(es, f"s5n3{i}", [128, SEG], F32) for i in range(2)]
        n4 = [P.sb(es, f"s5n4{i}", [128, SEG], F32) for i in range(2)]
        dre = [P.sb(es, f"s5dre{i}", [128, SEG], F32) for i in range(2)]
        dim = [P.sb(es, f"s5dim{i}", [128, SEG], F32) for i in range(2)]
        gre = [P.sb(es, f"s5gre{i}", [128, SEG], F32) for i in range(2)]
        gim = [P.sb(es, f"s5gim{i}", [128, SEG], F32) for i in range(2)]
        hre = [P.sb(es, f"s5hre{i}", [128, SEG], BF16) for i in range(2)]
        him = [P.sb(es, f"s5him{i}", [128, SEG], BF16) for i in range(2)]
        y1 = P.sb(es, "s5y1", [128, 2, SEG], F32)
        yt = P.sb(es, "s5yt", [128, 2, SEG], F32)
        yg = P.sb(es, "s5yg", [128, 2, SEG], F32)
        ygb = P.sb(es, "s5ygb", [128, 2, SEG], BF16)
        sg = P.sb(es, "s5sg", [128, 2, SEG], F32)
        y2 = P.sb(es, "s5y2", [128, 2, SEG], F32)
        rstd = P.sb(es, "s5rstd", [128, SEG], F32)
        yo = [P.sb(es, f"s5yo{i}", [128, 2, SEG], BF16) for i in range(2)]
        Pre = [P.ps(es, f"s5Pre{i}", [128, SEG], F32) for i in range(2)]
        Pim = [P.ps(es, f"s5Pim{i}", [128, SEG], F32) for i in range(2)]
        Y = [P.ps(es, f"s5Y{i}", [128, SEG], F32) for i in range(2)]
        Pg = P.ps(es, "s5Pg", [128, SEG], F32)
        Pt = P.ps(es, "s5Pt", [128, SEG], F32)
        GK = 2.0 * math.sqrt(2.0 / math.pi)
        for mt in range(NMT):
            cols = slice(mt * SEG, (mt + 1) * SEG)
            i2 = mt % 2
            uf, ub, yo_ = uTf[i2], uTb[i2], yo[i2]
            h.dma("sp", uf[:], k.u_d.t[:, cols].rearrange("(j p) t -> p j t", p=128), [k.r_pre[mt]], [uf], uf)
            h.cp("pool", ub[:], uf[:], [uf], [ub])
            for sc in range(8):
                i = sc % 2
                cc = sc // 4
                h.mm([(Pre[i][:], bTre[:, sc, :], ub[:, cc, :], True, True)], [bTre, ub], [Pre[i]])
                h.mm([(Pim[i][:], bTim[:, sc, :], ub[:, cc, :], True, True)], [bTim, ub], [Pim[i]])
                h.tt("dve", m1[i][:], Pre[i][:], tabr[:, sc, :], ALU.mult, [Pre[i], tabr], [m1[i]])
                h.tt("dve", m2[i][:], Pim[i][:], tabi[:, sc, :], ALU.mult, [Pim[i], tabi], [m2[i]])
                h.tt("pool", dre[i][:], m1[i][:], m2[i][:], ALU.subtract, [m1[i], m2[i]], [dre[i]])
                h.tt("dve", m3[i][:], Pre[i][:], tabi[:, sc, :], ALU.mult, [Pre[i], tabi], [m3[i]])
                h.tt("dve", m4[i][:], Pim[i][:], tabr[:, sc, :], ALU.mult, [Pim[i], tabr], [m4[i]])
                h.tt("pool", dim[i][:], m3[i][:], m4[i][:], ALU.add, [m3[i], m4[i]], [dim[i]])
                for (go, di, ini) in ((gre[i], dre[i], ire), (gim[i], dim[i], iim)):
                    P.op("dve", (lambda go, di, ini, sc: (lambda e: e.tensor_tensor_scan(
                        out=go[:], data0=rr[:, sc:sc + 1].to_broadcast([128, SEG]), data1=di[:],
                        initial=ini[:, sc:sc + 1], op0=ALU.mult, op1=ALU.add)))(go, di, ini, sc),
                        reads=[rr, di, ini], writes=[go])
                h.cp("act", gre_e[:, sc:sc + 1], gre[i][:, SEG - 1:SEG], [gre[i]], [gre_e])
                h.cp("act", gim_e[:, sc:sc + 1], gim[i][:, SEG - 1:SEG], [gim[i]], [gim_e])
                h.tt("pool", n1[i][:], gre[i][:], cosT[:, sc, :], ALU.mult, [gre[i], cosT], [n1[i]])
                h.tt("pool", n2[i][:], gim[i][:], sinT[:, sc, :], ALU.mult, [gim[i], sinT], [n2[i]])
                h.tt("pool", hre[i][:], n1[i][:], n2[i][:], ALU.subtract, [n1[i], n2[i]], [hre[i]])
                h.tt("pool", n3[i][:], gre[i][:], sinT[:, sc, :], ALU.mult, [gre[i], sinT], [n3[i]])
                h.tt("pool", n4[i][:], gim[i][:], cosT[:, sc, :], ALU.mult, [gim[i], cosT], [n4[i]])
                h.stt("pool", him[i][:], n3[i][:], -1.0, n4[i][:], ALU.mult, ALU.subtract, [n3[i], n4[i]], [him[i]])
                h.mm([(Y[cc][:], cTre[:, sc, :], hre[i][:], sc % 4 == 0, False),
                      (Y[cc][:], cTim[:, sc, :], him[i][:], False, sc % 4 == 3)], [cTre, cTim, hre[i], him[i]], [Y[cc]])
                if sc % 4 == 3:
                    h.stt("dve", y1[:, cc, :], uf[:, cc, :], dsk[:, cc:cc + 1], Y[cc][:], ALU.mult, ALU.add, [uf, dsk, Y[cc]], [y1])
                    h.tt("pool", yt[:, cc, :], y1[:, cc, :], y1[:, cc, :], ALU.mult, [y1], [yt])
                    h.ts("pool", yt[:, cc, :], yt[:, cc, :], 0.044715, 1.0, ALU.mult, ALU.add, [yt], [yt])
                    h.tt("pool", yt[:, cc, :], yt[:, cc, :], y1[:, cc, :], ALU.mult, [yt, y1], [yt])
                    h.act(yt[:, cc, :], yt[:, cc, :], AF.Sigmoid, [yt], [yt], scale=GK)
                    h.tt("pool", yg[:, cc, :], y1[:, cc, :], yt[:, cc, :], ALU.mult, [y1, yt], [yg])
                    h.cp("pool", ygb[:, cc, :], yg[:, cc, :], [yg], [ygb])
            h.tt("dve", q1[:], gre_e[:], cS[:], ALU.mult, [gre_e, cS], [q1])
            h.tt("dve", q2[:], gim_e[:], nsS[:], ALU.mult, [gim_e, nsS], [q2])
            h.tt("dve", ire[:], q1[:], q2[:], ALU.add, [q1, q2], [ire])
            h.tt("dve", q1[:], gre_e[:], sS[:], ALU.mult, [gre_e, sS], [q1])
            h.tt("dve", q2[:], gim_e[:], cS[:], ALU.mult, [gim_e, cS], [q2])
            h.tt("dve", iim[:], q1[:], q2[:], ALU.add, [q1, q2], [iim])
            for oc in range(2):
                h.mm([(Pg[:], wglu[:, kc, oc * 128:(oc + 1) * 128], ygb[:, kc, :], kc == 0, kc == 1) for kc in range(2)], [wglu, ygb], [Pg])
                h.act(sg[:, oc, :], Pg[:], AF.Sigmoid, [Pg], [sg])
                h.tt("pool", y2[:, oc, :], yg[:, oc, :], sg[:, oc, :], ALU.mult, [yg, sg], [y2])
                h.tt("pool", sg[:, oc, :], y2[:, oc, :], y2[:, oc, :], ALU.mult, [y2], [sg])
            h.mm([(Pt[:], onesf[:], sg[:, oc, :], oc == 0, oc == 1) for oc in range(2)], [onesf, sg], [Pt])
            h.act(rstd[:], Pt[:], AF.Sqrt, [Pt, epsb], [rstd], bias=epsb[:, 0:1], scale=1.0 / 256)
            h.recip(rstd[:], rstd[:], [rstd], [rstd])
            for oc in range(2):
                h.stt("dve", yo_[:, oc, :], y2[:, oc, :], nw5[:, oc:oc + 1], rstd[:], ALU.mult, ALU.mult, [y2, nw5, rstd], [yo_])
            h.dma("sp", k.y_d.t[0:256, cols].rearrange("(j p) t -> p j t", p=128), yo_[:], [yo_], [k.r_y[0][mt]], yo_)
        P.end_phase()


def gdn_phase(k, l):
    P, T, h, inp = k.P, k.T, k.h, k.inp
    NMT = T // 512
    MT = 512
    identf, identb = k.identf, k.identb
    with contextlib.ExitStack() as es:
        masks = make_masks(h, P, es)
        b12, na12 = gate_consts(k, es, l, "gd")
        gnw = P.sb(es, "gd_gnw", [128, 64], F32)
        k.load_row(gnw, inp["gdn_norm"].t[l:l + 1, :])
        Sf = P.sb(es, "gd_Sf", [128, 3, 128], F32)
        Sb = P.sb(es, "gd_Sb", [128, 3, 128], BF16)
        h.memset("pool", Sf[:], 0.0, [Sf])
        h.memset("pool", Sb[:], 0.0, [Sb])
        qnT = [P.sb(es, f"gd_qn{i}", [128, 3, MT], BF16) for i in range(2)]
        knT = [P.sb(es, f"gd_kn{i}", [128, 3, MT], BF16) for i in range(2)]
        vT = [P.sb(es, f"gd_vT{i}", [128, 3, MT], F32) for i in range(2)]
        pj = [P.sb(es, f"gd_pj{i}", [128, 4, 786], F32) for i in range(2)]
        yo = [P.sb(es, f"gd_yo{i}", [128, 3, MT], BF16) for i in range(2)]
        R_sp12 = Rot(P, es, "gd_sp12", [128, 12], F32)
        R_gda = Rot(P, es, "gd_gda", [128, 12], F32)
        R_cs12 = Rot(P, es, "gd_cs12", [128, 12], F32)
        R_cl12 = Rot(P, es, "gd_cl12", [128, 12], F32)
        R_beta = Rot(P, es, "gd_beta", [128, 6], F32)
        R_nbeta = Rot(P, es, "gd_nbeta", [128, 6], F32)
        R_egc = Rot(P, es, "gd_egc", [128, 6], F32)
        R_t6 = Rot(P, es, "gd_t6", [128, 6], F32)
        R_dkk = Rot(P, es, "gd_dkk", [128, 6], F32)
        R_gtot = Rot(P, es, "gd_gtot", [128, 6], F32)
        R_gtc = Rot(P, es, "gd_gtc", [128, 3], F32)
        R_dg = Rot(P, es, "gd_dg", [128, 6, 128], F32)
        R_arg = Rot(P, es, "gd_arg", [128, 6, 128], F32)
        R_E = Rot(P, es, "gd_E", [128, 6, 128], F32)
        R_EU = Rot(P, es, "gd_EU", [128, 6, 128], F32)
        R_ELn = Rot(P, es, "gd_ELn", [128, 6, 128], F32)
        R_attnT = Rot(P, es, "gd_attnT", [128, 6, 128], BF16)
        R_Pm = [Rot(P, es, f"gd_Pm{i}", [128, 6, 128], BF16) for i in range(2)]
        R_Qm = [Rot(P, es, f"gd_Qm{i}", [128, 6, 128], BF16) for i in range(2)]
        sp_fns = [softplus12(h, P, es, f"gd{i}") for i in range(2)]
        R_Xb = Rot(P, es, "gd_Xb", [128, 6, 128], BF16)
        R_kdec = Rot(P, es, "gd_kdec", [128, 384], BF16)
        R_v_tm = Rot(P, es, "gd_vtm", [128, 384], F32)
        R_rr_ = Rot(P, es, "gd_rr", [128, 384], F32)
        R_rb = Rot(P, es, "gd_rb", [128, 384], BF16)
        R_vnb = Rot(P, es, "gd_vnb", [128, 384], BF16)
        R_oa = Rot(P, es, "gd_oa", [128, 384], F32)
        R_o = Rot(P, es, "gd_o", [128, 384], F32)
        R_sq = Rot(P, es, "gd_sq", [128, 384], F32)
        R_ss6 = Rot(P, es, "gd_ss6", [128, 6], F32)
        R_rs6 = Rot(P, es, "gd_rs6", [128, 6], F32)
        R_zg = Rot(P, es, "gd_zg", [128, 384], F32)
        R_yb = Rot(P, es, "gd_yb", [128, 384], BF16)
        R_tmpS = Rot(P, es, "gd_tmpS", [128, 3, 128], F32)
        W0 = P.ps(es, "gd_W0", [128, 2, 512], F32)
        W1 = P.ps(es, "gd_W1", [128, 2, 512], F32)
        W2 = P.ps(es, "gd_W2", [128, 2, 512], F32)
        PB = P.ps(es, "gd_PB", [128, 1024], BF16)
        pA = P.ps(es, "gd_pA", [128, 32], F32)
        x3 = lambda ap: ap.rearrange("p (h d) -> p h d", h=6)
        kz = [P.sb(es, f"gd_kz{i}", [128, 3, MT], BF16) for i in range(2)]
        R_Tb = Rot(P, es, "gd_Tb", [128, 6, 128], BF16)
        R_M1b = Rot(P, es, "gd_M1b", [128, 6, 128], BF16)
        R_M1pb = Rot(P, es, "gd_M1pb", [128, 6, 128], BF16)
        cmask = P.sb(es, "gd_cmask", [128, 14, 128], BF16)
        h.dma("pool", cmask[:], inp["gdn_cmask"].t.rearrange("m p j -> p m j"), [], [cmask], cmask)
        rmask = P.sb(es, "gd_rmask", [128, 2], F32)
        h.memset("pool", rmask[:], 0.0, [rmask])
        h.memset("pool", rmask[0:64, 0:1], 1.0, [rmask])
        h.memset("pool", rmask[64:128, 1:2], 1.0, [rmask])

        def headmm(Wps, lh, rh, R):
            w = w4(Wps)
            h.mm([(w[:, hd // 3, hd % 3, :], lh[:, hd, :], rh[:, hd, :], True, True) for hd in range(6)], R, [Wps])

        stop = k.flags.get("gdn_stop", 99)
        for mt in range(NMT):
            cols = slice(mt * MT, (mt + 1) * MT)
            i2 = mt % 2
            rp = [k.r_pre[mt]]
            h.dma("sp", qnT[i2][:], k.qn_d.t[:, cols].rearrange("(j p) t -> p j t", p=128), rp, [qnT[i2]], qnT[i2])
            h.dma("sp", knT[i2][:], k.kn_d.t[:, cols].rearrange("(j p) t -> p j t", p=128), rp, [knT[i2]], knT[i2])
            h.dma("sp", vT[i2][:], k.v_d.t[:, cols].rearrange("(j p) t -> p j t", p=128), rp, [vT[i2]], vT[i2])
            h.dma("sp", pj[i2][:], k.pt_d.t[mt * MT:(mt + 1) * MT, :].rearrange("(a p) n -> p a n", p=128), rp, [pj[i2]], pj[i2])
            qn_, kn_, v_, pj_, yo_ = qnT[i2], knT[i2], vT[i2], pj[i2], yo[i2]
            for s_ in range(2):
                h.ts("dve", kz[s_][:], kn_[:], rmask[:, s_:s_ + 1], None, ALU.mult, None, [kn_, rmask], [kz[s_]])
            for ti in range(4):
                tc = slice(ti * 128, (ti + 1) * 128)
                tix = mt * 4 + ti
                sp12 = R_sp12.at(tix); gda = R_gda.at(tix); cs12 = R_cs12.at(tix); cl12 = R_cl12.at(tix); beta = R_beta.at(tix); nbeta = R_nbeta.at(tix); egc = R_egc.at(tix); t6 = R_t6.at(tix); dkk = R_dkk.at(tix); gtot = R_gtot.at(tix); gtc = R_gtc.at(tix); dg = R_dg.at(tix); arg = R_arg.at(tix); E = R_E.at(tix); EU = R_EU.at(tix); ELn = R_ELn.at(tix); attnT = R_attnT.at(tix); Xb = R_Xb.at(tix); kdec = R_kdec.at(tix); v_tm = R_v_tm.at(tix); rr_ = R_rr_.at(tix); rb = R_rb.at(tix); vnb = R_vnb.at(tix); oa = R_oa.at(tix); o = R_o.at(tix); sq = R_sq.at(tix); ss6 = R_ss6.at(tix); rs6 = R_rs6.at(tix); zg = R_zg.at(tix); yb = R_yb.at(tix); tmpS = R_tmpS.at(tix); Tb = R_Tb.at(tix); M1b = R_M1b.at(tix); M1pb = R_M1pb.at(tix)
                Pm = [r.at(tix) for r in R_Pm]; Qm = [r.at(tix) for r in R_Qm]; sp_fn = sp_fns[tix % 2]
                gate_tile(k, sp_fn, pj_[:, ti, 768:780], pj_, b12, na12, masks, sp12, gda, cs12, cl12, pA)
                gc = cs12[:, 0:6]
                h.act(beta[:], pj_[:, ti, 780:786], AF.Sigmoid, [pj_], [beta])
                h.ts("dve", nbeta[:], beta[:], -1.0, None, ALU.mult, None, [beta], [nbeta])
                h.act(egc[:], gc, AF.Exp, [cs12], [egc])
                h.tt("dve", t6[:], cl12[:, 0:6], gc, ALU.subtract, [cl12, cs12], [t6])
                h.act(dkk[:], t6[:], AF.Exp, [t6], [dkk])
                h.act(gtot[:], cl12[:, 0:6], AF.Exp, [cl12], [gtot])
                g2 = gtot[:].rearrange("p (j s) -> p j s", s=2)
                h.cp("dve", gtc[0:64, :], g2[0:64, :, 0], [gtot], [gtc])
                h.cp("dve", gtc[64:128, :], g2[64:128, :, 1], [gtot], [gtc])
                if stop < 1:
                    h.memset("pool", yo_[:, :, tc], 0.0, [yo_])
                    continue
                h.tt("pool", dg[:], b3(identf[:], 6, 128), s3(gc, 6, 128), ALU.mult, [identf, cs12], [dg])
                h.mm([(W0[:, 0, 0:384], masks["ones"][:], dg[:, 0:3, :].rearrange("p a l -> p (a l)"), True, True),
                      (W0[:, 1, 0:384], masks["ones"][:], dg[:, 3:6, :].rearrange("p a l -> p (a l)"), True, True)],
                     [masks["ones"], dg], [W0])
                if stop < 1.2:
                    h.memset("pool", yo_[:, :, tc], 0.0, [yo_])
                    continue
                h.tt("dve", v4(arg[:]), w4(W0), s4(gc), ALU.subtract, [W0, cs12], [arg])
                if stop < 1.4:
                    h.memset("pool", yo_[:, :, tc], 0.0, [yo_])
                    continue
                h.act(arg[:], arg[:], AF.Abs, [arg], [arg])
                h.act(E[:], arg[:], AF.Exp, [arg], [E], scale=-1.0)
                if stop < 1.6:
                    h.memset("pool", yo_[:, :, tc], 0.0, [yo_])
                    continue
                h.tt("pool", EU[:], E[:], b3(masks["U"][:], 6, 128), ALU.mult, [E, masks["U"]], [EU])
                if stop < 1.8:
                    h.memset("pool", yo_[:, :, tc], 0.0, [yo_])
                    continue
                h.tt("pool", ELn[:], E[:], b3(masks["Ls"][:], 6, 128), ALU.mult, [E, masks["Ls"]], [ELn])
                if stop < 1.9:
                    h.memset("pool", yo_[:, :, tc], 0.0, [yo_])
                    continue
                h.tt("dve", ELn[:], ELn[:], s3(nbeta[:], 6, 128), ALU.mult, [ELn, nbeta], [ELn])
                if stop < 2:
                    h.memset("pool", yo_[:, :, tc], 0.0, [yo_])
                    continue
                w1 = w4(W1)
                w2 = w4(W2)
                h.mm([(w1[:, hd // 3, hd % 3, :], kz[hd % 2][:, hd // 2, tc], kn_[:, hd // 2, tc], True, True) for hd in range(6)],
                     [kz[0], kz[1], kn_], [W1])
                h.mm([(w2[:, hd // 3, hd % 3, :], kz[hd % 2][:, hd // 2, tc], qn_[:, hd // 2, tc], True, True) for hd in range(6)],
                     [kz[0], kz[1], qn_], [W2])
                h.tt("dve", v4(Pm[0][:]), w1, v4(ELn[:]), ALU.mult, [W1, ELn], [Pm[0]])
                h.tt("dve", v4(attnT[:]), w2, v4(EU[:]), ALU.mult, [W2, EU], [attnT])
                if stop < 3:
                    h.memset("pool", yo_[:, :, tc], 0.0, [yo_])
                    continue
                h.tr([(PB[:, hd * 128:(hd + 1) * 128], Pm[0][:, hd, :]) for hd in range(6)], identb[:], [Pm[0], identb], [PB])
                h.cp("act", Qm[0][:], PB[:, 0:768].rearrange("p (a l) -> p a l", a=6), [PB], [Qm[0]])
                Nn, NT_ = Pm[0], Qm[0]
                h.tt("pool", Pm[1][:], Nn[:], b3(cmask[:, 0, :], 6, 128), ALU.mult, [Nn, cmask], [Pm[1]])
                h.tt("pool", Qm[1][:], NT_[:], b3(cmask[:, 7, :], 6, 128), ALU.mult, [NT_, cmask], [Qm[1]])
                h.tt("dve", Tb[:], Pm[1][:], b3(identf[:], 6, 128), ALU.add, [Pm[1], identf], [Tb])
                h.tt("dve", Xb[:], Qm[1][:], b3(identf[:], 6, 128), ALU.add, [Qm[1], identf], [Xb])
                for lv in range(1, 7):
                    h.tt("pool", Pm[1][:], Nn[:], b3(cmask[:, lv, :], 6, 128), ALU.mult, [Nn, cmask], [Pm[1]])
                    h.tt("pool", Qm[1][:], NT_[:], b3(cmask[:, 7 + lv, :], 6, 128), ALU.mult, [NT_, cmask], [Qm[1]])
                    headmm(W1, Qm[1], Tb, [Qm[1], Tb])
                    headmm(W2, Pm[1], Xb, [Pm[1], Xb])
                    h.cp("act", v4(M1b[:]), w4(W1), [W1], [M1b])
                    h.cp("dve", v4(M1pb[:]), w4(W2), [W2], [M1pb])
                    headmm(W1, Xb, M1b, [Xb, M1b])
                    headmm(W2, Tb, M1pb, [Tb, M1pb])
                    h.tt("dve", v4(Tb[:]), v4(Tb[:]), w4(W1), ALU.add, [Tb, W1], [Tb])
                    h.tt("dve", v4(Xb[:]), v4(Xb[:]), w4(W2), ALU.add, [Xb, W2], [Xb])
                if stop < 4:
                    h.memset("pool", yo_[:, :, tc], 0.0, [yo_])
                    continue
                h.tr([(PB[:, j * 128:(j + 1) * 128], kn_[:, j, tc]) for j in range(3)], identb[:], [kn_, identb], [PB])
                h.tt("dve", x3(kdec[:]), x3(PB[:, 0:384]), s3(dkk[:], 6, 64), ALU.mult, [PB, dkk], [kdec])
                h.tr([(W0[:, 0, j * 128:(j + 1) * 128], v_[:, j, tc]) for j in range(3)], identf[:], [v_, identf], [W0])
                h.cp("act", v_tm[:], W0[:, 0, 0:384], [W0], [v_tm])
                if stop < 5:
                    h.memset("pool", yo_[:, :, tc], 0.0, [yo_])
                    continue
                h.mm([(W1[:, 0, j * 128:(j + 1) * 128], kn_[:, j, tc], Sb[:, j, :], True, True) for j in range(3)], [kn_, Sb], [W1])
                h.tt("dve", x3(rr_[:]), x3(W1[:, 0, 0:384]), s3(egc[:], 6, 64), ALU.mult, [W1, egc], [rr_])
                h.tt("pool", rr_[:], rr_[:], v_tm[:], ALU.subtract, [rr_, v_tm], [rr_])
                h.tt("dve", x3(rb[:]), x3(rr_[:]), s3(nbeta[:], 6, 64), ALU.mult, [rr_, nbeta], [rb])
                h.mm([(W2[:, 0, hd * 64:(hd + 1) * 64], Xb[:, hd, :], rb[:, hd * 64:(hd + 1) * 64], True, True) for hd in range(6)], [Xb, rb], [W2])
                h.cp("act", vnb[:], W2[:, 0, 0:384], [W2], [vnb])
                h.mm([(W1[:, 0, j * 128:(j + 1) * 128], qn_[:, j, tc], Sb[:, j, :], True, True) for j in range(3)], [qn_, Sb], [W1])
                h.tt("dve", x3(oa[:]), x3(W1[:, 0, 0:384]), s3(egc[:], 6, 64), ALU.mult, [W1, egc], [oa])
                h.mm([(W2[:, 0, hd * 64:(hd + 1) * 64], attnT[:, hd, :], vnb[:, hd * 64:(hd + 1) * 64], True, True) for hd in range(6)], [attnT, vnb], [W2])
                h.tt("dve", o[:], W2[:, 0, 0:384], oa[:], ALU.add, [W2, oa], [o])
                h.mm([(W0[:, 0, j * 128:(j + 1) * 128], kdec[:, j * 128:(j + 1) * 128], vnb[:, j * 128:(j + 1) * 128], True, True) for j in range(3)],
                     [kdec, vnb], [W0])
                h.tt("dve", tmpS[:], W0[:, 0, 0:384].rearrange("p (j c) -> p j c", j=3), b3(masks["bd"][:], 3, 128), ALU.mult, [W0, masks["bd"]], [tmpS])
                h.tt("dve", Sf[:], Sf[:], s3(gtc[:], 3, 128), ALU.mult, [Sf, gtc], [Sf])
                h.tt("pool", Sf[:], Sf[:], tmpS[:], ALU.add, [Sf, tmpS], [Sf])
                h.cp("act", Sb[:], Sf[:], [Sf], [Sb])
                if stop < 6:
                    h.memset("pool", yo_[:, :, tc], 0.0, [yo_])
                    continue
                h.tt("pool", sq[:], o[:], o[:], ALU.mult, [o], [sq])
                h.reduce(ss6[:], x3(sq[:]), ALU.add, [sq], [ss6])
                h.ts("dve", rs6[:], ss6[:], 1.0 / 64, EPS, ALU.mult, ALU.add, [ss6], [rs6])
                h.act(rs6[:], rs6[:], AF.Sqrt, [rs6], [rs6])
                h.recip(rs6[:], rs6[:], [rs6], [rs6])
                h.tt("dve", x3(o[:]), x3(o[:]), s3(rs6[:], 6, 64), ALU.mult, [o, rs6], [o])
                h.tt("dve", x3(o[:]), x3(o[:]), b3(gnw[:], 6, 64), ALU.mult, [o, gnw], [o])
                h.act(zg[:], pj_[:, ti, 0:384], AF.Silu, [pj_], [zg])
                h.tt("pool", yb[:], o[:], zg[:], ALU.mult, [o, zg], [yb])
                h.tr([(PB[:, j * 128:(j + 1) * 128], yb[:, j * 128:(j + 1) * 128]) for j in range(3)], identb[:], [yb, identb], [PB])
                h.cp("act", yo_[:, :, tc], PB[:, 0:384].rearrange("p (j t) -> p j t", j=3), [PB], [yo_])
            h.dma("sp", k.y_d.t[256:640, cols].rearrange("(j p) t -> p j t", p=128), yo_[:], [yo_], [k.r_y[1][mt]], yo_)
        P.end_phase()


class K:
    def __init__(self, T, L, flags):
        self.T = T
        self.L = L
        self.NT = T // 128
        self.flags = flags


def bcast(ap, shape):
    return ap.to_broadcast(list(shape))


def build(T, L, flags=None):
    flags = flags or {}
    nc = bass.Bass("TRN2", target_bir_lowering=False)
    k = K(T, L, flags)
    NT = T // 128
    with contextlib.ExitStack() as es0:
        P = Prog(nc, es0)
        k.P = P
        inp = {}

        def ein(name, shape):
            inp[name] = P.dram(name, shape, F32, "ExternalInput")
            return inp[name]

        x_in = ein("x", [T, D])
        c_in = ein("c", [1, D])
        w_ada = ein("w_ada", [L, D, 6 * D])
        b_ada = ein("b_ada", [L, 6 * D])
        norm_mix = ein("norm_mix", [L, D])
        norm_ffn = ein("norm_ffn", [L, D])
        norm_final = ein("norm_final", [1, D])
        w_rt = ein("w_rt", [L, D, 36])
        b_rt = ein("b_rt", [L, 36])
        w_gate = ein("moe_w_gate", [L, NEXP, D, DEXP])
        w_up = ein("moe_w_up", [L, NEXP, D, DEXP])
        w_down = ein("moe_w_down", [L, NEXP, DEXP, D])
        ein("w_in_f", [L, D, 2304])
        ein("w_in_t", [L, D, 786])
        ein("w_out", [L, D, D])
        ein("conv_w", [L, 128, 16, 4])
        ein("conv_b", [L, 128, 16])
        ein("bias12", [L, 12])
        ein("alog12", [L, 12])
        ein("ssd_d", [L, 6])
        ein("ssd_norm", [L, 384])
        ein("gdn_norm", [L, 64])
        ein("gdn_cmask", [14, 128, 128])
        ein("s5_are", [L, 128, 8])
        ein("s5_aim", [L, 128, 8])
        ein("s5_ldt", [L, 128, 8])
        ein("s5_dcol", [L, 128, 2])
        ein("s5_ncol", [L, 128, 2])
        ein("s5_bT_re", [L, 8, 128, 128])
        ein("s5_bT_im", [L, 8, 128, 128])
        ein("s5_cT_re", [L, 8, 128, 128])
        ein("s5_cT_im", [L, 8, 128, 128])
        ein("s5_w_glu", [L, 256, 256])
        out = P.dram("out", [T, D], F32, "ExternalOutput")
        k.u_d = P.dram("u_d", [256, T], F32)
        k.qn_d = P.dram("qn_d", [384, T], BF16)
        k.kn_d = P.dram("kn_d", [384, T], BF16)
        k.v_d = P.dram("v_d", [384, T], F32)
        k.xs_d = P.dram("xs_d", [384, T], F32)
        k.B_d = P.dram("B_d", [256, T], BF16)
        k.C_d = P.dram("C_d", [256, T], BF16)
        k.pt_d = P.dram("pt_d", [T, 786], F32)
        k.y_d = P.dram("y_d", [D, T], BF16)
        k.r_pre = [P.region(f"pre_{i}") for i in range(max(1, T // 512))]
        k.r_y = [[P.region(f"y{j}_{i}") for i in range(max(1, T // 512))] for j in range(3)]
        k.h = H(P)
        h = k.h
        modv = P.dram("modv", [L, 6 * D], F32)
        dbg = flags.get("dbg", False)
        if dbg:
            dbg_coef = P.dram("dbg_coef", [T, 32], F32, "ExternalOutput")
            dbg_y = P.dram("dbg_y", [T, D], F32, "ExternalOutput")
            dbg_h = P.dram("dbg_h", [T, D], F32, "ExternalOutput")
            r_dbg = P.region("dbg")
        scr = [P.dram("xs0", [T, D], F32), P.dram("xs1", [T, D], F32)]
        k.inp = inp

        def regs(name):
            return [P.region(f"{name}_{i}") for i in range(NT)]
        r_x = regs("x")
        r_scr = [regs("xs0"), regs("xs1")]
        r_out = regs("out")
        r_modv = P.region("modv")

        identf = P.sb(es0, "identf", [128, 128], F32)
        identb = P.sb(es0, "identb", [128, 128], BF16)
        P.op("pool", lambda e: e.memset(identf[:], 0.0), writes=[identf])
        P.op("pool", lambda e: e.affine_select(out=identf[:], in_=identf[:], pattern=[[-1, 128]],
                                               compare_op=ALU.not_equal, fill=1.0, base=0,
                                               channel_multiplier=1),
             reads=[identf], writes=[identf])
        P.op("dve", lambda e: e.tensor_copy(out=identb[:], in_=identf[:]), reads=[identf], writes=[identb])
        k.identf, k.identb = identf, identb

        with contextlib.ExitStack() as es:
            ccol = P.sb(es, "ccol", [128, 8], F32)
            cb = P.sb(es, "cb", [128, 8, 128], BF16)
            wa = [P.sb(es, f"wa{i}", [128, 8, 512], BF16) for i in range(2)]
            pm = [P.ps(es, f"pm{i}", [128, 512], F32) for i in range(2)]
            brow = [P.sb(es, f"brow{i}", [1, 512], F32) for i in range(2)]
            mrow = [P.sb(es, f"mrow{i}", [1, 512], F32) for i in range(2)]
            P.op("sp", lambda e: e.dma_start(out=ccol[:], in_=c_in.t.rearrange("o (k p) -> p (o k)", p=128),
                                             allow_slow_non_contiguous=True),
                 writes=[ccol], dma=ccol)
            P.op("act", lambda e: e.activation(out=ccol[:], in_=ccol[:], func=AF.Silu), reads=[ccol], writes=[ccol])
            P.op("dve", lambda e: e.tensor_copy(out=cb[:], in_=bcast(ccol[:].unsqueeze(2), [128, 8, 128])),
                 reads=[ccol], writes=[cb])
            it = 0
            for l in range(L):
                for n in range(12):
                    i = it % 2
                    it += 1
                    P.op("pool", lambda e, l=l, n=n, i=i: e.dma_start(
                        out=wa[i][:], in_=w_ada.t[l, :, n * 512:(n + 1) * 512].rearrange("(k p) n -> p k n", p=128)),
                        writes=[wa[i]], dma=wa[i])
                    P.op("sp", lambda e, l=l, n=n, i=i: e.dma_start(
                        out=brow[i][:], in_=b_ada.t[l:l + 1, n * 512:(n + 1) * 512]),
                        writes=[brow[i]], dma=brow[i])

                    def mm(e, i=i):
                        r = None
                        for kk in range(8):
                            r = e.matmul(pm[i][:], lhsT=cb[:, kk, :], rhs=wa[i][:, kk, :], start=(kk == 0), stop=(kk == 7))
                        return r
                    P.op("pe", mm, reads=[cb, wa[i]], writes=[pm[i]])
                    P.op("dve", lambda e, i=i: e.tensor_tensor(out=mrow[i][:], in0=pm[i][0:1, :], in1=brow[i][:], op=ALU.add),
                         reads=[pm[i], brow[i]], writes=[mrow[i]])
                    P.op("sp", lambda e, l=l, n=n, i=i: e.dma_start(
                        out=modv.t[l:l + 1, n * 512:(n + 1) * 512], in_=mrow[i][:]),
                        reads=[mrow[i]], writes=[r_modv], dma=mrow[i])
            P.end_phase()

        def load_row(dst, src_ap, extra_reads=()):
            P.op("sp", lambda e: e.dma_start(out=dst[:], in_=src_ap.partition_broadcast(128)),
                 reads=list(extra_reads), writes=[dst], dma=dst)

        def norm_consts(es, l, nw, i_scale, i_shift, tag):
            A = P.sb(es, f"A{tag}", [128, D], F32)
            B = P.sb(es, f"B{tag}", [128, D], F32)
            W = P.sb(es, f"W{tag}", [128, D], F32)
            load_row(A, modv.t[l:l + 1, i_scale * D:(i_scale + 1) * D], [r_modv])
            load_row(B, modv.t[l:l + 1, i_shift * D:(i_shift + 1) * D], [r_modv])
            load_row(W, nw)
            P.op("dve", lambda e: e.scalar_tensor_tensor(out=A[:], in0=A[:], scalar=1.0, in1=W[:], op0=ALU.add, op1=ALU.mult),
                 reads=[A, W], writes=[A])
            return A, B

        def rms_mod(xt, A, B, hout, ssq, rstd, junk, tmp):
            P.op("act", lambda e: e.activation(out=junk[:], in_=xt[:], func=AF.Square, accum_out=ssq[:]),
                 reads=[xt], writes=[junk, ssq], est=0.9)
            P.op("dve", lambda e: e.tensor_scalar(out=rstd[:], in0=ssq[:], scalar1=1.0 / D, scalar2=EPS, op0=ALU.mult, op1=ALU.add),
                 reads=[ssq], writes=[rstd])
            P.op("act", lambda e: e.activation(out=rstd[:], in_=rstd[:], func=AF.Sqrt), reads=[rstd], writes=[rstd])
            P.op("dve", lambda e: e.reciprocal(out=rstd[:], in_=rstd[:]), reads=[rstd], writes=[rstd])
            if B is None:
                P.op("dve", lambda e: e.scalar_tensor_tensor(out=hout[:], in0=xt[:], scalar=rstd[:, 0:1], in1=A[:], op0=ALU.mult, op1=ALU.mult),
                     reads=[xt, rstd, A], writes=[hout], est=1.15)
            else:
                P.op("dve", lambda e: e.scalar_tensor_tensor(out=tmp[:], in0=xt[:], scalar=rstd[:, 0:1], in1=A[:], op0=ALU.mult, op1=ALU.mult),
                     reads=[xt, rstd, A], writes=[tmp], est=1.15)
                P.op("pool", lambda e: e.tensor_tensor(out=hout[:], in0=tmp[:], in1=B[:], op=ALU.add),
                     reads=[tmp, B], writes=[hout], est=1.3)

        k.modv, k.r_modv = modv, r_modv
        k.load_row, k.norm_consts, k.rms_mod = load_row, norm_consts, rms_mod
        cur = (x_in, r_x)
        nxt_i = 0

        def next_dst():
            nonlocal nxt_i
            d = (scr[nxt_i], r_scr[nxt_i])
            nxt_i ^= 1
            return d

        for l in range(L):
            if flags.get("mixer", True):
                dstt = next_dst()
                mixer_layer(k, l, cur, dstt)
                cur = dstt
            if flags.get("moe", True):
                src, rsrc = cur
                dst, rdst = next_dst()
                SBT = min(16, NT)
                with contextlib.ExitStack() as es:
                    A, B = norm_consts(es, l, norm_ffn.t[l:l + 1, :], 4, 3, "f")
                    G = P.sb(es, "Gf", [128, D], F32)
                    load_row(G, modv.t[l:l + 1, 5 * D:6 * D], [r_modv])
                    brt = P.sb(es, "brt", [128, 36], F32)
                    load_row(brt, b_rt.t[l:l + 1, :])
                    wrt = P.sb(es, "wrt", [128, 8, 36], F32)
                    P.op("sp", lambda e: e.dma_start(out=wrt[:], in_=w_rt.t[l].rearrange("(k p) n -> p k n", p=128)),
                         writes=[wrt], dma=wrt)
                    hT = P.sb(es, "hT", [128, 8, SBT * 128], BF16)
                    yacc = [P.sb(es, f"yacc{i}", [128, D], F32) for i in range(SBT)]
                    coef = P.sb(es, "coef", [128, SBT, 32], F32)
                    xt = [P.sb(es, f"xt{i}", [128, D], F32) for i in range(2)]
                    hf = P.sb(es, "hf", [128, D], F32)
                    tmp = P.sb(es, "tmpf", [128, D], F32)
                    junk = P.sb(es, "junkf", [128, D], F32)
                    hTf = P.sb(es, "hTf", [128, 8, 128], F32)
                    ssq = P.sb(es, "ssq", [128, 1], F32)
                    rstd = P.sb(es, "rstd", [128, 1], F32)
                    lg = P.sb(es, "lg", [128, 36], F32)
                    sm = P.sb(es, "sm", [128, 16], F32)
                    gm = P.sb(es, "gm", [128, 4], F32)
                    gex = P.sb(es, "gex", [128, 4], F32)
                    le4 = P.sb(es, "le4", [128, 4, 8], F32)
                    les = P.sb(es, "les", [128, 8], F32)
                    le2 = P.sb(es, "le2", [128, 8], F32)
                    mk1 = P.sb(es, "mk1", [128, 8], F32)
                    mk2 = P.sb(es, "mk2", [128, 8], F32)
                    csel = P.sb(es, "csel", [128, 8], F32)
                    wg = [P.sb(es, f"wg{i}", [128, 8, DEXP], BF16) for i in range(2)]
                    wu = [P.sb(es, f"wu{i}", [128, 8, DEXP], BF16) for i in range(2)]
                    wd = [P.sb(es, f"wd{i}", [128, 2, D], BF16) for i in range(2)]
                    sg = [P.sb(es, f"sg{i}", [128, 512], F32) for i in range(2)]
                    hid = [P.sb(es, f"hid{i}", [128, 2, 512], BF16) for i in range(2)]
                    ptr = P.ps(es, "ptr", [128, 8, 128], F32)
                    pg = [P.ps(es, f"pg{i}", [128, 512], F32) for i in range(2)]
                    pu = [P.ps(es, f"pu{i}", [128, 512], F32) for i in range(2)]
                    py = [P.ps(es, f"py{i}", [128, 512], F32) for i in range(2)]

                    wcnt = 0
                    for sb0 in range(0, NT, SBT):
                        for ti in range(SBT):
                            t = sb0 + ti
                            xb = xt[t % 2]
                            P.op("sp", lambda e, t=t, xb=xb: e.dma_start(out=xb[:], in_=src.t[t * 128:(t + 1) * 128, :]),
                                 reads=[rsrc[t]], writes=[xb], dma=xb)
                            rms_mod(xb, A, B, hf, ssq, rstd, junk, tmp)

                            if dbg and l == 0:
                                P.op("sp", lambda e, t=t: e.dma_start(out=dbg_h.t[t * 128:(t + 1) * 128, :], in_=hf[:]),
                                     reads=[hf], writes=[r_dbg], dma=hf)

                            def trf(e):
                                r = None
                                for kk in range(8):
                                    r = e.transpose(out=ptr[:, kk, :], in_=hf[:, kk * 128:(kk + 1) * 128], identity=identf[:])
                                return r
                            P.op("pe", trf, reads=[hf, identf], writes=[ptr])
                            P.op("act", lambda e: e.copy(out=hTf[:], in_=ptr[:]), reads=[ptr], writes=[hTf])
                            P.op("dve", lambda e, ti=ti: e.tensor_copy(out=hT[:, :, ti * 128:(ti + 1) * 128], in_=hTf[:]),
                                 reads=[hTf], writes=[hT])

                            def mrt(e):
                                r = None
                                for kk in range(8):
                                    r = e.matmul(ptr[:, 0, 0:36], lhsT=hTf[:, kk, :], rhs=wrt[:, kk, :], start=(kk == 0), stop=(kk == 7))
                                return r
                            P.op("pe", mrt, reads=[hTf, wrt], writes=[ptr])
                            P.op("dve", lambda e: e.tensor_tensor(out=lg[:], in0=ptr[:, 0, 0:36], in1=brt[:], op=ALU.add),
                                 reads=[ptr, brt], writes=[lg])
                            P.op("dve", lambda e: e.tensor_reduce(out=sm[:, 0:1], in_=lg[:, 0:4], axis=AX.X, op=ALU.max),
                                 reads=[lg], writes=[sm])
                            P.op("dve", lambda e: e.tensor_scalar(out=gm[:], in0=lg[:, 0:4], scalar1=sm[:, 0:1], scalar2=None, op0=ALU.is_equal),
                                 reads=[lg, sm], writes=[gm])
                            P.op("dve", lambda e: e.tensor_scalar(out=sm[:, 1:2], in0=sm[:, 0:1], scalar1=-1.0, scalar2=None, op0=ALU.mult),
                                 reads=[sm], writes=[sm])
                            P.op("act", lambda e: e.activation(out=gex[:], in_=lg[:, 0:4], func=AF.Exp, bias=sm[:, 1:2], accum_out=sm[:, 2:3]),
                                 reads=[lg, sm], writes=[gex, sm])
                            P.op("dve", lambda e: e.reciprocal(out=sm[:, 3:4], in_=sm[:, 2:3]), reads=[sm], writes=[sm])
                            P.op("dve", lambda e: e.tensor_tensor(out=le4[:], in0=lg[:, 4:36].rearrange("p (g e) -> p g e", g=4),
                                                                  in1=bcast(gm[:].unsqueeze(2), [128, 4, 8]), op=ALU.mult),
                                 reads=[lg, gm], writes=[le4])
                            P.op("dve", lambda e: e.tensor_reduce(out=les[:], in_=le4[:].rearrange("p g e -> p e g"), axis=AX.X, op=ALU.add),
                                 reads=[le4], writes=[les])
                            P.op("dve", lambda e: e.tensor_reduce(out=sm[:, 4:5], in_=les[:], axis=AX.X, op=ALU.max), reads=[les], writes=[sm])
                            P.op("dve", lambda e: e.tensor_scalar(out=mk1[:], in0=les[:], scalar1=sm[:, 4:5], scalar2=None, op0=ALU.is_equal),
                                 reads=[les, sm], writes=[mk1])
                            P.op("dve", lambda e: e.scalar_tensor_tensor(out=le2[:], in0=mk1[:], scalar=-1e30, in1=les[:], op0=ALU.mult, op1=ALU.add),
                                 reads=[mk1, les], writes=[le2])
                            P.op("dve", lambda e: e.tensor_reduce(out=sm[:, 5:6], in_=le2[:], axis=AX.X, op=ALU.max), reads=[le2], writes=[sm])
                            P.op("dve", lambda e: e.tensor_scalar(out=mk2[:], in0=le2[:], scalar1=sm[:, 5:6], scalar2=None, op0=ALU.is_equal),
                                 reads=[le2, sm], writes=[mk2])
                            P.op("dve", lambda e: e.tensor_tensor(out=sm[:, 6:7], in0=sm[:, 4:5], in1=sm[:, 5:6], op=ALU.subtract),
                                 reads=[sm], writes=[sm])
                            P.op("act", lambda e: e.activation(out=sm[:, 7:8], in_=sm[:, 6:7], func=AF.Sigmoid), reads=[sm], writes=[sm])
                            P.op("act", lambda e: e.activation(out=sm[:, 8:9], in_=sm[:, 6:7], func=AF.Sigmoid, scale=-1.0), reads=[sm], writes=[sm])
                            P.op("dve", lambda e: e.tensor_scalar(out=sm[:, 7:9], in0=sm[:, 7:9], scalar1=sm[:, 3:4], scalar2=None, op0=ALU.mult),
                                 reads=[sm], writes=[sm])
                            P.op("dve", lambda e: e.tensor_scalar(out=csel[:], in0=mk1[:], scalar1=sm[:, 7:8], scalar2=None, op0=ALU.mult),
                                 reads=[mk1, sm], writes=[csel])
                            P.op("dve", lambda e: e.scalar_tensor_tensor(out=csel[:], in0=mk2[:], scalar=sm[:, 8:9], in1=csel[:], op0=ALU.mult, op1=ALU.add),
                                 reads=[mk2, sm, csel], writes=[csel])
                            P.op("dve", lambda e, ti=ti: e.tensor_tensor(out=coef[:, ti, :].rearrange("p (g e) -> p g e", g=4),
                                                                         in0=bcast(gm[:].unsqueeze(2), [128, 4, 8]),
                                                                         in1=bcast(csel[:].unsqueeze(1), [128, 4, 8]), op=ALU.mult),
                                 reads=[gm, csel], writes=[coef])
                        nblk = (SBT * 128 + 511) // 512
                        seq = [(ex, blk) for ex in range(NEXP) for blk in range(nblk)]

                        def load_w(ex):
                            wi = ex % 2
                            h.dma("pool", wg[wi][:], w_gate.t[l, ex].rearrange("(k p) n -> p k n", p=128), [], [wg[wi]], wg[wi])
                            h.dma("pool", wu[wi][:], w_up.t[l, ex].rearrange("(k p) n -> p k n", p=128), [], [wu[wi]], wu[wi])
                            h.dma("pool", wd[wi][:], w_down.t[l, ex].rearrange("(k p) n -> p k n", p=128), [], [wd[wi]], wd[wi])

                        def GU(i):
                            ex, blk = seq[i]
                            wi, bi = ex % 2, i % 2
                            c0 = blk * 512
                            cw = min(512, SBT * 128 - c0)
                            for fc in range(2):
                                fs = slice(fc * 128, (fc + 1) * 128)
                                h.mm([(pg[fc][:, 0:cw], wg[wi][:, kk, fs], hT[:, kk, c0:c0 + cw], kk == 0, kk == 7) for kk in range(8)],
                                     [wg[wi], hT], [pg[fc]])
                                h.act(sg[fc][:, 0:cw], pg[fc][:, 0:cw], AF.Silu, [pg[fc]], [sg[fc]])
                                yield
                                h.mm([(pu[fc][:, 0:cw], wu[wi][:, kk, fs], hT[:, kk, c0:c0 + cw], kk == 0, kk == 7) for kk in range(8)],
                                     [wu[wi], hT], [pu[fc]])
                                h.tt("dve", hid[bi][:, fc, 0:cw], sg[fc][:, 0:cw], pu[fc][:, 0:cw], ALU.mult, [sg[fc], pu[fc]], [hid[bi]])
                                yield

                        def DN(i):
                            ex, blk = seq[i]
                            wi, bi = ex % 2, i % 2
                            c0 = blk * 512
                            cw = min(512, SBT * 128 - c0)
                            for st in range(cw // 128):
                                ti = blk * 4 + st
                                for n2 in range(2):
                                    pi = n2
                                    ns = slice(n2 * 512, (n2 + 1) * 512)
                                    h.mm([(py[pi][:], hid[bi][:, fc, st * 128:(st + 1) * 128], wd[wi][:, fc, ns], fc == 0, fc == 1) for fc in range(2)],
                                         [hid[bi], wd[wi]], [py[pi]])
                                    if ex == 0:
                                        h.ts("dve", yacc[ti][:, ns], py[pi][:], coef[:, ti, ex:ex + 1], None, ALU.mult, None, [py[pi], coef], [yacc[ti]])
                                    else:
                                        h.stt("dve", yacc[ti][:, ns], py[pi][:], coef[:, ti, ex:ex + 1], yacc[ti][:, ns], ALU.mult, ALU.add,
                                              [py[pi], coef, yacc[ti]], [yacc[ti]])
                                    yield
                            if blk == nblk - 1 and ex + 2 < NEXP:
                                load_w(ex + 2)

                        def drain(g):
                            for _ in g:
                                pass

                        def step(g, n):
                            for _ in range(n):
                                if next(g, "END") == "END":
                                    return

                        load_w(0)
                        load_w(1)
                        drain(GU(0))
                        for i in range(len(seq)):
                            gd = DN(i)
                            if i + 1 < len(seq):
                                gg = GU(i + 1)
                                for _ in range(4):
                                    step(gg, 1)
                                    step(gd, 2)
                                drain(gg)
                            drain(gd)
                        for ti in range(SBT):
                            t = sb0 + ti
                            if dbg and l == 0:
                                P.op("sp", lambda e, t=t, ti=ti: e.dma_start(out=dbg_y.t[t * 128:(t + 1) * 128, :], in_=yacc[ti][:]),
                                     reads=[yacc[ti]], writes=[r_dbg], dma=yacc[ti])
                                P.op("sp", lambda e, t=t, ti=ti: e.dma_start(out=dbg_coef.t[t * 128:(t + 1) * 128, :], in_=coef[:, ti, :]),
                                     reads=[coef], writes=[r_dbg], dma=coef)
                            xb = xt[t % 2]
                            P.op("sp", lambda e, t=t, xb=xb: e.dma_start(out=xb[:], in_=src.t[t * 128:(t + 1) * 128, :]),
                                 reads=[rsrc[t]], writes=[xb], dma=xb)
                            P.op("pool", lambda e, ti=ti: e.tensor_tensor(out=yacc[ti][:], in0=yacc[ti][:], in1=G[:], op=ALU.mult),
                                 reads=[yacc[ti], G], writes=[yacc[ti]])
                            P.op("dve", lambda e, ti=ti, xb=xb: e.tensor_tensor(out=yacc[ti][:], in0=yacc[ti][:], in1=xb[:], op=ALU.add),
                                 reads=[yacc[ti], xb], writes=[yacc[ti]])
                            P.op("sp", lambda e, t=t, ti=ti: e.dma_start(out=dst.t[t * 128:(t + 1) * 128, :], in_=yacc[ti][:]),
                                 reads=[yacc[ti]], writes=[rdst[t]], dma=yacc[ti])
                    P.end_phase()
                cur = (dst, rdst)

        src, rsrc = cur
        with contextlib.ExitStack() as es:
            Wn = P.sb(es, "Wn", [128, D], F32)
            load_row(Wn, norm_final.t[0:1, :])
            xt = [P.sb(es, f"xtn{i}", [128, D], F32) for i in range(2)]
            ho = [P.sb(es, f"hon{i}", [128, D], F32) for i in range(2)]
            junk = P.sb(es, "junkn", [128, D], F32)
            ssq = P.sb(es, "ssqn", [128, 1], F32)
            rstd = P.sb(es, "rstdn", [128, 1], F32)
            for t in range(NT):
                xb = xt[t % 2]
                hb = ho[t % 2]
                P.op("sp", lambda e, t=t, xb=xb: e.dma_start(out=xb[:], in_=src.t[t * 128:(t + 1) * 128, :]),
                     reads=[rsrc[t]], writes=[xb], dma=xb)
                rms_mod(xb, Wn, None, hb, ssq, rstd, junk, None)
                P.op("sp", lambda e, t=t, hb=hb: e.dma_start(out=out.t[t * 128:(t + 1) * 128, :], in_=hb[:]),
                     reads=[hb], writes=[r_out[t]], dma=hb)
            P.final_wait("sp", r_out)
            P.end_phase()
    return nc


def host_inputs(inputs, L, T):
    f = lambda a: np.ascontiguousarray(np.asarray(a, dtype=np.float32))
    w_rt = f(np.concatenate([inputs["moe_w_grp"][:L], inputs["moe_w_rt"][:L]], axis=-1))
    b_rt = f(np.concatenate([inputs["moe_b_grp"][:L], inputs["moe_b_rt"][:L]], axis=-1))
    w_in = np.asarray(inputs["w_in"][:L], dtype=np.float32)
    w_in_f = np.concatenate([w_in[:, :, 0:1408], w_in[:, :, 2188:3084]], axis=-1)
    w_in_t = np.concatenate([w_in[:, :, 1408:1792], w_in[:, :, 1804:2188], w_in[:, :, 1792:1798],
                             w_in[:, :, 3084:3090], w_in[:, :, 1798:1804]], axis=-1)
    gcw = np.asarray(inputs["gdn_conv_w"][:L], dtype=np.float32)
    scw = np.asarray(inputs["ssd_conv_w"][:L], dtype=np.float32)
    cw = np.concatenate([gcw, scw], axis=-1)
    conv_w = cw.reshape(L, 4, 16, 128).transpose(0, 3, 2, 1)
    cb = np.concatenate([np.zeros((L, 1152), np.float32), np.asarray(inputs["ssd_conv_b"][:L], dtype=np.float32)], axis=-1)
    conv_b = cb.reshape(L, 16, 128).transpose(0, 2, 1)
    bias12 = np.concatenate([inputs["gdn_dt_bias"][:L], inputs["ssd_dt_bias"][:L]], axis=-1)
    alog12 = np.concatenate([inputs["gdn_a_log"][:L], inputs["ssd_a_log"][:L]], axis=-1)
    def st_layout(a):
        a = np.asarray(a[:L], dtype=np.float32)
        return a.reshape(L, 8, 2, 64).transpose(0, 2, 3, 1).reshape(L, 128, 8)
    ldt = np.repeat(np.asarray(inputs["s5_log_dt"][:L], dtype=np.float32)[:, :, None], 64, axis=2)
    def bT_layout(b):
        b = np.asarray(b[:L], dtype=np.float32)
        o = np.zeros((L, 8, 128, 128), np.float32)
        for sc in range(8):
            for gl in range(2):
                r0 = 32 * (sc % 4) + 16 * gl
                o[:, sc, r0:r0 + 16, gl * 64:(gl + 1) * 64] = b[:, 2 * sc + gl].transpose(0, 2, 1)
        return o
    def cT_layout(c):
        c = np.asarray(c[:L], dtype=np.float32)
        o = np.zeros((L, 8, 128, 128), np.float32)
        for sc in range(8):
            for gl in range(2):
                r0 = 32 * (sc % 4) + 16 * gl
                o[:, sc, gl * 64:(gl + 1) * 64, r0:r0 + 16] = c[:, 2 * sc + gl].transpose(0, 2, 1)
        return o
    ii = np.arange(128)[:, None]
    jj = np.arange(128)[None, :]
    cm = []
    for lv in range(7):
        s_ = 1 << lv
        cm.append(((ii // (2 * s_) == jj // (2 * s_)) & (ii % (2 * s_) >= s_) & (jj % (2 * s_) < s_)).astype(np.float32))
    cmask = np.stack(cm + [m.T for m in cm], axis=0)
    col2 = lambda a: np.asarray(a[:L], dtype=np.float32).reshape(L, 2, 128).transpose(0, 2, 1)
    shared = {
        "gdn_cmask": f(cmask),
        "s5_are": f(st_layout(inputs["s5_a_re"])), "s5_aim": f(st_layout(inputs["s5_a_im"])), "s5_ldt": f(st_layout(ldt)),
        "s5_dcol": f(col2(inputs["s5_d"])), "s5_ncol": f(col2(inputs["s5_norm"])),
        "s5_bT_re": f(bT_layout(inputs["s5_b_re"])), "s5_bT_im": f(bT_layout(inputs["s5_b_im"])),
        "s5_cT_re": f(cT_layout(inputs["s5_c_re"])), "s5_cT_im": f(cT_layout(inputs["s5_c_im"])),
        "s5_w_glu": f(inputs["s5_w_glu"][:L]),
        "w_in_f": f(w_in_f), "w_in_t": f(w_in_t), "w_out": f(inputs["w_out"][:L]),
        "conv_w": f(conv_w), "conv_b": f(conv_b), "bias12": f(bias12), "alog12": f(alog12),
        "ssd_d": f(inputs["ssd_d"][:L]), "ssd_norm": f(inputs["ssd_norm"][:L]), "gdn_norm": f(inputs["gdn_norm"][:L]),
        "w_ada": f(inputs["w_ada"][:L]), "b_ada": f(inputs["b_ada"][:L]),
        "norm_mix": f(inputs["norm_mix"][:L]), "norm_ffn": f(inputs["norm_ffn"][:L]),
        "norm_final": f(inputs["norm_final"]).reshape(1, D),
        "w_rt": w_rt, "b_rt": b_rt,
        "moe_w_gate": f(inputs["moe_w_gate"][:L]), "moe_w_up": f(inputs["moe_w_up"][:L]),
        "moe_w_down": f(inputs["moe_w_down"][:L]),
    }
    maps = []
    B = inputs["x"].shape[0]
    for b in range(B):
        m = dict(shared)
        m["x"] = f(inputs["x"][b, :T])
        m["c"] = f(inputs["c"][b]).reshape(1, D)
        maps.append(m)
    return maps


def run(inputs, L, T, flags=None, trace=False):
    nc = build(T, L, flags)
    maps = host_inputs(inputs, L, T)
    res = run_bass_kernel_spmd(nc, maps, core_ids=list(range(len(maps))))
    if flags and flags.get("dbg"):
        return res.results
    return np.stack([r["out"] for r in res.results], axis=0)


def kernel(**inputs):
    return run(inputs, 4, 4096).astype(np.float32)
```

```python
import contextlib
import math
import numpy as np
import concourse.bass as bass
import concourse.mybir as mybir
from concourse.bass_utils import run_bass_kernel_spmd

F32 = mybir.dt.float32
BF16 = mybir.dt.bfloat16
ALU = mybir.AluOpType
AF = mybir.ActivationFunctionType
AX = mybir.AxisListType

D = 1024
NEXP = 32
DEXP = 256
EPS = 1e-6
ENGS = ("pe", "act", "dve", "pool", "sp")


class Buf:
    def __init__(self, t, name, multi=False):
        self.t = t
        self.name = name
        self.w = {}
        self.r = {}
        self.sem = None
        self.dcnt = 0
        self.multi = multi

    def __getitem__(self, k):
        return self.t[k]


class Prog:
    SEM_LAT = 0.15

    def __init__(self, nc, es):
        self.nc = nc
        self.es = es
        self.ops = []
        self.sems = []
        self.esem = {}
        self.ecnt = {e: 0 for e in ENGS}
        self.waited = {e: {} for e in ENGS}
        for e in ENGS:
            if e != "sp":
                self.esem[e] = self.newsem("e_" + e)
        self.uid = 0
        self.dsem_pool = []
        self.dbufs = []
        self.phase_bufs = []

    def newsem(self, name):
        s = self.es.enter_context(self.nc.semaphore(name))
        self.sems.append(s)
        return len(self.sems) - 1

    def sb(self, es, name, shape, dtype):
        self.uid += 1
        name = f"{name}_{self.uid}"
        t = es.enter_context(self.nc.sbuf_tensor(name, list(shape), dtype))
        b = Buf(t, name)
        self.phase_bufs.append(b)
        return b

    def ps(self, es, name, shape, dtype):
        self.uid += 1
        name = f"{name}_{self.uid}"
        t = es.enter_context(self.nc.psum_tensor(name, list(shape), dtype))
        return Buf(t, name)

    def dram(self, name, shape, dtype, kind="Internal"):
        t = self.nc.dram_tensor(name, list(shape), dtype, kind=kind).ap()
        return Buf(t, name)

    def region(self, name):
        return Buf(None, name, multi=True)

    def op(self, eng, fn, reads=(), writes=(), dma=None, est=None):
        if est is None:
            est = {"pe": 1.0, "act": 0.5, "dve": 0.35, "pool": 0.45, "sp": 3.0}[eng] if dma is None else 3.0
        self.ops.append((eng, fn, tuple(reads), tuple(writes), dma, est))

    def final_wait(self, eng, bufs):
        pass

    def end_phase(self):
        ops = self.ops
        self.ops = []
        n = len(ops)
        lw, rd = {}, {}
        deps = [None] * n
        for i, (eng, fn, reads, writes, dma, est) in enumerate(ops):
            d = set()
            for b in reads:
                d.update(lw.get(id(b), ()))
            for b in writes:
                d.update(rd.get(id(b), ()))
                if not b.multi:
                    d.update(lw.get(id(b), ()))
            deps[i] = d
            for b in reads:
                rd.setdefault(id(b), []).append(i)
            for b in writes:
                if b.multi:
                    lw.setdefault(id(b), []).append(i)
                else:
                    lw[id(b)] = [i]
                    rd[id(b)] = []
        import heapq
        succ = [[] for _ in range(n)]
        indeg = [0] * n
        for i in range(n):
            indeg[i] = len(deps[i])
            for j in deps[i]:
                succ[j].append(i)
        ready_t = [0.0] * n
        fin = [0.0] * n
        start = [0.0] * n
        efree = {e: 0.0 for e in ENGS}
        heap = [(0.0, i) for i in range(n) if indeg[i] == 0]
        heapq.heapify(heap)
        order = {e: [] for e in ENGS}
        glob = []
        while heap:
            rt, i = heapq.heappop(heap)
            eng, fn, reads, writes, dma, est = ops[i]
            st = max(rt, efree[eng])
            start[i] = st
            if dma is not None:
                occ = 0.5 if eng == "pool" else 0.08
                efree[eng] = st + occ
                fin[i] = st + occ + est
            else:
                efree[eng] = st + est
                fin[i] = st + est
            order[eng].append(i)
            glob.append(i)
            for k2 in succ[i]:
                indeg[k2] -= 1
                if ready_t[k2] < fin[i] + self.SEM_LAT:
                    ready_t[k2] = fin[i] + self.SEM_LAT
                if indeg[k2] == 0:
                    heapq.heappush(heap, (ready_t[k2], k2))
        assert len(glob) == n, "dependency cycle"
        tok = [None] * n
        for e in ENGS:
            if e == "sp":
                continue
        waits_raw = [None] * n
        cnt = dict(self.ecnt)
        for i in glob:
            eng, fn, reads, writes, dma, est = ops[i]
            w = {}
            for j in deps[i]:
                dj = ops[j][4]
                if dj is None:
                    s_, v_ = tok[j]
                else:
                    s_, v_ = dj.sem, dj.dcnt
                if w.get(s_, 0) < v_:
                    w[s_] = v_
            waits_raw[i] = w
            if dma is None:
                cnt[eng] += 1
                tok[i] = (self.esem[eng], cnt[eng])
            else:
                if dma.sem is None:
                    if self.dsem_pool:
                        dma.sem, dma.dcnt = self.dsem_pool.pop()
                    else:
                        dma.sem = self.newsem("d_" + dma.name)
                        dma.dcnt = 0
                    self.dbufs.append(dma)
                dma.dcnt += 16
                tok[i] = (dma.sem, dma.dcnt)
        self.ecnt = cnt
        nc = self.nc
        sems = self.sems
        qs = {}
        for e in ENGS:
            wd = self.waited[e]
            q = []
            for i in order[e]:
                ws = []
                for s_, v_ in waits_raw[i].items():
                    if wd.get(s_, 0) < v_:
                        ws.append((s_, v_))
                        wd[s_] = v_
                inc = (tok[i][0], 16 if ops[i][4] is not None else 1)
                q.append((ws, ops[i][1], inc))
            qs[e] = q
        toks = {}
        for e, s_ in self.esem.items():
            if self.ecnt[e] > 0:
                toks[s_] = self.ecnt[e]
        for b in self.dbufs:
            toks[b.sem] = max(toks.get(b.sem, 0), b.dcnt)
        for e in ENGS:
            wd = self.waited[e]
            ws = []
            for s_, v_ in toks.items():
                if wd.get(s_, 0) < v_:
                    ws.append((s_, v_))
                    wd[s_] = v_
            if ws:
                qs[e].append((ws, None, None))
        for b in self.phase_bufs:
            if b.sem is not None:
                self.dsem_pool.append((b.sem, b.dcnt))
                self.dbufs.remove(b)
                b.sem = None
        self.phase_bufs = []

        def mk(e):
            def f(eng):
                for waits, fn, inc in qs[e]:
                    for s_, v_ in waits:
                        eng.wait_ge(sems[s_], v_)
                    if fn is None:
                        continue
                    ins = fn(eng)
                    ins.then_inc(sems[inc[0]], inc[1])
            return f

        with nc.Block() as block:
            block.sync(mk("sp"))
            block.scalar(mk("act"))
            block.vector(mk("dve"))
            block.gpsimd(mk("pool"))
            block.tensor(mk("pe"))
        self.last_makespan = max(efree.values()) if n else 0.0
        if getattr(self, "verbose", False):
            busy = {e: 0.0 for e in ENGS}
            for i in range(n):
                if ops[i][4] is None:
                    busy[ops[i][0]] += ops[i][5]
            print(f"[phase] n_ops={n} model_makespan={self.last_makespan:.0f}us busy=" +
                  " ".join(f"{e}:{busy[e]:.0f}" for e in ENGS), flush=True)


def _fsz(ap):
    n = 1
    for d in ap.shape[1:]:
        n *= int(d)
    return n


def _est(eng, ap, psum=False):
    n = _fsz(ap)
    if eng == "dve":
        return (60 + n) / 960.0 + (0.06 if psum else 0.0)
    if eng == "act":
        return (220 + n) / 1400.0
    if eng == "pool":
        return (120 + n) / 900.0
    return 0.5


class H:
    def __init__(self, P):
        self.P = P

    def dma(self, eng, out, in_, R, W, buf, slow=False):
        nbytes = _fsz(out) * 128 * 4
        est = 2.0 + nbytes / 150000.0
        if slow:
            self.P.op(eng, lambda e: e.dma_start(out=out, in_=in_, allow_slow_non_contiguous=True), reads=R, writes=W, dma=buf, est=est)
        else:
            self.P.op(eng, lambda e: e.dma_start(out=out, in_=in_), reads=R, writes=W, dma=buf, est=est)

    def tt(self, eng, out, in0, in1, op, R, W):
        self.P.op(eng, lambda e: e.tensor_tensor(out=out, in0=in0, in1=in1, op=op), reads=R, writes=W, est=_est(eng, out))

    def ts(self, eng, out, in0, s1, s2, op0, op1, R, W):
        if s2 is None:
            self.P.op(eng, lambda e: e.tensor_scalar(out=out, in0=in0, scalar1=s1, scalar2=None, op0=op0), reads=R, writes=W, est=_est(eng, out))
        else:
            self.P.op(eng, lambda e: e.tensor_scalar(out=out, in0=in0, scalar1=s1, scalar2=s2, op0=op0, op1=op1), reads=R, writes=W, est=_est(eng, out))

    def stt(self, eng, out, in0, sc, in1, op0, op1, R, W):
        eng = "dve"
        self.P.op(eng, lambda e: e.scalar_tensor_tensor(out=out, in0=in0, scalar=sc, in1=in1, op0=op0, op1=op1), reads=R, writes=W, est=_est(eng, out))

    def act(self, out, in_, func, R, W, bias=None, scale=None, accum=None):
        kw = {}
        if bias is not None:
            kw["bias"] = bias
        if scale is not None:
            kw["scale"] = scale
        if accum is not None:
            kw["accum_out"] = accum
        self.P.op("act", lambda e: e.activation(out=out, in_=in_, func=func, **kw), reads=R, writes=W, est=_est("act", out))

    def cp(self, eng, out, in_, R, W):
        if eng == "act":
            self.P.op("act", lambda e: e.copy(out=out, in_=in_), reads=R, writes=W, est=_est("act", out))
        else:
            self.P.op(eng, lambda e: e.tensor_copy(out=out, in_=in_), reads=R, writes=W, est=_est(eng, out))

    def memset(self, eng, ap, val, W):
        self.P.op(eng, lambda e: e.memset(ap, val), writes=W, est=_est(eng, ap))

    def recip(self, out, in_, R, W):
        self.P.op("dve", lambda e: e.reciprocal(out=out, in_=in_), reads=R, writes=W, est=_est("dve", out))

    def reduce(self, out, in_, op, R, W):
        self.P.op("dve", lambda e: e.tensor_reduce(out=out, in_=in_, axis=AX.X, op=op), reads=R, writes=W, est=_est("dve", in_))

    def mm(self, items, R, W):
        est = 0.0
        for (o, l, rh, st, sp) in items:
            est += (max(64, _fsz(rh)) * (4 if l.dtype == F32 else 1)) / 2400.0 + 0.01
        est += 0.06

        def f(e):
            r = None
            for (o, l, rh, st, sp) in items:
                r = e.matmul(o, lhsT=l, rhs=rh, start=st, stop=sp)
            return r
        self.P.op("pe", f, reads=R, writes=W, est=est)

    def tr(self, items, ident, R, W):
        est = 0.06
        for (o, i) in items:
            est += (128 * (4 if i.dtype == F32 else 1)) / 2400.0 + 0.03

        def f(e):
            r = None
            for (o, i) in items:
                r = e.transpose(out=o, in_=i, identity=ident)
            return r
        self.P.op("pe", f, reads=R, writes=W, est=est)

    def select(self, out, in_, cmp, fill, base, cm, pattern, R, W):
        self.P.op("pool", lambda e: e.affine_select(out=out, in_=in_, pattern=pattern, compare_op=cmp, fill=fill,
                                                    base=base, channel_multiplier=cm), reads=R, writes=W, est=_est("pool", out))


class Rot:
    def __init__(self, P, es, name, shape, dtype, n=2):
        self.bufs = [P.sb(es, f"{name}r{i}", shape, dtype) for i in range(n)]

    def at(self, i):
        return self.bufs[i % len(self.bufs)]


def b3(ap, n, m):
    return ap.unsqueeze(1).to_broadcast([128, n, m])


def s3(ap, n, m):
    return ap.unsqueeze(2).to_broadcast([128, n, m])


def s4(ap):
    return ap.rearrange("p (b h) -> p b h", b=2).unsqueeze(3).to_broadcast([128, 2, 3, 128])


def v4(ap):
    return ap.rearrange("p (b h) l -> p b h l", b=2)


def w4(ps):
    return ps[:, :, 0:384].rearrange("p b (h l) -> p b h l", h=3)


def softplus12(h, P, es, tagp):
    xa = P.sb(es, tagp + "xa", [128, 12], F32)
    ax = P.sb(es, tagp + "ax", [128, 12], F32)
    ex = P.sb(es, tagp + "ex", [128, 12], F32)
    ln = P.sb(es, tagp + "ln", [128, 12], F32)
    one = P.sb(es, tagp + "one", [128, 1], F32)
    h.memset("pool", one[:], 1.0, [one])

    def f(xin, xin_buf, bias, out):
        h.tt("dve", xa[:], xin, bias[:], ALU.add, [xin_buf, bias], [xa])
        h.act(ax[:], xa[:], AF.Abs, [xa], [ax])
        h.act(ex[:], ax[:], AF.Exp, [ax], [ex], scale=-1.0)
        h.act(ln[:], ex[:], AF.Ln, [ex, one], [ln], bias=one[:, 0:1])
        h.ts("dve", xa[:], xa[:], 0.0, None, ALU.max, None, [xa], [xa])
        h.tt("dve", out[:], xa[:], ln[:], ALU.add, [xa, ln], [out])
    return f


def make_masks(h, P, es):
    m = {}
    ones = P.sb(es, "m_ones", [128, 128], F32)
    h.memset("pool", ones[:], 1.0, [ones])
    m["ones"] = ones
    for name, cmp, cm, st in (("U", ALU.is_ge, -1, 1), ("L", ALU.is_ge, 1, -1), ("Ls", ALU.is_gt, 1, -1)):
        t = P.sb(es, "m_" + name, [128, 128], F32)
        h.select(t[:], ones[:], cmp, 0.0, 0, cm, [[st, 128]], [ones], [t])
        m[name] = t
    sel = P.sb(es, "m_sel", [128, 128], F32)
    zer = P.sb(es, "m_zero", [128, 128], F32)
    h.memset("pool", zer[:], 0.0, [zer])
    h.select(sel[:], zer[:], ALU.not_equal, 1.0, -127, 1, [[0, 128]], [zer], [sel])
    m["sel"] = sel
    bd = P.sb(es, "m_bd", [128, 128], F32)
    h.memset("pool", bd[:], 0.0, [bd])
    h.memset("pool", bd[0:64, 0:64], 1.0, [bd])
    h.memset("pool", bd[64:128, 64:128], 1.0, [bd])
    m["bd"] = bd
    return m


def mixer_layer(k, l, cur, dstt):
    P, T, flags, h = k.P, k.T, k.flags, k.h
    inp = k.inp
    src, rsrc = cur
    dst, rdst = dstt
    NMT = T // 512
    MT = 512
    identf, identb = k.identf, k.identb
    u_d, qn_d, kn_d, v_d, xs_d, B_d, C_d, pt_d, y_d = k.u_d, k.qn_d, k.kn_d, k.v_d, k.xs_d, k.B_d, k.C_d, k.pt_d, k.y_d
    r_pre, r_y = k.r_pre, k.r_y

    with contextlib.ExitStack() as es:
        A, B = k.norm_consts(es, l, inp["norm_mix"].t[l:l + 1, :], 1, 0, "m")
        winf = P.sb(es, "winf", [128, 8, 2304], BF16)
        wint = P.sb(es, "wint", [128, 8, 786], BF16)
        for (c0, c1) in ((0, 1152), (1152, 2304)):
            h.dma("pool", winf[:, :, c0:c1], inp["w_in_f"].t[l, :, c0:c1].rearrange("(k p) n -> p k n", p=128), [], [winf], winf)
        h.dma("pool", wint[:], inp["w_in_t"].t[l].rearrange("(k p) n -> p k n", p=128), [], [wint], wint)
        cwt = P.sb(es, "cwt", [128, 16, 4], F32)
        cbt = P.sb(es, "cbt", [128, 16], F32)
        h.dma("sp", cwt[:], inp["conv_w"].t[l], [], [cwt], cwt)
        h.dma("sp", cbt[:], inp["conv_b"].t[l], [], [cbt], cbt)
        carry = P.sb(es, "carry", [128, 16, 3], F32)
        h.memset("pool", carry[:], 0.0, [carry])
        epsb = P.sb(es, "epsb", [128, 1], F32)
        h.memset("pool", epsb[:], EPS, [epsb])
        mhalf = P.sb(es, "mhalf", [128, MT], F32)
        h.memset("pool", mhalf[:], -0.5, [mhalf])
        bones = P.sb(es, "bones", [128, 128], F32)
        h.memset("pool", bones[:], 0.0, [bones])
        h.memset("pool", bones[0:64, 0:64], 1.0, [bones])
        h.memset("pool", bones[64:128, 64:128], 1.0, [bones])
        xt = [P.sb(es, f"m1x{i}", [128, D], F32) for i in range(2)]
        tmp = P.sb(es, "m1tmp", [128, D], F32)
        hb = P.sb(es, "m1hb", [128, D], BF16)
        hT = P.sb(es, "m1hT", [128, 8, MT], BF16)
        cin = [P.sb(es, f"cin{i}", [128, MT + 3], F32) for i in range(2)]
        acc = [P.sb(es, f"acc{i}", [128, MT], F32) for i in range(2)]
        so = [P.sb(es, f"so{i}", [128, MT], F32) for i in range(2)]
        sob = [P.sb(es, f"sob{i}", [128, MT], BF16) for i in range(2)]
        sq = P.sb(es, "m1sq", [128, MT], F32)
        rinv = P.sb(es, "m1rinv", [128, MT], F32)
        ptst = [P.sb(es, f"ptst{i}", [128, 786], F32) for i in range(2)]
        ssq = P.sb(es, "m1ssq", [128, 1], F32)
        rstd = P.sb(es, "m1rstd", [128, 1], F32)
        ptr = P.ps(es, "m1ptr", [128, 8, 128], BF16)
        pp = [P.ps(es, f"m1pp{i}", [128, MT], F32) for i in range(2)]
        pt = P.ps(es, "m1pt", [128, 2, 512], F32)
        pq = P.ps(es, "m1pq", [128, MT], F32)
        for mt in range(NMT):
            cols = slice(mt * MT, (mt + 1) * MT)
            for ti in range(4):
                t = mt * 4 + ti
                xb = xt[t % 2]
                h.dma("sp", xb[:], src.t[t * 128:(t + 1) * 128, :], [rsrc[t]], [xb], xb)
                k.rms_mod(xb, A, B, hb, ssq, rstd, tmp, tmp)
                h.tr([(ptr[:, kk, :], hb[:, kk * 128:(kk + 1) * 128]) for kk in range(8)], identb[:], [hb, identb], [ptr])
                h.cp("act", hT[:, :, ti * 128:(ti + 1) * 128], ptr[:], [ptr], [hT])
            for c in range(18):
                pb = pp[c % 2]
                h.mm([(pb[:], winf[:, kk, c * 128:(c + 1) * 128], hT[:, kk, :], kk == 0, kk == 7) for kk in range(8)], [winf, hT], [pb])
                if c < 2:
                    sb_ = so[c % 2]
                    h.cp("act", sb_[:], pb[:], [pb], [sb_])
                    h.dma("sp", u_d.t[c * 128:(c + 1) * 128, cols], sb_[:], [sb_], [r_pre[mt]], sb_)
                    continue
                ci = c - 2
                cb_ = cin[ci % 2]
                h.cp("pool", cb_[:, 0:3], carry[:, ci, :], [carry], [cb_])
                h.cp("act", cb_[:, 3:MT + 3], pb[:], [pb], [cb_])
                h.cp("pool", carry[:, ci, :], cb_[:, MT:MT + 3], [cb_], [carry])
                ab = acc[ci % 2]
                h.ts("dve", ab[:], cb_[:, 0:MT], cwt[:, ci, 0:1], None, ALU.mult, None, [cb_, cwt], [ab])
                for j in range(1, 4):
                    h.stt("dve", ab[:], cb_[:, j:j + MT], cwt[:, ci, j:j + 1], ab[:], ALU.mult, ALU.add, [cb_, cwt, ab], [ab])
                sb_ = so[ci % 2]
                h.act(sb_[:], ab[:], AF.Silu, [ab, cbt], [sb_], bias=cbt[:, ci:ci + 1])
                if ci < 6:
                    h.tt("pool", sq[:], sb_[:], sb_[:], ALU.mult, [sb_], [sq])
                    h.mm([(pq[:], bones[:], sq[:], True, True)], [bones, sq], [pq])
                    h.act(rinv[:], pq[:], AF.Sqrt, [pq, epsb], [rinv], bias=epsb[:, 0:1])
                    h.recip(rinv[:], rinv[:], [rinv], [rinv])
                    ob = sob[ci % 2]
                    if ci < 3:
                        h.stt("dve", ob[:], sb_[:], 0.125, rinv[:], ALU.mult, ALU.mult, [sb_, rinv], [ob])
                    else:
                        h.tt("dve", ob[:], sb_[:], rinv[:], ALU.mult, [sb_, rinv], [ob])
                    dd = qn_d if ci < 3 else kn_d
                    j = ci % 3
                    h.dma("sp", dd.t[j * 128:(j + 1) * 128, cols], ob[:], [ob], [r_pre[mt]], ob)
                elif ci < 12:
                    dd = v_d if ci < 9 else xs_d
                    j = (ci - 6) % 3
                    h.dma("sp", dd.t[j * 128:(j + 1) * 128, cols], sb_[:], [sb_], [r_pre[mt]], sb_)
                else:
                    ob = sob[ci % 2]
                    h.cp("pool", ob[:], sb_[:], [sb_], [ob])
                    dd = B_d if ci < 14 else C_d
                    j = (ci - 12) % 2
                    h.dma("sp", dd.t[j * 128:(j + 1) * 128, cols], ob[:], [ob], [r_pre[mt]], ob)
            for ti in range(4):
                t = mt * 4 + ti
                tc = slice(ti * 128, (ti + 1) * 128)
                h.mm([(pt[:, 0, :], hT[:, kk, tc], wint[:, kk, 0:512], kk == 0, kk == 7) for kk in range(8)]
                     + [(pt[:, 1, 0:274], hT[:, kk, tc], wint[:, kk, 512:786], kk == 0, kk == 7) for kk in range(8)],
                     [hT, wint], [pt])
                stg = ptst[t % 2]
                h.cp("act", stg[:, 0:512], pt[:, 0, :], [pt], [stg])
                h.cp("dve", stg[:, 512:786], pt[:, 1, 0:274], [pt], [stg])
                h.dma("sp", pt_d.t[t * 128:(t + 1) * 128, :], stg[:], [stg], [r_pre[mt]], stg)
        P.end_phase()

    if flags.get("s5", True):
        s5_phase(k, l)
    if flags.get("gdn", True):
        gdn_phase(k, l)
    if flags.get("ssd", True):
        ssd_phase(k, l)

    with contextlib.ExitStack() as es:
        wout = P.sb(es, "wout", [128, 8, D], BF16)
        h.dma("pool", wout[:], inp["w_out"].t[l].rearrange("(k p) n -> p k n", p=128), [], [wout], wout)
        G = P.sb(es, "Gm", [128, D], F32)
        k.load_row(G, k.modv.t[l:l + 1, 2 * D:3 * D], [k.r_modv])
        yT = [P.sb(es, f"m5y{i}", [128, 8, MT], BF16) for i in range(2)]
        xt = [P.sb(es, f"m5x{i}", [128, D], F32) for i in range(2)]
        tm = [P.sb(es, f"m5t{i}", [128, D], F32) for i in range(2)]
        po = [P.ps(es, f"m5p{i}", [128, 512], F32) for i in range(4)]
        for mt in range(NMT):
            cols = slice(mt * MT, (mt + 1) * MT)
            yb = yT[mt % 2]
            h.dma("sp", yb[:], y_d.t[:, cols].rearrange("(k p) t -> p k t", p=128), [r_y[0][mt], r_y[1][mt], r_y[2][mt]], [yb], yb)
            if not flags.get("s5", True):
                h.memset("pool", yb[:, 0:2, :], 0.0, [yb])
            if not flags.get("gdn", True):
                h.memset("pool", yb[:, 2:5, :], 0.0, [yb])
            if not flags.get("ssd", True):
                h.memset("pool", yb[:, 5:8, :], 0.0, [yb])
            for ti in range(4):
                t = mt * 4 + ti
                tc = slice(ti * 128, (ti + 1) * 128)
                xb = xt[t % 2]
                tb = tm[t % 2]
                h.dma("sp", xb[:], src.t[t * 128:(t + 1) * 128, :], [rsrc[t]], [xb], xb)
                for n2 in range(2):
                    pb = po[(t % 2) * 2 + n2]
                    nc_ = slice(n2 * 512, (n2 + 1) * 512)
                    h.mm([(pb[:], yb[:, kk, tc], wout[:, kk, nc_], kk == 0, kk == 7) for kk in range(8)], [yb, wout], [pb])
                    h.tt("dve", tb[:, nc_], pb[:], G[:, nc_], ALU.mult, [pb, G], [tb])
                h.tt("pool", tb[:], tb[:], xb[:], ALU.add, [tb, xb], [tb])
                h.dma("sp", dst.t[t * 128:(t + 1) * 128, :], tb[:], [tb], [rdst[t]], tb)
        P.end_phase()


def gate_consts(k, es, l, tag):
    P, h, inp = k.P, k.h, k.inp
    b12 = P.sb(es, tag + "b12", [128, 12], F32)
    na12 = P.sb(es, tag + "na12", [128, 12], F32)
    k.load_row(b12, inp["bias12"].t[l:l + 1, :])
    k.load_row(na12, inp["alog12"].t[l:l + 1, :])
    h.act(na12[:], na12[:], AF.Exp, [na12], [na12])
    h.ts("dve", na12[:], na12[:], -1.0, None, ALU.mult, None, [na12], [na12])
    return b12, na12


def gate_tile(k, sp_fn, pj_ap, pj_buf, b12, na12, masks, sp12, gda, cs12, cl12, pA):
    h = k.h
    sp_fn(pj_ap, pj_buf, b12, sp12)
    h.tt("dve", gda[:], sp12[:], na12[:], ALU.mult, [sp12, na12], [gda])
    h.mm([(pA[:, 0:12], masks["U"][:], gda[:], True, True)], [masks["U"], gda], [pA])
    h.cp("dve", cs12[:], pA[:, 0:12], [pA], [cs12])
    h.mm([(pA[:, 16:28], masks["sel"][:], cs12[:], True, True)], [masks["sel"], cs12], [pA])
    h.cp("dve", cl12[:], pA[:, 16:28], [pA], [cl12])


def ssd_phase(k, l):
    P, T, h, inp = k.P, k.T, k.h, k.inp
    NMT = T // 512
    MT = 512
    identf, identb = k.identf, k.identb
    with contextlib.ExitStack() as es:
        masks = make_masks(h, P, es)
        b12, na12 = gate_consts(k, es, l, "sd")
        sp_fns = [softplus12(h, P, es, f"sd{i}") for i in range(2)]
        epsb = P.sb(es, "sd_eps", [128, 1], F32)
        h.memset("pool", epsb[:], EPS, [epsb])
        dsk = P.sb(es, "sd_dsk", [128, 6], F32)
        k.load_row(dsk, inp["ssd_d"].t[l:l + 1, :])
        nws = P.sb(es, "sd_nws", [128, 384], F32)
        k.load_row(nws, inp["ssd_norm"].t[l:l + 1, :])
        stT = P.sb(es, "sd_stT", [128, 384], F32)
        stTb = P.sb(es, "sd_stTb", [128, 384], BF16)
        h.memset("pool", stT[:], 0.0, [stT])
        h.memset("pool", stTb[:], 0.0, [stTb])
        xsT = [P.sb(es, f"sd_xsT{i}", [128, 3, MT], F32) for i in range(2)]
        BTt = [P.sb(es, f"sd_BT{i}", [128, 2, MT], BF16) for i in range(2)]
        CTt = [P.sb(es, f"sd_CT{i}", [128, 2, MT], BF16) for i in range(2)]
        pj = [P.sb(es, f"sd_pj{i}", [128, 4, 786], F32) for i in range(2)]
        yo = [P.sb(es, f"sd_yo{i}", [128, 3, MT], BF16) for i in range(2)]
        R_sp12 = Rot(P, es, "sd_sp12", [128, 12], F32)
        R_gda = Rot(P, es, "sd_gda", [128, 12], F32)
        R_cs12 = Rot(P, es, "sd_cs12", [128, 12], F32)
        R_cl12 = Rot(P, es, "sd_cl12", [128, 12], F32)
        R_t6 = Rot(P, es, "sd_t6", [128, 6], F32)
        R_din = Rot(P, es, "sd_din", [128, 6], F32)
        R_eacs = Rot(P, es, "sd_eacs", [128, 6], F32)
        R_cd = Rot(P, es, "sd_cd", [128, 6], F32)
        R_dg = Rot(P, es, "sd_dg", [128, 6, 128], F32)
        R_arg = Rot(P, es, "sd_arg", [128, 6, 128], F32)
        R_seg = Rot(P, es, "sd_seg", [128, 6, 128], F32)
        R_WTb = Rot(P, es, "sd_WTb", [128, 6, 128], BF16)
        R_xs_tm = Rot(P, es, "sd_xstm", [128, 384], F32)
        R_xdtf = Rot(P, es, "sd_xdtf", [128, 384], F32)
        R_xdtb = Rot(P, es, "sd_xdtb", [128, 384], BF16)
        R_xddb = Rot(P, es, "sd_xddb", [128, 384], BF16)
        R_Btm = Rot(P, es, "sd_Btm", [128, 256], BF16)
        R_t1 = Rot(P, es, "sd_t1", [128, 384], F32)
        R_t2 = Rot(P, es, "sd_t2", [128, 384], F32)
        R_y = Rot(P, es, "sd_y", [128, 384], F32)
        R_zs = Rot(P, es, "sd_zs", [128, 384], F32)
        R_junk = Rot(P, es, "sd_junk", [128, 192], F32)
        R_yb = Rot(P, es, "sd_yb", [128, 384], BF16)
        R_ss2 = Rot(P, es, "sd_ss2", [128, 2], F32)
        R_rs2 = Rot(P, es, "sd_rs2", [128, 2], F32)
        W0 = P.ps(es, "sd_W0", [128, 2, 512], F32)
        S0 = P.ps(es, "sd_S0", [128, 512], F32)
        S1 = P.ps(es, "sd_S1", [128, 512], F32)
        S2 = P.ps(es, "sd_S2", [128, 512], F32)
        PB = P.ps(es, "sd_PB", [128, 512], BF16)
        pA = P.ps(es, "sd_pA", [128, 32], F32)
        for mt in range(NMT):
            cols = slice(mt * MT, (mt + 1) * MT)
            i2 = mt % 2
            rp = [k.r_pre[mt]]
            h.dma("sp", xsT[i2][:], k.xs_d.t[:, cols].rearrange("(j p) t -> p j t", p=128), rp, [xsT[i2]], xsT[i2])
            h.dma("sp", BTt[i2][:], k.B_d.t[:, cols].rearrange("(j p) t -> p j t", p=128), rp, [BTt[i2]], BTt[i2])
            h.dma("sp", CTt[i2][:], k.C_d.t[:, cols].rearrange("(j p) t -> p j t", p=128), rp, [CTt[i2]], CTt[i2])
            h.dma("sp", pj[i2][:], k.pt_d.t[mt * MT:(mt + 1) * MT, :].rearrange("(a p) n -> p a n", p=128), rp, [pj[i2]], pj[i2])
            xs_, B_, C_, pj_, yo_ = xsT[i2], BTt[i2], CTt[i2], pj[i2], yo[i2]
            for ti in range(4):
                tc = slice(ti * 128, (ti + 1) * 128)
                tix = mt * 4 + ti
                sp12 = R_sp12.at(tix); gda = R_gda.at(tix); cs12 = R_cs12.at(tix); cl12 = R_cl12.at(tix); t6 = R_t6.at(tix); din = R_din.at(tix); eacs = R_eacs.at(tix); cd = R_cd.at(tix); dg = R_dg.at(tix); arg = R_arg.at(tix); seg = R_seg.at(tix); WTb = R_WTb.at(tix); xs_tm = R_xs_tm.at(tix); xdtf = R_xdtf.at(tix); xdtb = R_xdtb.at(tix); xddb = R_xddb.at(tix); Btm = R_Btm.at(tix); t1 = R_t1.at(tix); t2 = R_t2.at(tix); y = R_y.at(tix); zs = R_zs.at(tix); junk = R_junk.at(tix); yb = R_yb.at(tix); ss2 = R_ss2.at(tix); rs2 = R_rs2.at(tix)
                sp_fn = sp_fns[tix % 2]
                gate_tile(k, sp_fn, pj_[:, ti, 768:780], pj_, b12, na12, masks, sp12, gda, cs12, cl12, pA)
                acs = cs12[:, 6:12]
                h.tt("pool", dg[:], b3(identf[:], 6, 128), s3(acs, 6, 128), ALU.mult, [identf, cs12], [dg])
                h.mm([(W0[:, 0, 0:384], masks["ones"][:], dg[:, 0:3, :].rearrange("p a l -> p (a l)"), True, True),
                      (W0[:, 1, 0:384], masks["ones"][:], dg[:, 3:6, :].rearrange("p a l -> p (a l)"), True, True)],
                     [masks["ones"], dg], [W0])
                h.tt("dve", v4(arg[:]), w4(W0), s4(acs), ALU.subtract, [W0, cs12], [arg])
                h.ts("pool", arg[:], arg[:], 0.0, None, ALU.min, None, [arg], [arg])
                h.act(seg[:], arg[:], AF.Exp, [arg], [seg])
                h.tt("pool", seg[:], seg[:], b3(masks["U"][:], 6, 128), ALU.mult, [seg, masks["U"]], [seg])
                h.mm([(S0[:, g * 128:(g + 1) * 128], B_[:, g, tc], C_[:, g, tc], True, True) for g in range(2)], [B_, C_], [S0])
                h.tt("dve", v4(WTb[:]), v4(seg[:]),
                     S0[:, 0:256].rearrange("p (g l) -> p g l", g=2).unsqueeze(2).to_broadcast([128, 2, 3, 128]),
                     ALU.mult, [seg, S0], [WTb])
                h.tr([(S1[:, j * 128:(j + 1) * 128], xs_[:, j, tc]) for j in range(3)], identf[:], [xs_, identf], [S1])
                h.cp("act", xs_tm[:], S1[:, 0:384], [S1], [xs_tm])
                h.tr([(PB[:, g * 128:(g + 1) * 128], B_[:, g, tc]) for g in range(2)], identb[:], [B_, identb], [PB])
                h.cp("act", Btm[:], PB[:, 0:256], [PB], [Btm])
                x3 = lambda ap: ap.rearrange("p (h d) -> p h d", h=6)
                h.tt("dve", x3(xdtf[:]), x3(xs_tm[:]), s3(sp12[:, 6:12], 6, 64), ALU.mult, [xs_tm, sp12], [xdtf])
                h.cp("pool", xdtb[:], xdtf[:], [xdtf], [xdtb])
                h.tt("dve", t6[:], cl12[:, 6:12], acs, ALU.subtract, [cl12, cs12], [t6])
                h.act(din[:], t6[:], AF.Exp, [t6], [din])
                h.tt("pool", x3(xddb[:]), x3(xdtf[:]), s3(din[:], 6, 64), ALU.mult, [xdtf, din], [xddb])
                h.mm([(S2[:, hd * 64:(hd + 1) * 64], WTb[:, hd, :], xdtb[:, hd * 64:(hd + 1) * 64], True, True) for hd in range(6)],
                     [WTb, xdtb], [S2])
                h.mm([(S0[:, g * 192:(g + 1) * 192], C_[:, g, tc], stTb[:, g * 192:(g + 1) * 192], True, True) for g in range(2)],
                     [C_, stTb], [S0])
                h.act(eacs[:], acs, AF.Exp, [cs12], [eacs])
                h.tt("dve", x3(t2[:]), x3(S0[:, 0:384]), s3(eacs[:], 6, 64), ALU.mult, [S0, eacs], [t2])
                h.tt("pool", x3(t1[:]), x3(xs_tm[:]), s3(dsk[:], 6, 64), ALU.mult, [xs_tm, dsk], [t1])
                h.tt("pool", t2[:], t2[:], t1[:], ALU.add, [t2, t1], [t2])
                h.tt("dve", y[:], S2[:, 0:384], t2[:], ALU.add, [S2, t2], [y])
                h.act(zs[:], pj_[:, ti, 384:768], AF.Silu, [pj_], [zs])
                h.tt("pool", y[:], y[:], zs[:], ALU.mult, [y, zs], [y])
                for g in range(2):
                    h.act(junk[:], y[:, g * 192:(g + 1) * 192], AF.Square, [y], [junk, ss2], accum=ss2[:, g:g + 1])
                h.act(rs2[:], ss2[:], AF.Ln, [ss2, epsb], [rs2], bias=epsb[:, 0:1], scale=1.0 / 192)
                h.act(rs2[:], rs2[:], AF.Exp, [rs2], [rs2], scale=-0.5)
                y3 = lambda ap: ap.rearrange("p (g c) -> p g c", g=2)
                h.tt("dve", y3(y[:]), y3(y[:]), s3(rs2[:], 2, 192), ALU.mult, [y, rs2], [y])
                h.tt("pool", yb[:], y[:], nws[:], ALU.mult, [y, nws], [yb])
                h.tr([(PB[:, j * 128:(j + 1) * 128], yb[:, j * 128:(j + 1) * 128]) for j in range(3)], identb[:], [yb, identb], [PB])
                h.cp("act", yo_[:, :, tc], PB[:, 0:384].rearrange("p (j t) -> p j t", j=3), [PB], [yo_])
                h.mm([(S1[:, g * 192:(g + 1) * 192], Btm[:, g * 128:(g + 1) * 128], xddb[:, g * 192:(g + 1) * 192], True, True) for g in range(2)],
                     [Btm, xddb], [S1])
                h.act(cd[:], cl12[:, 6:12], AF.Exp, [cl12], [cd])
                h.tt("pool", x3(stT[:]), x3(stT[:]), s3(cd[:], 6, 64), ALU.mult, [stT, cd], [stT])
                h.tt("dve", stT[:], stT[:], S1[:, 0:384], ALU.add, [stT, S1], [stT])
                h.cp("act", stTb[:], stT[:], [stT], [stTb])
            h.dma("sp", k.y_d.t[640:1024, cols].rearrange("(j p) t -> p j t", p=128), yo_[:], [yo_], [k.r_y[2][mt]], yo_)
        P.end_phase()


C1_2PI = 6.28125
C2_2PI = 2.0 * math.pi - 6.28125


def sincos(h, eng, x, out, b, ki, c, negpi, R, W, bufs, is_cos):
    bb, kb, cb_ = bufs
    off = 16.5 + (0.25 if is_cos else 0.0)
    add = 33.0 * math.pi + (0.5 * math.pi if is_cos else 0.0)
    h.ts(eng, b, x, 1.0 / (2.0 * math.pi), off, ALU.mult, ALU.add, R, [bb])
    h.cp(eng, ki, b, [bb], [kb])
    h.cp(eng, c, ki, [kb], [cb_])
    h.stt(eng, b, c, -C1_2PI, x, ALU.mult, ALU.add, R + [cb_], [bb])
    h.stt(eng, b, c, -C2_2PI, b, ALU.mult, ALU.add, [cb_, bb], [bb])
    h.ts(eng, b, b, add, None, ALU.add, None, [bb], [bb])
    h.ts(eng, c, b, 2.0 * math.pi, -2.0 * math.pi, ALU.is_gt, ALU.mult, [bb], [cb_])
    h.tt(eng, b, b, c, ALU.add, [bb, cb_], [bb])
    h.ts(eng, c, b, 0.0, 2.0 * math.pi, ALU.is_lt, ALU.mult, [bb], [cb_])
    h.tt(eng, b, b, c, ALU.add, [bb, cb_], [bb])
    h.act(out, b, AF.Sin, [bb, negpi], W, bias=negpi[:, 0:1])


def s5_phase(k, l):
    P, T, h, inp = k.P, k.T, k.h, k.inp
    NMT = T // 512
    SEG = 512
    with contextlib.ExitStack() as es:
        I32 = mybir.dt.int32
        are = P.sb(es, "s5are", [128, 8], F32)
        aim = P.sb(es, "s5aim", [128, 8], F32)
        stp = P.sb(es, "s5stp", [128, 8], F32)
        h.dma("sp", are[:], inp["s5_are"].t[l], [], [are], are)
        h.dma("sp", aim[:], inp["s5_aim"].t[l], [], [aim], aim)
        h.dma("sp", stp[:], inp["s5_ldt"].t[l], [], [stp], stp)
        dsk = P.sb(es, "s5dsk", [128, 2], F32)
        nw5 = P.sb(es, "s5nw", [128, 2], F32)
        h.dma("sp", dsk[:], inp["s5_dcol"].t[l], [], [dsk], dsk)
        h.dma("sp", nw5[:], inp["s5_ncol"].t[l], [], [nw5], nw5)
        bTre = P.sb(es, "s5bTre", [128, 8, 128], BF16)
        bTim = P.sb(es, "s5bTim", [128, 8, 128], BF16)
        cTre = P.sb(es, "s5cTre", [128, 8, 128], BF16)
        cTim = P.sb(es, "s5cTim", [128, 8, 128], BF16)
        for dstb, nm in ((bTre, "s5_bT_re"), (bTim, "s5_bT_im"), (cTre, "s5_cT_re"), (cTim, "s5_cT_im")):
            h.dma("pool", dstb[:], inp[nm].t[l].rearrange("s r m -> r s m"), [], [dstb], dstb)
        wglu = P.sb(es, "s5wglu", [128, 2, 256], BF16)
        h.dma("pool", wglu[:], inp["s5_w_glu"].t[l].rearrange("(k p) n -> p k n", p=128), [], [wglu], wglu)
        negpi = P.sb(es, "s5negpi", [128, 1], F32)
        h.memset("pool", negpi[:], -math.pi, [negpi])
        epsb = P.sb(es, "s5eps", [128, 1], F32)
        h.memset("pool", epsb[:], EPS, [epsb])
        onesf = P.sb(es, "s5ones", [128, 128], F32)
        h.memset("pool", onesf[:], 1.0, [onesf])
        jrow = P.sb(es, "s5jrow", [128, SEG], F32)
        P.op("pool", lambda e: e.iota(jrow[:], pattern=[[1, SEG]], base=0, channel_multiplier=0,
                                      allow_small_or_imprecise_dtypes=True), writes=[jrow])
        th = P.sb(es, "s5th", [128, 8], F32)
        rr = P.sb(es, "s5r", [128, 8], F32)
        sth = P.sb(es, "s5sth", [128, 8], F32)
        cth = P.sb(es, "s5cth", [128, 8], F32)
        thS = P.sb(es, "s5thS", [128, 8], F32)
        sS = P.sb(es, "s5sS", [128, 8], F32)
        cS = P.sb(es, "s5cS", [128, 8], F32)
        nsS = P.sb(es, "s5nsS", [128, 8], F32)
        cr = P.sb(es, "s5cr", [128, 8], F32)
        ci = P.sb(es, "s5ci", [128, 8], F32)
        ncr = P.sb(es, "s5ncr", [128, 8], F32)
        q1 = P.sb(es, "s5q1", [128, 8], F32)
        q2 = P.sb(es, "s5q2", [128, 8], F32)
        q3 = P.sb(es, "s5q3", [128, 8], F32)
        sb8 = P.sb(es, "s5sb8", [128, 8], F32)
        si8 = P.sb(es, "s5si8", [128, 8], I32)
        sc8 = P.sb(es, "s5sc8", [128, 8], F32)
        h.act(stp[:], stp[:], AF.Exp, [stp], [stp])
        h.tt("dve", th[:], aim[:], stp[:], ALU.mult, [aim, stp], [th])
        h.tt("dve", rr[:], are[:], stp[:], ALU.mult, [are, stp], [rr])
        h.act(rr[:], rr[:], AF.Exp, [rr], [rr])
        sm = (sb8, si8, sc8)
        sincos(h, "dve", th[:], sth[:], sb8[:], si8[:], sc8[:], negpi, [th], [sth], sm, False)
        sincos(h, "dve", th[:], cth[:], sb8[:], si8[:], sc8[:], negpi, [th], [cth], sm, True)
        h.ts("dve", thS[:], th[:], float(SEG), None, ALU.mult, None, [th], [thS])
        sincos(h, "dve", thS[:], sS[:], sb8[:], si8[:], sc8[:], negpi, [thS], [sS], sm, False)
        sincos(h, "dve", thS[:], cS[:], sb8[:], si8[:], sc8[:], negpi, [thS], [cS], sm, True)
        h.ts("dve", nsS[:], sS[:], -1.0, None, ALU.mult, None, [sS], [nsS])
        h.tt("dve", q1[:], rr[:], cth[:], ALU.mult, [rr, cth], [q1])
        h.ts("dve", q1[:], q1[:], -1.0, None, ALU.add, None, [q1], [q1])
        h.tt("dve", q2[:], rr[:], sth[:], ALU.mult, [rr, sth], [q2])
        h.tt("dve", q3[:], are[:], are[:], ALU.mult, [are], [q3])
        h.tt("dve", sc8[:], aim[:], aim[:], ALU.mult, [aim], [sc8])
        h.tt("dve", q3[:], q3[:], sc8[:], ALU.add, [q3, sc8], [q3])
        h.recip(q3[:], q3[:], [q3], [q3])
        h.tt("dve", cr[:], q1[:], are[:], ALU.mult, [q1, are], [cr])
        h.tt("dve", sc8[:], q2[:], aim[:], ALU.mult, [q2, aim], [sc8])
        h.tt("dve", cr[:], cr[:], sc8[:], ALU.add, [cr, sc8], [cr])
        h.tt("dve", cr[:], cr[:], q3[:], ALU.mult, [cr, q3], [cr])
        h.tt("dve", ci[:], q2[:], are[:], ALU.mult, [q2, are], [ci])
        h.tt("dve", sc8[:], q1[:], aim[:], ALU.mult, [q1, aim], [sc8])
        h.tt("dve", ci[:], ci[:], sc8[:], ALU.subtract, [ci, sc8], [ci])
        h.tt("dve", ci[:], ci[:], q3[:], ALU.mult, [ci, q3], [ci])
        h.ts("dve", ncr[:], cr[:], -1.0, None, ALU.mult, None, [cr], [ncr])
        cosT = P.sb(es, "s5cosT", [128, 8, SEG], F32)
        sinT = P.sb(es, "s5sinT", [128, 8, SEG], F32)
        tabr = P.sb(es, "s5tabr", [128, 8, SEG], F32)
        tabi = P.sb(es, "s5tabi", [128, 8, SEG], F32)
        ang = [P.sb(es, f"s5ang{i}", [128, SEG], F32) for i in range(2)]
        tb = [P.sb(es, f"s5tb{i}", [128, SEG], F32) for i in range(2)]
        tki = [P.sb(es, f"s5tki{i}", [128, SEG], I32) for i in range(2)]
        tcc = [P.sb(es, f"s5tc{i}", [128, SEG], F32) for i in range(2)]
        for sc in range(8):
            i = sc % 2
            eng = "dve" if i == 0 else "pool"
            h.ts(eng, ang[i][:], jrow[:], th[:, sc:sc + 1], None, ALU.mult, None, [jrow, th], [ang[i]])
            bufs = (tb[i], tki[i], tcc[i])
            sincos(h, eng, ang[i][:], sinT[:, sc, :], tb[i][:], tki[i][:], tcc[i][:], negpi, [ang[i]], [sinT], bufs, False)
            sincos(h, eng, ang[i][:], cosT[:, sc, :], tb[i][:], tki[i][:], tcc[i][:], negpi, [ang[i]], [cosT], bufs, True)
            h.ts(eng, tabr[:, sc, :], cosT[:, sc, :], cr[:, sc:sc + 1], None, ALU.mult, None, [cosT, cr], [tabr])
            h.stt(eng, tabr[:, sc, :], sinT[:, sc, :], ci[:, sc:sc + 1], tabr[:, sc, :], ALU.mult, ALU.add, [sinT, ci, tabr], [tabr])
            h.ts(eng, tabi[:, sc, :], cosT[:, sc, :], ci[:, sc:sc + 1], None, ALU.mult, None, [cosT, ci], [tabi])
            h.stt(eng, tabi[:, sc, :], sinT[:, sc, :], ncr[:, sc:sc + 1], tabi[:, sc, :], ALU.mult, ALU.add, [sinT, ncr, tabi], [tabi])
        ire = P.sb(es, "s5ire", [128, 8], F32)
        iim = P.sb(es, "s5iim", [128, 8], F32)
        gre_e = P.sb(es, "s5gree", [128, 8], F32)
        gim_e = P.sb(es, "s5gime", [128, 8], F32)
        h.memset("pool", ire[:], 0.0, [ire])
        h.memset("pool", iim[:], 0.0, [iim])
        uTf = [P.sb(es, f"s5uTf{i}", [128, 2, SEG], F32) for i in range(2)]
        uTb = [P.sb(es, f"s5uTb{i}", [128, 2, SEG], BF16) for i in range(2)]
        m1 = [P.sb(es, f"s5m1{i}", [128, SEG], F32) for i in range(2)]
        m2 = [P.sb(es, f"s5m2{i}", [128, SEG], F32) for i in range(2)]
        m3 = [P.sb(es, f"s5m3{i}", [128, SEG], F32) for i in range(2)]
        m4 = [P.sb(es, f"s5m4{i}", [128, SEG], F32) for i in range(2)]
        n1 = [P.sb(es, f"s5n1{i}", [128, SEG], F32) for i in range(2)]
        n2 = [P.sb(es, f"s5n2{i}", [128, SEG], F32) for i in range(2)]
        n3 = [P.sb(es, f"s5n3{i}", [128, SEG], F32) for i in range(2)]
        n4 = [P.sb(es, f"s5n4{i}", [128, SEG], F32) for i in range(2)]
        dre = [P.sb(es, f"s5dre{i}", [128, SEG], F32) for i in range(2)]
        dim = [P.sb(es, f"s5dim{i}", [128, SEG], F32) for i in range(2)]
        gre = [P.sb(es, f"s5gre{i}", [128, SEG], F32) for i in range(2)]
        gim = [P.sb(es, f"s5gim{i}", [128, SEG], F32) for i in range(2)]
        hre = [P.sb(es, f"s5hre{i}", [128, SEG], BF16) for i in range(2)]
        him = [P.sb(es, f"s5him{i}", [128, SEG], BF16) for i in range(2)]
        y1 = P.sb(es, "s5y1", [128, 2, SEG], F32)
        yt = P.sb(es, "s5yt", [128, 2, SEG], F32)
        yg = P.sb(es, "s5yg", [128, 2, SEG], F32)
        ygb = P.sb(es, "s5ygb", [128, 2, SEG], BF16)
        sg = P.sb(es, "s5sg", [128, 2, SEG], F32)
        y2 = P.sb(es, "s5y2", [128, 2, SEG], F32)
        rstd = P.sb(es, "s5rstd", [128, SEG], F32)
        yo = [P.sb(es, f"s5yo{i}", [128, 2, SEG], BF16) for i in range(2)]
        Pre = [P.ps(es, f"s5Pre{i}", [128, SEG], F32) for i in range(2)]
        Pim = [P.ps(es, f"s5Pim{i}", [128, SEG], F32) for i in range(2)]
        Y = [P.ps(es, f"s5Y{i}", [128, SEG], F32) for i in range(2)]
        Pg = P.ps(es, "s5Pg", [128, SEG], F32)
        Pt = P.ps(es, "s5Pt", [128, SEG], F32)
        GK = 2.0 * math.sqrt(2.0 / math.pi)
        for mt in range(NMT):
            cols = slice(mt * SEG, (mt + 1) * SEG)
            i2 = mt % 2
            uf, ub, yo_ = uTf[i2], uTb[i2], yo[i2]
            h.dma("sp", uf[:], k.u_d.t[:, cols].rearrange("(j p) t -> p j t", p=128), [k.r_pre[mt]], [uf], uf)
            h.cp("pool", ub[:], uf[:], [uf], [ub])
            for sc in range(8):
                i = sc % 2
                cc = sc // 4
                h.mm([(Pre[i][:], bTre[:, sc, :], ub[:, cc, :], True, True)], [bTre, ub], [Pre[i]])
                h.mm([(Pim[i][:], bTim[:, sc, :], ub[:, cc, :], True, True)], [bTim, ub], [Pim[i]])
                h.tt("dve", m1[i][:], Pre[i][:], tabr[:, sc, :], ALU.mult, [Pre[i], tabr], [m1[i]])
                h.tt("dve", m2[i][:], Pim[i][:], tabi[:, sc, :], ALU.mult, [Pim[i], tabi], [m2[i]])
                h.tt("pool", dre[i][:], m1[i][:], m2[i][:], ALU.subtract, [m1[i], m2[i]], [dre[i]])
                h.tt("dve", m3[i][:], Pre[i][:], tabi[:, sc, :], ALU.mult, [Pre[i], tabi], [m3[i]])
                h.tt("dve", m4[i][:], Pim[i][:], tabr[:, sc, :], ALU.mult, [Pim[i], tabr], [m4[i]])
                h.tt("pool", dim[i][:], m3[i][:], m4[i][:], ALU.add, [m3[i], m4[i]], [dim[i]])
                for (go, di, ini) in ((gre[i], dre[i], ire), (gim[i], dim[i], iim)):
                    P.op("dve", (lambda go, di, ini, sc: (lambda e: e.tensor_tensor_scan(
                        out=go[:], data0=rr[:, sc:sc + 1].to_broadcast([128, SEG]), data1=di[:],
                        initial=ini[:, sc:sc + 1], op0=ALU.mult, op1=ALU.add)))(go, di, ini, sc),
                        reads=[rr, di, ini], writes=[go])
                h.cp("act", gre_e[:, sc:sc + 1], gre[i][:, SEG - 1:SEG], [gre[i]], [gre_e])
                h.cp("act", gim_e[:, sc:sc + 1], gim[i][:, SEG - 1:SEG], [gim[i]], [gim_e])
                h.tt("pool", n1[i][:], gre[i][:], cosT[:, sc, :], ALU.mult, [gre[i], cosT], [n1[i]])
                h.tt("pool", n2[i][:], gim[i][:], sinT[:, sc, :], ALU.mult, [gim[i], sinT], [n2[i]])
                h.tt("pool", hre[i][:], n1[i][:], n2[i][:], ALU.subtract, [n1[i], n2[i]], [hre[i]])
                h.tt("dve", n3[i][:], gre[i][:], sinT[:, sc, :], ALU.mult, [gre[i], sinT], [n3[i]])
                h.tt("dve", n4[i][:], gim[i][:], cosT[:, sc, :], ALU.mult, [gim[i], cosT], [n4[i]])
                h.stt("pool", him[i][:], n3[i][:], -1.0, n4[i][:], ALU.mult, ALU.subtract, [n3[i], n4[i]], [him[i]])
                h.mm([(Y[cc][:], cTre[:, sc, :], hre[i][:], sc % 4 == 0, False),
                      (Y[cc][:], cTim[:, sc, :], him[i][:], False, sc % 4 == 3)], [cTre, cTim, hre[i], him[i]], [Y[cc]])
                if sc % 4 == 3:
                    h.stt("dve", y1[:, cc, :], uf[:, cc, :], dsk[:, cc:cc + 1], Y[cc][:], ALU.mult, ALU.add, [uf, dsk, Y[cc]], [y1])
                    h.tt("pool", yt[:, cc, :], y1[:, cc, :], y1[:, cc, :], ALU.mult, [y1], [yt])
                    h.ts("pool", yt[:, cc, :], yt[:, cc, :], 0.044715, 1.0, ALU.mult, ALU.add, [yt], [yt])
                    h.tt("pool", yt[:, cc, :], yt[:, cc, :], y1[:, cc, :], ALU.mult, [yt, y1], [yt])
                    h.act(yt[:, cc, :], yt[:, cc, :], AF.Sigmoid, [yt], [yt], scale=GK)
                    h.tt("pool", yg[:, cc, :], y1[:, cc, :], yt[:, cc, :], ALU.mult, [y1, yt], [yg])
                    h.cp("pool", ygb[:, cc, :], yg[:, cc, :], [yg], [ygb])
            h.tt("dve", q1[:], gre_e[:], cS[:], ALU.mult, [gre_e, cS], [q1])
            h.tt("dve", q2[:], gim_e[:], nsS[:], ALU.mult, [gim_e, nsS], [q2])
            h.tt("dve", ire[:], q1[:], q2[:], ALU.add, [q1, q2], [ire])
            h.tt("dve", q1[:], gre_e[:], sS[:], ALU.mult, [gre_e, sS], [q1])
            h.tt("dve", q2[:], gim_e[:], cS[:], ALU.mult, [gim_e, cS], [q2])
            h.tt("dve", iim[:], q1[:], q2[:], ALU.add, [q1, q2], [iim])
            for oc in range(2):
                h.mm([(Pg[:], wglu[:, kc, oc * 128:(oc + 1) * 128], ygb[:, kc, :], kc == 0, kc == 1) for kc in range(2)], [wglu, ygb], [Pg])
                h.act(sg[:, oc, :], Pg[:], AF.Sigmoid, [Pg], [sg])
                h.tt("pool", y2[:, oc, :], yg[:, oc, :], sg[:, oc, :], ALU.mult, [yg, sg], [y2])
                h.tt("pool", sg[:, oc, :], y2[:, oc, :], y2[:, oc, :], ALU.mult, [y2], [sg])
            h.mm([(Pt[:], onesf[:], sg[:, oc, :], oc == 0, oc == 1) for oc in range(2)], [onesf, sg], [Pt])
            h.act(rstd[:], Pt[:], AF.Sqrt, [Pt, epsb], [rstd], bias=epsb[:, 0:1], scale=1.0 / 256)
            h.recip(rstd[:], rstd[:], [rstd], [rstd])
            for oc in range(2):
                h.stt("dve", yo_[:, oc, :], y2[:, oc, :], nw5[:, oc:oc + 1], rstd[:], ALU.mult, ALU.mult, [y2, nw5, rstd], [yo_])
            h.dma("sp", k.y_d.t[0:256, cols].rearrange("(j p) t -> p j t", p=128), yo_[:], [yo_], [k.r_y[0][mt]], yo_)
        P.end_phase()


def gdn_phase(k, l):
    P, T, h, inp = k.P, k.T, k.h, k.inp
    NMT = T // 512
    MT = 512
    identf, identb = k.identf, k.identb
    with contextlib.ExitStack() as es:
        masks = make_masks(h, P, es)
        b12, na12 = gate_consts(k, es, l, "gd")
        gnw = P.sb(es, "gd_gnw", [128, 64], F32)
        k.load_row(gnw, inp["gdn_norm"].t[l:l + 1, :])
        Sf = P.sb(es, "gd_Sf", [128, 3, 128], F32)
        Sb = P.sb(es, "gd_Sb", [128, 3, 128], BF16)
        h.memset("pool", Sf[:], 0.0, [Sf])
        h.memset("pool", Sb[:], 0.0, [Sb])
        qnT = [P.sb(es, f"gd_qn{i}", [128, 3, MT], BF16) for i in range(2)]
        knT = [P.sb(es, f"gd_kn{i}", [128, 3, MT], BF16) for i in range(2)]
        vT = [P.sb(es, f"gd_vT{i}", [128, 3, MT], F32) for i in range(2)]
        pj = [P.sb(es, f"gd_pj{i}", [128, 4, 786], F32) for i in range(2)]
        yo = [P.sb(es, f"gd_yo{i}", [128, 3, MT], BF16) for i in range(2)]
        R_sp12 = Rot(P, es, "gd_sp12", [128, 12], F32)
        R_gda = Rot(P, es, "gd_gda", [128, 12], F32)
        R_cs12 = Rot(P, es, "gd_cs12", [128, 12], F32)
        R_cl12 = Rot(P, es, "gd_cl12", [128, 12], F32)
        R_beta = Rot(P, es, "gd_beta", [128, 6], F32)
        R_nbeta = Rot(P, es, "gd_nbeta", [128, 6], F32)
        R_egc = Rot(P, es, "gd_egc", [128, 6], F32)
        R_t6 = Rot(P, es, "gd_t6", [128, 6], F32)
        R_dkk = Rot(P, es, "gd_dkk", [128, 6], F32)
        R_gtot = Rot(P, es, "gd_gtot", [128, 6], F32)
        R_gtc = Rot(P, es, "gd_gtc", [128, 3], F32)
        R_dg = Rot(P, es, "gd_dg", [128, 6, 128], F32)
        R_arg = Rot(P, es, "gd_arg", [128, 6, 128], F32)
        R_E = Rot(P, es, "gd_E", [128, 6, 128], F32)
        R_EU = Rot(P, es, "gd_EU", [128, 6, 128], F32)
        R_ELn = Rot(P, es, "gd_ELn", [128, 6, 128], F32)
        R_attnT = Rot(P, es, "gd_attnT", [128, 6, 128], BF16)
        R_Pm = [Rot(P, es, f"gd_Pm{i}", [128, 6, 128], BF16) for i in range(2)]
        R_Qm = [Rot(P, es, f"gd_Qm{i}", [128, 6, 128], BF16) for i in range(2)]
        sp_fns = [softplus12(h, P, es, f"gd{i}") for i in range(2)]
        R_Xb = Rot(P, es, "gd_Xb", [128, 6, 128], BF16)
        R_kdec = Rot(P, es, "gd_kdec", [128, 384], BF16)
        R_v_tm = Rot(P, es, "gd_vtm", [128, 384], F32)
        R_rr_ = Rot(P, es, "gd_rr", [128, 384], F32)
        R_rb = Rot(P, es, "gd_rb", [128, 384], BF16)
        R_vnb = Rot(P, es, "gd_vnb", [128, 384], BF16)
        R_oa = Rot(P, es, "gd_oa", [128, 384], F32)
        R_o = Rot(P, es, "gd_o", [128, 384], F32)
        R_sq = Rot(P, es, "gd_sq", [128, 384], F32)
        R_ss6 = Rot(P, es, "gd_ss6", [128, 6], F32)
        R_rs6 = Rot(P, es, "gd_rs6", [128, 6], F32)
        R_zg = Rot(P, es, "gd_zg", [128, 384], F32)
        R_yb = Rot(P, es, "gd_yb", [128, 384], BF16)
        R_tmpS = Rot(P, es, "gd_tmpS", [128, 3, 128], F32)
        W0 = P.ps(es, "gd_W0", [128, 2, 512], F32)
        W1 = P.ps(es, "gd_W1", [128, 2, 512], F32)
        W2 = P.ps(es, "gd_W2", [128, 2, 512], F32)
        PB = P.ps(es, "gd_PB", [128, 1024], BF16)
        pA = P.ps(es, "gd_pA", [128, 32], F32)
        x3 = lambda ap: ap.rearrange("p (h d) -> p h d", h=6)
        kz = [P.sb(es, f"gd_kz{i}", [128, 3, MT], BF16) for i in range(2)]
        R_Tb = Rot(P, es, "gd_Tb", [128, 6, 128], BF16)
        R_Tb2 = Rot(P, es, "gd_Tb2", [128, 6, 128], BF16)
        R_Xb2 = Rot(P, es, "gd_Xb2", [128, 6, 128], BF16)
        epsb = P.sb(es, "gd_eps", [128, 1], F32)
        h.memset("pool", epsb[:], EPS, [epsb])
        R_ez = Rot(P, es, "gd_ez", [128, 384], F32)
        R_M1b = Rot(P, es, "gd_M1b", [128, 6, 128], BF16)
        R_M1pb = Rot(P, es, "gd_M1pb", [128, 6, 128], BF16)
        cmask = P.sb(es, "gd_cmask", [128, 14, 128], BF16)
        h.dma("pool", cmask[:], inp["gdn_cmask"].t.rearrange("m p j -> p m j"), [], [cmask], cmask)
        cmask6 = P.sb(es, "gd_cmask6", [128, 14, 6, 128], BF16)
        h.cp("pool", cmask6[:], cmask[:].unsqueeze(2).to_broadcast([128, 14, 6, 128]), [cmask], [cmask6])
        rmask = P.sb(es, "gd_rmask", [128, 2], F32)
        h.memset("pool", rmask[:], 0.0, [rmask])
        h.memset("pool", rmask[0:64, 0:1], 1.0, [rmask])
        h.memset("pool", rmask[64:128, 1:2], 1.0, [rmask])

        def headmm(Wps, lh, rh, R):
            w = w4(Wps)
            h.mm([(w[:, hd // 3, hd % 3, :], lh[:, hd, :], rh[:, hd, :], True, True) for hd in range(6)], R, [Wps])

        stop = k.flags.get("gdn_stop", 99)
        for mt in range(NMT):
            cols = slice(mt * MT, (mt + 1) * MT)
            i2 = mt % 2
            rp = [k.r_pre[mt]]
            h.dma("sp", qnT[i2][:], k.qn_d.t[:, cols].rearrange("(j p) t -> p j t", p=128), rp, [qnT[i2]], qnT[i2])
            h.dma("sp", knT[i2][:], k.kn_d.t[:, cols].rearrange("(j p) t -> p j t", p=128), rp, [knT[i2]], knT[i2])
            h.dma("sp", vT[i2][:], k.v_d.t[:, cols].rearrange("(j p) t -> p j t", p=128), rp, [vT[i2]], vT[i2])
            h.dma("sp", pj[i2][:], k.pt_d.t[mt * MT:(mt + 1) * MT, :].rearrange("(a p) n -> p a n", p=128), rp, [pj[i2]], pj[i2])
            qn_, kn_, v_, pj_, yo_ = qnT[i2], knT[i2], vT[i2], pj[i2], yo[i2]
            for s_ in range(2):
                h.ts("dve", kz[s_][:], kn_[:], rmask[:, s_:s_ + 1], None, ALU.mult, None, [kn_, rmask], [kz[s_]])
            for ti in range(4):
                tc = slice(ti * 128, (ti + 1) * 128)
                tix = mt * 4 + ti
                sp12 = R_sp12.at(tix); gda = R_gda.at(tix); cs12 = R_cs12.at(tix); cl12 = R_cl12.at(tix); beta = R_beta.at(tix); nbeta = R_nbeta.at(tix); egc = R_egc.at(tix); t6 = R_t6.at(tix); dkk = R_dkk.at(tix); gtot = R_gtot.at(tix); gtc = R_gtc.at(tix); dg = R_dg.at(tix); arg = R_arg.at(tix); E = R_E.at(tix); EU = R_EU.at(tix); ELn = R_ELn.at(tix); attnT = R_attnT.at(tix); Xb = R_Xb.at(tix); kdec = R_kdec.at(tix); v_tm = R_v_tm.at(tix); rr_ = R_rr_.at(tix); rb = R_rb.at(tix); vnb = R_vnb.at(tix); oa = R_oa.at(tix); o = R_o.at(tix); sq = R_sq.at(tix); ss6 = R_ss6.at(tix); rs6 = R_rs6.at(tix); zg = R_zg.at(tix); yb = R_yb.at(tix); tmpS = R_tmpS.at(tix); Tb = R_Tb.at(tix); M1b = R_M1b.at(tix); M1pb = R_M1pb.at(tix)
                Pm = [r.at(tix) for r in R_Pm]; Qm = [r.at(tix) for r in R_Qm]; sp_fn = sp_fns[tix % 2]; Tb2 = R_Tb2.at(tix); Xb2 = R_Xb2.at(tix); ez = R_ez.at(tix)
                gate_tile(k, sp_fn, pj_[:, ti, 768:780], pj_, b12, na12, masks, sp12, gda, cs12, cl12, pA)
                gc = cs12[:, 0:6]
                h.act(beta[:], pj_[:, ti, 780:786], AF.Exp, [pj_], [beta], scale=-1.0)
                h.ts("dve", beta[:], beta[:], 1.0, None, ALU.add, None, [beta], [beta])
                h.recip(beta[:], beta[:], [beta], [beta])
                h.ts("dve", nbeta[:], beta[:], -1.0, None, ALU.mult, None, [beta], [nbeta])
                h.act(egc[:], gc, AF.Exp, [cs12], [egc])
                h.tt("dve", t6[:], cl12[:, 0:6], gc, ALU.subtract, [cl12, cs12], [t6])
                h.act(dkk[:], t6[:], AF.Exp, [t6], [dkk])
                h.act(gtot[:], cl12[:, 0:6], AF.Exp, [cl12], [gtot])
                g2 = gtot[:].rearrange("p (j s) -> p j s", s=2)
                h.cp("dve", gtc[0:64, :], g2[0:64, :, 0], [gtot], [gtc])
                h.cp("dve", gtc[64:128, :], g2[64:128, :, 1], [gtot], [gtc])
                if stop < 1:
                    h.memset("pool", yo_[:, :, tc], 0.0, [yo_])
                    continue
                h.tt("pool", dg[:], b3(identf[:], 6, 128), s3(gc, 6, 128), ALU.mult, [identf, cs12], [dg])
                h.mm([(W0[:, 0, 0:384], masks["ones"][:], dg[:, 0:3, :].rearrange("p a l -> p (a l)"), True, True),
                      (W0[:, 1, 0:384], masks["ones"][:], dg[:, 3:6, :].rearrange("p a l -> p (a l)"), True, True)],
                     [masks["ones"], dg], [W0])
                if stop < 1.2:
                    h.memset("pool", yo_[:, :, tc], 0.0, [yo_])
                    continue
                h.tt("dve", v4(arg[:]), w4(W0), s4(gc), ALU.subtract, [W0, cs12], [arg])
                if stop < 1.4:
                    h.memset("pool", yo_[:, :, tc], 0.0, [yo_])
                    continue
                h.act(arg[:], arg[:], AF.Abs, [arg], [arg])
                h.act(E[:], arg[:], AF.Exp, [arg], [E], scale=-1.0)
                if stop < 1.6:
                    h.memset("pool", yo_[:, :, tc], 0.0, [yo_])
                    continue
                h.tt("pool", EU[:], E[:], b3(masks["U"][:], 6, 128), ALU.mult, [E, masks["U"]], [EU])
                if stop < 1.8:
                    h.memset("pool", yo_[:, :, tc], 0.0, [yo_])
                    continue
                h.tt("pool", ELn[:], E[:], b3(masks["Ls"][:], 6, 128), ALU.mult, [E, masks["Ls"]], [ELn])
                if stop < 1.9:
                    h.memset("pool", yo_[:, :, tc], 0.0, [yo_])
                    continue
                h.tt("pool", ELn[:], ELn[:], s3(nbeta[:], 6, 128), ALU.mult, [ELn, nbeta], [ELn])
                if stop < 2:
                    h.memset("pool", yo_[:, :, tc], 0.0, [yo_])
                    continue
                w1 = w4(W1)
                w2 = w4(W2)
                h.mm([(w1[:, hd // 3, hd % 3, :], kz[hd % 2][:, hd // 2, tc], kn_[:, hd // 2, tc], True, True) for hd in range(6)],
                     [kz[0], kz[1], kn_], [W1])
                h.mm([(w2[:, hd // 3, hd % 3, :], kz[hd % 2][:, hd // 2, tc], qn_[:, hd // 2, tc], True, True) for hd in range(6)],
                     [kz[0], kz[1], qn_], [W2])
                h.tt("dve", v4(Pm[0][:]), w1, v4(ELn[:]), ALU.mult, [W1, ELn], [Pm[0]])
                h.tt("dve", v4(attnT[:]), w2, v4(EU[:]), ALU.mult, [W2, EU], [attnT])
                if stop < 3:
                    h.memset("pool", yo_[:, :, tc], 0.0, [yo_])
                    continue
                h.tr([(PB[:, hd * 128:(hd + 1) * 128], Pm[0][:, hd, :]) for hd in range(6)], identb[:], [Pm[0], identb], [PB])
                h.cp("act", Qm[0][:], PB[:, 0:768].rearrange("p (a l) -> p a l", a=6), [PB], [Qm[0]])
                Nn, NT_ = Pm[0], Qm[0]
                h.tt("pool", Pm[1][:], Nn[:], b3(cmask[:, 0, :], 6, 128), ALU.mult, [Nn, cmask], [Pm[1]])
                h.tt("pool", Qm[1][:], NT_[:], b3(cmask[:, 7, :], 6, 128), ALU.mult, [NT_, cmask], [Qm[1]])
                h.tt("dve", Tb[:], Pm[1][:], b3(identf[:], 6, 128), ALU.add, [Pm[1], identf], [Tb])
                h.tt("dve", Xb[:], Qm[1][:], b3(identf[:], 6, 128), ALU.add, [Qm[1], identf], [Xb])
                Tc, Xc = Tb, Xb
                Tn, Xn = Tb2, Xb2
                for lv in range(1, 7):
                    headmm(W1, NT_, Tc, [NT_, Tc])
                    headmm(W2, Nn, Xc, [Nn, Xc])
                    h.tt("dve", v4(M1b[:]), w4(W1), v4(cmask6[:, lv, :, :]), ALU.mult, [W1, cmask6], [M1b])
                    h.tt("dve", v4(M1pb[:]), w4(W2), v4(cmask6[:, 7 + lv, :, :]), ALU.mult, [W2, cmask6], [M1pb])
                    w1_ = w4(W1)
                    w2_ = w4(W2)
                    h.mm([x_ for hd in range(6) for x_ in (
                        (w1_[:, hd // 3, hd % 3, :], identb[:], Tc[:, hd, :], True, False),
                        (w1_[:, hd // 3, hd % 3, :], Xc[:, hd, :], M1b[:, hd, :], False, True))],
                        [identb, Tc, Xc, M1b], [W1])
                    h.mm([x_ for hd in range(6) for x_ in (
                        (w2_[:, hd // 3, hd % 3, :], identb[:], Xc[:, hd, :], True, False),
                        (w2_[:, hd // 3, hd % 3, :], Tc[:, hd, :], M1pb[:, hd, :], False, True))],
                        [identb, Tc, Xc, M1pb], [W2])
                    h.cp("act", v4(Tn[:]), w4(W1), [W1], [Tn])
                    h.cp("act", v4(Xn[:]), w4(W2), [W2], [Xn])
                    Tc, Xc, Tn, Xn = Tn, Xn, Tc, Xc
                Xb = Xc
                if stop < 4:
                    h.memset("pool", yo_[:, :, tc], 0.0, [yo_])
                    continue
                h.tr([(PB[:, j * 128:(j + 1) * 128], kn_[:, j, tc]) for j in range(3)], identb[:], [kn_, identb], [PB])
                h.tt("dve", x3(kdec[:]), x3(PB[:, 0:384]), s3(dkk[:], 6, 64), ALU.mult, [PB, dkk], [kdec])
                h.tr([(W0[:, 0, j * 128:(j + 1) * 128], v_[:, j, tc]) for j in range(3)], identf[:], [v_, identf], [W0])
                h.cp("act", v_tm[:], W0[:, 0, 0:384], [W0], [v_tm])
                if stop < 5:
                    h.memset("pool", yo_[:, :, tc], 0.0, [yo_])
                    continue
                h.mm([(W1[:, 0, j * 128:(j + 1) * 128], kn_[:, j, tc], Sb[:, j, :], True, True) for j in range(3)], [kn_, Sb], [W1])
                h.tt("dve", x3(rr_[:]), x3(W1[:, 0, 0:384]), s3(egc[:], 6, 64), ALU.mult, [W1, egc], [rr_])
                h.tt("pool", rr_[:], rr_[:], v_tm[:], ALU.subtract, [rr_, v_tm], [rr_])
                h.tt("pool", x3(rb[:]), x3(rr_[:]), s3(nbeta[:], 6, 64), ALU.mult, [rr_, nbeta], [rb])
                h.mm([(W2[:, 0, hd * 64:(hd + 1) * 64], Xb[:, hd, :], rb[:, hd * 64:(hd + 1) * 64], True, True) for hd in range(6)], [Xb, rb], [W2])
                h.cp("act", vnb[:], W2[:, 0, 0:384], [W2], [vnb])
                h.mm([(W1[:, 0, j * 128:(j + 1) * 128], qn_[:, j, tc], Sb[:, j, :], True, True) for j in range(3)], [qn_, Sb], [W1])
                h.tt("dve", x3(oa[:]), x3(W1[:, 0, 0:384]), s3(egc[:], 6, 64), ALU.mult, [W1, egc], [oa])
                h.mm([(W2[:, 0, hd * 64:(hd + 1) * 64], attnT[:, hd, :], vnb[:, hd * 64:(hd + 1) * 64], True, True) for hd in range(6)], [attnT, vnb], [W2])
                h.tt("dve", o[:], W2[:, 0, 0:384], oa[:], ALU.add, [W2, oa], [o])
                h.mm([(W0[:, 0, j * 128:(j + 1) * 128], kdec[:, j * 128:(j + 1) * 128], vnb[:, j * 128:(j + 1) * 128], True, True) for j in range(3)],
                     [kdec, vnb], [W0])
                h.tt("dve", tmpS[:], W0[:, 0, 0:384].rearrange("p (j c) -> p j c", j=3), b3(masks["bd"][:], 3, 128), ALU.mult, [W0, masks["bd"]], [tmpS])
                h.tt("pool", Sf[:], Sf[:], s3(gtc[:], 3, 128), ALU.mult, [Sf, gtc], [Sf])
                h.tt("pool", Sf[:], Sf[:], tmpS[:], ALU.add, [Sf, tmpS], [Sf])
                h.cp("act", Sb[:], Sf[:], [Sf], [Sb])
                if stop < 6:
                    h.memset("pool", yo_[:, :, tc], 0.0, [yo_])
                    continue
                h.tt("pool", sq[:], o[:], o[:], ALU.mult, [o], [sq])
                h.reduce(ss6[:], x3(sq[:]), ALU.add, [sq], [ss6])
                h.act(rs6[:], ss6[:], AF.Ln, [ss6, epsb], [rs6], bias=epsb[:, 0:1], scale=1.0 / 64)
                h.act(rs6[:], rs6[:], AF.Exp, [rs6], [rs6], scale=-0.5)
                h.tt("dve", x3(o[:]), x3(o[:]), s3(rs6[:], 6, 64), ALU.mult, [o, rs6], [o])
                h.tt("pool", x3(o[:]), x3(o[:]), b3(gnw[:], 6, 64), ALU.mult, [o, gnw], [o])
                h.act(ez[:], pj_[:, ti, 0:384], AF.Exp, [pj_], [ez], scale=-1.0)
                h.ts("pool", ez[:], ez[:], 1.0, None, ALU.add, None, [ez], [ez])
                h.tt("pool", zg[:], o[:], pj_[:, ti, 0:384], ALU.mult, [o, pj_], [zg])
                h.recip(ez[:], ez[:], [ez], [ez])
                h.tt("pool", yb[:], zg[:], ez[:], ALU.mult, [zg, ez], [yb])
                h.tr([(PB[:, j * 128:(j + 1) * 128], yb[:, j * 128:(j + 1) * 128]) for j in range(3)], identb[:], [yb, identb], [PB])
                h.cp("act", yo_[:, :, tc], PB[:, 0:384].rearrange("p (j t) -> p j t", j=3), [PB], [yo_])
            h.dma("sp", k.y_d.t[256:640, cols].rearrange("(j p) t -> p j t", p=128), yo_[:], [yo_], [k.r_y[1][mt]], yo_)
        P.end_phase()


class K:
    def __init__(self, T, L, flags):
        self.T = T
        self.L = L
        self.NT = T // 128
        self.flags = flags


def bcast(ap, shape):
    return ap.to_broadcast(list(shape))


def build(T, L, flags=None):
    flags = flags or {}
    nc = bass.Bass("TRN2", target_bir_lowering=False)
    k = K(T, L, flags)
    NT = T // 128
    with contextlib.ExitStack() as es0:
        P = Prog(nc, es0)
        P.verbose = bool(flags.get("verbose"))
        k.P = P
        inp = {}

        def ein(name, shape):
            inp[name] = P.dram(name, shape, F32, "ExternalInput")
            return inp[name]

        x_in = ein("x", [T, D])
        c_in = ein("c", [1, D])
        w_ada = ein("w_ada", [L, D, 6 * D])
        b_ada = ein("b_ada", [L, 6 * D])
        norm_mix = ein("norm_mix", [L, D])
        norm_ffn = ein("norm_ffn", [L, D])
        norm_final = ein("norm_final", [1, D])
        w_rt = ein("w_rt", [L, D, 36])
        b_rt = ein("b_rt", [L, 36])
        w_gate = ein("moe_w_gate", [L, NEXP, D, DEXP])
        w_up = ein("moe_w_up", [L, NEXP, D, DEXP])
        w_down = ein("moe_w_down", [L, NEXP, DEXP, D])
        ein("w_in_f", [L, D, 2304])
        ein("w_in_t", [L, D, 786])
        ein("w_out", [L, D, D])
        ein("conv_w", [L, 128, 16, 4])
        ein("conv_b", [L, 128, 16])
        ein("bias12", [L, 12])
        ein("alog12", [L, 12])
        ein("ssd_d", [L, 6])
        ein("ssd_norm", [L, 384])
        ein("gdn_norm", [L, 64])
        ein("gdn_cmask", [14, 128, 128])
        ein("s5_are", [L, 128, 8])
        ein("s5_aim", [L, 128, 8])
        ein("s5_ldt", [L, 128, 8])
        ein("s5_dcol", [L, 128, 2])
        ein("s5_ncol", [L, 128, 2])
        ein("s5_bT_re", [L, 8, 128, 128])
        ein("s5_bT_im", [L, 8, 128, 128])
        ein("s5_cT_re", [L, 8, 128, 128])
        ein("s5_cT_im", [L, 8, 128, 128])
        ein("s5_w_glu", [L, 256, 256])
        out = P.dram("out", [T, D], F32, "ExternalOutput")
        k.u_d = P.dram("u_d", [256, T], F32)
        k.qn_d = P.dram("qn_d", [384, T], BF16)
        k.kn_d = P.dram("kn_d", [384, T], BF16)
        k.v_d = P.dram("v_d", [384, T], F32)
        k.xs_d = P.dram("xs_d", [384, T], F32)
        k.B_d = P.dram("B_d", [256, T], BF16)
        k.C_d = P.dram("C_d", [256, T], BF16)
        k.pt_d = P.dram("pt_d", [T, 786], F32)
        k.y_d = P.dram("y_d", [D, T], BF16)
        k.r_pre = [P.region(f"pre_{i}") for i in range(max(1, T // 512))]
        k.r_y = [[P.region(f"y{j}_{i}") for i in range(max(1, T // 512))] for j in range(3)]
        k.h = H(P)
        h = k.h
        modv = P.dram("modv", [L, 6 * D], F32)
        dbg = flags.get("dbg", False)
        if dbg:
            dbg_coef = P.dram("dbg_coef", [T, 32], F32, "ExternalOutput")
            dbg_y = P.dram("dbg_y", [T, D], F32, "ExternalOutput")
            dbg_h = P.dram("dbg_h", [T, D], F32, "ExternalOutput")
            r_dbg = P.region("dbg")
        scr = [P.dram("xs0", [T, D], F32), P.dram("xs1", [T, D], F32)]
        k.inp = inp

        def regs(name):
            return [P.region(f"{name}_{i}") for i in range(NT)]
        r_x = regs("x")
        r_scr = [regs("xs0"), regs("xs1")]
        r_out = regs("out")
        r_modv = P.region("modv")

        identf = P.sb(es0, "identf", [128, 128], F32)
        identb = P.sb(es0, "identb", [128, 128], BF16)
        P.op("pool", lambda e: e.memset(identf[:], 0.0), writes=[identf])
        P.op("pool", lambda e: e.affine_select(out=identf[:], in_=identf[:], pattern=[[-1, 128]],
                                               compare_op=ALU.not_equal, fill=1.0, base=0,
                                               channel_multiplier=1),
             reads=[identf], writes=[identf])
        P.op("dve", lambda e: e.tensor_copy(out=identb[:], in_=identf[:]), reads=[identf], writes=[identb])
        k.identf, k.identb = identf, identb

        with contextlib.ExitStack() as es:
            ccol = P.sb(es, "ccol", [128, 8], F32)
            cb = P.sb(es, "cb", [128, 8, 128], BF16)
            wa = [P.sb(es, f"wa{i}", [128, 8, 512], BF16) for i in range(2)]
            pm = [P.ps(es, f"pm{i}", [128, 512], F32) for i in range(2)]
            brow = [P.sb(es, f"brow{i}", [1, 512], F32) for i in range(2)]
            mrow = [P.sb(es, f"mrow{i}", [1, 512], F32) for i in range(2)]
            P.op("sp", lambda e: e.dma_start(out=ccol[:], in_=c_in.t.rearrange("o (k p) -> p (o k)", p=128),
                                             allow_slow_non_contiguous=True),
                 writes=[ccol], dma=ccol)
            P.op("act", lambda e: e.activation(out=ccol[:], in_=ccol[:], func=AF.Silu), reads=[ccol], writes=[ccol])
            P.op("dve", lambda e: e.tensor_copy(out=cb[:], in_=bcast(ccol[:].unsqueeze(2), [128, 8, 128])),
                 reads=[ccol], writes=[cb])
            it = 0
            for l in range(L):
                for n in range(12):
                    i = it % 2
                    it += 1
                    P.op("pool", lambda e, l=l, n=n, i=i: e.dma_start(
                        out=wa[i][:], in_=w_ada.t[l, :, n * 512:(n + 1) * 512].rearrange("(k p) n -> p k n", p=128)),
                        writes=[wa[i]], dma=wa[i])
                    P.op("sp", lambda e, l=l, n=n, i=i: e.dma_start(
                        out=brow[i][:], in_=b_ada.t[l:l + 1, n * 512:(n + 1) * 512]),
                        writes=[brow[i]], dma=brow[i])

                    def mm(e, i=i):
                        r = None
                        for kk in range(8):
                            r = e.matmul(pm[i][:], lhsT=cb[:, kk, :], rhs=wa[i][:, kk, :], start=(kk == 0), stop=(kk == 7))
                        return r
                    P.op("pe", mm, reads=[cb, wa[i]], writes=[pm[i]])
                    P.op("dve", lambda e, i=i: e.tensor_tensor(out=mrow[i][:], in0=pm[i][0:1, :], in1=brow[i][:], op=ALU.add),
                         reads=[pm[i], brow[i]], writes=[mrow[i]])
                    P.op("sp", lambda e, l=l, n=n, i=i: e.dma_start(
                        out=modv.t[l:l + 1, n * 512:(n + 1) * 512], in_=mrow[i][:]),
                        reads=[mrow[i]], writes=[r_modv], dma=mrow[i])
            P.end_phase()

        def load_row(dst, src_ap, extra_reads=()):
            P.op("sp", lambda e: e.dma_start(out=dst[:], in_=src_ap.partition_broadcast(128)),
                 reads=list(extra_reads), writes=[dst], dma=dst)

        def norm_consts(es, l, nw, i_scale, i_shift, tag):
            A = P.sb(es, f"A{tag}", [128, D], F32)
            B = P.sb(es, f"B{tag}", [128, D], F32)
            W = P.sb(es, f"W{tag}", [128, D], F32)
            load_row(A, modv.t[l:l + 1, i_scale * D:(i_scale + 1) * D], [r_modv])
            load_row(B, modv.t[l:l + 1, i_shift * D:(i_shift + 1) * D], [r_modv])
            load_row(W, nw)
            P.op("dve", lambda e: e.scalar_tensor_tensor(out=A[:], in0=A[:], scalar=1.0, in1=W[:], op0=ALU.add, op1=ALU.mult),
                 reads=[A, W], writes=[A])
            return A, B

        def rms_mod(xt, A, B, hout, ssq, rstd, junk, tmp):
            P.op("act", lambda e: e.activation(out=junk[:], in_=xt[:], func=AF.Square, accum_out=ssq[:]),
                 reads=[xt], writes=[junk, ssq], est=0.9)
            P.op("dve", lambda e: e.tensor_scalar(out=rstd[:], in0=ssq[:], scalar1=1.0 / D, scalar2=EPS, op0=ALU.mult, op1=ALU.add),
                 reads=[ssq], writes=[rstd])
            P.op("act", lambda e: e.activation(out=rstd[:], in_=rstd[:], func=AF.Sqrt), reads=[rstd], writes=[rstd])
            P.op("dve", lambda e: e.reciprocal(out=rstd[:], in_=rstd[:]), reads=[rstd], writes=[rstd])
            if B is None:
                P.op("dve", lambda e: e.scalar_tensor_tensor(out=hout[:], in0=xt[:], scalar=rstd[:, 0:1], in1=A[:], op0=ALU.mult, op1=ALU.mult),
                     reads=[xt, rstd, A], writes=[hout], est=1.15)
            else:
                P.op("dve", lambda e: e.scalar_tensor_tensor(out=tmp[:], in0=xt[:], scalar=rstd[:, 0:1], in1=A[:], op0=ALU.mult, op1=ALU.mult),
                     reads=[xt, rstd, A], writes=[tmp], est=1.15)
                P.op("pool", lambda e: e.tensor_tensor(out=hout[:], in0=tmp[:], in1=B[:], op=ALU.add),
                     reads=[tmp, B], writes=[hout], est=1.3)

        k.modv, k.r_modv = modv, r_modv
        k.load_row, k.norm_consts, k.rms_mod = load_row, norm_consts, rms_mod
        cur = (x_in, r_x)
        nxt_i = 0

        def next_dst():
            nonlocal nxt_i
            d = (scr[nxt_i], r_scr[nxt_i])
            nxt_i ^= 1
            return d

        for l in range(L):
            if flags.get("mixer", True):
                dstt = next_dst()
                mixer_layer(k, l, cur, dstt)
                cur = dstt
            if flags.get("moe", True):
                src, rsrc = cur
                dst, rdst = next_dst()
                SBT = min(16, NT)
                with contextlib.ExitStack() as es:
                    A, B = norm_consts(es, l, norm_ffn.t[l:l + 1, :], 4, 3, "f")
                    G = P.sb(es, "Gf", [128, D], F32)
                    load_row(G, modv.t[l:l + 1, 5 * D:6 * D], [r_modv])
                    brt = P.sb(es, "brt", [128, 36], F32)
                    load_row(brt, b_rt.t[l:l + 1, :])
                    wrt = P.sb(es, "wrt", [128, 8, 36], F32)
                    P.op("sp", lambda e: e.dma_start(out=wrt[:], in_=w_rt.t[l].rearrange("(k p) n -> p k n", p=128)),
                         writes=[wrt], dma=wrt)
                    hT = P.sb(es, "hT", [128, 8, SBT * 128], BF16)
                    yacc = [P.sb(es, f"yacc{i}", [128, D], F32) for i in range(SBT)]
                    coef = P.sb(es, "coef", [128, SBT, 32], F32)
                    xt = [P.sb(es, f"xt{i}", [128, D], F32) for i in range(2)]
                    hf = P.sb(es, "hf", [128, D], F32)
                    tmp = P.sb(es, "tmpf", [128, D], F32)
                    junk = P.sb(es, "junkf", [128, D], F32)
                    hTf = P.sb(es, "hTf", [128, 8, 128], F32)
                    ssq = P.sb(es, "ssq", [128, 1], F32)
                    rstd = P.sb(es, "rstd", [128, 1], F32)
                    lg = P.sb(es, "lg", [128, 36], F32)
                    sm = P.sb(es, "sm", [128, 16], F32)
                    gm = P.sb(es, "gm", [128, 4], F32)
                    gex = P.sb(es, "gex", [128, 4], F32)
                    le4 = P.sb(es, "le4", [128, 4, 8], F32)
                    les = P.sb(es, "les", [128, 8], F32)
                    le2 = P.sb(es, "le2", [128, 8], F32)
                    mk1 = P.sb(es, "mk1", [128, 8], F32)
                    mk2 = P.sb(es, "mk2", [128, 8], F32)
                    csel = P.sb(es, "csel", [128, 8], F32)
                    wg = [P.sb(es, f"wg{i}", [128, 8, DEXP], BF16) for i in range(2)]
                    wu = [P.sb(es, f"wu{i}", [128, 8, DEXP], BF16) for i in range(2)]
                    wd = [P.sb(es, f"wd{i}", [128, 2, D], BF16) for i in range(2)]
                    sg = [P.sb(es, f"sg{i}", [128, 512], F32) for i in range(2)]
                    hid = [P.sb(es, f"hid{i}", [128, 2, 512], BF16) for i in range(2)]
                    ptr = P.ps(es, "ptr", [128, 8, 128], F32)
                    pg = [P.ps(es, f"pg{i}", [128, 512], F32) for i in range(2)]
                    pu = [P.ps(es, f"pu{i}", [128, 512], F32) for i in range(2)]
                    py = [P.ps(es, f"py{i}", [128, 512], F32) for i in range(2)]

                    wcnt = 0
                    for sb0 in range(0, NT, SBT):
                        for ti in range(SBT):
                            t = sb0 + ti
                            xb = xt[t % 2]
                            P.op("sp", lambda e, t=t, xb=xb: e.dma_start(out=xb[:], in_=src.t[t * 128:(t + 1) * 128, :]),
                                 reads=[rsrc[t]], writes=[xb], dma=xb)
                            rms_mod(xb, A, B, hf, ssq, rstd, junk, tmp)

                            if dbg and l == 0:
                                P.op("sp", lambda e, t=t: e.dma_start(out=dbg_h.t[t * 128:(t + 1) * 128, :], in_=hf[:]),
                                     reads=[hf], writes=[r_dbg], dma=hf)

                            def trf(e):
                                r = None
                                for kk in range(8):
                                    r = e.transpose(out=ptr[:, kk, :], in_=hf[:, kk * 128:(kk + 1) * 128], identity=identf[:])
                                return r
                            P.op("pe", trf, reads=[hf, identf], writes=[ptr])
                            P.op("act", lambda e: e.copy(out=hTf[:], in_=ptr[:]), reads=[ptr], writes=[hTf])
                            P.op("dve", lambda e, ti=ti: e.tensor_copy(out=hT[:, :, ti * 128:(ti + 1) * 128], in_=hTf[:]),
                                 reads=[hTf], writes=[hT])

                            def mrt(e):
                                r = None
                                for kk in range(8):
                                    r = e.matmul(ptr[:, 0, 0:36], lhsT=hTf[:, kk, :], rhs=wrt[:, kk, :], start=(kk == 0), stop=(kk == 7))
                                return r
                            P.op("pe", mrt, reads=[hTf, wrt], writes=[ptr])
                            P.op("dve", lambda e: e.tensor_tensor(out=lg[:], in0=ptr[:, 0, 0:36], in1=brt[:], op=ALU.add),
                                 reads=[ptr, brt], writes=[lg])
                            P.op("dve", lambda e: e.tensor_reduce(out=sm[:, 0:1], in_=lg[:, 0:4], axis=AX.X, op=ALU.max),
                                 reads=[lg], writes=[sm])
                            P.op("dve", lambda e: e.tensor_scalar(out=gm[:], in0=lg[:, 0:4], scalar1=sm[:, 0:1], scalar2=None, op0=ALU.is_equal),
                                 reads=[lg, sm], writes=[gm])
                            P.op("dve", lambda e: e.tensor_scalar(out=sm[:, 1:2], in0=sm[:, 0:1], scalar1=-1.0, scalar2=None, op0=ALU.mult),
                                 reads=[sm], writes=[sm])
                            P.op("act", lambda e: e.activation(out=gex[:], in_=lg[:, 0:4], func=AF.Exp, bias=sm[:, 1:2], accum_out=sm[:, 2:3]),
                                 reads=[lg, sm], writes=[gex, sm])
                            P.op("dve", lambda e: e.reciprocal(out=sm[:, 3:4], in_=sm[:, 2:3]), reads=[sm], writes=[sm])
                            P.op("dve", lambda e: e.tensor_tensor(out=le4[:], in0=lg[:, 4:36].rearrange("p (g e) -> p g e", g=4),
                                                                  in1=bcast(gm[:].unsqueeze(2), [128, 4, 8]), op=ALU.mult),
                                 reads=[lg, gm], writes=[le4])
                            P.op("dve", lambda e: e.tensor_reduce(out=les[:], in_=le4[:].rearrange("p g e -> p e g"), axis=AX.X, op=ALU.add),
                                 reads=[le4], writes=[les])
                            P.op("dve", lambda e: e.tensor_reduce(out=sm[:, 4:5], in_=les[:], axis=AX.X, op=ALU.max), reads=[les], writes=[sm])
                            P.op("dve", lambda e: e.tensor_scalar(out=mk1[:], in0=les[:], scalar1=sm[:, 4:5], scalar2=None, op0=ALU.is_equal),
                                 reads=[les, sm], writes=[mk1])
                            P.op("dve", lambda e: e.scalar_tensor_tensor(out=le2[:], in0=mk1[:], scalar=-1e30, in1=les[:], op0=ALU.mult, op1=ALU.add),
                                 reads=[mk1, les], writes=[le2])
                            P.op("dve", lambda e: e.tensor_reduce(out=sm[:, 5:6], in_=le2[:], axis=AX.X, op=ALU.max), reads=[le2], writes=[sm])
                            P.op("dve", lambda e: e.tensor_scalar(out=mk2[:], in0=le2[:], scalar1=sm[:, 5:6], scalar2=None, op0=ALU.is_equal),
                                 reads=[le2, sm], writes=[mk2])
                            P.op("dve", lambda e: e.tensor_tensor(out=sm[:, 6:7], in0=sm[:, 4:5], in1=sm[:, 5:6], op=ALU.subtract),
                                 reads=[sm], writes=[sm])
                            P.op("act", lambda e: e.activation(out=sm[:, 7:8], in_=sm[:, 6:7], func=AF.Sigmoid), reads=[sm], writes=[sm])
                            P.op("act", lambda e: e.activation(out=sm[:, 8:9], in_=sm[:, 6:7], func=AF.Sigmoid, scale=-1.0), reads=[sm], writes=[sm])
                            P.op("dve", lambda e: e.tensor_scalar(out=sm[:, 7:9], in0=sm[:, 7:9], scalar1=sm[:, 3:4], scalar2=None, op0=ALU.mult),
                                 reads=[sm], writes=[sm])
                            P.op("dve", lambda e: e.tensor_scalar(out=csel[:], in0=mk1[:], scalar1=sm[:, 7:8], scalar2=None, op0=ALU.mult),
                                 reads=[mk1, sm], writes=[csel])
                            P.op("dve", lambda e: e.scalar_tensor_tensor(out=csel[:], in0=mk2[:], scalar=sm[:, 8:9], in1=csel[:], op0=ALU.mult, op1=ALU.add),
                                 reads=[mk2, sm, csel], writes=[csel])
                            P.op("dve", lambda e, ti=ti: e.tensor_tensor(out=coef[:, ti, :].rearrange("p (g e) -> p g e", g=4),
                                                                         in0=bcast(gm[:].unsqueeze(2), [128, 4, 8]),
                                                                         in1=bcast(csel[:].unsqueeze(1), [128, 4, 8]), op=ALU.mult),
                                 reads=[gm, csel], writes=[coef])
                        nblk = (SBT * 128 + 511) // 512
                        seq = [(ex, blk) for ex in range(NEXP) for blk in range(nblk)]

                        def load_w(ex):
                            wi = ex % 2
                            h.dma("pool", wg[wi][:], w_gate.t[l, ex].rearrange("(k p) n -> p k n", p=128), [], [wg[wi]], wg[wi])
                            h.dma("pool", wu[wi][:], w_up.t[l, ex].rearrange("(k p) n -> p k n", p=128), [], [wu[wi]], wu[wi])
                            h.dma("pool", wd[wi][:], w_down.t[l, ex].rearrange("(k p) n -> p k n", p=128), [], [wd[wi]], wd[wi])

                        def GU(i):
                            ex, blk = seq[i]
                            wi, bi = ex % 2, i % 2
                            c0 = blk * 512
                            cw = min(512, SBT * 128 - c0)
                            for fc in range(2):
                                fs = slice(fc * 128, (fc + 1) * 128)
                                h.mm([(pg[fc][:, 0:cw], wg[wi][:, kk, fs], hT[:, kk, c0:c0 + cw], kk == 0, kk == 7) for kk in range(8)],
                                     [wg[wi], hT], [pg[fc]])
                                h.act(sg[fc][:, 0:cw], pg[fc][:, 0:cw], AF.Silu, [pg[fc]], [sg[fc]])
                                yield
                                h.mm([(pu[fc][:, 0:cw], wu[wi][:, kk, fs], hT[:, kk, c0:c0 + cw], kk == 0, kk == 7) for kk in range(8)],
                                     [wu[wi], hT], [pu[fc]])
                                h.tt("dve", hid[bi][:, fc, 0:cw], sg[fc][:, 0:cw], pu[fc][:, 0:cw], ALU.mult, [sg[fc], pu[fc]], [hid[bi]])
                                yield

                        def DN(i):
                            ex, blk = seq[i]
                            wi, bi = ex % 2, i % 2
                            c0 = blk * 512
                            cw = min(512, SBT * 128 - c0)
                            for st in range(cw // 128):
                                ti = blk * 4 + st
                                for n2 in range(2):
                                    pi = n2
                                    ns = slice(n2 * 512, (n2 + 1) * 512)
                                    h.mm([(py[pi][:], hid[bi][:, fc, st * 128:(st + 1) * 128], wd[wi][:, fc, ns], fc == 0, fc == 1) for fc in range(2)],
                                         [hid[bi], wd[wi]], [py[pi]])
                                    if ex == 0:
                                        h.ts("dve", yacc[ti][:, ns], py[pi][:], coef[:, ti, ex:ex + 1], None, ALU.mult, None, [py[pi], coef], [yacc[ti]])
                                    else:
                                        h.stt("dve", yacc[ti][:, ns], py[pi][:], coef[:, ti, ex:ex + 1], yacc[ti][:, ns], ALU.mult, ALU.add,
                                              [py[pi], coef, yacc[ti]], [yacc[ti]])
                                    yield
                            if blk == nblk - 1 and ex + 2 < NEXP:
                                load_w(ex + 2)

                        def drain(g):
                            for _ in g:
                                pass

                        def step(g, n):
                            for _ in range(n):
                                if next(g, "END") == "END":
                                    return

                        load_w(0)
                        load_w(1)
                        drain(GU(0))
                        for i in range(len(seq)):
                            gd = DN(i)
                            if i + 1 < len(seq):
                                gg = GU(i + 1)
                                for _ in range(4):
                                    step(gg, 1)
                                    step(gd, 2)
                                drain(gg)
                            drain(gd)
                        for ti in range(SBT):
                            t = sb0 + ti
                            if dbg and l == 0:
                                P.op("sp", lambda e, t=t, ti=ti: e.dma_start(out=dbg_y.t[t * 128:(t + 1) * 128, :], in_=yacc[ti][:]),
                                     reads=[yacc[ti]], writes=[r_dbg], dma=yacc[ti])
                                P.op("sp", lambda e, t=t, ti=ti: e.dma_start(out=dbg_coef.t[t * 128:(t + 1) * 128, :], in_=coef[:, ti, :]),
                                     reads=[coef], writes=[r_dbg], dma=coef)
                            xb = xt[t % 2]
                            P.op("sp", lambda e, t=t, xb=xb: e.dma_start(out=xb[:], in_=src.t[t * 128:(t + 1) * 128, :]),
                                 reads=[rsrc[t]], writes=[xb], dma=xb)
                            P.op("pool", lambda e, ti=ti: e.tensor_tensor(out=yacc[ti][:], in0=yacc[ti][:], in1=G[:], op=ALU.mult),
                                 reads=[yacc[ti], G], writes=[yacc[ti]])
                            P.op("dve", lambda e, ti=ti, xb=xb: e.tensor_tensor(out=yacc[ti][:], in0=yacc[ti][:], in1=xb[:], op=ALU.add),
                                 reads=[yacc[ti], xb], writes=[yacc[ti]])
                            P.op("sp", lambda e, t=t, ti=ti: e.dma_start(out=dst.t[t * 128:(t + 1) * 128, :], in_=yacc[ti][:]),
                                 reads=[yacc[ti]], writes=[rdst[t]], dma=yacc[ti])
                    P.end_phase()
                cur = (dst, rdst)

        src, rsrc = cur
        with contextlib.ExitStack() as es:
            Wn = P.sb(es, "Wn", [128, D], F32)
            load_row(Wn, norm_final.t[0:1, :])
            xt = [P.sb(es, f"xtn{i}", [128, D], F32) for i in range(2)]
            ho = [P.sb(es, f"hon{i}", [128, D], F32) for i in range(2)]
            junk = P.sb(es, "junkn", [128, D], F32)
            ssq = P.sb(es, "ssqn", [128, 1], F32)
            rstd = P.sb(es, "rstdn", [128, 1], F32)
            for t in range(NT):
                xb = xt[t % 2]
                hb = ho[t % 2]
                P.op("sp", lambda e, t=t, xb=xb: e.dma_start(out=xb[:], in_=src.t[t * 128:(t + 1) * 128, :]),
                     reads=[rsrc[t]], writes=[xb], dma=xb)
                rms_mod(xb, Wn, None, hb, ssq, rstd, junk, None)
                P.op("sp", lambda e, t=t, hb=hb: e.dma_start(out=out.t[t * 128:(t + 1) * 128, :], in_=hb[:]),
                     reads=[hb], writes=[r_out[t]], dma=hb)
            P.final_wait("sp", r_out)
            P.end_phase()
    return nc


def host_inputs(inputs, L, T):
    f = lambda a: np.ascontiguousarray(np.asarray(a, dtype=np.float32))
    w_rt = f(np.concatenate([inputs["moe_w_grp"][:L], inputs["moe_w_rt"][:L]], axis=-1))
    b_rt = f(np.concatenate([inputs["moe_b_grp"][:L], inputs["moe_b_rt"][:L]], axis=-1))
    w_in = np.asarray(inputs["w_in"][:L], dtype=np.float32)
    w_in_f = np.concatenate([w_in[:, :, 0:1408], w_in[:, :, 2188:3084]], axis=-1)
    w_in_t = np.concatenate([w_in[:, :, 1408:1792], w_in[:, :, 1804:2188], w_in[:, :, 1792:1798],
                             w_in[:, :, 3084:3090], w_in[:, :, 1798:1804]], axis=-1)
    gcw = np.asarray(inputs["gdn_conv_w"][:L], dtype=np.float32)
    scw = np.asarray(inputs["ssd_conv_w"][:L], dtype=np.float32)
    cw = np.concatenate([gcw, scw], axis=-1)
    conv_w = cw.reshape(L, 4, 16, 128).transpose(0, 3, 2, 1)
    cb = np.concatenate([np.zeros((L, 1152), np.float32), np.asarray(inputs["ssd_conv_b"][:L], dtype=np.float32)], axis=-1)
    conv_b = cb.reshape(L, 16, 128).transpose(0, 2, 1)
    bias12 = np.concatenate([inputs["gdn_dt_bias"][:L], inputs["ssd_dt_bias"][:L]], axis=-1)
    alog12 = np.concatenate([inputs["gdn_a_log"][:L], inputs["ssd_a_log"][:L]], axis=-1)
    def st_layout(a):
        a = np.asarray(a[:L], dtype=np.float32)
        return a.reshape(L, 8, 2, 64).transpose(0, 2, 3, 1).reshape(L, 128, 8)
    ldt = np.repeat(np.asarray(inputs["s5_log_dt"][:L], dtype=np.float32)[:, :, None], 64, axis=2)
    def bT_layout(b):
        b = np.asarray(b[:L], dtype=np.float32)
        o = np.zeros((L, 8, 128, 128), np.float32)
        for sc in range(8):
            for gl in range(2):
                r0 = 32 * (sc % 4) + 16 * gl
                o[:, sc, r0:r0 + 16, gl * 64:(gl + 1) * 64] = b[:, 2 * sc + gl].transpose(0, 2, 1)
        return o
    def cT_layout(c):
        c = np.asarray(c[:L], dtype=np.float32)
        o = np.zeros((L, 8, 128, 128), np.float32)
        for sc in range(8):
            for gl in range(2):
                r0 = 32 * (sc % 4) + 16 * gl
                o[:, sc, gl * 64:(gl + 1) * 64, r0:r0 + 16] = c[:, 2 * sc + gl].transpose(0, 2, 1)
        return o
    ii = np.arange(128)[:, None]
    jj = np.arange(128)[None, :]
    cm = []
    for lv in range(7):
        s_ = 1 << lv
        cm.append(((ii // (2 * s_) == jj // (2 * s_)) & (ii % (2 * s_) >= s_) & (jj % (2 * s_) < s_)).astype(np.float32))
    cmask = np.stack(cm + [m.T for m in cm], axis=0)
    col2 = lambda a: np.asarray(a[:L], dtype=np.float32).reshape(L, 2, 128).transpose(0, 2, 1)
    shared = {
        "gdn_cmask": f(cmask),
        "s5_are": f(st_layout(inputs["s5_a_re"])), "s5_aim": f(st_layout(inputs["s5_a_im"])), "s5_ldt": f(st_layout(ldt)),
        "s5_dcol": f(col2(inputs["s5_d"])), "s5_ncol": f(col2(inputs["s5_norm"])),
        "s5_bT_re": f(bT_layout(inputs["s5_b_re"])), "s5_bT_im": f(bT_layout(inputs["s5_b_im"])),
        "s5_cT_re": f(cT_layout(inputs["s5_c_re"])), "s5_cT_im": f(cT_layout(inputs["s5_c_im"])),
        "s5_w_glu": f(inputs["s5_w_glu"][:L]),
        "w_in_f": f(w_in_f), "w_in_t": f(w_in_t), "w_out": f(inputs["w_out"][:L]),
        "conv_w": f(conv_w), "conv_b": f(conv_b), "bias12": f(bias12), "alog12": f(alog12),
        "ssd_d": f(inputs["ssd_d"][:L]), "ssd_norm": f(inputs["ssd_norm"][:L]), "gdn_norm": f(inputs["gdn_norm"][:L]),
        "w_ada": f(inputs["w_ada"][:L]), "b_ada": f(inputs["b_ada"][:L]),
        "norm_mix": f(inputs["norm_mix"][:L]), "norm_ffn": f(inputs["norm_ffn"][:L]),
        "norm_final": f(inputs["norm_final"]).reshape(1, D),
        "w_rt": w_rt, "b_rt": b_rt,
        "moe_w_gate": f(inputs["moe_w_gate"][:L]), "moe_w_up": f(inputs["moe_w_up"][:L]),
        "moe_w_down": f(inputs["moe_w_down"][:L]),
    }
    maps = []
    B = inputs["x"].shape[0]
    for b in range(B):
        m = dict(shared)
        m["x"] = f(inputs["x"][b, :T])
        m["c"] = f(inputs["c"][b]).reshape(1, D)
        maps.append(m)
    return maps


def run(inputs, L, T, flags=None, trace=False):
    nc = build(T, L, flags)
    maps = host_inputs(inputs, L, T)
    res = run_bass_kernel_spmd(nc, maps, core_ids=list(range(len(maps))))
    if flags and flags.get("dbg"):
        return res.results
    return np.stack([r["out"] for r in res.results], axis=0)


def kernel(**inputs):
    return run(inputs, 4, 4096).astype(np.float32)
```

```python
import contextlib
import math
import numpy as np
import concourse.bass as bass
import concourse.mybir as mybir
from concourse.bass_utils import run_bass_kernel_spmd

F32 = mybir.dt.float32
BF16 = mybir.dt.bfloat16
ALU = mybir.AluOpType
AF = mybir.ActivationFunctionType
AX = mybir.AxisListType

D = 1024
NEXP = 32
DEXP = 256
EPS = 1e-6
ENGS = ("pe", "act", "dve", "pool", "sp")


class Buf:
    def __init__(self, t, name, multi=False):
        self.t = t
        self.name = name
        self.w = {}
        self.r = {}
        self.sem = None
        self.dcnt = 0
        self.multi = multi

    def __getitem__(self, k):
        return self.t[k]


class Prog:
    SEM_LAT = 0.15

    def __init__(self, nc, es):
        self.nc = nc
        self.es = es
        self.ops = []
        self.sems = []
        self.esem = {}
        self.ecnt = {e: 0 for e in ENGS}
        self.waited = {e: {} for e in ENGS}
        for e in ENGS:
            if e != "sp":
                self.esem[e] = self.newsem("e_" + e)
        self.uid = 0
        self.dsem_pool = []
        self.dbufs = []
        self.phase_bufs = []

    def newsem(self, name):
        s = self.es.enter_context(self.nc.semaphore(name))
        self.sems.append(s)
        return len(self.sems) - 1

    def sb(self, es, name, shape, dtype):
        self.uid += 1
        name = f"{name}_{self.uid}"
        t = es.enter_context(self.nc.sbuf_tensor(name, list(shape), dtype))
        b = Buf(t, name)
        self.phase_bufs.append(b)
        return b

    def ps(self, es, name, shape, dtype):
        self.uid += 1
        name = f"{name}_{self.uid}"
        t = es.enter_context(self.nc.psum_tensor(name, list(shape), dtype))
        return Buf(t, name)

    def dram(self, name, shape, dtype, kind="Internal"):
        t = self.nc.dram_tensor(name, list(shape), dtype, kind=kind).ap()
        return Buf(t, name)

    def region(self, name):
        return Buf(None, name, multi=True)

    def op(self, eng, fn, reads=(), writes=(), dma=None, est=None):
        if est is None:
            est = {"pe": 1.0, "act": 0.5, "dve": 0.35, "pool": 0.45, "sp": 3.0}[eng] if dma is None else 3.0
        self.ops.append((eng, fn, tuple(reads), tuple(writes), dma, est))

    def final_wait(self, eng, bufs):
        pass

    def end_phase(self):
        ops = self.ops
        self.ops = []
        n = len(ops)
        lw, rd = {}, {}
        deps = [None] * n
        for i, (eng, fn, reads, writes, dma, est) in enumerate(ops):
            d = set()
            for b in reads:
                d.update(lw.get(id(b), ()))
            for b in writes:
                d.update(rd.get(id(b), ()))
                if not b.multi:
                    d.update(lw.get(id(b), ()))
            deps[i] = d
            for b in reads:
                rd.setdefault(id(b), []).append(i)
            for b in writes:
                if b.multi:
                    lw.setdefault(id(b), []).append(i)
                else:
                    lw[id(b)] = [i]
                    rd[id(b)] = []
        import heapq
        succ = [[] for _ in range(n)]
        indeg = [0] * n
        for i in range(n):
            indeg[i] = len(deps[i])
            for j in deps[i]:
                succ[j].append(i)
        ready_t = [0.0] * n
        fin = [0.0] * n
        start = [0.0] * n
        efree = {e: 0.0 for e in ENGS}
        heap = [(0.0, i) for i in range(n) if indeg[i] == 0]
        heapq.heapify(heap)
        order = {e: [] for e in ENGS}
        glob = []
        while heap:
            rt, i = heapq.heappop(heap)
            eng, fn, reads, writes, dma, est = ops[i]
            st = max(rt, efree[eng])
            start[i] = st
            if dma is not None:
                occ = 0.5 if eng == "pool" else 0.08
                efree[eng] = st + occ
                fin[i] = st + occ + est
            else:
                efree[eng] = st + est
                fin[i] = st + est
            order[eng].append(i)
            glob.append(i)
            for k2 in succ[i]:
                indeg[k2] -= 1
                if ready_t[k2] < fin[i] + self.SEM_LAT:
                    ready_t[k2] = fin[i] + self.SEM_LAT
                if indeg[k2] == 0:
                    heapq.heappush(heap, (ready_t[k2], k2))
        assert len(glob) == n, "dependency cycle"
        tok = [None] * n
        for e in ENGS:
            if e == "sp":
                continue
        waits_raw = [None] * n
        cnt = dict(self.ecnt)
        for i in glob:
            eng, fn, reads, writes, dma, est = ops[i]
            w = {}
            for j in deps[i]:
                dj = ops[j][4]
                if dj is None:
                    s_, v_ = tok[j]
                else:
                    s_, v_ = dj.sem, dj.dcnt
                if w.get(s_, 0) < v_:
                    w[s_] = v_
            waits_raw[i] = w
            if dma is None:
                cnt[eng] += 1
                tok[i] = (self.esem[eng], cnt[eng])
            else:
                if dma.sem is None:
                    if self.dsem_pool:
                        dma.sem, dma.dcnt = self.dsem_pool.pop()
                    else:
                        dma.sem = self.newsem("d_" + dma.name)
                        dma.dcnt = 0
                    self.dbufs.append(dma)
                dma.dcnt += 16
                tok[i] = (dma.sem, dma.dcnt)
        self.ecnt = cnt
        nc = self.nc
        sems = self.sems
        qs = {}
        for e in ENGS:
            wd = self.waited[e]
            q = []
            for i in order[e]:
                ws = []
                for s_, v_ in waits_raw[i].items():
                    if wd.get(s_, 0) < v_:
                        ws.append((s_, v_))
                        wd[s_] = v_
                inc = (tok[i][0], 16 if ops[i][4] is not None else 1)
                q.append((ws, ops[i][1], inc))
            qs[e] = q
        toks = {}
        for e, s_ in self.esem.items():
            if self.ecnt[e] > 0:
                toks[s_] = self.ecnt[e]
        for b in self.dbufs:
            toks[b.sem] = max(toks.get(b.sem, 0), b.dcnt)
        for e in ENGS:
            wd = self.waited[e]
            ws = []
            for s_, v_ in toks.items():
                if wd.get(s_, 0) < v_:
                    ws.append((s_, v_))
                    wd[s_] = v_
            if ws:
                qs[e].append((ws, None, None))
        for b in self.phase_bufs:
            if b.sem is not None:
                self.dsem_pool.append((b.sem, b.dcnt))
                self.dbufs.remove(b)
                b.sem = None
        self.phase_bufs = []

        def mk(e):
            def f(eng):
                for waits, fn, inc in qs[e]:
                    for s_, v_ in waits:
                        eng.wait_ge(sems[s_], v_)
                    if fn is None:
                        continue
                    ins = fn(eng)
                    ins.then_inc(sems[inc[0]], inc[1])
            return f

        with nc.Block() as block:
            block.sync(mk("sp"))
            block.scalar(mk("act"))
            block.vector(mk("dve"))
            block.gpsimd(mk("pool"))
            block.tensor(mk("pe"))
        self.last_makespan = max(efree.values()) if n else 0.0
        if getattr(self, "verbose", False):
            busy = {e: 0.0 for e in ENGS}
            for i in range(n):
                if ops[i][4] is None:
                    busy[ops[i][0]] += ops[i][5]
            print(f"[phase] n_ops={n} model_makespan={self.last_makespan:.0f}us busy=" +
                  " ".join(f"{e}:{busy[e]:.0f}" for e in ENGS), flush=True)


def _fsz(ap):
    n = 1
    for d in ap.shape[1:]:
        n *= int(d)
    return n


def _est(eng, ap, psum=False):
    n = _fsz(ap)
    if eng == "dve":
        return (60 + n) / 960.0 + (0.06 if psum else 0.0)
    if eng == "act":
        return (220 + n) / 1400.0
    if eng == "pool":
        return (120 + n) / 900.0
    return 0.5


class H:
    def __init__(self, P):
        self.P = P

    def dma(self, eng, out, in_, R, W, buf, slow=False):
        nbytes = _fsz(out) * 128 * 4
        est = 2.0 + nbytes / 150000.0
        if slow:
            self.P.op(eng, lambda e: e.dma_start(out=out, in_=in_, allow_slow_non_contiguous=True), reads=R, writes=W, dma=buf, est=est)
        else:
            self.P.op(eng, lambda e: e.dma_start(out=out, in_=in_), reads=R, writes=W, dma=buf, est=est)

    def tt(self, eng, out, in0, in1, op, R, W):
        self.P.op(eng, lambda e: e.tensor_tensor(out=out, in0=in0, in1=in1, op=op), reads=R, writes=W, est=_est(eng, out))

    def ts(self, eng, out, in0, s1, s2, op0, op1, R, W):
        if s2 is None:
            self.P.op(eng, lambda e: e.tensor_scalar(out=out, in0=in0, scalar1=s1, scalar2=None, op0=op0), reads=R, writes=W, est=_est(eng, out))
        else:
            self.P.op(eng, lambda e: e.tensor_scalar(out=out, in0=in0, scalar1=s1, scalar2=s2, op0=op0, op1=op1), reads=R, writes=W, est=_est(eng, out))

    def stt(self, eng, out, in0, sc, in1, op0, op1, R, W):
        eng = "dve"
        self.P.op(eng, lambda e: e.scalar_tensor_tensor(out=out, in0=in0, scalar=sc, in1=in1, op0=op0, op1=op1), reads=R, writes=W, est=_est(eng, out))

    def act(self, out, in_, func, R, W, bias=None, scale=None, accum=None):
        kw = {}
        if bias is not None:
            kw["bias"] = bias
        if scale is not None:
            kw["scale"] = scale
        if accum is not None:
            kw["accum_out"] = accum
        self.P.op("act", lambda e: e.activation(out=out, in_=in_, func=func, **kw), reads=R, writes=W, est=_est("act", out))

    def cp(self, eng, out, in_, R, W):
        if eng == "act":
            self.P.op("act", lambda e: e.copy(out=out, in_=in_), reads=R, writes=W, est=_est("act", out))
        else:
            self.P.op(eng, lambda e: e.tensor_copy(out=out, in_=in_), reads=R, writes=W, est=_est(eng, out))

    def memset(self, eng, ap, val, W):
        self.P.op(eng, lambda e: e.memset(ap, val), writes=W, est=_est(eng, ap))

    def recip(self, out, in_, R, W):
        self.P.op("dve", lambda e: e.reciprocal(out=out, in_=in_), reads=R, writes=W, est=_est("dve", out))

    def reduce(self, out, in_, op, R, W):
        self.P.op("dve", lambda e: e.tensor_reduce(out=out, in_=in_, axis=AX.X, op=op), reads=R, writes=W, est=_est("dve", in_))

    def mm(self, items, R, W):
        est = 0.0
        for (o, l, rh, st, sp) in items:
            est += (max(64, _fsz(rh)) * (4 if l.dtype == F32 else 1)) / 2400.0 + 0.01
        est += 0.06

        def f(e):
            r = None
            for (o, l, rh, st, sp) in items:
                r = e.matmul(o, lhsT=l, rhs=rh, start=st, stop=sp)
            return r
        self.P.op("pe", f, reads=R, writes=W, est=est)

    def tr(self, items, ident, R, W):
        est = 0.06
        for (o, i) in items:
            est += (128 * (4 if i.dtype == F32 else 1)) / 2400.0 + 0.03

        def f(e):
            r = None
            for (o, i) in items:
                r = e.transpose(out=o, in_=i, identity=ident)
            return r
        self.P.op("pe", f, reads=R, writes=W, est=est)

    def select(self, out, in_, cmp, fill, base, cm, pattern, R, W):
        self.P.op("pool", lambda e: e.affine_select(out=out, in_=in_, pattern=pattern, compare_op=cmp, fill=fill,
                                                    base=base, channel_multiplier=cm), reads=R, writes=W, est=_est("pool", out))


class Rot:
    def __init__(self, P, es, name, shape, dtype, n=2):
        self.bufs = [P.sb(es, f"{name}r{i}", shape, dtype) for i in range(n)]

    def at(self, i):
        return self.bufs[i % len(self.bufs)]


def b3(ap, n, m):
    return ap.unsqueeze(1).to_broadcast([128, n, m])


def s3(ap, n, m):
    return ap.unsqueeze(2).to_broadcast([128, n, m])


def s4(ap):
    return ap.rearrange("p (b h) -> p b h", b=2).unsqueeze(3).to_broadcast([128, 2, 3, 128])


def v4(ap):
    return ap.rearrange("p (b h) l -> p b h l", b=2)


def w4(ps):
    return ps[:, :, 0:384].rearrange("p b (h l) -> p b h l", h=3)


def softplus12(h, P, es, tagp):
    xa = P.sb(es, tagp + "xa", [128, 12], F32)
    ax = P.sb(es, tagp + "ax", [128, 12], F32)
    ex = P.sb(es, tagp + "ex", [128, 12], F32)
    ln = P.sb(es, tagp + "ln", [128, 12], F32)
    one = P.sb(es, tagp + "one", [128, 1], F32)
    h.memset("pool", one[:], 1.0, [one])

    def f(xin, xin_buf, bias, out):
        h.tt("dve", xa[:], xin, bias[:], ALU.add, [xin_buf, bias], [xa])
        h.act(ax[:], xa[:], AF.Abs, [xa], [ax])
        h.act(ex[:], ax[:], AF.Exp, [ax], [ex], scale=-1.0)
        h.act(ln[:], ex[:], AF.Ln, [ex, one], [ln], bias=one[:, 0:1])
        h.ts("dve", xa[:], xa[:], 0.0, None, ALU.max, None, [xa], [xa])
        h.tt("dve", out[:], xa[:], ln[:], ALU.add, [xa, ln], [out])
    return f


def make_masks(h, P, es):
    m = {}
    ones = P.sb(es, "m_ones", [128, 128], F32)
    h.memset("pool", ones[:], 1.0, [ones])
    m["ones"] = ones
    for name, cmp, cm, st in (("U", ALU.is_ge, -1, 1), ("L", ALU.is_ge, 1, -1), ("Ls", ALU.is_gt, 1, -1)):
        t = P.sb(es, "m_" + name, [128, 128], F32)
        h.select(t[:], ones[:], cmp, 0.0, 0, cm, [[st, 128]], [ones], [t])
        m[name] = t
    sel = P.sb(es, "m_sel", [128, 128], F32)
    zer = P.sb(es, "m_zero", [128, 128], F32)
    h.memset("pool", zer[:], 0.0, [zer])
    h.select(sel[:], zer[:], ALU.not_equal, 1.0, -127, 1, [[0, 128]], [zer], [sel])
    m["sel"] = sel
    bd = P.sb(es, "m_bd", [128, 128], F32)
    h.memset("pool", bd[:], 0.0, [bd])
    h.memset("pool", bd[0:64, 0:64], 1.0, [bd])
    h.memset("pool", bd[64:128, 64:128], 1.0, [bd])
    m["bd"] = bd
    return m


def mixer_layer(k, l, cur, dstt):
    P, T, flags, h = k.P, k.T, k.flags, k.h
    inp = k.inp
    src, rsrc = cur
    dst, rdst = dstt
    NMT = T // 512
    MT = 512
    identf, identb = k.identf, k.identb
    u_d, qn_d, kn_d, v_d, xs_d, B_d, C_d, pt_d, y_d = k.u_d, k.qn_d, k.kn_d, k.v_d, k.xs_d, k.B_d, k.C_d, k.pt_d, k.y_d
    r_pre, r_y = k.r_pre, k.r_y

    with contextlib.ExitStack() as es:
        A, B = k.norm_consts(es, l, inp["norm_mix"].t[l:l + 1, :], 1, 0, "m")
        winf = P.sb(es, "winf", [128, 8, 2304], BF16)
        wint = P.sb(es, "wint", [128, 8, 786], BF16)
        for (c0, c1) in ((0, 1152), (1152, 2304)):
            h.dma("pool", winf[:, :, c0:c1], inp["w_in_f"].t[l, :, c0:c1].rearrange("(k p) n -> p k n", p=128), [], [winf], winf)
        h.dma("pool", wint[:], inp["w_in_t"].t[l].rearrange("(k p) n -> p k n", p=128), [], [wint], wint)
        cwt = P.sb(es, "cwt", [128, 16, 4], F32)
        cbt = P.sb(es, "cbt", [128, 16], F32)
        h.dma("sp", cwt[:], inp["conv_w"].t[l], [], [cwt], cwt)
        h.dma("sp", cbt[:], inp["conv_b"].t[l], [], [cbt], cbt)
        carry = P.sb(es, "carry", [128, 16, 3], F32)
        h.memset("pool", carry[:], 0.0, [carry])
        epsb = P.sb(es, "epsb", [128, 1], F32)
        h.memset("pool", epsb[:], EPS, [epsb])
        mhalf = P.sb(es, "mhalf", [128, MT], F32)
        h.memset("pool", mhalf[:], -0.5, [mhalf])
        bones = P.sb(es, "bones", [128, 128], F32)
        h.memset("pool", bones[:], 0.0, [bones])
        h.memset("pool", bones[0:64, 0:64], 1.0, [bones])
        h.memset("pool", bones[64:128, 64:128], 1.0, [bones])
        xt = [P.sb(es, f"m1x{i}", [128, D], F32) for i in range(2)]
        tmp = P.sb(es, "m1tmp", [128, D], F32)
        hb = P.sb(es, "m1hb", [128, D], BF16)
        hT = P.sb(es, "m1hT", [128, 8, MT], BF16)
        cin = [P.sb(es, f"cin{i}", [128, MT + 3], F32) for i in range(2)]
        acc = [P.sb(es, f"acc{i}", [128, MT], F32) for i in range(2)]
        so = [P.sb(es, f"so{i}", [128, MT], F32) for i in range(2)]
        sob = [P.sb(es, f"sob{i}", [128, MT], BF16) for i in range(2)]
        sq = P.sb(es, "m1sq", [128, MT], F32)
        rinv = P.sb(es, "m1rinv", [128, MT], F32)
        ptst = [P.sb(es, f"ptst{i}", [128, 786], F32) for i in range(2)]
        ssq = P.sb(es, "m1ssq", [128, 1], F32)
        rstd = P.sb(es, "m1rstd", [128, 1], F32)
        ptr = P.ps(es, "m1ptr", [128, 8, 128], BF16)
        pp = [P.ps(es, f"m1pp{i}", [128, MT], F32) for i in range(2)]
        pt = P.ps(es, "m1pt", [128, 2, 512], F32)
        pq = P.ps(es, "m1pq", [128, MT], F32)
        for mt in range(NMT):
            cols = slice(mt * MT, (mt + 1) * MT)
            for ti in range(4):
                t = mt * 4 + ti
                xb = xt[t % 2]
                h.dma("sp", xb[:], src.t[t * 128:(t + 1) * 128, :], [rsrc[t]], [xb], xb)
                k.rms_mod(xb, A, B, hb, ssq, rstd, tmp, tmp)
                h.tr([(ptr[:, kk, :], hb[:, kk * 128:(kk + 1) * 128]) for kk in range(8)], identb[:], [hb, identb], [ptr])
                h.cp("act", hT[:, :, ti * 128:(ti + 1) * 128], ptr[:], [ptr], [hT])
            for c in range(18):
                pb = pp[c % 2]
                h.mm([(pb[:], winf[:, kk, c * 128:(c + 1) * 128], hT[:, kk, :], kk == 0, kk == 7) for kk in range(8)], [winf, hT], [pb])
                if c < 2:
                    sb_ = so[c % 2]
                    h.cp("act", sb_[:], pb[:], [pb], [sb_])
                    h.dma("sp", u_d.t[c * 128:(c + 1) * 128, cols], sb_[:], [sb_], [r_pre[mt]], sb_)
                    continue
                ci = c - 2
                cb_ = cin[ci % 2]
                h.cp("pool", cb_[:, 0:3], carry[:, ci, :], [carry], [cb_])
                h.cp("act", cb_[:, 3:MT + 3], pb[:], [pb], [cb_])
                h.cp("pool", carry[:, ci, :], cb_[:, MT:MT + 3], [cb_], [carry])
                ab = acc[ci % 2]
                h.ts("dve", ab[:], cb_[:, 0:MT], cwt[:, ci, 0:1], None, ALU.mult, None, [cb_, cwt], [ab])
                for j in range(1, 4):
                    h.stt("dve", ab[:], cb_[:, j:j + MT], cwt[:, ci, j:j + 1], ab[:], ALU.mult, ALU.add, [cb_, cwt, ab], [ab])
                sb_ = so[ci % 2]
                h.act(sb_[:], ab[:], AF.Silu, [ab, cbt], [sb_], bias=cbt[:, ci:ci + 1])
                if ci < 6:
                    h.tt("pool", sq[:], sb_[:], sb_[:], ALU.mult, [sb_], [sq])
                    h.mm([(pq[:], bones[:], sq[:], True, True)], [bones, sq], [pq])
                    h.act(rinv[:], pq[:], AF.Ln, [pq, epsb], [rinv], bias=epsb[:, 0:1])
                    h.act(rinv[:], rinv[:], AF.Exp, [rinv], [rinv], scale=-0.5)
                    ob = sob[ci % 2]
                    if ci < 3:
                        h.stt("dve", ob[:], sb_[:], 0.125, rinv[:], ALU.mult, ALU.mult, [sb_, rinv], [ob])
                    else:
                        h.tt("dve", ob[:], sb_[:], rinv[:], ALU.mult, [sb_, rinv], [ob])
                    dd = qn_d if ci < 3 else kn_d
                    j = ci % 3
                    h.dma("sp", dd.t[j * 128:(j + 1) * 128, cols], ob[:], [ob], [r_pre[mt]], ob)
                elif ci < 12:
                    dd = v_d if ci < 9 else xs_d
                    j = (ci - 6) % 3
                    h.dma("sp", dd.t[j * 128:(j + 1) * 128, cols], sb_[:], [sb_], [r_pre[mt]], sb_)
                else:
                    ob = sob[ci % 2]
                    h.cp("pool", ob[:], sb_[:], [sb_], [ob])
                    dd = B_d if ci < 14 else C_d
                    j = (ci - 12) % 2
                    h.dma("sp", dd.t[j * 128:(j + 1) * 128, cols], ob[:], [ob], [r_pre[mt]], ob)
            for ti in range(4):
                t = mt * 4 + ti
                tc = slice(ti * 128, (ti + 1) * 128)
                h.mm([(pt[:, 0, :], hT[:, kk, tc], wint[:, kk, 0:512], kk == 0, kk == 7) for kk in range(8)]
                     + [(pt[:, 1, 0:274], hT[:, kk, tc], wint[:, kk, 512:786], kk == 0, kk == 7) for kk in range(8)],
                     [hT, wint], [pt])
                stg = ptst[t % 2]
                h.cp("act", stg[:, 0:512], pt[:, 0, :], [pt], [stg])
                h.cp("dve", stg[:, 512:786], pt[:, 1, 0:274], [pt], [stg])
                h.dma("sp", pt_d.t[t * 128:(t + 1) * 128, :], stg[:], [stg], [r_pre[mt]], stg)
        P.end_phase()

    if flags.get("s5", True):
        s5_phase(k, l)
    if flags.get("gdn", True):
        gdn_phase(k, l)
    if flags.get("ssd", True):
        ssd_phase(k, l)

    with contextlib.ExitStack() as es:
        wout = P.sb(es, "wout", [128, 8, D], BF16)
        h.dma("pool", wout[:], inp["w_out"].t[l].rearrange("(k p) n -> p k n", p=128), [], [wout], wout)
        G = P.sb(es, "Gm", [128, D], F32)
        k.load_row(G, k.modv.t[l:l + 1, 2 * D:3 * D], [k.r_modv])
        yT = [P.sb(es, f"m5y{i}", [128, 8, MT], BF16) for i in range(2)]
        xt = [P.sb(es, f"m5x{i}", [128, D], F32) for i in range(2)]
        tm = [P.sb(es, f"m5t{i}", [128, D], F32) for i in range(2)]
        po = [P.ps(es, f"m5p{i}", [128, 512], F32) for i in range(4)]
        for mt in range(NMT):
            cols = slice(mt * MT, (mt + 1) * MT)
            yb = yT[mt % 2]
            h.dma("sp", yb[:], y_d.t[:, cols].rearrange("(k p) t -> p k t", p=128), [r_y[0][mt], r_y[1][mt], r_y[2][mt]], [yb], yb)
            if not flags.get("s5", True):
                h.memset("pool", yb[:, 0:2, :], 0.0, [yb])
            if not flags.get("gdn", True):
                h.memset("pool", yb[:, 2:5, :], 0.0, [yb])
            if not flags.get("ssd", True):
                h.memset("pool", yb[:, 5:8, :], 0.0, [yb])
            for ti in range(4):
                t = mt * 4 + ti
                tc = slice(ti * 128, (ti + 1) * 128)
                xb = xt[t % 2]
                tb = tm[t % 2]
                h.dma("sp", xb[:], src.t[t * 128:(t + 1) * 128, :], [rsrc[t]], [xb], xb)
                for n2 in range(2):
                    pb = po[(t % 2) * 2 + n2]
                    nc_ = slice(n2 * 512, (n2 + 1) * 512)
                    h.mm([(pb[:], yb[:, kk, tc], wout[:, kk, nc_], kk == 0, kk == 7) for kk in range(8)], [yb, wout], [pb])
                    h.tt("dve", tb[:, nc_], pb[:], G[:, nc_], ALU.mult, [pb, G], [tb])
                h.tt("pool", tb[:], tb[:], xb[:], ALU.add, [tb, xb], [tb])
                h.dma("sp", dst.t[t * 128:(t + 1) * 128, :], tb[:], [tb], [rdst[t]], tb)
        P.end_phase()


def gate_consts(k, es, l, tag):
    P, h, inp = k.P, k.h, k.inp
    b12 = P.sb(es, tag + "b12", [128, 12], F32)
    na12 = P.sb(es, tag + "na12", [128, 12], F32)
    k.load_row(b12, inp["bias12"].t[l:l + 1, :])
    k.load_row(na12, inp["alog12"].t[l:l + 1, :])
    h.act(na12[:], na12[:], AF.Exp, [na12], [na12])
    h.ts("dve", na12[:], na12[:], -1.0, None, ALU.mult, None, [na12], [na12])
    return b12, na12


def gate_tile(k, sp_fn, pj_ap, pj_buf, b12, na12, masks, sp12, gda, cs12, cl12, pA):
    h = k.h
    sp_fn(pj_ap, pj_buf, b12, sp12)
    h.tt("dve", gda[:], sp12[:], na12[:], ALU.mult, [sp12, na12], [gda])
    h.mm([(pA[:, 0:12], masks["U"][:], gda[:], True, True)], [masks["U"], gda], [pA])
    h.cp("dve", cs12[:], pA[:, 0:12], [pA], [cs12])
    h.mm([(pA[:, 16:28], masks["sel"][:], cs12[:], True, True)], [masks["sel"], cs12], [pA])
    h.cp("dve", cl12[:], pA[:, 16:28], [pA], [cl12])


def ssd_phase(k, l):
    P, T, h, inp = k.P, k.T, k.h, k.inp
    NMT = T // 512
    MT = 512
    identf, identb = k.identf, k.identb
    with contextlib.ExitStack() as es:
        masks = make_masks(h, P, es)
        b12, na12 = gate_consts(k, es, l, "sd")
        sp_fns = [softplus12(h, P, es, f"sd{i}") for i in range(2)]
        epsb = P.sb(es, "sd_eps", [128, 1], F32)
        h.memset("pool", epsb[:], EPS, [epsb])
        dsk = P.sb(es, "sd_dsk", [128, 6], F32)
        k.load_row(dsk, inp["ssd_d"].t[l:l + 1, :])
        nws = P.sb(es, "sd_nws", [128, 384], F32)
        k.load_row(nws, inp["ssd_norm"].t[l:l + 1, :])
        stT = P.sb(es, "sd_stT", [128, 384], F32)
        stTb = P.sb(es, "sd_stTb", [128, 384], BF16)
        h.memset("pool", stT[:], 0.0, [stT])
        h.memset("pool", stTb[:], 0.0, [stTb])
        xsT = [P.sb(es, f"sd_xsT{i}", [128, 3, MT], F32) for i in range(2)]
        BTt = [P.sb(es, f"sd_BT{i}", [128, 2, MT], BF16) for i in range(2)]
        CTt = [P.sb(es, f"sd_CT{i}", [128, 2, MT], BF16) for i in range(2)]
        pj = [P.sb(es, f"sd_pj{i}", [128, 4, 786], F32) for i in range(2)]
        yo = [P.sb(es, f"sd_yo{i}", [128, 3, MT], BF16) for i in range(2)]
        R_sp12 = Rot(P, es, "sd_sp12", [128, 12], F32)
        R_gda = Rot(P, es, "sd_gda", [128, 12], F32)
        R_cs12 = Rot(P, es, "sd_cs12", [128, 12], F32)
        R_cl12 = Rot(P, es, "sd_cl12", [128, 12], F32)
        R_t6 = Rot(P, es, "sd_t6", [128, 6], F32)
        R_din = Rot(P, es, "sd_din", [128, 6], F32)
        R_eacs = Rot(P, es, "sd_eacs", [128, 6], F32)
        R_cd = Rot(P, es, "sd_cd", [128, 6], F32)
        R_dg = Rot(P, es, "sd_dg", [128, 6, 128], F32)
        R_arg = Rot(P, es, "sd_arg", [128, 6, 128], F32)
        R_seg = Rot(P, es, "sd_seg", [128, 6, 128], F32)
        R_WTb = Rot(P, es, "sd_WTb", [128, 6, 128], BF16)
        R_xs_tm = Rot(P, es, "sd_xstm", [128, 384], F32)
        R_xdtf = Rot(P, es, "sd_xdtf", [128, 384], F32)
        R_xdtb = Rot(P, es, "sd_xdtb", [128, 384], BF16)
        R_xddb = Rot(P, es, "sd_xddb", [128, 384], BF16)
        R_Btm = Rot(P, es, "sd_Btm", [128, 256], BF16)
        R_t1 = Rot(P, es, "sd_t1", [128, 384], F32)
        R_t2 = Rot(P, es, "sd_t2", [128, 384], F32)
        R_y = Rot(P, es, "sd_y", [128, 384], F32)
        R_zs = Rot(P, es, "sd_zs", [128, 384], F32)
        R_junk = Rot(P, es, "sd_junk", [128, 192], F32)
        R_yb = Rot(P, es, "sd_yb", [128, 384], BF16)
        R_ss2 = Rot(P, es, "sd_ss2", [128, 2], F32)
        R_rs2 = Rot(P, es, "sd_rs2", [128, 2], F32)
        W0 = P.ps(es, "sd_W0", [128, 2, 512], F32)
        S0 = P.ps(es, "sd_S0", [128, 512], F32)
        S1 = P.ps(es, "sd_S1", [128, 512], F32)
        S2 = P.ps(es, "sd_S2", [128, 512], F32)
        PB = P.ps(es, "sd_PB", [128, 512], BF16)
        pA = P.ps(es, "sd_pA", [128, 32], F32)
        for mt in range(NMT):
            cols = slice(mt * MT, (mt + 1) * MT)
            i2 = mt % 2
            rp = [k.r_pre[mt]]
            h.dma("sp", xsT[i2][:], k.xs_d.t[:, cols].rearrange("(j p) t -> p j t", p=128), rp, [xsT[i2]], xsT[i2])
            h.dma("sp", BTt[i2][:], k.B_d.t[:, cols].rearrange("(j p) t -> p j t", p=128), rp, [BTt[i2]], BTt[i2])
            h.dma("sp", CTt[i2][:], k.C_d.t[:, cols].rearrange("(j p) t -> p j t", p=128), rp, [CTt[i2]], CTt[i2])
            h.dma("sp", pj[i2][:], k.pt_d.t[mt * MT:(mt + 1) * MT, :].rearrange("(a p) n -> p a n", p=128), rp, [pj[i2]], pj[i2])
            xs_, B_, C_, pj_, yo_ = xsT[i2], BTt[i2], CTt[i2], pj[i2], yo[i2]
            for ti in range(4):
                tc = slice(ti * 128, (ti + 1) * 128)
                tix = mt * 4 + ti
                sp12 = R_sp12.at(tix); gda = R_gda.at(tix); cs12 = R_cs12.at(tix); cl12 = R_cl12.at(tix); t6 = R_t6.at(tix); din = R_din.at(tix); eacs = R_eacs.at(tix); cd = R_cd.at(tix); dg = R_dg.at(tix); arg = R_arg.at(tix); seg = R_seg.at(tix); WTb = R_WTb.at(tix); xs_tm = R_xs_tm.at(tix); xdtf = R_xdtf.at(tix); xdtb = R_xdtb.at(tix); xddb = R_xddb.at(tix); Btm = R_Btm.at(tix); t1 = R_t1.at(tix); t2 = R_t2.at(tix); y = R_y.at(tix); zs = R_zs.at(tix); junk = R_junk.at(tix); yb = R_yb.at(tix); ss2 = R_ss2.at(tix); rs2 = R_rs2.at(tix)
                sp_fn = sp_fns[tix % 2]
                gate_tile(k, sp_fn, pj_[:, ti, 768:780], pj_, b12, na12, masks, sp12, gda, cs12, cl12, pA)
                acs = cs12[:, 6:12]
                h.tt("pool", dg[:], b3(identf[:], 6, 128), s3(acs, 6, 128), ALU.mult, [identf, cs12], [dg])
                h.mm([(W0[:, 0, 0:384], masks["ones"][:], dg[:, 0:3, :].rearrange("p a l -> p (a l)"), True, True),
                      (W0[:, 1, 0:384], masks["ones"][:], dg[:, 3:6, :].rearrange("p a l -> p (a l)"), True, True)],
                     [masks["ones"], dg], [W0])
                h.tt("dve", v4(arg[:]), w4(W0), s4(acs), ALU.subtract, [W0, cs12], [arg])
                h.ts("pool", arg[:], arg[:], 0.0, None, ALU.min, None, [arg], [arg])
                h.act(seg[:], arg[:], AF.Exp, [arg], [seg])
                h.tt("pool", seg[:], seg[:], b3(masks["U"][:], 6, 128), ALU.mult, [seg, masks["U"]], [seg])
                h.mm([(S0[:, g * 128:(g + 1) * 128], B_[:, g, tc], C_[:, g, tc], True, True) for g in range(2)], [B_, C_], [S0])
                h.tt("dve", v4(WTb[:]), v4(seg[:]),
                     S0[:, 0:256].rearrange("p (g l) -> p g l", g=2).unsqueeze(2).to_broadcast([128, 2, 3, 128]),
                     ALU.mult, [seg, S0], [WTb])
                h.tr([(S1[:, j * 128:(j + 1) * 128], xs_[:, j, tc]) for j in range(3)], identf[:], [xs_, identf], [S1])
                h.cp("act", xs_tm[:], S1[:, 0:384], [S1], [xs_tm])
                h.tr([(PB[:, g * 128:(g + 1) * 128], B_[:, g, tc]) for g in range(2)], identb[:], [B_, identb], [PB])
                h.cp("act", Btm[:], PB[:, 0:256], [PB], [Btm])
                x3 = lambda ap: ap.rearrange("p (h d) -> p h d", h=6)
                h.tt("dve", x3(xdtf[:]), x3(xs_tm[:]), s3(sp12[:, 6:12], 6, 64), ALU.mult, [xs_tm, sp12], [xdtf])
                h.cp("pool", xdtb[:], xdtf[:], [xdtf], [xdtb])
                h.tt("dve", t6[:], cl12[:, 6:12], acs, ALU.subtract, [cl12, cs12], [t6])
                h.act(din[:], t6[:], AF.Exp, [t6], [din])
                h.tt("pool", x3(xddb[:]), x3(xdtf[:]), s3(din[:], 6, 64), ALU.mult, [xdtf, din], [xddb])
                h.mm([(S2[:, hd * 64:(hd + 1) * 64], WTb[:, hd, :], xdtb[:, hd * 64:(hd + 1) * 64], True, True) for hd in range(6)],
                     [WTb, xdtb], [S2])
                h.mm([(S0[:, g * 192:(g + 1) * 192], C_[:, g, tc], stTb[:, g * 192:(g + 1) * 192], True, True) for g in range(2)],
                     [C_, stTb], [S0])
                h.act(eacs[:], acs, AF.Exp, [cs12], [eacs])
                h.tt("dve", x3(t2[:]), x3(S0[:, 0:384]), s3(eacs[:], 6, 64), ALU.mult, [S0, eacs], [t2])
                h.tt("pool", x3(t1[:]), x3(xs_tm[:]), s3(dsk[:], 6, 64), ALU.mult, [xs_tm, dsk], [t1])
                h.tt("pool", t2[:], t2[:], t1[:], ALU.add, [t2, t1], [t2])
                h.tt("dve", y[:], S2[:, 0:384], t2[:], ALU.add, [S2, t2], [y])
                h.act(zs[:], pj_[:, ti, 384:768], AF.Silu, [pj_], [zs])
                h.tt("pool", y[:], y[:], zs[:], ALU.mult, [y, zs], [y])
                for g in range(2):
                    h.act(junk[:], y[:, g * 192:(g + 1) * 192], AF.Square, [y], [junk, ss2], accum=ss2[:, g:g + 1])
                h.act(rs2[:], ss2[:], AF.Ln, [ss2, epsb], [rs2], bias=epsb[:, 0:1], scale=1.0 / 192)
                h.act(rs2[:], rs2[:], AF.Exp, [rs2], [rs2], scale=-0.5)
                y3 = lambda ap: ap.rearrange("p (g c) -> p g c", g=2)
                h.tt("dve", y3(y[:]), y3(y[:]), s3(rs2[:], 2, 192), ALU.mult, [y, rs2], [y])
                h.tt("pool", yb[:], y[:], nws[:], ALU.mult, [y, nws], [yb])
                h.tr([(PB[:, j * 128:(j + 1) * 128], yb[:, j * 128:(j + 1) * 128]) for j in range(3)], identb[:], [yb, identb], [PB])
                h.cp("act", yo_[:, :, tc], PB[:, 0:384].rearrange("p (j t) -> p j t", j=3), [PB], [yo_])
                h.mm([(S1[:, g * 192:(g + 1) * 192], Btm[:, g * 128:(g + 1) * 128], xddb[:, g * 192:(g + 1) * 192], True, True) for g in range(2)],
                     [Btm, xddb], [S1])
                h.act(cd[:], cl12[:, 6:12], AF.Exp, [cl12], [cd])
                h.tt("pool", x3(stT[:]), x3(stT[:]), s3(cd[:], 6, 64), ALU.mult, [stT, cd], [stT])
                h.tt("dve", stT[:], stT[:], S1[:, 0:384], ALU.add, [stT, S1], [stT])
                h.cp("act", stTb[:], stT[:], [stT], [stTb])
            h.dma("sp", k.y_d.t[640:1024, cols].rearrange("(j p) t -> p j t", p=128), yo_[:], [yo_], [k.r_y[2][mt]], yo_)
        P.end_phase()


C1_2PI = 6.28125
C2_2PI = 2.0 * math.pi - 6.28125


def sincos(h, eng, x, out, b, ki, c, negpi, R, W, bufs, is_cos):
    bb, kb, cb_ = bufs
    off = 16.5 + (0.25 if is_cos else 0.0)
    add = 33.0 * math.pi + (0.5 * math.pi if is_cos else 0.0)
    h.ts(eng, b, x, 1.0 / (2.0 * math.pi), off, ALU.mult, ALU.add, R, [bb])
    h.cp(eng, ki, b, [bb], [kb])
    h.cp(eng, c, ki, [kb], [cb_])
    h.stt(eng, b, c, -C1_2PI, x, ALU.mult, ALU.add, R + [cb_], [bb])
    h.stt(eng, b, c, -C2_2PI, b, ALU.mult, ALU.add, [cb_, bb], [bb])
    h.ts(eng, b, b, add, None, ALU.add, None, [bb], [bb])
    h.ts(eng, c, b, 2.0 * math.pi, -2.0 * math.pi, ALU.is_gt, ALU.mult, [bb], [cb_])
    h.tt(eng, b, b, c, ALU.add, [bb, cb_], [bb])
    h.ts(eng, c, b, 0.0, 2.0 * math.pi, ALU.is_lt, ALU.mult, [bb], [cb_])
    h.tt(eng, b, b, c, ALU.add, [bb, cb_], [bb])
    h.act(out, b, AF.Sin, [bb, negpi], W, bias=negpi[:, 0:1])


def s5_phase(k, l):
    P, T, h, inp = k.P, k.T, k.h, k.inp
    NMT = T // 512
    SEG = 512
    with contextlib.ExitStack() as es:
        I32 = mybir.dt.int32
        are = P.sb(es, "s5are", [128, 8], F32)
        aim = P.sb(es, "s5aim", [128, 8], F32)
        stp = P.sb(es, "s5stp", [128, 8], F32)
        h.dma("sp", are[:], inp["s5_are"].t[l], [], [are], are)
        h.dma("sp", aim[:], inp["s5_aim"].t[l], [], [aim], aim)
        h.dma("sp", stp[:], inp["s5_ldt"].t[l], [], [stp], stp)
        dsk = P.sb(es, "s5dsk", [128, 2], F32)
        nw5 = P.sb(es, "s5nw", [128, 2], F32)
        h.dma("sp", dsk[:], inp["s5_dcol"].t[l], [], [dsk], dsk)
        h.dma("sp", nw5[:], inp["s5_ncol"].t[l], [], [nw5], nw5)
        bTre = P.sb(es, "s5bTre", [128, 8, 128], BF16)
        bTim = P.sb(es, "s5bTim", [128, 8, 128], BF16)
        cTre = P.sb(es, "s5cTre", [128, 8, 128], BF16)
        cTim = P.sb(es, "s5cTim", [128, 8, 128], BF16)
        for dstb, nm in ((bTre, "s5_bT_re"), (bTim, "s5_bT_im"), (cTre, "s5_cT_re"), (cTim, "s5_cT_im")):
            h.dma("pool", dstb[:], inp[nm].t[l].rearrange("s r m -> r s m"), [], [dstb], dstb)
        wglu = P.sb(es, "s5wglu", [128, 2, 256], BF16)
        h.dma("pool", wglu[:], inp["s5_w_glu"].t[l].rearrange("(k p) n -> p k n", p=128), [], [wglu], wglu)
        negpi = P.sb(es, "s5negpi", [128, 1], F32)
        h.memset("pool", negpi[:], -math.pi, [negpi])
        epsb = P.sb(es, "s5eps", [128, 1], F32)
        h.memset("pool", epsb[:], EPS, [epsb])
        onesf = P.sb(es, "s5ones", [128, 128], F32)
        h.memset("pool", onesf[:], 1.0, [onesf])
        jrow = P.sb(es, "s5jrow", [128, SEG], F32)
        P.op("pool", lambda e: e.iota(jrow[:], pattern=[[1, SEG]], base=0, channel_multiplier=0,
                                      allow_small_or_imprecise_dtypes=True), writes=[jrow])
        th = P.sb(es, "s5th", [128, 8], F32)
        rr = P.sb(es, "s5r", [128, 8], F32)
        sth = P.sb(es, "s5sth", [128, 8], F32)
        cth = P.sb(es, "s5cth", [128, 8], F32)
        thS = P.sb(es, "s5thS", [128, 8], F32)
        sS = P.sb(es, "s5sS", [128, 8], F32)
        cS = P.sb(es, "s5cS", [128, 8], F32)
        nsS = P.sb(es, "s5nsS", [128, 8], F32)
        cr = P.sb(es, "s5cr", [128, 8], F32)
        ci = P.sb(es, "s5ci", [128, 8], F32)
        ncr = P.sb(es, "s5ncr", [128, 8], F32)
        q1 = P.sb(es, "s5q1", [128, 8], F32)
        q2 = P.sb(es, "s5q2", [128, 8], F32)
        q3 = P.sb(es, "s5q3", [128, 8], F32)
        sb8 = P.sb(es, "s5sb8", [128, 8], F32)
        si8 = P.sb(es, "s5si8", [128, 8], I32)
        sc8 = P.sb(es, "s5sc8", [128, 8], F32)
        h.act(stp[:], stp[:], AF.Exp, [stp], [stp])
        h.tt("dve", th[:], aim[:], stp[:], ALU.mult, [aim, stp], [th])
        h.tt("dve", rr[:], are[:], stp[:], ALU.mult, [are, stp], [rr])
        h.act(rr[:], rr[:], AF.Exp, [rr], [rr])
        sm = (sb8, si8, sc8)
        sincos(h, "dve", th[:], sth[:], sb8[:], si8[:], sc8[:], negpi, [th], [sth], sm, False)
        sincos(h, "dve", th[:], cth[:], sb8[:], si8[:], sc8[:], negpi, [th], [cth], sm, True)
        h.ts("dve", thS[:], th[:], float(SEG), None, ALU.mult, None, [th], [thS])
        sincos(h, "dve", thS[:], sS[:], sb8[:], si8[:], sc8[:], negpi, [thS], [sS], sm, False)
        sincos(h, "dve", thS[:], cS[:], sb8[:], si8[:], sc8[:], negpi, [thS], [cS], sm, True)
        h.ts("dve", nsS[:], sS[:], -1.0, None, ALU.mult, None, [sS], [nsS])
        h.tt("dve", q1[:], rr[:], cth[:], ALU.mult, [rr, cth], [q1])
        h.ts("dve", q1[:], q1[:], -1.0, None, ALU.add, None, [q1], [q1])
        h.tt("dve", q2[:], rr[:], sth[:], ALU.mult, [rr, sth], [q2])
        h.tt("dve", q3[:], are[:], are[:], ALU.mult, [are], [q3])
        h.tt("dve", sc8[:], aim[:], aim[:], ALU.mult, [aim], [sc8])
        h.tt("dve", q3[:], q3[:], sc8[:], ALU.add, [q3, sc8], [q3])
        h.recip(q3[:], q3[:], [q3], [q3])
        h.tt("dve", cr[:], q1[:], are[:], ALU.mult, [q1, are], [cr])
        h.tt("dve", sc8[:], q2[:], aim[:], ALU.mult, [q2, aim], [sc8])
        h.tt("dve", cr[:], cr[:], sc8[:], ALU.add, [cr, sc8], [cr])
        h.tt("dve", cr[:], cr[:], q3[:], ALU.mult, [cr, q3], [cr])
        h.tt("dve", ci[:], q2[:], are[:], ALU.mult, [q2, are], [ci])
        h.tt("dve", sc8[:], q1[:], aim[:], ALU.mult, [q1, aim], [sc8])
        h.tt("dve", ci[:], ci[:], sc8[:], ALU.subtract, [ci, sc8], [ci])
        h.tt("dve", ci[:], ci[:], q3[:], ALU.mult, [ci, q3], [ci])
        h.ts("dve", ncr[:], cr[:], -1.0, None, ALU.mult, None, [cr], [ncr])
        cosT = P.sb(es, "s5cosT", [128, 8, SEG], F32)
        sinT = P.sb(es, "s5sinT", [128, 8, SEG], F32)
        tabr = P.sb(es, "s5tabr", [128, 8, SEG], F32)
        tabi = P.sb(es, "s5tabi", [128, 8, SEG], F32)
        ang = [P.sb(es, f"s5ang{i}", [128, SEG], F32) for i in range(2)]
        tb = [P.sb(es, f"s5tb{i}", [128, SEG], F32) for i in range(2)]
        tki = [P.sb(es, f"s5tki{i}", [128, SEG], I32) for i in range(2)]
        tcc = [P.sb(es, f"s5tc{i}", [128, SEG], F32) for i in range(2)]
        for sc in range(8):
            i = sc % 2
            eng = "dve" if i == 0 else "pool"
            h.ts(eng, ang[i][:], jrow[:], th[:, sc:sc + 1], None, ALU.mult, None, [jrow, th], [ang[i]])
            bufs = (tb[i], tki[i], tcc[i])
            sincos(h, eng, ang[i][:], sinT[:, sc, :], tb[i][:], tki[i][:], tcc[i][:], negpi, [ang[i]], [sinT], bufs, False)
            sincos(h, eng, ang[i][:], cosT[:, sc, :], tb[i][:], tki[i][:], tcc[i][:], negpi, [ang[i]], [cosT], bufs, True)
            h.ts(eng, tabr[:, sc, :], cosT[:, sc, :], cr[:, sc:sc + 1], None, ALU.mult, None, [cosT, cr], [tabr])
            h.stt(eng, tabr[:, sc, :], sinT[:, sc, :], ci[:, sc:sc + 1], tabr[:, sc, :], ALU.mult, ALU.add, [sinT, ci, tabr], [tabr])
            h.ts(eng, tabi[:, sc, :], cosT[:, sc, :], ci[:, sc:sc + 1], None, ALU.mult, None, [cosT, ci], [tabi])
            h.stt(eng, tabi[:, sc, :], sinT[:, sc, :], ncr[:, sc:sc + 1], tabi[:, sc, :], ALU.mult, ALU.add, [sinT, ncr, tabi], [tabi])
        ire = P.sb(es, "s5ire", [128, 8], F32)
        iim = P.sb(es, "s5iim", [128, 8], F32)
        gre_e = P.sb(es, "s5gree", [128, 8], F32)
        gim_e = P.sb(es, "s5gime", [128, 8], F32)
        h.memset("pool", ire[:], 0.0, [ire])
        h.memset("pool", iim[:], 0.0, [iim])
        uTf = [P.sb(es, f"s5uTf{i}", [128, 2, SEG], F32) for i in range(2)]
        uTb = [P.sb(es, f"s5uTb{i}", [128, 2, SEG], BF16) for i in range(2)]
        m1 = [P.sb(es, f"s5m1{i}", [128, SEG], F32) for i in range(2)]
        m2 = [P.sb(es, f"s5m2{i}", [128, SEG], F32) for i in range(2)]
        m3 = [P.sb(es, f"s5m3{i}", [128, SEG], F32) for i in range(2)]
        m4 = [P.sb(es, f"s5m4{i}", [128, SEG], F32) for i in range(2)]
        p1 = [P.sb(es, f"s5p1{i}", [128, SEG], BF16) for i in range(2)]
        p2 = [P.sb(es, f"s5p2{i}", [128, SEG], BF16) for i in range(2)]
        p3 = [P.sb(es, f"s5p3{i}", [128, SEG], BF16) for i in range(2)]
        p4 = [P.sb(es, f"s5p4{i}", [128, SEG], BF16) for i in range(2)]
        ncTre = P.sb(es, "s5ncTre", [128, 8, 128], BF16)
        ncTim = P.sb(es, "s5ncTim", [128, 8, 128], BF16)
        h.ts("pool", ncTre[:], cTre[:], -1.0, None, ALU.mult, None, [cTre], [ncTre])
        h.ts("pool", ncTim[:], cTim[:], -1.0, None, ALU.mult, None, [cTim], [ncTim])
        dre = [P.sb(es, f"s5dre{i}", [128, SEG], F32) for i in range(2)]
        dim = [P.sb(es, f"s5dim{i}", [128, SEG], F32) for i in range(2)]
        gre = [P.sb(es, f"s5gre{i}", [128, SEG], F32) for i in range(2)]
        gim = [P.sb(es, f"s5gim{i}", [128, SEG], F32) for i in range(2)]
        hre = [P.sb(es, f"s5hre{i}", [128, SEG], BF16) for i in range(2)]
        him = [P.sb(es, f"s5him{i}", [128, SEG], BF16) for i in range(2)]
        y1 = P.sb(es, "s5y1", [128, 2, SEG], F32)
        yt = P.sb(es, "s5yt", [128, 2, SEG], F32)
        yg = P.sb(es, "s5yg", [128, 2, SEG], F32)
        ygb = P.sb(es, "s5ygb", [128, 2, SEG], BF16)
        sg = P.sb(es, "s5sg", [128, 2, SEG], F32)
        y2 = P.sb(es, "s5y2", [128, 2, SEG], F32)
        rstd = P.sb(es, "s5rstd", [128, SEG], F32)
        yo = [P.sb(es, f"s5yo{i}", [128, 2, SEG], BF16) for i in range(2)]
        Pre = [P.ps(es, f"s5Pre{i}", [128, SEG], F32) for i in range(2)]
        Pim = [P.ps(es, f"s5Pim{i}", [128, SEG], F32) for i in range(2)]
        Y = [P.ps(es, f"s5Y{i}", [128, SEG], F32) for i in range(2)]
        Pg = P.ps(es, "s5Pg", [128, SEG], F32)
        Pt = P.ps(es, "s5Pt", [128, SEG], F32)
        GK = 2.0 * math.sqrt(2.0 / math.pi)
        for mt in range(NMT):
            cols = slice(mt * SEG, (mt + 1) * SEG)
            i2 = mt % 2
            uf, ub, yo_ = uTf[i2], uTb[i2], yo[i2]
            h.dma("sp", uf[:], k.u_d.t[:, cols].rearrange("(j p) t -> p j t", p=128), [k.r_pre[mt]], [uf], uf)
            h.cp("pool", ub[:], uf[:], [uf], [ub])
            for sc in range(8):
                i = sc % 2
                cc = sc // 4
                h.mm([(Pre[i][:], bTre[:, sc, :], ub[:, cc, :], True, True)], [bTre, ub], [Pre[i]])
                h.mm([(Pim[i][:], bTim[:, sc, :], ub[:, cc, :], True, True)], [bTim, ub], [Pim[i]])
                h.tt("dve", m1[i][:], Pre[i][:], tabr[:, sc, :], ALU.mult, [Pre[i], tabr], [m1[i]])
                h.tt("dve", m2[i][:], Pim[i][:], tabi[:, sc, :], ALU.mult, [Pim[i], tabi], [m2[i]])
                h.tt("pool", dre[i][:], m1[i][:], m2[i][:], ALU.subtract, [m1[i], m2[i]], [dre[i]])
                h.tt("dve", m3[i][:], Pre[i][:], tabi[:, sc, :], ALU.mult, [Pre[i], tabi], [m3[i]])
                h.tt("dve", m4[i][:], Pim[i][:], tabr[:, sc, :], ALU.mult, [Pim[i], tabr], [m4[i]])
                h.tt("pool", dim[i][:], m3[i][:], m4[i][:], ALU.add, [m3[i], m4[i]], [dim[i]])
                for (go, di, ini) in ((gre[i], dre[i], ire), (gim[i], dim[i], iim)):
                    P.op("dve", (lambda go, di, ini, sc: (lambda e: e.tensor_tensor_scan(
                        out=go[:], data0=rr[:, sc:sc + 1].to_broadcast([128, SEG]), data1=di[:],
                        initial=ini[:, sc:sc + 1], op0=ALU.mult, op1=ALU.add)))(go, di, ini, sc),
                        reads=[rr, di, ini], writes=[go])
                h.cp("act", gre_e[:, sc:sc + 1], gre[i][:, SEG - 1:SEG], [gre[i]], [gre_e])
                h.cp("act", gim_e[:, sc:sc + 1], gim[i][:, SEG - 1:SEG], [gim[i]], [gim_e])
                h.tt("pool", p1[i][:], gre[i][:], cosT[:, sc, :], ALU.mult, [gre[i], cosT], [p1[i]])
                h.tt("pool", p2[i][:], gim[i][:], sinT[:, sc, :], ALU.mult, [gim[i], sinT], [p2[i]])
                h.tt("dve", p3[i][:], gre[i][:], sinT[:, sc, :], ALU.mult, [gre[i], sinT], [p3[i]])
                h.tt("dve", p4[i][:], gim[i][:], cosT[:, sc, :], ALU.mult, [gim[i], cosT], [p4[i]])
                h.mm([(Y[cc][:], cTre[:, sc, :], p1[i][:], sc % 4 == 0, False),
                      (Y[cc][:], ncTre[:, sc, :], p2[i][:], False, False),
                      (Y[cc][:], ncTim[:, sc, :], p3[i][:], False, False),
                      (Y[cc][:], ncTim[:, sc, :], p4[i][:], False, sc % 4 == 3)],
                     [cTre, ncTre, ncTim, p1[i], p2[i], p3[i], p4[i]], [Y[cc]])
                if sc % 4 == 3:
                    h.stt("dve", y1[:, cc, :], uf[:, cc, :], dsk[:, cc:cc + 1], Y[cc][:], ALU.mult, ALU.add, [uf, dsk, Y[cc]], [y1])
                    h.tt("pool", yt[:, cc, :], y1[:, cc, :], y1[:, cc, :], ALU.mult, [y1], [yt])
                    h.ts("pool", yt[:, cc, :], yt[:, cc, :], 0.044715, 1.0, ALU.mult, ALU.add, [yt], [yt])
                    h.tt("pool", yt[:, cc, :], yt[:, cc, :], y1[:, cc, :], ALU.mult, [yt, y1], [yt])
                    h.act(yt[:, cc, :], yt[:, cc, :], AF.Sigmoid, [yt], [yt], scale=GK)
                    h.tt("pool", yg[:, cc, :], y1[:, cc, :], yt[:, cc, :], ALU.mult, [y1, yt], [yg])
                    h.cp("pool", ygb[:, cc, :], yg[:, cc, :], [yg], [ygb])
            h.tt("dve", q1[:], gre_e[:], cS[:], ALU.mult, [gre_e, cS], [q1])
            h.tt("dve", q2[:], gim_e[:], nsS[:], ALU.mult, [gim_e, nsS], [q2])
            h.tt("dve", ire[:], q1[:], q2[:], ALU.add, [q1, q2], [ire])
            h.tt("dve", q1[:], gre_e[:], sS[:], ALU.mult, [gre_e, sS], [q1])
            h.tt("dve", q2[:], gim_e[:], cS[:], ALU.mult, [gim_e, cS], [q2])
            h.tt("dve", iim[:], q1[:], q2[:], ALU.add, [q1, q2], [iim])
            for oc in range(2):
                h.mm([(Pg[:], wglu[:, kc, oc * 128:(oc + 1) * 128], ygb[:, kc, :], kc == 0, kc == 1) for kc in range(2)], [wglu, ygb], [Pg])
                h.act(sg[:, oc, :], Pg[:], AF.Sigmoid, [Pg], [sg])
                h.tt("pool", y2[:, oc, :], yg[:, oc, :], sg[:, oc, :], ALU.mult, [yg, sg], [y2])
                h.tt("pool", sg[:, oc, :], y2[:, oc, :], y2[:, oc, :], ALU.mult, [y2], [sg])
            h.mm([(Pt[:], onesf[:], sg[:, oc, :], oc == 0, oc == 1) for oc in range(2)], [onesf, sg], [Pt])
            h.act(rstd[:], Pt[:], AF.Sqrt, [Pt, epsb], [rstd], bias=epsb[:, 0:1], scale=1.0 / 256)
            h.recip(rstd[:], rstd[:], [rstd], [rstd])
            for oc in range(2):
                h.stt("dve", yo_[:, oc, :], y2[:, oc, :], nw5[:, oc:oc + 1], rstd[:], ALU.mult, ALU.mult, [y2, nw5, rstd], [yo_])
            h.dma("sp", k.y_d.t[0:256, cols].rearrange("(j p) t -> p j t", p=128), yo_[:], [yo_], [k.r_y[0][mt]], yo_)
        P.end_phase()


def gdn_phase(k, l):
    P, T, h, inp = k.P, k.T, k.h, k.inp
    NMT = T // 512
    MT = 512
    identf, identb = k.identf, k.identb
    with contextlib.ExitStack() as es:
        masks = make_masks(h, P, es)
        b12, na12 = gate_consts(k, es, l, "gd")
        gnw = P.sb(es, "gd_gnw", [128, 64], F32)
        k.load_row(gnw, inp["gdn_norm"].t[l:l + 1, :])
        Sf = P.sb(es, "gd_Sf", [128, 3, 128], F32)
        Sb = P.sb(es, "gd_Sb", [128, 3, 128], BF16)
        h.memset("pool", Sf[:], 0.0, [Sf])
        h.memset("pool", Sb[:], 0.0, [Sb])
        qnT = [P.sb(es, f"gd_qn{i}", [128, 3, MT], BF16) for i in range(2)]
        knT = [P.sb(es, f"gd_kn{i}", [128, 3, MT], BF16) for i in range(2)]
        vT = [P.sb(es, f"gd_vT{i}", [128, 3, MT], F32) for i in range(2)]
        pj = [P.sb(es, f"gd_pj{i}", [128, 4, 786], F32) for i in range(2)]
        yo = [P.sb(es, f"gd_yo{i}", [128, 3, MT], BF16) for i in range(2)]
        R_sp12 = Rot(P, es, "gd_sp12", [128, 12], F32)
        R_gda = Rot(P, es, "gd_gda", [128, 12], F32)
        R_cs12 = Rot(P, es, "gd_cs12", [128, 12], F32)
        R_cl12 = Rot(P, es, "gd_cl12", [128, 12], F32)
        R_beta = Rot(P, es, "gd_beta", [128, 6], F32)
        R_nbeta = Rot(P, es, "gd_nbeta", [128, 6], F32)
        R_egc = Rot(P, es, "gd_egc", [128, 6], F32)
        R_t6 = Rot(P, es, "gd_t6", [128, 6], F32)
        R_dkk = Rot(P, es, "gd_dkk", [128, 6], F32)
        R_gtot = Rot(P, es, "gd_gtot", [128, 6], F32)
        R_gtc = Rot(P, es, "gd_gtc", [128, 3], F32)
        R_dg = Rot(P, es, "gd_dg", [128, 6, 128], F32)
        R_arg = Rot(P, es, "gd_arg", [128, 6, 128], F32)
        R_E = Rot(P, es, "gd_E", [128, 6, 128], F32)
        R_EU = Rot(P, es, "gd_EU", [128, 6, 128], F32)
        R_ELn = Rot(P, es, "gd_ELn", [128, 6, 128], F32)
        R_attnT = Rot(P, es, "gd_attnT", [128, 6, 128], BF16)
        R_Pm = [Rot(P, es, f"gd_Pm{i}", [128, 6, 128], BF16) for i in range(2)]
        R_Qm = [Rot(P, es, f"gd_Qm{i}", [128, 6, 128], BF16) for i in range(2)]
        sp_fns = [softplus12(h, P, es, f"gd{i}") for i in range(2)]
        R_Xb = Rot(P, es, "gd_Xb", [128, 6, 128], BF16)
        R_kdec = Rot(P, es, "gd_kdec", [128, 384], BF16)
        R_v_tm = Rot(P, es, "gd_vtm", [128, 384], F32)
        R_rr_ = Rot(P, es, "gd_rr", [128, 384], F32)
        R_rb = Rot(P, es, "gd_rb", [128, 384], BF16)
        R_vnb = Rot(P, es, "gd_vnb", [128, 384], BF16)
        R_oa = Rot(P, es, "gd_oa", [128, 384], F32)
        R_o = Rot(P, es, "gd_o", [128, 384], F32)
        R_sq = Rot(P, es, "gd_sq", [128, 384], F32)
        R_ss6 = Rot(P, es, "gd_ss6", [128, 6], F32)
        R_rs6 = Rot(P, es, "gd_rs6", [128, 6], F32)
        R_zg = Rot(P, es, "gd_zg", [128, 384], F32)
        R_yb = Rot(P, es, "gd_yb", [128, 384], BF16)
        R_tmpS = Rot(P, es, "gd_tmpS", [128, 3, 128], F32)
        W0 = P.ps(es, "gd_W0", [128, 2, 512], F32)
        W1 = P.ps(es, "gd_W1", [128, 2, 512], F32)
        W2 = P.ps(es, "gd_W2", [128, 2, 512], F32)
        PB = P.ps(es, "gd_PB", [128, 1024], BF16)
        pA = P.ps(es, "gd_pA", [128, 32], F32)
        x3 = lambda ap: ap.rearrange("p (h d) -> p h d", h=6)
        kz = [P.sb(es, f"gd_kz{i}", [128, 3, MT], BF16) for i in range(2)]
        R_Tb = Rot(P, es, "gd_Tb", [128, 6, 128], BF16)
        R_Tb2 = Rot(P, es, "gd_Tb2", [128, 6, 128], BF16)
        R_Xb2 = Rot(P, es, "gd_Xb2", [128, 6, 128], BF16)
        epsb = P.sb(es, "gd_eps", [128, 1], F32)
        h.memset("pool", epsb[:], EPS, [epsb])
        R_ez = Rot(P, es, "gd_ez", [128, 384], F32)
        R_M1b = Rot(P, es, "gd_M1b", [128, 6, 128], BF16)
        R_M1pb = Rot(P, es, "gd_M1pb", [128, 6, 128], BF16)
        cmask = P.sb(es, "gd_cmask", [128, 14, 128], BF16)
        h.dma("pool", cmask[:], inp["gdn_cmask"].t.rearrange("m p j -> p m j"), [], [cmask], cmask)
        cmask6 = P.sb(es, "gd_cmask6", [128, 14, 6, 128], BF16)
        h.cp("pool", cmask6[:], cmask[:].unsqueeze(2).to_broadcast([128, 14, 6, 128]), [cmask], [cmask6])
        rmask = P.sb(es, "gd_rmask", [128, 2], F32)
        h.memset("pool", rmask[:], 0.0, [rmask])
        h.memset("pool", rmask[0:64, 0:1], 1.0, [rmask])
        h.memset("pool", rmask[64:128, 1:2], 1.0, [rmask])

        def headmm(Wps, lh, rh, R):
            w = w4(Wps)
            h.mm([(w[:, hd // 3, hd % 3, :], lh[:, hd, :], rh[:, hd, :], True, True) for hd in range(6)], R, [Wps])

        stop = k.flags.get("gdn_stop", 99)
        for mt in range(NMT):
            cols = slice(mt * MT, (mt + 1) * MT)
            i2 = mt % 2
            rp = [k.r_pre[mt]]
            h.dma("sp", qnT[i2][:], k.qn_d.t[:, cols].rearrange("(j p) t -> p j t", p=128), rp, [qnT[i2]], qnT[i2])
            h.dma("sp", knT[i2][:], k.kn_d.t[:, cols].rearrange("(j p) t -> p j t", p=128), rp, [knT[i2]], knT[i2])
            h.dma("sp", vT[i2][:], k.v_d.t[:, cols].rearrange("(j p) t -> p j t", p=128), rp, [vT[i2]], vT[i2])
            h.dma("sp", pj[i2][:], k.pt_d.t[mt * MT:(mt + 1) * MT, :].rearrange("(a p) n -> p a n", p=128), rp, [pj[i2]], pj[i2])
            qn_, kn_, v_, pj_, yo_ = qnT[i2], knT[i2], vT[i2], pj[i2], yo[i2]
            for s_ in range(2):
                h.ts("dve", kz[s_][:], kn_[:], rmask[:, s_:s_ + 1], None, ALU.mult, None, [kn_, rmask], [kz[s_]])
            for ti in range(4):
                tc = slice(ti * 128, (ti + 1) * 128)
                tix = mt * 4 + ti
                sp12 = R_sp12.at(tix); gda = R_gda.at(tix); cs12 = R_cs12.at(tix); cl12 = R_cl12.at(tix); beta = R_beta.at(tix); nbeta = R_nbeta.at(tix); egc = R_egc.at(tix); t6 = R_t6.at(tix); dkk = R_dkk.at(tix); gtot = R_gtot.at(tix); gtc = R_gtc.at(tix); dg = R_dg.at(tix); arg = R_arg.at(tix); E = R_E.at(tix); EU = R_EU.at(tix); ELn = R_ELn.at(tix); attnT = R_attnT.at(tix); Xb = R_Xb.at(tix); kdec = R_kdec.at(tix); v_tm = R_v_tm.at(tix); rr_ = R_rr_.at(tix); rb = R_rb.at(tix); vnb = R_vnb.at(tix); oa = R_oa.at(tix); o = R_o.at(tix); sq = R_sq.at(tix); ss6 = R_ss6.at(tix); rs6 = R_rs6.at(tix); zg = R_zg.at(tix); yb = R_yb.at(tix); tmpS = R_tmpS.at(tix); Tb = R_Tb.at(tix); M1b = R_M1b.at(tix); M1pb = R_M1pb.at(tix)
                Pm = [r.at(tix) for r in R_Pm]; Qm = [r.at(tix) for r in R_Qm]; sp_fn = sp_fns[tix % 2]; Tb2 = R_Tb2.at(tix); Xb2 = R_Xb2.at(tix); ez = R_ez.at(tix)
                gate_tile(k, sp_fn, pj_[:, ti, 768:780], pj_, b12, na12, masks, sp12, gda, cs12, cl12, pA)
                gc = cs12[:, 0:6]
                h.act(beta[:], pj_[:, ti, 780:786], AF.Exp, [pj_], [beta], scale=-1.0)
                h.ts("dve", beta[:], beta[:], 1.0, None, ALU.add, None, [beta], [beta])
                h.recip(beta[:], beta[:], [beta], [beta])
                h.ts("dve", nbeta[:], beta[:], -1.0, None, ALU.mult, None, [beta], [nbeta])
                h.act(egc[:], gc, AF.Exp, [cs12], [egc])
                h.tt("dve", t6[:], cl12[:, 0:6], gc, ALU.subtract, [cl12, cs12], [t6])
                h.act(dkk[:], t6[:], AF.Exp, [t6], [dkk])
                h.act(gtot[:], cl12[:, 0:6], AF.Exp, [cl12], [gtot])
                g2 = gtot[:].rearrange("p (j s) -> p j s", s=2)
                h.cp("dve", gtc[0:64, :], g2[0:64, :, 0], [gtot], [gtc])
                h.cp("dve", gtc[64:128, :], g2[64:128, :, 1], [gtot], [gtc])
                if stop < 1:
                    h.memset("pool", yo_[:, :, tc], 0.0, [yo_])
                    continue
                h.tt("pool", dg[:], b3(identf[:], 6, 128), s3(gc, 6, 128), ALU.mult, [identf, cs12], [dg])
                h.mm([(W0[:, 0, 0:384], masks["ones"][:], dg[:, 0:3, :].rearrange("p a l -> p (a l)"), True, True),
                      (W0[:, 1, 0:384], masks["ones"][:], dg[:, 3:6, :].rearrange("p a l -> p (a l)"), True, True)],
                     [masks["ones"], dg], [W0])
                if stop < 1.2:
                    h.memset("pool", yo_[:, :, tc], 0.0, [yo_])
                    continue
                h.tt("dve", v4(arg[:]), w4(W0), s4(gc), ALU.subtract, [W0, cs12], [arg])
                if stop < 1.4:
                    h.memset("pool", yo_[:, :, tc], 0.0, [yo_])
                    continue
                h.act(arg[:], arg[:], AF.Abs, [arg], [arg])
                h.act(E[:], arg[:], AF.Exp, [arg], [E], scale=-1.0)
                if stop < 1.6:
                    h.memset("pool", yo_[:, :, tc], 0.0, [yo_])
                    continue
                h.tt("pool", EU[:], E[:], b3(masks["U"][:], 6, 128), ALU.mult, [E, masks["U"]], [EU])
                if stop < 1.8:
                    h.memset("pool", yo_[:, :, tc], 0.0, [yo_])
                    continue
                h.tt("pool", ELn[:], E[:], b3(masks["Ls"][:], 6, 128), ALU.mult, [E, masks["Ls"]], [ELn])
                if stop < 1.9:
                    h.memset("pool", yo_[:, :, tc], 0.0, [yo_])
                    continue
                h.tt("pool", ELn[:], ELn[:], s3(nbeta[:], 6, 128), ALU.mult, [ELn, nbeta], [ELn])
                if stop < 2:
                    h.memset("pool", yo_[:, :, tc], 0.0, [yo_])
                    continue
                w1 = w4(W1)
                w2 = w4(W2)
                h.mm([(w1[:, hd // 3, hd % 3, :], kz[hd % 2][:, hd // 2, tc], kn_[:, hd // 2, tc], True, True) for hd in range(6)],
                     [kz[0], kz[1], kn_], [W1])
                h.mm([(w2[:, hd // 3, hd % 3, :], kz[hd % 2][:, hd // 2, tc], qn_[:, hd // 2, tc], True, True) for hd in range(6)],
                     [kz[0], kz[1], qn_], [W2])
                h.tt("dve", v4(Pm[0][:]), w1, v4(ELn[:]), ALU.mult, [W1, ELn], [Pm[0]])
                h.tt("dve", v4(attnT[:]), w2, v4(EU[:]), ALU.mult, [W2, EU], [attnT])
                if stop < 3:
                    h.memset("pool", yo_[:, :, tc], 0.0, [yo_])
                    continue
                h.tr([(PB[:, hd * 128:(hd + 1) * 128], Pm[0][:, hd, :]) for hd in range(6)], identb[:], [Pm[0], identb], [PB])
                h.cp("act", Qm[0][:], PB[:, 0:768].rearrange("p (a l) -> p a l", a=6), [PB], [Qm[0]])
                Nn, NT_ = Pm[0], Qm[0]
                h.tt("pool", Pm[1][:], Nn[:], b3(cmask[:, 0, :], 6, 128), ALU.mult, [Nn, cmask], [Pm[1]])
                h.tt("pool", Qm[1][:], NT_[:], b3(cmask[:, 7, :], 6, 128), ALU.mult, [NT_, cmask], [Qm[1]])
                h.tt("dve", Tb[:], Pm[1][:], b3(identf[:], 6, 128), ALU.add, [Pm[1], identf], [Tb])
                h.tt("dve", Xb[:], Qm[1][:], b3(identf[:], 6, 128), ALU.add, [Qm[1], identf], [Xb])
                Tc, Xc = Tb, Xb
                Tn, Xn = Tb2, Xb2
                for lv in range(1, 7):
                    headmm(W1, NT_, Tc, [NT_, Tc])
                    headmm(W2, Nn, Xc, [Nn, Xc])
                    h.tt("dve", v4(M1b[:]), w4(W1), v4(cmask6[:, lv, :, :]), ALU.mult, [W1, cmask6], [M1b])
                    h.tt("dve", v4(M1pb[:]), w4(W2), v4(cmask6[:, 7 + lv, :, :]), ALU.mult, [W2, cmask6], [M1pb])
                    w1_ = w4(W1)
                    w2_ = w4(W2)
                    h.mm([x_ for hd in range(6) for x_ in (
                        (w1_[:, hd // 3, hd % 3, :], identb[:], Tc[:, hd, :], True, False),
                        (w1_[:, hd // 3, hd % 3, :], Xc[:, hd, :], M1b[:, hd, :], False, True))],
                        [identb, Tc, Xc, M1b], [W1])
                    h.mm([x_ for hd in range(6) for x_ in (
                        (w2_[:, hd // 3, hd % 3, :], identb[:], Xc[:, hd, :], True, False),
                        (w2_[:, hd // 3, hd % 3, :], Tc[:, hd, :], M1pb[:, hd, :], False, True))],
                        [identb, Tc, Xc, M1pb], [W2])
                    h.cp("act", v4(Tn[:]), w4(W1), [W1], [Tn])
                    h.cp("act", v4(Xn[:]), w4(W2), [W2], [Xn])
                    Tc, Xc, Tn, Xn = Tn, Xn, Tc, Xc
                Xb = Xc
                if stop < 4:
                    h.memset("pool", yo_[:, :, tc], 0.0, [yo_])
                    continue
                h.tr([(PB[:, j * 128:(j + 1) * 128], kn_[:, j, tc]) for j in range(3)], identb[:], [kn_, identb], [PB])
                h.tt("dve", x3(kdec[:]), x3(PB[:, 0:384]), s3(dkk[:], 6, 64), ALU.mult, [PB, dkk], [kdec])
                h.tr([(W0[:, 0, j * 128:(j + 1) * 128], v_[:, j, tc]) for j in range(3)], identf[:], [v_, identf], [W0])
                h.cp("act", v_tm[:], W0[:, 0, 0:384], [W0], [v_tm])
                if stop < 5:
                    h.memset("pool", yo_[:, :, tc], 0.0, [yo_])
                    continue
                h.mm([(W1[:, 0, j * 128:(j + 1) * 128], kn_[:, j, tc], Sb[:, j, :], True, True) for j in range(3)], [kn_, Sb], [W1])
                h.tt("dve", x3(rr_[:]), x3(W1[:, 0, 0:384]), s3(egc[:], 6, 64), ALU.mult, [W1, egc], [rr_])
                h.tt("pool", rr_[:], rr_[:], v_tm[:], ALU.subtract, [rr_, v_tm], [rr_])
                h.tt("pool", x3(rb[:]), x3(rr_[:]), s3(nbeta[:], 6, 64), ALU.mult, [rr_, nbeta], [rb])
                h.mm([(W2[:, 0, hd * 64:(hd + 1) * 64], Xb[:, hd, :], rb[:, hd * 64:(hd + 1) * 64], True, True) for hd in range(6)], [Xb, rb], [W2])
                h.cp("act", vnb[:], W2[:, 0, 0:384], [W2], [vnb])
                h.mm([(W1[:, 0, j * 128:(j + 1) * 128], qn_[:, j, tc], Sb[:, j, :], True, True) for j in range(3)], [qn_, Sb], [W1])
                h.tt("dve", x3(oa[:]), x3(W1[:, 0, 0:384]), s3(egc[:], 6, 64), ALU.mult, [W1, egc], [oa])
                h.mm([(W2[:, 0, hd * 64:(hd + 1) * 64], attnT[:, hd, :], vnb[:, hd * 64:(hd + 1) * 64], True, True) for hd in range(6)], [attnT, vnb], [W2])
                h.tt("dve", o[:], W2[:, 0, 0:384], oa[:], ALU.add, [W2, oa], [o])
                h.mm([(W0[:, 0, j * 128:(j + 1) * 128], kdec[:, j * 128:(j + 1) * 128], vnb[:, j * 128:(j + 1) * 128], True, True) for j in range(3)],
                     [kdec, vnb], [W0])
                h.tt("dve", tmpS[:], W0[:, 0, 0:384].rearrange("p (j c) -> p j c", j=3), b3(masks["bd"][:], 3, 128), ALU.mult, [W0, masks["bd"]], [tmpS])
                h.tt("pool", Sf[:], Sf[:], s3(gtc[:], 3, 128), ALU.mult, [Sf, gtc], [Sf])
                h.tt("pool", Sf[:], Sf[:], tmpS[:], ALU.add, [Sf, tmpS], [Sf])
                h.cp("act", Sb[:], Sf[:], [Sf], [Sb])
                if stop < 6:
                    h.memset("pool", yo_[:, :, tc], 0.0, [yo_])
                    continue
                h.tt("pool", sq[:], o[:], o[:], ALU.mult, [o], [sq])
                h.reduce(ss6[:], x3(sq[:]), ALU.add, [sq], [ss6])
                h.act(rs6[:], ss6[:], AF.Ln, [ss6, epsb], [rs6], bias=epsb[:, 0:1], scale=1.0 / 64)
                h.act(rs6[:], rs6[:], AF.Exp, [rs6], [rs6], scale=-0.5)
                h.tt("dve", x3(o[:]), x3(o[:]), s3(rs6[:], 6, 64), ALU.mult, [o, rs6], [o])
                h.tt("pool", x3(o[:]), x3(o[:]), b3(gnw[:], 6, 64), ALU.mult, [o, gnw], [o])
                h.act(ez[:], pj_[:, ti, 0:384], AF.Exp, [pj_], [ez], scale=-1.0)
                h.ts("pool", ez[:], ez[:], 1.0, None, ALU.add, None, [ez], [ez])
                h.tt("pool", zg[:], o[:], pj_[:, ti, 0:384], ALU.mult, [o, pj_], [zg])
                h.recip(ez[:], ez[:], [ez], [ez])
                h.tt("pool", yb[:], zg[:], ez[:], ALU.mult, [zg, ez], [yb])
                h.tr([(PB[:, j * 128:(j + 1) * 128], yb[:, j * 128:(j + 1) * 128]) for j in range(3)], identb[:], [yb, identb], [PB])
                h.cp("act", yo_[:, :, tc], PB[:, 0:384].rearrange("p (j t) -> p j t", j=3), [PB], [yo_])
            h.dma("sp", k.y_d.t[256:640, cols].rearrange("(j p) t -> p j t", p=128), yo_[:], [yo_], [k.r_y[1][mt]], yo_)
        P.end_phase()


class K:
    def __init__(self, T, L, flags):
        self.T = T
        self.L = L
        self.NT = T // 128
        self.flags = flags


def bcast(ap, shape):
    return ap.to_broadcast(list(shape))


def build(T, L, flags=None):
    flags = flags or {}
    nc = bass.Bass("TRN2", target_bir_lowering=False)
    k = K(T, L, flags)
    NT = T // 128
    with contextlib.ExitStack() as es0:
        P = Prog(nc, es0)
        P.verbose = bool(flags.get("verbose"))
        k.P = P
        inp = {}

        def ein(name, shape):
            inp[name] = P.dram(name, shape, F32, "ExternalInput")
            return inp[name]

        x_in = ein("x", [T, D])
        c_in = ein("c", [1, D])
        w_ada = ein("w_ada", [L, D, 6 * D])
        b_ada = ein("b_ada", [L, 6 * D])
        norm_mix = ein("norm_mix", [L, D])
        norm_ffn = ein("norm_ffn", [L, D])
        norm_final = ein("norm_final", [1, D])
        w_rt = ein("w_rt", [L, D, 36])
        b_rt = ein("b_rt", [L, 36])
        w_gate = ein("moe_w_gate", [L, NEXP, D, DEXP])
        w_up = ein("moe_w_up", [L, NEXP, D, DEXP])
        w_down = ein("moe_w_down", [L, NEXP, DEXP, D])
        ein("w_in_f", [L, D, 2304])
        ein("w_in_t", [L, D, 786])
        ein("w_out", [L, D, D])
        ein("conv_w", [L, 128, 16, 4])
        ein("conv_b", [L, 128, 16])
        ein("bias12", [L, 12])
        ein("alog12", [L, 12])
        ein("ssd_d", [L, 6])
        ein("ssd_norm", [L, 384])
        ein("gdn_norm", [L, 64])
        ein("gdn_cmask", [14, 128, 128])
        ein("s5_are", [L, 128, 8])
        ein("s5_aim", [L, 128, 8])
        ein("s5_ldt", [L, 128, 8])
        ein("s5_dcol", [L, 128, 2])
        ein("s5_ncol", [L, 128, 2])
        ein("s5_bT_re", [L, 8, 128, 128])
        ein("s5_bT_im", [L, 8, 128, 128])
        ein("s5_cT_re", [L, 8, 128, 128])
        ein("s5_cT_im", [L, 8, 128, 128])
        ein("s5_w_glu", [L, 256, 256])
        out = P.dram("out", [T, D], F32, "ExternalOutput")
        k.u_d = P.dram("u_d", [256, T], F32)
        k.qn_d = P.dram("qn_d", [384, T], BF16)
        k.kn_d = P.dram("kn_d", [384, T], BF16)
        k.v_d = P.dram("v_d", [384, T], F32)
        k.xs_d = P.dram("xs_d", [384, T], F32)
        k.B_d = P.dram("B_d", [256, T], BF16)
        k.C_d = P.dram("C_d", [256, T], BF16)
        k.pt_d = P.dram("pt_d", [T, 786], F32)
        k.y_d = P.dram("y_d", [D, T], BF16)
        k.r_pre = [P.region(f"pre_{i}") for i in range(max(1, T // 512))]
        k.r_y = [[P.region(f"y{j}_{i}") for i in range(max(1, T // 512))] for j in range(3)]
        k.h = H(P)
        h = k.h
        modv = P.dram("modv", [L, 6 * D], F32)
        dbg = flags.get("dbg", False)
        if dbg:
            dbg_coef = P.dram("dbg_coef", [T, 32], F32, "ExternalOutput")
            dbg_y = P.dram("dbg_y", [T, D], F32, "ExternalOutput")
            dbg_h = P.dram("dbg_h", [T, D], F32, "ExternalOutput")
            r_dbg = P.region("dbg")
        scr = [P.dram("xs0", [T, D], F32), P.dram("xs1", [T, D], F32)]
        k.inp = inp

        def regs(name):
            return [P.region(f"{name}_{i}") for i in range(NT)]
        r_x = regs("x")
        r_scr = [regs("xs0"), regs("xs1")]
        r_out = regs("out")
        r_modv = P.region("modv")

        identf = P.sb(es0, "identf", [128, 128], F32)
        identb = P.sb(es0, "identb", [128, 128], BF16)
        P.op("pool", lambda e: e.memset(identf[:], 0.0), writes=[identf])
        P.op("pool", lambda e: e.affine_select(out=identf[:], in_=identf[:], pattern=[[-1, 128]],
                                               compare_op=ALU.not_equal, fill=1.0, base=0,
                                               channel_multiplier=1),
             reads=[identf], writes=[identf])
        P.op("dve", lambda e: e.tensor_copy(out=identb[:], in_=identf[:]), reads=[identf], writes=[identb])
        k.identf, k.identb = identf, identb

        with contextlib.ExitStack() as es:
            ccol = P.sb(es, "ccol", [128, 8], F32)
            cb = P.sb(es, "cb", [128, 8, 128], BF16)
            wa = [P.sb(es, f"wa{i}", [128, 8, 512], BF16) for i in range(2)]
            pm = [P.ps(es, f"pm{i}", [128, 512], F32) for i in range(2)]
            brow = [P.sb(es, f"brow{i}", [1, 512], F32) for i in range(2)]
            mrow = [P.sb(es, f"mrow{i}", [1, 512], F32) for i in range(2)]
            P.op("sp", lambda e: e.dma_start(out=ccol[:], in_=c_in.t.rearrange("o (k p) -> p (o k)", p=128),
                                             allow_slow_non_contiguous=True),
                 writes=[ccol], dma=ccol)
            P.op("act", lambda e: e.activation(out=ccol[:], in_=ccol[:], func=AF.Silu), reads=[ccol], writes=[ccol])
            P.op("dve", lambda e: e.tensor_copy(out=cb[:], in_=bcast(ccol[:].unsqueeze(2), [128, 8, 128])),
                 reads=[ccol], writes=[cb])
            it = 0
            for l in range(L):
                for n in range(12):
                    i = it % 2
                    it += 1
                    P.op("pool", lambda e, l=l, n=n, i=i: e.dma_start(
                        out=wa[i][:], in_=w_ada.t[l, :, n * 512:(n + 1) * 512].rearrange("(k p) n -> p k n", p=128)),
                        writes=[wa[i]], dma=wa[i])
                    P.op("sp", lambda e, l=l, n=n, i=i: e.dma_start(
                        out=brow[i][:], in_=b_ada.t[l:l + 1, n * 512:(n + 1) * 512]),
                        writes=[brow[i]], dma=brow[i])

                    def mm(e, i=i):
                        r = None
                        for kk in range(8):
                            r = e.matmul(pm[i][:], lhsT=cb[:, kk, :], rhs=wa[i][:, kk, :], start=(kk == 0), stop=(kk == 7))
                        return r
                    P.op("pe", mm, reads=[cb, wa[i]], writes=[pm[i]])
                    P.op("dve", lambda e, i=i: e.tensor_tensor(out=mrow[i][:], in0=pm[i][0:1, :], in1=brow[i][:], op=ALU.add),
                         reads=[pm[i], brow[i]], writes=[mrow[i]])
                    P.op("sp", lambda e, l=l, n=n, i=i: e.dma_start(
                        out=modv.t[l:l + 1, n * 512:(n + 1) * 512], in_=mrow[i][:]),
                        reads=[mrow[i]], writes=[r_modv], dma=mrow[i])
            P.end_phase()

        def load_row(dst, src_ap, extra_reads=()):
            P.op("sp", lambda e: e.dma_start(out=dst[:], in_=src_ap.partition_broadcast(128)),
                 reads=list(extra_reads), writes=[dst], dma=dst)

        def norm_consts(es, l, nw, i_scale, i_shift, tag):
            A = P.sb(es, f"A{tag}", [128, D], F32)
            B = P.sb(es, f"B{tag}", [128, D], F32)
            W = P.sb(es, f"W{tag}", [128, D], F32)
            load_row(A, modv.t[l:l + 1, i_scale * D:(i_scale + 1) * D], [r_modv])
            load_row(B, modv.t[l:l + 1, i_shift * D:(i_shift + 1) * D], [r_modv])
            load_row(W, nw)
            P.op("dve", lambda e: e.scalar_tensor_tensor(out=A[:], in0=A[:], scalar=1.0, in1=W[:], op0=ALU.add, op1=ALU.mult),
                 reads=[A, W], writes=[A])
            return A, B

        def rms_mod(xt, A, B, hout, ssq, rstd, junk, tmp):
            P.op("act", lambda e: e.activation(out=junk[:], in_=xt[:], func=AF.Square, accum_out=ssq[:]),
                 reads=[xt], writes=[junk, ssq], est=0.9)
            P.op("dve", lambda e: e.tensor_scalar(out=rstd[:], in0=ssq[:], scalar1=1.0 / D, scalar2=EPS, op0=ALU.mult, op1=ALU.add),
                 reads=[ssq], writes=[rstd])
            P.op("act", lambda e: e.activation(out=rstd[:], in_=rstd[:], func=AF.Sqrt), reads=[rstd], writes=[rstd])
            P.op("dve", lambda e: e.reciprocal(out=rstd[:], in_=rstd[:]), reads=[rstd], writes=[rstd])
            if B is None:
                P.op("dve", lambda e: e.scalar_tensor_tensor(out=hout[:], in0=xt[:], scalar=rstd[:, 0:1], in1=A[:], op0=ALU.mult, op1=ALU.mult),
                     reads=[xt, rstd, A], writes=[hout], est=1.15)
            else:
                P.op("dve", lambda e: e.scalar_tensor_tensor(out=tmp[:], in0=xt[:], scalar=rstd[:, 0:1], in1=A[:], op0=ALU.mult, op1=ALU.mult),
                     reads=[xt, rstd, A], writes=[tmp], est=1.15)
                P.op("pool", lambda e: e.tensor_tensor(out=hout[:], in0=tmp[:], in1=B[:], op=ALU.add),
                     reads=[tmp, B], writes=[hout], est=1.3)

        k.modv, k.r_modv = modv, r_modv
        k.load_row, k.norm_consts, k.rms_mod = load_row, norm_consts, rms_mod
        cur = (x_in, r_x)
        nxt_i = 0

        def next_dst():
            nonlocal nxt_i
            d = (scr[nxt_i], r_scr[nxt_i])
            nxt_i ^= 1
            return d

        for l in range(L):
            if flags.get("mixer", True):
                dstt = next_dst()
                mixer_layer(k, l, cur, dstt)
                cur = dstt
            if flags.get("moe", True):
                src, rsrc = cur
                dst, rdst = next_dst()
                SBT = min(16, NT)
                with contextlib.ExitStack() as es:
                    A, B = norm_consts(es, l, norm_ffn.t[l:l + 1, :], 4, 3, "f")
                    G = P.sb(es, "Gf", [128, D], F32)
                    load_row(G, modv.t[l:l + 1, 5 * D:6 * D], [r_modv])
                    brt = P.sb(es, "brt", [128, 36], F32)
                    load_row(brt, b_rt.t[l:l + 1, :])
                    wrt = P.sb(es, "wrt", [128, 8, 36], F32)
                    P.op("sp", lambda e: e.dma_start(out=wrt[:], in_=w_rt.t[l].rearrange("(k p) n -> p k n", p=128)),
                         writes=[wrt], dma=wrt)
                    hT = P.sb(es, "hT", [128, 8, SBT * 128], BF16)
                    yacc = [P.sb(es, f"yacc{i}", [128, D], F32) for i in range(SBT)]
                    coef = P.sb(es, "coef", [128, SBT, 32], F32)
                    xt = [P.sb(es, f"xt{i}", [128, D], F32) for i in range(2)]
                    R_hf = Rot(P, es, "hf", [128, D], F32)
                    R_tmp = Rot(P, es, "tmpf", [128, D], F32)
                    R_junk = Rot(P, es, "junkf", [128, D], F32)
                    R_hTf = Rot(P, es, "hTf", [128, 8, 128], F32)
                    R_ssq = Rot(P, es, "ssq", [128, 1], F32)
                    R_rstd = Rot(P, es, "rstd", [128, 1], F32)
                    R_lg = Rot(P, es, "lg", [128, 36], F32)
                    R_sm = Rot(P, es, "sm", [128, 16], F32)
                    R_gm = Rot(P, es, "gm", [128, 4], F32)
                    R_gex = Rot(P, es, "gex", [128, 4], F32)
                    R_le4 = Rot(P, es, "le4", [128, 4, 8], F32)
                    R_les = Rot(P, es, "les", [128, 8], F32)
                    R_le2 = Rot(P, es, "le2", [128, 8], F32)
                    R_mk1 = Rot(P, es, "mk1", [128, 8], F32)
                    R_mk2 = Rot(P, es, "mk2", [128, 8], F32)
                    R_csel = Rot(P, es, "csel", [128, 8], F32)
                    wg = [P.sb(es, f"wg{i}", [128, 8, DEXP], BF16) for i in range(2)]
                    wu = [P.sb(es, f"wu{i}", [128, 8, DEXP], BF16) for i in range(2)]
                    wd = [P.sb(es, f"wd{i}", [128, 2, D], BF16) for i in range(2)]
                    sg = [P.sb(es, f"sg{i}", [128, 512], F32) for i in range(2)]
                    hid = [P.sb(es, f"hid{i}", [128, 2, 512], BF16) for i in range(2)]
                    ptr = P.ps(es, "ptr", [128, 8, 128], F32)
                    pg = [P.ps(es, f"pg{i}", [128, 512], F32) for i in range(2)]
                    pu = [P.ps(es, f"pu{i}", [128, 512], F32) for i in range(2)]
                    py = [P.ps(es, f"py{i}", [128, 512], F32) for i in range(2)]

                    wcnt = 0
                    for sb0 in range(0, NT, SBT):
                        def router_tile(t, ti, xb, hf, tmp, junk, hTf, ssq, rstd, lg, sm, gm, gex, le4, les, le2, mk1, mk2, csel):
                                P.op("sp", lambda e, t=t, xb=xb: e.dma_start(out=xb[:], in_=src.t[t * 128:(t + 1) * 128, :]),
                                     reads=[rsrc[t]], writes=[xb], dma=xb)
                                rms_mod(xb, A, B, hf, ssq, rstd, junk, tmp)

                                if dbg and l == 0:
                                    P.op("sp", lambda e, t=t: e.dma_start(out=dbg_h.t[t * 128:(t + 1) * 128, :], in_=hf[:]),
                                         reads=[hf], writes=[r_dbg], dma=hf)

                                def trf(e):
                                    r = None
                                    for kk in range(8):
                                        r = e.transpose(out=ptr[:, kk, :], in_=hf[:, kk * 128:(kk + 1) * 128], identity=identf[:])
                                    return r
                                P.op("pe", trf, reads=[hf, identf], writes=[ptr])
                                P.op("act", lambda e: e.copy(out=hTf[:], in_=ptr[:]), reads=[ptr], writes=[hTf])
                                P.op("dve", lambda e, ti=ti: e.tensor_copy(out=hT[:, :, ti * 128:(ti + 1) * 128], in_=hTf[:]),
                                     reads=[hTf], writes=[hT])

                                def mrt(e):
                                    r = None
                                    for kk in range(8):
                                        r = e.matmul(ptr[:, 0, 0:36], lhsT=hTf[:, kk, :], rhs=wrt[:, kk, :], start=(kk == 0), stop=(kk == 7))
                                    return r
                                P.op("pe", mrt, reads=[hTf, wrt], writes=[ptr])
                                P.op("dve", lambda e: e.tensor_tensor(out=lg[:], in0=ptr[:, 0, 0:36], in1=brt[:], op=ALU.add),
                                     reads=[ptr, brt], writes=[lg])
                                P.op("dve", lambda e: e.tensor_reduce(out=sm[:, 0:1], in_=lg[:, 0:4], axis=AX.X, op=ALU.max),
                                     reads=[lg], writes=[sm])
                                P.op("dve", lambda e: e.tensor_scalar(out=gm[:], in0=lg[:, 0:4], scalar1=sm[:, 0:1], scalar2=None, op0=ALU.is_equal),
                                     reads=[lg, sm], writes=[gm])
                                P.op("dve", lambda e: e.tensor_scalar(out=sm[:, 1:2], in0=sm[:, 0:1], scalar1=-1.0, scalar2=None, op0=ALU.mult),
                                     reads=[sm], writes=[sm])
                                P.op("act", lambda e: e.activation(out=gex[:], in_=lg[:, 0:4], func=AF.Exp, bias=sm[:, 1:2], accum_out=sm[:, 2:3]),
                                     reads=[lg, sm], writes=[gex, sm])
                                P.op("dve", lambda e: e.reciprocal(out=sm[:, 3:4], in_=sm[:, 2:3]), reads=[sm], writes=[sm])
                                P.op("dve", lambda e: e.tensor_tensor(out=le4[:], in0=lg[:, 4:36].rearrange("p (g e) -> p g e", g=4),
                                                                      in1=bcast(gm[:].unsqueeze(2), [128, 4, 8]), op=ALU.mult),
                                     reads=[lg, gm], writes=[le4])
                                P.op("dve", lambda e: e.tensor_reduce(out=les[:], in_=le4[:].rearrange("p g e -> p e g"), axis=AX.X, op=ALU.add),
                                     reads=[le4], writes=[les])
                                P.op("dve", lambda e: e.tensor_reduce(out=sm[:, 4:5], in_=les[:], axis=AX.X, op=ALU.max), reads=[les], writes=[sm])
                                P.op("dve", lambda e: e.tensor_scalar(out=mk1[:], in0=les[:], scalar1=sm[:, 4:5], scalar2=None, op0=ALU.is_equal),
                                     reads=[les, sm], writes=[mk1])
                                P.op("dve", lambda e: e.scalar_tensor_tensor(out=le2[:], in0=mk1[:], scalar=-1e30, in1=les[:], op0=ALU.mult, op1=ALU.add),
                                     reads=[mk1, les], writes=[le2])
                                P.op("dve", lambda e: e.tensor_reduce(out=sm[:, 5:6], in_=le2[:], axis=AX.X, op=ALU.max), reads=[le2], writes=[sm])
                                P.op("dve", lambda e: e.tensor_scalar(out=mk2[:], in0=le2[:], scalar1=sm[:, 5:6], scalar2=None, op0=ALU.is_equal),
                                     reads=[le2, sm], writes=[mk2])
                                P.op("dve", lambda e: e.tensor_tensor(out=sm[:, 6:7], in0=sm[:, 4:5], in1=sm[:, 5:6], op=ALU.subtract),
                                     reads=[sm], writes=[sm])
                                P.op("act", lambda e: e.activation(out=sm[:, 7:8], in_=sm[:, 6:7], func=AF.Sigmoid), reads=[sm], writes=[sm])
                                P.op("act", lambda e: e.activation(out=sm[:, 8:9], in_=sm[:, 6:7], func=AF.Sigmoid, scale=-1.0), reads=[sm], writes=[sm])
                                P.op("dve", lambda e: e.tensor_scalar(out=sm[:, 7:9], in0=sm[:, 7:9], scalar1=sm[:, 3:4], scalar2=None, op0=ALU.mult),
                                     reads=[sm], writes=[sm])
                                P.op("dve", lambda e: e.tensor_scalar(out=csel[:], in0=mk1[:], scalar1=sm[:, 7:8], scalar2=None, op0=ALU.mult),
                                     reads=[mk1, sm], writes=[csel])
                                P.op("dve", lambda e: e.scalar_tensor_tensor(out=csel[:], in0=mk2[:], scalar=sm[:, 8:9], in1=csel[:], op0=ALU.mult, op1=ALU.add),
                                     reads=[mk2, sm, csel], writes=[csel])
                                P.op("dve", lambda e, ti=ti: e.tensor_tensor(out=coef[:, ti, :].rearrange("p (g e) -> p g e", g=4),
                                                                             in0=bcast(gm[:].unsqueeze(2), [128, 4, 8]),
                                                                             in1=bcast(csel[:].unsqueeze(1), [128, 4, 8]), op=ALU.mult),
                                     reads=[gm, csel], writes=[coef])

                        for ti in range(SBT):
                            t = sb0 + ti
                            router_tile(t, ti, xt[t % 2], R_hf.at(t), R_tmp.at(t), R_junk.at(t), R_hTf.at(t), R_ssq.at(t), R_rstd.at(t), R_lg.at(t), R_sm.at(t), R_gm.at(t), R_gex.at(t), R_le4.at(t), R_les.at(t), R_le2.at(t), R_mk1.at(t), R_mk2.at(t), R_csel.at(t))
                        nblk = (SBT * 128 + 511) // 512
                        seq = [(ex, blk) for ex in range(NEXP) for blk in range(nblk)]

                        def load_w(ex):
                            wi = ex % 2
                            h.dma("pool", wg[wi][:], w_gate.t[l, ex].rearrange("(k p) n -> p k n", p=128), [], [wg[wi]], wg[wi])
                            h.dma("pool", wu[wi][:], w_up.t[l, ex].rearrange("(k p) n -> p k n", p=128), [], [wu[wi]], wu[wi])
                            h.dma("pool", wd[wi][:], w_down.t[l, ex].rearrange("(k p) n -> p k n", p=128), [], [wd[wi]], wd[wi])

                        def GU(i):
                            ex, blk = seq[i]
                            wi, bi = ex % 2, i % 2
                            c0 = blk * 512
                            cw = min(512, SBT * 128 - c0)
                            for fc in range(2):
                                fs = slice(fc * 128, (fc + 1) * 128)
                                h.mm([(pg[fc][:, 0:cw], wg[wi][:, kk, fs], hT[:, kk, c0:c0 + cw], kk == 0, kk == 7) for kk in range(8)],
                                     [wg[wi], hT], [pg[fc]])
                                h.act(sg[fc][:, 0:cw], pg[fc][:, 0:cw], AF.Silu, [pg[fc]], [sg[fc]])
                                yield
                                h.mm([(pu[fc][:, 0:cw], wu[wi][:, kk, fs], hT[:, kk, c0:c0 + cw], kk == 0, kk == 7) for kk in range(8)],
                                     [wu[wi], hT], [pu[fc]])
                                h.tt("dve", hid[bi][:, fc, 0:cw], sg[fc][:, 0:cw], pu[fc][:, 0:cw], ALU.mult, [sg[fc], pu[fc]], [hid[bi]])
                                yield

                        def DN(i):
                            ex, blk = seq[i]
                            wi, bi = ex % 2, i % 2
                            c0 = blk * 512
                            cw = min(512, SBT * 128 - c0)
                            for st in range(cw // 128):
                                ti = blk * 4 + st
                                for n2 in range(2):
                                    pi = n2
                                    ns = slice(n2 * 512, (n2 + 1) * 512)
                                    h.mm([(py[pi][:], hid[bi][:, fc, st * 128:(st + 1) * 128], wd[wi][:, fc, ns], fc == 0, fc == 1) for fc in range(2)],
                                         [hid[bi], wd[wi]], [py[pi]])
                                    if ex == 0:
                                        h.ts("dve", yacc[ti][:, ns], py[pi][:], coef[:, ti, ex:ex + 1], None, ALU.mult, None, [py[pi], coef], [yacc[ti]])
                                    else:
                                        h.stt("dve", yacc[ti][:, ns], py[pi][:], coef[:, ti, ex:ex + 1], yacc[ti][:, ns], ALU.mult, ALU.add,
                                              [py[pi], coef, yacc[ti]], [yacc[ti]])
                                    yield
                            if blk == nblk - 1 and ex + 2 < NEXP:
                                load_w(ex + 2)

                        def drain(g):
                            for _ in g:
                                pass

                        def step(g, n):
                            for _ in range(n):
                                if next(g, "END") == "END":
                                    return

                        load_w(0)
                        load_w(1)
                        drain(GU(0))
                        for i in range(len(seq)):
                            gd = DN(i)
                            if i + 1 < len(seq):
                                gg = GU(i + 1)
                                for _ in range(4):
                                    step(gg, 1)
                                    step(gd, 2)
                                drain(gg)
                            drain(gd)
                        for ti in range(SBT):
                            t = sb0 + ti
                            if dbg and l == 0:
                                P.op("sp", lambda e, t=t, ti=ti: e.dma_start(out=dbg_y.t[t * 128:(t + 1) * 128, :], in_=yacc[ti][:]),
                                     reads=[yacc[ti]], writes=[r_dbg], dma=yacc[ti])
                                P.op("sp", lambda e, t=t, ti=ti: e.dma_start(out=dbg_coef.t[t * 128:(t + 1) * 128, :], in_=coef[:, ti, :]),
                                     reads=[coef], writes=[r_dbg], dma=coef)
                            xb = xt[t % 2]
                            P.op("sp", lambda e, t=t, xb=xb: e.dma_start(out=xb[:], in_=src.t[t * 128:(t + 1) * 128, :]),
                                 reads=[rsrc[t]], writes=[xb], dma=xb)
                            P.op("pool", lambda e, ti=ti: e.tensor_tensor(out=yacc[ti][:], in0=yacc[ti][:], in1=G[:], op=ALU.mult),
                                 reads=[yacc[ti], G], writes=[yacc[ti]])
                            P.op("dve", lambda e, ti=ti, xb=xb: e.tensor_tensor(out=yacc[ti][:], in0=yacc[ti][:], in1=xb[:], op=ALU.add),
                                 reads=[yacc[ti], xb], writes=[yacc[ti]])
                            P.op("sp", lambda e, t=t, ti=ti: e.dma_start(out=dst.t[t * 128:(t + 1) * 128, :], in_=yacc[ti][:]),
                                 reads=[yacc[ti]], writes=[rdst[t]], dma=yacc[ti])
                    P.end_phase()
                cur = (dst, rdst)

        src, rsrc = cur
        with contextlib.ExitStack() as es:
            Wn = P.sb(es, "Wn", [128, D], F32)
            load_row(Wn, norm_final.t[0:1, :])
            xt = [P.sb(es, f"xtn{i}", [128, D], F32) for i in range(2)]
            ho = [P.sb(es, f"hon{i}", [128, D], F32) for i in range(2)]
            junk = P.sb(es, "junkn", [128, D], F32)
            ssq = P.sb(es, "ssqn", [128, 1], F32)
            rstd = P.sb(es, "rstdn", [128, 1], F32)
            for t in range(NT):
                xb = xt[t % 2]
                hb = ho[t % 2]
                P.op("sp", lambda e, t=t, xb=xb: e.dma_start(out=xb[:], in_=src.t[t * 128:(t + 1) * 128, :]),
                     reads=[rsrc[t]], writes=[xb], dma=xb)
                rms_mod(xb, Wn, None, hb, ssq, rstd, junk, None)
                P.op("sp", lambda e, t=t, hb=hb: e.dma_start(out=out.t[t * 128:(t + 1) * 128, :], in_=hb[:]),
                     reads=[hb], writes=[r_out[t]], dma=hb)
            P.final_wait("sp", r_out)
            P.end_phase()
    return nc


def host_inputs(inputs, L, T):
    f = lambda a: np.ascontiguousarray(np.asarray(a, dtype=np.float32))
    w_rt = f(np.concatenate([inputs["moe_w_grp"][:L], inputs["moe_w_rt"][:L]], axis=-1))
    b_rt = f(np.concatenate([inputs["moe_b_grp"][:L], inputs["moe_b_rt"][:L]], axis=-1))
    w_in = np.asarray(inputs["w_in"][:L], dtype=np.float32)
    w_in_f = np.concatenate([w_in[:, :, 0:1408], w_in[:, :, 2188:3084]], axis=-1)
    w_in_t = np.concatenate([w_in[:, :, 1408:1792], w_in[:, :, 1804:2188], w_in[:, :, 1792:1798],
                             w_in[:, :, 3084:3090], w_in[:, :, 1798:1804]], axis=-1)
    gcw = np.asarray(inputs["gdn_conv_w"][:L], dtype=np.float32)
    scw = np.asarray(inputs["ssd_conv_w"][:L], dtype=np.float32)
    cw = np.concatenate([gcw, scw], axis=-1)
    conv_w = cw.reshape(L, 4, 16, 128).transpose(0, 3, 2, 1)
    cb = np.concatenate([np.zeros((L, 1152), np.float32), np.asarray(inputs["ssd_conv_b"][:L], dtype=np.float32)], axis=-1)
    conv_b = cb.reshape(L, 16, 128).transpose(0, 2, 1)
    bias12 = np.concatenate([inputs["gdn_dt_bias"][:L], inputs["ssd_dt_bias"][:L]], axis=-1)
    alog12 = np.concatenate([inputs["gdn_a_log"][:L], inputs["ssd_a_log"][:L]], axis=-1)
    def st_layout(a):
        a = np.asarray(a[:L], dtype=np.float32)
        return a.reshape(L, 8, 2, 64).transpose(0, 2, 3, 1).reshape(L, 128, 8)
    ldt = np.repeat(np.asarray(inputs["s5_log_dt"][:L], dtype=np.float32)[:, :, None], 64, axis=2)
    def bT_layout(b):
        b = np.asarray(b[:L], dtype=np.float32)
        o = np.zeros((L, 8, 128, 128), np.float32)
        for sc in range(8):
            for gl in range(2):
                r0 = 32 * (sc % 4) + 16 * gl
                o[:, sc, r0:r0 + 16, gl * 64:(gl + 1) * 64] = b[:, 2 * sc + gl].transpose(0, 2, 1)
        return o
    def cT_layout(c):
        c = np.asarray(c[:L], dtype=np.float32)
        o = np.zeros((L, 8, 128, 128), np.float32)
        for sc in range(8):
            for gl in range(2):
                r0 = 32 * (sc % 4) + 16 * gl
                o[:, sc, gl * 64:(gl + 1) * 64, r0:r0 + 16] = c[:, 2 * sc + gl].transpose(0, 2, 1)
        return o
    ii = np.arange(128)[:, None]
    jj = np.arange(128)[None, :]
    cm = []
    for lv in range(7):
        s_ = 1 << lv
        cm.append(((ii // (2 * s_) == jj // (2 * s_)) & (ii % (2 * s_) >= s_) & (jj % (2 * s_) < s_)).astype(np.float32))
    cmask = np.stack(cm + [m.T for m in cm], axis=0)
    col2 = lambda a: np.asarray(a[:L], dtype=np.float32).reshape(L, 2, 128).transpose(0, 2, 1)
    shared = {
        "gdn_cmask": f(cmask),
        "s5_are": f(st_layout(inputs["s5_a_re"])), "s5_aim": f(st_layout(inputs["s5_a_im"])), "s5_ldt": f(st_layout(ldt)),
        "s5_dcol": f(col2(inputs["s5_d"])), "s5_ncol": f(col2(inputs["s5_norm"])),
        "s5_bT_re": f(bT_layout(inputs["s5_b_re"])), "s5_bT_im": f(bT_layout(inputs["s5_b_im"])),
        "s5_cT_re": f(cT_layout(inputs["s5_c_re"])), "s5_cT_im": f(cT_layout(inputs["s5_c_im"])),
        "s5_w_glu": f(inputs["s5_w_glu"][:L]),
        "w_in_f": f(w_in_f), "w_in_t": f(w_in_t), "w_out": f(inputs["w_out"][:L]),
        "conv_w": f(conv_w), "conv_b": f(conv_b), "bias12": f(bias12), "alog12": f(alog12),
        "ssd_d": f(inputs["ssd_d"][:L]), "ssd_norm": f(inputs["ssd_norm"][:L]), "gdn_norm": f(inputs["gdn_norm"][:L]),
        "w_ada": f(inputs["w_ada"][:L]), "b_ada": f(inputs["b_ada"][:L]),
        "norm_mix": f(inputs["norm_mix"][:L]), "norm_ffn": f(inputs["norm_ffn"][:L]),
        "norm_final": f(inputs["norm_final"]).reshape(1, D),
        "w_rt": w_rt, "b_rt": b_rt,
        "moe_w_gate": f(inputs["moe_w_gate"][:L]), "moe_w_up": f(inputs["moe_w_up"][:L]),
        "moe_w_down": f(inputs["moe_w_down"][:L]),
    }
    maps = []
    B = inputs["x"].shape[0]
    for b in range(B):
        m = dict(shared)
        m["x"] = f(inputs["x"][b, :T])
        m["c"] = f(inputs["c"][b]).reshape(1, D)
        maps.append(m)
    return maps


def run(inputs, L, T, flags=None, trace=False):
    nc = build(T, L, flags)
    maps = host_inputs(inputs, L, T)
    res = run_bass_kernel_spmd(nc, maps, core_ids=list(range(len(maps))))
    if flags and flags.get("dbg"):
        return res.results
    return np.stack([r["out"] for r in res.results], axis=0)


def kernel(**inputs):
    return run(inputs, 4, 4096).astype(np.float32)
```

```python
import contextlib
import math
import numpy as np
import concourse.bass as bass
import concourse.mybir as mybir
from concourse.bass_utils import run_bass_kernel_spmd

F32 = mybir.dt.float32
BF16 = mybir.dt.bfloat16
ALU = mybir.AluOpType
AF = mybir.ActivationFunctionType
AX = mybir.AxisListType

D = 1024
NEXP = 32
DEXP = 256
EPS = 1e-6
ENGS = ("pe", "act", "dve", "pool", "sp")


class Buf:
    def __init__(self, t, name, multi=False):
        self.t = t
        self.name = name
        self.w = {}
        self.r = {}
        self.sem = None
        self.dcnt = 0
        self.multi = multi

    def __getitem__(self, k):
        return self.t[k]


class Prog:
    SEM_LAT = 0.15

    def __init__(self, nc, es):
        self.nc = nc
        self.es = es
        self.ops = []
        self.sems = []
        self.esem = {}
        self.ecnt = {e: 0 for e in ENGS}
        self.waited = {e: {} for e in ENGS}
        for e in ENGS:
            if e != "sp":
                self.esem[e] = self.newsem("e_" + e)
        self.uid = 0
        self.dsem_pool = []
        self.dbufs = []
        self.phase_bufs = []

    def newsem(self, name):
        s = self.es.enter_context(self.nc.semaphore(name))
        self.sems.append(s)
        return len(self.sems) - 1

    def sb(self, es, name, shape, dtype):
        self.uid += 1
        name = f"{name}_{self.uid}"
        t = es.enter_context(self.nc.sbuf_tensor(name, list(shape), dtype))
        b = Buf(t, name)
        self.phase_bufs.append(b)
        return b

    def ps(self, es, name, shape, dtype):
        self.uid += 1
        name = f"{name}_{self.uid}"
        t = es.enter_context(self.nc.psum_tensor(name, list(shape), dtype))
        return Buf(t, name)

    def dram(self, name, shape, dtype, kind="Internal"):
        t = self.nc.dram_tensor(name, list(shape), dtype, kind=kind).ap()
        return Buf(t, name)

    def region(self, name):
        return Buf(None, name, multi=True)

    def op(self, eng, fn, reads=(), writes=(), dma=None, est=None):
        if est is None:
            est = {"pe": 1.0, "act": 0.5, "dve": 0.35, "pool": 0.45, "sp": 3.0}[eng] if dma is None else 3.0
        self.ops.append((eng, fn, tuple(reads), tuple(writes), dma, est))

    def final_wait(self, eng, bufs):
        pass

    def end_phase(self):
        ops = self.ops
        self.ops = []
        n = len(ops)
        lw, rd = {}, {}
        deps = [None] * n
        for i, (eng, fn, reads, writes, dma, est) in enumerate(ops):
            d = set()
            for b in reads:
                d.update(lw.get(id(b), ()))
            for b in writes:
                d.update(rd.get(id(b), ()))
                if not b.multi:
                    d.update(lw.get(id(b), ()))
            deps[i] = d
            for b in reads:
                rd.setdefault(id(b), []).append(i)
            for b in writes:
                if b.multi:
                    lw.setdefault(id(b), []).append(i)
                else:
                    lw[id(b)] = [i]
                    rd[id(b)] = []
        import heapq
        succ = [[] for _ in range(n)]
        indeg = [0] * n
        for i in range(n):
            indeg[i] = len(deps[i])
            for j in deps[i]:
                succ[j].append(i)
        ready_t = [0.0] * n
        fin = [0.0] * n
        start = [0.0] * n
        efree = {e: 0.0 for e in ENGS}
        heap = [(0.0, i) for i in range(n) if indeg[i] == 0]
        heapq.heapify(heap)
        order = {e: [] for e in ENGS}
        glob = []
        while heap:
            rt, i = heapq.heappop(heap)
            eng, fn, reads, writes, dma, est = ops[i]
            st = max(rt, efree[eng])
            start[i] = st
            if dma is not None:
                occ = 0.5 if eng == "pool" else 0.08
                efree[eng] = st + occ
                fin[i] = st + occ + est
            else:
                efree[eng] = st + est
                fin[i] = st + est
            order[eng].append(i)
            glob.append(i)
            for k2 in succ[i]:
                indeg[k2] -= 1
                if ready_t[k2] < fin[i] + self.SEM_LAT:
                    ready_t[k2] = fin[i] + self.SEM_LAT
                if indeg[k2] == 0:
                    heapq.heappush(heap, (ready_t[k2], k2))
        assert len(glob) == n, "dependency cycle"
        tok = [None] * n
        for e in ENGS:
            if e == "sp":
                continue
        waits_raw = [None] * n
        cnt = dict(self.ecnt)
        for i in glob:
            eng, fn, reads, writes, dma, est = ops[i]
            w = {}
            for j in deps[i]:
                dj = ops[j][4]
                if dj is None:
                    s_, v_ = tok[j]
                else:
                    s_, v_ = dj.sem, dj.dcnt
                if w.get(s_, 0) < v_:
                    w[s_] = v_
            waits_raw[i] = w
            if dma is None:
                cnt[eng] += 1
                tok[i] = (self.esem[eng], cnt[eng])
            else:
                if dma.sem is None:
                    if self.dsem_pool:
                        dma.sem, dma.dcnt = self.dsem_pool.pop()
                    else:
                        dma.sem = self.newsem("d_" + dma.name)
                        dma.dcnt = 0
                    self.dbufs.append(dma)
                dma.dcnt += 16
                tok[i] = (dma.sem, dma.dcnt)
        self.ecnt = cnt
        nc = self.nc
        sems = self.sems
        qs = {}
        for e in ENGS:
            wd = self.waited[e]
            q = []
            for i in order[e]:
                ws = []
                for s_, v_ in waits_raw[i].items():
                    if wd.get(s_, 0) < v_:
                        ws.append((s_, v_))
                        wd[s_] = v_
                inc = (tok[i][0], 16 if ops[i][4] is not None else 1)
                q.append((ws, ops[i][1], inc))
            qs[e] = q
        toks = {}
        for e, s_ in self.esem.items():
            if self.ecnt[e] > 0:
                toks[s_] = self.ecnt[e]
        for b in self.dbufs:
            toks[b.sem] = max(toks.get(b.sem, 0), b.dcnt)
        for e in ENGS:
            wd = self.waited[e]
            ws = []
            for s_, v_ in toks.items():
                if wd.get(s_, 0) < v_:
                    ws.append((s_, v_))
                    wd[s_] = v_
            if ws:
                qs[e].append((ws, None, None))
        for b in self.phase_bufs:
            if b.sem is not None:
                self.dsem_pool.append((b.sem, b.dcnt))
                self.dbufs.remove(b)
                b.sem = None
        self.phase_bufs = []

        def mk(e):
            def f(eng):
                for waits, fn, inc in qs[e]:
                    for s_, v_ in waits:
                        eng.wait_ge(sems[s_], v_)
                    if fn is None:
                        continue
                    ins = fn(eng)
                    ins.then_inc(sems[inc[0]], inc[1])
            return f

        with nc.Block() as block:
            block.sync(mk("sp"))
            block.scalar(mk("act"))
            block.vector(mk("dve"))
            block.gpsimd(mk("pool"))
            block.tensor(mk("pe"))
        self.last_makespan = max(efree.values()) if n else 0.0
        if getattr(self, "verbose", False):
            busy = {e: 0.0 for e in ENGS}
            for i in range(n):
                if ops[i][4] is None:
                    busy[ops[i][0]] += ops[i][5]
            print(f"[phase] n_ops={n} model_makespan={self.last_makespan:.0f}us busy=" +
                  " ".join(f"{e}:{busy[e]:.0f}" for e in ENGS), flush=True)


def _fsz(ap):
    n = 1
    for d in ap.shape[1:]:
        n *= int(d)
    return n


def _est(eng, ap, psum=False):
    n = _fsz(ap)
    if eng == "dve":
        return (60 + n) / 960.0 + (0.06 if psum else 0.0)
    if eng == "act":
        return (220 + n) / 1400.0
    if eng == "pool":
        return (120 + n) / 900.0
    return 0.5


class H:
    def __init__(self, P):
        self.P = P

    def dma(self, eng, out, in_, R, W, buf, slow=False):
        nbytes = _fsz(out) * 128 * 4
        est = 2.0 + nbytes / 150000.0
        if slow:
            self.P.op(eng, lambda e: e.dma_start(out=out, in_=in_, allow_slow_non_contiguous=True), reads=R, writes=W, dma=buf, est=est)
        else:
            self.P.op(eng, lambda e: e.dma_start(out=out, in_=in_), reads=R, writes=W, dma=buf, est=est)

    def tt(self, eng, out, in0, in1, op, R, W):
        self.P.op(eng, lambda e: e.tensor_tensor(out=out, in0=in0, in1=in1, op=op), reads=R, writes=W, est=_est(eng, out))

    def ts(self, eng, out, in0, s1, s2, op0, op1, R, W):
        if s2 is None:
            self.P.op(eng, lambda e: e.tensor_scalar(out=out, in0=in0, scalar1=s1, scalar2=None, op0=op0), reads=R, writes=W, est=_est(eng, out))
        else:
            self.P.op(eng, lambda e: e.tensor_scalar(out=out, in0=in0, scalar1=s1, scalar2=s2, op0=op0, op1=op1), reads=R, writes=W, est=_est(eng, out))

    def stt(self, eng, out, in0, sc, in1, op0, op1, R, W):
        eng = "dve"
        self.P.op(eng, lambda e: e.scalar_tensor_tensor(out=out, in0=in0, scalar=sc, in1=in1, op0=op0, op1=op1), reads=R, writes=W, est=_est(eng, out))

    def act(self, out, in_, func, R, W, bias=None, scale=None, accum=None):
        kw = {}
        if bias is not None:
            kw["bias"] = bias
        if scale is not None:
            kw["scale"] = scale
        if accum is not None:
            kw["accum_out"] = accum
        self.P.op("act", lambda e: e.activation(out=out, in_=in_, func=func, **kw), reads=R, writes=W, est=_est("act", out))

    def cp(self, eng, out, in_, R, W):
        if eng == "act":
            self.P.op("act", lambda e: e.copy(out=out, in_=in_), reads=R, writes=W, est=_est("act", out))
        else:
            self.P.op(eng, lambda e: e.tensor_copy(out=out, in_=in_), reads=R, writes=W, est=_est(eng, out))

    def memset(self, eng, ap, val, W):
        self.P.op(eng, lambda e: e.memset(ap, val), writes=W, est=_est(eng, ap))

    def recip(self, out, in_, R, W):
        self.P.op("dve", lambda e: e.reciprocal(out=out, in_=in_), reads=R, writes=W, est=_est("dve", out))

    def reduce(self, out, in_, op, R, W):
        self.P.op("dve", lambda e: e.tensor_reduce(out=out, in_=in_, axis=AX.X, op=op), reads=R, writes=W, est=_est("dve", in_))

    def mm(self, items, R, W):
        est = 0.0
        for (o, l, rh, st, sp) in items:
            est += (max(64, _fsz(rh)) * (4 if l.dtype == F32 else 1)) / 2400.0 + 0.01
        est += 0.06

        def f(e):
            r = None
            for (o, l, rh, st, sp) in items:
                r = e.matmul(o, lhsT=l, rhs=rh, start=st, stop=sp)
            return r
        self.P.op("pe", f, reads=R, writes=W, est=est)

    def tr(self, items, ident, R, W):
        est = 0.06
        for (o, i) in items:
            est += (128 * (4 if i.dtype == F32 else 1)) / 2400.0 + 0.03

        def f(e):
            r = None
            for (o, i) in items:
                r = e.transpose(out=o, in_=i, identity=ident)
            return r
        self.P.op("pe", f, reads=R, writes=W, est=est)

    def select(self, out, in_, cmp, fill, base, cm, pattern, R, W):
        self.P.op("pool", lambda e: e.affine_select(out=out, in_=in_, pattern=pattern, compare_op=cmp, fill=fill,
                                                    base=base, channel_multiplier=cm), reads=R, writes=W, est=_est("pool", out))


class Rot:
    def __init__(self, P, es, name, shape, dtype, n=2):
        self.bufs = [P.sb(es, f"{name}r{i}", shape, dtype) for i in range(n)]

    def at(self, i):
        return self.bufs[i % len(self.bufs)]


def b3(ap, n, m):
    return ap.unsqueeze(1).to_broadcast([128, n, m])


def s3(ap, n, m):
    return ap.unsqueeze(2).to_broadcast([128, n, m])


def s4(ap):
    return ap.rearrange("p (b h) -> p b h", b=2).unsqueeze(3).to_broadcast([128, 2, 3, 128])


def v4(ap):
    return ap.rearrange("p (b h) l -> p b h l", b=2)


def w4(ps):
    return ps[:, :, 0:384].rearrange("p b (h l) -> p b h l", h=3)


def softplus12(h, P, es, tagp):
    xa = P.sb(es, tagp + "xa", [128, 12], F32)
    ax = P.sb(es, tagp + "ax", [128, 12], F32)
    ex = P.sb(es, tagp + "ex", [128, 12], F32)
    ln = P.sb(es, tagp + "ln", [128, 12], F32)
    one = P.sb(es, tagp + "one", [128, 1], F32)
    h.memset("pool", one[:], 1.0, [one])

    def f(xin, xin_buf, bias, out):
        h.tt("dve", xa[:], xin, bias[:], ALU.add, [xin_buf, bias], [xa])
        h.act(ax[:], xa[:], AF.Abs, [xa], [ax])
        h.act(ex[:], ax[:], AF.Exp, [ax], [ex], scale=-1.0)
        h.act(ln[:], ex[:], AF.Ln, [ex, one], [ln], bias=one[:, 0:1])
        h.ts("dve", xa[:], xa[:], 0.0, None, ALU.max, None, [xa], [xa])
        h.tt("dve", out[:], xa[:], ln[:], ALU.add, [xa, ln], [out])
    return f


def make_masks(h, P, es):
    m = {}
    ones = P.sb(es, "m_ones", [128, 128], F32)
    h.memset("pool", ones[:], 1.0, [ones])
    m["ones"] = ones
    for name, cmp, cm, st in (("U", ALU.is_ge, -1, 1), ("L", ALU.is_ge, 1, -1), ("Ls", ALU.is_gt, 1, -1)):
        t = P.sb(es, "m_" + name, [128, 128], F32)
        h.select(t[:], ones[:], cmp, 0.0, 0, cm, [[st, 128]], [ones], [t])
        m[name] = t
    sel = P.sb(es, "m_sel", [128, 128], F32)
    zer = P.sb(es, "m_zero", [128, 128], F32)
    h.memset("pool", zer[:], 0.0, [zer])
    h.select(sel[:], zer[:], ALU.not_equal, 1.0, -127, 1, [[0, 128]], [zer], [sel])
    m["sel"] = sel
    bd = P.sb(es, "m_bd", [128, 128], F32)
    h.memset("pool", bd[:], 0.0, [bd])
    h.memset("pool", bd[0:64, 0:64], 1.0, [bd])
    h.memset("pool", bd[64:128, 64:128], 1.0, [bd])
    m["bd"] = bd
    return m


def mixer_layer(k, l, cur, dstt):
    P, T, flags, h = k.P, k.T, k.flags, k.h
    inp = k.inp
    src, rsrc = cur
    dst, rdst = dstt
    NMT = T // 512
    MT = 512
    identf, identb = k.identf, k.identb
    u_d, qn_d, kn_d, v_d, xs_d, B_d, C_d, pt_d, y_d = k.u_d, k.qn_d, k.kn_d, k.v_d, k.xs_d, k.B_d, k.C_d, k.pt_d, k.y_d
    r_pre, r_y = k.r_pre, k.r_y

    with contextlib.ExitStack() as es:
        A, B = k.norm_consts(es, l, inp["norm_mix"].t[l:l + 1, :], 1, 0, "m")
        winf = P.sb(es, "winf", [128, 8, 2304], BF16)
        wint = P.sb(es, "wint", [128, 8, 786], BF16)
        for (c0, c1) in ((0, 1152), (1152, 2304)):
            h.dma("pool", winf[:, :, c0:c1], inp["w_in_f"].t[l, :, c0:c1].rearrange("(k p) n -> p k n", p=128), [], [winf], winf)
        h.dma("pool", wint[:], inp["w_in_t"].t[l].rearrange("(k p) n -> p k n", p=128), [], [wint], wint)
        cwt = P.sb(es, "cwt", [128, 16, 4], F32)
        cbt = P.sb(es, "cbt", [128, 16], F32)
        h.dma("sp", cwt[:], inp["conv_w"].t[l], [], [cwt], cwt)
        h.dma("sp", cbt[:], inp["conv_b"].t[l], [], [cbt], cbt)
        carry = P.sb(es, "carry", [128, 16, 3], F32)
        h.memset("pool", carry[:], 0.0, [carry])
        epsb = P.sb(es, "epsb", [128, 1], F32)
        h.memset("pool", epsb[:], EPS, [epsb])
        mhalf = P.sb(es, "mhalf", [128, MT], F32)
        h.memset("pool", mhalf[:], -0.5, [mhalf])
        bones = P.sb(es, "bones", [128, 128], F32)
        h.memset("pool", bones[:], 0.0, [bones])
        h.memset("pool", bones[0:64, 0:64], 1.0, [bones])
        h.memset("pool", bones[64:128, 64:128], 1.0, [bones])
        xt = [P.sb(es, f"m1x{i}", [128, D], F32) for i in range(2)]
        tmp = P.sb(es, "m1tmp", [128, D], F32)
        hb = P.sb(es, "m1hb", [128, D], BF16)
        hT = P.sb(es, "m1hT", [128, 8, MT], BF16)
        cin = [P.sb(es, f"cin{i}", [128, MT + 3], F32) for i in range(2)]
        acc = [P.sb(es, f"acc{i}", [128, MT], F32) for i in range(2)]
        so = [P.sb(es, f"so{i}", [128, MT], F32) for i in range(2)]
        sob = [P.sb(es, f"sob{i}", [128, MT], BF16) for i in range(2)]
        sq = P.sb(es, "m1sq", [128, MT], F32)
        rinv = P.sb(es, "m1rinv", [128, MT], F32)
        ptst = [P.sb(es, f"ptst{i}", [128, 786], F32) for i in range(2)]
        ssq = P.sb(es, "m1ssq", [128, 1], F32)
        rstd = P.sb(es, "m1rstd", [128, 1], F32)
        ptr = P.ps(es, "m1ptr", [128, 8, 128], BF16)
        pp = [P.ps(es, f"m1pp{i}", [128, MT], F32) for i in range(2)]
        pt = P.ps(es, "m1pt", [128, 2, 512], F32)
        pq = P.ps(es, "m1pq", [128, MT], F32)
        for mt in range(NMT):
            cols = slice(mt * MT, (mt + 1) * MT)
            for ti in range(4):
                t = mt * 4 + ti
                xb = xt[t % 2]
                h.dma("sp", xb[:], src.t[t * 128:(t + 1) * 128, :], [rsrc[t]], [xb], xb)
                k.rms_mod(xb, A, B, hb, ssq, rstd, tmp, tmp)
                h.tr([(ptr[:, kk, :], hb[:, kk * 128:(kk + 1) * 128]) for kk in range(8)], identb[:], [hb, identb], [ptr])
                h.cp("act", hT[:, :, ti * 128:(ti + 1) * 128], ptr[:], [ptr], [hT])
            for c in range(18):
                pb = pp[c % 2]
                h.mm([(pb[:], winf[:, kk, c * 128:(c + 1) * 128], hT[:, kk, :], kk == 0, kk == 7) for kk in range(8)], [winf, hT], [pb])
                if c < 2:
                    sb_ = so[c % 2]
                    h.cp("act", sb_[:], pb[:], [pb], [sb_])
                    h.dma("sp", u_d.t[c * 128:(c + 1) * 128, cols], sb_[:], [sb_], [r_pre[mt]], sb_)
                    continue
                ci = c - 2
                cb_ = cin[ci % 2]
                h.cp("pool", cb_[:, 0:3], carry[:, ci, :], [carry], [cb_])
                h.cp("act", cb_[:, 3:MT + 3], pb[:], [pb], [cb_])
                h.cp("pool", carry[:, ci, :], cb_[:, MT:MT + 3], [cb_], [carry])
                ab = acc[ci % 2]
                h.ts("dve", ab[:], cb_[:, 0:MT], cwt[:, ci, 0:1], None, ALU.mult, None, [cb_, cwt], [ab])
                for j in range(1, 4):
                    h.stt("dve", ab[:], cb_[:, j:j + MT], cwt[:, ci, j:j + 1], ab[:], ALU.mult, ALU.add, [cb_, cwt, ab], [ab])
                sb_ = so[ci % 2]
                h.act(sb_[:], ab[:], AF.Silu, [ab, cbt], [sb_], bias=cbt[:, ci:ci + 1])
                if ci < 6:
                    h.tt("pool", sq[:], sb_[:], sb_[:], ALU.mult, [sb_], [sq])
                    h.mm([(pq[:], bones[:], sq[:], True, True)], [bones, sq], [pq])
                    h.act(rinv[:], pq[:], AF.Ln, [pq, epsb], [rinv], bias=epsb[:, 0:1])
                    h.act(rinv[:], rinv[:], AF.Exp, [rinv], [rinv], scale=-0.5)
                    ob = sob[ci % 2]
                    if ci < 3:
                        h.stt("dve", ob[:], sb_[:], 0.125, rinv[:], ALU.mult, ALU.mult, [sb_, rinv], [ob])
                    else:
                        h.tt("dve", ob[:], sb_[:], rinv[:], ALU.mult, [sb_, rinv], [ob])
                    dd = qn_d if ci < 3 else kn_d
                    j = ci % 3
                    h.dma("sp", dd.t[j * 128:(j + 1) * 128, cols], ob[:], [ob], [r_pre[mt]], ob)
                elif ci < 12:
                    dd = v_d if ci < 9 else xs_d
                    j = (ci - 6) % 3
                    h.dma("sp", dd.t[j * 128:(j + 1) * 128, cols], sb_[:], [sb_], [r_pre[mt]], sb_)
                else:
                    ob = sob[ci % 2]
                    h.cp("pool", ob[:], sb_[:], [sb_], [ob])
                    dd = B_d if ci < 14 else C_d
                    j = (ci - 12) % 2
                    h.dma("sp", dd.t[j * 128:(j + 1) * 128, cols], ob[:], [ob], [r_pre[mt]], ob)
            for ti in range(4):
                t = mt * 4 + ti
                tc = slice(ti * 128, (ti + 1) * 128)
                h.mm([(pt[:, 0, :], hT[:, kk, tc], wint[:, kk, 0:512], kk == 0, kk == 7) for kk in range(8)]
                     + [(pt[:, 1, 0:274], hT[:, kk, tc], wint[:, kk, 512:786], kk == 0, kk == 7) for kk in range(8)],
                     [hT, wint], [pt])
                stg = ptst[t % 2]
                h.cp("act", stg[:, 0:512], pt[:, 0, :], [pt], [stg])
                h.cp("dve", stg[:, 512:786], pt[:, 1, 0:274], [pt], [stg])
                h.dma("sp", pt_d.t[t * 128:(t + 1) * 128, :], stg[:], [stg], [r_pre[mt]], stg)
        P.end_phase()

    if flags.get("s5", True):
        s5_phase(k, l)
    if flags.get("gdn", True):
        gdn_phase(k, l)
    if flags.get("ssd", True):
        ssd_phase(k, l)

    with contextlib.ExitStack() as es:
        wout = P.sb(es, "wout", [128, 8, D], BF16)
        h.dma("pool", wout[:], inp["w_out"].t[l].rearrange("(k p) n -> p k n", p=128), [], [wout], wout)
        G = P.sb(es, "Gm", [128, D], F32)
        k.load_row(G, k.modv.t[l:l + 1, 2 * D:3 * D], [k.r_modv])
        yT = [P.sb(es, f"m5y{i}", [128, 8, MT], BF16) for i in range(2)]
        xt = [P.sb(es, f"m5x{i}", [128, D], F32) for i in range(2)]
        tm = [P.sb(es, f"m5t{i}", [128, D], F32) for i in range(2)]
        po = [P.ps(es, f"m5p{i}", [128, 512], F32) for i in range(4)]
        for mt in range(NMT):
            cols = slice(mt * MT, (mt + 1) * MT)
            yb = yT[mt % 2]
            h.dma("sp", yb[:], y_d.t[:, cols].rearrange("(k p) t -> p k t", p=128), [r_y[0][mt], r_y[1][mt], r_y[2][mt]], [yb], yb)
            if not flags.get("s5", True):
                h.memset("pool", yb[:, 0:2, :], 0.0, [yb])
            if not flags.get("gdn", True):
                h.memset("pool", yb[:, 2:5, :], 0.0, [yb])
            if not flags.get("ssd", True):
                h.memset("pool", yb[:, 5:8, :], 0.0, [yb])
            for ti in range(4):
                t = mt * 4 + ti
                tc = slice(ti * 128, (ti + 1) * 128)
                xb = xt[t % 2]
                tb = tm[t % 2]
                h.dma("sp", xb[:], src.t[t * 128:(t + 1) * 128, :], [rsrc[t]], [xb], xb)
                for n2 in range(2):
                    pb = po[(t % 2) * 2 + n2]
                    nc_ = slice(n2 * 512, (n2 + 1) * 512)
                    h.mm([(pb[:], yb[:, kk, tc], wout[:, kk, nc_], kk == 0, kk == 7) for kk in range(8)], [yb, wout], [pb])
                    h.tt("dve", tb[:, nc_], pb[:], G[:, nc_], ALU.mult, [pb, G], [tb])
                h.tt("pool", tb[:], tb[:], xb[:], ALU.add, [tb, xb], [tb])
                h.dma("sp", dst.t[t * 128:(t + 1) * 128, :], tb[:], [tb], [rdst[t]], tb)
        P.end_phase()


def gate_consts(k, es, l, tag):
    P, h, inp = k.P, k.h, k.inp
    b12 = P.sb(es, tag + "b12", [128, 12], F32)
    na12 = P.sb(es, tag + "na12", [128, 12], F32)
    k.load_row(b12, inp["bias12"].t[l:l + 1, :])
    k.load_row(na12, inp["alog12"].t[l:l + 1, :])
    h.act(na12[:], na12[:], AF.Exp, [na12], [na12])
    h.ts("dve", na12[:], na12[:], -1.0, None, ALU.mult, None, [na12], [na12])
    return b12, na12


def gate_tile(k, sp_fn, pj_ap, pj_buf, b12, na12, masks, sp12, gda, cs12, cl12, pA):
    h = k.h
    sp_fn(pj_ap, pj_buf, b12, sp12)
    h.tt("dve", gda[:], sp12[:], na12[:], ALU.mult, [sp12, na12], [gda])
    h.mm([(pA[:, 0:12], masks["U"][:], gda[:], True, True)], [masks["U"], gda], [pA])
    h.cp("dve", cs12[:], pA[:, 0:12], [pA], [cs12])
    h.mm([(pA[:, 16:28], masks["sel"][:], cs12[:], True, True)], [masks["sel"], cs12], [pA])
    h.cp("dve", cl12[:], pA[:, 16:28], [pA], [cl12])


def ssd_phase(k, l):
    P, T, h, inp = k.P, k.T, k.h, k.inp
    NMT = T // 512
    MT = 512
    identf, identb = k.identf, k.identb
    with contextlib.ExitStack() as es:
        masks = make_masks(h, P, es)
        b12, na12 = gate_consts(k, es, l, "sd")
        sp_fns = [softplus12(h, P, es, f"sd{i}") for i in range(2)]
        epsb = P.sb(es, "sd_eps", [128, 1], F32)
        h.memset("pool", epsb[:], EPS, [epsb])
        dsk = P.sb(es, "sd_dsk", [128, 6], F32)
        k.load_row(dsk, inp["ssd_d"].t[l:l + 1, :])
        nws = P.sb(es, "sd_nws", [128, 384], F32)
        k.load_row(nws, inp["ssd_norm"].t[l:l + 1, :])
        stT = P.sb(es, "sd_stT", [128, 384], F32)
        stTb = P.sb(es, "sd_stTb", [128, 384], BF16)
        h.memset("pool", stT[:], 0.0, [stT])
        h.memset("pool", stTb[:], 0.0, [stTb])
        xsT = [P.sb(es, f"sd_xsT{i}", [128, 3, MT], F32) for i in range(2)]
        BTt = [P.sb(es, f"sd_BT{i}", [128, 2, MT], BF16) for i in range(2)]
        CTt = [P.sb(es, f"sd_CT{i}", [128, 2, MT], BF16) for i in range(2)]
        pj = [P.sb(es, f"sd_pj{i}", [128, 4, 786], F32) for i in range(2)]
        yo = [P.sb(es, f"sd_yo{i}", [128, 3, MT], BF16) for i in range(2)]
        R_sp12 = Rot(P, es, "sd_sp12", [128, 12], F32)
        R_gda = Rot(P, es, "sd_gda", [128, 12], F32)
        R_cs12 = Rot(P, es, "sd_cs12", [128, 12], F32)
        R_cl12 = Rot(P, es, "sd_cl12", [128, 12], F32)
        R_t6 = Rot(P, es, "sd_t6", [128, 6], F32)
        R_din = Rot(P, es, "sd_din", [128, 6], F32)
        R_eacs = Rot(P, es, "sd_eacs", [128, 6], F32)
        R_cd = Rot(P, es, "sd_cd", [128, 6], F32)
        R_dg = Rot(P, es, "sd_dg", [128, 6, 128], F32)
        R_arg = Rot(P, es, "sd_arg", [128, 6, 128], F32)
        R_seg = Rot(P, es, "sd_seg", [128, 6, 128], F32)
        R_WTb = Rot(P, es, "sd_WTb", [128, 6, 128], BF16)
        R_xs_tm = Rot(P, es, "sd_xstm", [128, 384], F32)
        R_xdtf = Rot(P, es, "sd_xdtf", [128, 384], F32)
        R_xdtb = Rot(P, es, "sd_xdtb", [128, 384], BF16)
        R_xddb = Rot(P, es, "sd_xddb", [128, 384], BF16)
        R_Btm = Rot(P, es, "sd_Btm", [128, 256], BF16)
        R_t1 = Rot(P, es, "sd_t1", [128, 384], F32)
        R_t2 = Rot(P, es, "sd_t2", [128, 384], F32)
        R_y = Rot(P, es, "sd_y", [128, 384], F32)
        R_zs = Rot(P, es, "sd_zs", [128, 384], F32)
        R_junk = Rot(P, es, "sd_junk", [128, 192], F32)
        R_yb = Rot(P, es, "sd_yb", [128, 384], BF16)
        R_ss2 = Rot(P, es, "sd_ss2", [128, 2], F32)
        R_rs2 = Rot(P, es, "sd_rs2", [128, 2], F32)
        W0 = P.ps(es, "sd_W0", [128, 2, 512], F32)
        S0 = P.ps(es, "sd_S0", [128, 512], F32)
        S1 = P.ps(es, "sd_S1", [128, 512], F32)
        S2 = P.ps(es, "sd_S2", [128, 512], F32)
        PB = P.ps(es, "sd_PB", [128, 512], BF16)
        pA = P.ps(es, "sd_pA", [128, 32], F32)
        for mt in range(NMT):
            cols = slice(mt * MT, (mt + 1) * MT)
            i2 = mt % 2
            rp = [k.r_pre[mt]]
            h.dma("sp", xsT[i2][:], k.xs_d.t[:, cols].rearrange("(j p) t -> p j t", p=128), rp, [xsT[i2]], xsT[i2])
            h.dma("sp", BTt[i2][:], k.B_d.t[:, cols].rearrange("(j p) t -> p j t", p=128), rp, [BTt[i2]], BTt[i2])
            h.dma("sp", CTt[i2][:], k.C_d.t[:, cols].rearrange("(j p) t -> p j t", p=128), rp, [CTt[i2]], CTt[i2])
            h.dma("sp", pj[i2][:], k.pt_d.t[mt * MT:(mt + 1) * MT, :].rearrange("(a p) n -> p a n", p=128), rp, [pj[i2]], pj[i2])
            xs_, B_, C_, pj_, yo_ = xsT[i2], BTt[i2], CTt[i2], pj[i2], yo[i2]
            for ti in range(4):
                tc = slice(ti * 128, (ti + 1) * 128)
                tix = mt * 4 + ti
                sp12 = R_sp12.at(tix); gda = R_gda.at(tix); cs12 = R_cs12.at(tix); cl12 = R_cl12.at(tix); t6 = R_t6.at(tix); din = R_din.at(tix); eacs = R_eacs.at(tix); cd = R_cd.at(tix); dg = R_dg.at(tix); arg = R_arg.at(tix); seg = R_seg.at(tix); WTb = R_WTb.at(tix); xs_tm = R_xs_tm.at(tix); xdtf = R_xdtf.at(tix); xdtb = R_xdtb.at(tix); xddb = R_xddb.at(tix); Btm = R_Btm.at(tix); t1 = R_t1.at(tix); t2 = R_t2.at(tix); y = R_y.at(tix); zs = R_zs.at(tix); junk = R_junk.at(tix); yb = R_yb.at(tix); ss2 = R_ss2.at(tix); rs2 = R_rs2.at(tix)
                sp_fn = sp_fns[tix % 2]
                gate_tile(k, sp_fn, pj_[:, ti, 768:780], pj_, b12, na12, masks, sp12, gda, cs12, cl12, pA)
                acs = cs12[:, 6:12]
                h.tt("pool", dg[:], b3(identf[:], 6, 128), s3(acs, 6, 128), ALU.mult, [identf, cs12], [dg])
                h.mm([(W0[:, 0, 0:384], masks["ones"][:], dg[:, 0:3, :].rearrange("p a l -> p (a l)"), True, True),
                      (W0[:, 1, 0:384], masks["ones"][:], dg[:, 3:6, :].rearrange("p a l -> p (a l)"), True, True)],
                     [masks["ones"], dg], [W0])
                h.tt("dve", v4(arg[:]), w4(W0), s4(acs), ALU.subtract, [W0, cs12], [arg])
                h.ts("pool", arg[:], arg[:], 0.0, None, ALU.min, None, [arg], [arg])
                h.act(seg[:], arg[:], AF.Exp, [arg], [seg])
                h.tt("pool", seg[:], seg[:], b3(masks["U"][:], 6, 128), ALU.mult, [seg, masks["U"]], [seg])
                h.mm([(S0[:, g * 128:(g + 1) * 128], B_[:, g, tc], C_[:, g, tc], True, True) for g in range(2)], [B_, C_], [S0])
                h.tt("dve", v4(WTb[:]), v4(seg[:]),
                     S0[:, 0:256].rearrange("p (g l) -> p g l", g=2).unsqueeze(2).to_broadcast([128, 2, 3, 128]),
                     ALU.mult, [seg, S0], [WTb])
                h.tr([(S1[:, j * 128:(j + 1) * 128], xs_[:, j, tc]) for j in range(3)], identf[:], [xs_, identf], [S1])
                h.cp("act", xs_tm[:], S1[:, 0:384], [S1], [xs_tm])
                h.tr([(PB[:, g * 128:(g + 1) * 128], B_[:, g, tc]) for g in range(2)], identb[:], [B_, identb], [PB])
                h.cp("act", Btm[:], PB[:, 0:256], [PB], [Btm])
                x3 = lambda ap: ap.rearrange("p (h d) -> p h d", h=6)
                h.tt("dve", x3(xdtf[:]), x3(xs_tm[:]), s3(sp12[:, 6:12], 6, 64), ALU.mult, [xs_tm, sp12], [xdtf])
                h.cp("pool", xdtb[:], xdtf[:], [xdtf], [xdtb])
                h.tt("dve", t6[:], cl12[:, 6:12], acs, ALU.subtract, [cl12, cs12], [t6])
                h.act(din[:], t6[:], AF.Exp, [t6], [din])
                h.tt("pool", x3(xddb[:]), x3(xdtf[:]), s3(din[:], 6, 64), ALU.mult, [xdtf, din], [xddb])
                h.mm([(S2[:, hd * 64:(hd + 1) * 64], WTb[:, hd, :], xdtb[:, hd * 64:(hd + 1) * 64], True, True) for hd in range(6)],
                     [WTb, xdtb], [S2])
                h.mm([(S0[:, g * 192:(g + 1) * 192], C_[:, g, tc], stTb[:, g * 192:(g + 1) * 192], True, True) for g in range(2)],
                     [C_, stTb], [S0])
                h.act(eacs[:], acs, AF.Exp, [cs12], [eacs])
                h.tt("dve", x3(t2[:]), x3(S0[:, 0:384]), s3(eacs[:], 6, 64), ALU.mult, [S0, eacs], [t2])
                h.tt("pool", x3(t1[:]), x3(xs_tm[:]), s3(dsk[:], 6, 64), ALU.mult, [xs_tm, dsk], [t1])
                h.tt("pool", t2[:], t2[:], t1[:], ALU.add, [t2, t1], [t2])
                h.tt("dve", y[:], S2[:, 0:384], t2[:], ALU.add, [S2, t2], [y])
                h.act(zs[:], pj_[:, ti, 384:768], AF.Silu, [pj_], [zs])
                h.tt("pool", y[:], y[:], zs[:], ALU.mult, [y, zs], [y])
                for g in range(2):
                    h.act(junk[:], y[:, g * 192:(g + 1) * 192], AF.Square, [y], [junk, ss2], accum=ss2[:, g:g + 1])
                h.act(rs2[:], ss2[:], AF.Ln, [ss2, epsb], [rs2], bias=epsb[:, 0:1], scale=1.0 / 192)
                h.act(rs2[:], rs2[:], AF.Exp, [rs2], [rs2], scale=-0.5)
                y3 = lambda ap: ap.rearrange("p (g c) -> p g c", g=2)
                h.tt("dve", y3(y[:]), y3(y[:]), s3(rs2[:], 2, 192), ALU.mult, [y, rs2], [y])
                h.tt("pool", yb[:], y[:], nws[:], ALU.mult, [y, nws], [yb])
                h.tr([(PB[:, j * 128:(j + 1) * 128], yb[:, j * 128:(j + 1) * 128]) for j in range(3)], identb[:], [yb, identb], [PB])
                h.cp("act", yo_[:, :, tc], PB[:, 0:384].rearrange("p (j t) -> p j t", j=3), [PB], [yo_])
                h.mm([(S1[:, g * 192:(g + 1) * 192], Btm[:, g * 128:(g + 1) * 128], xddb[:, g * 192:(g + 1) * 192], True, True) for g in range(2)],
                     [Btm, xddb], [S1])
                h.act(cd[:], cl12[:, 6:12], AF.Exp, [cl12], [cd])
                h.tt("pool", x3(stT[:]), x3(stT[:]), s3(cd[:], 6, 64), ALU.mult, [stT, cd], [stT])
                h.tt("dve", stT[:], stT[:], S1[:, 0:384], ALU.add, [stT, S1], [stT])
                h.cp("act", stTb[:], stT[:], [stT], [stTb])
            h.dma("sp", k.y_d.t[640:1024, cols].rearrange("(j p) t -> p j t", p=128), yo_[:], [yo_], [k.r_y[2][mt]], yo_)
        P.end_phase()


C1_2PI = 6.28125
C2_2PI = 2.0 * math.pi - 6.28125


def sincos(h, eng, x, out, b, ki, c, negpi, R, W, bufs, is_cos):
    bb, kb, cb_ = bufs
    off = 16.5 + (0.25 if is_cos else 0.0)
    add = 33.0 * math.pi + (0.5 * math.pi if is_cos else 0.0)
    h.ts(eng, b, x, 1.0 / (2.0 * math.pi), off, ALU.mult, ALU.add, R, [bb])
    h.cp(eng, ki, b, [bb], [kb])
    h.cp(eng, c, ki, [kb], [cb_])
    h.stt(eng, b, c, -C1_2PI, x, ALU.mult, ALU.add, R + [cb_], [bb])
    h.stt(eng, b, c, -C2_2PI, b, ALU.mult, ALU.add, [cb_, bb], [bb])
    h.ts(eng, b, b, add, None, ALU.add, None, [bb], [bb])
    h.ts(eng, c, b, 2.0 * math.pi, -2.0 * math.pi, ALU.is_gt, ALU.mult, [bb], [cb_])
    h.tt(eng, b, b, c, ALU.add, [bb, cb_], [bb])
    h.ts(eng, c, b, 0.0, 2.0 * math.pi, ALU.is_lt, ALU.mult, [bb], [cb_])
    h.tt(eng, b, b, c, ALU.add, [bb, cb_], [bb])
    h.act(out, b, AF.Sin, [bb, negpi], W, bias=negpi[:, 0:1])


def s5_phase(k, l):
    P, T, h, inp = k.P, k.T, k.h, k.inp
    NMT = T // 512
    SEG = 512
    with contextlib.ExitStack() as es:
        I32 = mybir.dt.int32
        are = P.sb(es, "s5are", [128, 8], F32)
        aim = P.sb(es, "s5aim", [128, 8], F32)
        stp = P.sb(es, "s5stp", [128, 8], F32)
        h.dma("sp", are[:], inp["s5_are"].t[l], [], [are], are)
        h.dma("sp", aim[:], inp["s5_aim"].t[l], [], [aim], aim)
        h.dma("sp", stp[:], inp["s5_ldt"].t[l], [], [stp], stp)
        dsk = P.sb(es, "s5dsk", [128, 2], F32)
        nw5 = P.sb(es, "s5nw", [128, 2], F32)
        h.dma("sp", dsk[:], inp["s5_dcol"].t[l], [], [dsk], dsk)
        h.dma("sp", nw5[:], inp["s5_ncol"].t[l], [], [nw5], nw5)
        bTre = P.sb(es, "s5bTre", [128, 8, 128], BF16)
        bTim = P.sb(es, "s5bTim", [128, 8, 128], BF16)
        cTre = P.sb(es, "s5cTre", [128, 8, 128], BF16)
        cTim = P.sb(es, "s5cTim", [128, 8, 128], BF16)
        for dstb, nm in ((bTre, "s5_bT_re"), (bTim, "s5_bT_im"), (cTre, "s5_cT_re"), (cTim, "s5_cT_im")):
            h.dma("pool", dstb[:], inp[nm].t[l].rearrange("s r m -> r s m"), [], [dstb], dstb)
        wglu = P.sb(es, "s5wglu", [128, 2, 256], BF16)
        h.dma("pool", wglu[:], inp["s5_w_glu"].t[l].rearrange("(k p) n -> p k n", p=128), [], [wglu], wglu)
        negpi = P.sb(es, "s5negpi", [128, 1], F32)
        h.memset("pool", negpi[:], -math.pi, [negpi])
        epsb = P.sb(es, "s5eps", [128, 1], F32)
        h.memset("pool", epsb[:], EPS, [epsb])
        onesf = P.sb(es, "s5ones", [128, 128], F32)
        h.memset("pool", onesf[:], 1.0, [onesf])
        jrow = P.sb(es, "s5jrow", [128, SEG], F32)
        P.op("pool", lambda e: e.iota(jrow[:], pattern=[[1, SEG]], base=0, channel_multiplier=0,
                                      allow_small_or_imprecise_dtypes=True), writes=[jrow])
        th = P.sb(es, "s5th", [128, 8], F32)
        rr = P.sb(es, "s5r", [128, 8], F32)
        sth = P.sb(es, "s5sth", [128, 8], F32)
        cth = P.sb(es, "s5cth", [128, 8], F32)
        thS = P.sb(es, "s5thS", [128, 8], F32)
        sS = P.sb(es, "s5sS", [128, 8], F32)
        cS = P.sb(es, "s5cS", [128, 8], F32)
        nsS = P.sb(es, "s5nsS", [128, 8], F32)
        cr = P.sb(es, "s5cr", [128, 8], F32)
        ci = P.sb(es, "s5ci", [128, 8], F32)
        ncr = P.sb(es, "s5ncr", [128, 8], F32)
        q1 = P.sb(es, "s5q1", [128, 8], F32)
        q2 = P.sb(es, "s5q2", [128, 8], F32)
        q3 = P.sb(es, "s5q3", [128, 8], F32)
        sb8 = P.sb(es, "s5sb8", [128, 8], F32)
        si8 = P.sb(es, "s5si8", [128, 8], I32)
        sc8 = P.sb(es, "s5sc8", [128, 8], F32)
        h.act(stp[:], stp[:], AF.Exp, [stp], [stp])
        h.tt("dve", th[:], aim[:], stp[:], ALU.mult, [aim, stp], [th])
        h.tt("dve", rr[:], are[:], stp[:], ALU.mult, [are, stp], [rr])
        h.act(rr[:], rr[:], AF.Exp, [rr], [rr])
        sm = (sb8, si8, sc8)
        sincos(h, "dve", th[:], sth[:], sb8[:], si8[:], sc8[:], negpi, [th], [sth], sm, False)
        sincos(h, "dve", th[:], cth[:], sb8[:], si8[:], sc8[:], negpi, [th], [cth], sm, True)
        h.ts("dve", thS[:], th[:], float(SEG), None, ALU.mult, None, [th], [thS])
        sincos(h, "dve", thS[:], sS[:], sb8[:], si8[:], sc8[:], negpi, [thS], [sS], sm, False)
        sincos(h, "dve", thS[:], cS[:], sb8[:], si8[:], sc8[:], negpi, [thS], [cS], sm, True)
        h.ts("dve", nsS[:], sS[:], -1.0, None, ALU.mult, None, [sS], [nsS])
        h.tt("dve", q1[:], rr[:], cth[:], ALU.mult, [rr, cth], [q1])
        h.ts("dve", q1[:], q1[:], -1.0, None, ALU.add, None, [q1], [q1])
        h.tt("dve", q2[:], rr[:], sth[:], ALU.mult, [rr, sth], [q2])
        h.tt("dve", q3[:], are[:], are[:], ALU.mult, [are], [q3])
        h.tt("dve", sc8[:], aim[:], aim[:], ALU.mult, [aim], [sc8])
        h.tt("dve", q3[:], q3[:], sc8[:], ALU.add, [q3, sc8], [q3])
        h.recip(q3[:], q3[:], [q3], [q3])
        h.tt("dve", cr[:], q1[:], are[:], ALU.mult, [q1, are], [cr])
        h.tt("dve", sc8[:], q2[:], aim[:], ALU.mult, [q2, aim], [sc8])
        h.tt("dve", cr[:], cr[:], sc8[:], ALU.add, [cr, sc8], [cr])
        h.tt("dve", cr[:], cr[:], q3[:], ALU.mult, [cr, q3], [cr])
        h.tt("dve", ci[:], q2[:], are[:], ALU.mult, [q2, are], [ci])
        h.tt("dve", sc8[:], q1[:], aim[:], ALU.mult, [q1, aim], [sc8])
        h.tt("dve", ci[:], ci[:], sc8[:], ALU.subtract, [ci, sc8], [ci])
        h.tt("dve", ci[:], ci[:], q3[:], ALU.mult, [ci, q3], [ci])
        h.ts("dve", ncr[:], cr[:], -1.0, None, ALU.mult, None, [cr], [ncr])
        cosT = P.sb(es, "s5cosT", [128, 8, SEG], F32)
        sinT = P.sb(es, "s5sinT", [128, 8, SEG], F32)
        tabr = P.sb(es, "s5tabr", [128, 8, SEG], F32)
        tabi = P.sb(es, "s5tabi", [128, 8, SEG], F32)
        ang = [P.sb(es, f"s5ang{i}", [128, SEG], F32) for i in range(2)]
        tb = [P.sb(es, f"s5tb{i}", [128, SEG], F32) for i in range(2)]
        tki = [P.sb(es, f"s5tki{i}", [128, SEG], I32) for i in range(2)]
        tcc = [P.sb(es, f"s5tc{i}", [128, SEG], F32) for i in range(2)]
        for sc in range(8):
            i = sc % 2
            eng = "dve" if i == 0 else "pool"
            h.ts(eng, ang[i][:], jrow[:], th[:, sc:sc + 1], None, ALU.mult, None, [jrow, th], [ang[i]])
            bufs = (tb[i], tki[i], tcc[i])
            sincos(h, eng, ang[i][:], sinT[:, sc, :], tb[i][:], tki[i][:], tcc[i][:], negpi, [ang[i]], [sinT], bufs, False)
            sincos(h, eng, ang[i][:], cosT[:, sc, :], tb[i][:], tki[i][:], tcc[i][:], negpi, [ang[i]], [cosT], bufs, True)
            h.ts(eng, tabr[:, sc, :], cosT[:, sc, :], cr[:, sc:sc + 1], None, ALU.mult, None, [cosT, cr], [tabr])
            h.stt(eng, tabr[:, sc, :], sinT[:, sc, :], ci[:, sc:sc + 1], tabr[:, sc, :], ALU.mult, ALU.add, [sinT, ci, tabr], [tabr])
            h.ts(eng, tabi[:, sc, :], cosT[:, sc, :], ci[:, sc:sc + 1], None, ALU.mult, None, [cosT, ci], [tabi])
            h.stt(eng, tabi[:, sc, :], sinT[:, sc, :], ncr[:, sc:sc + 1], tabi[:, sc, :], ALU.mult, ALU.add, [sinT, ncr, tabi], [tabi])
        ire = P.sb(es, "s5ire", [128, 8], F32)
        iim = P.sb(es, "s5iim", [128, 8], F32)
        gre_e = P.sb(es, "s5gree", [128, 8], F32)
        gim_e = P.sb(es, "s5gime", [128, 8], F32)
        h.memset("pool", ire[:], 0.0, [ire])
        h.memset("pool", iim[:], 0.0, [iim])
        uTf = [P.sb(es, f"s5uTf{i}", [128, 2, SEG], F32) for i in range(2)]
        uTb = [P.sb(es, f"s5uTb{i}", [128, 2, SEG], BF16) for i in range(2)]
        m1 = [P.sb(es, f"s5m1{i}", [128, SEG], F32) for i in range(2)]
        m2 = [P.sb(es, f"s5m2{i}", [128, SEG], F32) for i in range(2)]
        m3 = [P.sb(es, f"s5m3{i}", [128, SEG], F32) for i in range(2)]
        m4 = [P.sb(es, f"s5m4{i}", [128, SEG], F32) for i in range(2)]
        p1 = [P.sb(es, f"s5p1{i}", [128, SEG], BF16) for i in range(2)]
        p2 = [P.sb(es, f"s5p2{i}", [128, SEG], BF16) for i in range(2)]
        p3 = [P.sb(es, f"s5p3{i}", [128, SEG], BF16) for i in range(2)]
        p4 = [P.sb(es, f"s5p4{i}", [128, SEG], BF16) for i in range(2)]
        ncTre = P.sb(es, "s5ncTre", [128, 8, 128], BF16)
        ncTim = P.sb(es, "s5ncTim", [128, 8, 128], BF16)
        h.ts("pool", ncTre[:], cTre[:], -1.0, None, ALU.mult, None, [cTre], [ncTre])
        h.ts("pool", ncTim[:], cTim[:], -1.0, None, ALU.mult, None, [cTim], [ncTim])
        dre = [P.sb(es, f"s5dre{i}", [128, SEG], F32) for i in range(2)]
        dim = [P.sb(es, f"s5dim{i}", [128, SEG], F32) for i in range(2)]
        gre = [P.sb(es, f"s5gre{i}", [128, SEG], F32) for i in range(2)]
        gim = [P.sb(es, f"s5gim{i}", [128, SEG], F32) for i in range(2)]
        hre = [P.sb(es, f"s5hre{i}", [128, SEG], BF16) for i in range(2)]
        him = [P.sb(es, f"s5him{i}", [128, SEG], BF16) for i in range(2)]
        y1 = P.sb(es, "s5y1", [128, 2, SEG], F32)
        yt = P.sb(es, "s5yt", [128, 2, SEG], F32)
        yg = P.sb(es, "s5yg", [128, 2, SEG], F32)
        ygb = P.sb(es, "s5ygb", [128, 2, SEG], BF16)
        sg = P.sb(es, "s5sg", [128, 2, SEG], F32)
        y2 = P.sb(es, "s5y2", [128, 2, SEG], F32)
        rstd = P.sb(es, "s5rstd", [128, SEG], F32)
        yo = [P.sb(es, f"s5yo{i}", [128, 2, SEG], BF16) for i in range(2)]
        Pre = [P.ps(es, f"s5Pre{i}", [128, SEG], F32) for i in range(2)]
        Pim = [P.ps(es, f"s5Pim{i}", [128, SEG], F32) for i in range(2)]
        Y = [P.ps(es, f"s5Y{i}", [128, SEG], F32) for i in range(2)]
        Pg = P.ps(es, "s5Pg", [128, SEG], F32)
        Pt = P.ps(es, "s5Pt", [128, SEG], F32)
        GK = 2.0 * math.sqrt(2.0 / math.pi)
        for mt in range(NMT):
            cols = slice(mt * SEG, (mt + 1) * SEG)
            i2 = mt % 2
            uf, ub, yo_ = uTf[i2], uTb[i2], yo[i2]
            h.dma("sp", uf[:], k.u_d.t[:, cols].rearrange("(j p) t -> p j t", p=128), [k.r_pre[mt]], [uf], uf)
            h.cp("pool", ub[:], uf[:], [uf], [ub])
            for sc in range(8):
                i = sc % 2
                cc = sc // 4
                h.mm([(Pre[i][:], bTre[:, sc, :], ub[:, cc, :], True, True)], [bTre, ub], [Pre[i]])
                h.mm([(Pim[i][:], bTim[:, sc, :], ub[:, cc, :], True, True)], [bTim, ub], [Pim[i]])
                h.tt("dve", m1[i][:], Pre[i][:], tabr[:, sc, :], ALU.mult, [Pre[i], tabr], [m1[i]])
                h.tt("dve", m2[i][:], Pim[i][:], tabi[:, sc, :], ALU.mult, [Pim[i], tabi], [m2[i]])
                h.tt("pool", dre[i][:], m1[i][:], m2[i][:], ALU.subtract, [m1[i], m2[i]], [dre[i]])
                h.tt("dve", m3[i][:], Pre[i][:], tabi[:, sc, :], ALU.mult, [Pre[i], tabi], [m3[i]])
                h.tt("dve", m4[i][:], Pim[i][:], tabr[:, sc, :], ALU.mult, [Pim[i], tabr], [m4[i]])
                h.tt("pool", dim[i][:], m3[i][:], m4[i][:], ALU.add, [m3[i], m4[i]], [dim[i]])
                for (go, di, ini) in ((gre[i], dre[i], ire), (gim[i], dim[i], iim)):
                    P.op("dve", (lambda go, di, ini, sc: (lambda e: e.tensor_tensor_scan(
                        out=go[:], data0=rr[:, sc:sc + 1].to_broadcast([128, SEG]), data1=di[:],
                        initial=ini[:, sc:sc + 1], op0=ALU.mult, op1=ALU.add)))(go, di, ini, sc),
                        reads=[rr, di, ini], writes=[go])
                h.cp("act", gre_e[:, sc:sc + 1], gre[i][:, SEG - 1:SEG], [gre[i]], [gre_e])
                h.cp("act", gim_e[:, sc:sc + 1], gim[i][:, SEG - 1:SEG], [gim[i]], [gim_e])
                h.tt("pool", p1[i][:], gre[i][:], cosT[:, sc, :], ALU.mult, [gre[i], cosT], [p1[i]])
                h.tt("pool", p2[i][:], gim[i][:], sinT[:, sc, :], ALU.mult, [gim[i], sinT], [p2[i]])
                h.tt("dve", p3[i][:], gre[i][:], sinT[:, sc, :], ALU.mult, [gre[i], sinT], [p3[i]])
                h.tt("dve", p4[i][:], gim[i][:], cosT[:, sc, :], ALU.mult, [gim[i], cosT], [p4[i]])
                h.mm([(Y[cc][:], cTre[:, sc, :], p1[i][:], sc % 4 == 0, False),
                      (Y[cc][:], ncTre[:, sc, :], p2[i][:], False, False),
                      (Y[cc][:], ncTim[:, sc, :], p3[i][:], False, False),
                      (Y[cc][:], ncTim[:, sc, :], p4[i][:], False, sc % 4 == 3)],
                     [cTre, ncTre, ncTim, p1[i], p2[i], p3[i], p4[i]], [Y[cc]])
                if sc % 4 == 3:
                    h.stt("dve", y1[:, cc, :], uf[:, cc, :], dsk[:, cc:cc + 1], Y[cc][:], ALU.mult, ALU.add, [uf, dsk, Y[cc]], [y1])
                    h.tt("pool", yt[:, cc, :], y1[:, cc, :], y1[:, cc, :], ALU.mult, [y1], [yt])
                    h.ts("pool", yt[:, cc, :], yt[:, cc, :], 0.044715, 1.0, ALU.mult, ALU.add, [yt], [yt])
                    h.tt("pool", yt[:, cc, :], yt[:, cc, :], y1[:, cc, :], ALU.mult, [yt, y1], [yt])
                    h.act(yt[:, cc, :], yt[:, cc, :], AF.Sigmoid, [yt], [yt], scale=GK)
                    h.tt("pool", yg[:, cc, :], y1[:, cc, :], yt[:, cc, :], ALU.mult, [y1, yt], [yg])
                    h.cp("pool", ygb[:, cc, :], yg[:, cc, :], [yg], [ygb])
            h.tt("dve", q1[:], gre_e[:], cS[:], ALU.mult, [gre_e, cS], [q1])
            h.tt("dve", q2[:], gim_e[:], nsS[:], ALU.mult, [gim_e, nsS], [q2])
            h.tt("dve", ire[:], q1[:], q2[:], ALU.add, [q1, q2], [ire])
            h.tt("dve", q1[:], gre_e[:], sS[:], ALU.mult, [gre_e, sS], [q1])
            h.tt("dve", q2[:], gim_e[:], cS[:], ALU.mult, [gim_e, cS], [q2])
            h.tt("dve", iim[:], q1[:], q2[:], ALU.add, [q1, q2], [iim])
            for oc in range(2):
                h.mm([(Pg[:], wglu[:, kc, oc * 128:(oc + 1) * 128], ygb[:, kc, :], kc == 0, kc == 1) for kc in range(2)], [wglu, ygb], [Pg])
                h.act(sg[:, oc, :], Pg[:], AF.Sigmoid, [Pg], [sg])
                h.tt("pool", y2[:, oc, :], yg[:, oc, :], sg[:, oc, :], ALU.mult, [yg, sg], [y2])
                h.tt("pool", sg[:, oc, :], y2[:, oc, :], y2[:, oc, :], ALU.mult, [y2], [sg])
            h.mm([(Pt[:], onesf[:], sg[:, oc, :], oc == 0, oc == 1) for oc in range(2)], [onesf, sg], [Pt])
            h.act(rstd[:], Pt[:], AF.Sqrt, [Pt, epsb], [rstd], bias=epsb[:, 0:1], scale=1.0 / 256)
            h.recip(rstd[:], rstd[:], [rstd], [rstd])
            for oc in range(2):
                h.stt("dve", yo_[:, oc, :], y2[:, oc, :], nw5[:, oc:oc + 1], rstd[:], ALU.mult, ALU.mult, [y2, nw5, rstd], [yo_])
            h.dma("sp", k.y_d.t[0:256, cols].rearrange("(j p) t -> p j t", p=128), yo_[:], [yo_], [k.r_y[0][mt]], yo_)
        P.end_phase()


def gdn_phase(k, l):
    P, T, h, inp = k.P, k.T, k.h, k.inp
    NMT = T // 512
    MT = 512
    identf, identb = k.identf, k.identb
    with contextlib.ExitStack() as es:
        masks = make_masks(h, P, es)
        b12, na12 = gate_consts(k, es, l, "gd")
        gnw = P.sb(es, "gd_gnw", [128, 64], F32)
        k.load_row(gnw, inp["gdn_norm"].t[l:l + 1, :])
        Sf = P.sb(es, "gd_Sf", [128, 3, 128], F32)
        Sb = P.sb(es, "gd_Sb", [128, 3, 128], BF16)
        h.memset("pool", Sf[:], 0.0, [Sf])
        h.memset("pool", Sb[:], 0.0, [Sb])
        qnT = [P.sb(es, f"gd_qn{i}", [128, 3, MT], BF16) for i in range(2)]
        knT = [P.sb(es, f"gd_kn{i}", [128, 3, MT], BF16) for i in range(2)]
        vT = [P.sb(es, f"gd_vT{i}", [128, 3, MT], F32) for i in range(2)]
        pj = [P.sb(es, f"gd_pj{i}", [128, 4, 786], F32) for i in range(2)]
        yo = [P.sb(es, f"gd_yo{i}", [128, 3, MT], BF16) for i in range(2)]
        R_sp12 = Rot(P, es, "gd_sp12", [128, 12], F32)
        R_gda = Rot(P, es, "gd_gda", [128, 12], F32)
        R_cs12 = Rot(P, es, "gd_cs12", [128, 12], F32)
        R_cl12 = Rot(P, es, "gd_cl12", [128, 12], F32)
        R_beta = Rot(P, es, "gd_beta", [128, 6], F32)
        R_nbeta = Rot(P, es, "gd_nbeta", [128, 6], F32)
        R_egc = Rot(P, es, "gd_egc", [128, 6], F32)
        R_t6 = Rot(P, es, "gd_t6", [128, 6], F32)
        R_dkk = Rot(P, es, "gd_dkk", [128, 6], F32)
        R_gtot = Rot(P, es, "gd_gtot", [128, 6], F32)
        R_gtc = Rot(P, es, "gd_gtc", [128, 3], F32)
        R_dg = Rot(P, es, "gd_dg", [128, 6, 128], F32)
        R_arg = Rot(P, es, "gd_arg", [128, 6, 128], F32)
        R_E = Rot(P, es, "gd_E", [128, 6, 128], F32)
        R_EU = Rot(P, es, "gd_EU", [128, 6, 128], F32)
        R_ELn = Rot(P, es, "gd_ELn", [128, 6, 128], F32)
        R_attnT = Rot(P, es, "gd_attnT", [128, 6, 128], BF16)
        R_Pm = [Rot(P, es, f"gd_Pm{i}", [128, 6, 128], BF16) for i in range(2)]
        R_Qm = [Rot(P, es, f"gd_Qm{i}", [128, 6, 128], BF16) for i in range(2)]
        sp_fns = [softplus12(h, P, es, f"gd{i}") for i in range(2)]
        R_Xb = Rot(P, es, "gd_Xb", [128, 6, 128], BF16)
        R_kdec = Rot(P, es, "gd_kdec", [128, 384], BF16)
        R_v_tm = Rot(P, es, "gd_vtm", [128, 384], F32)
        R_rr_ = Rot(P, es, "gd_rr", [128, 384], F32)
        R_rb = Rot(P, es, "gd_rb", [128, 384], BF16)
        R_vnb = Rot(P, es, "gd_vnb", [128, 384], BF16)
        R_oa = Rot(P, es, "gd_oa", [128, 384], F32)
        R_o = Rot(P, es, "gd_o", [128, 384], F32)
        R_sq = Rot(P, es, "gd_sq", [128, 384], F32)
        R_ss6 = Rot(P, es, "gd_ss6", [128, 6], F32)
        R_rs6 = Rot(P, es, "gd_rs6", [128, 6], F32)
        R_zg = Rot(P, es, "gd_zg", [128, 384], F32)
        R_yb = Rot(P, es, "gd_yb", [128, 384], BF16)
        R_tmpS = Rot(P, es, "gd_tmpS", [128, 3, 128], F32)
        W0 = P.ps(es, "gd_W0", [128, 2, 512], F32)
        W1 = P.ps(es, "gd_W1", [128, 2, 512], F32)
        W2 = P.ps(es, "gd_W2", [128, 2, 512], F32)
        W1a, W1b, W2a, W2b = Buf(None, "W1a"), Buf(None, "W1b"), Buf(None, "W2a"), Buf(None, "W2b")
        W1h, W2h = (W1a, W1b), (W2a, W2b)
        M1bh_s = [(Buf(None, f"m1a{i}"), Buf(None, f"m1b{i}")) for i in range(2)]
        M1pbh_s = [(Buf(None, f"m1pa{i}"), Buf(None, f"m1pb{i}")) for i in range(2)]
        PB = P.ps(es, "gd_PB", [128, 1024], BF16)
        pA = P.ps(es, "gd_pA", [128, 32], F32)
        x3 = lambda ap: ap.rearrange("p (h d) -> p h d", h=6)
        kz = [P.sb(es, f"gd_kz{i}", [128, 3, MT], BF16) for i in range(2)]
        R_Tb = Rot(P, es, "gd_Tb", [128, 6, 128], BF16)
        R_Tb2 = Rot(P, es, "gd_Tb2", [128, 6, 128], BF16)
        R_Xb2 = Rot(P, es, "gd_Xb2", [128, 6, 128], BF16)
        epsb = P.sb(es, "gd_eps", [128, 1], F32)
        h.memset("pool", epsb[:], EPS, [epsb])
        R_ez = Rot(P, es, "gd_ez", [128, 384], F32)
        R_M1b = Rot(P, es, "gd_M1b", [128, 6, 128], BF16)
        R_M1pb = Rot(P, es, "gd_M1pb", [128, 6, 128], BF16)
        cmask = P.sb(es, "gd_cmask", [128, 14, 128], BF16)
        h.dma("pool", cmask[:], inp["gdn_cmask"].t.rearrange("m p j -> p m j"), [], [cmask], cmask)
        cmask6 = P.sb(es, "gd_cmask6", [128, 14, 6, 128], BF16)
        h.cp("pool", cmask6[:], cmask[:].unsqueeze(2).to_broadcast([128, 14, 6, 128]), [cmask], [cmask6])
        rmask = P.sb(es, "gd_rmask", [128, 2], F32)
        h.memset("pool", rmask[:], 0.0, [rmask])
        h.memset("pool", rmask[0:64, 0:1], 1.0, [rmask])
        h.memset("pool", rmask[64:128, 1:2], 1.0, [rmask])

        def headmm(Wps, lh, rh, R, Wh):
            w = w4(Wps)
            h.mm([(w[:, hd // 3, hd % 3, :], lh[:, hd, :], rh[:, hd, :], True, True) for hd in range(6)], R, list(Wh))

        stop = k.flags.get("gdn_stop", 99)
        for mt in range(NMT):
            cols = slice(mt * MT, (mt + 1) * MT)
            i2 = mt % 2
            rp = [k.r_pre[mt]]
            h.dma("sp", qnT[i2][:], k.qn_d.t[:, cols].rearrange("(j p) t -> p j t", p=128), rp, [qnT[i2]], qnT[i2])
            h.dma("sp", knT[i2][:], k.kn_d.t[:, cols].rearrange("(j p) t -> p j t", p=128), rp, [knT[i2]], knT[i2])
            h.dma("sp", vT[i2][:], k.v_d.t[:, cols].rearrange("(j p) t -> p j t", p=128), rp, [vT[i2]], vT[i2])
            h.dma("sp", pj[i2][:], k.pt_d.t[mt * MT:(mt + 1) * MT, :].rearrange("(a p) n -> p a n", p=128), rp, [pj[i2]], pj[i2])
            qn_, kn_, v_, pj_, yo_ = qnT[i2], knT[i2], vT[i2], pj[i2], yo[i2]
            for s_ in range(2):
                h.ts("dve", kz[s_][:], kn_[:], rmask[:, s_:s_ + 1], None, ALU.mult, None, [kn_, rmask], [kz[s_]])
            for ti in range(4):
                tc = slice(ti * 128, (ti + 1) * 128)
                tix = mt * 4 + ti
                sp12 = R_sp12.at(tix); gda = R_gda.at(tix); cs12 = R_cs12.at(tix); cl12 = R_cl12.at(tix); beta = R_beta.at(tix); nbeta = R_nbeta.at(tix); egc = R_egc.at(tix); t6 = R_t6.at(tix); dkk = R_dkk.at(tix); gtot = R_gtot.at(tix); gtc = R_gtc.at(tix); dg = R_dg.at(tix); arg = R_arg.at(tix); E = R_E.at(tix); EU = R_EU.at(tix); ELn = R_ELn.at(tix); attnT = R_attnT.at(tix); Xb = R_Xb.at(tix); kdec = R_kdec.at(tix); v_tm = R_v_tm.at(tix); rr_ = R_rr_.at(tix); rb = R_rb.at(tix); vnb = R_vnb.at(tix); oa = R_oa.at(tix); o = R_o.at(tix); sq = R_sq.at(tix); ss6 = R_ss6.at(tix); rs6 = R_rs6.at(tix); zg = R_zg.at(tix); yb = R_yb.at(tix); tmpS = R_tmpS.at(tix); Tb = R_Tb.at(tix); M1b = R_M1b.at(tix); M1pb = R_M1pb.at(tix)
                Pm = [r.at(tix) for r in R_Pm]; Qm = [r.at(tix) for r in R_Qm]; sp_fn = sp_fns[tix % 2]; M1bh = M1bh_s[tix % 2]; M1pbh = M1pbh_s[tix % 2]; Tb2 = R_Tb2.at(tix); Xb2 = R_Xb2.at(tix); ez = R_ez.at(tix)
                gate_tile(k, sp_fn, pj_[:, ti, 768:780], pj_, b12, na12, masks, sp12, gda, cs12, cl12, pA)
                gc = cs12[:, 0:6]
                h.act(beta[:], pj_[:, ti, 780:786], AF.Exp, [pj_], [beta], scale=-1.0)
                h.ts("dve", beta[:], beta[:], 1.0, None, ALU.add, None, [beta], [beta])
                h.recip(beta[:], beta[:], [beta], [beta])
                h.ts("dve", nbeta[:], beta[:], -1.0, None, ALU.mult, None, [beta], [nbeta])
                h.act(egc[:], gc, AF.Exp, [cs12], [egc])
                h.tt("dve", t6[:], cl12[:, 0:6], gc, ALU.subtract, [cl12, cs12], [t6])
                h.act(dkk[:], t6[:], AF.Exp, [t6], [dkk])
                h.act(gtot[:], cl12[:, 0:6], AF.Exp, [cl12], [gtot])
                g2 = gtot[:].rearrange("p (j s) -> p j s", s=2)
                h.cp("dve", gtc[0:64, :], g2[0:64, :, 0], [gtot], [gtc])
                h.cp("dve", gtc[64:128, :], g2[64:128, :, 1], [gtot], [gtc])
                if stop < 1:
                    h.memset("pool", yo_[:, :, tc], 0.0, [yo_])
                    continue
                h.tt("pool", dg[:], b3(identf[:], 6, 128), s3(gc, 6, 128), ALU.mult, [identf, cs12], [dg])
                h.mm([(W0[:, 0, 0:384], masks["ones"][:], dg[:, 0:3, :].rearrange("p a l -> p (a l)"), True, True),
                      (W0[:, 1, 0:384], masks["ones"][:], dg[:, 3:6, :].rearrange("p a l -> p (a l)"), True, True)],
                     [masks["ones"], dg], [W0])
                if stop < 1.2:
                    h.memset("pool", yo_[:, :, tc], 0.0, [yo_])
                    continue
                h.tt("dve", v4(arg[:]), w4(W0), s4(gc), ALU.subtract, [W0, cs12], [arg])
                if stop < 1.4:
                    h.memset("pool", yo_[:, :, tc], 0.0, [yo_])
                    continue
                h.act(arg[:], arg[:], AF.Abs, [arg], [arg])
                h.act(E[:], arg[:], AF.Exp, [arg], [E], scale=-1.0)
                if stop < 1.6:
                    h.memset("pool", yo_[:, :, tc], 0.0, [yo_])
                    continue
                h.tt("pool", EU[:], E[:], b3(masks["U"][:], 6, 128), ALU.mult, [E, masks["U"]], [EU])
                if stop < 1.8:
                    h.memset("pool", yo_[:, :, tc], 0.0, [yo_])
                    continue
                h.tt("pool", ELn[:], E[:], b3(masks["Ls"][:], 6, 128), ALU.mult, [E, masks["Ls"]], [ELn])
                if stop < 1.9:
                    h.memset("pool", yo_[:, :, tc], 0.0, [yo_])
                    continue
                h.tt("pool", ELn[:], ELn[:], s3(nbeta[:], 6, 128), ALU.mult, [ELn, nbeta], [ELn])
                if stop < 2:
                    h.memset("pool", yo_[:, :, tc], 0.0, [yo_])
                    continue
                w1 = w4(W1)
                w2 = w4(W2)
                h.mm([(w1[:, hd // 3, hd % 3, :], kz[hd % 2][:, hd // 2, tc], kn_[:, hd // 2, tc], True, True) for hd in range(6)],
                     [kz[0], kz[1], kn_], [W1a, W1b])
                h.mm([(w2[:, hd // 3, hd % 3, :], kz[hd % 2][:, hd // 2, tc], qn_[:, hd // 2, tc], True, True) for hd in range(6)],
                     [kz[0], kz[1], qn_], [W2a, W2b])
                h.tt("dve", v4(Pm[0][:]), w1, v4(ELn[:]), ALU.mult, [W1a, W1b, ELn], [Pm[0]])
                h.tt("dve", v4(attnT[:]), w2, v4(EU[:]), ALU.mult, [W2a, W2b, EU], [attnT])
                if stop < 3:
                    h.memset("pool", yo_[:, :, tc], 0.0, [yo_])
                    continue
                h.tr([(PB[:, hd * 128:(hd + 1) * 128], Pm[0][:, hd, :]) for hd in range(6)], identb[:], [Pm[0], identb], [PB])
                h.cp("act", Qm[0][:], PB[:, 0:768].rearrange("p (a l) -> p a l", a=6), [PB], [Qm[0]])
                Nn, NT_ = Pm[0], Qm[0]
                h.tt("pool", Pm[1][:], Nn[:], b3(cmask[:, 0, :], 6, 128), ALU.mult, [Nn, cmask], [Pm[1]])
                h.tt("pool", Qm[1][:], NT_[:], b3(cmask[:, 7, :], 6, 128), ALU.mult, [NT_, cmask], [Qm[1]])
                h.tt("dve", Tb[:], Pm[1][:], b3(identf[:], 6, 128), ALU.add, [Pm[1], identf], [Tb])
                h.tt("dve", Xb[:], Qm[1][:], b3(identf[:], 6, 128), ALU.add, [Qm[1], identf], [Xb])
                Tc, Xc = Tb, Xb
                Tn, Xn = Tb2, Xb2
                for lv in range(1, 7):
                    w1_ = w4(W1)
                    w2_ = w4(W2)
                    for hf_ in range(2):
                        hs = range(3 * hf_, 3 * hf_ + 3)
                        h.mm([(w1_[:, hf_, hd % 3, :], NT_[:, hd, :], Tc[:, hd, :], True, True) for hd in hs], [NT_, Tc], [W1h[hf_]])
                        h.mm([(w2_[:, hf_, hd % 3, :], Nn[:, hd, :], Xc[:, hd, :], True, True) for hd in hs], [Nn, Xc], [W2h[hf_]])
                    for hf_ in range(2):
                        sl = slice(3 * hf_, 3 * hf_ + 3)
                        h.tt("dve", M1b[:, sl, :], w1_[:, hf_, :, :], cmask6[:, lv, sl, :], ALU.mult, [W1h[hf_], cmask6], [M1bh[hf_]])
                        h.tt("dve", M1pb[:, sl, :], w2_[:, hf_, :, :], cmask6[:, 7 + lv, sl, :], ALU.mult, [W2h[hf_], cmask6], [M1pbh[hf_]])
                    for hf_ in range(2):
                        hs = range(3 * hf_, 3 * hf_ + 3)
                        sl = slice(3 * hf_, 3 * hf_ + 3)
                        h.mm([(W1[:, hf_, 0:384], identb[:], Tc[:, sl, :].rearrange("p a l -> p (a l)"), True, False)]
                             + [(w1_[:, hf_, hd % 3, :], Xc[:, hd, :], M1b[:, hd, :], False, True) for hd in hs],
                             [identb, Tc, Xc, M1bh[hf_]], [W1h[hf_]])
                        h.mm([(W2[:, hf_, 0:384], identb[:], Xc[:, sl, :].rearrange("p a l -> p (a l)"), True, False)]
                             + [(w2_[:, hf_, hd % 3, :], Tc[:, hd, :], M1pb[:, hd, :], False, True) for hd in hs],
                             [identb, Tc, Xc, M1pbh[hf_]], [W2h[hf_]])
                    for hf_ in range(2):
                        sl = slice(3 * hf_, 3 * hf_ + 3)
                        h.cp("act", Tn[:, sl, :], w1_[:, hf_, :, :], [W1h[hf_]], [Tn])
                        h.cp("act", Xn[:, sl, :], w2_[:, hf_, :, :], [W2h[hf_]], [Xn])
                    Tc, Xc, Tn, Xn = Tn, Xn, Tc, Xc
                Xb = Xc
                if stop < 4:
                    h.memset("pool", yo_[:, :, tc], 0.0, [yo_])
                    continue
                h.tr([(PB[:, j * 128:(j + 1) * 128], kn_[:, j, tc]) for j in range(3)], identb[:], [kn_, identb], [PB])
                h.tt("dve", x3(kdec[:]), x3(PB[:, 0:384]), s3(dkk[:], 6, 64), ALU.mult, [PB, dkk], [kdec])
                h.tr([(W0[:, 0, j * 128:(j + 1) * 128], v_[:, j, tc]) for j in range(3)], identf[:], [v_, identf], [W0])
                h.cp("act", v_tm[:], W0[:, 0, 0:384], [W0], [v_tm])
                if stop < 5:
                    h.memset("pool", yo_[:, :, tc], 0.0, [yo_])
                    continue
                h.mm([(W1[:, 0, j * 128:(j + 1) * 128], kn_[:, j, tc], Sb[:, j, :], True, True) for j in range(3)], [kn_, Sb], [W1a, W1b])
                h.tt("dve", x3(rr_[:]), x3(W1[:, 0, 0:384]), s3(egc[:], 6, 64), ALU.mult, [W1a, W1b, egc], [rr_])
                h.tt("pool", rr_[:], rr_[:], v_tm[:], ALU.subtract, [rr_, v_tm], [rr_])
                h.tt("pool", x3(rb[:]), x3(rr_[:]), s3(nbeta[:], 6, 64), ALU.mult, [rr_, nbeta], [rb])
                h.mm([(W2[:, 0, hd * 64:(hd + 1) * 64], Xb[:, hd, :], rb[:, hd * 64:(hd + 1) * 64], True, True) for hd in range(6)], [Xb, rb], [W2a, W2b])
                h.cp("act", vnb[:], W2[:, 0, 0:384], [W2a, W2b], [vnb])
                h.mm([(W1[:, 0, j * 128:(j + 1) * 128], qn_[:, j, tc], Sb[:, j, :], True, True) for j in range(3)], [qn_, Sb], [W1a, W1b])
                h.tt("dve", x3(oa[:]), x3(W1[:, 0, 0:384]), s3(egc[:], 6, 64), ALU.mult, [W1a, W1b, egc], [oa])
                h.mm([(W2[:, 0, hd * 64:(hd + 1) * 64], attnT[:, hd, :], vnb[:, hd * 64:(hd + 1) * 64], True, True) for hd in range(6)], [attnT, vnb], [W2a, W2b])
                h.tt("dve", o[:], W2[:, 0, 0:384], oa[:], ALU.add, [W2a, W2b, oa], [o])
                h.mm([(W0[:, 0, j * 128:(j + 1) * 128], kdec[:, j * 128:(j + 1) * 128], vnb[:, j * 128:(j + 1) * 128], True, True) for j in range(3)],
                     [kdec, vnb], [W0])
                h.tt("dve", tmpS[:], W0[:, 0, 0:384].rearrange("p (j c) -> p j c", j=3), b3(masks["bd"][:], 3, 128), ALU.mult, [W0, masks["bd"]], [tmpS])
                h.tt("pool", Sf[:], Sf[:], s3(gtc[:], 3, 128), ALU.mult, [Sf, gtc], [Sf])
                h.tt("pool", Sf[:], Sf[:], tmpS[:], ALU.add, [Sf, tmpS], [Sf])
                h.cp("act", Sb[:], Sf[:], [Sf], [Sb])
                if stop < 6:
                    h.memset("pool", yo_[:, :, tc], 0.0, [yo_])
                    continue
                h.tt("pool", sq[:], o[:], o[:], ALU.mult, [o], [sq])
                h.reduce(ss6[:], x3(sq[:]), ALU.add, [sq], [ss6])
                h.act(rs6[:], ss6[:], AF.Ln, [ss6, epsb], [rs6], bias=epsb[:, 0:1], scale=1.0 / 64)
                h.act(rs6[:], rs6[:], AF.Exp, [rs6], [rs6], scale=-0.5)
                h.tt("dve", x3(o[:]), x3(o[:]), s3(rs6[:], 6, 64), ALU.mult, [o, rs6], [o])
                h.tt("pool", x3(o[:]), x3(o[:]), b3(gnw[:], 6, 64), ALU.mult, [o, gnw], [o])
                h.act(ez[:], pj_[:, ti, 0:384], AF.Exp, [pj_], [ez], scale=-1.0)
                h.ts("pool", ez[:], ez[:], 1.0, None, ALU.add, None, [ez], [ez])
                h.tt("pool", zg[:], o[:], pj_[:, ti, 0:384], ALU.mult, [o, pj_], [zg])
                h.recip(ez[:], ez[:], [ez], [ez])
                h.tt("pool", yb[:], zg[:], ez[:], ALU.mult, [zg, ez], [yb])
                h.tr([(PB[:, j * 128:(j + 1) * 128], yb[:, j * 128:(j + 1) * 128]) for j in range(3)], identb[:], [yb, identb], [PB])
                h.cp("act", yo_[:, :, tc], PB[:, 0:384].rearrange("p (j t) -> p j t", j=3), [PB], [yo_])
            h.dma("sp", k.y_d.t[256:640, cols].rearrange("(j p) t -> p j t", p=128), yo_[:], [yo_], [k.r_y[1][mt]], yo_)
        P.end_phase()


class K:
    def __init__(self, T, L, flags):
        self.T = T
        self.L = L
        self.NT = T // 128
        self.flags = flags


def bcast(ap, shape):
    return ap.to_broadcast(list(shape))


def build(T, L, flags=None):
    flags = flags or {}
    nc = bass.Bass("TRN2", target_bir_lowering=False)
    k = K(T, L, flags)
    NT = T // 128
    with contextlib.ExitStack() as es0:
        P = Prog(nc, es0)
        P.verbose = bool(flags.get("verbose"))
        k.P = P
        inp = {}

        def ein(name, shape):
            inp[name] = P.dram(name, shape, F32, "ExternalInput")
            return inp[name]

        x_in = ein("x", [T, D])
        c_in = ein("c", [1, D])
        w_ada = ein("w_ada", [L, D, 6 * D])
        b_ada = ein("b_ada", [L, 6 * D])
        norm_mix = ein("norm_mix", [L, D])
        norm_ffn = ein("norm_ffn", [L, D])
        norm_final = ein("norm_final", [1, D])
        w_rt = ein("w_rt", [L, D, 36])
        b_rt = ein("b_rt", [L, 36])
        w_gate = ein("moe_w_gate", [L, NEXP, D, DEXP])
        w_up = ein("moe_w_up", [L, NEXP, D, DEXP])
        w_down = ein("moe_w_down", [L, NEXP, DEXP, D])
        ein("w_in_f", [L, D, 2304])
        ein("w_in_t", [L, D, 786])
        ein("w_out", [L, D, D])
        ein("conv_w", [L, 128, 16, 4])
        ein("conv_b", [L, 128, 16])
        ein("bias12", [L, 12])
        ein("alog12", [L, 12])
        ein("ssd_d", [L, 6])
        ein("ssd_norm", [L, 384])
        ein("gdn_norm", [L, 64])
        ein("gdn_cmask", [14, 128, 128])
        ein("s5_are", [L, 128, 8])
        ein("s5_aim", [L, 128, 8])
        ein("s5_ldt", [L, 128, 8])
        ein("s5_dcol", [L, 128, 2])
        ein("s5_ncol", [L, 128, 2])
        ein("s5_bT_re", [L, 8, 128, 128])
        ein("s5_bT_im", [L, 8, 128, 128])
        ein("s5_cT_re", [L, 8, 128, 128])
        ein("s5_cT_im", [L, 8, 128, 128])
        ein("s5_w_glu", [L, 256, 256])
        out = P.dram("out", [T, D], F32, "ExternalOutput")
        k.u_d = P.dram("u_d", [256, T], F32)
        k.qn_d = P.dram("qn_d", [384, T], BF16)
        k.kn_d = P.dram("kn_d", [384, T], BF16)
        k.v_d = P.dram("v_d", [384, T], F32)
        k.xs_d = P.dram("xs_d", [384, T], F32)
        k.B_d = P.dram("B_d", [256, T], BF16)
        k.C_d = P.dram("C_d", [256, T], BF16)
        k.pt_d = P.dram("pt_d", [T, 786], F32)
        k.y_d = P.dram("y_d", [D, T], BF16)
        k.r_pre = [P.region(f"pre_{i}") for i in range(max(1, T // 512))]
        k.r_y = [[P.region(f"y{j}_{i}") for i in range(max(1, T // 512))] for j in range(3)]
        k.h = H(P)
        h = k.h
        modv = P.dram("modv", [L, 6 * D], F32)
        dbg = flags.get("dbg", False)
        if dbg:
            dbg_coef = P.dram("dbg_coef", [T, 32], F32, "ExternalOutput")
            dbg_y = P.dram("dbg_y", [T, D], F32, "ExternalOutput")
            dbg_h = P.dram("dbg_h", [T, D], F32, "ExternalOutput")
            r_dbg = P.region("dbg")
        scr = [P.dram("xs0", [T, D], F32), P.dram("xs1", [T, D], F32)]
        k.inp = inp

        def regs(name):
            return [P.region(f"{name}_{i}") for i in range(NT)]
        r_x = regs("x")
        r_scr = [regs("xs0"), regs("xs1")]
        r_out = regs("out")
        r_modv = P.region("modv")

        identf = P.sb(es0, "identf", [128, 128], F32)
        identb = P.sb(es0, "identb", [128, 128], BF16)
        P.op("pool", lambda e: e.memset(identf[:], 0.0), writes=[identf])
        P.op("pool", lambda e: e.affine_select(out=identf[:], in_=identf[:], pattern=[[-1, 128]],
                                               compare_op=ALU.not_equal, fill=1.0, base=0,
                                               channel_multiplier=1),
             reads=[identf], writes=[identf])
        P.op("dve", lambda e: e.tensor_copy(out=identb[:], in_=identf[:]), reads=[identf], writes=[identb])
        k.identf, k.identb = identf, identb

        with contextlib.ExitStack() as es:
            ccol = P.sb(es, "ccol", [128, 8], F32)
            cb = P.sb(es, "cb", [128, 8, 128], BF16)
            wa = [P.sb(es, f"wa{i}", [128, 8, 512], BF16) for i in range(2)]
            pm = [P.ps(es, f"pm{i}", [128, 512], F32) for i in range(2)]
            brow = [P.sb(es, f"brow{i}", [1, 512], F32) for i in range(2)]
            mrow = [P.sb(es, f"mrow{i}", [1, 512], F32) for i in range(2)]
            P.op("sp", lambda e: e.dma_start(out=ccol[:], in_=c_in.t.rearrange("o (k p) -> p (o k)", p=128),
                                             allow_slow_non_contiguous=True),
                 writes=[ccol], dma=ccol)
            P.op("act", lambda e: e.activation(out=ccol[:], in_=ccol[:], func=AF.Silu), reads=[ccol], writes=[ccol])
            P.op("dve", lambda e: e.tensor_copy(out=cb[:], in_=bcast(ccol[:].unsqueeze(2), [128, 8, 128])),
                 reads=[ccol], writes=[cb])
            it = 0
            for l in range(L):
                for n in range(12):
                    i = it % 2
                    it += 1
                    P.op("pool", lambda e, l=l, n=n, i=i: e.dma_start(
                        out=wa[i][:], in_=w_ada.t[l, :, n * 512:(n + 1) * 512].rearrange("(k p) n -> p k n", p=128)),
                        writes=[wa[i]], dma=wa[i])
                    P.op("sp", lambda e, l=l, n=n, i=i: e.dma_start(
                        out=brow[i][:], in_=b_ada.t[l:l + 1, n * 512:(n + 1) * 512]),
                        writes=[brow[i]], dma=brow[i])

                    def mm(e, i=i):
                        r = None
                        for kk in range(8):
                            r = e.matmul(pm[i][:], lhsT=cb[:, kk, :], rhs=wa[i][:, kk, :], start=(kk == 0), stop=(kk == 7))
                        return r
                    P.op("pe", mm, reads=[cb, wa[i]], writes=[pm[i]])
                    P.op("dve", lambda e, i=i: e.tensor_tensor(out=mrow[i][:], in0=pm[i][0:1, :], in1=brow[i][:], op=ALU.add),
                         reads=[pm[i], brow[i]], writes=[mrow[i]])
                    P.op("sp", lambda e, l=l, n=n, i=i: e.dma_start(
                        out=modv.t[l:l + 1, n * 512:(n + 1) * 512], in_=mrow[i][:]),
                        reads=[mrow[i]], writes=[r_modv], dma=mrow[i])
            P.end_phase()

        def load_row(dst, src_ap, extra_reads=()):
            P.op("sp", lambda e: e.dma_start(out=dst[:], in_=src_ap.partition_broadcast(128)),
                 reads=list(extra_reads), writes=[dst], dma=dst)

        def norm_consts(es, l, nw, i_scale, i_shift, tag):
            A = P.sb(es, f"A{tag}", [128, D], F32)
            B = P.sb(es, f"B{tag}", [128, D], F32)
            W = P.sb(es, f"W{tag}", [128, D], F32)
            load_row(A, modv.t[l:l + 1, i_scale * D:(i_scale + 1) * D], [r_modv])
            load_row(B, modv.t[l:l + 1, i_shift * D:(i_shift + 1) * D], [r_modv])
            load_row(W, nw)
            P.op("dve", lambda e: e.scalar_tensor_tensor(out=A[:], in0=A[:], scalar=1.0, in1=W[:], op0=ALU.add, op1=ALU.mult),
                 reads=[A, W], writes=[A])
            return A, B

        def rms_mod(xt, A, B, hout, ssq, rstd, junk, tmp):
            P.op("act", lambda e: e.activation(out=junk[:], in_=xt[:], func=AF.Square, accum_out=ssq[:]),
                 reads=[xt], writes=[junk, ssq], est=0.9)
            P.op("dve", lambda e: e.tensor_scalar(out=rstd[:], in0=ssq[:], scalar1=1.0 / D, scalar2=EPS, op0=ALU.mult, op1=ALU.add),
                 reads=[ssq], writes=[rstd])
            P.op("act", lambda e: e.activation(out=rstd[:], in_=rstd[:], func=AF.Sqrt), reads=[rstd], writes=[rstd])
            P.op("dve", lambda e: e.reciprocal(out=rstd[:], in_=rstd[:]), reads=[rstd], writes=[rstd])
            if B is None:
                P.op("dve", lambda e: e.scalar_tensor_tensor(out=hout[:], in0=xt[:], scalar=rstd[:, 0:1], in1=A[:], op0=ALU.mult, op1=ALU.mult),
                     reads=[xt, rstd, A], writes=[hout], est=1.15)
            else:
                P.op("dve", lambda e: e.scalar_tensor_tensor(out=tmp[:], in0=xt[:], scalar=rstd[:, 0:1], in1=A[:], op0=ALU.mult, op1=ALU.mult),
                     reads=[xt, rstd, A], writes=[tmp], est=1.15)
                P.op("pool", lambda e: e.tensor_tensor(out=hout[:], in0=tmp[:], in1=B[:], op=ALU.add),
                     reads=[tmp, B], writes=[hout], est=1.3)

        k.modv, k.r_modv = modv, r_modv
        k.load_row, k.norm_consts, k.rms_mod = load_row, norm_consts, rms_mod
        cur = (x_in, r_x)
        nxt_i = 0

        def next_dst():
            nonlocal nxt_i
            d = (scr[nxt_i], r_scr[nxt_i])
            nxt_i ^= 1
            return d

        for l in range(L):
            if flags.get("mixer", True):
                dstt = next_dst()
                mixer_layer(k, l, cur, dstt)
                cur = dstt
            if flags.get("moe", True):
                src, rsrc = cur
                dst, rdst = next_dst()
                SBT = min(16, NT)
                with contextlib.ExitStack() as es:
                    A, B = norm_consts(es, l, norm_ffn.t[l:l + 1, :], 4, 3, "f")
                    G = P.sb(es, "Gf", [128, D], F32)
                    load_row(G, modv.t[l:l + 1, 5 * D:6 * D], [r_modv])
                    brt = P.sb(es, "brt", [128, 36], F32)
                    load_row(brt, b_rt.t[l:l + 1, :])
                    wrt = P.sb(es, "wrt", [128, 8, 36], F32)
                    P.op("sp", lambda e: e.dma_start(out=wrt[:], in_=w_rt.t[l].rearrange("(k p) n -> p k n", p=128)),
                         writes=[wrt], dma=wrt)
                    hT = P.sb(es, "hT", [128, 8, SBT * 128], BF16)
                    yacc = [P.sb(es, f"yacc{i}", [128, D], F32) for i in range(SBT)]
                    coef = P.sb(es, "coef", [128, SBT, 32], F32)
                    xt = [P.sb(es, f"xt{i}", [128, D], F32) for i in range(2)]
                    R_hf = Rot(P, es, "hf", [128, D], F32)
                    R_tmp = Rot(P, es, "tmpf", [128, D], F32)
                    R_junk = Rot(P, es, "junkf", [128, D], F32)
                    R_hTf = Rot(P, es, "hTf", [128, 8, 128], F32)
                    R_ssq = Rot(P, es, "ssq", [128, 1], F32)
                    R_rstd = Rot(P, es, "rstd", [128, 1], F32)
                    R_lg = Rot(P, es, "lg", [128, 36], F32)
                    R_sm = Rot(P, es, "sm", [128, 16], F32)
                    R_gm = Rot(P, es, "gm", [128, 4], F32)
                    R_gex = Rot(P, es, "gex", [128, 4], F32)
                    R_le4 = Rot(P, es, "le4", [128, 4, 8], F32)
                    R_les = Rot(P, es, "les", [128, 8], F32)
                    R_le2 = Rot(P, es, "le2", [128, 8], F32)
                    R_mk1 = Rot(P, es, "mk1", [128, 8], F32)
                    R_mk2 = Rot(P, es, "mk2", [128, 8], F32)
                    R_csel = Rot(P, es, "csel", [128, 8], F32)
                    wg = [P.sb(es, f"wg{i}", [128, 8, DEXP], BF16) for i in range(2)]
                    wu = [P.sb(es, f"wu{i}", [128, 8, DEXP], BF16) for i in range(2)]
                    wd = [P.sb(es, f"wd{i}", [128, 2, D], BF16) for i in range(2)]
                    sg = [P.sb(es, f"sg{i}", [128, 512], F32) for i in range(2)]
                    hid = [P.sb(es, f"hid{i}", [128, 2, 512], BF16) for i in range(2)]
                    ptr = P.ps(es, "ptr", [128, 8, 128], F32)
                    pg = [P.ps(es, f"pg{i}", [128, 512], F32) for i in range(2)]
                    pu = [P.ps(es, f"pu{i}", [128, 512], F32) for i in range(2)]
                    py = [P.ps(es, f"py{i}", [128, 512], F32) for i in range(2)]

                    wcnt = 0
                    for sb0 in range(0, NT, SBT):
                        def router_tile(t, ti, xb, hf, tmp, junk, hTf, ssq, rstd, lg, sm, gm, gex, le4, les, le2, mk1, mk2, csel):
                                P.op("sp", lambda e, t=t, xb=xb: e.dma_start(out=xb[:], in_=src.t[t * 128:(t + 1) * 128, :]),
                                     reads=[rsrc[t]], writes=[xb], dma=xb)
                                rms_mod(xb, A, B, hf, ssq, rstd, junk, tmp)

                                if dbg and l == 0:
                                    P.op("sp", lambda e, t=t: e.dma_start(out=dbg_h.t[t * 128:(t + 1) * 128, :], in_=hf[:]),
                                         reads=[hf], writes=[r_dbg], dma=hf)

                                def trf(e):
                                    r = None
                                    for kk in range(8):
                                        r = e.transpose(out=ptr[:, kk, :], in_=hf[:, kk * 128:(kk + 1) * 128], identity=identf[:])
                                    return r
                                P.op("pe", trf, reads=[hf, identf], writes=[ptr])
                                P.op("act", lambda e: e.copy(out=hTf[:], in_=ptr[:]), reads=[ptr], writes=[hTf])
                                P.op("dve", lambda e, ti=ti: e.tensor_copy(out=hT[:, :, ti * 128:(ti + 1) * 128], in_=hTf[:]),
                                     reads=[hTf], writes=[hT])

                                def mrt(e):
                                    r = None
                                    for kk in range(8):
                                        r = e.matmul(ptr[:, 0, 0:36], lhsT=hTf[:, kk, :], rhs=wrt[:, kk, :], start=(kk == 0), stop=(kk == 7))
                                    return r
                                P.op("pe", mrt, reads=[hTf, wrt], writes=[ptr])
                                P.op("dve", lambda e: e.tensor_tensor(out=lg[:], in0=ptr[:, 0, 0:36], in1=brt[:], op=ALU.add),
                                     reads=[ptr, brt], writes=[lg])
                                P.op("dve", lambda e: e.tensor_reduce(out=sm[:, 0:1], in_=lg[:, 0:4], axis=AX.X, op=ALU.max),
                                     reads=[lg], writes=[sm])
                                P.op("dve", lambda e: e.tensor_scalar(out=gm[:], in0=lg[:, 0:4], scalar1=sm[:, 0:1], scalar2=None, op0=ALU.is_equal),
                                     reads=[lg, sm], writes=[gm])
                                P.op("dve", lambda e: e.tensor_scalar(out=sm[:, 1:2], in0=sm[:, 0:1], scalar1=-1.0, scalar2=None, op0=ALU.mult),
                                     reads=[sm], writes=[sm])
                                P.op("act", lambda e: e.activation(out=gex[:], in_=lg[:, 0:4], func=AF.Exp, bias=sm[:, 1:2], accum_out=sm[:, 2:3]),
                                     reads=[lg, sm], writes=[gex, sm])
                                P.op("dve", lambda e: e.reciprocal(out=sm[:, 3:4], in_=sm[:, 2:3]), reads=[sm], writes=[sm])
                                P.op("dve", lambda e: e.tensor_tensor(out=le4[:], in0=lg[:, 4:36].rearrange("p (g e) -> p g e", g=4),
                                                                      in1=bcast(gm[:].unsqueeze(2), [128, 4, 8]), op=ALU.mult),
                                     reads=[lg, gm], writes=[le4])
                                P.op("dve", lambda e: e.tensor_reduce(out=les[:], in_=le4[:].rearrange("p g e -> p e g"), axis=AX.X, op=ALU.add),
                                     reads=[le4], writes=[les])
                                P.op("dve", lambda e: e.tensor_reduce(out=sm[:, 4:5], in_=les[:], axis=AX.X, op=ALU.max), reads=[les], writes=[sm])
                                P.op("dve", lambda e: e.tensor_scalar(out=mk1[:], in0=les[:], scalar1=sm[:, 4:5], scalar2=None, op0=ALU.is_equal),
                                     reads=[les, sm], writes=[mk1])
                                P.op("dve", lambda e: e.scalar_tensor_tensor(out=le2[:], in0=mk1[:], scalar=-1e30, in1=les[:], op0=ALU.mult, op1=ALU.add),
                                     reads=[mk1, les], writes=[le2])
                                P.op("dve", lambda e: e.tensor_reduce(out=sm[:, 5:6], in_=le2[:], axis=AX.X, op=ALU.max), reads=[le2], writes=[sm])
                                P.op("dve", lambda e: e.tensor_scalar(out=mk2[:], in0=le2[:], scalar1=sm[:, 5:6], scalar2=None, op0=ALU.is_equal),
                                     reads=[le2, sm], writes=[mk2])
                                P.op("dve", lambda e: e.tensor_tensor(out=sm[:, 6:7], in0=sm[:, 4:5], in1=sm[:, 5:6], op=ALU.subtract),
                                     reads=[sm], writes=[sm])
                                P.op("act", lambda e: e.activation(out=sm[:, 7:8], in_=sm[:, 6:7], func=AF.Sigmoid), reads=[sm], writes=[sm])
                                P.op("act", lambda e: e.activation(out=sm[:, 8:9], in_=sm[:, 6:7], func=AF.Sigmoid, scale=-1.0), reads=[sm], writes=[sm])
                                P.op("dve", lambda e: e.tensor_scalar(out=sm[:, 7:9], in0=sm[:, 7:9], scalar1=sm[:, 3:4], scalar2=None, op0=ALU.mult),
                                     reads=[sm], writes=[sm])
                                P.op("dve", lambda e: e.tensor_scalar(out=csel[:], in0=mk1[:], scalar1=sm[:, 7:8], scalar2=None, op0=ALU.mult),
                                     reads=[mk1, sm], writes=[csel])
                                P.op("dve", lambda e: e.scalar_tensor_tensor(out=csel[:], in0=mk2[:], scalar=sm[:, 8:9], in1=csel[:], op0=ALU.mult, op1=ALU.add),
                                     reads=[mk2, sm, csel], writes=[csel])
                                P.op("dve", lambda e, ti=ti: e.tensor_tensor(out=coef[:, ti, :].rearrange("p (g e) -> p g e", g=4),
                                                                             in0=bcast(gm[:].unsqueeze(2), [128, 4, 8]),
                                                                             in1=bcast(csel[:].unsqueeze(1), [128, 4, 8]), op=ALU.mult),
                                     reads=[gm, csel], writes=[coef])

                        for ti in range(SBT):
                            t = sb0 + ti
                            router_tile(t, ti, xt[t % 2], R_hf.at(t), R_tmp.at(t), R_junk.at(t), R_hTf.at(t), R_ssq.at(t), R_rstd.at(t), R_lg.at(t), R_sm.at(t), R_gm.at(t), R_gex.at(t), R_le4.at(t), R_les.at(t), R_le2.at(t), R_mk1.at(t), R_mk2.at(t), R_csel.at(t))
                        nblk = (SBT * 128 + 511) // 512
                        seq = [(ex, blk) for ex in range(NEXP) for blk in range(nblk)]

                        def load_w(ex):
                            wi = ex % 2
                            h.dma("pool", wg[wi][:], w_gate.t[l, ex].rearrange("(k p) n -> p k n", p=128), [], [wg[wi]], wg[wi])
                            h.dma("pool", wu[wi][:], w_up.t[l, ex].rearrange("(k p) n -> p k n", p=128), [], [wu[wi]], wu[wi])
                            h.dma("pool", wd[wi][:], w_down.t[l, ex].rearrange("(k p) n -> p k n", p=128), [], [wd[wi]], wd[wi])

                        def GU(i):
                            ex, blk = seq[i]
                            wi, bi = ex % 2, i % 2
                            c0 = blk * 512
                            cw = min(512, SBT * 128 - c0)
                            for fc in range(2):
                                fs = slice(fc * 128, (fc + 1) * 128)
                                h.mm([(pg[fc][:, 0:cw], wg[wi][:, kk, fs], hT[:, kk, c0:c0 + cw], kk == 0, kk == 7) for kk in range(8)],
                                     [wg[wi], hT], [pg[fc]])
                                h.act(sg[fc][:, 0:cw], pg[fc][:, 0:cw], AF.Silu, [pg[fc]], [sg[fc]])
                                yield
                                h.mm([(pu[fc][:, 0:cw], wu[wi][:, kk, fs], hT[:, kk, c0:c0 + cw], kk == 0, kk == 7) for kk in range(8)],
                                     [wu[wi], hT], [pu[fc]])
                                h.tt("dve", hid[bi][:, fc, 0:cw], sg[fc][:, 0:cw], pu[fc][:, 0:cw], ALU.mult, [sg[fc], pu[fc]], [hid[bi]])
                                yield

                        def DN(i):
                            ex, blk = seq[i]
                            wi, bi = ex % 2, i % 2
                            c0 = blk * 512
                            cw = min(512, SBT * 128 - c0)
                            for st in range(cw // 128):
                                ti = blk * 4 + st
                                for n2 in range(2):
                                    pi = n2
                                    ns = slice(n2 * 512, (n2 + 1) * 512)
                                    h.mm([(py[pi][:], hid[bi][:, fc, st * 128:(st + 1) * 128], wd[wi][:, fc, ns], fc == 0, fc == 1) for fc in range(2)],
                                         [hid[bi], wd[wi]], [py[pi]])
                                    if ex == 0:
                                        h.ts("dve", yacc[ti][:, ns], py[pi][:], coef[:, ti, ex:ex + 1], None, ALU.mult, None, [py[pi], coef], [yacc[ti]])
                                    else:
                                        h.stt("dve", yacc[ti][:, ns], py[pi][:], coef[:, ti, ex:ex + 1], yacc[ti][:, ns], ALU.mult, ALU.add,
                                              [py[pi], coef, yacc[ti]], [yacc[ti]])
                                    yield
                            if blk == nblk - 1 and ex + 2 < NEXP:
                                load_w(ex + 2)

                        def drain(g):
                            for _ in g:
                                pass

                        def step(g, n):
                            for _ in range(n):
                                if next(g, "END") == "END":
                                    return

                        load_w(0)
                        load_w(1)
                        drain(GU(0))
                        for i in range(len(seq)):
                            gd = DN(i)
                            if i + 1 < len(seq):
                                gg = GU(i + 1)
                                for _ in range(4):
                                    step(gg, 1)
                                    step(gd, 2)
                                drain(gg)
                            drain(gd)
                        for ti in range(SBT):
                            t = sb0 + ti
                            if dbg and l == 0:
                                P.op("sp", lambda e, t=t, ti=ti: e.dma_start(out=dbg_y.t[t * 128:(t + 1) * 128, :], in_=yacc[ti][:]),
                                     reads=[yacc[ti]], writes=[r_dbg], dma=yacc[ti])
                                P.op("sp", lambda e, t=t, ti=ti: e.dma_start(out=dbg_coef.t[t * 128:(t + 1) * 128, :], in_=coef[:, ti, :]),
                                     reads=[coef], writes=[r_dbg], dma=coef)
                            xb = xt[t % 2]
                            P.op("sp", lambda e, t=t, xb=xb: e.dma_start(out=xb[:], in_=src.t[t * 128:(t + 1) * 128, :]),
                                 reads=[rsrc[t]], writes=[xb], dma=xb)
                            P.op("pool", lambda e, ti=ti: e.tensor_tensor(out=yacc[ti][:], in0=yacc[ti][:], in1=G[:], op=ALU.mult),
                                 reads=[yacc[ti], G], writes=[yacc[ti]])
                            P.op("dve", lambda e, ti=ti, xb=xb: e.tensor_tensor(out=yacc[ti][:], in0=yacc[ti][:], in1=xb[:], op=ALU.add),
                                 reads=[yacc[ti], xb], writes=[yacc[ti]])
                            P.op("sp", lambda e, t=t, ti=ti: e.dma_start(out=dst.t[t * 128:(t + 1) * 128, :], in_=yacc[ti][:]),
                                 reads=[yacc[ti]], writes=[rdst[t]], dma=yacc[ti])
                    P.end_phase()
                cur = (dst, rdst)

        src, rsrc = cur
        with contextlib.ExitStack() as es:
            Wn = P.sb(es, "Wn", [128, D], F32)
            load_row(Wn, norm_final.t[0:1, :])
            xt = [P.sb(es, f"xtn{i}", [128, D], F32) for i in range(2)]
            ho = [P.sb(es, f"hon{i}", [128, D], F32) for i in range(2)]
            junk = P.sb(es, "junkn", [128, D], F32)
            ssq = P.sb(es, "ssqn", [128, 1], F32)
            rstd = P.sb(es, "rstdn", [128, 1], F32)
            for t in range(NT):
                xb = xt[t % 2]
                hb = ho[t % 2]
                P.op("sp", lambda e, t=t, xb=xb: e.dma_start(out=xb[:], in_=src.t[t * 128:(t + 1) * 128, :]),
                     reads=[rsrc[t]], writes=[xb], dma=xb)
                rms_mod(xb, Wn, None, hb, ssq, rstd, junk, None)
                P.op("sp", lambda e, t=t, hb=hb: e.dma_start(out=out.t[t * 128:(t + 1) * 128, :], in_=hb[:]),
                     reads=[hb], writes=[r_out[t]], dma=hb)
            P.final_wait("sp", r_out)
            P.end_phase()
    return nc


def host_inputs(inputs, L, T):
    f = lambda a: np.ascontiguousarray(np.asarray(a, dtype=np.float32))
    w_rt = f(np.concatenate([inputs["moe_w_grp"][:L], inputs["moe_w_rt"][:L]], axis=-1))
    b_rt = f(np.concatenate([inputs["moe_b_grp"][:L], inputs["moe_b_rt"][:L]], axis=-1))
    w_in = np.asarray(inputs["w_in"][:L], dtype=np.float32)
    w_in_f = np.concatenate([w_in[:, :, 0:1408], w_in[:, :, 2188:3084]], axis=-1)
    w_in_t = np.concatenate([w_in[:, :, 1408:1792], w_in[:, :, 1804:2188], w_in[:, :, 1792:1798],
                             w_in[:, :, 3084:3090], w_in[:, :, 1798:1804]], axis=-1)
    gcw = np.asarray(inputs["gdn_conv_w"][:L], dtype=np.float32)
    scw = np.asarray(inputs["ssd_conv_w"][:L], dtype=np.float32)
    cw = np.concatenate([gcw, scw], axis=-1)
    conv_w = cw.reshape(L, 4, 16, 128).transpose(0, 3, 2, 1)
    cb = np.concatenate([np.zeros((L, 1152), np.float32), np.asarray(inputs["ssd_conv_b"][:L], dtype=np.float32)], axis=-1)
    conv_b = cb.reshape(L, 16, 128).transpose(0, 2, 1)
    bias12 = np.concatenate([inputs["gdn_dt_bias"][:L], inputs["ssd_dt_bias"][:L]], axis=-1)
    alog12 = np.concatenate([inputs["gdn_a_log"][:L], inputs["ssd_a_log"][:L]], axis=-1)
    def st_layout(a):
        a = np.asarray(a[:L], dtype=np.float32)
        return a.reshape(L, 8, 2, 64).transpose(0, 2, 3, 1).reshape(L, 128, 8)
    ldt = np.repeat(np.asarray(inputs["s5_log_dt"][:L], dtype=np.float32)[:, :, None], 64, axis=2)
    def bT_layout(b):
        b = np.asarray(b[:L], dtype=np.float32)
        o = np.zeros((L, 8, 128, 128), np.float32)
        for sc in range(8):
            for gl in range(2):
                r0 = 32 * (sc % 4) + 16 * gl
                o[:, sc, r0:r0 + 16, gl * 64:(gl + 1) * 64] = b[:, 2 * sc + gl].transpose(0, 2, 1)
        return o
    def cT_layout(c):
        c = np.asarray(c[:L], dtype=np.float32)
        o = np.zeros((L, 8, 128, 128), np.float32)
        for sc in range(8):
            for gl in range(2):
                r0 = 32 * (sc % 4) + 16 * gl
                o[:, sc, gl * 64:(gl + 1) * 64, r0:r0 + 16] = c[:, 2 * sc + gl].transpose(0, 2, 1)
        return o
    ii = np.arange(128)[:, None]
    jj = np.arange(128)[None, :]
    cm = []
    for lv in range(7):
        s_ = 1 << lv
        cm.append(((ii // (2 * s_) == jj // (2 * s_)) & (ii % (2 * s_) >= s_) & (jj % (2 * s_) < s_)).astype(np.float32))
    cmask = np.stack(cm + [m.T for m in cm], axis=0)
    col2 = lambda a: np.asarray(a[:L], dtype=np.float32).reshape(L, 2, 128).transpose(0, 2, 1)
    shared = {
        "gdn_cmask": f(cmask),
        "s5_are": f(st_layout(inputs["s5_a_re"])), "s5_aim": f(st_layout(inputs["s5_a_im"])), "s5_ldt": f(st_layout(ldt)),
        "s5_dcol": f(col2(inputs["s5_d"])), "s5_ncol": f(col2(inputs["s5_norm"])),
        "s5_bT_re": f(bT_layout(inputs["s5_b_re"])), "s5_bT_im": f(bT_layout(inputs["s5_b_im"])),
        "s5_cT_re": f(cT_layout(inputs["s5_c_re"])), "s5_cT_im": f(cT_layout(inputs["s5_c_im"])),
        "s5_w_glu": f(inputs["s5_w_glu"][:L]),
        "w_in_f": f(w_in_f), "w_in_t": f(w_in_t), "w_out": f(inputs["w_out"][:L]),
        "conv_w": f(conv_w), "conv_b": f(conv_b), "bias12": f(bias12), "alog12": f(alog12),
        "ssd_d": f(inputs["ssd_d"][:L]), "ssd_norm": f(inputs["ssd_norm"][:L]), "gdn_norm": f(inputs["gdn_norm"][:L]),
        "w_ada": f(inputs["w_ada"][:L]), "b_ada": f(inputs["b_ada"][:L]),
        "norm_mix": f(inputs["norm_mix"][:L]), "norm_ffn": f(inputs["norm_ffn"][:L]),
        "norm_final": f(inputs["norm_final"]).reshape(1, D),
        "w_rt": w_rt, "b_rt": b_rt,
        "moe_w_gate": f(inputs["moe_w_gate"][:L]), "moe_w_up": f(inputs["moe_w_up"][:L]),
        "moe_w_down": f(inputs["moe_w_down"][:L]),
    }
    maps = []
    B = inputs["x"].shape[0]
    for b in range(B):
        m = dict(shared)
        m["x"] = f(inputs["x"][b, :T])
        m["c"] = f(inputs["c"][b]).reshape(1, D)
        maps.append(m)
    return maps


def run(inputs, L, T, flags=None, trace=False):
    nc = build(T, L, flags)
    maps = host_inputs(inputs, L, T)
    res = run_bass_kernel_spmd(nc, maps, core_ids=list(range(len(maps))))
    if flags and flags.get("dbg"):
        return res.results
    return np.stack([r["out"] for r in res.results], axis=0)


def kernel(**inputs):
    return run(inputs, 4, 4096).astype(np.float32)
```

```python
import contextlib
import math
import numpy as np
import concourse.bass as bass
import concourse.mybir as mybir
from concourse.bass_utils import run_bass_kernel_spmd

F32 = mybir.dt.float32
BF16 = mybir.dt.bfloat16
ALU = mybir.AluOpType
AF = mybir.ActivationFunctionType
AX = mybir.AxisListType

D = 1024
NEXP = 32
DEXP = 256
EPS = 1e-6
ENGS = ("pe", "act", "dve", "pool", "sp")


class Buf:
    def __init__(self, t, name, multi=False):
        self.t = t
        self.name = name
        self.w = {}
        self.r = {}
        self.sem = None
        self.dcnt = 0
        self.multi = multi

    def __getitem__(self, k):
        return self.t[k]


class Prog:
    SEM_LAT = 0.15

    def __init__(self, nc, es):
        self.nc = nc
        self.es = es
        self.ops = []
        self.sems = []
        self.esem = {}
        self.ecnt = {e: 0 for e in ENGS}
        self.waited = {e: {} for e in ENGS}
        for e in ENGS:
            if e != "sp":
                self.esem[e] = self.newsem("e_" + e)
        self.uid = 0
        self.dsem_pool = []
        self.dbufs = []
        self.phase_bufs = []

    def newsem(self, name):
        s = self.es.enter_context(self.nc.semaphore(name))
        self.sems.append(s)
        return len(self.sems) - 1

    def sb(self, es, name, shape, dtype):
        self.uid += 1
        name = f"{name}_{self.uid}"
        t = es.enter_context(self.nc.sbuf_tensor(name, list(shape), dtype))
        b = Buf(t, name)
        self.phase_bufs.append(b)
        return b

    def ps(self, es, name, shape, dtype):
        self.uid += 1
        name = f"{name}_{self.uid}"
        t = es.enter_context(self.nc.psum_tensor(name, list(shape), dtype))
        return Buf(t, name)

    def dram(self, name, shape, dtype, kind="Internal"):
        t = self.nc.dram_tensor(name, list(shape), dtype, kind=kind).ap()
        return Buf(t, name)

    def region(self, name):
        return Buf(None, name, multi=True)

    def op(self, eng, fn, reads=(), writes=(), dma=None, est=None):
        if est is None:
            est = {"pe": 1.0, "act": 0.5, "dve": 0.35, "pool": 0.45, "sp": 3.0}[eng] if dma is None else 3.0
        self.ops.append((eng, fn, tuple(reads), tuple(writes), dma, est))

    def final_wait(self, eng, bufs):
        pass

    def end_phase(self):
        ops = self.ops
        self.ops = []
        n = len(ops)
        lw, rd = {}, {}
        deps = [None] * n
        for i, (eng, fn, reads, writes, dma, est) in enumerate(ops):
            d = set()
            for b in reads:
                d.update(lw.get(id(b), ()))
            for b in writes:
                d.update(rd.get(id(b), ()))
                if not b.multi:
                    d.update(lw.get(id(b), ()))
            deps[i] = d
            for b in reads:
                rd.setdefault(id(b), []).append(i)
            for b in writes:
                if b.multi:
                    lw.setdefault(id(b), []).append(i)
                else:
                    lw[id(b)] = [i]
                    rd[id(b)] = []
        import heapq
        succ = [[] for _ in range(n)]
        indeg = [0] * n
        for i in range(n):
            indeg[i] = len(deps[i])
            for j in deps[i]:
                succ[j].append(i)
        ready_t = [0.0] * n
        fin = [0.0] * n
        start = [0.0] * n
        efree = {e: 0.0 for e in ENGS}
        heap = [(0.0, i) for i in range(n) if indeg[i] == 0]
        heapq.heapify(heap)
        order = {e: [] for e in ENGS}
        glob = []
        while heap:
            rt, i = heapq.heappop(heap)
            eng, fn, reads, writes, dma, est = ops[i]
            st = max(rt, efree[eng])
            start[i] = st
            if dma is not None:
                occ = 0.5 if eng == "pool" else 0.08
                efree[eng] = st + occ
                fin[i] = st + occ + est
            else:
                efree[eng] = st + est
                fin[i] = st + est
            order[eng].append(i)
            glob.append(i)
            for k2 in succ[i]:
                indeg[k2] -= 1
                if ready_t[k2] < fin[i] + self.SEM_LAT:
                    ready_t[k2] = fin[i] + self.SEM_LAT
                if indeg[k2] == 0:
                    heapq.heappush(heap, (ready_t[k2], k2))
        assert len(glob) == n, "dependency cycle"
        tok = [None] * n
        for e in ENGS:
            if e == "sp":
                continue
        waits_raw = [None] * n
        cnt = dict(self.ecnt)
        for i in glob:
            eng, fn, reads, writes, dma, est = ops[i]
            w = {}
            for j in deps[i]:
                dj = ops[j][4]
                if dj is None:
                    s_, v_ = tok[j]
                else:
                    s_, v_ = dj.sem, dj.dcnt
                if w.get(s_, 0) < v_:
                    w[s_] = v_
            waits_raw[i] = w
            if dma is None:
                cnt[eng] += 1
                tok[i] = (self.esem[eng], cnt[eng])
            else:
                if dma.sem is None:
                    if self.dsem_pool:
                        dma.sem, dma.dcnt = self.dsem_pool.pop()
                    else:
                        dma.sem = self.newsem("d_" + dma.name)
                        dma.dcnt = 0
                    self.dbufs.append(dma)
                dma.dcnt += 16
                tok[i] = (dma.sem, dma.dcnt)
        self.ecnt = cnt
        nc = self.nc
        sems = self.sems
        qs = {}
        for e in ENGS:
            wd = self.waited[e]
            q = []
            for i in order[e]:
                ws = []
                for s_, v_ in waits_raw[i].items():
                    if wd.get(s_, 0) < v_:
                        ws.append((s_, v_))
                        wd[s_] = v_
                inc = (tok[i][0], 16 if ops[i][4] is not None else 1)
                q.append((ws, ops[i][1], inc))
            qs[e] = q
        toks = {}
        for e, s_ in self.esem.items():
            if self.ecnt[e] > 0:
                toks[s_] = self.ecnt[e]
        for b in self.dbufs:
            toks[b.sem] = max(toks.get(b.sem, 0), b.dcnt)
        for e in ENGS:
            wd = self.waited[e]
            ws = []
            for s_, v_ in toks.items():
                if wd.get(s_, 0) < v_:
                    ws.append((s_, v_))
                    wd[s_] = v_
            if ws:
                qs[e].append((ws, None, None))
        for b in self.phase_bufs:
            if b.sem is not None:
                self.dsem_pool.append((b.sem, b.dcnt))
                self.dbufs.remove(b)
                b.sem = None
        self.phase_bufs = []

        def mk(e):
            def f(eng):
                for waits, fn, inc in qs[e]:
                    for s_, v_ in waits:
                        eng.wait_ge(sems[s_], v_)
                    if fn is None:
                        continue
                    ins = fn(eng)
                    ins.then_inc(sems[inc[0]], inc[1])
            return f

        with nc.Block() as block:
            block.sync(mk("sp"))
            block.scalar(mk("act"))
            block.vector(mk("dve"))
            block.gpsimd(mk("pool"))
            block.tensor(mk("pe"))
        self.last_makespan = max(efree.values()) if n else 0.0
        if getattr(self, "verbose", False):
            busy = {e: 0.0 for e in ENGS}
            for i in range(n):
                if ops[i][4] is None:
                    busy[ops[i][0]] += ops[i][5]
            print(f"[phase] n_ops={n} model_makespan={self.last_makespan:.0f}us busy=" +
                  " ".join(f"{e}:{busy[e]:.0f}" for e in ENGS), flush=True)


def _fsz(ap):
    n = 1
    for d in ap.shape[1:]:
        n *= int(d)
    return n


def _est(eng, ap, psum=False):
    n = _fsz(ap)
    if eng == "dve":
        return (60 + n) / 960.0 + (0.06 if psum else 0.0)
    if eng == "act":
        return (220 + n) / 1400.0
    if eng == "pool":
        return (120 + n) / 900.0
    return 0.5


class H:
    def __init__(self, P):
        self.P = P

    def dma(self, eng, out, in_, R, W, buf, slow=False):
        nbytes = _fsz(out) * 128 * 4
        est = 2.0 + nbytes / 150000.0
        if slow:
            self.P.op(eng, lambda e: e.dma_start(out=out, in_=in_, allow_slow_non_contiguous=True), reads=R, writes=W, dma=buf, est=est)
        else:
            self.P.op(eng, lambda e: e.dma_start(out=out, in_=in_), reads=R, writes=W, dma=buf, est=est)

    def tt(self, eng, out, in0, in1, op, R, W):
        self.P.op(eng, lambda e: e.tensor_tensor(out=out, in0=in0, in1=in1, op=op), reads=R, writes=W, est=_est(eng, out))

    def ts(self, eng, out, in0, s1, s2, op0, op1, R, W):
        if s2 is None:
            self.P.op(eng, lambda e: e.tensor_scalar(out=out, in0=in0, scalar1=s1, scalar2=None, op0=op0), reads=R, writes=W, est=_est(eng, out))
        else:
            self.P.op(eng, lambda e: e.tensor_scalar(out=out, in0=in0, scalar1=s1, scalar2=s2, op0=op0, op1=op1), reads=R, writes=W, est=_est(eng, out))

    def stt(self, eng, out, in0, sc, in1, op0, op1, R, W):
        eng = "dve"
        self.P.op(eng, lambda e: e.scalar_tensor_tensor(out=out, in0=in0, scalar=sc, in1=in1, op0=op0, op1=op1), reads=R, writes=W, est=_est(eng, out))

    def act(self, out, in_, func, R, W, bias=None, scale=None, accum=None):
        kw = {}
        if bias is not None:
            kw["bias"] = bias
        if scale is not None:
            kw["scale"] = scale
        if accum is not None:
            kw["accum_out"] = accum
        self.P.op("act", lambda e: e.activation(out=out, in_=in_, func=func, **kw), reads=R, writes=W, est=_est("act", out))

    def cp(self, eng, out, in_, R, W):
        if eng == "act":
            self.P.op("act", lambda e: e.copy(out=out, in_=in_), reads=R, writes=W, est=_est("act", out))
        else:
            self.P.op(eng, lambda e: e.tensor_copy(out=out, in_=in_), reads=R, writes=W, est=_est(eng, out))

    def memset(self, eng, ap, val, W):
        self.P.op(eng, lambda e: e.memset(ap, val), writes=W, est=_est(eng, ap))

    def recip(self, out, in_, R, W):
        self.P.op("dve", lambda e: e.reciprocal(out=out, in_=in_), reads=R, writes=W, est=_est("dve", out))

    def reduce(self, out, in_, op, R, W):
        self.P.op("dve", lambda e: e.tensor_reduce(out=out, in_=in_, axis=AX.X, op=op), reads=R, writes=W, est=_est("dve", in_))

    def mm(self, items, R, W):
        est = 0.0
        for (o, l, rh, st, sp) in items:
            est += (max(64, _fsz(rh)) * (4 if l.dtype == F32 else 1)) / 2400.0 + 0.01
        est += 0.06

        def f(e):
            r = None
            for (o, l, rh, st, sp) in items:
                r = e.matmul(o, lhsT=l, rhs=rh, start=st, stop=sp)
            return r
        self.P.op("pe", f, reads=R, writes=W, est=est)

    def tr(self, items, ident, R, W):
        est = 0.06
        for (o, i) in items:
            est += (128 * (4 if i.dtype == F32 else 1)) / 2400.0 + 0.03

        def f(e):
            r = None
            for (o, i) in items:
                r = e.transpose(out=o, in_=i, identity=ident)
            return r
        self.P.op("pe", f, reads=R, writes=W, est=est)

    def select(self, out, in_, cmp, fill, base, cm, pattern, R, W):
        self.P.op("pool", lambda e: e.affine_select(out=out, in_=in_, pattern=pattern, compare_op=cmp, fill=fill,
                                                    base=base, channel_multiplier=cm), reads=R, writes=W, est=_est("pool", out))


class Rot:
    def __init__(self, P, es, name, shape, dtype, n=2):
        self.bufs = [P.sb(es, f"{name}r{i}", shape, dtype) for i in range(n)]

    def at(self, i):
        return self.bufs[i % len(self.bufs)]


def b3(ap, n, m):
    return ap.unsqueeze(1).to_broadcast([128, n, m])


def s3(ap, n, m):
    return ap.unsqueeze(2).to_broadcast([128, n, m])


def s4(ap):
    return ap.rearrange("p (b h) -> p b h", b=2).unsqueeze(3).to_broadcast([128, 2, 3, 128])


def v4(ap):
    return ap.rearrange("p (b h) l -> p b h l", b=2)


def w4(ps):
    return ps[:, :, 0:384].rearrange("p b (h l) -> p b h l", h=3)


def softplus12(h, P, es, tagp):
    xa = P.sb(es, tagp + "xa", [128, 12], F32)
    ax = P.sb(es, tagp + "ax", [128, 12], F32)
    ex = P.sb(es, tagp + "ex", [128, 12], F32)
    ln = P.sb(es, tagp + "ln", [128, 12], F32)
    one = P.sb(es, tagp + "one", [128, 1], F32)
    h.memset("pool", one[:], 1.0, [one])

    def f(xin, xin_buf, bias, out):
        h.tt("dve", xa[:], xin, bias[:], ALU.add, [xin_buf, bias], [xa])
        h.act(ax[:], xa[:], AF.Abs, [xa], [ax])
        h.act(ex[:], ax[:], AF.Exp, [ax], [ex], scale=-1.0)
        h.act(ln[:], ex[:], AF.Ln, [ex, one], [ln], bias=one[:, 0:1])
        h.ts("dve", xa[:], xa[:], 0.0, None, ALU.max, None, [xa], [xa])
        h.tt("dve", out[:], xa[:], ln[:], ALU.add, [xa, ln], [out])
    return f


def make_masks(h, P, es):
    m = {}
    ones = P.sb(es, "m_ones", [128, 128], F32)
    h.memset("pool", ones[:], 1.0, [ones])
    m["ones"] = ones
    for name, cmp, cm, st in (("U", ALU.is_ge, -1, 1), ("L", ALU.is_ge, 1, -1), ("Ls", ALU.is_gt, 1, -1)):
        t = P.sb(es, "m_" + name, [128, 128], F32)
        h.select(t[:], ones[:], cmp, 0.0, 0, cm, [[st, 128]], [ones], [t])
        m[name] = t
    sel = P.sb(es, "m_sel", [128, 128], F32)
    zer = P.sb(es, "m_zero", [128, 128], F32)
    h.memset("pool", zer[:], 0.0, [zer])
    h.select(sel[:], zer[:], ALU.not_equal, 1.0, -127, 1, [[0, 128]], [zer], [sel])
    m["sel"] = sel
    bd = P.sb(es, "m_bd", [128, 128], F32)
    h.memset("pool", bd[:], 0.0, [bd])
    h.memset("pool", bd[0:64, 0:64], 1.0, [bd])
    h.memset("pool", bd[64:128, 64:128], 1.0, [bd])
    m["bd"] = bd
    return m


def mixer_layer(k, l, cur, dstt):
    P, T, flags, h = k.P, k.T, k.flags, k.h
    inp = k.inp
    src, rsrc = cur
    dst, rdst = dstt
    NMT = T // 512
    MT = 512
    identf, identb = k.identf, k.identb
    u_d, qn_d, kn_d, v_d, xs_d, B_d, C_d, pt_d, y_d = k.u_d, k.qn_d, k.kn_d, k.v_d, k.xs_d, k.B_d, k.C_d, k.pt_d, k.y_d
    r_pre, r_y = k.r_pre, k.r_y

    with contextlib.ExitStack() as es:
        A, B = k.norm_consts(es, l, inp["norm_mix"].t[l:l + 1, :], 1, 0, "m")
        winf = P.sb(es, "winf", [128, 8, 2304], BF16)
        wint = P.sb(es, "wint", [128, 8, 786], BF16)
        for (c0, c1) in ((0, 1152), (1152, 2304)):
            h.dma("pool", winf[:, :, c0:c1], inp["w_in_f"].t[l, :, c0:c1].rearrange("(k p) n -> p k n", p=128), [], [winf], winf)
        h.dma("pool", wint[:], inp["w_in_t"].t[l].rearrange("(k p) n -> p k n", p=128), [], [wint], wint)
        cwt = P.sb(es, "cwt", [128, 16, 4], F32)
        cbt = P.sb(es, "cbt", [128, 16], F32)
        h.dma("sp", cwt[:], inp["conv_w"].t[l], [], [cwt], cwt)
        h.dma("sp", cbt[:], inp["conv_b"].t[l], [], [cbt], cbt)
        carry = P.sb(es, "carry", [128, 16, 3], F32)
        h.memset("pool", carry[:], 0.0, [carry])
        epsb = P.sb(es, "epsb", [128, 1], F32)
        h.memset("pool", epsb[:], EPS, [epsb])
        mhalf = P.sb(es, "mhalf", [128, MT], F32)
        h.memset("pool", mhalf[:], -0.5, [mhalf])
        bones = P.sb(es, "bones", [128, 128], F32)
        h.memset("pool", bones[:], 0.0, [bones])
        h.memset("pool", bones[0:64, 0:64], 1.0, [bones])
        h.memset("pool", bones[64:128, 64:128], 1.0, [bones])
        xt = [P.sb(es, f"m1x{i}", [128, D], F32) for i in range(2)]
        tmp = P.sb(es, "m1tmp", [128, D], F32)
        hb = P.sb(es, "m1hb", [128, D], BF16)
        hT = P.sb(es, "m1hT", [128, 8, MT], BF16)
        cin = [P.sb(es, f"cin{i}", [128, MT + 3], F32) for i in range(2)]
        acc = [P.sb(es, f"acc{i}", [128, MT], F32) for i in range(2)]
        so = [P.sb(es, f"so{i}", [128, MT], F32) for i in range(2)]
        sob = [P.sb(es, f"sob{i}", [128, MT], BF16) for i in range(2)]
        sq = P.sb(es, "m1sq", [128, MT], F32)
        rinv = P.sb(es, "m1rinv", [128, MT], F32)
        ptst = [P.sb(es, f"ptst{i}", [128, 786], F32) for i in range(2)]
        ssq = P.sb(es, "m1ssq", [128, 1], F32)
        rstd = P.sb(es, "m1rstd", [128, 1], F32)
        ptr = P.ps(es, "m1ptr", [128, 8, 128], BF16)
        pp = [P.ps(es, f"m1pp{i}", [128, MT], F32) for i in range(2)]
        pt = P.ps(es, "m1pt", [128, 2, 512], F32)
        pq = P.ps(es, "m1pq", [128, MT], F32)
        for mt in range(NMT):
            cols = slice(mt * MT, (mt + 1) * MT)
            for ti in range(4):
                t = mt * 4 + ti
                xb = xt[t % 2]
                h.dma("sp", xb[:], src.t[t * 128:(t + 1) * 128, :], [rsrc[t]], [xb], xb)
                k.rms_mod(xb, A, B, hb, ssq, rstd, tmp, tmp)
                h.tr([(ptr[:, kk, :], hb[:, kk * 128:(kk + 1) * 128]) for kk in range(8)], identb[:], [hb, identb], [ptr])
                h.cp("act", hT[:, :, ti * 128:(ti + 1) * 128], ptr[:], [ptr], [hT])
            for c in range(18):
                pb = pp[c % 2]
                h.mm([(pb[:], winf[:, kk, c * 128:(c + 1) * 128], hT[:, kk, :], kk == 0, kk == 7) for kk in range(8)], [winf, hT], [pb])
                if c < 2:
                    sb_ = so[c % 2]
                    h.cp("act", sb_[:], pb[:], [pb], [sb_])
                    h.dma("sp", u_d.t[c * 128:(c + 1) * 128, cols], sb_[:], [sb_], [r_pre[mt]], sb_)
                    continue
                ci = c - 2
                cb_ = cin[ci % 2]
                h.cp("pool", cb_[:, 0:3], carry[:, ci, :], [carry], [cb_])
                h.cp("act", cb_[:, 3:MT + 3], pb[:], [pb], [cb_])
                h.cp("pool", carry[:, ci, :], cb_[:, MT:MT + 3], [cb_], [carry])
                ab = acc[ci % 2]
                h.ts("dve", ab[:], cb_[:, 0:MT], cwt[:, ci, 0:1], None, ALU.mult, None, [cb_, cwt], [ab])
                for j in range(1, 4):
                    h.stt("dve", ab[:], cb_[:, j:j + MT], cwt[:, ci, j:j + 1], ab[:], ALU.mult, ALU.add, [cb_, cwt, ab], [ab])
                sb_ = so[ci % 2]
                h.act(sb_[:], ab[:], AF.Silu, [ab, cbt], [sb_], bias=cbt[:, ci:ci + 1])
                if ci < 6:
                    h.tt("pool", sq[:], sb_[:], sb_[:], ALU.mult, [sb_], [sq])
                    h.mm([(pq[:], bones[:], sq[:], True, True)], [bones, sq], [pq])
                    h.act(rinv[:], pq[:], AF.Ln, [pq, epsb], [rinv], bias=epsb[:, 0:1])
                    h.act(rinv[:], rinv[:], AF.Exp, [rinv], [rinv], scale=-0.5)
                    ob = sob[ci % 2]
                    if ci < 3:
                        h.stt("dve", ob[:], sb_[:], 0.125, rinv[:], ALU.mult, ALU.mult, [sb_, rinv], [ob])
                    else:
                        h.tt("dve", ob[:], sb_[:], rinv[:], ALU.mult, [sb_, rinv], [ob])
                    dd = qn_d if ci < 3 else kn_d
                    j = ci % 3
                    h.dma("sp", dd.t[j * 128:(j + 1) * 128, cols], ob[:], [ob], [r_pre[mt]], ob)
                elif ci < 12:
                    dd = v_d if ci < 9 else xs_d
                    j = (ci - 6) % 3
                    h.dma("sp", dd.t[j * 128:(j + 1) * 128, cols], sb_[:], [sb_], [r_pre[mt]], sb_)
                else:
                    ob = sob[ci % 2]
                    h.cp("pool", ob[:], sb_[:], [sb_], [ob])
                    dd = B_d if ci < 14 else C_d
                    j = (ci - 12) % 2
                    h.dma("sp", dd.t[j * 128:(j + 1) * 128, cols], ob[:], [ob], [r_pre[mt]], ob)
            for ti in range(4):
                t = mt * 4 + ti
                tc = slice(ti * 128, (ti + 1) * 128)
                h.mm([(pt[:, 0, :], hT[:, kk, tc], wint[:, kk, 0:512], kk == 0, kk == 7) for kk in range(8)]
                     + [(pt[:, 1, 0:274], hT[:, kk, tc], wint[:, kk, 512:786], kk == 0, kk == 7) for kk in range(8)],
                     [hT, wint], [pt])
                stg = ptst[t % 2]
                h.cp("act", stg[:, 0:512], pt[:, 0, :], [pt], [stg])
                h.cp("dve", stg[:, 512:786], pt[:, 1, 0:274], [pt], [stg])
                h.dma("sp", pt_d.t[t * 128:(t + 1) * 128, :], stg[:], [stg], [r_pre[mt]], stg)
        P.end_phase()

    if flags.get("s5", True):
        s5_phase(k, l)
    if flags.get("gdn", True):
        gdn_phase(k, l)
    if flags.get("ssd", True):
        ssd_phase(k, l)

    with contextlib.ExitStack() as es:
        wout = P.sb(es, "wout", [128, 8, D], BF16)
        h.dma("pool", wout[:], inp["w_out"].t[l].rearrange("(k p) n -> p k n", p=128), [], [wout], wout)
        G = P.sb(es, "Gm", [128, D], F32)
        k.load_row(G, k.modv.t[l:l + 1, 2 * D:3 * D], [k.r_modv])
        yT = [P.sb(es, f"m5y{i}", [128, 8, MT], BF16) for i in range(2)]
        xt = [P.sb(es, f"m5x{i}", [128, D], F32) for i in range(2)]
        tm = [P.sb(es, f"m5t{i}", [128, D], F32) for i in range(2)]
        po = [P.ps(es, f"m5p{i}", [128, 512], F32) for i in range(4)]
        for mt in range(NMT):
            cols = slice(mt * MT, (mt + 1) * MT)
            yb = yT[mt % 2]
            h.dma("sp", yb[:], y_d.t[:, cols].rearrange("(k p) t -> p k t", p=128), [r_y[0][mt], r_y[1][mt], r_y[2][mt]], [yb], yb)
            if not flags.get("s5", True):
                h.memset("pool", yb[:, 0:2, :], 0.0, [yb])
            if not flags.get("gdn", True):
                h.memset("pool", yb[:, 2:5, :], 0.0, [yb])
            if not flags.get("ssd", True):
                h.memset("pool", yb[:, 5:8, :], 0.0, [yb])
            for ti in range(4):
                t = mt * 4 + ti
                tc = slice(ti * 128, (ti + 1) * 128)
                xb = xt[t % 2]
                tb = tm[t % 2]
                h.dma("sp", xb[:], src.t[t * 128:(t + 1) * 128, :], [rsrc[t]], [xb], xb)
                for n2 in range(2):
                    pb = po[(t % 2) * 2 + n2]
                    nc_ = slice(n2 * 512, (n2 + 1) * 512)
                    h.mm([(pb[:], yb[:, kk, tc], wout[:, kk, nc_], kk == 0, kk == 7) for kk in range(8)], [yb, wout], [pb])
                    h.tt("dve", tb[:, nc_], pb[:], G[:, nc_], ALU.mult, [pb, G], [tb])
                h.tt("pool", tb[:], tb[:], xb[:], ALU.add, [tb, xb], [tb])
                h.dma("sp", dst.t[t * 128:(t + 1) * 128, :], tb[:], [tb], [rdst[t]], tb)
        P.end_phase()


def gate_consts(k, es, l, tag):
    P, h, inp = k.P, k.h, k.inp
    b12 = P.sb(es, tag + "b12", [128, 12], F32)
    na12 = P.sb(es, tag + "na12", [128, 12], F32)
    k.load_row(b12, inp["bias12"].t[l:l + 1, :])
    k.load_row(na12, inp["alog12"].t[l:l + 1, :])
    h.act(na12[:], na12[:], AF.Exp, [na12], [na12])
    h.ts("dve", na12[:], na12[:], -1.0, None, ALU.mult, None, [na12], [na12])
    return b12, na12


def gate_tile(k, sp_fn, pj_ap, pj_buf, b12, na12, masks, sp12, gda, cs12, cl12, pA):
    h = k.h
    sp_fn(pj_ap, pj_buf, b12, sp12)
    h.tt("dve", gda[:], sp12[:], na12[:], ALU.mult, [sp12, na12], [gda])
    h.mm([(pA[:, 0:12], masks["U"][:], gda[:], True, True)], [masks["U"], gda], [pA])
    h.cp("dve", cs12[:], pA[:, 0:12], [pA], [cs12])
    h.mm([(pA[:, 16:28], masks["sel"][:], cs12[:], True, True)], [masks["sel"], cs12], [pA])
    h.cp("dve", cl12[:], pA[:, 16:28], [pA], [cl12])


def ssd_phase(k, l):
    P, T, h, inp = k.P, k.T, k.h, k.inp
    NMT = T // 512
    MT = 512
    identf, identb = k.identf, k.identb
    with contextlib.ExitStack() as es:
        masks = make_masks(h, P, es)
        b12, na12 = gate_consts(k, es, l, "sd")
        sp_fns = [softplus12(h, P, es, f"sd{i}") for i in range(2)]
        epsb = P.sb(es, "sd_eps", [128, 1], F32)
        h.memset("pool", epsb[:], EPS, [epsb])
        dsk = P.sb(es, "sd_dsk", [128, 6], F32)
        k.load_row(dsk, inp["ssd_d"].t[l:l + 1, :])
        nws = P.sb(es, "sd_nws", [128, 384], F32)
        k.load_row(nws, inp["ssd_norm"].t[l:l + 1, :])
        stT = P.sb(es, "sd_stT", [128, 384], F32)
        stTb = P.sb(es, "sd_stTb", [128, 384], BF16)
        h.memset("pool", stT[:], 0.0, [stT])
        h.memset("pool", stTb[:], 0.0, [stTb])
        xsT = [P.sb(es, f"sd_xsT{i}", [128, 3, MT], F32) for i in range(2)]
        BTt = [P.sb(es, f"sd_BT{i}", [128, 2, MT], BF16) for i in range(2)]
        CTt = [P.sb(es, f"sd_CT{i}", [128, 2, MT], BF16) for i in range(2)]
        pj = [P.sb(es, f"sd_pj{i}", [128, 4, 786], F32) for i in range(2)]
        yo = [P.sb(es, f"sd_yo{i}", [128, 3, MT], BF16) for i in range(2)]
        R_sp12 = Rot(P, es, "sd_sp12", [128, 12], F32)
        R_gda = Rot(P, es, "sd_gda", [128, 12], F32)
        R_cs12 = Rot(P, es, "sd_cs12", [128, 12], F32)
        R_cl12 = Rot(P, es, "sd_cl12", [128, 12], F32)
        R_t6 = Rot(P, es, "sd_t6", [128, 6], F32)
        R_din = Rot(P, es, "sd_din", [128, 6], F32)
        R_eacs = Rot(P, es, "sd_eacs", [128, 6], F32)
        R_cd = Rot(P, es, "sd_cd", [128, 6], F32)
        R_dg = Rot(P, es, "sd_dg", [128, 6, 128], F32)
        R_arg = Rot(P, es, "sd_arg", [128, 6, 128], F32)
        R_seg = Rot(P, es, "sd_seg", [128, 6, 128], F32)
        R_WTb = Rot(P, es, "sd_WTb", [128, 6, 128], BF16)
        R_xs_tm = Rot(P, es, "sd_xstm", [128, 384], F32)
        R_xdtf = Rot(P, es, "sd_xdtf", [128, 384], F32)
        R_xdtb = Rot(P, es, "sd_xdtb", [128, 384], BF16)
        R_xddb = Rot(P, es, "sd_xddb", [128, 384], BF16)
        R_Btm = Rot(P, es, "sd_Btm", [128, 256], BF16)
        R_t1 = Rot(P, es, "sd_t1", [128, 384], F32)
        R_t2 = Rot(P, es, "sd_t2", [128, 384], F32)
        R_y = Rot(P, es, "sd_y", [128, 384], F32)
        R_zs = Rot(P, es, "sd_zs", [128, 384], F32)
        R_junk = Rot(P, es, "sd_junk", [128, 192], F32)
        R_yb = Rot(P, es, "sd_yb", [128, 384], BF16)
        R_ss2 = Rot(P, es, "sd_ss2", [128, 2], F32)
        R_rs2 = Rot(P, es, "sd_rs2", [128, 2], F32)
        W0 = P.ps(es, "sd_W0", [128, 2, 512], F32)
        S0 = P.ps(es, "sd_S0", [128, 512], F32)
        S1 = P.ps(es, "sd_S1", [128, 512], F32)
        S2 = P.ps(es, "sd_S2", [128, 512], F32)
        PB = P.ps(es, "sd_PB", [128, 512], BF16)
        pA = P.ps(es, "sd_pA", [128, 32], F32)
        for mt in range(NMT):
            cols = slice(mt * MT, (mt + 1) * MT)
            i2 = mt % 2
            rp = [k.r_pre[mt]]
            h.dma("sp", xsT[i2][:], k.xs_d.t[:, cols].rearrange("(j p) t -> p j t", p=128), rp, [xsT[i2]], xsT[i2])
            h.dma("sp", BTt[i2][:], k.B_d.t[:, cols].rearrange("(j p) t -> p j t", p=128), rp, [BTt[i2]], BTt[i2])
            h.dma("sp", CTt[i2][:], k.C_d.t[:, cols].rearrange("(j p) t -> p j t", p=128), rp, [CTt[i2]], CTt[i2])
            h.dma("sp", pj[i2][:], k.pt_d.t[mt * MT:(mt + 1) * MT, :].rearrange("(a p) n -> p a n", p=128), rp, [pj[i2]], pj[i2])
            xs_, B_, C_, pj_, yo_ = xsT[i2], BTt[i2], CTt[i2], pj[i2], yo[i2]
            for ti in range(4):
                tc = slice(ti * 128, (ti + 1) * 128)
                tix = mt * 4 + ti
                sp12 = R_sp12.at(tix); gda = R_gda.at(tix); cs12 = R_cs12.at(tix); cl12 = R_cl12.at(tix); t6 = R_t6.at(tix); din = R_din.at(tix); eacs = R_eacs.at(tix); cd = R_cd.at(tix); dg = R_dg.at(tix); arg = R_arg.at(tix); seg = R_seg.at(tix); WTb = R_WTb.at(tix); xs_tm = R_xs_tm.at(tix); xdtf = R_xdtf.at(tix); xdtb = R_xdtb.at(tix); xddb = R_xddb.at(tix); Btm = R_Btm.at(tix); t1 = R_t1.at(tix); t2 = R_t2.at(tix); y = R_y.at(tix); zs = R_zs.at(tix); junk = R_junk.at(tix); yb = R_yb.at(tix); ss2 = R_ss2.at(tix); rs2 = R_rs2.at(tix)
                sp_fn = sp_fns[tix % 2]
                gate_tile(k, sp_fn, pj_[:, ti, 768:780], pj_, b12, na12, masks, sp12, gda, cs12, cl12, pA)
                acs = cs12[:, 6:12]
                h.tt("pool", dg[:], b3(identf[:], 6, 128), s3(acs, 6, 128), ALU.mult, [identf, cs12], [dg])
                h.mm([(W0[:, 0, 0:384], masks["ones"][:], dg[:, 0:3, :].rearrange("p a l -> p (a l)"), True, True),
                      (W0[:, 1, 0:384], masks["ones"][:], dg[:, 3:6, :].rearrange("p a l -> p (a l)"), True, True)],
                     [masks["ones"], dg], [W0])
                h.tt("dve", v4(arg[:]), w4(W0), s4(acs), ALU.subtract, [W0, cs12], [arg])
                h.ts("pool", arg[:], arg[:], 0.0, None, ALU.min, None, [arg], [arg])
                h.act(seg[:], arg[:], AF.Exp, [arg], [seg])
                h.tt("pool", seg[:], seg[:], b3(masks["U"][:], 6, 128), ALU.mult, [seg, masks["U"]], [seg])
                h.mm([(S0[:, g * 128:(g + 1) * 128], B_[:, g, tc], C_[:, g, tc], True, True) for g in range(2)], [B_, C_], [S0])
                h.tt("dve", v4(WTb[:]), v4(seg[:]),
                     S0[:, 0:256].rearrange("p (g l) -> p g l", g=2).unsqueeze(2).to_broadcast([128, 2, 3, 128]),
                     ALU.mult, [seg, S0], [WTb])
                h.tr([(S1[:, j * 128:(j + 1) * 128], xs_[:, j, tc]) for j in range(3)], identf[:], [xs_, identf], [S1])
                h.cp("act", xs_tm[:], S1[:, 0:384], [S1], [xs_tm])
                h.tr([(PB[:, g * 128:(g + 1) * 128], B_[:, g, tc]) for g in range(2)], identb[:], [B_, identb], [PB])
                h.cp("act", Btm[:], PB[:, 0:256], [PB], [Btm])
                x3 = lambda ap: ap.rearrange("p (h d) -> p h d", h=6)
                h.tt("dve", x3(xdtf[:]), x3(xs_tm[:]), s3(sp12[:, 6:12], 6, 64), ALU.mult, [xs_tm, sp12], [xdtf])
                h.cp("pool", xdtb[:], xdtf[:], [xdtf], [xdtb])
                h.tt("dve", t6[:], cl12[:, 6:12], acs, ALU.subtract, [cl12, cs12], [t6])
                h.act(din[:], t6[:], AF.Exp, [t6], [din])
                h.tt("pool", x3(xddb[:]), x3(xdtf[:]), s3(din[:], 6, 64), ALU.mult, [xdtf, din], [xddb])
                h.mm([(S2[:, hd * 64:(hd + 1) * 64], WTb[:, hd, :], xdtb[:, hd * 64:(hd + 1) * 64], True, True) for hd in range(6)],
                     [WTb, xdtb], [S2])
                h.mm([(S0[:, g * 192:(g + 1) * 192], C_[:, g, tc], stTb[:, g * 192:(g + 1) * 192], True, True) for g in range(2)],
                     [C_, stTb], [S0])
                h.act(eacs[:], acs, AF.Exp, [cs12], [eacs])
                h.tt("dve", x3(t2[:]), x3(S0[:, 0:384]), s3(eacs[:], 6, 64), ALU.mult, [S0, eacs], [t2])
                h.tt("pool", x3(t1[:]), x3(xs_tm[:]), s3(dsk[:], 6, 64), ALU.mult, [xs_tm, dsk], [t1])
                h.tt("pool", t2[:], t2[:], t1[:], ALU.add, [t2, t1], [t2])
                h.tt("dve", y[:], S2[:, 0:384], t2[:], ALU.add, [S2, t2], [y])
                h.act(zs[:], pj_[:, ti, 384:768], AF.Silu, [pj_], [zs])
                h.tt("pool", y[:], y[:], zs[:], ALU.mult, [y, zs], [y])
                for g in range(2):
                    h.act(junk[:], y[:, g * 192:(g + 1) * 192], AF.Square, [y], [junk, ss2], accum=ss2[:, g:g + 1])
                h.act(rs2[:], ss2[:], AF.Ln, [ss2, epsb], [rs2], bias=epsb[:, 0:1], scale=1.0 / 192)
                h.act(rs2[:], rs2[:], AF.Exp, [rs2], [rs2], scale=-0.5)
                y3 = lambda ap: ap.rearrange("p (g c) -> p g c", g=2)
                h.tt("dve", y3(y[:]), y3(y[:]), s3(rs2[:], 2, 192), ALU.mult, [y, rs2], [y])
                h.tt("pool", yb[:], y[:], nws[:], ALU.mult, [y, nws], [yb])
                h.tr([(PB[:, j * 128:(j + 1) * 128], yb[:, j * 128:(j + 1) * 128]) for j in range(3)], identb[:], [yb, identb], [PB])
                h.cp("act", yo_[:, :, tc], PB[:, 0:384].rearrange("p (j t) -> p j t", j=3), [PB], [yo_])
                h.mm([(S1[:, g * 192:(g + 1) * 192], Btm[:, g * 128:(g + 1) * 128], xddb[:, g * 192:(g + 1) * 192], True, True) for g in range(2)],
                     [Btm, xddb], [S1])
                h.act(cd[:], cl12[:, 6:12], AF.Exp, [cl12], [cd])
                h.tt("pool", x3(stT[:]), x3(stT[:]), s3(cd[:], 6, 64), ALU.mult, [stT, cd], [stT])
                h.tt("dve", stT[:], stT[:], S1[:, 0:384], ALU.add, [stT, S1], [stT])
                h.cp("act", stTb[:], stT[:], [stT], [stTb])
            h.dma("sp", k.y_d.t[640:1024, cols].rearrange("(j p) t -> p j t", p=128), yo_[:], [yo_], [k.r_y[2][mt]], yo_)
        P.end_phase()


C1_2PI = 6.28125
C2_2PI = 2.0 * math.pi - 6.28125


def sincos(h, eng, x, out, b, ki, c, negpi, R, W, bufs, is_cos):
    bb, kb, cb_ = bufs
    off = 16.5 + (0.25 if is_cos else 0.0)
    add = 33.0 * math.pi + (0.5 * math.pi if is_cos else 0.0)
    h.ts(eng, b, x, 1.0 / (2.0 * math.pi), off, ALU.mult, ALU.add, R, [bb])
    h.cp(eng, ki, b, [bb], [kb])
    h.cp(eng, c, ki, [kb], [cb_])
    h.stt(eng, b, c, -C1_2PI, x, ALU.mult, ALU.add, R + [cb_], [bb])
    h.stt(eng, b, c, -C2_2PI, b, ALU.mult, ALU.add, [cb_, bb], [bb])
    h.ts(eng, b, b, add, None, ALU.add, None, [bb], [bb])
    h.ts(eng, c, b, 2.0 * math.pi, -2.0 * math.pi, ALU.is_gt, ALU.mult, [bb], [cb_])
    h.tt(eng, b, b, c, ALU.add, [bb, cb_], [bb])
    h.ts(eng, c, b, 0.0, 2.0 * math.pi, ALU.is_lt, ALU.mult, [bb], [cb_])
    h.tt(eng, b, b, c, ALU.add, [bb, cb_], [bb])
    h.act(out, b, AF.Sin, [bb, negpi], W, bias=negpi[:, 0:1])


def s5_phase(k, l):
    P, T, h, inp = k.P, k.T, k.h, k.inp
    NMT = T // 512
    SEG = 512
    with contextlib.ExitStack() as es:
        I32 = mybir.dt.int32
        are = P.sb(es, "s5are", [128, 8], F32)
        aim = P.sb(es, "s5aim", [128, 8], F32)
        stp = P.sb(es, "s5stp", [128, 8], F32)
        h.dma("sp", are[:], inp["s5_are"].t[l], [], [are], are)
        h.dma("sp", aim[:], inp["s5_aim"].t[l], [], [aim], aim)
        h.dma("sp", stp[:], inp["s5_ldt"].t[l], [], [stp], stp)
        dsk = P.sb(es, "s5dsk", [128, 2], F32)
        nw5 = P.sb(es, "s5nw", [128, 2], F32)
        h.dma("sp", dsk[:], inp["s5_dcol"].t[l], [], [dsk], dsk)
        h.dma("sp", nw5[:], inp["s5_ncol"].t[l], [], [nw5], nw5)
        bTre = P.sb(es, "s5bTre", [128, 8, 128], BF16)
        bTim = P.sb(es, "s5bTim", [128, 8, 128], BF16)
        cTre = P.sb(es, "s5cTre", [128, 8, 128], BF16)
        cTim = P.sb(es, "s5cTim", [128, 8, 128], BF16)
        for dstb, nm in ((bTre, "s5_bT_re"), (bTim, "s5_bT_im"), (cTre, "s5_cT_re"), (cTim, "s5_cT_im")):
            h.dma("pool", dstb[:], inp[nm].t[l].rearrange("s r m -> r s m"), [], [dstb], dstb)
        wglu = P.sb(es, "s5wglu", [128, 2, 256], BF16)
        h.dma("pool", wglu[:], inp["s5_w_glu"].t[l].rearrange("(k p) n -> p k n", p=128), [], [wglu], wglu)
        negpi = P.sb(es, "s5negpi", [128, 1], F32)
        h.memset("pool", negpi[:], -math.pi, [negpi])
        epsb = P.sb(es, "s5eps", [128, 1], F32)
        h.memset("pool", epsb[:], EPS, [epsb])
        onesf = P.sb(es, "s5ones", [128, 128], F32)
        h.memset("pool", onesf[:], 1.0, [onesf])
        jrow = P.sb(es, "s5jrow", [128, SEG], F32)
        P.op("pool", lambda e: e.iota(jrow[:], pattern=[[1, SEG]], base=0, channel_multiplier=0,
                                      allow_small_or_imprecise_dtypes=True), writes=[jrow])
        th = P.sb(es, "s5th", [128, 8], F32)
        rr = P.sb(es, "s5r", [128, 8], F32)
        sth = P.sb(es, "s5sth", [128, 8], F32)
        cth = P.sb(es, "s5cth", [128, 8], F32)
        thS = P.sb(es, "s5thS", [128, 8], F32)
        sS = P.sb(es, "s5sS", [128, 8], F32)
        cS = P.sb(es, "s5cS", [128, 8], F32)
        nsS = P.sb(es, "s5nsS", [128, 8], F32)
        cr = P.sb(es, "s5cr", [128, 8], F32)
        ci = P.sb(es, "s5ci", [128, 8], F32)
        ncr = P.sb(es, "s5ncr", [128, 8], F32)
        q1 = P.sb(es, "s5q1", [128, 8], F32)
        q2 = P.sb(es, "s5q2", [128, 8], F32)
        q3 = P.sb(es, "s5q3", [128, 8], F32)
        sb8 = P.sb(es, "s5sb8", [128, 8], F32)
        si8 = P.sb(es, "s5si8", [128, 8], I32)
        sc8 = P.sb(es, "s5sc8", [128, 8], F32)
        h.act(stp[:], stp[:], AF.Exp, [stp], [stp])
        h.tt("dve", th[:], aim[:], stp[:], ALU.mult, [aim, stp], [th])
        h.tt("dve", rr[:], are[:], stp[:], ALU.mult, [are, stp], [rr])
        h.act(rr[:], rr[:], AF.Exp, [rr], [rr])
        sm = (sb8, si8, sc8)
        sincos(h, "dve", th[:], sth[:], sb8[:], si8[:], sc8[:], negpi, [th], [sth], sm, False)
        sincos(h, "dve", th[:], cth[:], sb8[:], si8[:], sc8[:], negpi, [th], [cth], sm, True)
        h.ts("dve", thS[:], th[:], float(SEG), None, ALU.mult, None, [th], [thS])
        sincos(h, "dve", thS[:], sS[:], sb8[:], si8[:], sc8[:], negpi, [thS], [sS], sm, False)
        sincos(h, "dve", thS[:], cS[:], sb8[:], si8[:], sc8[:], negpi, [thS], [cS], sm, True)
        h.ts("dve", nsS[:], sS[:], -1.0, None, ALU.mult, None, [sS], [nsS])
        h.tt("dve", q1[:], rr[:], cth[:], ALU.mult, [rr, cth], [q1])
        h.ts("dve", q1[:], q1[:], -1.0, None, ALU.add, None, [q1], [q1])
        h.tt("dve", q2[:], rr[:], sth[:], ALU.mult, [rr, sth], [q2])
        h.tt("dve", q3[:], are[:], are[:], ALU.mult, [are], [q3])
        h.tt("dve", sc8[:], aim[:], aim[:], ALU.mult, [aim], [sc8])
        h.tt("dve", q3[:], q3[:], sc8[:], ALU.add, [q3, sc8], [q3])
        h.recip(q3[:], q3[:], [q3], [q3])
        h.tt("dve", cr[:], q1[:], are[:], ALU.mult, [q1, are], [cr])
        h.tt("dve", sc8[:], q2[:], aim[:], ALU.mult, [q2, aim], [sc8])
        h.tt("dve", cr[:], cr[:], sc8[:], ALU.add, [cr, sc8], [cr])
        h.tt("dve", cr[:], cr[:], q3[:], ALU.mult, [cr, q3], [cr])
        h.tt("dve", ci[:], q2[:], are[:], ALU.mult, [q2, are], [ci])
        h.tt("dve", sc8[:], q1[:], aim[:], ALU.mult, [q1, aim], [sc8])
        h.tt("dve", ci[:], ci[:], sc8[:], ALU.subtract, [ci, sc8], [ci])
        h.tt("dve", ci[:], ci[:], q3[:], ALU.mult, [ci, q3], [ci])
        h.ts("dve", ncr[:], cr[:], -1.0, None, ALU.mult, None, [cr], [ncr])
        cosT = P.sb(es, "s5cosT", [128, 8, SEG], F32)
        sinT = P.sb(es, "s5sinT", [128, 8, SEG], F32)
        tabr = P.sb(es, "s5tabr", [128, 8, SEG], F32)
        tabi = P.sb(es, "s5tabi", [128, 8, SEG], F32)
        ang = [P.sb(es, f"s5ang{i}", [128, SEG], F32) for i in range(2)]
        tb = [P.sb(es, f"s5tb{i}", [128, SEG], F32) for i in range(2)]
        tki = [P.sb(es, f"s5tki{i}", [128, SEG], I32) for i in range(2)]
        tcc = [P.sb(es, f"s5tc{i}", [128, SEG], F32) for i in range(2)]
        for sc in range(8):
            i = sc % 2
            eng = "dve" if i == 0 else "pool"
            h.ts(eng, ang[i][:], jrow[:], th[:, sc:sc + 1], None, ALU.mult, None, [jrow, th], [ang[i]])
            bufs = (tb[i], tki[i], tcc[i])
            sincos(h, eng, ang[i][:], sinT[:, sc, :], tb[i][:], tki[i][:], tcc[i][:], negpi, [ang[i]], [sinT], bufs, False)
            sincos(h, eng, ang[i][:], cosT[:, sc, :], tb[i][:], tki[i][:], tcc[i][:], negpi, [ang[i]], [cosT], bufs, True)
            h.ts(eng, tabr[:, sc, :], cosT[:, sc, :], cr[:, sc:sc + 1], None, ALU.mult, None, [cosT, cr], [tabr])
            h.stt(eng, tabr[:, sc, :], sinT[:, sc, :], ci[:, sc:sc + 1], tabr[:, sc, :], ALU.mult, ALU.add, [sinT, ci, tabr], [tabr])
            h.ts(eng, tabi[:, sc, :], cosT[:, sc, :], ci[:, sc:sc + 1], None, ALU.mult, None, [cosT, ci], [tabi])
            h.stt(eng, tabi[:, sc, :], sinT[:, sc, :], ncr[:, sc:sc + 1], tabi[:, sc, :], ALU.mult, ALU.add, [sinT, ncr, tabi], [tabi])
        ire = P.sb(es, "s5ire", [128, 8], F32)
        iim = P.sb(es, "s5iim", [128, 8], F32)
        gre_e = P.sb(es, "s5gree", [128, 8], F32)
        gim_e = P.sb(es, "s5gime", [128, 8], F32)
        h.memset("pool", ire[:], 0.0, [ire])
        h.memset("pool", iim[:], 0.0, [iim])
        uTf = [P.sb(es, f"s5uTf{i}", [128, 2, SEG], F32) for i in range(2)]
        uTb = [P.sb(es, f"s5uTb{i}", [128, 2, SEG], BF16) for i in range(2)]
        m1 = [P.sb(es, f"s5m1{i}", [128, SEG], F32) for i in range(2)]
        m2 = [P.sb(es, f"s5m2{i}", [128, SEG], F32) for i in range(2)]
        m3 = [P.sb(es, f"s5m3{i}", [128, SEG], F32) for i in range(2)]
        m4 = [P.sb(es, f"s5m4{i}", [128, SEG], F32) for i in range(2)]
        p1 = [P.sb(es, f"s5p1{i}", [128, SEG], BF16) for i in range(2)]
        p2 = [P.sb(es, f"s5p2{i}", [128, SEG], BF16) for i in range(2)]
        p3 = [P.sb(es, f"s5p3{i}", [128, SEG], BF16) for i in range(2)]
        p4 = [P.sb(es, f"s5p4{i}", [128, SEG], BF16) for i in range(2)]
        ncTre = P.sb(es, "s5ncTre", [128, 8, 128], BF16)
        ncTim = P.sb(es, "s5ncTim", [128, 8, 128], BF16)
        h.ts("pool", ncTre[:], cTre[:], -1.0, None, ALU.mult, None, [cTre], [ncTre])
        h.ts("pool", ncTim[:], cTim[:], -1.0, None, ALU.mult, None, [cTim], [ncTim])
        dre = [P.sb(es, f"s5dre{i}", [128, SEG], F32) for i in range(2)]
        dim = [P.sb(es, f"s5dim{i}", [128, SEG], F32) for i in range(2)]
        gre = [P.sb(es, f"s5gre{i}", [128, SEG], F32) for i in range(2)]
        gim = [P.sb(es, f"s5gim{i}", [128, SEG], F32) for i in range(2)]
        hre = [P.sb(es, f"s5hre{i}", [128, SEG], BF16) for i in range(2)]
        him = [P.sb(es, f"s5him{i}", [128, SEG], BF16) for i in range(2)]
        y1 = P.sb(es, "s5y1", [128, 2, SEG], F32)
        yt = P.sb(es, "s5yt", [128, 2, SEG], F32)
        yg = P.sb(es, "s5yg", [128, 2, SEG], F32)
        ygb = P.sb(es, "s5ygb", [128, 2, SEG], BF16)
        sg = P.sb(es, "s5sg", [128, 2, SEG], F32)
        y2 = P.sb(es, "s5y2", [128, 2, SEG], F32)
        rstd = P.sb(es, "s5rstd", [128, SEG], F32)
        yo = [P.sb(es, f"s5yo{i}", [128, 2, SEG], BF16) for i in range(2)]
        Pre = [P.ps(es, f"s5Pre{i}", [128, SEG], F32) for i in range(2)]
        Pim = [P.ps(es, f"s5Pim{i}", [128, SEG], F32) for i in range(2)]
        Y = [P.ps(es, f"s5Y{i}", [128, SEG], F32) for i in range(2)]
        Pg = P.ps(es, "s5Pg", [128, SEG], F32)
        Pt = P.ps(es, "s5Pt", [128, SEG], F32)
        GK = 2.0 * math.sqrt(2.0 / math.pi)
        for mt in range(NMT):
            cols = slice(mt * SEG, (mt + 1) * SEG)
            i2 = mt % 2
            uf, ub, yo_ = uTf[i2], uTb[i2], yo[i2]
            h.dma("sp", uf[:], k.u_d.t[:, cols].rearrange("(j p) t -> p j t", p=128), [k.r_pre[mt]], [uf], uf)
            h.cp("pool", ub[:], uf[:], [uf], [ub])
            for sc in range(8):
                i = sc % 2
                cc = sc // 4
                h.mm([(Pre[i][:], bTre[:, sc, :], ub[:, cc, :], True, True)], [bTre, ub], [Pre[i]])
                h.mm([(Pim[i][:], bTim[:, sc, :], ub[:, cc, :], True, True)], [bTim, ub], [Pim[i]])
                h.tt("dve", m1[i][:], Pre[i][:], tabr[:, sc, :], ALU.mult, [Pre[i], tabr], [m1[i]])
                h.tt("dve", m2[i][:], Pim[i][:], tabi[:, sc, :], ALU.mult, [Pim[i], tabi], [m2[i]])
                h.tt("pool", dre[i][:], m1[i][:], m2[i][:], ALU.subtract, [m1[i], m2[i]], [dre[i]])
                h.tt("dve", m3[i][:], Pre[i][:], tabi[:, sc, :], ALU.mult, [Pre[i], tabi], [m3[i]])
                h.tt("dve", m4[i][:], Pim[i][:], tabr[:, sc, :], ALU.mult, [Pim[i], tabr], [m4[i]])
                h.tt("pool", dim[i][:], m3[i][:], m4[i][:], ALU.add, [m3[i], m4[i]], [dim[i]])
                for (go, di, ini) in ((gre[i], dre[i], ire), (gim[i], dim[i], iim)):
                    P.op("dve", (lambda go, di, ini, sc: (lambda e: e.tensor_tensor_scan(
                        out=go[:], data0=rr[:, sc:sc + 1].to_broadcast([128, SEG]), data1=di[:],
                        initial=ini[:, sc:sc + 1], op0=ALU.mult, op1=ALU.add)))(go, di, ini, sc),
                        reads=[rr, di, ini], writes=[go])
                h.cp("act", gre_e[:, sc:sc + 1], gre[i][:, SEG - 1:SEG], [gre[i]], [gre_e])
                h.cp("act", gim_e[:, sc:sc + 1], gim[i][:, SEG - 1:SEG], [gim[i]], [gim_e])
                h.tt("pool", p1[i][:], gre[i][:], cosT[:, sc, :], ALU.mult, [gre[i], cosT], [p1[i]])
                h.tt("pool", p2[i][:], gim[i][:], sinT[:, sc, :], ALU.mult, [gim[i], sinT], [p2[i]])
                h.tt("dve", p3[i][:], gre[i][:], sinT[:, sc, :], ALU.mult, [gre[i], sinT], [p3[i]])
                h.tt("dve", p4[i][:], gim[i][:], cosT[:, sc, :], ALU.mult, [gim[i], cosT], [p4[i]])
                h.mm([(Y[cc][:], cTre[:, sc, :], p1[i][:], sc % 4 == 0, False),
                      (Y[cc][:], ncTre[:, sc, :], p2[i][:], False, False),
                      (Y[cc][:], ncTim[:, sc, :], p3[i][:], False, False),
                      (Y[cc][:], ncTim[:, sc, :], p4[i][:], False, sc % 4 == 3)],
                     [cTre, ncTre, ncTim, p1[i], p2[i], p3[i], p4[i]], [Y[cc]])
                if sc % 4 == 3:
                    h.stt("dve", y1[:, cc, :], uf[:, cc, :], dsk[:, cc:cc + 1], Y[cc][:], ALU.mult, ALU.add, [uf, dsk, Y[cc]], [y1])
                    h.tt("pool", yt[:, cc, :], y1[:, cc, :], y1[:, cc, :], ALU.mult, [y1], [yt])
                    h.ts("pool", yt[:, cc, :], yt[:, cc, :], 0.044715, 1.0, ALU.mult, ALU.add, [yt], [yt])
                    h.tt("pool", yt[:, cc, :], yt[:, cc, :], y1[:, cc, :], ALU.mult, [yt, y1], [yt])
                    h.act(yt[:, cc, :], yt[:, cc, :], AF.Sigmoid, [yt], [yt], scale=GK)
                    h.tt("pool", yg[:, cc, :], y1[:, cc, :], yt[:, cc, :], ALU.mult, [y1, yt], [yg])
                    h.cp("pool", ygb[:, cc, :], yg[:, cc, :], [yg], [ygb])
            h.tt("dve", q1[:], gre_e[:], cS[:], ALU.mult, [gre_e, cS], [q1])
            h.tt("dve", q2[:], gim_e[:], nsS[:], ALU.mult, [gim_e, nsS], [q2])
            h.tt("dve", ire[:], q1[:], q2[:], ALU.add, [q1, q2], [ire])
            h.tt("dve", q1[:], gre_e[:], sS[:], ALU.mult, [gre_e, sS], [q1])
            h.tt("dve", q2[:], gim_e[:], cS[:], ALU.mult, [gim_e, cS], [q2])
            h.tt("dve", iim[:], q1[:], q2[:], ALU.add, [q1, q2], [iim])
            for oc in range(2):
                h.mm([(Pg[:], wglu[:, kc, oc * 128:(oc + 1) * 128], ygb[:, kc, :], kc == 0, kc == 1) for kc in range(2)], [wglu, ygb], [Pg])
                h.act(sg[:, oc, :], Pg[:], AF.Sigmoid, [Pg], [sg])
                h.tt("pool", y2[:, oc, :], yg[:, oc, :], sg[:, oc, :], ALU.mult, [yg, sg], [y2])
                h.tt("pool", sg[:, oc, :], y2[:, oc, :], y2[:, oc, :], ALU.mult, [y2], [sg])
            h.mm([(Pt[:], onesf[:], sg[:, oc, :], oc == 0, oc == 1) for oc in range(2)], [onesf, sg], [Pt])
            h.act(rstd[:], Pt[:], AF.Sqrt, [Pt, epsb], [rstd], bias=epsb[:, 0:1], scale=1.0 / 256)
            h.recip(rstd[:], rstd[:], [rstd], [rstd])
            for oc in range(2):
                h.stt("dve", yo_[:, oc, :], y2[:, oc, :], nw5[:, oc:oc + 1], rstd[:], ALU.mult, ALU.mult, [y2, nw5, rstd], [yo_])
            h.dma("sp", k.y_d.t[0:256, cols].rearrange("(j p) t -> p j t", p=128), yo_[:], [yo_], [k.r_y[0][mt]], yo_)
        P.end_phase()


def gdn_phase(k, l):
    P, T, h, inp = k.P, k.T, k.h, k.inp
    NMT = T // 512
    MT = 512
    identf, identb = k.identf, k.identb
    with contextlib.ExitStack() as es:
        masks = make_masks(h, P, es)
        b12, na12 = gate_consts(k, es, l, "gd")
        gnw = P.sb(es, "gd_gnw", [128, 64], F32)
        k.load_row(gnw, inp["gdn_norm"].t[l:l + 1, :])
        Sf = P.sb(es, "gd_Sf", [128, 3, 128], F32)
        Sb = P.sb(es, "gd_Sb", [128, 3, 128], BF16)
        h.memset("pool", Sf[:], 0.0, [Sf])
        h.memset("pool", Sb[:], 0.0, [Sb])
        qnT = [P.sb(es, f"gd_qn{i}", [128, 3, MT], BF16) for i in range(2)]
        knT = [P.sb(es, f"gd_kn{i}", [128, 3, MT], BF16) for i in range(2)]
        vT = [P.sb(es, f"gd_vT{i}", [128, 3, MT], F32) for i in range(2)]
        pj = [P.sb(es, f"gd_pj{i}", [128, 4, 786], F32) for i in range(2)]
        yo = [P.sb(es, f"gd_yo{i}", [128, 3, MT], BF16) for i in range(2)]
        R_sp12 = Rot(P, es, "gd_sp12", [128, 12], F32)
        R_gda = Rot(P, es, "gd_gda", [128, 12], F32)
        R_cs12 = Rot(P, es, "gd_cs12", [128, 12], F32)
        R_cl12 = Rot(P, es, "gd_cl12", [128, 12], F32)
        R_beta = Rot(P, es, "gd_beta", [128, 6], F32)
        R_nbeta = Rot(P, es, "gd_nbeta", [128, 6], F32)
        R_egc = Rot(P, es, "gd_egc", [128, 6], F32)
        R_t6 = Rot(P, es, "gd_t6", [128, 6], F32)
        R_dkk = Rot(P, es, "gd_dkk", [128, 6], F32)
        R_gtot = Rot(P, es, "gd_gtot", [128, 6], F32)
        R_gtc = Rot(P, es, "gd_gtc", [128, 3], F32)
        R_dg = Rot(P, es, "gd_dg", [128, 6, 128], F32)
        R_arg = Rot(P, es, "gd_arg", [128, 6, 128], F32)
        R_E = Rot(P, es, "gd_E", [128, 6, 128], F32)
        R_EU = Rot(P, es, "gd_EU", [128, 6, 128], F32)
        R_ELn = Rot(P, es, "gd_ELn", [128, 6, 128], F32)
        R_attnT = Rot(P, es, "gd_attnT", [128, 6, 128], BF16)
        R_Pm = [Rot(P, es, f"gd_Pm{i}", [128, 6, 128], BF16) for i in range(2)]
        R_Qm = [Rot(P, es, f"gd_Qm{i}", [128, 6, 128], BF16) for i in range(2)]
        sp_fns = [softplus12(h, P, es, f"gd{i}") for i in range(2)]
        R_Xb = Rot(P, es, "gd_Xb", [128, 6, 128], BF16)
        R_kdec = Rot(P, es, "gd_kdec", [128, 384], BF16)
        R_v_tm = Rot(P, es, "gd_vtm", [128, 384], F32)
        R_rr_ = Rot(P, es, "gd_rr", [128, 384], F32)
        R_rb = Rot(P, es, "gd_rb", [128, 384], BF16)
        R_vnb = Rot(P, es, "gd_vnb", [128, 384], BF16)
        R_oa = Rot(P, es, "gd_oa", [128, 384], F32)
        R_o = Rot(P, es, "gd_o", [128, 384], F32)
        R_sq = Rot(P, es, "gd_sq", [128, 384], F32)
        R_ss6 = Rot(P, es, "gd_ss6", [128, 6], F32)
        R_rs6 = Rot(P, es, "gd_rs6", [128, 6], F32)
        R_zg = Rot(P, es, "gd_zg", [128, 384], F32)
        R_yb = Rot(P, es, "gd_yb", [128, 384], BF16)
        R_tmpS = Rot(P, es, "gd_tmpS", [128, 3, 128], F32)
        W0 = P.ps(es, "gd_W0", [128, 2, 512], F32)
        W1 = P.ps(es, "gd_W1", [128, 2, 512], F32)
        W2 = P.ps(es, "gd_W2", [128, 2, 512], F32)
        W1a, W1b, W2a, W2b = Buf(None, "W1a"), Buf(None, "W1b"), Buf(None, "W2a"), Buf(None, "W2b")
        W1h, W2h = (W1a, W1b), (W2a, W2b)
        M1bh_s = [(Buf(None, f"m1a{i}"), Buf(None, f"m1b{i}")) for i in range(2)]
        M1pbh_s = [(Buf(None, f"m1pa{i}"), Buf(None, f"m1pb{i}")) for i in range(2)]
        PB = P.ps(es, "gd_PB", [128, 1024], BF16)
        pA = P.ps(es, "gd_pA", [128, 32], F32)
        x3 = lambda ap: ap.rearrange("p (h d) -> p h d", h=6)
        kz = [P.sb(es, f"gd_kz{i}", [128, 3, MT], BF16) for i in range(2)]
        R_Tb = Rot(P, es, "gd_Tb", [128, 6, 128], BF16)
        R_Tb2 = Rot(P, es, "gd_Tb2", [128, 6, 128], BF16)
        R_Xb2 = Rot(P, es, "gd_Xb2", [128, 6, 128], BF16)
        epsb = P.sb(es, "gd_eps", [128, 1], F32)
        h.memset("pool", epsb[:], EPS, [epsb])
        R_ez = Rot(P, es, "gd_ez", [128, 384], F32)
        R_M1b = Rot(P, es, "gd_M1b", [128, 6, 128], BF16)
        R_M1pb = Rot(P, es, "gd_M1pb", [128, 6, 128], BF16)
        cmask = P.sb(es, "gd_cmask", [128, 14, 128], BF16)
        h.dma("pool", cmask[:], inp["gdn_cmask"].t.rearrange("m p j -> p m j"), [], [cmask], cmask)
        cmask6 = P.sb(es, "gd_cmask6", [128, 14, 6, 128], BF16)
        h.cp("pool", cmask6[:], cmask[:].unsqueeze(2).to_broadcast([128, 14, 6, 128]), [cmask], [cmask6])
        rmask = P.sb(es, "gd_rmask", [128, 2], F32)
        h.memset("pool", rmask[:], 0.0, [rmask])
        h.memset("pool", rmask[0:64, 0:1], 1.0, [rmask])
        h.memset("pool", rmask[64:128, 1:2], 1.0, [rmask])

        def headmm(Wps, lh, rh, R, Wh):
            w = w4(Wps)
            h.mm([(w[:, hd // 3, hd % 3, :], lh[:, hd, :], rh[:, hd, :], True, True) for hd in range(6)], R, list(Wh))

        stop = k.flags.get("gdn_stop", 99)
        for mt in range(NMT):
            cols = slice(mt * MT, (mt + 1) * MT)
            i2 = mt % 2
            rp = [k.r_pre[mt]]
            h.dma("sp", qnT[i2][:], k.qn_d.t[:, cols].rearrange("(j p) t -> p j t", p=128), rp, [qnT[i2]], qnT[i2])
            h.dma("sp", knT[i2][:], k.kn_d.t[:, cols].rearrange("(j p) t -> p j t", p=128), rp, [knT[i2]], knT[i2])
            h.dma("sp", vT[i2][:], k.v_d.t[:, cols].rearrange("(j p) t -> p j t", p=128), rp, [vT[i2]], vT[i2])
            h.dma("sp", pj[i2][:], k.pt_d.t[mt * MT:(mt + 1) * MT, :].rearrange("(a p) n -> p a n", p=128), rp, [pj[i2]], pj[i2])
            qn_, kn_, v_, pj_, yo_ = qnT[i2], knT[i2], vT[i2], pj[i2], yo[i2]
            for s_ in range(2):
                h.ts("dve", kz[s_][:], kn_[:], rmask[:, s_:s_ + 1], None, ALU.mult, None, [kn_, rmask], [kz[s_]])
            for ti in range(4):
                tc = slice(ti * 128, (ti + 1) * 128)
                tix = mt * 4 + ti
                sp12 = R_sp12.at(tix); gda = R_gda.at(tix); cs12 = R_cs12.at(tix); cl12 = R_cl12.at(tix); beta = R_beta.at(tix); nbeta = R_nbeta.at(tix); egc = R_egc.at(tix); t6 = R_t6.at(tix); dkk = R_dkk.at(tix); gtot = R_gtot.at(tix); gtc = R_gtc.at(tix); dg = R_dg.at(tix); arg = R_arg.at(tix); E = R_E.at(tix); EU = R_EU.at(tix); ELn = R_ELn.at(tix); attnT = R_attnT.at(tix); Xb = R_Xb.at(tix); kdec = R_kdec.at(tix); v_tm = R_v_tm.at(tix); rr_ = R_rr_.at(tix); rb = R_rb.at(tix); vnb = R_vnb.at(tix); oa = R_oa.at(tix); o = R_o.at(tix); sq = R_sq.at(tix); ss6 = R_ss6.at(tix); rs6 = R_rs6.at(tix); zg = R_zg.at(tix); yb = R_yb.at(tix); tmpS = R_tmpS.at(tix); Tb = R_Tb.at(tix); M1b = R_M1b.at(tix); M1pb = R_M1pb.at(tix)
                Pm = [r.at(tix) for r in R_Pm]; Qm = [r.at(tix) for r in R_Qm]; sp_fn = sp_fns[tix % 2]; M1bh = M1bh_s[tix % 2]; M1pbh = M1pbh_s[tix % 2]; Tb2 = R_Tb2.at(tix); Xb2 = R_Xb2.at(tix); ez = R_ez.at(tix)
                gate_tile(k, sp_fn, pj_[:, ti, 768:780], pj_, b12, na12, masks, sp12, gda, cs12, cl12, pA)
                gc = cs12[:, 0:6]
                h.act(beta[:], pj_[:, ti, 780:786], AF.Exp, [pj_], [beta], scale=-1.0)
                h.ts("dve", beta[:], beta[:], 1.0, None, ALU.add, None, [beta], [beta])
                h.recip(beta[:], beta[:], [beta], [beta])
                h.ts("dve", nbeta[:], beta[:], -1.0, None, ALU.mult, None, [beta], [nbeta])
                h.act(egc[:], gc, AF.Exp, [cs12], [egc])
                h.tt("dve", t6[:], cl12[:, 0:6], gc, ALU.subtract, [cl12, cs12], [t6])
                h.act(dkk[:], t6[:], AF.Exp, [t6], [dkk])
                h.act(gtot[:], cl12[:, 0:6], AF.Exp, [cl12], [gtot])
                g2 = gtot[:].rearrange("p (j s) -> p j s", s=2)
                h.cp("dve", gtc[0:64, :], g2[0:64, :, 0], [gtot], [gtc])
                h.cp("dve", gtc[64:128, :], g2[64:128, :, 1], [gtot], [gtc])
                if stop < 1:
                    h.memset("pool", yo_[:, :, tc], 0.0, [yo_])
                    continue
                h.tt("pool", dg[:], b3(identf[:], 6, 128), s3(gc, 6, 128), ALU.mult, [identf, cs12], [dg])
                h.mm([(W0[:, 0, 0:384], masks["ones"][:], dg[:, 0:3, :].rearrange("p a l -> p (a l)"), True, True),
                      (W0[:, 1, 0:384], masks["ones"][:], dg[:, 3:6, :].rearrange("p a l -> p (a l)"), True, True)],
                     [masks["ones"], dg], [W0])
                if stop < 1.2:
                    h.memset("pool", yo_[:, :, tc], 0.0, [yo_])
                    continue
                h.tt("dve", v4(arg[:]), w4(W0), s4(gc), ALU.subtract, [W0, cs12], [arg])
                if stop < 1.4:
                    h.memset("pool", yo_[:, :, tc], 0.0, [yo_])
                    continue
                h.act(arg[:], arg[:], AF.Abs, [arg], [arg])
                h.act(E[:], arg[:], AF.Exp, [arg], [E], scale=-1.0)
                if stop < 1.6:
                    h.memset("pool", yo_[:, :, tc], 0.0, [yo_])
                    continue
                h.tt("pool", EU[:], E[:], b3(masks["U"][:], 6, 128), ALU.mult, [E, masks["U"]], [EU])
                if stop < 1.8:
                    h.memset("pool", yo_[:, :, tc], 0.0, [yo_])
                    continue
                h.tt("pool", ELn[:], E[:], b3(masks["Ls"][:], 6, 128), ALU.mult, [E, masks["Ls"]], [ELn])
                if stop < 1.9:
                    h.memset("pool", yo_[:, :, tc], 0.0, [yo_])
                    continue
                h.tt("pool", ELn[:], ELn[:], s3(nbeta[:], 6, 128), ALU.mult, [ELn, nbeta], [ELn])
                if stop < 2:
                    h.memset("pool", yo_[:, :, tc], 0.0, [yo_])
                    continue
                w1 = w4(W1)
                w2 = w4(W2)
                h.mm([(w1[:, hd // 3, hd % 3, :], kz[hd % 2][:, hd // 2, tc], kn_[:, hd // 2, tc], True, True) for hd in range(6)],
                     [kz[0], kz[1], kn_], [W1a, W1b])
                h.mm([(w2[:, hd // 3, hd % 3, :], kz[hd % 2][:, hd // 2, tc], qn_[:, hd // 2, tc], True, True) for hd in range(6)],
                     [kz[0], kz[1], qn_], [W2a, W2b])
                h.tt("dve", v4(Pm[0][:]), w1, v4(ELn[:]), ALU.mult, [W1a, W1b, ELn], [Pm[0]])
                h.tt("dve", v4(attnT[:]), w2, v4(EU[:]), ALU.mult, [W2a, W2b, EU], [attnT])
                if stop < 3:
                    h.memset("pool", yo_[:, :, tc], 0.0, [yo_])
                    continue
                h.tr([(PB[:, hd * 128:(hd + 1) * 128], Pm[0][:, hd, :]) for hd in range(6)], identb[:], [Pm[0], identb], [PB])
                h.cp("act", Qm[0][:], PB[:, 0:768].rearrange("p (a l) -> p a l", a=6), [PB], [Qm[0]])
                Nn, NT_ = Pm[0], Qm[0]
                h.tt("pool", Pm[1][:], Nn[:], b3(cmask[:, 0, :], 6, 128), ALU.mult, [Nn, cmask], [Pm[1]])
                h.tt("pool", Qm[1][:], NT_[:], b3(cmask[:, 7, :], 6, 128), ALU.mult, [NT_, cmask], [Qm[1]])
                h.tt("dve", Tb[:], Pm[1][:], b3(identf[:], 6, 128), ALU.add, [Pm[1], identf], [Tb])
                h.tt("dve", Xb[:], Qm[1][:], b3(identf[:], 6, 128), ALU.add, [Qm[1], identf], [Xb])
                Tc, Xc = Tb, Xb
                Tn, Xn = Tb2, Xb2
                for lv in range(1, 7):
                    w1_ = w4(W1)
                    w2_ = w4(W2)
                    for hf_ in range(2):
                        hs = range(3 * hf_, 3 * hf_ + 3)
                        h.mm([(w1_[:, hf_, hd % 3, :], NT_[:, hd, :], Tc[:, hd, :], True, True) for hd in hs], [NT_, Tc], [W1h[hf_]])
                        h.mm([(w2_[:, hf_, hd % 3, :], Nn[:, hd, :], Xc[:, hd, :], True, True) for hd in hs], [Nn, Xc], [W2h[hf_]])
                    for hf_ in range(2):
                        sl = slice(3 * hf_, 3 * hf_ + 3)
                        h.tt("dve", M1b[:, sl, :], w1_[:, hf_, :, :], cmask6[:, lv, sl, :], ALU.mult, [W1h[hf_], cmask6], [M1bh[hf_]])
                        h.tt("dve", M1pb[:, sl, :], w2_[:, hf_, :, :], cmask6[:, 7 + lv, sl, :], ALU.mult, [W2h[hf_], cmask6], [M1pbh[hf_]])
                    for hf_ in range(2):
                        hs = range(3 * hf_, 3 * hf_ + 3)
                        sl = slice(3 * hf_, 3 * hf_ + 3)
                        h.mm([(W1[:, hf_, 0:384], identb[:], Tc[:, sl, :].rearrange("p a l -> p (a l)"), True, False)]
                             + [(w1_[:, hf_, hd % 3, :], Xc[:, hd, :], M1b[:, hd, :], False, True) for hd in hs],
                             [identb, Tc, Xc, M1bh[hf_]], [W1h[hf_]])
                        h.mm([(W2[:, hf_, 0:384], identb[:], Xc[:, sl, :].rearrange("p a l -> p (a l)"), True, False)]
                             + [(w2_[:, hf_, hd % 3, :], Tc[:, hd, :], M1pb[:, hd, :], False, True) for hd in hs],
                             [identb, Tc, Xc, M1pbh[hf_]], [W2h[hf_]])
                    for hf_ in range(2):
                        sl = slice(3 * hf_, 3 * hf_ + 3)
                        h.cp("act", Tn[:, sl, :], w1_[:, hf_, :, :], [W1h[hf_]], [Tn])
                        h.cp("act", Xn[:, sl, :], w2_[:, hf_, :, :], [W2h[hf_]], [Xn])
                    Tc, Xc, Tn, Xn = Tn, Xn, Tc, Xc
                Xb = Xc
                if stop < 4:
                    h.memset("pool", yo_[:, :, tc], 0.0, [yo_])
                    continue
                h.tr([(PB[:, j * 128:(j + 1) * 128], kn_[:, j, tc]) for j in range(3)], identb[:], [kn_, identb], [PB])
                h.tt("dve", x3(kdec[:]), x3(PB[:, 0:384]), s3(dkk[:], 6, 64), ALU.mult, [PB, dkk], [kdec])
                h.tr([(W0[:, 0, j * 128:(j + 1) * 128], v_[:, j, tc]) for j in range(3)], identf[:], [v_, identf], [W0])
                h.cp("act", v_tm[:], W0[:, 0, 0:384], [W0], [v_tm])
                if stop < 5:
                    h.memset("pool", yo_[:, :, tc], 0.0, [yo_])
                    continue
                h.mm([(W1[:, 0, j * 128:(j + 1) * 128], kn_[:, j, tc], Sb[:, j, :], True, True) for j in range(3)], [kn_, Sb], [W1a, W1b])
                h.tt("dve", x3(rr_[:]), x3(W1[:, 0, 0:384]), s3(egc[:], 6, 64), ALU.mult, [W1a, W1b, egc], [rr_])
                h.tt("pool", rr_[:], rr_[:], v_tm[:], ALU.subtract, [rr_, v_tm], [rr_])
                h.tt("pool", x3(rb[:]), x3(rr_[:]), s3(nbeta[:], 6, 64), ALU.mult, [rr_, nbeta], [rb])
                h.mm([(W2[:, 0, hd * 64:(hd + 1) * 64], Xb[:, hd, :], rb[:, hd * 64:(hd + 1) * 64], True, True) for hd in range(6)], [Xb, rb], [W2a, W2b])
                h.cp("act", vnb[:], W2[:, 0, 0:384], [W2a, W2b], [vnb])
                h.mm([(W1[:, 0, j * 128:(j + 1) * 128], qn_[:, j, tc], Sb[:, j, :], True, True) for j in range(3)], [qn_, Sb], [W1a, W1b])
                h.tt("dve", x3(oa[:]), x3(W1[:, 0, 0:384]), s3(egc[:], 6, 64), ALU.mult, [W1a, W1b, egc], [oa])
                h.mm([(W2[:, 0, hd * 64:(hd + 1) * 64], attnT[:, hd, :], vnb[:, hd * 64:(hd + 1) * 64], True, True) for hd in range(6)], [attnT, vnb], [W2a, W2b])
                h.tt("dve", o[:], W2[:, 0, 0:384], oa[:], ALU.add, [W2a, W2b, oa], [o])
                h.mm([(W0[:, 0, j * 128:(j + 1) * 128], kdec[:, j * 128:(j + 1) * 128], vnb[:, j * 128:(j + 1) * 128], True, True) for j in range(3)],
                     [kdec, vnb], [W0])
                h.tt("dve", tmpS[:], W0[:, 0, 0:384].rearrange("p (j c) -> p j c", j=3), b3(masks["bd"][:], 3, 128), ALU.mult, [W0, masks["bd"]], [tmpS])
                h.tt("pool", Sf[:], Sf[:], s3(gtc[:], 3, 128), ALU.mult, [Sf, gtc], [Sf])
                h.tt("pool", Sf[:], Sf[:], tmpS[:], ALU.add, [Sf, tmpS], [Sf])
                h.cp("act", Sb[:], Sf[:], [Sf], [Sb])
                if stop < 6:
                    h.memset("pool", yo_[:, :, tc], 0.0, [yo_])
                    continue
                h.tt("pool", sq[:], o[:], o[:], ALU.mult, [o], [sq])
                h.reduce(ss6[:], x3(sq[:]), ALU.add, [sq], [ss6])
                h.act(rs6[:], ss6[:], AF.Ln, [ss6, epsb], [rs6], bias=epsb[:, 0:1], scale=1.0 / 64)
                h.act(rs6[:], rs6[:], AF.Exp, [rs6], [rs6], scale=-0.5)
                h.tt("dve", x3(o[:]), x3(o[:]), s3(rs6[:], 6, 64), ALU.mult, [o, rs6], [o])
                h.tt("pool", x3(o[:]), x3(o[:]), b3(gnw[:], 6, 64), ALU.mult, [o, gnw], [o])
                h.act(ez[:], pj_[:, ti, 0:384], AF.Exp, [pj_], [ez], scale=-1.0)
                h.ts("pool", ez[:], ez[:], 1.0, None, ALU.add, None, [ez], [ez])
                h.tt("pool", zg[:], o[:], pj_[:, ti, 0:384], ALU.mult, [o, pj_], [zg])
                h.recip(ez[:], ez[:], [ez], [ez])
                h.tt("pool", yb[:], zg[:], ez[:], ALU.mult, [zg, ez], [yb])
                h.tr([(PB[:, j * 128:(j + 1) * 128], yb[:, j * 128:(j + 1) * 128]) for j in range(3)], identb[:], [yb, identb], [PB])
                h.cp("act", yo_[:, :, tc], PB[:, 0:384].rearrange("p (j t) -> p j t", j=3), [PB], [yo_])
            h.dma("sp", k.y_d.t[256:640, cols].rearrange("(j p) t -> p j t", p=128), yo_[:], [yo_], [k.r_y[1][mt]], yo_)
        P.end_phase()


class K:
    def __init__(self, T, L, flags):
        self.T = T
        self.L = L
        self.NT = T // 128
        self.flags = flags


def bcast(ap, shape):
    return ap.to_broadcast(list(shape))


def build(T, L, flags=None):
    flags = flags or {}
    nc = bass.Bass("TRN2", target_bir_lowering=False)
    k = K(T, L, flags)
    NT = T // 128
    with contextlib.ExitStack() as es0:
        P = Prog(nc, es0)
        P.verbose = bool(flags.get("verbose"))
        k.P = P
        inp = {}

        def ein(name, shape):
            inp[name] = P.dram(name, shape, F32, "ExternalInput")
            return inp[name]

        x_in = ein("x", [T, D])
        c_in = ein("c", [1, D])
        w_ada = ein("w_ada", [L, D, 6 * D])
        b_ada = ein("b_ada", [L, 6 * D])
        norm_mix = ein("norm_mix", [L, D])
        norm_ffn = ein("norm_ffn", [L, D])
        norm_final = ein("norm_final", [1, D])
        w_rt = ein("w_rt", [L, D, 36])
        b_rt = ein("b_rt", [L, 36])
        w_gate = ein("moe_w_gate", [L, NEXP, D, DEXP])
        w_up = ein("moe_w_up", [L, NEXP, D, DEXP])
        w_down = ein("moe_w_down", [L, NEXP, DEXP, D])
        ein("w_in_f", [L, D, 2304])
        ein("w_in_t", [L, D, 786])
        ein("w_out", [L, D, D])
        ein("conv_w", [L, 128, 16, 4])
        ein("conv_b", [L, 128, 16])
        ein("bias12", [L, 12])
        ein("alog12", [L, 12])
        ein("ssd_d", [L, 6])
        ein("ssd_norm", [L, 384])
        ein("gdn_norm", [L, 64])
        ein("gdn_cmask", [14, 128, 128])
        ein("s5_are", [L, 128, 8])
        ein("s5_aim", [L, 128, 8])
        ein("s5_ldt", [L, 128, 8])
        ein("s5_dcol", [L, 128, 2])
        ein("s5_ncol", [L, 128, 2])
        ein("s5_bT_re", [L, 8, 128, 128])
        ein("s5_bT_im", [L, 8, 128, 128])
        ein("s5_cT_re", [L, 8, 128, 128])
        ein("s5_cT_im", [L, 8, 128, 128])
        ein("s5_w_glu", [L, 256, 256])
        out = P.dram("out", [T, D], F32, "ExternalOutput")
        k.u_d = P.dram("u_d", [256, T], F32)
        k.qn_d = P.dram("qn_d", [384, T], BF16)
        k.kn_d = P.dram("kn_d", [384, T], BF16)
        k.v_d = P.dram("v_d", [384, T], F32)
        k.xs_d = P.dram("xs_d", [384, T], F32)
        k.B_d = P.dram("B_d", [256, T], BF16)
        k.C_d = P.dram("C_d", [256, T], BF16)
        k.pt_d = P.dram("pt_d", [T, 786], F32)
        k.y_d = P.dram("y_d", [D, T], BF16)
        k.r_pre = [P.region(f"pre_{i}") for i in range(max(1, T // 512))]
        k.r_y = [[P.region(f"y{j}_{i}") for i in range(max(1, T // 512))] for j in range(3)]
        k.h = H(P)
        h = k.h
        modv = P.dram("modv", [L, 6 * D], F32)
        dbg = flags.get("dbg", False)
        if dbg:
            dbg_coef = P.dram("dbg_coef", [T, 32], F32, "ExternalOutput")
            dbg_y = P.dram("dbg_y", [T, D], F32, "ExternalOutput")
            dbg_h = P.dram("dbg_h", [T, D], F32, "ExternalOutput")
            r_dbg = P.region("dbg")
        scr = [P.dram("xs0", [T, D], F32), P.dram("xs1", [T, D], F32)]
        k.inp = inp

        def regs(name):
            return [P.region(f"{name}_{i}") for i in range(NT)]
        r_x = regs("x")
        r_scr = [regs("xs0"), regs("xs1")]
        r_out = regs("out")
        r_modv = P.region("modv")

        identf = P.sb(es0, "identf", [128, 128], F32)
        identb = P.sb(es0, "identb", [128, 128], BF16)
        P.op("pool", lambda e: e.memset(identf[:], 0.0), writes=[identf])
        P.op("pool", lambda e: e.affine_select(out=identf[:], in_=identf[:], pattern=[[-1, 128]],
                                               compare_op=ALU.not_equal, fill=1.0, base=0,
                                               channel_multiplier=1),
             reads=[identf], writes=[identf])
        P.op("dve", lambda e: e.tensor_copy(out=identb[:], in_=identf[:]), reads=[identf], writes=[identb])
        k.identf, k.identb = identf, identb

        with contextlib.ExitStack() as es:
            ccol = P.sb(es, "ccol", [128, 8], F32)
            cb = P.sb(es, "cb", [128, 8, 128], BF16)
            wa = [P.sb(es, f"wa{i}", [128, 8, 512], BF16) for i in range(2)]
            pm = [P.ps(es, f"pm{i}", [128, 512], F32) for i in range(2)]
            brow = [P.sb(es, f"brow{i}", [1, 512], F32) for i in range(2)]
            mrow = [P.sb(es, f"mrow{i}", [1, 512], F32) for i in range(2)]
            P.op("sp", lambda e: e.dma_start(out=ccol[:], in_=c_in.t.rearrange("o (k p) -> p (o k)", p=128),
                                             allow_slow_non_contiguous=True),
                 writes=[ccol], dma=ccol)
            P.op("act", lambda e: e.activation(out=ccol[:], in_=ccol[:], func=AF.Silu), reads=[ccol], writes=[ccol])
            P.op("dve", lambda e: e.tensor_copy(out=cb[:], in_=bcast(ccol[:].unsqueeze(2), [128, 8, 128])),
                 reads=[ccol], writes=[cb])
            it = 0
            for l in range(L):
                for n in range(12):
                    i = it % 2
                    it += 1
                    P.op("pool", lambda e, l=l, n=n, i=i: e.dma_start(
                        out=wa[i][:], in_=w_ada.t[l, :, n * 512:(n + 1) * 512].rearrange("(k p) n -> p k n", p=128)),
                        writes=[wa[i]], dma=wa[i])
                    P.op("sp", lambda e, l=l, n=n, i=i: e.dma_start(
                        out=brow[i][:], in_=b_ada.t[l:l + 1, n * 512:(n + 1) * 512]),
                        writes=[brow[i]], dma=brow[i])

                    def mm(e, i=i):
                        r = None
                        for kk in range(8):
                            r = e.matmul(pm[i][:], lhsT=cb[:, kk, :], rhs=wa[i][:, kk, :], start=(kk == 0), stop=(kk == 7))
                        return r
                    P.op("pe", mm, reads=[cb, wa[i]], writes=[pm[i]])
                    P.op("dve", lambda e, i=i: e.tensor_tensor(out=mrow[i][:], in0=pm[i][0:1, :], in1=brow[i][:], op=ALU.add),
                         reads=[pm[i], brow[i]], writes=[mrow[i]])
                    P.op("sp", lambda e, l=l, n=n, i=i: e.dma_start(
                        out=modv.t[l:l + 1, n * 512:(n + 1) * 512], in_=mrow[i][:]),
                        reads=[mrow[i]], writes=[r_modv], dma=mrow[i])
            P.end_phase()

        def load_row(dst, src_ap, extra_reads=()):
            P.op("sp", lambda e: e.dma_start(out=dst[:], in_=src_ap.partition_broadcast(128)),
                 reads=list(extra_reads), writes=[dst], dma=dst)

        def norm_consts(es, l, nw, i_scale, i_shift, tag):
            A = P.sb(es, f"A{tag}", [128, D], F32)
            B = P.sb(es, f"B{tag}", [128, D], F32)
            W = P.sb(es, f"W{tag}", [128, D], F32)
            load_row(A, modv.t[l:l + 1, i_scale * D:(i_scale + 1) * D], [r_modv])
            load_row(B, modv.t[l:l + 1, i_shift * D:(i_shift + 1) * D], [r_modv])
            load_row(W, nw)
            P.op("dve", lambda e: e.scalar_tensor_tensor(out=A[:], in0=A[:], scalar=1.0, in1=W[:], op0=ALU.add, op1=ALU.mult),
                 reads=[A, W], writes=[A])
            return A, B

        def rms_mod(xt, A, B, hout, ssq, rstd, junk, tmp):
            P.op("act", lambda e: e.activation(out=junk[:], in_=xt[:], func=AF.Square, accum_out=ssq[:]),
                 reads=[xt], writes=[junk, ssq], est=0.9)
            P.op("dve", lambda e: e.tensor_scalar(out=rstd[:], in0=ssq[:], scalar1=1.0 / D, scalar2=EPS, op0=ALU.mult, op1=ALU.add),
                 reads=[ssq], writes=[rstd])
            P.op("act", lambda e: e.activation(out=rstd[:], in_=rstd[:], func=AF.Sqrt), reads=[rstd], writes=[rstd])
            P.op("dve", lambda e: e.reciprocal(out=rstd[:], in_=rstd[:]), reads=[rstd], writes=[rstd])
            if B is None:
                P.op("dve", lambda e: e.scalar_tensor_tensor(out=hout[:], in0=xt[:], scalar=rstd[:, 0:1], in1=A[:], op0=ALU.mult, op1=ALU.mult),
                     reads=[xt, rstd, A], writes=[hout], est=1.15)
            else:
                P.op("dve", lambda e: e.scalar_tensor_tensor(out=tmp[:], in0=xt[:], scalar=rstd[:, 0:1], in1=A[:], op0=ALU.mult, op1=ALU.mult),
                     reads=[xt, rstd, A], writes=[tmp], est=1.15)
                P.op("pool", lambda e: e.tensor_tensor(out=hout[:], in0=tmp[:], in1=B[:], op=ALU.add),
                     reads=[tmp, B], writes=[hout], est=1.3)

        k.modv, k.r_modv = modv, r_modv
        k.load_row, k.norm_consts, k.rms_mod = load_row, norm_consts, rms_mod
        cur = (x_in, r_x)
        nxt_i = 0

        def next_dst():
            nonlocal nxt_i
            d = (scr[nxt_i], r_scr[nxt_i])
            nxt_i ^= 1
            return d

        for l in range(L):
            if flags.get("mixer", True):
                dstt = next_dst()
                mixer_layer(k, l, cur, dstt)
                cur = dstt
            if flags.get("moe", True):
                src, rsrc = cur
                dst, rdst = next_dst()
                SBT = min(16, NT)
                with contextlib.ExitStack() as es:
                    A, B = norm_consts(es, l, norm_ffn.t[l:l + 1, :], 4, 3, "f")
                    G = P.sb(es, "Gf", [128, D], F32)
                    load_row(G, modv.t[l:l + 1, 5 * D:6 * D], [r_modv])
                    brt = P.sb(es, "brt", [128, 36], F32)
                    load_row(brt, b_rt.t[l:l + 1, :])
                    wrt = P.sb(es, "wrt", [128, 8, 36], F32)
                    P.op("sp", lambda e: e.dma_start(out=wrt[:], in_=w_rt.t[l].rearrange("(k p) n -> p k n", p=128)),
                         writes=[wrt], dma=wrt)
                    hT = P.sb(es, "hT", [128, 8, SBT * 128], BF16)
                    yacc = [P.sb(es, f"yacc{i}", [128, D], F32) for i in range(SBT)]
                    coef = P.sb(es, "coef", [128, SBT, 32], F32)
                    xt = [P.sb(es, f"xt{i}", [128, D], F32) for i in range(2)]
                    R_hf = Rot(P, es, "hf", [128, D], F32)
                    R_tmp = Rot(P, es, "tmpf", [128, D], F32)
                    R_junk = Rot(P, es, "junkf", [128, D], F32)
                    R_hTf = Rot(P, es, "hTf", [128, 8, 128], F32)
                    R_ssq = Rot(P, es, "ssq", [128, 1], F32)
                    R_rstd = Rot(P, es, "rstd", [128, 1], F32)
                    R_lg = Rot(P, es, "lg", [128, 36], F32)
                    R_sm = Rot(P, es, "sm", [128, 16], F32)
                    R_gm = Rot(P, es, "gm", [128, 4], F32)
                    R_gex = Rot(P, es, "gex", [128, 4], F32)
                    R_le4 = Rot(P, es, "le4", [128, 4, 8], F32)
                    R_les = Rot(P, es, "les", [128, 8], F32)
                    R_le2 = Rot(P, es, "le2", [128, 8], F32)
                    R_mk1 = Rot(P, es, "mk1", [128, 8], F32)
                    R_mk2 = Rot(P, es, "mk2", [128, 8], F32)
                    R_csel = Rot(P, es, "csel", [128, 8], F32)
                    wg = [P.sb(es, f"wg{i}", [128, 8, DEXP], BF16) for i in range(2)]
                    wu = [P.sb(es, f"wu{i}", [128, 8, DEXP], BF16) for i in range(2)]
                    wd = [P.sb(es, f"wd{i}", [128, 2, D], BF16) for i in range(2)]
                    sg = [P.sb(es, f"sg{i}", [128, 512], F32) for i in range(2)]
                    hid = [P.sb(es, f"hid{i}", [128, 2, 512], BF16) for i in range(2)]
                    ptr = P.ps(es, "ptr", [128, 8, 128], F32)
                    pg = [P.ps(es, f"pg{i}", [128, 512], F32) for i in range(2)]
                    pu = [P.ps(es, f"pu{i}", [128, 512], F32) for i in range(2)]
                    py = [P.ps(es, f"py{i}", [128, 512], F32) for i in range(2)]
                    py.append(Buf(ptr.t[:, 0:4, :].rearrange("p a b -> p (a b)"), "py2"))
                    py.append(Buf(ptr.t[:, 4:8, :].rearrange("p a b -> p (a b)"), "py3"))
                    ptrA, ptrB = py[2], py[3]

                    wcnt = 0
                    for sb0 in range(0, NT, SBT):
                        def router_tile(t, ti, xb, hf, tmp, junk, hTf, ssq, rstd, lg, sm, gm, gex, le4, les, le2, mk1, mk2, csel):
                                P.op("sp", lambda e, t=t, xb=xb: e.dma_start(out=xb[:], in_=src.t[t * 128:(t + 1) * 128, :]),
                                     reads=[rsrc[t]], writes=[xb], dma=xb)
                                rms_mod(xb, A, B, hf, ssq, rstd, junk, tmp)

                                if dbg and l == 0:
                                    P.op("sp", lambda e, t=t: e.dma_start(out=dbg_h.t[t * 128:(t + 1) * 128, :], in_=hf[:]),
                                         reads=[hf], writes=[r_dbg], dma=hf)

                                def trf(e):
                                    r = None
                                    for kk in range(8):
                                        r = e.transpose(out=ptr[:, kk, :], in_=hf[:, kk * 128:(kk + 1) * 128], identity=identf[:])
                                    return r
                                P.op("pe", trf, reads=[hf, identf], writes=[ptrA, ptrB])
                                P.op("act", lambda e: e.copy(out=hTf[:], in_=ptr[:]), reads=[ptrA, ptrB], writes=[hTf])
                                P.op("dve", lambda e, ti=ti: e.tensor_copy(out=hT[:, :, ti * 128:(ti + 1) * 128], in_=hTf[:]),
                                     reads=[hTf], writes=[hT])

                                def mrt(e):
                                    r = None
                                    for kk in range(8):
                                        r = e.matmul(ptr[:, 0, 0:36], lhsT=hTf[:, kk, :], rhs=wrt[:, kk, :], start=(kk == 0), stop=(kk == 7))
                                    return r
                                P.op("pe", mrt, reads=[hTf, wrt], writes=[ptrA, ptrB])
                                P.op("dve", lambda e: e.tensor_tensor(out=lg[:], in0=ptr[:, 0, 0:36], in1=brt[:], op=ALU.add),
                                     reads=[ptrA, ptrB, brt], writes=[lg])
                                P.op("dve", lambda e: e.tensor_reduce(out=sm[:, 0:1], in_=lg[:, 0:4], axis=AX.X, op=ALU.max),
                                     reads=[lg], writes=[sm])
                                P.op("dve", lambda e: e.tensor_scalar(out=gm[:], in0=lg[:, 0:4], scalar1=sm[:, 0:1], scalar2=None, op0=ALU.is_equal),
                                     reads=[lg, sm], writes=[gm])
                                P.op("dve", lambda e: e.tensor_scalar(out=sm[:, 1:2], in0=sm[:, 0:1], scalar1=-1.0, scalar2=None, op0=ALU.mult),
                                     reads=[sm], writes=[sm])
                                P.op("act", lambda e: e.activation(out=gex[:], in_=lg[:, 0:4], func=AF.Exp, bias=sm[:, 1:2], accum_out=sm[:, 2:3]),
                                     reads=[lg, sm], writes=[gex, sm])
                                P.op("dve", lambda e: e.reciprocal(out=sm[:, 3:4], in_=sm[:, 2:3]), reads=[sm], writes=[sm])
                                P.op("dve", lambda e: e.tensor_tensor(out=le4[:], in0=lg[:, 4:36].rearrange("p (g e) -> p g e", g=4),
                                                                      in1=bcast(gm[:].unsqueeze(2), [128, 4, 8]), op=ALU.mult),
                                     reads=[lg, gm], writes=[le4])
                                P.op("dve", lambda e: e.tensor_reduce(out=les[:], in_=le4[:].rearrange("p g e -> p e g"), axis=AX.X, op=ALU.add),
                                     reads=[le4], writes=[les])
                                P.op("dve", lambda e: e.tensor_reduce(out=sm[:, 4:5], in_=les[:], axis=AX.X, op=ALU.max), reads=[les], writes=[sm])
                                P.op("dve", lambda e: e.tensor_scalar(out=mk1[:], in0=les[:], scalar1=sm[:, 4:5], scalar2=None, op0=ALU.is_equal),
                                     reads=[les, sm], writes=[mk1])
                                P.op("dve", lambda e: e.scalar_tensor_tensor(out=le2[:], in0=mk1[:], scalar=-1e30, in1=les[:], op0=ALU.mult, op1=ALU.add),
                                     reads=[mk1, les], writes=[le2])
                                P.op("dve", lambda e: e.tensor_reduce(out=sm[:, 5:6], in_=le2[:], axis=AX.X, op=ALU.max), reads=[le2], writes=[sm])
                                P.op("dve", lambda e: e.tensor_scalar(out=mk2[:], in0=le2[:], scalar1=sm[:, 5:6], scalar2=None, op0=ALU.is_equal),
                                     reads=[le2, sm], writes=[mk2])
                                P.op("dve", lambda e: e.tensor_tensor(out=sm[:, 6:7], in0=sm[:, 4:5], in1=sm[:, 5:6], op=ALU.subtract),
                                     reads=[sm], writes=[sm])
                                P.op("act", lambda e: e.activation(out=sm[:, 7:8], in_=sm[:, 6:7], func=AF.Sigmoid), reads=[sm], writes=[sm])
                                P.op("act", lambda e: e.activation(out=sm[:, 8:9], in_=sm[:, 6:7], func=AF.Sigmoid, scale=-1.0), reads=[sm], writes=[sm])
                                P.op("dve", lambda e: e.tensor_scalar(out=sm[:, 7:9], in0=sm[:, 7:9], scalar1=sm[:, 3:4], scalar2=None, op0=ALU.mult),
                                     reads=[sm], writes=[sm])
                                P.op("dve", lambda e: e.tensor_scalar(out=csel[:], in0=mk1[:], scalar1=sm[:, 7:8], scalar2=None, op0=ALU.mult),
                                     reads=[mk1, sm], writes=[csel])
                                P.op("dve", lambda e: e.scalar_tensor_tensor(out=csel[:], in0=mk2[:], scalar=sm[:, 8:9], in1=csel[:], op0=ALU.mult, op1=ALU.add),
                                     reads=[mk2, sm, csel], writes=[csel])
                                P.op("dve", lambda e, ti=ti: e.tensor_tensor(out=coef[:, ti, :].rearrange("p (g e) -> p g e", g=4),
                                                                             in0=bcast(gm[:].unsqueeze(2), [128, 4, 8]),
                                                                             in1=bcast(csel[:].unsqueeze(1), [128, 4, 8]), op=ALU.mult),
                                     reads=[gm, csel], writes=[coef])

                        for ti in range(SBT):
                            t = sb0 + ti
                            router_tile(t, ti, xt[t % 2], R_hf.at(t), R_tmp.at(t), R_junk.at(t), R_hTf.at(t), R_ssq.at(t), R_rstd.at(t), R_lg.at(t), R_sm.at(t), R_gm.at(t), R_gex.at(t), R_le4.at(t), R_les.at(t), R_le2.at(t), R_mk1.at(t), R_mk2.at(t), R_csel.at(t))
                        nblk = (SBT * 128 + 511) // 512
                        seq = [(ex, blk) for ex in range(NEXP) for blk in range(nblk)]

                        def load_w(ex):
                            wi = ex % 2
                            h.dma("pool", wg[wi][:], w_gate.t[l, ex].rearrange("(k p) n -> p k n", p=128), [], [wg[wi]], wg[wi])
                            h.dma("pool", wu[wi][:], w_up.t[l, ex].rearrange("(k p) n -> p k n", p=128), [], [wu[wi]], wu[wi])
                            h.dma("pool", wd[wi][:], w_down.t[l, ex].rearrange("(k p) n -> p k n", p=128), [], [wd[wi]], wd[wi])

                        def GU(i):
                            ex, blk = seq[i]
                            wi, bi = ex % 2, i % 2
                            c0 = blk * 512
                            cw = min(512, SBT * 128 - c0)
                            for fc in range(2):
                                fs = slice(fc * 128, (fc + 1) * 128)
                                h.mm([(pg[fc][:, 0:cw], wg[wi][:, kk, fs], hT[:, kk, c0:c0 + cw], kk == 0, kk == 7) for kk in range(8)],
                                     [wg[wi], hT], [pg[fc]])
                                h.act(sg[fc][:, 0:cw], pg[fc][:, 0:cw], AF.Silu, [pg[fc]], [sg[fc]])
                                yield
                                h.mm([(pu[fc][:, 0:cw], wu[wi][:, kk, fs], hT[:, kk, c0:c0 + cw], kk == 0, kk == 7) for kk in range(8)],
                                     [wu[wi], hT], [pu[fc]])
                                h.tt("dve", hid[bi][:, fc, 0:cw], sg[fc][:, 0:cw], pu[fc][:, 0:cw], ALU.mult, [sg[fc], pu[fc]], [hid[bi]])
                                yield

                        def DN(i):
                            ex, blk = seq[i]
                            wi, bi = ex % 2, i % 2
                            c0 = blk * 512
                            cw = min(512, SBT * 128 - c0)
                            for st in range(cw // 128):
                                ti = blk * 4 + st
                                for n2 in range(2):
                                    pi = (st * 2 + n2) % 4
                                    ns = slice(n2 * 512, (n2 + 1) * 512)
                                    h.mm([(py[pi][:], hid[bi][:, fc, st * 128:(st + 1) * 128], wd[wi][:, fc, ns], fc == 0, fc == 1) for fc in range(2)],
                                         [hid[bi], wd[wi]], [py[pi]])
                                    if ex == 0:
                                        h.ts("dve", yacc[ti][:, ns], py[pi][:], coef[:, ti, ex:ex + 1], None, ALU.mult, None, [py[pi], coef], [yacc[ti]])
                                    else:
                                        h.stt("dve", yacc[ti][:, ns], py[pi][:], coef[:, ti, ex:ex + 1], yacc[ti][:, ns], ALU.mult, ALU.add,
                                              [py[pi], coef, yacc[ti]], [yacc[ti]])
                                    yield
                            if blk == nblk - 1 and ex + 2 < NEXP:
                                load_w(ex + 2)

                        def drain(g):
                            for _ in g:
                                pass

                        def step(g, n):
                            for _ in range(n):
                                if next(g, "END") == "END":
                                    return

                        load_w(0)
                        load_w(1)
                        drain(GU(0))
                        for i in range(len(seq)):
                            gd = DN(i)
                            if i + 1 < len(seq):
                                gg = GU(i + 1)
                                for _ in range(4):
                                    step(gg, 1)
                                    step(gd, 2)
                                drain(gg)
                            drain(gd)
                        for ti in range(SBT):
                            t = sb0 + ti
                            if dbg and l == 0:
                                P.op("sp", lambda e, t=t, ti=ti: e.dma_start(out=dbg_y.t[t * 128:(t + 1) * 128, :], in_=yacc[ti][:]),
                                     reads=[yacc[ti]], writes=[r_dbg], dma=yacc[ti])
                                P.op("sp", lambda e, t=t, ti=ti: e.dma_start(out=dbg_coef.t[t * 128:(t + 1) * 128, :], in_=coef[:, ti, :]),
                                     reads=[coef], writes=[r_dbg], dma=coef)
                            xb = xt[t % 2]
                            P.op("sp", lambda e, t=t, xb=xb: e.dma_start(out=xb[:], in_=src.t[t * 128:(t + 1) * 128, :]),
                                 reads=[rsrc[t]], writes=[xb], dma=xb)
                            P.op("pool", lambda e, ti=ti: e.tensor_tensor(out=yacc[ti][:], in0=yacc[ti][:], in1=G[:], op=ALU.mult),
                                 reads=[yacc[ti], G], writes=[yacc[ti]])
                            P.op("dve", lambda e, ti=ti, xb=xb: e.tensor_tensor(out=yacc[ti][:], in0=yacc[ti][:], in1=xb[:], op=ALU.add),
                                 reads=[yacc[ti], xb], writes=[yacc[ti]])
                            P.op("sp", lambda e, t=t, ti=ti: e.dma_start(out=dst.t[t * 128:(t + 1) * 128, :], in_=yacc[ti][:]),
                                 reads=[yacc[ti]], writes=[rdst[t]], dma=yacc[ti])
                    P.end_phase()
                cur = (dst, rdst)

        src, rsrc = cur
        with contextlib.ExitStack() as es:
            Wn = P.sb(es, "Wn", [128, D], F32)
            load_row(Wn, norm_final.t[0:1, :])
            xt = [P.sb(es, f"xtn{i}", [128, D], F32) for i in range(2)]
            ho = [P.sb(es, f"hon{i}", [128, D], F32) for i in range(2)]
            junk = P.sb(es, "junkn", [128, D], F32)
            ssq = P.sb(es, "ssqn", [128, 1], F32)
            rstd = P.sb(es, "rstdn", [128, 1], F32)
            for t in range(NT):
                xb = xt[t % 2]
                hb = ho[t % 2]
                P.op("sp", lambda e, t=t, xb=xb: e.dma_start(out=xb[:], in_=src.t[t * 128:(t + 1) * 128, :]),
                     reads=[rsrc[t]], writes=[xb], dma=xb)
                rms_mod(xb, Wn, None, hb, ssq, rstd, junk, None)
                P.op("sp", lambda e, t=t, hb=hb: e.dma_start(out=out.t[t * 128:(t + 1) * 128, :], in_=hb[:]),
                     reads=[hb], writes=[r_out[t]], dma=hb)
            P.final_wait("sp", r_out)
            P.end_phase()
    return nc


def host_inputs(inputs, L, T):
    f = lambda a: np.ascontiguousarray(np.asarray(a, dtype=np.float32))
    w_rt = f(np.concatenate([inputs["moe_w_grp"][:L], inputs["moe_w_rt"][:L]], axis=-1))
    b_rt = f(np.concatenate([inputs["moe_b_grp"][:L], inputs["moe_b_rt"][:L]], axis=-1))
    w_in = np.asarray(inputs["w_in"][:L], dtype=np.float32)
    w_in_f = np.concatenate([w_in[:, :, 0:1408], w_in[:, :, 2188:3084]], axis=-1)
    w_in_t = np.concatenate([w_in[:, :, 1408:1792], w_in[:, :, 1804:2188], w_in[:, :, 1792:1798],
                             w_in[:, :, 3084:3090], w_in[:, :, 1798:1804]], axis=-1)
    gcw = np.asarray(inputs["gdn_conv_w"][:L], dtype=np.float32)
    scw = np.asarray(inputs["ssd_conv_w"][:L], dtype=np.float32)
    cw = np.concatenate([gcw, scw], axis=-1)
    conv_w = cw.reshape(L, 4, 16, 128).transpose(0, 3, 2, 1)
    cb = np.concatenate([np.zeros((L, 1152), np.float32), np.asarray(inputs["ssd_conv_b"][:L], dtype=np.float32)], axis=-1)
    conv_b = cb.reshape(L, 16, 128).transpose(0, 2, 1)
    bias12 = np.concatenate([inputs["gdn_dt_bias"][:L], inputs["ssd_dt_bias"][:L]], axis=-1)
    alog12 = np.concatenate([inputs["gdn_a_log"][:L], inputs["ssd_a_log"][:L]], axis=-1)
    def st_layout(a):
        a = np.asarray(a[:L], dtype=np.float32)
        return a.reshape(L, 8, 2, 64).transpose(0, 2, 3, 1).reshape(L, 128, 8)
    ldt = np.repeat(np.asarray(inputs["s5_log_dt"][:L], dtype=np.float32)[:, :, None], 64, axis=2)
    def bT_layout(b):
        b = np.asarray(b[:L], dtype=np.float32)
        o = np.zeros((L, 8, 128, 128), np.float32)
        for sc in range(8):
            for gl in range(2):
                r0 = 32 * (sc % 4) + 16 * gl
                o[:, sc, r0:r0 + 16, gl * 64:(gl + 1) * 64] = b[:, 2 * sc + gl].transpose(0, 2, 1)
        return o
    def cT_layout(c):
        c = np.asarray(c[:L], dtype=np.float32)
        o = np.zeros((L, 8, 128, 128), np.float32)
        for sc in range(8):
            for gl in range(2):
                r0 = 32 * (sc % 4) + 16 * gl
                o[:, sc, gl * 64:(gl + 1) * 64, r0:r0 + 16] = c[:, 2 * sc + gl].transpose(0, 2, 1)
        return o
    ii = np.arange(128)[:, None]
    jj = np.arange(128)[None, :]
    cm = []
    for lv in range(7):
        s_ = 1 << lv
        cm.append(((ii // (2 * s_) == jj // (2 * s_)) & (ii % (2 * s_) >= s_) & (jj % (2 * s_) < s_)).astype(np.float32))
    cmask = np.stack(cm + [m.T for m in cm], axis=0)
    col2 = lambda a: np.asarray(a[:L], dtype=np.float32).reshape(L, 2, 128).transpose(0, 2, 1)
    shared = {
        "gdn_cmask": f(cmask),
        "s5_are": f(st_layout(inputs["s5_a_re"])), "s5_aim": f(st_layout(inputs["s5_a_im"])), "s5_ldt": f(st_layout(ldt)),
        "s5_dcol": f(col2(inputs["s5_d"])), "s5_ncol": f(col2(inputs["s5_norm"])),
        "s5_bT_re": f(bT_layout(inputs["s5_b_re"])), "s5_bT_im": f(bT_layout(inputs["s5_b_im"])),
        "s5_cT_re": f(cT_layout(inputs["s5_c_re"])), "s5_cT_im": f(cT_layout(inputs["s5_c_im"])),
        "s5_w_glu": f(inputs["s5_w_glu"][:L]),
        "w_in_f": f(w_in_f), "w_in_t": f(w_in_t), "w_out": f(inputs["w_out"][:L]),
        "conv_w": f(conv_w), "conv_b": f(conv_b), "bias12": f(bias12), "alog12": f(alog12),
        "ssd_d": f(inputs["ssd_d"][:L]), "ssd_norm": f(inputs["ssd_norm"][:L]), "gdn_norm": f(inputs["gdn_norm"][:L]),
        "w_ada": f(inputs["w_ada"][:L]), "b_ada": f(inputs["b_ada"][:L]),
        "norm_mix": f(inputs["norm_mix"][:L]), "norm_ffn": f(inputs["norm_ffn"][:L]),
        "norm_final": f(inputs["norm_final"]).reshape(1, D),
        "w_rt": w_rt, "b_rt": b_rt,
        "moe_w_gate": f(inputs["moe_w_gate"][:L]), "moe_w_up": f(inputs["moe_w_up"][:L]),
        "moe_w_down": f(inputs["moe_w_down"][:L]),
    }
    maps = []
    B = inputs["x"].shape[0]
    for b in range(B):
        m = dict(shared)
        m["x"] = f(inputs["x"][b, :T])
        m["c"] = f(inputs["c"][b]).reshape(1, D)
        maps.append(m)
    return maps


def run(inputs, L, T, flags=None, trace=False):
    nc = build(T, L, flags)
    maps = host_inputs(inputs, L, T)
    res = run_bass_kernel_spmd(nc, maps, core_ids=list(range(len(maps))))
    if flags and flags.get("dbg"):
        return res.results
    return np.stack([r["out"] for r in res.results], axis=0)


def kernel(**inputs):
    return run(inputs, 4, 4096).astype(np.float32)
```

```python
import contextlib
import math
import numpy as np
import concourse.bass as bass
import concourse.mybir as mybir
from concourse.bass_utils import run_bass_kernel_spmd

F32 = mybir.dt.float32
BF16 = mybir.dt.bfloat16
ALU = mybir.AluOpType
AF = mybir.ActivationFunctionType
AX = mybir.AxisListType

D = 1024
NEXP = 32
DEXP = 256
EPS = 1e-6
ENGS = ("pe", "act", "dve", "pool", "sp")


class Buf:
    def __init__(self, t, name, multi=False):
        self.t = t
        self.name = name
        self.w = {}
        self.r = {}
        self.sem = None
        self.dcnt = 0
        self.multi = multi

    def __getitem__(self, k):
        return self.t[k]


class Prog:
    SEM_LAT = 0.15

    def __init__(self, nc, es):
        self.nc = nc
        self.es = es
        self.ops = []
        self.sems = []
        self.esem = {}
        self.ecnt = {e: 0 for e in ENGS}
        self.waited = {e: {} for e in ENGS}
        for e in ENGS:
            if e != "sp":
                self.esem[e] = self.newsem("e_" + e)
        self.uid = 0
        self.dsem_pool = []
        self.dbufs = []
        self.phase_bufs = []

    def newsem(self, name):
        s = self.es.enter_context(self.nc.semaphore(name))
        self.sems.append(s)
        return len(self.sems) - 1

    def sb(self, es, name, shape, dtype):
        self.uid += 1
        name = f"{name}_{self.uid}"
        t = es.enter_context(self.nc.sbuf_tensor(name, list(shape), dtype))
        b = Buf(t, name)
        self.phase_bufs.append(b)
        return b

    def ps(self, es, name, shape, dtype):
        self.uid += 1
        name = f"{name}_{self.uid}"
        t = es.enter_context(self.nc.psum_tensor(name, list(shape), dtype))
        return Buf(t, name)

    def dram(self, name, shape, dtype, kind="Internal"):
        t = self.nc.dram_tensor(name, list(shape), dtype, kind=kind).ap()
        return Buf(t, name)

    def region(self, name):
        return Buf(None, name, multi=True)

    def op(self, eng, fn, reads=(), writes=(), dma=None, est=None):
        if est is None:
            est = {"pe": 1.0, "act": 0.5, "dve": 0.35, "pool": 0.45, "sp": 3.0}[eng] if dma is None else 3.0
        self.ops.append((eng, fn, tuple(reads), tuple(writes), dma, est))

    def final_wait(self, eng, bufs):
        pass

    def end_phase(self):
        ops = self.ops
        self.ops = []
        n = len(ops)
        lw, rd = {}, {}
        deps = [None] * n
        for i, (eng, fn, reads, writes, dma, est) in enumerate(ops):
            d = set()
            for b in reads:
                d.update(lw.get(id(b), ()))
            for b in writes:
                d.update(rd.get(id(b), ()))
                if not b.multi:
                    d.update(lw.get(id(b), ()))
            deps[i] = d
            for b in reads:
                rd.setdefault(id(b), []).append(i)
            for b in writes:
                if b.multi:
                    lw.setdefault(id(b), []).append(i)
                else:
                    lw[id(b)] = [i]
                    rd[id(b)] = []
        import heapq
        succ = [[] for _ in range(n)]
        indeg = [0] * n
        for i in range(n):
            indeg[i] = len(deps[i])
            for j in deps[i]:
                succ[j].append(i)
        ready_t = [0.0] * n
        fin = [0.0] * n
        start = [0.0] * n
        efree = {e: 0.0 for e in ENGS}
        heap = [(0.0, i) for i in range(n) if indeg[i] == 0]
        heapq.heapify(heap)
        order = {e: [] for e in ENGS}
        glob = []
        while heap:
            rt, i = heapq.heappop(heap)
            eng, fn, reads, writes, dma, est = ops[i]
            st = max(rt, efree[eng])
            start[i] = st
            if dma is not None:
                occ = 0.5 if eng == "pool" else 0.08
                efree[eng] = st + occ
                fin[i] = st + occ + est
            else:
                efree[eng] = st + est
                fin[i] = st + est
            order[eng].append(i)
            glob.append(i)
            for k2 in succ[i]:
                indeg[k2] -= 1
                if ready_t[k2] < fin[i] + self.SEM_LAT:
                    ready_t[k2] = fin[i] + self.SEM_LAT
                if indeg[k2] == 0:
                    heapq.heappush(heap, (ready_t[k2], k2))
        assert len(glob) == n, "dependency cycle"
        tok = [None] * n
        for e in ENGS:
            if e == "sp":
                continue
        waits_raw = [None] * n
        cnt = dict(self.ecnt)
        for i in glob:
            eng, fn, reads, writes, dma, est = ops[i]
            w = {}
            for j in deps[i]:
                dj = ops[j][4]
                if dj is None:
                    s_, v_ = tok[j]
                else:
                    s_, v_ = dj.sem, dj.dcnt
                if w.get(s_, 0) < v_:
                    w[s_] = v_
            waits_raw[i] = w
            if dma is None:
                cnt[eng] += 1
                tok[i] = (self.esem[eng], cnt[eng])
            else:
                if dma.sem is None:
                    if self.dsem_pool:
                        dma.sem, dma.dcnt = self.dsem_pool.pop()
                    else:
                        dma.sem = self.newsem("d_" + dma.name)
                        dma.dcnt = 0
                    self.dbufs.append(dma)
                dma.dcnt += 16
                tok[i] = (dma.sem, dma.dcnt)
        self.ecnt = cnt
        nc = self.nc
        sems = self.sems
        qs = {}
        for e in ENGS:
            wd = self.waited[e]
            q = []
            for i in order[e]:
                ws = []
                for s_, v_ in waits_raw[i].items():
                    if wd.get(s_, 0) < v_:
                        ws.append((s_, v_))
                        wd[s_] = v_
                inc = (tok[i][0], 16 if ops[i][4] is not None else 1)
                q.append((ws, ops[i][1], inc))
            qs[e] = q
        toks = {}
        for e, s_ in self.esem.items():
            if self.ecnt[e] > 0:
                toks[s_] = self.ecnt[e]
        for b in self.dbufs:
            toks[b.sem] = max(toks.get(b.sem, 0), b.dcnt)
        for e in ENGS:
            wd = self.waited[e]
            ws = []
            for s_, v_ in toks.items():
                if wd.get(s_, 0) < v_:
                    ws.append((s_, v_))
                    wd[s_] = v_
            if ws:
                qs[e].append((ws, None, None))
        for b in self.phase_bufs:
            if b.sem is not None:
                self.dsem_pool.append((b.sem, b.dcnt))
                self.dbufs.remove(b)
                b.sem = None
        self.phase_bufs = []

        def mk(e):
            def f(eng):
                for waits, fn, inc in qs[e]:
                    for s_, v_ in waits:
                        eng.wait_ge(sems[s_], v_)
                    if fn is None:
                        continue
                    ins = fn(eng)
                    ins.then_inc(sems[inc[0]], inc[1])
            return f

        with nc.Block() as block:
            block.sync(mk("sp"))
            block.scalar(mk("act"))
            block.vector(mk("dve"))
            block.gpsimd(mk("pool"))
            block.tensor(mk("pe"))
        self.last_makespan = max(efree.values()) if n else 0.0
        if getattr(self, "verbose", False):
            busy = {e: 0.0 for e in ENGS}
            for i in range(n):
                if ops[i][4] is None:
                    busy[ops[i][0]] += ops[i][5]
            print(f"[phase] n_ops={n} model_makespan={self.last_makespan:.0f}us busy=" +
                  " ".join(f"{e}:{busy[e]:.0f}" for e in ENGS), flush=True)


def _fsz(ap):
    n = 1
    for d in ap.shape[1:]:
        n *= int(d)
    return n


def _est(eng, ap, psum=False):
    n = _fsz(ap)
    if eng == "dve":
        return (60 + n) / 960.0 + (0.06 if psum else 0.0)
    if eng == "act":
        return (220 + n) / 1400.0
    if eng == "pool":
        return (120 + n) / 900.0
    return 0.5


class H:
    def __init__(self, P):
        self.P = P

    def dma(self, eng, out, in_, R, W, buf, slow=False):
        nbytes = _fsz(out) * 128 * 4
        est = 2.0 + nbytes / 150000.0
        if slow:
            self.P.op(eng, lambda e: e.dma_start(out=out, in_=in_, allow_slow_non_contiguous=True), reads=R, writes=W, dma=buf, est=est)
        else:
            self.P.op(eng, lambda e: e.dma_start(out=out, in_=in_), reads=R, writes=W, dma=buf, est=est)

    def tt(self, eng, out, in0, in1, op, R, W):
        self.P.op(eng, lambda e: e.tensor_tensor(out=out, in0=in0, in1=in1, op=op), reads=R, writes=W, est=_est(eng, out))

    def ts(self, eng, out, in0, s1, s2, op0, op1, R, W):
        if s2 is None:
            self.P.op(eng, lambda e: e.tensor_scalar(out=out, in0=in0, scalar1=s1, scalar2=None, op0=op0), reads=R, writes=W, est=_est(eng, out))
        else:
            self.P.op(eng, lambda e: e.tensor_scalar(out=out, in0=in0, scalar1=s1, scalar2=s2, op0=op0, op1=op1), reads=R, writes=W, est=_est(eng, out))

    def stt(self, eng, out, in0, sc, in1, op0, op1, R, W):
        eng = "dve"
        self.P.op(eng, lambda e: e.scalar_tensor_tensor(out=out, in0=in0, scalar=sc, in1=in1, op0=op0, op1=op1), reads=R, writes=W, est=_est(eng, out))

    def act(self, out, in_, func, R, W, bias=None, scale=None, accum=None):
        kw = {}
        if bias is not None:
            kw["bias"] = bias
        if scale is not None:
            kw["scale"] = scale
        if accum is not None:
            kw["accum_out"] = accum
        self.P.op("act", lambda e: e.activation(out=out, in_=in_, func=func, **kw), reads=R, writes=W, est=_est("act", out))

    def cp(self, eng, out, in_, R, W):
        if eng == "act":
            self.P.op("act", lambda e: e.copy(out=out, in_=in_), reads=R, writes=W, est=_est("act", out))
        else:
            self.P.op(eng, lambda e: e.tensor_copy(out=out, in_=in_), reads=R, writes=W, est=_est(eng, out))

    def memset(self, eng, ap, val, W):
        self.P.op(eng, lambda e: e.memset(ap, val), writes=W, est=_est(eng, ap))

    def recip(self, out, in_, R, W):
        self.P.op("dve", lambda e: e.reciprocal(out=out, in_=in_), reads=R, writes=W, est=_est("dve", out))

    def reduce(self, out, in_, op, R, W):
        self.P.op("dve", lambda e: e.tensor_reduce(out=out, in_=in_, axis=AX.X, op=op), reads=R, writes=W, est=_est("dve", in_))

    def mm(self, items, R, W):
        est = 0.0
        for (o, l, rh, st, sp) in items:
            est += (max(64, _fsz(rh)) * (4 if l.dtype == F32 else 1)) / 2400.0 + 0.01
        est += 0.06

        def f(e):
            r = None
            for (o, l, rh, st, sp) in items:
                r = e.matmul(o, lhsT=l, rhs=rh, start=st, stop=sp)
            return r
        self.P.op("pe", f, reads=R, writes=W, est=est)

    def tr(self, items, ident, R, W):
        est = 0.06
        for (o, i) in items:
            est += (128 * (4 if i.dtype == F32 else 1)) / 2400.0 + 0.03

        def f(e):
            r = None
            for (o, i) in items:
                r = e.transpose(out=o, in_=i, identity=ident)
            return r
        self.P.op("pe", f, reads=R, writes=W, est=est)

    def select(self, out, in_, cmp, fill, base, cm, pattern, R, W):
        self.P.op("pool", lambda e: e.affine_select(out=out, in_=in_, pattern=pattern, compare_op=cmp, fill=fill,
                                                    base=base, channel_multiplier=cm), reads=R, writes=W, est=_est("pool", out))


class Rot:
    def __init__(self, P, es, name, shape, dtype, n=2):
        self.bufs = [P.sb(es, f"{name}r{i}", shape, dtype) for i in range(n)]

    def at(self, i):
        return self.bufs[i % len(self.bufs)]


def b3(ap, n, m):
    return ap.unsqueeze(1).to_broadcast([128, n, m])


def s3(ap, n, m):
    return ap.unsqueeze(2).to_broadcast([128, n, m])


def s4(ap):
    return ap.rearrange("p (b h) -> p b h", b=2).unsqueeze(3).to_broadcast([128, 2, 3, 128])


def v4(ap):
    return ap.rearrange("p (b h) l -> p b h l", b=2)


def w4(ps):
    return ps[:, :, 0:384].rearrange("p b (h l) -> p b h l", h=3)


def softplus12(h, P, es, tagp):
    xa = P.sb(es, tagp + "xa", [128, 12], F32)
    ax = P.sb(es, tagp + "ax", [128, 12], F32)
    ex = P.sb(es, tagp + "ex", [128, 12], F32)
    ln = P.sb(es, tagp + "ln", [128, 12], F32)
    one = P.sb(es, tagp + "one", [128, 1], F32)
    h.memset("pool", one[:], 1.0, [one])

    def f(xin, xin_buf, bias, out):
        h.tt("dve", xa[:], xin, bias[:], ALU.add, [xin_buf, bias], [xa])
        h.act(ax[:], xa[:], AF.Abs, [xa], [ax])
        h.act(ex[:], ax[:], AF.Exp, [ax], [ex], scale=-1.0)
        h.act(ln[:], ex[:], AF.Ln, [ex, one], [ln], bias=one[:, 0:1])
        h.ts("dve", xa[:], xa[:], 0.0, None, ALU.max, None, [xa], [xa])
        h.tt("dve", out[:], xa[:], ln[:], ALU.add, [xa, ln], [out])
    return f


def make_masks(h, P, es):
    m = {}
    ones = P.sb(es, "m_ones", [128, 128], F32)
    h.memset("pool", ones[:], 1.0, [ones])
    m["ones"] = ones
    for name, cmp, cm, st in (("U", ALU.is_ge, -1, 1), ("L", ALU.is_ge, 1, -1), ("Ls", ALU.is_gt, 1, -1)):
        t = P.sb(es, "m_" + name, [128, 128], F32)
        h.select(t[:], ones[:], cmp, 0.0, 0, cm, [[st, 128]], [ones], [t])
        m[name] = t
    sel = P.sb(es, "m_sel", [128, 128], F32)
    zer = P.sb(es, "m_zero", [128, 128], F32)
    h.memset("pool", zer[:], 0.0, [zer])
    h.select(sel[:], zer[:], ALU.not_equal, 1.0, -127, 1, [[0, 128]], [zer], [sel])
    m["sel"] = sel
    bd = P.sb(es, "m_bd", [128, 128], F32)
    h.memset("pool", bd[:], 0.0, [bd])
    h.memset("pool", bd[0:64, 0:64], 1.0, [bd])
    h.memset("pool", bd[64:128, 64:128], 1.0, [bd])
    m["bd"] = bd
    return m


def mixer_layer(k, l, cur, dstt):
    P, T, flags, h = k.P, k.T, k.flags, k.h
    inp = k.inp
    src, rsrc = cur
    dst, rdst = dstt
    NMT = T // 512
    MT = 512
    identf, identb = k.identf, k.identb
    u_d, qn_d, kn_d, v_d, xs_d, B_d, C_d, pt_d, y_d = k.u_d, k.qn_d, k.kn_d, k.v_d, k.xs_d, k.B_d, k.C_d, k.pt_d, k.y_d
    r_pre, r_y = k.r_pre, k.r_y

    with contextlib.ExitStack() as es:
        A, B = k.norm_consts(es, l, inp["norm_mix"].t[l:l + 1, :], 1, 0, "m")
        winf = P.sb(es, "winf", [128, 8, 2304], BF16)
        wint = P.sb(es, "wint", [128, 8, 786], BF16)
        for (c0, c1) in ((0, 1152), (1152, 2304)):
            h.dma("pool", winf[:, :, c0:c1], inp["w_in_f"].t[l, :, c0:c1].rearrange("(k p) n -> p k n", p=128), [], [winf], winf)
        h.dma("pool", wint[:], inp["w_in_t"].t[l].rearrange("(k p) n -> p k n", p=128), [], [wint], wint)
        cwt = P.sb(es, "cwt", [128, 16, 4], F32)
        cbt = P.sb(es, "cbt", [128, 16], F32)
        h.dma("sp", cwt[:], inp["conv_w"].t[l], [], [cwt], cwt)
        h.dma("sp", cbt[:], inp["conv_b"].t[l], [], [cbt], cbt)
        carry = P.sb(es, "carry", [128, 16, 3], F32)
        h.memset("pool", carry[:], 0.0, [carry])
        epsb = P.sb(es, "epsb", [128, 1], F32)
        h.memset("pool", epsb[:], EPS, [epsb])
        mhalf = P.sb(es, "mhalf", [128, MT], F32)
        h.memset("pool", mhalf[:], -0.5, [mhalf])
        bones = P.sb(es, "bones", [128, 128], F32)
        h.memset("pool", bones[:], 0.0, [bones])
        h.memset("pool", bones[0:64, 0:64], 1.0, [bones])
        h.memset("pool", bones[64:128, 64:128], 1.0, [bones])
        xt = [P.sb(es, f"m1x{i}", [128, D], F32) for i in range(2)]
        tmp = P.sb(es, "m1tmp", [128, D], F32)
        hb = P.sb(es, "m1hb", [128, D], BF16)
        hT = P.sb(es, "m1hT", [128, 8, MT], BF16)
        cin = [P.sb(es, f"cin{i}", [128, MT + 3], F32) for i in range(4)]
        acc = [P.sb(es, f"acc{i}", [128, MT], F32) for i in range(4)]
        so = [P.sb(es, f"so{i}", [128, MT], F32) for i in range(4)]
        sob = [P.sb(es, f"sob{i}", [128, MT], BF16) for i in range(4)]
        sq = P.sb(es, "m1sq", [128, MT], F32)
        rinv = P.sb(es, "m1rinv", [128, MT], F32)
        ptst = [P.sb(es, f"ptst{i}", [128, 786], F32) for i in range(2)]
        ssq = P.sb(es, "m1ssq", [128, 1], F32)
        rstd = P.sb(es, "m1rstd", [128, 1], F32)
        ptr = P.ps(es, "m1ptr", [128, 8, 128], BF16)
        pp = [P.ps(es, f"m1pp{i}", [128, MT], F32) for i in range(4)]
        pt = P.ps(es, "m1pt", [128, 2, 512], F32)
        pq = P.ps(es, "m1pq", [128, MT], F32)
        for mt in range(NMT):
            cols = slice(mt * MT, (mt + 1) * MT)
            for ti in range(4):
                t = mt * 4 + ti
                xb = xt[t % 2]
                h.dma("sp", xb[:], src.t[t * 128:(t + 1) * 128, :], [rsrc[t]], [xb], xb)
                k.rms_mod(xb, A, B, hb, ssq, rstd, tmp, tmp)
                h.tr([(ptr[:, kk, :], hb[:, kk * 128:(kk + 1) * 128]) for kk in range(8)], identb[:], [hb, identb], [ptr])
                h.cp("act", hT[:, :, ti * 128:(ti + 1) * 128], ptr[:], [ptr], [hT])
            for c in range(18):
                pb = pp[c % 4]
                h.mm([(pb[:], winf[:, kk, c * 128:(c + 1) * 128], hT[:, kk, :], kk == 0, kk == 7) for kk in range(8)], [winf, hT], [pb])
                if c < 2:
                    sb_ = so[c % 4]
                    h.cp("act", sb_[:], pb[:], [pb], [sb_])
                    h.dma("sp", u_d.t[c * 128:(c + 1) * 128, cols], sb_[:], [sb_], [r_pre[mt]], sb_)
                    continue
                ci = c - 2
                cb_ = cin[ci % 4]
                h.cp("pool", cb_[:, 0:3], carry[:, ci, :], [carry], [cb_])
                h.cp("act", cb_[:, 3:MT + 3], pb[:], [pb], [cb_])
                h.cp("pool", carry[:, ci, :], cb_[:, MT:MT + 3], [cb_], [carry])
                ab = acc[ci % 4]
                h.ts("dve", ab[:], cb_[:, 0:MT], cwt[:, ci, 0:1], None, ALU.mult, None, [cb_, cwt], [ab])
                for j in range(1, 4):
                    h.stt("dve", ab[:], cb_[:, j:j + MT], cwt[:, ci, j:j + 1], ab[:], ALU.mult, ALU.add, [cb_, cwt, ab], [ab])
                sb_ = so[ci % 4]
                h.act(sb_[:], ab[:], AF.Silu, [ab, cbt], [sb_], bias=cbt[:, ci:ci + 1])
                if ci < 6:
                    h.tt("pool", sq[:], sb_[:], sb_[:], ALU.mult, [sb_], [sq])
                    h.mm([(pq[:], bones[:], sq[:], True, True)], [bones, sq], [pq])
                    h.act(rinv[:], pq[:], AF.Ln, [pq, epsb], [rinv], bias=epsb[:, 0:1])
                    h.act(rinv[:], rinv[:], AF.Exp, [rinv], [rinv], scale=-0.5)
                    ob = sob[ci % 4]
                    if ci < 3:
                        h.stt("dve", ob[:], sb_[:], 0.125, rinv[:], ALU.mult, ALU.mult, [sb_, rinv], [ob])
                    else:
                        h.tt("dve", ob[:], sb_[:], rinv[:], ALU.mult, [sb_, rinv], [ob])
                    dd = qn_d if ci < 3 else kn_d
                    j = ci % 3
                    h.dma("sp", dd.t[j * 128:(j + 1) * 128, cols], ob[:], [ob], [r_pre[mt]], ob)
                elif ci < 12:
                    dd = v_d if ci < 9 else xs_d
                    j = (ci - 6) % 3
                    h.dma("sp", dd.t[j * 128:(j + 1) * 128, cols], sb_[:], [sb_], [r_pre[mt]], sb_)
                else:
                    ob = sob[ci % 4]
                    h.cp("pool", ob[:], sb_[:], [sb_], [ob])
                    dd = B_d if ci < 14 else C_d
                    j = (ci - 12) % 2
                    h.dma("sp", dd.t[j * 128:(j + 1) * 128, cols], ob[:], [ob], [r_pre[mt]], ob)
            for ti in range(4):
                t = mt * 4 + ti
                tc = slice(ti * 128, (ti + 1) * 128)
                h.mm([(pt[:, 0, :], hT[:, kk, tc], wint[:, kk, 0:512], kk == 0, kk == 7) for kk in range(8)]
                     + [(pt[:, 1, 0:274], hT[:, kk, tc], wint[:, kk, 512:786], kk == 0, kk == 7) for kk in range(8)],
                     [hT, wint], [pt])
                stg = ptst[t % 2]
                h.cp("act", stg[:, 0:512], pt[:, 0, :], [pt], [stg])
                h.cp("dve", stg[:, 512:786], pt[:, 1, 0:274], [pt], [stg])
                h.dma("sp", pt_d.t[t * 128:(t + 1) * 128, :], stg[:], [stg], [r_pre[mt]], stg)
        P.end_phase()

    if flags.get("s5", True):
        s5_phase(k, l)
    if flags.get("gdn", True):
        gdn_phase(k, l)
    if flags.get("ssd", True):
        ssd_phase(k, l)

    with contextlib.ExitStack() as es:
        wout = P.sb(es, "wout", [128, 8, D], BF16)
        h.dma("pool", wout[:], inp["w_out"].t[l].rearrange("(k p) n -> p k n", p=128), [], [wout], wout)
        G = P.sb(es, "Gm", [128, D], F32)
        k.load_row(G, k.modv.t[l:l + 1, 2 * D:3 * D], [k.r_modv])
        yT = [P.sb(es, f"m5y{i}", [128, 8, MT], BF16) for i in range(2)]
        xt = [P.sb(es, f"m5x{i}", [128, D], F32) for i in range(2)]
        tm = [P.sb(es, f"m5t{i}", [128, D], F32) for i in range(2)]
        po = [P.ps(es, f"m5p{i}", [128, 512], F32) for i in range(4)]
        for mt in range(NMT):
            cols = slice(mt * MT, (mt + 1) * MT)
            yb = yT[mt % 2]
            h.dma("sp", yb[:], y_d.t[:, cols].rearrange("(k p) t -> p k t", p=128), [r_y[0][mt], r_y[1][mt], r_y[2][mt]], [yb], yb)
            if not flags.get("s5", True):
                h.memset("pool", yb[:, 0:2, :], 0.0, [yb])
            if not flags.get("gdn", True):
                h.memset("pool", yb[:, 2:5, :], 0.0, [yb])
            if not flags.get("ssd", True):
                h.memset("pool", yb[:, 5:8, :], 0.0, [yb])
            for ti in range(4):
                t = mt * 4 + ti
                tc = slice(ti * 128, (ti + 1) * 128)
                xb = xt[t % 2]
                tb = tm[t % 2]
                h.dma("sp", xb[:], src.t[t * 128:(t + 1) * 128, :], [rsrc[t]], [xb], xb)
                for n2 in range(2):
                    pb = po[(t % 2) * 2 + n2]
                    nc_ = slice(n2 * 512, (n2 + 1) * 512)
                    h.mm([(pb[:], yb[:, kk, tc], wout[:, kk, nc_], kk == 0, kk == 7) for kk in range(8)], [yb, wout], [pb])
                    h.tt("dve", tb[:, nc_], pb[:], G[:, nc_], ALU.mult, [pb, G], [tb])
                h.tt("pool", tb[:], tb[:], xb[:], ALU.add, [tb, xb], [tb])
                h.dma("sp", dst.t[t * 128:(t + 1) * 128, :], tb[:], [tb], [rdst[t]], tb)
        P.end_phase()


def gate_consts(k, es, l, tag):
    P, h, inp = k.P, k.h, k.inp
    b12 = P.sb(es, tag + "b12", [128, 12], F32)
    na12 = P.sb(es, tag + "na12", [128, 12], F32)
    k.load_row(b12, inp["bias12"].t[l:l + 1, :])
    k.load_row(na12, inp["alog12"].t[l:l + 1, :])
    h.act(na12[:], na12[:], AF.Exp, [na12], [na12])
    h.ts("dve", na12[:], na12[:], -1.0, None, ALU.mult, None, [na12], [na12])
    return b12, na12


def gate_tile(k, sp_fn, pj_ap, pj_buf, b12, na12, masks, sp12, gda, cs12, cl12, pA):
    h = k.h
    sp_fn(pj_ap, pj_buf, b12, sp12)
    h.tt("dve", gda[:], sp12[:], na12[:], ALU.mult, [sp12, na12], [gda])
    h.mm([(pA[:, 0:12], masks["U"][:], gda[:], True, True)], [masks["U"], gda], [pA])
    h.cp("dve", cs12[:], pA[:, 0:12], [pA], [cs12])
    h.mm([(pA[:, 16:28], masks["sel"][:], cs12[:], True, True)], [masks["sel"], cs12], [pA])
    h.cp("dve", cl12[:], pA[:, 16:28], [pA], [cl12])


def ssd_phase(k, l):
    P, T, h, inp = k.P, k.T, k.h, k.inp
    NMT = T // 512
    MT = 512
    identf, identb = k.identf, k.identb
    with contextlib.ExitStack() as es:
        masks = make_masks(h, P, es)
        b12, na12 = gate_consts(k, es, l, "sd")
        sp_fns = [softplus12(h, P, es, f"sd{i}") for i in range(3)]
        epsb = P.sb(es, "sd_eps", [128, 1], F32)
        h.memset("pool", epsb[:], EPS, [epsb])
        dsk = P.sb(es, "sd_dsk", [128, 6], F32)
        k.load_row(dsk, inp["ssd_d"].t[l:l + 1, :])
        nws = P.sb(es, "sd_nws", [128, 384], F32)
        k.load_row(nws, inp["ssd_norm"].t[l:l + 1, :])
        stT = P.sb(es, "sd_stT", [128, 384], F32)
        stTb = P.sb(es, "sd_stTb", [128, 384], BF16)
        h.memset("pool", stT[:], 0.0, [stT])
        h.memset("pool", stTb[:], 0.0, [stTb])
        xsT = [P.sb(es, f"sd_xsT{i}", [128, 3, MT], F32) for i in range(2)]
        BTt = [P.sb(es, f"sd_BT{i}", [128, 2, MT], BF16) for i in range(2)]
        CTt = [P.sb(es, f"sd_CT{i}", [128, 2, MT], BF16) for i in range(2)]
        pj = [P.sb(es, f"sd_pj{i}", [128, 4, 786], F32) for i in range(2)]
        yo = [P.sb(es, f"sd_yo{i}", [128, 3, MT], BF16) for i in range(2)]
        R_sp12 = Rot(P, es, "sd_sp12", [128, 12], F32, 3)
        R_gda = Rot(P, es, "sd_gda", [128, 12], F32, 3)
        R_cs12 = Rot(P, es, "sd_cs12", [128, 12], F32, 3)
        R_cl12 = Rot(P, es, "sd_cl12", [128, 12], F32, 3)
        R_t6 = Rot(P, es, "sd_t6", [128, 6], F32, 3)
        R_din = Rot(P, es, "sd_din", [128, 6], F32, 3)
        R_eacs = Rot(P, es, "sd_eacs", [128, 6], F32, 3)
        R_cd = Rot(P, es, "sd_cd", [128, 6], F32, 3)
        R_dg = Rot(P, es, "sd_dg", [128, 6, 128], F32, 3)
        R_arg = Rot(P, es, "sd_arg", [128, 6, 128], F32, 3)
        R_seg = Rot(P, es, "sd_seg", [128, 6, 128], F32, 3)
        R_WTb = Rot(P, es, "sd_WTb", [128, 6, 128], BF16, 3)
        R_xs_tm = Rot(P, es, "sd_xstm", [128, 384], F32, 3)
        R_xdtf = Rot(P, es, "sd_xdtf", [128, 384], F32, 3)
        R_xdtb = Rot(P, es, "sd_xdtb", [128, 384], BF16, 3)
        R_xddb = Rot(P, es, "sd_xddb", [128, 384], BF16, 3)
        R_Btm = Rot(P, es, "sd_Btm", [128, 256], BF16, 3)
        R_t1 = Rot(P, es, "sd_t1", [128, 384], F32, 3)
        R_t2 = Rot(P, es, "sd_t2", [128, 384], F32, 3)
        R_y = Rot(P, es, "sd_y", [128, 384], F32, 3)
        R_zs = Rot(P, es, "sd_zs", [128, 384], F32, 3)
        R_junk = Rot(P, es, "sd_junk", [128, 192], F32, 3)
        R_yb = Rot(P, es, "sd_yb", [128, 384], BF16, 3)
        R_ss2 = Rot(P, es, "sd_ss2", [128, 2], F32, 3)
        R_rs2 = Rot(P, es, "sd_rs2", [128, 2], F32, 3)
        W0 = P.ps(es, "sd_W0", [128, 2, 512], F32)
        S0 = P.ps(es, "sd_S0", [128, 512], F32)
        S1 = P.ps(es, "sd_S1", [128, 512], F32)
        S2 = P.ps(es, "sd_S2", [128, 512], F32)
        PB = P.ps(es, "sd_PB", [128, 512], BF16)
        pA = P.ps(es, "sd_pA", [128, 32], F32)
        for mt in range(NMT):
            cols = slice(mt * MT, (mt + 1) * MT)
            i2 = mt % 2
            rp = [k.r_pre[mt]]
            h.dma("sp", xsT[i2][:], k.xs_d.t[:, cols].rearrange("(j p) t -> p j t", p=128), rp, [xsT[i2]], xsT[i2])
            h.dma("sp", BTt[i2][:], k.B_d.t[:, cols].rearrange("(j p) t -> p j t", p=128), rp, [BTt[i2]], BTt[i2])
            h.dma("sp", CTt[i2][:], k.C_d.t[:, cols].rearrange("(j p) t -> p j t", p=128), rp, [CTt[i2]], CTt[i2])
            h.dma("sp", pj[i2][:], k.pt_d.t[mt * MT:(mt + 1) * MT, :].rearrange("(a p) n -> p a n", p=128), rp, [pj[i2]], pj[i2])
            xs_, B_, C_, pj_, yo_ = xsT[i2], BTt[i2], CTt[i2], pj[i2], yo[i2]
            for ti in range(4):
                tc = slice(ti * 128, (ti + 1) * 128)
                tix = mt * 4 + ti
                sp12 = R_sp12.at(tix); gda = R_gda.at(tix); cs12 = R_cs12.at(tix); cl12 = R_cl12.at(tix); t6 = R_t6.at(tix); din = R_din.at(tix); eacs = R_eacs.at(tix); cd = R_cd.at(tix); dg = R_dg.at(tix); arg = R_arg.at(tix); seg = R_seg.at(tix); WTb = R_WTb.at(tix); xs_tm = R_xs_tm.at(tix); xdtf = R_xdtf.at(tix); xdtb = R_xdtb.at(tix); xddb = R_xddb.at(tix); Btm = R_Btm.at(tix); t1 = R_t1.at(tix); t2 = R_t2.at(tix); y = R_y.at(tix); zs = R_zs.at(tix); junk = R_junk.at(tix); yb = R_yb.at(tix); ss2 = R_ss2.at(tix); rs2 = R_rs2.at(tix)
                sp_fn = sp_fns[tix % 3]
                gate_tile(k, sp_fn, pj_[:, ti, 768:780], pj_, b12, na12, masks, sp12, gda, cs12, cl12, pA)
                acs = cs12[:, 6:12]
                h.tt("pool", dg[:], b3(identf[:], 6, 128), s3(acs, 6, 128), ALU.mult, [identf, cs12], [dg])
                h.mm([(W0[:, 0, 0:384], masks["ones"][:], dg[:, 0:3, :].rearrange("p a l -> p (a l)"), True, True),
                      (W0[:, 1, 0:384], masks["ones"][:], dg[:, 3:6, :].rearrange("p a l -> p (a l)"), True, True)],
                     [masks["ones"], dg], [W0])
                h.tt("dve", v4(arg[:]), w4(W0), s4(acs), ALU.subtract, [W0, cs12], [arg])
                h.ts("pool", arg[:], arg[:], 0.0, None, ALU.min, None, [arg], [arg])
                h.act(seg[:], arg[:], AF.Exp, [arg], [seg])
                h.tt("pool", seg[:], seg[:], b3(masks["U"][:], 6, 128), ALU.mult, [seg, masks["U"]], [seg])
                h.mm([(S0[:, g * 128:(g + 1) * 128], B_[:, g, tc], C_[:, g, tc], True, True) for g in range(2)], [B_, C_], [S0])
                h.tt("dve", v4(WTb[:]), v4(seg[:]),
                     S0[:, 0:256].rearrange("p (g l) -> p g l", g=2).unsqueeze(2).to_broadcast([128, 2, 3, 128]),
                     ALU.mult, [seg, S0], [WTb])
                h.tr([(S1[:, j * 128:(j + 1) * 128], xs_[:, j, tc]) for j in range(3)], identf[:], [xs_, identf], [S1])
                h.cp("act", xs_tm[:], S1[:, 0:384], [S1], [xs_tm])
                h.tr([(PB[:, g * 128:(g + 1) * 128], B_[:, g, tc]) for g in range(2)], identb[:], [B_, identb], [PB])
                h.cp("act", Btm[:], PB[:, 0:256], [PB], [Btm])
                x3 = lambda ap: ap.rearrange("p (h d) -> p h d", h=6)
                h.tt("dve", x3(xdtf[:]), x3(xs_tm[:]), s3(sp12[:, 6:12], 6, 64), ALU.mult, [xs_tm, sp12], [xdtf])
                h.cp("pool", xdtb[:], xdtf[:], [xdtf], [xdtb])
                h.tt("dve", t6[:], cl12[:, 6:12], acs, ALU.subtract, [cl12, cs12], [t6])
                h.act(din[:], t6[:], AF.Exp, [t6], [din])
                h.tt("pool", x3(xddb[:]), x3(xdtf[:]), s3(din[:], 6, 64), ALU.mult, [xdtf, din], [xddb])
                h.mm([(S2[:, hd * 64:(hd + 1) * 64], WTb[:, hd, :], xdtb[:, hd * 64:(hd + 1) * 64], True, True) for hd in range(6)],
                     [WTb, xdtb], [S2])
                h.mm([(S0[:, g * 192:(g + 1) * 192], C_[:, g, tc], stTb[:, g * 192:(g + 1) * 192], True, True) for g in range(2)],
                     [C_, stTb], [S0])
                h.act(eacs[:], acs, AF.Exp, [cs12], [eacs])
                h.tt("dve", x3(t2[:]), x3(S0[:, 0:384]), s3(eacs[:], 6, 64), ALU.mult, [S0, eacs], [t2])
                h.tt("pool", x3(t1[:]), x3(xs_tm[:]), s3(dsk[:], 6, 64), ALU.mult, [xs_tm, dsk], [t1])
                h.tt("pool", t2[:], t2[:], t1[:], ALU.add, [t2, t1], [t2])
                h.tt("dve", y[:], S2[:, 0:384], t2[:], ALU.add, [S2, t2], [y])
                h.act(zs[:], pj_[:, ti, 384:768], AF.Silu, [pj_], [zs])
                h.tt("pool", y[:], y[:], zs[:], ALU.mult, [y, zs], [y])
                for g in range(2):
                    h.act(junk[:], y[:, g * 192:(g + 1) * 192], AF.Square, [y], [junk, ss2], accum=ss2[:, g:g + 1])
                h.act(rs2[:], ss2[:], AF.Ln, [ss2, epsb], [rs2], bias=epsb[:, 0:1], scale=1.0 / 192)
                h.act(rs2[:], rs2[:], AF.Exp, [rs2], [rs2], scale=-0.5)
                y3 = lambda ap: ap.rearrange("p (g c) -> p g c", g=2)
                h.tt("dve", y3(y[:]), y3(y[:]), s3(rs2[:], 2, 192), ALU.mult, [y, rs2], [y])
                h.tt("pool", yb[:], y[:], nws[:], ALU.mult, [y, nws], [yb])
                h.tr([(PB[:, j * 128:(j + 1) * 128], yb[:, j * 128:(j + 1) * 128]) for j in range(3)], identb[:], [yb, identb], [PB])
                h.cp("act", yo_[:, :, tc], PB[:, 0:384].rearrange("p (j t) -> p j t", j=3), [PB], [yo_])
                h.mm([(S1[:, g * 192:(g + 1) * 192], Btm[:, g * 128:(g + 1) * 128], xddb[:, g * 192:(g + 1) * 192], True, True) for g in range(2)],
                     [Btm, xddb], [S1])
                h.act(cd[:], cl12[:, 6:12], AF.Exp, [cl12], [cd])
                h.tt("pool", x3(stT[:]), x3(stT[:]), s3(cd[:], 6, 64), ALU.mult, [stT, cd], [stT])
                h.tt("dve", stT[:], stT[:], S1[:, 0:384], ALU.add, [stT, S1], [stT])
                h.cp("act", stTb[:], stT[:], [stT], [stTb])
            h.dma("sp", k.y_d.t[640:1024, cols].rearrange("(j p) t -> p j t", p=128), yo_[:], [yo_], [k.r_y[2][mt]], yo_)
        P.end_phase()


C1_2PI = 6.28125
C2_2PI = 2.0 * math.pi - 6.28125


def sincos(h, eng, x, out, b, ki, c, negpi, R, W, bufs, is_cos):
    bb, kb, cb_ = bufs
    off = 16.5 + (0.25 if is_cos else 0.0)
    add = 33.0 * math.pi + (0.5 * math.pi if is_cos else 0.0)
    h.ts(eng, b, x, 1.0 / (2.0 * math.pi), off, ALU.mult, ALU.add, R, [bb])
    h.cp(eng, ki, b, [bb], [kb])
    h.cp(eng, c, ki, [kb], [cb_])
    h.stt(eng, b, c, -C1_2PI, x, ALU.mult, ALU.add, R + [cb_], [bb])
    h.stt(eng, b, c, -C2_2PI, b, ALU.mult, ALU.add, [cb_, bb], [bb])
    h.ts(eng, b, b, add, None, ALU.add, None, [bb], [bb])
    h.ts(eng, c, b, 2.0 * math.pi, -2.0 * math.pi, ALU.is_gt, ALU.mult, [bb], [cb_])
    h.tt(eng, b, b, c, ALU.add, [bb, cb_], [bb])
    h.ts(eng, c, b, 0.0, 2.0 * math.pi, ALU.is_lt, ALU.mult, [bb], [cb_])
    h.tt(eng, b, b, c, ALU.add, [bb, cb_], [bb])
    h.act(out, b, AF.Sin, [bb, negpi], W, bias=negpi[:, 0:1])


def s5_phase(k, l):
    P, T, h, inp = k.P, k.T, k.h, k.inp
    NMT = T // 512
    SEG = 512
    with contextlib.ExitStack() as es:
        I32 = mybir.dt.int32
        are = P.sb(es, "s5are", [128, 8], F32)
        aim = P.sb(es, "s5aim", [128, 8], F32)
        stp = P.sb(es, "s5stp", [128, 8], F32)
        h.dma("sp", are[:], inp["s5_are"].t[l], [], [are], are)
        h.dma("sp", aim[:], inp["s5_aim"].t[l], [], [aim], aim)
        h.dma("sp", stp[:], inp["s5_ldt"].t[l], [], [stp], stp)
        dsk = P.sb(es, "s5dsk", [128, 2], F32)
        nw5 = P.sb(es, "s5nw", [128, 2], F32)
        h.dma("sp", dsk[:], inp["s5_dcol"].t[l], [], [dsk], dsk)
        h.dma("sp", nw5[:], inp["s5_ncol"].t[l], [], [nw5], nw5)
        bTre = P.sb(es, "s5bTre", [128, 8, 128], BF16)
        bTim = P.sb(es, "s5bTim", [128, 8, 128], BF16)
        cTre = P.sb(es, "s5cTre", [128, 8, 128], BF16)
        cTim = P.sb(es, "s5cTim", [128, 8, 128], BF16)
        for dstb, nm in ((bTre, "s5_bT_re"), (bTim, "s5_bT_im"), (cTre, "s5_cT_re"), (cTim, "s5_cT_im")):
            h.dma("pool", dstb[:], inp[nm].t[l].rearrange("s r m -> r s m"), [], [dstb], dstb)
        wglu = P.sb(es, "s5wglu", [128, 2, 256], BF16)
        h.dma("pool", wglu[:], inp["s5_w_glu"].t[l].rearrange("(k p) n -> p k n", p=128), [], [wglu], wglu)
        negpi = P.sb(es, "s5negpi", [128, 1], F32)
        h.memset("pool", negpi[:], -math.pi, [negpi])
        epsb = P.sb(es, "s5eps", [128, 1], F32)
        h.memset("pool", epsb[:], EPS, [epsb])
        onesf = P.sb(es, "s5ones", [128, 128], F32)
        h.memset("pool", onesf[:], 1.0, [onesf])
        jrow = P.sb(es, "s5jrow", [128, SEG], F32)
        P.op("pool", lambda e: e.iota(jrow[:], pattern=[[1, SEG]], base=0, channel_multiplier=0,
                                      allow_small_or_imprecise_dtypes=True), writes=[jrow])
        th = P.sb(es, "s5th", [128, 8], F32)
        rr = P.sb(es, "s5r", [128, 8], F32)
        sth = P.sb(es, "s5sth", [128, 8], F32)
        cth = P.sb(es, "s5cth", [128, 8], F32)
        thS = P.sb(es, "s5thS", [128, 8], F32)
        sS = P.sb(es, "s5sS", [128, 8], F32)
        cS = P.sb(es, "s5cS", [128, 8], F32)
        nsS = P.sb(es, "s5nsS", [128, 8], F32)
        cr = P.sb(es, "s5cr", [128, 8], F32)
        ci = P.sb(es, "s5ci", [128, 8], F32)
        ncr = P.sb(es, "s5ncr", [128, 8], F32)
        q1 = P.sb(es, "s5q1", [128, 8], F32)
        q2 = P.sb(es, "s5q2", [128, 8], F32)
        q3 = P.sb(es, "s5q3", [128, 8], F32)
        sb8 = P.sb(es, "s5sb8", [128, 8], F32)
        si8 = P.sb(es, "s5si8", [128, 8], I32)
        sc8 = P.sb(es, "s5sc8", [128, 8], F32)
        h.act(stp[:], stp[:], AF.Exp, [stp], [stp])
        h.tt("dve", th[:], aim[:], stp[:], ALU.mult, [aim, stp], [th])
        h.tt("dve", rr[:], are[:], stp[:], ALU.mult, [are, stp], [rr])
        h.act(rr[:], rr[:], AF.Exp, [rr], [rr])
        sm = (sb8, si8, sc8)
        sincos(h, "dve", th[:], sth[:], sb8[:], si8[:], sc8[:], negpi, [th], [sth], sm, False)
        sincos(h, "dve", th[:], cth[:], sb8[:], si8[:], sc8[:], negpi, [th], [cth], sm, True)
        h.ts("dve", thS[:], th[:], float(SEG), None, ALU.mult, None, [th], [thS])
        sincos(h, "dve", thS[:], sS[:], sb8[:], si8[:], sc8[:], negpi, [thS], [sS], sm, False)
        sincos(h, "dve", thS[:], cS[:], sb8[:], si8[:], sc8[:], negpi, [thS], [cS], sm, True)
        h.ts("dve", nsS[:], sS[:], -1.0, None, ALU.mult, None, [sS], [nsS])
        h.tt("dve", q1[:], rr[:], cth[:], ALU.mult, [rr, cth], [q1])
        h.ts("dve", q1[:], q1[:], -1.0, None, ALU.add, None, [q1], [q1])
        h.tt("dve", q2[:], rr[:], sth[:], ALU.mult, [rr, sth], [q2])
        h.tt("dve", q3[:], are[:], are[:], ALU.mult, [are], [q3])
        h.tt("dve", sc8[:], aim[:], aim[:], ALU.mult, [aim], [sc8])
        h.tt("dve", q3[:], q3[:], sc8[:], ALU.add, [q3, sc8], [q3])
        h.recip(q3[:], q3[:], [q3], [q3])
        h.tt("dve", cr[:], q1[:], are[:], ALU.mult, [q1, are], [cr])
        h.tt("dve", sc8[:], q2[:], aim[:], ALU.mult, [q2, aim], [sc8])
        h.tt("dve", cr[:], cr[:], sc8[:], ALU.add, [cr, sc8], [cr])
        h.tt("dve", cr[:], cr[:], q3[:], ALU.mult, [cr, q3], [cr])
        h.tt("dve", ci[:], q2[:], are[:], ALU.mult, [q2, are], [ci])
        h.tt("dve", sc8[:], q1[:], aim[:], ALU.mult, [q1, aim], [sc8])
        h.tt("dve", ci[:], ci[:], sc8[:], ALU.subtract, [ci, sc8], [ci])
        h.tt("dve", ci[:], ci[:], q3[:], ALU.mult, [ci, q3], [ci])
        h.ts("dve", ncr[:], cr[:], -1.0, None, ALU.mult, None, [cr], [ncr])
        cosT = P.sb(es, "s5cosT", [128, 8, SEG], F32)
        sinT = P.sb(es, "s5sinT", [128, 8, SEG], F32)
        tabr = P.sb(es, "s5tabr", [128, 8, SEG], F32)
        tabi = P.sb(es, "s5tabi", [128, 8, SEG], F32)
        ang = [P.sb(es, f"s5ang{i}", [128, SEG], F32) for i in range(2)]
        tb = [P.sb(es, f"s5tb{i}", [128, SEG], F32) for i in range(2)]
        tki = [P.sb(es, f"s5tki{i}", [128, SEG], I32) for i in range(2)]
        tcc = [P.sb(es, f"s5tc{i}", [128, SEG], F32) for i in range(2)]
        for sc in range(8):
            i = sc % 2
            eng = "dve" if i == 0 else "pool"
            h.ts(eng, ang[i][:], jrow[:], th[:, sc:sc + 1], None, ALU.mult, None, [jrow, th], [ang[i]])
            bufs = (tb[i], tki[i], tcc[i])
            sincos(h, eng, ang[i][:], sinT[:, sc, :], tb[i][:], tki[i][:], tcc[i][:], negpi, [ang[i]], [sinT], bufs, False)
            sincos(h, eng, ang[i][:], cosT[:, sc, :], tb[i][:], tki[i][:], tcc[i][:], negpi, [ang[i]], [cosT], bufs, True)
            h.ts(eng, tabr[:, sc, :], cosT[:, sc, :], cr[:, sc:sc + 1], None, ALU.mult, None, [cosT, cr], [tabr])
            h.stt(eng, tabr[:, sc, :], sinT[:, sc, :], ci[:, sc:sc + 1], tabr[:, sc, :], ALU.mult, ALU.add, [sinT, ci, tabr], [tabr])
            h.ts(eng, tabi[:, sc, :], cosT[:, sc, :], ci[:, sc:sc + 1], None, ALU.mult, None, [cosT, ci], [tabi])
            h.stt(eng, tabi[:, sc, :], sinT[:, sc, :], ncr[:, sc:sc + 1], tabi[:, sc, :], ALU.mult, ALU.add, [sinT, ncr, tabi], [tabi])
        ire = P.sb(es, "s5ire", [128, 8], F32)
        iim = P.sb(es, "s5iim", [128, 8], F32)
        gre_e = P.sb(es, "s5gree", [128, 8], F32)
        gim_e = P.sb(es, "s5gime", [128, 8], F32)
        h.memset("pool", ire[:], 0.0, [ire])
        h.memset("pool", iim[:], 0.0, [iim])
        uTf = [P.sb(es, f"s5uTf{i}", [128, 2, SEG], F32) for i in range(2)]
        uTb = [P.sb(es, f"s5uTb{i}", [128, 2, SEG], BF16) for i in range(2)]
        m1 = [P.sb(es, f"s5m1{i}", [128, SEG], F32) for i in range(3)]
        m2 = [P.sb(es, f"s5m2{i}", [128, SEG], F32) for i in range(3)]
        m3 = [P.sb(es, f"s5m3{i}", [128, SEG], F32) for i in range(3)]
        m4 = [P.sb(es, f"s5m4{i}", [128, SEG], F32) for i in range(3)]
        p1 = [P.sb(es, f"s5p1{i}", [128, SEG], BF16) for i in range(3)]
        p2 = [P.sb(es, f"s5p2{i}", [128, SEG], BF16) for i in range(3)]
        p3 = [P.sb(es, f"s5p3{i}", [128, SEG], BF16) for i in range(3)]
        p4 = [P.sb(es, f"s5p4{i}", [128, SEG], BF16) for i in range(3)]
        ncTre = P.sb(es, "s5ncTre", [128, 8, 128], BF16)
        ncTim = P.sb(es, "s5ncTim", [128, 8, 128], BF16)
        h.ts("pool", ncTre[:], cTre[:], -1.0, None, ALU.mult, None, [cTre], [ncTre])
        h.ts("pool", ncTim[:], cTim[:], -1.0, None, ALU.mult, None, [cTim], [ncTim])
        dre = [P.sb(es, f"s5dre{i}", [128, SEG], F32) for i in range(3)]
        dim = [P.sb(es, f"s5dim{i}", [128, SEG], F32) for i in range(3)]
        gre = [P.sb(es, f"s5gre{i}", [128, SEG], F32) for i in range(3)]
        gim = [P.sb(es, f"s5gim{i}", [128, SEG], F32) for i in range(3)]
        hre = [P.sb(es, f"s5hre{i}", [128, SEG], BF16) for i in range(2)]
        him = [P.sb(es, f"s5him{i}", [128, SEG], BF16) for i in range(2)]
        y1 = P.sb(es, "s5y1", [128, 2, SEG], F32)
        yt = P.sb(es, "s5yt", [128, 2, SEG], F32)
        yg = P.sb(es, "s5yg", [128, 2, SEG], F32)
        ygb = P.sb(es, "s5ygb", [128, 2, SEG], BF16)
        sg = P.sb(es, "s5sg", [128, 2, SEG], F32)
        y2 = P.sb(es, "s5y2", [128, 2, SEG], F32)
        rstd = P.sb(es, "s5rstd", [128, SEG], F32)
        yo = [P.sb(es, f"s5yo{i}", [128, 2, SEG], BF16) for i in range(2)]
        Pre = [P.ps(es, f"s5Pre{i}", [128, SEG], F32) for i in range(2)]
        Pim = [P.ps(es, f"s5Pim{i}", [128, SEG], F32) for i in range(2)]
        Y = [P.ps(es, f"s5Y{i}", [128, SEG], F32) for i in range(2)]
        Pg = P.ps(es, "s5Pg", [128, SEG], F32)
        Pt = P.ps(es, "s5Pt", [128, SEG], F32)
        GK = 2.0 * math.sqrt(2.0 / math.pi)
        for mt in range(NMT):
            cols = slice(mt * SEG, (mt + 1) * SEG)
            i2 = mt % 2
            uf, ub, yo_ = uTf[i2], uTb[i2], yo[i2]
            h.dma("sp", uf[:], k.u_d.t[:, cols].rearrange("(j p) t -> p j t", p=128), [k.r_pre[mt]], [uf], uf)
            h.cp("pool", ub[:], uf[:], [uf], [ub])
            for sc in range(8):
                i = sc % 2
                i3 = sc % 3
                cc = sc // 4
                h.mm([(Pre[i][:], bTre[:, sc, :], ub[:, cc, :], True, True)], [bTre, ub], [Pre[i]])
                h.mm([(Pim[i][:], bTim[:, sc, :], ub[:, cc, :], True, True)], [bTim, ub], [Pim[i]])
                h.tt("dve", m1[i3][:], Pre[i][:], tabr[:, sc, :], ALU.mult, [Pre[i], tabr], [m1[i3]])
                h.tt("dve", m2[i3][:], Pim[i][:], tabi[:, sc, :], ALU.mult, [Pim[i], tabi], [m2[i3]])
                h.tt("pool", dre[i3][:], m1[i3][:], m2[i3][:], ALU.subtract, [m1[i3], m2[i3]], [dre[i3]])
                h.tt("dve", m3[i3][:], Pre[i][:], tabi[:, sc, :], ALU.mult, [Pre[i], tabi], [m3[i3]])
                h.tt("dve", m4[i3][:], Pim[i][:], tabr[:, sc, :], ALU.mult, [Pim[i], tabr], [m4[i3]])
                h.tt("pool", dim[i3][:], m3[i3][:], m4[i3][:], ALU.add, [m3[i3], m4[i3]], [dim[i3]])
                for (go, di, ini) in ((gre[i3], dre[i3], ire), (gim[i3], dim[i3], iim)):
                    P.op("dve", (lambda go, di, ini, sc: (lambda e: e.tensor_tensor_scan(
                        out=go[:], data0=rr[:, sc:sc + 1].to_broadcast([128, SEG]), data1=di[:],
                        initial=ini[:, sc:sc + 1], op0=ALU.mult, op1=ALU.add)))(go, di, ini, sc),
                        reads=[rr, di, ini], writes=[go])
                h.cp("act", gre_e[:, sc:sc + 1], gre[i3][:, SEG - 1:SEG], [gre[i3]], [gre_e])
                h.cp("act", gim_e[:, sc:sc + 1], gim[i3][:, SEG - 1:SEG], [gim[i3]], [gim_e])
                h.tt("pool", p1[i3][:], gre[i3][:], cosT[:, sc, :], ALU.mult, [gre[i3], cosT], [p1[i3]])
                h.tt("pool", p2[i3][:], gim[i3][:], sinT[:, sc, :], ALU.mult, [gim[i3], sinT], [p2[i3]])
                h.tt("dve", p3[i3][:], gre[i3][:], sinT[:, sc, :], ALU.mult, [gre[i3], sinT], [p3[i3]])
                h.tt("dve", p4[i3][:], gim[i3][:], cosT[:, sc, :], ALU.mult, [gim[i3], cosT], [p4[i3]])
                h.mm([(Y[cc][:], cTre[:, sc, :], p1[i3][:], sc % 4 == 0, False),
                      (Y[cc][:], ncTre[:, sc, :], p2[i3][:], False, False),
                      (Y[cc][:], ncTim[:, sc, :], p3[i3][:], False, False),
                      (Y[cc][:], ncTim[:, sc, :], p4[i3][:], False, sc % 4 == 3)],
                     [cTre, ncTre, ncTim, p1[i3], p2[i3], p3[i3], p4[i3]], [Y[cc]])
                if sc % 4 == 3:
                    h.stt("dve", y1[:, cc, :], uf[:, cc, :], dsk[:, cc:cc + 1], Y[cc][:], ALU.mult, ALU.add, [uf, dsk, Y[cc]], [y1])
                    h.tt("pool", yt[:, cc, :], y1[:, cc, :], y1[:, cc, :], ALU.mult, [y1], [yt])
                    h.ts("pool", yt[:, cc, :], yt[:, cc, :], 0.044715, 1.0, ALU.mult, ALU.add, [yt], [yt])
                    h.tt("pool", yt[:, cc, :], yt[:, cc, :], y1[:, cc, :], ALU.mult, [yt, y1], [yt])
                    h.act(yt[:, cc, :], yt[:, cc, :], AF.Sigmoid, [yt], [yt], scale=GK)
                    h.tt("pool", yg[:, cc, :], y1[:, cc, :], yt[:, cc, :], ALU.mult, [y1, yt], [yg])
                    h.cp("pool", ygb[:, cc, :], yg[:, cc, :], [yg], [ygb])
            h.tt("dve", q1[:], gre_e[:], cS[:], ALU.mult, [gre_e, cS], [q1])
            h.tt("dve", q2[:], gim_e[:], nsS[:], ALU.mult, [gim_e, nsS], [q2])
            h.tt("dve", ire[:], q1[:], q2[:], ALU.add, [q1, q2], [ire])
            h.tt("dve", q1[:], gre_e[:], sS[:], ALU.mult, [gre_e, sS], [q1])
            h.tt("dve", q2[:], gim_e[:], cS[:], ALU.mult, [gim_e, cS], [q2])
            h.tt("dve", iim[:], q1[:], q2[:], ALU.add, [q1, q2], [iim])
            for oc in range(2):
                h.mm([(Pg[:], wglu[:, kc, oc * 128:(oc + 1) * 128], ygb[:, kc, :], kc == 0, kc == 1) for kc in range(2)], [wglu, ygb], [Pg])
                h.act(sg[:, oc, :], Pg[:], AF.Sigmoid, [Pg], [sg])
                h.tt("pool", y2[:, oc, :], yg[:, oc, :], sg[:, oc, :], ALU.mult, [yg, sg], [y2])
                h.tt("pool", sg[:, oc, :], y2[:, oc, :], y2[:, oc, :], ALU.mult, [y2], [sg])
            h.mm([(Pt[:], onesf[:], sg[:, oc, :], oc == 0, oc == 1) for oc in range(2)], [onesf, sg], [Pt])
            h.act(rstd[:], Pt[:], AF.Sqrt, [Pt, epsb], [rstd], bias=epsb[:, 0:1], scale=1.0 / 256)
            h.recip(rstd[:], rstd[:], [rstd], [rstd])
            for oc in range(2):
                h.stt("dve", yo_[:, oc, :], y2[:, oc, :], nw5[:, oc:oc + 1], rstd[:], ALU.mult, ALU.mult, [y2, nw5, rstd], [yo_])
            h.dma("sp", k.y_d.t[0:256, cols].rearrange("(j p) t -> p j t", p=128), yo_[:], [yo_], [k.r_y[0][mt]], yo_)
        P.end_phase()


def gdn_phase(k, l):
    P, T, h, inp = k.P, k.T, k.h, k.inp
    NMT = T // 512
    MT = 512
    identf, identb = k.identf, k.identb
    with contextlib.ExitStack() as es:
        masks = make_masks(h, P, es)
        b12, na12 = gate_consts(k, es, l, "gd")
        gnw = P.sb(es, "gd_gnw", [128, 64], F32)
        k.load_row(gnw, inp["gdn_norm"].t[l:l + 1, :])
        Sf = P.sb(es, "gd_Sf", [128, 3, 128], F32)
        Sb = P.sb(es, "gd_Sb", [128, 3, 128], BF16)
        h.memset("pool", Sf[:], 0.0, [Sf])
        h.memset("pool", Sb[:], 0.0, [Sb])
        qnT = [P.sb(es, f"gd_qn{i}", [128, 3, MT], BF16) for i in range(2)]
        knT = [P.sb(es, f"gd_kn{i}", [128, 3, MT], BF16) for i in range(2)]
        vT = [P.sb(es, f"gd_vT{i}", [128, 3, MT], F32) for i in range(2)]
        pj = [P.sb(es, f"gd_pj{i}", [128, 4, 786], F32) for i in range(2)]
        yo = [P.sb(es, f"gd_yo{i}", [128, 3, MT], BF16) for i in range(2)]
        R_sp12 = Rot(P, es, "gd_sp12", [128, 12], F32)
        R_gda = Rot(P, es, "gd_gda", [128, 12], F32)
        R_cs12 = Rot(P, es, "gd_cs12", [128, 12], F32)
        R_cl12 = Rot(P, es, "gd_cl12", [128, 12], F32)
        R_beta = Rot(P, es, "gd_beta", [128, 6], F32)
        R_nbeta = Rot(P, es, "gd_nbeta", [128, 6], F32)
        R_egc = Rot(P, es, "gd_egc", [128, 6], F32)
        R_t6 = Rot(P, es, "gd_t6", [128, 6], F32)
        R_dkk = Rot(P, es, "gd_dkk", [128, 6], F32)
        R_gtot = Rot(P, es, "gd_gtot", [128, 6], F32)
        R_gtc = Rot(P, es, "gd_gtc", [128, 3], F32)
        R_dg = Rot(P, es, "gd_dg", [128, 6, 128], F32)
        R_arg = Rot(P, es, "gd_arg", [128, 6, 128], F32)
        R_E = Rot(P, es, "gd_E", [128, 6, 128], F32)
        R_EU = Rot(P, es, "gd_EU", [128, 6, 128], F32)
        R_ELn = Rot(P, es, "gd_ELn", [128, 6, 128], F32)
        R_attnT = Rot(P, es, "gd_attnT", [128, 6, 128], BF16)
        R_Pm = [Rot(P, es, f"gd_Pm{i}", [128, 6, 128], BF16) for i in range(2)]
        R_Qm = [Rot(P, es, f"gd_Qm{i}", [128, 6, 128], BF16) for i in range(2)]
        sp_fns = [softplus12(h, P, es, f"gd{i}") for i in range(2)]
        R_Xb = Rot(P, es, "gd_Xb", [128, 6, 128], BF16)
        R_kdec = Rot(P, es, "gd_kdec", [128, 384], BF16)
        R_v_tm = Rot(P, es, "gd_vtm", [128, 384], F32)
        R_rr_ = Rot(P, es, "gd_rr", [128, 384], F32)
        R_rb = Rot(P, es, "gd_rb", [128, 384], BF16)
        R_vnb = Rot(P, es, "gd_vnb", [128, 384], BF16)
        R_oa = Rot(P, es, "gd_oa", [128, 384], F32)
        R_o = Rot(P, es, "gd_o", [128, 384], F32)
        R_sq = Rot(P, es, "gd_sq", [128, 384], F32)
        R_ss6 = Rot(P, es, "gd_ss6", [128, 6], F32)
        R_rs6 = Rot(P, es, "gd_rs6", [128, 6], F32)
        R_zg = Rot(P, es, "gd_zg", [128, 384], F32)
        R_yb = Rot(P, es, "gd_yb", [128, 384], BF16)
        R_tmpS = Rot(P, es, "gd_tmpS", [128, 3, 128], F32)
        W0 = P.ps(es, "gd_W0", [128, 2, 512], F32)
        W1 = P.ps(es, "gd_W1", [128, 2, 512], F32)
        W2 = P.ps(es, "gd_W2", [128, 2, 512], F32)
        W1a, W1b, W2a, W2b = Buf(None, "W1a"), Buf(None, "W1b"), Buf(None, "W2a"), Buf(None, "W2b")
        W1h, W2h = (W1a, W1b), (W2a, W2b)
        M1bh_s = [(Buf(None, f"m1a{i}"), Buf(None, f"m1b{i}")) for i in range(2)]
        M1pbh_s = [(Buf(None, f"m1pa{i}"), Buf(None, f"m1pb{i}")) for i in range(2)]
        PB = P.ps(es, "gd_PB", [128, 1024], BF16)
        pA = P.ps(es, "gd_pA", [128, 32], F32)
        x3 = lambda ap: ap.rearrange("p (h d) -> p h d", h=6)
        kz = [P.sb(es, f"gd_kz{i}", [128, 3, MT], BF16) for i in range(2)]
        R_Tb = Rot(P, es, "gd_Tb", [128, 6, 128], BF16)
        R_Tb2 = Rot(P, es, "gd_Tb2", [128, 6, 128], BF16)
        R_Xb2 = Rot(P, es, "gd_Xb2", [128, 6, 128], BF16)
        epsb = P.sb(es, "gd_eps", [128, 1], F32)
        h.memset("pool", epsb[:], EPS, [epsb])
        R_ez = Rot(P, es, "gd_ez", [128, 384], F32)
        R_M1b = Rot(P, es, "gd_M1b", [128, 6, 128], BF16)
        R_M1pb = Rot(P, es, "gd_M1pb", [128, 6, 128], BF16)
        cmask = P.sb(es, "gd_cmask", [128, 14, 128], BF16)
        h.dma("pool", cmask[:], inp["gdn_cmask"].t.rearrange("m p j -> p m j"), [], [cmask], cmask)
        cmask6 = P.sb(es, "gd_cmask6", [128, 14, 6, 128], BF16)
        h.cp("pool", cmask6[:], cmask[:].unsqueeze(2).to_broadcast([128, 14, 6, 128]), [cmask], [cmask6])
        rmask = P.sb(es, "gd_rmask", [128, 2], F32)
        h.memset("pool", rmask[:], 0.0, [rmask])
        h.memset("pool", rmask[0:64, 0:1], 1.0, [rmask])
        h.memset("pool", rmask[64:128, 1:2], 1.0, [rmask])

        def headmm(Wps, lh, rh, R, Wh):
            w = w4(Wps)
            h.mm([(w[:, hd // 3, hd % 3, :], lh[:, hd, :], rh[:, hd, :], True, True) for hd in range(6)], R, list(Wh))

        stop = k.flags.get("gdn_stop", 99)
        for mt in range(NMT):
            cols = slice(mt * MT, (mt + 1) * MT)
            i2 = mt % 2
            rp = [k.r_pre[mt]]
            h.dma("sp", qnT[i2][:], k.qn_d.t[:, cols].rearrange("(j p) t -> p j t", p=128), rp, [qnT[i2]], qnT[i2])
            h.dma("sp", knT[i2][:], k.kn_d.t[:, cols].rearrange("(j p) t -> p j t", p=128), rp, [knT[i2]], knT[i2])
            h.dma("sp", vT[i2][:], k.v_d.t[:, cols].rearrange("(j p) t -> p j t", p=128), rp, [vT[i2]], vT[i2])
            h.dma("sp", pj[i2][:], k.pt_d.t[mt * MT:(mt + 1) * MT, :].rearrange("(a p) n -> p a n", p=128), rp, [pj[i2]], pj[i2])
            qn_, kn_, v_, pj_, yo_ = qnT[i2], knT[i2], vT[i2], pj[i2], yo[i2]
            for s_ in range(2):
                h.ts("dve", kz[s_][:], kn_[:], rmask[:, s_:s_ + 1], None, ALU.mult, None, [kn_, rmask], [kz[s_]])
            for ti in range(4):
                tc = slice(ti * 128, (ti + 1) * 128)
                tix = mt * 4 + ti
                sp12 = R_sp12.at(tix); gda = R_gda.at(tix); cs12 = R_cs12.at(tix); cl12 = R_cl12.at(tix); beta = R_beta.at(tix); nbeta = R_nbeta.at(tix); egc = R_egc.at(tix); t6 = R_t6.at(tix); dkk = R_dkk.at(tix); gtot = R_gtot.at(tix); gtc = R_gtc.at(tix); dg = R_dg.at(tix); arg = R_arg.at(tix); E = R_E.at(tix); EU = R_EU.at(tix); ELn = R_ELn.at(tix); attnT = R_attnT.at(tix); Xb = R_Xb.at(tix); kdec = R_kdec.at(tix); v_tm = R_v_tm.at(tix); rr_ = R_rr_.at(tix); rb = R_rb.at(tix); vnb = R_vnb.at(tix); oa = R_oa.at(tix); o = R_o.at(tix); sq = R_sq.at(tix); ss6 = R_ss6.at(tix); rs6 = R_rs6.at(tix); zg = R_zg.at(tix); yb = R_yb.at(tix); tmpS = R_tmpS.at(tix); Tb = R_Tb.at(tix); M1b = R_M1b.at(tix); M1pb = R_M1pb.at(tix)
                Pm = [r.at(tix) for r in R_Pm]; Qm = [r.at(tix) for r in R_Qm]; sp_fn = sp_fns[tix % 2]; M1bh = M1bh_s[tix % 2]; M1pbh = M1pbh_s[tix % 2]; Tb2 = R_Tb2.at(tix); Xb2 = R_Xb2.at(tix); ez = R_ez.at(tix)
                gate_tile(k, sp_fn, pj_[:, ti, 768:780], pj_, b12, na12, masks, sp12, gda, cs12, cl12, pA)
                gc = cs12[:, 0:6]
                h.act(beta[:], pj_[:, ti, 780:786], AF.Exp, [pj_], [beta], scale=-1.0)
                h.ts("dve", beta[:], beta[:], 1.0, None, ALU.add, None, [beta], [beta])
                h.recip(beta[:], beta[:], [beta], [beta])
                h.ts("dve", nbeta[:], beta[:], -1.0, None, ALU.mult, None, [beta], [nbeta])
                h.act(egc[:], gc, AF.Exp, [cs12], [egc])
                h.tt("dve", t6[:], cl12[:, 0:6], gc, ALU.subtract, [cl12, cs12], [t6])
                h.act(dkk[:], t6[:], AF.Exp, [t6], [dkk])
                h.act(gtot[:], cl12[:, 0:6], AF.Exp, [cl12], [gtot])
                g2 = gtot[:].rearrange("p (j s) -> p j s", s=2)
                h.cp("dve", gtc[0:64, :], g2[0:64, :, 0], [gtot], [gtc])
                h.cp("dve", gtc[64:128, :], g2[64:128, :, 1], [gtot], [gtc])
                if stop < 1:
                    h.memset("pool", yo_[:, :, tc], 0.0, [yo_])
                    continue
                h.tt("pool", dg[:], b3(identf[:], 6, 128), s3(gc, 6, 128), ALU.mult, [identf, cs12], [dg])
                h.mm([(W0[:, 0, 0:384], masks["ones"][:], dg[:, 0:3, :].rearrange("p a l -> p (a l)"), True, True),
                      (W0[:, 1, 0:384], masks["ones"][:], dg[:, 3:6, :].rearrange("p a l -> p (a l)"), True, True)],
                     [masks["ones"], dg], [W0])
                if stop < 1.2:
                    h.memset("pool", yo_[:, :, tc], 0.0, [yo_])
                    continue
                h.tt("dve", v4(arg[:]), w4(W0), s4(gc), ALU.subtract, [W0, cs12], [arg])
                if stop < 1.4:
                    h.memset("pool", yo_[:, :, tc], 0.0, [yo_])
                    continue
                h.act(arg[:], arg[:], AF.Abs, [arg], [arg])
                h.act(E[:], arg[:], AF.Exp, [arg], [E], scale=-1.0)
                if stop < 1.6:
                    h.memset("pool", yo_[:, :, tc], 0.0, [yo_])
                    continue
                h.tt("pool", EU[:], E[:], b3(masks["U"][:], 6, 128), ALU.mult, [E, masks["U"]], [EU])
                if stop < 1.8:
                    h.memset("pool", yo_[:, :, tc], 0.0, [yo_])
                    continue
                h.tt("pool", ELn[:], E[:], b3(masks["Ls"][:], 6, 128), ALU.mult, [E, masks["Ls"]], [ELn])
                if stop < 1.9:
                    h.memset("pool", yo_[:, :, tc], 0.0, [yo_])
                    continue
                h.tt("pool", ELn[:], ELn[:], s3(nbeta[:], 6, 128), ALU.mult, [ELn, nbeta], [ELn])
                if stop < 2:
                    h.memset("pool", yo_[:, :, tc], 0.0, [yo_])
                    continue
                w1 = w4(W1)
                w2 = w4(W2)
                h.mm([(w1[:, hd // 3, hd % 3, :], kz[hd % 2][:, hd // 2, tc], kn_[:, hd // 2, tc], True, True) for hd in range(6)],
                     [kz[0], kz[1], kn_], [W1a, W1b])
                h.mm([(w2[:, hd // 3, hd % 3, :], kz[hd % 2][:, hd // 2, tc], qn_[:, hd // 2, tc], True, True) for hd in range(6)],
                     [kz[0], kz[1], qn_], [W2a, W2b])
                h.tt("dve", v4(Pm[0][:]), w1, v4(ELn[:]), ALU.mult, [W1a, W1b, ELn], [Pm[0]])
                h.tt("dve", v4(attnT[:]), w2, v4(EU[:]), ALU.mult, [W2a, W2b, EU], [attnT])
                if stop < 3:
                    h.memset("pool", yo_[:, :, tc], 0.0, [yo_])
                    continue
                h.tr([(PB[:, hd * 128:(hd + 1) * 128], Pm[0][:, hd, :]) for hd in range(6)], identb[:], [Pm[0], identb], [PB])
                h.cp("act", Qm[0][:], PB[:, 0:768].rearrange("p (a l) -> p a l", a=6), [PB], [Qm[0]])
                Nn, NT_ = Pm[0], Qm[0]
                h.tt("pool", Pm[1][:], Nn[:], b3(cmask[:, 0, :], 6, 128), ALU.mult, [Nn, cmask], [Pm[1]])
                h.tt("pool", Qm[1][:], NT_[:], b3(cmask[:, 7, :], 6, 128), ALU.mult, [NT_, cmask], [Qm[1]])
                h.tt("dve", Tb[:], Pm[1][:], b3(identf[:], 6, 128), ALU.add, [Pm[1], identf], [Tb])
                h.tt("dve", Xb[:], Qm[1][:], b3(identf[:], 6, 128), ALU.add, [Qm[1], identf], [Xb])
                Tc, Xc = Tb, Xb
                Tn, Xn = Tb2, Xb2
                for lv in range(1, 7):
                    w1_ = w4(W1)
                    w2_ = w4(W2)
                    for hf_ in range(2):
                        hs = range(3 * hf_, 3 * hf_ + 3)
                        h.mm([(w1_[:, hf_, hd % 3, :], NT_[:, hd, :], Tc[:, hd, :], True, True) for hd in hs], [NT_, Tc], [W1h[hf_]])
                        h.mm([(w2_[:, hf_, hd % 3, :], Nn[:, hd, :], Xc[:, hd, :], True, True) for hd in hs], [Nn, Xc], [W2h[hf_]])
                    for hf_ in range(2):
                        sl = slice(3 * hf_, 3 * hf_ + 3)
                        h.tt("dve", M1b[:, sl, :], w1_[:, hf_, :, :], cmask6[:, lv, sl, :], ALU.mult, [W1h[hf_], cmask6], [M1bh[hf_]])
                        h.tt("dve", M1pb[:, sl, :], w2_[:, hf_, :, :], cmask6[:, 7 + lv, sl, :], ALU.mult, [W2h[hf_], cmask6], [M1pbh[hf_]])
                    for hf_ in range(2):
                        hs = range(3 * hf_, 3 * hf_ + 3)
                        sl = slice(3 * hf_, 3 * hf_ + 3)
                        h.mm([(W1[:, hf_, 0:384], identb[:], Tc[:, sl, :].rearrange("p a l -> p (a l)"), True, False)]
                             + [(w1_[:, hf_, hd % 3, :], Xc[:, hd, :], M1b[:, hd, :], False, True) for hd in hs],
                             [identb, Tc, Xc, M1bh[hf_]], [W1h[hf_]])
                        h.mm([(W2[:, hf_, 0:384], identb[:], Xc[:, sl, :].rearrange("p a l -> p (a l)"), True, False)]
                             + [(w2_[:, hf_, hd % 3, :], Tc[:, hd, :], M1pb[:, hd, :], False, True) for hd in hs],
                             [identb, Tc, Xc, M1pbh[hf_]], [W2h[hf_]])
                    for hf_ in range(2):
                        sl = slice(3 * hf_, 3 * hf_ + 3)
                        h.cp("act", Tn[:, sl, :], w1_[:, hf_, :, :], [W1h[hf_]], [Tn])
                        h.cp("act", Xn[:, sl, :], w2_[:, hf_, :, :], [W2h[hf_]], [Xn])
                    Tc, Xc, Tn, Xn = Tn, Xn, Tc, Xc
                Xb = Xc
                if stop < 4:
                    h.memset("pool", yo_[:, :, tc], 0.0, [yo_])
                    continue
                h.tr([(PB[:, j * 128:(j + 1) * 128], kn_[:, j, tc]) for j in range(3)], identb[:], [kn_, identb], [PB])
                h.tt("dve", x3(kdec[:]), x3(PB[:, 0:384]), s3(dkk[:], 6, 64), ALU.mult, [PB, dkk], [kdec])
                h.tr([(W0[:, 0, j * 128:(j + 1) * 128], v_[:, j, tc]) for j in range(3)], identf[:], [v_, identf], [W0])
                h.cp("act", v_tm[:], W0[:, 0, 0:384], [W0], [v_tm])
                if stop < 5:
                    h.memset("pool", yo_[:, :, tc], 0.0, [yo_])
                    continue
                h.mm([(W1[:, 0, j * 128:(j + 1) * 128], kn_[:, j, tc], Sb[:, j, :], True, True) for j in range(3)], [kn_, Sb], [W1a, W1b])
                h.tt("dve", x3(rr_[:]), x3(W1[:, 0, 0:384]), s3(egc[:], 6, 64), ALU.mult, [W1a, W1b, egc], [rr_])
                h.tt("pool", rr_[:], rr_[:], v_tm[:], ALU.subtract, [rr_, v_tm], [rr_])
                h.tt("pool", x3(rb[:]), x3(rr_[:]), s3(nbeta[:], 6, 64), ALU.mult, [rr_, nbeta], [rb])
                h.mm([(W2[:, 0, hd * 64:(hd + 1) * 64], Xb[:, hd, :], rb[:, hd * 64:(hd + 1) * 64], True, True) for hd in range(6)], [Xb, rb], [W2a, W2b])
                h.cp("act", vnb[:], W2[:, 0, 0:384], [W2a, W2b], [vnb])
                h.mm([(W1[:, 0, j * 128:(j + 1) * 128], qn_[:, j, tc], Sb[:, j, :], True, True) for j in range(3)], [qn_, Sb], [W1a, W1b])
                h.tt("dve", x3(oa[:]), x3(W1[:, 0, 0:384]), s3(egc[:], 6, 64), ALU.mult, [W1a, W1b, egc], [oa])
                h.mm([(W2[:, 0, hd * 64:(hd + 1) * 64], attnT[:, hd, :], vnb[:, hd * 64:(hd + 1) * 64], True, True) for hd in range(6)], [attnT, vnb], [W2a, W2b])
                h.tt("dve", o[:], W2[:, 0, 0:384], oa[:], ALU.add, [W2a, W2b, oa], [o])
                h.mm([(W0[:, 0, j * 128:(j + 1) * 128], kdec[:, j * 128:(j + 1) * 128], vnb[:, j * 128:(j + 1) * 128], True, True) for j in range(3)],
                     [kdec, vnb], [W0])
                h.tt("dve", tmpS[:], W0[:, 0, 0:384].rearrange("p (j c) -> p j c", j=3), b3(masks["bd"][:], 3, 128), ALU.mult, [W0, masks["bd"]], [tmpS])
                h.tt("pool", Sf[:], Sf[:], s3(gtc[:], 3, 128), ALU.mult, [Sf, gtc], [Sf])
                h.tt("pool", Sf[:], Sf[:], tmpS[:], ALU.add, [Sf, tmpS], [Sf])
                h.cp("act", Sb[:], Sf[:], [Sf], [Sb])
                if stop < 6:
                    h.memset("pool", yo_[:, :, tc], 0.0, [yo_])
                    continue
                h.tt("pool", sq[:], o[:], o[:], ALU.mult, [o], [sq])
                h.reduce(ss6[:], x3(sq[:]), ALU.add, [sq], [ss6])
                h.act(rs6[:], ss6[:], AF.Ln, [ss6, epsb], [rs6], bias=epsb[:, 0:1], scale=1.0 / 64)
                h.act(rs6[:], rs6[:], AF.Exp, [rs6], [rs6], scale=-0.5)
                h.tt("dve", x3(o[:]), x3(o[:]), s3(rs6[:], 6, 64), ALU.mult, [o, rs6], [o])
                h.tt("pool", x3(o[:]), x3(o[:]), b3(gnw[:], 6, 64), ALU.mult, [o, gnw], [o])
                h.act(ez[:], pj_[:, ti, 0:384], AF.Exp, [pj_], [ez], scale=-1.0)
                h.ts("pool", ez[:], ez[:], 1.0, None, ALU.add, None, [ez], [ez])
                h.tt("pool", zg[:], o[:], pj_[:, ti, 0:384], ALU.mult, [o, pj_], [zg])
                h.recip(ez[:], ez[:], [ez], [ez])
                h.tt("pool", yb[:], zg[:], ez[:], ALU.mult, [zg, ez], [yb])
                h.tr([(PB[:, j * 128:(j + 1) * 128], yb[:, j * 128:(j + 1) * 128]) for j in range(3)], identb[:], [yb, identb], [PB])
                h.cp("act", yo_[:, :, tc], PB[:, 0:384].rearrange("p (j t) -> p j t", j=3), [PB], [yo_])
            h.dma("sp", k.y_d.t[256:640, cols].rearrange("(j p) t -> p j t", p=128), yo_[:], [yo_], [k.r_y[1][mt]], yo_)
        P.end_phase()


class K:
    def __init__(self, T, L, flags):
        self.T = T
        self.L = L
        self.NT = T // 128
        self.flags = flags


def bcast(ap, shape):
    return ap.to_broadcast(list(shape))


def build(T, L, flags=None):
    flags = flags or {}
    nc = bass.Bass("TRN2", target_bir_lowering=False)
    k = K(T, L, flags)
    NT = T // 128
    with contextlib.ExitStack() as es0:
        P = Prog(nc, es0)
        P.verbose = bool(flags.get("verbose"))
        k.P = P
        inp = {}

        def ein(name, shape):
            inp[name] = P.dram(name, shape, F32, "ExternalInput")
            return inp[name]

        x_in = ein("x", [T, D])
        c_in = ein("c", [1, D])
        w_ada = ein("w_ada", [L, D, 6 * D])
        b_ada = ein("b_ada", [L, 6 * D])
        norm_mix = ein("norm_mix", [L, D])
        norm_ffn = ein("norm_ffn", [L, D])
        norm_final = ein("norm_final", [1, D])
        w_rt = ein("w_rt", [L, D, 36])
        b_rt = ein("b_rt", [L, 36])
        w_gate = ein("moe_w_gate", [L, NEXP, D, DEXP])
        w_up = ein("moe_w_up", [L, NEXP, D, DEXP])
        w_down = ein("moe_w_down", [L, NEXP, DEXP, D])
        ein("w_in_f", [L, D, 2304])
        ein("w_in_t", [L, D, 786])
        ein("w_out", [L, D, D])
        ein("conv_w", [L, 128, 16, 4])
        ein("conv_b", [L, 128, 16])
        ein("bias12", [L, 12])
        ein("alog12", [L, 12])
        ein("ssd_d", [L, 6])
        ein("ssd_norm", [L, 384])
        ein("gdn_norm", [L, 64])
        ein("gdn_cmask", [14, 128, 128])
        ein("s5_are", [L, 128, 8])
        ein("s5_aim", [L, 128, 8])
        ein("s5_ldt", [L, 128, 8])
        ein("s5_dcol", [L, 128, 2])
        ein("s5_ncol", [L, 128, 2])
        ein("s5_bT_re", [L, 8, 128, 128])
        ein("s5_bT_im", [L, 8, 128, 128])
        ein("s5_cT_re", [L, 8, 128, 128])
        ein("s5_cT_im", [L, 8, 128, 128])
        ein("s5_w_glu", [L, 256, 256])
        out = P.dram("out", [T, D], F32, "ExternalOutput")
        k.u_d = P.dram("u_d", [256, T], F32)
        k.qn_d = P.dram("qn_d", [384, T], BF16)
        k.kn_d = P.dram("kn_d", [384, T], BF16)
        k.v_d = P.dram("v_d", [384, T], F32)
        k.xs_d = P.dram("xs_d", [384, T], F32)
        k.B_d = P.dram("B_d", [256, T], BF16)
        k.C_d = P.dram("C_d", [256, T], BF16)
        k.pt_d = P.dram("pt_d", [T, 786], F32)
        k.y_d = P.dram("y_d", [D, T], BF16)
        k.r_pre = [P.region(f"pre_{i}") for i in range(max(1, T // 512))]
        k.r_y = [[P.region(f"y{j}_{i}") for i in range(max(1, T // 512))] for j in range(3)]
        k.h = H(P)
        h = k.h
        modv = P.dram("modv", [L, 6 * D], F32)
        dbg = flags.get("dbg", False)
        if dbg:
            dbg_coef = P.dram("dbg_coef", [T, 32], F32, "ExternalOutput")
            dbg_y = P.dram("dbg_y", [T, D], F32, "ExternalOutput")
            dbg_h = P.dram("dbg_h", [T, D], F32, "ExternalOutput")
            r_dbg = P.region("dbg")
        scr = [P.dram("xs0", [T, D], F32), P.dram("xs1", [T, D], F32)]
        k.inp = inp

        def regs(name):
            return [P.region(f"{name}_{i}") for i in range(NT)]
        r_x = regs("x")
        r_scr = [regs("xs0"), regs("xs1")]
        r_out = regs("out")
        r_modv = P.region("modv")

        identf = P.sb(es0, "identf", [128, 128], F32)
        identb = P.sb(es0, "identb", [128, 128], BF16)
        P.op("pool", lambda e: e.memset(identf[:], 0.0), writes=[identf])
        P.op("pool", lambda e: e.affine_select(out=identf[:], in_=identf[:], pattern=[[-1, 128]],
                                               compare_op=ALU.not_equal, fill=1.0, base=0,
                                               channel_multiplier=1),
             reads=[identf], writes=[identf])
        P.op("dve", lambda e: e.tensor_copy(out=identb[:], in_=identf[:]), reads=[identf], writes=[identb])
        k.identf, k.identb = identf, identb

        with contextlib.ExitStack() as es:
            ccol = P.sb(es, "ccol", [128, 8], F32)
            cb = P.sb(es, "cb", [128, 8, 128], BF16)
            wa = [P.sb(es, f"wa{i}", [128, 8, 512], BF16) for i in range(2)]
            pm = [P.ps(es, f"pm{i}", [128, 512], F32) for i in range(2)]
            brow = [P.sb(es, f"brow{i}", [1, 512], F32) for i in range(2)]
            mrow = [P.sb(es, f"mrow{i}", [1, 512], F32) for i in range(2)]
            P.op("sp", lambda e: e.dma_start(out=ccol[:], in_=c_in.t.rearrange("o (k p) -> p (o k)", p=128),
                                             allow_slow_non_contiguous=True),
                 writes=[ccol], dma=ccol)
            P.op("act", lambda e: e.activation(out=ccol[:], in_=ccol[:], func=AF.Silu), reads=[ccol], writes=[ccol])
            P.op("dve", lambda e: e.tensor_copy(out=cb[:], in_=bcast(ccol[:].unsqueeze(2), [128, 8, 128])),
                 reads=[ccol], writes=[cb])
            it = 0
            for l in range(L):
                for n in range(12):
                    i = it % 2
                    it += 1
                    P.op("pool", lambda e, l=l, n=n, i=i: e.dma_start(
                        out=wa[i][:], in_=w_ada.t[l, :, n * 512:(n + 1) * 512].rearrange("(k p) n -> p k n", p=128)),
                        writes=[wa[i]], dma=wa[i])
                    P.op("sp", lambda e, l=l, n=n, i=i: e.dma_start(
                        out=brow[i][:], in_=b_ada.t[l:l + 1, n * 512:(n + 1) * 512]),
                        writes=[brow[i]], dma=brow[i])

                    def mm(e, i=i):
                        r = None
                        for kk in range(8):
                            r = e.matmul(pm[i][:], lhsT=cb[:, kk, :], rhs=wa[i][:, kk, :], start=(kk == 0), stop=(kk == 7))
                        return r
                    P.op("pe", mm, reads=[cb, wa[i]], writes=[pm[i]])
                    P.op("dve", lambda e, i=i: e.tensor_tensor(out=mrow[i][:], in0=pm[i][0:1, :], in1=brow[i][:], op=ALU.add),
                         reads=[pm[i], brow[i]], writes=[mrow[i]])
                    P.op("sp", lambda e, l=l, n=n, i=i: e.dma_start(
                        out=modv.t[l:l + 1, n * 512:(n + 1) * 512], in_=mrow[i][:]),
                        reads=[mrow[i]], writes=[r_modv], dma=mrow[i])
            P.end_phase()

        def load_row(dst, src_ap, extra_reads=()):
            P.op("sp", lambda e: e.dma_start(out=dst[:], in_=src_ap.partition_broadcast(128)),
                 reads=list(extra_reads), writes=[dst], dma=dst)

        def norm_consts(es, l, nw, i_scale, i_shift, tag):
            A = P.sb(es, f"A{tag}", [128, D], F32)
            B = P.sb(es, f"B{tag}", [128, D], F32)
            W = P.sb(es, f"W{tag}", [128, D], F32)
            load_row(A, modv.t[l:l + 1, i_scale * D:(i_scale + 1) * D], [r_modv])
            load_row(B, modv.t[l:l + 1, i_shift * D:(i_shift + 1) * D], [r_modv])
            load_row(W, nw)
            P.op("dve", lambda e: e.scalar_tensor_tensor(out=A[:], in0=A[:], scalar=1.0, in1=W[:], op0=ALU.add, op1=ALU.mult),
                 reads=[A, W], writes=[A])
            return A, B

        def rms_mod(xt, A, B, hout, ssq, rstd, junk, tmp):
            P.op("act", lambda e: e.activation(out=junk[:], in_=xt[:], func=AF.Square, accum_out=ssq[:]),
                 reads=[xt], writes=[junk, ssq], est=0.9)
            P.op("dve", lambda e: e.tensor_scalar(out=rstd[:], in0=ssq[:], scalar1=1.0 / D, scalar2=EPS, op0=ALU.mult, op1=ALU.add),
                 reads=[ssq], writes=[rstd])
            P.op("act", lambda e: e.activation(out=rstd[:], in_=rstd[:], func=AF.Sqrt), reads=[rstd], writes=[rstd])
            P.op("dve", lambda e: e.reciprocal(out=rstd[:], in_=rstd[:]), reads=[rstd], writes=[rstd])
            if B is None:
                P.op("dve", lambda e: e.scalar_tensor_tensor(out=hout[:], in0=xt[:], scalar=rstd[:, 0:1], in1=A[:], op0=ALU.mult, op1=ALU.mult),
                     reads=[xt, rstd, A], writes=[hout], est=1.15)
            else:
                P.op("dve", lambda e: e.scalar_tensor_tensor(out=tmp[:], in0=xt[:], scalar=rstd[:, 0:1], in1=A[:], op0=ALU.mult, op1=ALU.mult),
                     reads=[xt, rstd, A], writes=[tmp], est=1.15)
                P.op("pool", lambda e: e.tensor_tensor(out=hout[:], in0=tmp[:], in1=B[:], op=ALU.add),
                     reads=[tmp, B], writes=[hout], est=1.3)

        k.modv, k.r_modv = modv, r_modv
        k.load_row, k.norm_consts, k.rms_mod = load_row, norm_consts, rms_mod
        cur = (x_in, r_x)
        nxt_i = 0

        def next_dst():
            nonlocal nxt_i
            d = (scr[nxt_i], r_scr[nxt_i])
            nxt_i ^= 1
            return d

        for l in range(L):
            if flags.get("mixer", True):
                dstt = next_dst()
                mixer_layer(k, l, cur, dstt)
                cur = dstt
            if flags.get("moe", True):
                src, rsrc = cur
                dst, rdst = next_dst()
                SBT = min(16, NT)
                with contextlib.ExitStack() as es:
                    A, B = norm_consts(es, l, norm_ffn.t[l:l + 1, :], 4, 3, "f")
                    G = P.sb(es, "Gf", [128, D], F32)
                    load_row(G, modv.t[l:l + 1, 5 * D:6 * D], [r_modv])
                    brt = P.sb(es, "brt", [128, 36], F32)
                    load_row(brt, b_rt.t[l:l + 1, :])
                    wrt = P.sb(es, "wrt", [128, 8, 36], F32)
                    P.op("sp", lambda e: e.dma_start(out=wrt[:], in_=w_rt.t[l].rearrange("(k p) n -> p k n", p=128)),
                         writes=[wrt], dma=wrt)
                    hT = P.sb(es, "hT", [128, 8, SBT * 128], BF16)
                    yacc = [P.sb(es, f"yacc{i}", [128, D], F32) for i in range(SBT)]
                    coef = P.sb(es, "coef", [128, SBT, 32], F32)
                    xt = [P.sb(es, f"xt{i}", [128, D], F32) for i in range(2)]
                    R_hf = Rot(P, es, "hf", [128, D], F32)
                    R_tmp = Rot(P, es, "tmpf", [128, D], F32)
                    R_junk = Rot(P, es, "junkf", [128, D], F32)
                    R_hTf = Rot(P, es, "hTf", [128, 8, 128], F32)
                    R_ssq = Rot(P, es, "ssq", [128, 1], F32)
                    R_rstd = Rot(P, es, "rstd", [128, 1], F32)
                    R_lg = Rot(P, es, "lg", [128, 36], F32)
                    R_sm = Rot(P, es, "sm", [128, 16], F32)
                    R_gm = Rot(P, es, "gm", [128, 4], F32)
                    R_gex = Rot(P, es, "gex", [128, 4], F32)
                    R_le4 = Rot(P, es, "le4", [128, 4, 8], F32)
                    R_les = Rot(P, es, "les", [128, 8], F32)
                    R_le2 = Rot(P, es, "le2", [128, 8], F32)
                    R_mk1 = Rot(P, es, "mk1", [128, 8], F32)
                    R_mk2 = Rot(P, es, "mk2", [128, 8], F32)
                    R_csel = Rot(P, es, "csel", [128, 8], F32)
                    wg = [P.sb(es, f"wg{i}", [128, 8, DEXP], BF16) for i in range(2)]
                    wu = [P.sb(es, f"wu{i}", [128, 8, DEXP], BF16) for i in range(2)]
                    wd = [P.sb(es, f"wd{i}", [128, 2, D], BF16) for i in range(2)]
                    sg = [P.sb(es, f"sg{i}", [128, 512], F32) for i in range(2)]
                    hid = [P.sb(es, f"hid{i}", [128, 2, 512], BF16) for i in range(2)]
                    ptr = P.ps(es, "ptr", [128, 8, 128], F32)
                    pg = [P.ps(es, f"pg{i}", [128, 512], F32) for i in range(2)]
                    pu = [P.ps(es, f"pu{i}", [128, 512], F32) for i in range(2)]
                    py = [P.ps(es, f"py{i}", [128, 512], F32) for i in range(2)]
                    py.append(Buf(ptr.t[:, 0:4, :].rearrange("p a b -> p (a b)"), "py2"))
                    py.append(Buf(ptr.t[:, 4:8, :].rearrange("p a b -> p (a b)"), "py3"))
                    ptrA, ptrB = py[2], py[3]

                    wcnt = 0
                    for sb0 in range(0, NT, SBT):
                        def router_tile(t, ti, xb, hf, tmp, junk, hTf, ssq, rstd, lg, sm, gm, gex, le4, les, le2, mk1, mk2, csel):
                                P.op("sp", lambda e, t=t, xb=xb: e.dma_start(out=xb[:], in_=src.t[t * 128:(t + 1) * 128, :]),
                                     reads=[rsrc[t]], writes=[xb], dma=xb)
                                rms_mod(xb, A, B, hf, ssq, rstd, junk, tmp)

                                if dbg and l == 0:
                                    P.op("sp", lambda e, t=t: e.dma_start(out=dbg_h.t[t * 128:(t + 1) * 128, :], in_=hf[:]),
                                         reads=[hf], writes=[r_dbg], dma=hf)

                                def trf(e):
                                    r = None
                                    for kk in range(8):
                                        r = e.transpose(out=ptr[:, kk, :], in_=hf[:, kk * 128:(kk + 1) * 128], identity=identf[:])
                                    return r
                                P.op("pe", trf, reads=[hf, identf], writes=[ptrA, ptrB])
                                P.op("act", lambda e: e.copy(out=hTf[:], in_=ptr[:]), reads=[ptrA, ptrB], writes=[hTf])
                                P.op("dve", lambda e, ti=ti: e.tensor_copy(out=hT[:, :, ti * 128:(ti + 1) * 128], in_=hTf[:]),
                                     reads=[hTf], writes=[hT])

                                def mrt(e):
                                    r = None
                                    for kk in range(8):
                                        r = e.matmul(ptr[:, 0, 0:36], lhsT=hTf[:, kk, :], rhs=wrt[:, kk, :], start=(kk == 0), stop=(kk == 7))
                                    return r
                                P.op("pe", mrt, reads=[hTf, wrt], writes=[ptrA, ptrB])
                                P.op("dve", lambda e: e.tensor_tensor(out=lg[:], in0=ptr[:, 0, 0:36], in1=brt[:], op=ALU.add),
                                     reads=[ptrA, ptrB, brt], writes=[lg])
                                P.op("dve", lambda e: e.tensor_reduce(out=sm[:, 0:1], in_=lg[:, 0:4], axis=AX.X, op=ALU.max),
                                     reads=[lg], writes=[sm])
                                P.op("dve", lambda e: e.tensor_scalar(out=gm[:], in0=lg[:, 0:4], scalar1=sm[:, 0:1], scalar2=None, op0=ALU.is_equal),
                                     reads=[lg, sm], writes=[gm])
                                P.op("dve", lambda e: e.tensor_scalar(out=sm[:, 1:2], in0=sm[:, 0:1], scalar1=-1.0, scalar2=None, op0=ALU.mult),
                                     reads=[sm], writes=[sm])
                                P.op("act", lambda e: e.activation(out=gex[:], in_=lg[:, 0:4], func=AF.Exp, bias=sm[:, 1:2], accum_out=sm[:, 2:3]),
                                     reads=[lg, sm], writes=[gex, sm])
                                P.op("dve", lambda e: e.reciprocal(out=sm[:, 3:4], in_=sm[:, 2:3]), reads=[sm], writes=[sm])
                                P.op("dve", lambda e: e.tensor_tensor(out=le4[:], in0=lg[:, 4:36].rearrange("p (g e) -> p g e", g=4),
                                                                      in1=bcast(gm[:].unsqueeze(2), [128, 4, 8]), op=ALU.mult),
                                     reads=[lg, gm], writes=[le4])
                                P.op("dve", lambda e: e.tensor_reduce(out=les[:], in_=le4[:].rearrange("p g e -> p e g"), axis=AX.X, op=ALU.add),
                                     reads=[le4], writes=[les])
                                P.op("dve", lambda e: e.tensor_reduce(out=sm[:, 4:5], in_=les[:], axis=AX.X, op=ALU.max), reads=[les], writes=[sm])
                                P.op("dve", lambda e: e.tensor_scalar(out=mk1[:], in0=les[:], scalar1=sm[:, 4:5], scalar2=None, op0=ALU.is_equal),
                                     reads=[les, sm], writes=[mk1])
                                P.op("dve", lambda e: e.scalar_tensor_tensor(out=le2[:], in0=mk1[:], scalar=-1e30, in1=les[:], op0=ALU.mult, op1=ALU.add),
                                     reads=[mk1, les], writes=[le2])
                                P.op("dve", lambda e: e.tensor_reduce(out=sm[:, 5:6], in_=le2[:], axis=AX.X, op=ALU.max), reads=[le2], writes=[sm])
                                P.op("dve", lambda e: e.tensor_scalar(out=mk2[:], in0=le2[:], scalar1=sm[:, 5:6], scalar2=None, op0=ALU.is_equal),
                                     reads=[le2, sm], writes=[mk2])
                                P.op("dve", lambda e: e.tensor_tensor(out=sm[:, 6:7], in0=sm[:, 4:5], in1=sm[:, 5:6], op=ALU.subtract),
                                     reads=[sm], writes=[sm])
                                P.op("act", lambda e: e.activation(out=sm[:, 7:8], in_=sm[:, 6:7], func=AF.Sigmoid), reads=[sm], writes=[sm])
                                P.op("act", lambda e: e.activation(out=sm[:, 8:9], in_=sm[:, 6:7], func=AF.Sigmoid, scale=-1.0), reads=[sm], writes=[sm])
                                P.op("dve", lambda e: e.tensor_scalar(out=sm[:, 7:9], in0=sm[:, 7:9], scalar1=sm[:, 3:4], scalar2=None, op0=ALU.mult),
                                     reads=[sm], writes=[sm])
                                P.op("dve", lambda e: e.tensor_scalar(out=csel[:], in0=mk1[:], scalar1=sm[:, 7:8], scalar2=None, op0=ALU.mult),
                                     reads=[mk1, sm], writes=[csel])
                                P.op("dve", lambda e: e.scalar_tensor_tensor(out=csel[:], in0=mk2[:], scalar=sm[:, 8:9], in1=csel[:], op0=ALU.mult, op1=ALU.add),
                                     reads=[mk2, sm, csel], writes=[csel])
                                P.op("dve", lambda e, ti=ti: e.tensor_tensor(out=coef[:, ti, :].rearrange("p (g e) -> p g e", g=4),
                                                                             in0=bcast(gm[:].unsqueeze(2), [128, 4, 8]),
                                                                             in1=bcast(csel[:].unsqueeze(1), [128, 4, 8]), op=ALU.mult),
                                     reads=[gm, csel], writes=[coef])

                        for ti in range(SBT):
                            t = sb0 + ti
                            router_tile(t, ti, xt[t % 2], R_hf.at(t), R_tmp.at(t), R_junk.at(t), R_hTf.at(t), R_ssq.at(t), R_rstd.at(t), R_lg.at(t), R_sm.at(t), R_gm.at(t), R_gex.at(t), R_le4.at(t), R_les.at(t), R_le2.at(t), R_mk1.at(t), R_mk2.at(t), R_csel.at(t))
                        nblk = (SBT * 128 + 511) // 512
                        seq = [(ex, blk) for ex in range(NEXP) for blk in range(nblk)]

                        def load_w(ex):
                            wi = ex % 2
                            h.dma("pool", wg[wi][:], w_gate.t[l, ex].rearrange("(k p) n -> p k n", p=128), [], [wg[wi]], wg[wi])
                            h.dma("pool", wu[wi][:], w_up.t[l, ex].rearrange("(k p) n -> p k n", p=128), [], [wu[wi]], wu[wi])
                            h.dma("pool", wd[wi][:], w_down.t[l, ex].rearrange("(k p) n -> p k n", p=128), [], [wd[wi]], wd[wi])

                        def GU(i):
                            ex, blk = seq[i]
                            wi, bi = ex % 2, i % 2
                            c0 = blk * 512
                            cw = min(512, SBT * 128 - c0)
                            for fc in range(2):
                                fs = slice(fc * 128, (fc + 1) * 128)
                                h.mm([(pg[fc][:, 0:cw], wg[wi][:, kk, fs], hT[:, kk, c0:c0 + cw], kk == 0, kk == 7) for kk in range(8)],
                                     [wg[wi], hT], [pg[fc]])
                                h.act(sg[fc][:, 0:cw], pg[fc][:, 0:cw], AF.Silu, [pg[fc]], [sg[fc]])
                                yield
                                h.mm([(pu[fc][:, 0:cw], wu[wi][:, kk, fs], hT[:, kk, c0:c0 + cw], kk == 0, kk == 7) for kk in range(8)],
                                     [wu[wi], hT], [pu[fc]])
                                h.tt("dve", hid[bi][:, fc, 0:cw], sg[fc][:, 0:cw], pu[fc][:, 0:cw], ALU.mult, [sg[fc], pu[fc]], [hid[bi]])
                                yield

                        def DN(i):
                            ex, blk = seq[i]
                            wi, bi = ex % 2, i % 2
                            c0 = blk * 512
                            cw = min(512, SBT * 128 - c0)
                            for st in range(cw // 128):
                                ti = blk * 4 + st
                                for n2 in range(2):
                                    pi = (st * 2 + n2) % 4
                                    ns = slice(n2 * 512, (n2 + 1) * 512)
                                    h.mm([(py[pi][:], hid[bi][:, fc, st * 128:(st + 1) * 128], wd[wi][:, fc, ns], fc == 0, fc == 1) for fc in range(2)],
                                         [hid[bi], wd[wi]], [py[pi]])
                                    if ex == 0:
                                        h.ts("dve", yacc[ti][:, ns], py[pi][:], coef[:, ti, ex:ex + 1], None, ALU.mult, None, [py[pi], coef], [yacc[ti]])
                                    else:
                                        h.stt("dve", yacc[ti][:, ns], py[pi][:], coef[:, ti, ex:ex + 1], yacc[ti][:, ns], ALU.mult, ALU.add,
                                              [py[pi], coef, yacc[ti]], [yacc[ti]])
                                    yield
                            if blk == nblk - 1 and ex + 2 < NEXP:
                                load_w(ex + 2)

                        def drain(g):
                            for _ in g:
                                pass

                        def step(g, n):
                            for _ in range(n):
                                if next(g, "END") == "END":
                                    return

                        load_w(0)
                        load_w(1)
                        drain(GU(0))
                        for i in range(len(seq)):
                            gd = DN(i)
                            if i + 1 < len(seq):
                                gg = GU(i + 1)
                                for _ in range(4):
                                    step(gg, 1)
                                    step(gd, 2)
                                drain(gg)
                            drain(gd)
                        for ti in range(SBT):
                            t = sb0 + ti
                            if dbg and l == 0:
                                P.op("sp", lambda e, t=t, ti=ti: e.dma_start(out=dbg_y.t[t * 128:(t + 1) * 128, :], in_=yacc[ti][:]),
                                     reads=[yacc[ti]], writes=[r_dbg], dma=yacc[ti])
                                P.op("sp", lambda e, t=t, ti=ti: e.dma_start(out=dbg_coef.t[t * 128:(t + 1) * 128, :], in_=coef[:, ti, :]),
                                     reads=[coef], writes=[r_dbg], dma=coef)
                            xb = xt[t % 2]
                            P.op("sp", lambda e, t=t, xb=xb: e.dma_start(out=xb[:], in_=src.t[t * 128:(t + 1) * 128, :]),
                                 reads=[rsrc[t]], writes=[xb], dma=xb)
                            P.op("pool", lambda e, ti=ti: e.tensor_tensor(out=yacc[ti][:], in0=yacc[ti][:], in1=G[:], op=ALU.mult),
                                 reads=[yacc[ti], G], writes=[yacc[ti]])
                            P.op("dve", lambda e, ti=ti, xb=xb: e.tensor_tensor(out=yacc[ti][:], in0=yacc[ti][:], in1=xb[:], op=ALU.add),
                                 reads=[yacc[ti], xb], writes=[yacc[ti]])
                            P.op("sp", lambda e, t=t, ti=ti: e.dma_start(out=dst.t[t * 128:(t + 1) * 128, :], in_=yacc[ti][:]),
                                 reads=[yacc[ti]], writes=[rdst[t]], dma=yacc[ti])
                    P.end_phase()
                cur = (dst, rdst)

        src, rsrc = cur
        with contextlib.ExitStack() as es:
            Wn = P.sb(es, "Wn", [128, D], F32)
            load_row(Wn, norm_final.t[0:1, :])
            xt = [P.sb(es, f"xtn{i}", [128, D], F32) for i in range(2)]
            ho = [P.sb(es, f"hon{i}", [128, D], F32) for i in range(2)]
            junk = P.sb(es, "junkn", [128, D], F32)
            ssq = P.sb(es, "ssqn", [128, 1], F32)
            rstd = P.sb(es, "rstdn", [128, 1], F32)
            for t in range(NT):
                xb = xt[t % 2]
                hb = ho[t % 2]
                P.op("sp", lambda e, t=t, xb=xb: e.dma_start(out=xb[:], in_=src.t[t * 128:(t + 1) * 128, :]),
                     reads=[rsrc[t]], writes=[xb], dma=xb)
                rms_mod(xb, Wn, None, hb, ssq, rstd, junk, None)
                P.op("sp", lambda e, t=t, hb=hb: e.dma_start(out=out.t[t * 128:(t + 1) * 128, :], in_=hb[:]),
                     reads=[hb], writes=[r_out[t]], dma=hb)
            P.final_wait("sp", r_out)
            P.end_phase()
    return nc


def host_inputs(inputs, L, T):
    f = lambda a: np.ascontiguousarray(np.asarray(a, dtype=np.float32))
    w_rt = f(np.concatenate([inputs["moe_w_grp"][:L], inputs["moe_w_rt"][:L]], axis=-1))
    b_rt = f(np.concatenate([inputs["moe_b_grp"][:L], inputs["moe_b_rt"][:L]], axis=-1))
    w_in = np.asarray(inputs["w_in"][:L], dtype=np.float32)
    w_in_f = np.concatenate([w_in[:, :, 0:1408], w_in[:, :, 2188:3084]], axis=-1)
    w_in_t = np.concatenate([w_in[:, :, 1408:1792], w_in[:, :, 1804:2188], w_in[:, :, 1792:1798],
                             w_in[:, :, 3084:3090], w_in[:, :, 1798:1804]], axis=-1)
    gcw = np.asarray(inputs["gdn_conv_w"][:L], dtype=np.float32)
    scw = np.asarray(inputs["ssd_conv_w"][:L], dtype=np.float32)
    cw = np.concatenate([gcw, scw], axis=-1)
    conv_w = cw.reshape(L, 4, 16, 128).transpose(0, 3, 2, 1)
    cb = np.concatenate([np.zeros((L, 1152), np.float32), np.asarray(inputs["ssd_conv_b"][:L], dtype=np.float32)], axis=-1)
    conv_b = cb.reshape(L, 16, 128).transpose(0, 2, 1)
    bias12 = np.concatenate([inputs["gdn_dt_bias"][:L], inputs["ssd_dt_bias"][:L]], axis=-1)
    alog12 = np.concatenate([inputs["gdn_a_log"][:L], inputs["ssd_a_log"][:L]], axis=-1)
    def st_layout(a):
        a = np.asarray(a[:L], dtype=np.float32)
        return a.reshape(L, 8, 2, 64).transpose(0, 2, 3, 1).reshape(L, 128, 8)
    ldt = np.repeat(np.asarray(inputs["s5_log_dt"][:L], dtype=np.float32)[:, :, None], 64, axis=2)
    def bT_layout(b):
        b = np.asarray(b[:L], dtype=np.float32)
        o = np.zeros((L, 8, 128, 128), np.float32)
        for sc in range(8):
            for gl in range(2):
                r0 = 32 * (sc % 4) + 16 * gl
                o[:, sc, r0:r0 + 16, gl * 64:(gl + 1) * 64] = b[:, 2 * sc + gl].transpose(0, 2, 1)
        return o
    def cT_layout(c):
        c = np.asarray(c[:L], dtype=np.float32)
        o = np.zeros((L, 8, 128, 128), np.float32)
        for sc in range(8):
            for gl in range(2):
                r0 = 32 * (sc % 4) + 16 * gl
                o[:, sc, gl * 64:(gl + 1) * 64, r0:r0 + 16] = c[:, 2 * sc + gl].transpose(0, 2, 1)
        return o
    ii = np.arange(128)[:, None]
    jj = np.arange(128)[None, :]
    cm = []
    for lv in range(7):
        s_ = 1 << lv
        cm.append(((ii // (2 * s_) == jj // (2 * s_)) & (ii % (2 * s_) >= s_) & (jj % (2 * s_) < s_)).astype(np.float32))
    cmask = np.stack(cm + [m.T for m in cm], axis=0)
    col2 = lambda a: np.asarray(a[:L], dtype=np.float32).reshape(L, 2, 128).transpose(0, 2, 1)
    shared = {
        "gdn_cmask": f(cmask),
        "s5_are": f(st_layout(inputs["s5_a_re"])), "s5_aim": f(st_layout(inputs["s5_a_im"])), "s5_ldt": f(st_layout(ldt)),
        "s5_dcol": f(col2(inputs["s5_d"])), "s5_ncol": f(col2(inputs["s5_norm"])),
        "s5_bT_re": f(bT_layout(inputs["s5_b_re"])), "s5_bT_im": f(bT_layout(inputs["s5_b_im"])),
        "s5_cT_re": f(cT_layout(inputs["s5_c_re"])), "s5_cT_im": f(cT_layout(inputs["s5_c_im"])),
        "s5_w_glu": f(inputs["s5_w_glu"][:L]),
        "w_in_f": f(w_in_f), "w_in_t": f(w_in_t), "w_out": f(inputs["w_out"][:L]),
        "conv_w": f(conv_w), "conv_b": f(conv_b), "bias12": f(bias12), "alog12": f(alog12),
        "ssd_d": f(inputs["ssd_d"][:L]), "ssd_norm": f(inputs["ssd_norm"][:L]), "gdn_norm": f(inputs["gdn_norm"][:L]),
        "w_ada": f(inputs["w_ada"][:L]), "b_ada": f(inputs["b_ada"][:L]),
        "norm_mix": f(inputs["norm_mix"][:L]), "norm_ffn": f(inputs["norm_ffn"][:L]),
        "norm_final": f(inputs["norm_final"]).reshape(1, D),
        "w_rt": w_rt, "b_rt": b_rt,
        "moe_w_gate": f(inputs["moe_w_gate"][:L]), "moe_w_up": f(inputs["moe_w_up"][:L]),
        "moe_w_down": f(inputs["moe_w_down"][:L]),
    }
    maps = []
    B = inputs["x"].shape[0]
    for b in range(B):
        m = dict(shared)
        m["x"] = f(inputs["x"][b, :T])
        m["c"] = f(inputs["c"][b]).reshape(1, D)
        maps.append(m)
    return maps


def run(inputs, L, T, flags=None, trace=False):
    nc = build(T, L, flags)
    maps = host_inputs(inputs, L, T)
    res = run_bass_kernel_spmd(nc, maps, core_ids=list(range(len(maps))))
    if flags and flags.get("dbg"):
        return res.results
    return np.stack([r["out"] for r in res.results], axis=0)


def kernel(**inputs):
    return run(inputs, 4, 4096).astype(np.float32)
```

```python
import contextlib
import math
import numpy as np
import concourse.bass as bass
import concourse.mybir as mybir
from concourse.bass_utils import run_bass_kernel_spmd

F32 = mybir.dt.float32
BF16 = mybir.dt.bfloat16
ALU = mybir.AluOpType
AF = mybir.ActivationFunctionType
AX = mybir.AxisListType

D = 1024
NEXP = 32
DEXP = 256
EPS = 1e-6
ENGS = ("pe", "act", "dve", "pool", "sp")


class Buf:
    def __init__(self, t, name, multi=False):
        self.t = t
        self.name = name
        self.w = {}
        self.r = {}
        self.sem = None
        self.dcnt = 0
        self.multi = multi

    def __getitem__(self, k):
        return self.t[k]


class Prog:
    SEM_LAT = 0.15

    def __init__(self, nc, es):
        self.nc = nc
        self.es = es
        self.ops = []
        self.sems = []
        self.esem = {}
        self.ecnt = {e: 0 for e in ENGS}
        self.waited = {e: {} for e in ENGS}
        for e in ENGS:
            if e != "sp":
                self.esem[e] = self.newsem("e_" + e)
        self.uid = 0
        self.dsem_pool = []
        self.dsem_pool_sw = []
        self.dbufs = []
        self.phase_bufs = []

    def newsem(self, name):
        s = self.es.enter_context(self.nc.semaphore(name))
        self.sems.append(s)
        return len(self.sems) - 1

    def sb(self, es, name, shape, dtype):
        self.uid += 1
        name = f"{name}_{self.uid}"
        t = es.enter_context(self.nc.sbuf_tensor(name, list(shape), dtype))
        b = Buf(t, name)
        self.phase_bufs.append(b)
        return b

    def ps(self, es, name, shape, dtype):
        self.uid += 1
        name = f"{name}_{self.uid}"
        t = es.enter_context(self.nc.psum_tensor(name, list(shape), dtype))
        return Buf(t, name)

    def dram(self, name, shape, dtype, kind="Internal"):
        t = self.nc.dram_tensor(name, list(shape), dtype, kind=kind).ap()
        return Buf(t, name)

    def region(self, name):
        return Buf(None, name, multi=True)

    def op(self, eng, fn, reads=(), writes=(), dma=None, est=None):
        if est is None:
            est = {"pe": 1.0, "act": 0.5, "dve": 0.35, "pool": 0.45, "sp": 3.0}[eng] if dma is None else 3.0
        self.ops.append((eng, fn, tuple(reads), tuple(writes), dma, est))

    def final_wait(self, eng, bufs):
        pass

    def end_phase(self):
        ops = self.ops
        self.ops = []
        n = len(ops)
        lw, rd = {}, {}
        deps = [None] * n
        for i, (eng, fn, reads, writes, dma, est) in enumerate(ops):
            d = set()
            for b in reads:
                d.update(lw.get(id(b), ()))
            for b in writes:
                d.update(rd.get(id(b), ()))
                if not b.multi:
                    d.update(lw.get(id(b), ()))
            deps[i] = d
            for b in reads:
                rd.setdefault(id(b), []).append(i)
            for b in writes:
                if b.multi:
                    lw.setdefault(id(b), []).append(i)
                else:
                    lw[id(b)] = [i]
                    rd[id(b)] = []
        import heapq
        succ = [[] for _ in range(n)]
        indeg = [0] * n
        for i in range(n):
            indeg[i] = len(deps[i])
            for j in deps[i]:
                succ[j].append(i)
        ready_t = [0.0] * n
        fin = [0.0] * n
        start = [0.0] * n
        efree = {e: 0.0 for e in ENGS}
        heap = [(0.0, i) for i in range(n) if indeg[i] == 0]
        heapq.heapify(heap)
        order = {e: [] for e in ENGS}
        glob = []
        while heap:
            rt, i = heapq.heappop(heap)
            eng, fn, reads, writes, dma, est = ops[i]
            st = max(rt, efree[eng])
            start[i] = st
            if dma is not None:
                occ = 0.5 if eng == "pool" else 0.08
                efree[eng] = st + occ
                fin[i] = st + occ + est
            else:
                efree[eng] = st + est
                fin[i] = st + est
            order[eng].append(i)
            glob.append(i)
            for k2 in succ[i]:
                indeg[k2] -= 1
                if ready_t[k2] < fin[i] + self.SEM_LAT:
                    ready_t[k2] = fin[i] + self.SEM_LAT
                if indeg[k2] == 0:
                    heapq.heappush(heap, (ready_t[k2], k2))
        assert len(glob) == n, "dependency cycle"
        tok = [None] * n
        for e in ENGS:
            if e == "sp":
                continue
        waits_raw = [None] * n
        cnt = dict(self.ecnt)
        for i in glob:
            eng, fn, reads, writes, dma, est = ops[i]
            w = {}
            for j in deps[i]:
                dj = ops[j][4]
                if dj is None:
                    s_, v_ = tok[j]
                else:
                    s_, v_ = dj.sem, dj.dcnt
                if w.get(s_, 0) < v_:
                    w[s_] = v_
            waits_raw[i] = w
            if dma is None:
                cnt[eng] += 1
                tok[i] = (self.esem[eng], cnt[eng])
            else:
                if dma.sem is None:
                    pool_ = self.dsem_pool_sw if eng == "pool" else self.dsem_pool
                    if pool_:
                        dma.sem, dma.dcnt = pool_.pop()
                    else:
                        dma.sem = self.newsem("d_" + dma.name)
                        dma.dcnt = 0
                    dma.sw = (eng == "pool")
                    self.dbufs.append(dma)
                dma.dcnt += 16
                tok[i] = (dma.sem, dma.dcnt)
        self.ecnt = cnt
        nc = self.nc
        sems = self.sems
        qs = {}
        for e in ENGS:
            wd = self.waited[e]
            q = []
            for i in order[e]:
                ws = []
                for s_, v_ in waits_raw[i].items():
                    if wd.get(s_, 0) < v_:
                        ws.append((s_, v_))
                        wd[s_] = v_
                inc = (tok[i][0], 16 if ops[i][4] is not None else 1)
                q.append((ws, ops[i][1], inc))
            qs[e] = q
        toks = {}
        for e, s_ in self.esem.items():
            if self.ecnt[e] > 0:
                toks[s_] = self.ecnt[e]
        for b in self.dbufs:
            toks[b.sem] = max(toks.get(b.sem, 0), b.dcnt)
        for e in ENGS:
            wd = self.waited[e]
            ws = []
            for s_, v_ in toks.items():
                if wd.get(s_, 0) < v_:
                    ws.append((s_, v_))
                    wd[s_] = v_
            if ws:
                qs[e].append((ws, None, None))
        for b in self.phase_bufs:
            if b.sem is not None:
                (self.dsem_pool_sw if getattr(b, "sw", False) else self.dsem_pool).append((b.sem, b.dcnt))
                self.dbufs.remove(b)
                b.sem = None
        self.phase_bufs = []

        def mk(e):
            def f(eng):
                for waits, fn, inc in qs[e]:
                    for s_, v_ in waits:
                        eng.wait_ge(sems[s_], v_)
                    if fn is None:
                        continue
                    ins = fn(eng)
                    ins.then_inc(sems[inc[0]], inc[1])
            return f

        with nc.Block() as block:
            block.sync(mk("sp"))
            block.scalar(mk("act"))
            block.vector(mk("dve"))
            block.gpsimd(mk("pool"))
            block.tensor(mk("pe"))
        self.last_makespan = max(efree.values()) if n else 0.0
        if getattr(self, "verbose", False):
            busy = {e: 0.0 for e in ENGS}
            for i in range(n):
                if ops[i][4] is None:
                    busy[ops[i][0]] += ops[i][5]
            print(f"[phase] n_ops={n} model_makespan={self.last_makespan:.0f}us busy=" +
                  " ".join(f"{e}:{busy[e]:.0f}" for e in ENGS), flush=True)


def _fsz(ap):
    n = 1
    for d in ap.shape[1:]:
        n *= int(d)
    return n


def _est(eng, ap, psum=False):
    n = _fsz(ap)
    if eng == "dve":
        return (60 + n) / 960.0 + (0.06 if psum else 0.0)
    if eng == "act":
        return (220 + n) / 1400.0
    if eng == "pool":
        return (120 + n) / 900.0
    return 0.5


class H:
    def __init__(self, P):
        self.P = P

    def dma(self, eng, out, in_, R, W, buf, slow=False):
        nbytes = _fsz(out) * 128 * 4
        est = 2.0 + nbytes / 150000.0
        if slow:
            self.P.op(eng, lambda e: e.dma_start(out=out, in_=in_, allow_slow_non_contiguous=True), reads=R, writes=W, dma=buf, est=est)
        else:
            self.P.op(eng, lambda e: e.dma_start(out=out, in_=in_), reads=R, writes=W, dma=buf, est=est)

    def tt(self, eng, out, in0, in1, op, R, W):
        self.P.op(eng, lambda e: e.tensor_tensor(out=out, in0=in0, in1=in1, op=op), reads=R, writes=W, est=_est(eng, out))

    def ts(self, eng, out, in0, s1, s2, op0, op1, R, W):
        if s2 is None:
            self.P.op(eng, lambda e: e.tensor_scalar(out=out, in0=in0, scalar1=s1, scalar2=None, op0=op0), reads=R, writes=W, est=_est(eng, out))
        else:
            self.P.op(eng, lambda e: e.tensor_scalar(out=out, in0=in0, scalar1=s1, scalar2=s2, op0=op0, op1=op1), reads=R, writes=W, est=_est(eng, out))

    def stt(self, eng, out, in0, sc, in1, op0, op1, R, W):
        eng = "dve"
        self.P.op(eng, lambda e: e.scalar_tensor_tensor(out=out, in0=in0, scalar=sc, in1=in1, op0=op0, op1=op1), reads=R, writes=W, est=_est(eng, out))

    def act(self, out, in_, func, R, W, bias=None, scale=None, accum=None):
        kw = {}
        if bias is not None:
            kw["bias"] = bias
        if scale is not None:
            kw["scale"] = scale
        if accum is not None:
            kw["accum_out"] = accum
        self.P.op("act", lambda e: e.activation(out=out, in_=in_, func=func, **kw), reads=R, writes=W, est=_est("act", out))

    def cp(self, eng, out, in_, R, W):
        if eng == "act":
            self.P.op("act", lambda e: e.copy(out=out, in_=in_), reads=R, writes=W, est=_est("act", out))
        else:
            self.P.op(eng, lambda e: e.tensor_copy(out=out, in_=in_), reads=R, writes=W, est=_est(eng, out))

    def memset(self, eng, ap, val, W):
        self.P.op(eng, lambda e: e.memset(ap, val), writes=W, est=_est(eng, ap))

    def recip(self, out, in_, R, W):
        self.P.op("dve", lambda e: e.reciprocal(out=out, in_=in_), reads=R, writes=W, est=_est("dve", out))

    def reduce(self, out, in_, op, R, W):
        self.P.op("dve", lambda e: e.tensor_reduce(out=out, in_=in_, axis=AX.X, op=op), reads=R, writes=W, est=_est("dve", in_))

    def mm(self, items, R, W):
        est = 0.0
        for (o, l, rh, st, sp) in items:
            est += (max(64, _fsz(rh)) * (4 if l.dtype == F32 else 1)) / 2400.0 + 0.01
        est += 0.06

        def f(e):
            r = None
            for (o, l, rh, st, sp) in items:
                r = e.matmul(o, lhsT=l, rhs=rh, start=st, stop=sp)
            return r
        self.P.op("pe", f, reads=R, writes=W, est=est)

    def tr(self, items, ident, R, W):
        est = 0.06
        for (o, i) in items:
            est += (128 * (4 if i.dtype == F32 else 1)) / 2400.0 + 0.03

        def f(e):
            r = None
            for (o, i) in items:
                r = e.transpose(out=o, in_=i, identity=ident)
            return r
        self.P.op("pe", f, reads=R, writes=W, est=est)

    def select(self, out, in_, cmp, fill, base, cm, pattern, R, W):
        self.P.op("pool", lambda e: e.affine_select(out=out, in_=in_, pattern=pattern, compare_op=cmp, fill=fill,
                                                    base=base, channel_multiplier=cm), reads=R, writes=W, est=_est("pool", out))


class Rot:
    def __init__(self, P, es, name, shape, dtype, n=2):
        self.bufs = [P.sb(es, f"{name}r{i}", shape, dtype) for i in range(n)]

    def at(self, i):
        return self.bufs[i % len(self.bufs)]


def b3(ap, n, m):
    return ap.unsqueeze(1).to_broadcast([128, n, m])


def s3(ap, n, m):
    return ap.unsqueeze(2).to_broadcast([128, n, m])


def s4(ap):
    return ap.rearrange("p (b h) -> p b h", b=2).unsqueeze(3).to_broadcast([128, 2, 3, 128])


def v4(ap):
    return ap.rearrange("p (b h) l -> p b h l", b=2)


def w4(ps):
    return ps[:, :, 0:384].rearrange("p b (h l) -> p b h l", h=3)


def softplus12(h, P, es, tagp):
    xa = P.sb(es, tagp + "xa", [128, 12], F32)
    ax = P.sb(es, tagp + "ax", [128, 12], F32)
    ex = P.sb(es, tagp + "ex", [128, 12], F32)
    ln = P.sb(es, tagp + "ln", [128, 12], F32)
    one = P.sb(es, tagp + "one", [128, 1], F32)
    h.memset("pool", one[:], 1.0, [one])

    def f(xin, xin_buf, bias, out):
        h.tt("dve", xa[:], xin, bias[:], ALU.add, [xin_buf, bias], [xa])
        h.act(ax[:], xa[:], AF.Abs, [xa], [ax])
        h.act(ex[:], ax[:], AF.Exp, [ax], [ex], scale=-1.0)
        h.act(ln[:], ex[:], AF.Ln, [ex, one], [ln], bias=one[:, 0:1])
        h.ts("dve", xa[:], xa[:], 0.0, None, ALU.max, None, [xa], [xa])
        h.tt("dve", out[:], xa[:], ln[:], ALU.add, [xa, ln], [out])
    return f


def make_masks(h, P, es):
    m = {}
    ones = P.sb(es, "m_ones", [128, 128], F32)
    h.memset("pool", ones[:], 1.0, [ones])
    m["ones"] = ones
    for name, cmp, cm, st in (("U", ALU.is_ge, -1, 1), ("L", ALU.is_ge, 1, -1), ("Ls", ALU.is_gt, 1, -1)):
        t = P.sb(es, "m_" + name, [128, 128], F32)
        h.select(t[:], ones[:], cmp, 0.0, 0, cm, [[st, 128]], [ones], [t])
        m[name] = t
    sel = P.sb(es, "m_sel", [128, 128], F32)
    zer = P.sb(es, "m_zero", [128, 128], F32)
    h.memset("pool", zer[:], 0.0, [zer])
    h.select(sel[:], zer[:], ALU.not_equal, 1.0, -127, 1, [[0, 128]], [zer], [sel])
    m["sel"] = sel
    bd = P.sb(es, "m_bd", [128, 128], F32)
    h.memset("pool", bd[:], 0.0, [bd])
    h.memset("pool", bd[0:64, 0:64], 1.0, [bd])
    h.memset("pool", bd[64:128, 64:128], 1.0, [bd])
    m["bd"] = bd
    return m


def mixer_layer(k, l, cur, dstt):
    P, T, flags, h = k.P, k.T, k.flags, k.h
    inp = k.inp
    src, rsrc = cur
    dst, rdst = dstt
    NMT = T // 512
    MT = 512
    identf, identb = k.identf, k.identb
    u_d, qn_d, kn_d, v_d, xs_d, B_d, C_d, pt_d, y_d = k.u_d, k.qn_d, k.kn_d, k.v_d, k.xs_d, k.B_d, k.C_d, k.pt_d, k.y_d
    r_pre, r_y = k.r_pre, k.r_y

    with contextlib.ExitStack() as es:
        A, B = k.norm_consts(es, l, inp["norm_mix"].t[l:l + 1, :], 1, 0, "m")
        winf = P.sb(es, "winf", [128, 8, 2304], BF16)
        wint = P.sb(es, "wint", [128, 8, 786], BF16)
        for (c0, c1) in ((0, 1152), (1152, 2304)):
            h.dma("pool", winf[:, :, c0:c1], inp["w_in_f"].t[l, :, c0:c1].rearrange("(k p) n -> p k n", p=128), [], [winf], winf)
        h.dma("pool", wint[:], inp["w_in_t"].t[l].rearrange("(k p) n -> p k n", p=128), [], [wint], wint)
        cwt = P.sb(es, "cwt", [128, 16, 4], F32)
        cbt = P.sb(es, "cbt", [128, 16], F32)
        h.dma("sp", cwt[:], inp["conv_w"].t[l], [], [cwt], cwt)
        h.dma("sp", cbt[:], inp["conv_b"].t[l], [], [cbt], cbt)
        carry = P.sb(es, "carry", [128, 16, 3], F32)
        h.memset("pool", carry[:], 0.0, [carry])
        epsb = P.sb(es, "epsb", [128, 1], F32)
        h.memset("pool", epsb[:], EPS, [epsb])
        mhalf = P.sb(es, "mhalf", [128, MT], F32)
        h.memset("pool", mhalf[:], -0.5, [mhalf])
        bones = P.sb(es, "bones", [128, 128], F32)
        h.memset("pool", bones[:], 0.0, [bones])
        h.memset("pool", bones[0:64, 0:64], 1.0, [bones])
        h.memset("pool", bones[64:128, 64:128], 1.0, [bones])
        xt = [P.sb(es, f"m1x{i}", [128, D], F32) for i in range(2)]
        tmp = P.sb(es, "m1tmp", [128, D], F32)
        hb = P.sb(es, "m1hb", [128, D], BF16)
        hT = P.sb(es, "m1hT", [128, 8, MT], BF16)
        cin = [P.sb(es, f"cin{i}", [128, MT + 3], F32) for i in range(4)]
        acc = [P.sb(es, f"acc{i}", [128, MT], F32) for i in range(4)]
        so = [P.sb(es, f"so{i}", [128, MT], F32) for i in range(4)]
        sob = [P.sb(es, f"sob{i}", [128, MT], BF16) for i in range(4)]
        sq = P.sb(es, "m1sq", [128, MT], F32)
        rinv = P.sb(es, "m1rinv", [128, MT], F32)
        ptst = [P.sb(es, f"ptst{i}", [128, 786], F32) for i in range(2)]
        ssq = P.sb(es, "m1ssq", [128, 1], F32)
        rstd = P.sb(es, "m1rstd", [128, 1], F32)
        ptr = P.ps(es, "m1ptr", [128, 8, 128], BF16)
        pp = [P.ps(es, f"m1pp{i}", [128, MT], F32) for i in range(4)]
        pt = P.ps(es, "m1pt", [128, 2, 512], F32)
        pq = P.ps(es, "m1pq", [128, MT], F32)
        for mt in range(NMT):
            cols = slice(mt * MT, (mt + 1) * MT)
            for ti in range(4):
                t = mt * 4 + ti
                xb = xt[t % 2]
                h.dma("sp", xb[:], src.t[t * 128:(t + 1) * 128, :], [rsrc[t]], [xb], xb)
                k.rms_mod(xb, A, B, hb, ssq, rstd, tmp, tmp)
                h.tr([(ptr[:, kk, :], hb[:, kk * 128:(kk + 1) * 128]) for kk in range(8)], identb[:], [hb, identb], [ptr])
                h.cp("act", hT[:, :, ti * 128:(ti + 1) * 128], ptr[:], [ptr], [hT])
            for c in range(18):
                pb = pp[c % 4]
                h.mm([(pb[:], winf[:, kk, c * 128:(c + 1) * 128], hT[:, kk, :], kk == 0, kk == 7) for kk in range(8)], [winf, hT], [pb])
                if c < 2:
                    sb_ = so[c % 4]
                    h.cp("act", sb_[:], pb[:], [pb], [sb_])
                    h.dma("sp", u_d.t[c * 128:(c + 1) * 128, cols], sb_[:], [sb_], [r_pre[mt]], sb_)
                    continue
                ci = c - 2
                cb_ = cin[ci % 4]
                h.cp("pool", cb_[:, 0:3], carry[:, ci, :], [carry], [cb_])
                h.cp("act", cb_[:, 3:MT + 3], pb[:], [pb], [cb_])
                h.cp("pool", carry[:, ci, :], cb_[:, MT:MT + 3], [cb_], [carry])
                ab = acc[ci % 4]
                h.ts("dve", ab[:], cb_[:, 0:MT], cwt[:, ci, 0:1], None, ALU.mult, None, [cb_, cwt], [ab])
                for j in range(1, 4):
                    h.stt("dve", ab[:], cb_[:, j:j + MT], cwt[:, ci, j:j + 1], ab[:], ALU.mult, ALU.add, [cb_, cwt, ab], [ab])
                sb_ = so[ci % 4]
                h.act(sb_[:], ab[:], AF.Silu, [ab, cbt], [sb_], bias=cbt[:, ci:ci + 1])
                if ci < 6:
                    h.tt("pool", sq[:], sb_[:], sb_[:], ALU.mult, [sb_], [sq])
                    h.mm([(pq[:], bones[:], sq[:], True, True)], [bones, sq], [pq])
                    h.act(rinv[:], pq[:], AF.Ln, [pq, epsb], [rinv], bias=epsb[:, 0:1])
                    h.act(rinv[:], rinv[:], AF.Exp, [rinv], [rinv], scale=-0.5)
                    ob = sob[ci % 4]
                    if ci < 3:
                        h.stt("dve", ob[:], sb_[:], 0.125, rinv[:], ALU.mult, ALU.mult, [sb_, rinv], [ob])
                    else:
                        h.tt("dve", ob[:], sb_[:], rinv[:], ALU.mult, [sb_, rinv], [ob])
                    dd = qn_d if ci < 3 else kn_d
                    j = ci % 3
                    h.dma("sp", dd.t[j * 128:(j + 1) * 128, cols], ob[:], [ob], [r_pre[mt]], ob)
                elif ci < 12:
                    dd = v_d if ci < 9 else xs_d
                    j = (ci - 6) % 3
                    h.dma("sp", dd.t[j * 128:(j + 1) * 128, cols], sb_[:], [sb_], [r_pre[mt]], sb_)
                else:
                    ob = sob[ci % 4]
                    h.cp("pool", ob[:], sb_[:], [sb_], [ob])
                    dd = B_d if ci < 14 else C_d
                    j = (ci - 12) % 2
                    h.dma("sp", dd.t[j * 128:(j + 1) * 128, cols], ob[:], [ob], [r_pre[mt]], ob)
            for ti in range(4):
                t = mt * 4 + ti
                tc = slice(ti * 128, (ti + 1) * 128)
                h.mm([(pt[:, 0, :], hT[:, kk, tc], wint[:, kk, 0:512], kk == 0, kk == 7) for kk in range(8)]
                     + [(pt[:, 1, 0:274], hT[:, kk, tc], wint[:, kk, 512:786], kk == 0, kk == 7) for kk in range(8)],
                     [hT, wint], [pt])
                stg = ptst[t % 2]
                h.cp("act", stg[:, 0:512], pt[:, 0, :], [pt], [stg])
                h.cp("dve", stg[:, 512:786], pt[:, 1, 0:274], [pt], [stg])
                h.dma("sp", pt_d.t[t * 128:(t + 1) * 128, :], stg[:], [stg], [r_pre[mt]], stg)
        P.end_phase()

    if flags.get("s5", True):
        s5_phase(k, l)
    if flags.get("gdn", True):
        gdn_phase(k, l)
    if flags.get("ssd", True):
        ssd_phase(k, l)

    with contextlib.ExitStack() as es:
        wout = P.sb(es, "wout", [128, 8, D], BF16)
        h.dma("pool", wout[:], inp["w_out"].t[l].rearrange("(k p) n -> p k n", p=128), [], [wout], wout)
        G = P.sb(es, "Gm", [128, D], F32)
        k.load_row(G, k.modv.t[l:l + 1, 2 * D:3 * D], [k.r_modv])
        yT = [P.sb(es, f"m5y{i}", [128, 8, MT], BF16) for i in range(2)]
        xt = [P.sb(es, f"m5x{i}", [128, D], F32) for i in range(2)]
        tm = [P.sb(es, f"m5t{i}", [128, D], F32) for i in range(2)]
        po = [P.ps(es, f"m5p{i}", [128, 512], F32) for i in range(4)]
        for mt in range(NMT):
            cols = slice(mt * MT, (mt + 1) * MT)
            yb = yT[mt % 2]
            h.dma("sp", yb[:], y_d.t[:, cols].rearrange("(k p) t -> p k t", p=128), [r_y[0][mt], r_y[1][mt], r_y[2][mt]], [yb], yb)
            if not flags.get("s5", True):
                h.memset("pool", yb[:, 0:2, :], 0.0, [yb])
            if not flags.get("gdn", True):
                h.memset("pool", yb[:, 2:5, :], 0.0, [yb])
            if not flags.get("ssd", True):
                h.memset("pool", yb[:, 5:8, :], 0.0, [yb])
            for ti in range(4):
                t = mt * 4 + ti
                tc = slice(ti * 128, (ti + 1) * 128)
                xb = xt[t % 2]
                tb = tm[t % 2]
                h.dma("sp", xb[:], src.t[t * 128:(t + 1) * 128, :], [rsrc[t]], [xb], xb)
                for n2 in range(2):
                    pb = po[(t % 2) * 2 + n2]
                    nc_ = slice(n2 * 512, (n2 + 1) * 512)
                    h.mm([(pb[:], yb[:, kk, tc], wout[:, kk, nc_], kk == 0, kk == 7) for kk in range(8)], [yb, wout], [pb])
                    h.tt("dve", tb[:, nc_], pb[:], G[:, nc_], ALU.mult, [pb, G], [tb])
                h.tt("pool", tb[:], tb[:], xb[:], ALU.add, [tb, xb], [tb])
                h.dma("sp", dst.t[t * 128:(t + 1) * 128, :], tb[:], [tb], [rdst[t]], tb)
        P.end_phase()


def gate_consts(k, es, l, tag):
    P, h, inp = k.P, k.h, k.inp
    b12 = P.sb(es, tag + "b12", [128, 12], F32)
    na12 = P.sb(es, tag + "na12", [128, 12], F32)
    k.load_row(b12, inp["bias12"].t[l:l + 1, :])
    k.load_row(na12, inp["alog12"].t[l:l + 1, :])
    h.act(na12[:], na12[:], AF.Exp, [na12], [na12])
    h.ts("dve", na12[:], na12[:], -1.0, None, ALU.mult, None, [na12], [na12])
    return b12, na12


def gate_tile(k, sp_fn, pj_ap, pj_buf, b12, na12, masks, sp12, gda, cs12, cl12, pA):
    h = k.h
    sp_fn(pj_ap, pj_buf, b12, sp12)
    h.tt("dve", gda[:], sp12[:], na12[:], ALU.mult, [sp12, na12], [gda])
    h.mm([(pA[:, 0:12], masks["U"][:], gda[:], True, True)], [masks["U"], gda], [pA])
    h.cp("dve", cs12[:], pA[:, 0:12], [pA], [cs12])
    h.mm([(pA[:, 16:28], masks["sel"][:], cs12[:], True, True)], [masks["sel"], cs12], [pA])
    h.cp("dve", cl12[:], pA[:, 16:28], [pA], [cl12])


def ssd_phase(k, l):
    P, T, h, inp = k.P, k.T, k.h, k.inp
    NMT = T // 512
    MT = 512
    identf, identb = k.identf, k.identb
    with contextlib.ExitStack() as es:
        masks = make_masks(h, P, es)
        b12, na12 = gate_consts(k, es, l, "sd")
        sp_fns = [softplus12(h, P, es, f"sd{i}") for i in range(3)]
        epsb = P.sb(es, "sd_eps", [128, 1], F32)
        h.memset("pool", epsb[:], EPS, [epsb])
        dsk = P.sb(es, "sd_dsk", [128, 6], F32)
        k.load_row(dsk, inp["ssd_d"].t[l:l + 1, :])
        nws = P.sb(es, "sd_nws", [128, 384], F32)
        k.load_row(nws, inp["ssd_norm"].t[l:l + 1, :])
        stT = P.sb(es, "sd_stT", [128, 384], F32)
        stTb = P.sb(es, "sd_stTb", [128, 384], BF16)
        h.memset("pool", stT[:], 0.0, [stT])
        h.memset("pool", stTb[:], 0.0, [stTb])
        xsT = [P.sb(es, f"sd_xsT{i}", [128, 3, MT], F32) for i in range(2)]
        BTt = [P.sb(es, f"sd_BT{i}", [128, 2, MT], BF16) for i in range(2)]
        CTt = [P.sb(es, f"sd_CT{i}", [128, 2, MT], BF16) for i in range(2)]
        pj = [P.sb(es, f"sd_pj{i}", [128, 4, 786], F32) for i in range(2)]
        yo = [P.sb(es, f"sd_yo{i}", [128, 3, MT], BF16) for i in range(2)]
        R_sp12 = Rot(P, es, "sd_sp12", [128, 12], F32, 3)
        R_gda = Rot(P, es, "sd_gda", [128, 12], F32, 3)
        R_cs12 = Rot(P, es, "sd_cs12", [128, 12], F32, 3)
        R_cl12 = Rot(P, es, "sd_cl12", [128, 12], F32, 3)
        R_t6 = Rot(P, es, "sd_t6", [128, 6], F32, 3)
        R_din = Rot(P, es, "sd_din", [128, 6], F32, 3)
        R_eacs = Rot(P, es, "sd_eacs", [128, 6], F32, 3)
        R_cd = Rot(P, es, "sd_cd", [128, 6], F32, 3)
        R_dg = Rot(P, es, "sd_dg", [128, 6, 128], F32, 3)
        R_arg = Rot(P, es, "sd_arg", [128, 6, 128], F32, 3)
        R_seg = Rot(P, es, "sd_seg", [128, 6, 128], F32, 3)
        R_WTb = Rot(P, es, "sd_WTb", [128, 6, 128], BF16, 3)
        R_xs_tm = Rot(P, es, "sd_xstm", [128, 384], F32, 3)
        R_xdtf = Rot(P, es, "sd_xdtf", [128, 384], F32, 3)
        R_xdtb = Rot(P, es, "sd_xdtb", [128, 384], BF16, 3)
        R_xddb = Rot(P, es, "sd_xddb", [128, 384], BF16, 3)
        R_Btm = Rot(P, es, "sd_Btm", [128, 256], BF16, 3)
        R_t1 = Rot(P, es, "sd_t1", [128, 384], F32, 3)
        R_t2 = Rot(P, es, "sd_t2", [128, 384], F32, 3)
        R_y = Rot(P, es, "sd_y", [128, 384], F32, 3)
        R_zs = Rot(P, es, "sd_zs", [128, 384], F32, 3)
        R_junk = Rot(P, es, "sd_junk", [128, 192], F32, 3)
        R_yb = Rot(P, es, "sd_yb", [128, 384], BF16, 3)
        R_ss2 = Rot(P, es, "sd_ss2", [128, 2], F32, 3)
        R_rs2 = Rot(P, es, "sd_rs2", [128, 2], F32, 3)
        W0 = P.ps(es, "sd_W0", [128, 2, 512], F32)
        S0 = P.ps(es, "sd_S0", [128, 512], F32)
        S1 = P.ps(es, "sd_S1", [128, 512], F32)
        S2 = P.ps(es, "sd_S2", [128, 512], F32)
        PB = P.ps(es, "sd_PB", [128, 512], BF16)
        pA = P.ps(es, "sd_pA", [128, 32], F32)
        for mt in range(NMT):
            cols = slice(mt * MT, (mt + 1) * MT)
            i2 = mt % 2
            rp = [k.r_pre[mt]]
            h.dma("sp", xsT[i2][:], k.xs_d.t[:, cols].rearrange("(j p) t -> p j t", p=128), rp, [xsT[i2]], xsT[i2])
            h.dma("sp", BTt[i2][:], k.B_d.t[:, cols].rearrange("(j p) t -> p j t", p=128), rp, [BTt[i2]], BTt[i2])
            h.dma("sp", CTt[i2][:], k.C_d.t[:, cols].rearrange("(j p) t -> p j t", p=128), rp, [CTt[i2]], CTt[i2])
            h.dma("sp", pj[i2][:], k.pt_d.t[mt * MT:(mt + 1) * MT, :].rearrange("(a p) n -> p a n", p=128), rp, [pj[i2]], pj[i2])
            xs_, B_, C_, pj_, yo_ = xsT[i2], BTt[i2], CTt[i2], pj[i2], yo[i2]
            for ti in range(4):
                tc = slice(ti * 128, (ti + 1) * 128)
                tix = mt * 4 + ti
                sp12 = R_sp12.at(tix); gda = R_gda.at(tix); cs12 = R_cs12.at(tix); cl12 = R_cl12.at(tix); t6 = R_t6.at(tix); din = R_din.at(tix); eacs = R_eacs.at(tix); cd = R_cd.at(tix); dg = R_dg.at(tix); arg = R_arg.at(tix); seg = R_seg.at(tix); WTb = R_WTb.at(tix); xs_tm = R_xs_tm.at(tix); xdtf = R_xdtf.at(tix); xdtb = R_xdtb.at(tix); xddb = R_xddb.at(tix); Btm = R_Btm.at(tix); t1 = R_t1.at(tix); t2 = R_t2.at(tix); y = R_y.at(tix); zs = R_zs.at(tix); junk = R_junk.at(tix); yb = R_yb.at(tix); ss2 = R_ss2.at(tix); rs2 = R_rs2.at(tix)
                sp_fn = sp_fns[tix % 3]
                gate_tile(k, sp_fn, pj_[:, ti, 768:780], pj_, b12, na12, masks, sp12, gda, cs12, cl12, pA)
                acs = cs12[:, 6:12]
                h.tt("pool", dg[:], b3(identf[:], 6, 128), s3(acs, 6, 128), ALU.mult, [identf, cs12], [dg])
                h.mm([(W0[:, 0, 0:384], masks["ones"][:], dg[:, 0:3, :].rearrange("p a l -> p (a l)"), True, True),
                      (W0[:, 1, 0:384], masks["ones"][:], dg[:, 3:6, :].rearrange("p a l -> p (a l)"), True, True)],
                     [masks["ones"], dg], [W0])
                h.tt("dve", v4(arg[:]), w4(W0), s4(acs), ALU.subtract, [W0, cs12], [arg])
                h.ts("pool", arg[:], arg[:], 0.0, None, ALU.min, None, [arg], [arg])
                h.act(seg[:], arg[:], AF.Exp, [arg], [seg])
                h.tt("pool", seg[:], seg[:], b3(masks["U"][:], 6, 128), ALU.mult, [seg, masks["U"]], [seg])
                h.mm([(S0[:, g * 128:(g + 1) * 128], B_[:, g, tc], C_[:, g, tc], True, True) for g in range(2)], [B_, C_], [S0])
                h.tt("dve", v4(WTb[:]), v4(seg[:]),
                     S0[:, 0:256].rearrange("p (g l) -> p g l", g=2).unsqueeze(2).to_broadcast([128, 2, 3, 128]),
                     ALU.mult, [seg, S0], [WTb])
                h.tr([(S1[:, j * 128:(j + 1) * 128], xs_[:, j, tc]) for j in range(3)], identf[:], [xs_, identf], [S1])
                h.cp("act", xs_tm[:], S1[:, 0:384], [S1], [xs_tm])
                h.tr([(PB[:, g * 128:(g + 1) * 128], B_[:, g, tc]) for g in range(2)], identb[:], [B_, identb], [PB])
                h.cp("act", Btm[:], PB[:, 0:256], [PB], [Btm])
                x3 = lambda ap: ap.rearrange("p (h d) -> p h d", h=6)
                h.tt("dve", x3(xdtf[:]), x3(xs_tm[:]), s3(sp12[:, 6:12], 6, 64), ALU.mult, [xs_tm, sp12], [xdtf])
                h.cp("pool", xdtb[:], xdtf[:], [xdtf], [xdtb])
                h.tt("dve", t6[:], cl12[:, 6:12], acs, ALU.subtract, [cl12, cs12], [t6])
                h.act(din[:], t6[:], AF.Exp, [t6], [din])
                h.tt("pool", x3(xddb[:]), x3(xdtf[:]), s3(din[:], 6, 64), ALU.mult, [xdtf, din], [xddb])
                h.mm([(S2[:, hd * 64:(hd + 1) * 64], WTb[:, hd, :], xdtb[:, hd * 64:(hd + 1) * 64], True, True) for hd in range(6)],
                     [WTb, xdtb], [S2])
                h.mm([(S0[:, g * 192:(g + 1) * 192], C_[:, g, tc], stTb[:, g * 192:(g + 1) * 192], True, True) for g in range(2)],
                     [C_, stTb], [S0])
                h.act(eacs[:], acs, AF.Exp, [cs12], [eacs])
                h.tt("dve", x3(t2[:]), x3(S0[:, 0:384]), s3(eacs[:], 6, 64), ALU.mult, [S0, eacs], [t2])
                h.tt("pool", x3(t1[:]), x3(xs_tm[:]), s3(dsk[:], 6, 64), ALU.mult, [xs_tm, dsk], [t1])
                h.tt("pool", t2[:], t2[:], t1[:], ALU.add, [t2, t1], [t2])
                h.tt("dve", y[:], S2[:, 0:384], t2[:], ALU.add, [S2, t2], [y])
                h.act(zs[:], pj_[:, ti, 384:768], AF.Silu, [pj_], [zs])
                h.tt("pool", y[:], y[:], zs[:], ALU.mult, [y, zs], [y])
                for g in range(2):
                    h.act(junk[:], y[:, g * 192:(g + 1) * 192], AF.Square, [y], [junk, ss2], accum=ss2[:, g:g + 1])
                h.act(rs2[:], ss2[:], AF.Ln, [ss2, epsb], [rs2], bias=epsb[:, 0:1], scale=1.0 / 192)
                h.act(rs2[:], rs2[:], AF.Exp, [rs2], [rs2], scale=-0.5)
                y3 = lambda ap: ap.rearrange("p (g c) -> p g c", g=2)
                h.tt("dve", y3(y[:]), y3(y[:]), s3(rs2[:], 2, 192), ALU.mult, [y, rs2], [y])
                h.tt("pool", yb[:], y[:], nws[:], ALU.mult, [y, nws], [yb])
                h.tr([(PB[:, j * 128:(j + 1) * 128], yb[:, j * 128:(j + 1) * 128]) for j in range(3)], identb[:], [yb, identb], [PB])
                h.cp("act", yo_[:, :, tc], PB[:, 0:384].rearrange("p (j t) -> p j t", j=3), [PB], [yo_])
                h.mm([(S1[:, g * 192:(g + 1) * 192], Btm[:, g * 128:(g + 1) * 128], xddb[:, g * 192:(g + 1) * 192], True, True) for g in range(2)],
                     [Btm, xddb], [S1])
                h.act(cd[:], cl12[:, 6:12], AF.Exp, [cl12], [cd])
                h.tt("pool", x3(stT[:]), x3(stT[:]), s3(cd[:], 6, 64), ALU.mult, [stT, cd], [stT])
                h.tt("dve", stT[:], stT[:], S1[:, 0:384], ALU.add, [stT, S1], [stT])
                h.cp("act", stTb[:], stT[:], [stT], [stTb])
            h.dma("sp", k.y_d.t[640:1024, cols].rearrange("(j p) t -> p j t", p=128), yo_[:], [yo_], [k.r_y[2][mt]], yo_)
        P.end_phase()


C1_2PI = 6.28125
C2_2PI = 2.0 * math.pi - 6.28125


def sincos(h, eng, x, out, b, ki, c, negpi, R, W, bufs, is_cos):
    bb, kb, cb_ = bufs
    off = 16.5 + (0.25 if is_cos else 0.0)
    add = 33.0 * math.pi + (0.5 * math.pi if is_cos else 0.0)
    h.ts(eng, b, x, 1.0 / (2.0 * math.pi), off, ALU.mult, ALU.add, R, [bb])
    h.cp(eng, ki, b, [bb], [kb])
    h.cp(eng, c, ki, [kb], [cb_])
    h.stt(eng, b, c, -C1_2PI, x, ALU.mult, ALU.add, R + [cb_], [bb])
    h.stt(eng, b, c, -C2_2PI, b, ALU.mult, ALU.add, [cb_, bb], [bb])
    h.ts(eng, b, b, add, None, ALU.add, None, [bb], [bb])
    h.ts(eng, c, b, 2.0 * math.pi, -2.0 * math.pi, ALU.is_gt, ALU.mult, [bb], [cb_])
    h.tt(eng, b, b, c, ALU.add, [bb, cb_], [bb])
    h.ts(eng, c, b, 0.0, 2.0 * math.pi, ALU.is_lt, ALU.mult, [bb], [cb_])
    h.tt(eng, b, b, c, ALU.add, [bb, cb_], [bb])
    h.act(out, b, AF.Sin, [bb, negpi], W, bias=negpi[:, 0:1])


def s5_phase(k, l):
    P, T, h, inp = k.P, k.T, k.h, k.inp
    NMT = T // 512
    SEG = 512
    with contextlib.ExitStack() as es:
        I32 = mybir.dt.int32
        are = P.sb(es, "s5are", [128, 8], F32)
        aim = P.sb(es, "s5aim", [128, 8], F32)
        stp = P.sb(es, "s5stp", [128, 8], F32)
        h.dma("sp", are[:], inp["s5_are"].t[l], [], [are], are)
        h.dma("sp", aim[:], inp["s5_aim"].t[l], [], [aim], aim)
        h.dma("sp", stp[:], inp["s5_ldt"].t[l], [], [stp], stp)
        dsk = P.sb(es, "s5dsk", [128, 2], F32)
        nw5 = P.sb(es, "s5nw", [128, 2], F32)
        h.dma("sp", dsk[:], inp["s5_dcol"].t[l], [], [dsk], dsk)
        h.dma("sp", nw5[:], inp["s5_ncol"].t[l], [], [nw5], nw5)
        bTre = P.sb(es, "s5bTre", [128, 8, 128], BF16)
        bTim = P.sb(es, "s5bTim", [128, 8, 128], BF16)
        cTre = P.sb(es, "s5cTre", [128, 8, 128], BF16)
        cTim = P.sb(es, "s5cTim", [128, 8, 128], BF16)
        for dstb, nm in ((bTre, "s5_bT_re"), (bTim, "s5_bT_im"), (cTre, "s5_cT_re"), (cTim, "s5_cT_im")):
            h.dma("pool", dstb[:], inp[nm].t[l].rearrange("s r m -> r s m"), [], [dstb], dstb)
        wglu = P.sb(es, "s5wglu", [128, 2, 256], BF16)
        h.dma("pool", wglu[:], inp["s5_w_glu"].t[l].rearrange("(k p) n -> p k n", p=128), [], [wglu], wglu)
        negpi = P.sb(es, "s5negpi", [128, 1], F32)
        h.memset("pool", negpi[:], -math.pi, [negpi])
        epsb = P.sb(es, "s5eps", [128, 1], F32)
        h.memset("pool", epsb[:], EPS, [epsb])
        onesf = P.sb(es, "s5ones", [128, 128], F32)
        h.memset("pool", onesf[:], 1.0, [onesf])
        jrow = P.sb(es, "s5jrow", [128, SEG], F32)
        P.op("pool", lambda e: e.iota(jrow[:], pattern=[[1, SEG]], base=0, channel_multiplier=0,
                                      allow_small_or_imprecise_dtypes=True), writes=[jrow])
        th = P.sb(es, "s5th", [128, 8], F32)
        rr = P.sb(es, "s5r", [128, 8], F32)
        sth = P.sb(es, "s5sth", [128, 8], F32)
        cth = P.sb(es, "s5cth", [128, 8], F32)
        thS = P.sb(es, "s5thS", [128, 8], F32)
        sS = P.sb(es, "s5sS", [128, 8], F32)
        cS = P.sb(es, "s5cS", [128, 8], F32)
        nsS = P.sb(es, "s5nsS", [128, 8], F32)
        cr = P.sb(es, "s5cr", [128, 8], F32)
        ci = P.sb(es, "s5ci", [128, 8], F32)
        ncr = P.sb(es, "s5ncr", [128, 8], F32)
        q1 = P.sb(es, "s5q1", [128, 8], F32)
        q2 = P.sb(es, "s5q2", [128, 8], F32)
        q3 = P.sb(es, "s5q3", [128, 8], F32)
        sb8 = P.sb(es, "s5sb8", [128, 8], F32)
        si8 = P.sb(es, "s5si8", [128, 8], I32)
        sc8 = P.sb(es, "s5sc8", [128, 8], F32)
        h.act(stp[:], stp[:], AF.Exp, [stp], [stp])
        h.tt("dve", th[:], aim[:], stp[:], ALU.mult, [aim, stp], [th])
        h.tt("dve", rr[:], are[:], stp[:], ALU.mult, [are, stp], [rr])
        h.act(rr[:], rr[:], AF.Exp, [rr], [rr])
        sm = (sb8, si8, sc8)
        sincos(h, "dve", th[:], sth[:], sb8[:], si8[:], sc8[:], negpi, [th], [sth], sm, False)
        sincos(h, "dve", th[:], cth[:], sb8[:], si8[:], sc8[:], negpi, [th], [cth], sm, True)
        h.ts("dve", thS[:], th[:], float(SEG), None, ALU.mult, None, [th], [thS])
        sincos(h, "dve", thS[:], sS[:], sb8[:], si8[:], sc8[:], negpi, [thS], [sS], sm, False)
        sincos(h, "dve", thS[:], cS[:], sb8[:], si8[:], sc8[:], negpi, [thS], [cS], sm, True)
        h.ts("dve", nsS[:], sS[:], -1.0, None, ALU.mult, None, [sS], [nsS])
        h.tt("dve", q1[:], rr[:], cth[:], ALU.mult, [rr, cth], [q1])
        h.ts("dve", q1[:], q1[:], -1.0, None, ALU.add, None, [q1], [q1])
        h.tt("dve", q2[:], rr[:], sth[:], ALU.mult, [rr, sth], [q2])
        h.tt("dve", q3[:], are[:], are[:], ALU.mult, [are], [q3])
        h.tt("dve", sc8[:], aim[:], aim[:], ALU.mult, [aim], [sc8])
        h.tt("dve", q3[:], q3[:], sc8[:], ALU.add, [q3, sc8], [q3])
        h.recip(q3[:], q3[:], [q3], [q3])
        h.tt("dve", cr[:], q1[:], are[:], ALU.mult, [q1, are], [cr])
        h.tt("dve", sc8[:], q2[:], aim[:], ALU.mult, [q2, aim], [sc8])
        h.tt("dve", cr[:], cr[:], sc8[:], ALU.add, [cr, sc8], [cr])
        h.tt("dve", cr[:], cr[:], q3[:], ALU.mult, [cr, q3], [cr])
        h.tt("dve", ci[:], q2[:], are[:], ALU.mult, [q2, are], [ci])
        h.tt("dve", sc8[:], q1[:], aim[:], ALU.mult, [q1, aim], [sc8])
        h.tt("dve", ci[:], ci[:], sc8[:], ALU.subtract, [ci, sc8], [ci])
        h.tt("dve", ci[:], ci[:], q3[:], ALU.mult, [ci, q3], [ci])
        h.ts("dve", ncr[:], cr[:], -1.0, None, ALU.mult, None, [cr], [ncr])
        cosT = P.sb(es, "s5cosT", [128, 8, SEG], F32)
        sinT = P.sb(es, "s5sinT", [128, 8, SEG], F32)
        tabr = P.sb(es, "s5tabr", [128, 8, SEG], F32)
        tabi = P.sb(es, "s5tabi", [128, 8, SEG], F32)
        ang = [P.sb(es, f"s5ang{i}", [128, SEG], F32) for i in range(2)]
        tb = [P.sb(es, f"s5tb{i}", [128, SEG], F32) for i in range(2)]
        tki = [P.sb(es, f"s5tki{i}", [128, SEG], I32) for i in range(2)]
        tcc = [P.sb(es, f"s5tc{i}", [128, SEG], F32) for i in range(2)]
        for sc in range(8):
            i = sc % 2
            eng = "dve" if i == 0 else "pool"
            h.ts(eng, ang[i][:], jrow[:], th[:, sc:sc + 1], None, ALU.mult, None, [jrow, th], [ang[i]])
            bufs = (tb[i], tki[i], tcc[i])
            sincos(h, eng, ang[i][:], sinT[:, sc, :], tb[i][:], tki[i][:], tcc[i][:], negpi, [ang[i]], [sinT], bufs, False)
            sincos(h, eng, ang[i][:], cosT[:, sc, :], tb[i][:], tki[i][:], tcc[i][:], negpi, [ang[i]], [cosT], bufs, True)
            h.ts(eng, tabr[:, sc, :], cosT[:, sc, :], cr[:, sc:sc + 1], None, ALU.mult, None, [cosT, cr], [tabr])
            h.stt(eng, tabr[:, sc, :], sinT[:, sc, :], ci[:, sc:sc + 1], tabr[:, sc, :], ALU.mult, ALU.add, [sinT, ci, tabr], [tabr])
            h.ts(eng, tabi[:, sc, :], cosT[:, sc, :], ci[:, sc:sc + 1], None, ALU.mult, None, [cosT, ci], [tabi])
            h.stt(eng, tabi[:, sc, :], sinT[:, sc, :], ncr[:, sc:sc + 1], tabi[:, sc, :], ALU.mult, ALU.add, [sinT, ncr, tabi], [tabi])
        ire = P.sb(es, "s5ire", [128, 8], F32)
        iim = P.sb(es, "s5iim", [128, 8], F32)
        gre_e = P.sb(es, "s5gree", [128, 8], F32)
        gim_e = P.sb(es, "s5gime", [128, 8], F32)
        h.memset("pool", ire[:], 0.0, [ire])
        h.memset("pool", iim[:], 0.0, [iim])
        uTf = [P.sb(es, f"s5uTf{i}", [128, 2, SEG], F32) for i in range(2)]
        uTb = [P.sb(es, f"s5uTb{i}", [128, 2, SEG], BF16) for i in range(2)]
        m1 = [P.sb(es, f"s5m1{i}", [128, SEG], F32) for i in range(3)]
        m2 = [P.sb(es, f"s5m2{i}", [128, SEG], F32) for i in range(3)]
        m3 = [P.sb(es, f"s5m3{i}", [128, SEG], F32) for i in range(3)]
        m4 = [P.sb(es, f"s5m4{i}", [128, SEG], F32) for i in range(3)]
        p1 = [P.sb(es, f"s5p1{i}", [128, SEG], BF16) for i in range(3)]
        p2 = [P.sb(es, f"s5p2{i}", [128, SEG], BF16) for i in range(3)]
        p3 = [P.sb(es, f"s5p3{i}", [128, SEG], BF16) for i in range(3)]
        p4 = [P.sb(es, f"s5p4{i}", [128, SEG], BF16) for i in range(3)]
        ncTre = P.sb(es, "s5ncTre", [128, 8, 128], BF16)
        ncTim = P.sb(es, "s5ncTim", [128, 8, 128], BF16)
        h.ts("pool", ncTre[:], cTre[:], -1.0, None, ALU.mult, None, [cTre], [ncTre])
        h.ts("pool", ncTim[:], cTim[:], -1.0, None, ALU.mult, None, [cTim], [ncTim])
        dre = [P.sb(es, f"s5dre{i}", [128, SEG], F32) for i in range(3)]
        dim = [P.sb(es, f"s5dim{i}", [128, SEG], F32) for i in range(3)]
        gre = [P.sb(es, f"s5gre{i}", [128, SEG], F32) for i in range(3)]
        gim = [P.sb(es, f"s5gim{i}", [128, SEG], F32) for i in range(3)]
        hre = [P.sb(es, f"s5hre{i}", [128, SEG], BF16) for i in range(2)]
        him = [P.sb(es, f"s5him{i}", [128, SEG], BF16) for i in range(2)]
        y1 = P.sb(es, "s5y1", [128, 2, SEG], F32)
        yt = P.sb(es, "s5yt", [128, 2, SEG], F32)
        yg = P.sb(es, "s5yg", [128, 2, SEG], F32)
        ygb = P.sb(es, "s5ygb", [128, 2, SEG], BF16)
        sg = P.sb(es, "s5sg", [128, 2, SEG], F32)
        y2 = P.sb(es, "s5y2", [128, 2, SEG], F32)
        rstd = P.sb(es, "s5rstd", [128, SEG], F32)
        yo = [P.sb(es, f"s5yo{i}", [128, 2, SEG], BF16) for i in range(2)]
        Pre = [P.ps(es, f"s5Pre{i}", [128, SEG], F32) for i in range(2)]
        Pim = [P.ps(es, f"s5Pim{i}", [128, SEG], F32) for i in range(2)]
        Y = [P.ps(es, f"s5Y{i}", [128, SEG], F32) for i in range(2)]
        Pg = P.ps(es, "s5Pg", [128, SEG], F32)
        Pt = P.ps(es, "s5Pt", [128, SEG], F32)
        GK = 2.0 * math.sqrt(2.0 / math.pi)
        for mt in range(NMT):
            cols = slice(mt * SEG, (mt + 1) * SEG)
            i2 = mt % 2
            uf, ub, yo_ = uTf[i2], uTb[i2], yo[i2]
            h.dma("sp", uf[:], k.u_d.t[:, cols].rearrange("(j p) t -> p j t", p=128), [k.r_pre[mt]], [uf], uf)
            h.cp("pool", ub[:], uf[:], [uf], [ub])
            for sc in range(8):
                i = sc % 2
                i3 = sc % 3
                cc = sc // 4
                h.mm([(Pre[i][:], bTre[:, sc, :], ub[:, cc, :], True, True)], [bTre, ub], [Pre[i]])
                h.mm([(Pim[i][:], bTim[:, sc, :], ub[:, cc, :], True, True)], [bTim, ub], [Pim[i]])
                h.tt("dve", m1[i3][:], Pre[i][:], tabr[:, sc, :], ALU.mult, [Pre[i], tabr], [m1[i3]])
                h.tt("dve", m2[i3][:], Pim[i][:], tabi[:, sc, :], ALU.mult, [Pim[i], tabi], [m2[i3]])
                h.tt("pool", dre[i3][:], m1[i3][:], m2[i3][:], ALU.subtract, [m1[i3], m2[i3]], [dre[i3]])
                h.tt("dve", m3[i3][:], Pre[i][:], tabi[:, sc, :], ALU.mult, [Pre[i], tabi], [m3[i3]])
                h.tt("dve", m4[i3][:], Pim[i][:], tabr[:, sc, :], ALU.mult, [Pim[i], tabr], [m4[i3]])
                h.tt("pool", dim[i3][:], m3[i3][:], m4[i3][:], ALU.add, [m3[i3], m4[i3]], [dim[i3]])
                for (go, di, ini) in ((gre[i3], dre[i3], ire), (gim[i3], dim[i3], iim)):
                    P.op("dve", (lambda go, di, ini, sc: (lambda e: e.tensor_tensor_scan(
                        out=go[:], data0=rr[:, sc:sc + 1].to_broadcast([128, SEG]), data1=di[:],
                        initial=ini[:, sc:sc + 1], op0=ALU.mult, op1=ALU.add)))(go, di, ini, sc),
                        reads=[rr, di, ini], writes=[go])
                h.cp("act", gre_e[:, sc:sc + 1], gre[i3][:, SEG - 1:SEG], [gre[i3]], [gre_e])
                h.cp("act", gim_e[:, sc:sc + 1], gim[i3][:, SEG - 1:SEG], [gim[i3]], [gim_e])
                h.tt("pool", p1[i3][:], gre[i3][:], cosT[:, sc, :], ALU.mult, [gre[i3], cosT], [p1[i3]])
                h.tt("pool", p2[i3][:], gim[i3][:], sinT[:, sc, :], ALU.mult, [gim[i3], sinT], [p2[i3]])
                h.tt("dve", p3[i3][:], gre[i3][:], sinT[:, sc, :], ALU.mult, [gre[i3], sinT], [p3[i3]])
                h.tt("dve", p4[i3][:], gim[i3][:], cosT[:, sc, :], ALU.mult, [gim[i3], cosT], [p4[i3]])
                h.mm([(Y[cc][:], cTre[:, sc, :], p1[i3][:], sc % 4 == 0, False),
                      (Y[cc][:], ncTre[:, sc, :], p2[i3][:], False, False),
                      (Y[cc][:], ncTim[:, sc, :], p3[i3][:], False, False),
                      (Y[cc][:], ncTim[:, sc, :], p4[i3][:], False, sc % 4 == 3)],
                     [cTre, ncTre, ncTim, p1[i3], p2[i3], p3[i3], p4[i3]], [Y[cc]])
                if sc % 4 == 3:
                    h.stt("dve", y1[:, cc, :], uf[:, cc, :], dsk[:, cc:cc + 1], Y[cc][:], ALU.mult, ALU.add, [uf, dsk, Y[cc]], [y1])
                    h.tt("pool", yt[:, cc, :], y1[:, cc, :], y1[:, cc, :], ALU.mult, [y1], [yt])
                    h.ts("pool", yt[:, cc, :], yt[:, cc, :], 0.044715, 1.0, ALU.mult, ALU.add, [yt], [yt])
                    h.tt("pool", yt[:, cc, :], yt[:, cc, :], y1[:, cc, :], ALU.mult, [yt, y1], [yt])
                    h.act(yt[:, cc, :], yt[:, cc, :], AF.Sigmoid, [yt], [yt], scale=GK)
                    h.tt("pool", yg[:, cc, :], y1[:, cc, :], yt[:, cc, :], ALU.mult, [y1, yt], [yg])
                    h.cp("pool", ygb[:, cc, :], yg[:, cc, :], [yg], [ygb])
            h.tt("dve", q1[:], gre_e[:], cS[:], ALU.mult, [gre_e, cS], [q1])
            h.tt("dve", q2[:], gim_e[:], nsS[:], ALU.mult, [gim_e, nsS], [q2])
            h.tt("dve", ire[:], q1[:], q2[:], ALU.add, [q1, q2], [ire])
            h.tt("dve", q1[:], gre_e[:], sS[:], ALU.mult, [gre_e, sS], [q1])
            h.tt("dve", q2[:], gim_e[:], cS[:], ALU.mult, [gim_e, cS], [q2])
            h.tt("dve", iim[:], q1[:], q2[:], ALU.add, [q1, q2], [iim])
            for oc in range(2):
                h.mm([(Pg[:], wglu[:, kc, oc * 128:(oc + 1) * 128], ygb[:, kc, :], kc == 0, kc == 1) for kc in range(2)], [wglu, ygb], [Pg])
                h.act(sg[:, oc, :], Pg[:], AF.Sigmoid, [Pg], [sg])
                h.tt("pool", y2[:, oc, :], yg[:, oc, :], sg[:, oc, :], ALU.mult, [yg, sg], [y2])
                h.tt("pool", sg[:, oc, :], y2[:, oc, :], y2[:, oc, :], ALU.mult, [y2], [sg])
            h.mm([(Pt[:], onesf[:], sg[:, oc, :], oc == 0, oc == 1) for oc in range(2)], [onesf, sg], [Pt])
            h.act(rstd[:], Pt[:], AF.Sqrt, [Pt, epsb], [rstd], bias=epsb[:, 0:1], scale=1.0 / 256)
            h.recip(rstd[:], rstd[:], [rstd], [rstd])
            for oc in range(2):
                h.stt("dve", yo_[:, oc, :], y2[:, oc, :], nw5[:, oc:oc + 1], rstd[:], ALU.mult, ALU.mult, [y2, nw5, rstd], [yo_])
            h.dma("sp", k.y_d.t[0:256, cols].rearrange("(j p) t -> p j t", p=128), yo_[:], [yo_], [k.r_y[0][mt]], yo_)
        P.end_phase()


def gdn_phase(k, l):
    P, T, h, inp = k.P, k.T, k.h, k.inp
    NMT = T // 512
    MT = 512
    identf, identb = k.identf, k.identb
    with contextlib.ExitStack() as es:
        masks = make_masks(h, P, es)
        b12, na12 = gate_consts(k, es, l, "gd")
        gnw = P.sb(es, "gd_gnw", [128, 64], F32)
        k.load_row(gnw, inp["gdn_norm"].t[l:l + 1, :])
        Sf = P.sb(es, "gd_Sf", [128, 3, 128], F32)
        Sb = P.sb(es, "gd_Sb", [128, 3, 128], BF16)
        h.memset("pool", Sf[:], 0.0, [Sf])
        h.memset("pool", Sb[:], 0.0, [Sb])
        qnT = [P.sb(es, f"gd_qn{i}", [128, 3, MT], BF16) for i in range(2)]
        knT = [P.sb(es, f"gd_kn{i}", [128, 3, MT], BF16) for i in range(2)]
        vT = [P.sb(es, f"gd_vT{i}", [128, 3, MT], F32) for i in range(2)]
        pj = [P.sb(es, f"gd_pj{i}", [128, 4, 786], F32) for i in range(2)]
        yo = [P.sb(es, f"gd_yo{i}", [128, 3, MT], BF16) for i in range(2)]
        R_sp12 = Rot(P, es, "gd_sp12", [128, 12], F32)
        R_gda = Rot(P, es, "gd_gda", [128, 12], F32)
        R_cs12 = Rot(P, es, "gd_cs12", [128, 12], F32)
        R_cl12 = Rot(P, es, "gd_cl12", [128, 12], F32)
        R_beta = Rot(P, es, "gd_beta", [128, 6], F32)
        R_nbeta = Rot(P, es, "gd_nbeta", [128, 6], F32)
        R_egc = Rot(P, es, "gd_egc", [128, 6], F32)
        R_t6 = Rot(P, es, "gd_t6", [128, 6], F32)
        R_dkk = Rot(P, es, "gd_dkk", [128, 6], F32)
        R_gtot = Rot(P, es, "gd_gtot", [128, 6], F32)
        R_gtc = Rot(P, es, "gd_gtc", [128, 3], F32)
        R_dg = Rot(P, es, "gd_dg", [128, 6, 128], F32)
        R_arg = Rot(P, es, "gd_arg", [128, 6, 128], F32)
        R_E = Rot(P, es, "gd_E", [128, 6, 128], F32)
        R_EU = Rot(P, es, "gd_EU", [128, 6, 128], F32)
        R_ELn = Rot(P, es, "gd_ELn", [128, 6, 128], F32)
        R_attnT = Rot(P, es, "gd_attnT", [128, 6, 128], BF16)
        R_Pm = [Rot(P, es, f"gd_Pm{i}", [128, 6, 128], BF16) for i in range(2)]
        R_Qm = [Rot(P, es, f"gd_Qm{i}", [128, 6, 128], BF16) for i in range(2)]
        sp_fns = [softplus12(h, P, es, f"gd{i}") for i in range(2)]
        R_Xb = Rot(P, es, "gd_Xb", [128, 6, 128], BF16)
        R_kdec = Rot(P, es, "gd_kdec", [128, 384], BF16)
        R_v_tm = Rot(P, es, "gd_vtm", [128, 384], F32)
        R_rr_ = Rot(P, es, "gd_rr", [128, 384], F32)
        R_rb = Rot(P, es, "gd_rb", [128, 384], BF16)
        R_vnb = Rot(P, es, "gd_vnb", [128, 384], BF16)
        R_oa = Rot(P, es, "gd_oa", [128, 384], F32)
        R_o = Rot(P, es, "gd_o", [128, 384], F32)
        R_sq = Rot(P, es, "gd_sq", [128, 384], F32)
        R_ss6 = Rot(P, es, "gd_ss6", [128, 6], F32)
        R_rs6 = Rot(P, es, "gd_rs6", [128, 6], F32)
        R_zg = Rot(P, es, "gd_zg", [128, 384], F32)
        R_yb = Rot(P, es, "gd_yb", [128, 384], BF16)
        R_tmpS = Rot(P, es, "gd_tmpS", [128, 3, 128], F32)
        W0 = P.ps(es, "gd_W0", [128, 2, 512], F32)
        W1 = P.ps(es, "gd_W1", [128, 2, 512], F32)
        W2 = P.ps(es, "gd_W2", [128, 2, 512], F32)
        W1a, W1b, W2a, W2b = Buf(None, "W1a"), Buf(None, "W1b"), Buf(None, "W2a"), Buf(None, "W2b")
        W1h, W2h = (W1a, W1b), (W2a, W2b)
        M1bh_s = [(Buf(None, f"m1a{i}"), Buf(None, f"m1b{i}")) for i in range(2)]
        M1pbh_s = [(Buf(None, f"m1pa{i}"), Buf(None, f"m1pb{i}")) for i in range(2)]
        PB = P.ps(es, "gd_PB", [128, 1024], BF16)
        pA = P.ps(es, "gd_pA", [128, 32], F32)
        x3 = lambda ap: ap.rearrange("p (h d) -> p h d", h=6)
        kz = [P.sb(es, f"gd_kz{i}", [128, 3, MT], BF16) for i in range(2)]
        R_Tb = Rot(P, es, "gd_Tb", [128, 6, 128], BF16)
        R_Tb2 = Rot(P, es, "gd_Tb2", [128, 6, 128], BF16)
        R_Xb2 = Rot(P, es, "gd_Xb2", [128, 6, 128], BF16)
        epsb = P.sb(es, "gd_eps", [128, 1], F32)
        h.memset("pool", epsb[:], EPS, [epsb])
        R_ez = Rot(P, es, "gd_ez", [128, 384], F32)
        R_M1b = Rot(P, es, "gd_M1b", [128, 6, 128], BF16)
        R_M1pb = Rot(P, es, "gd_M1pb", [128, 6, 128], BF16)
        cmask = P.sb(es, "gd_cmask", [128, 14, 128], BF16)
        h.dma("pool", cmask[:], inp["gdn_cmask"].t.rearrange("m p j -> p m j"), [], [cmask], cmask)
        cmask6 = P.sb(es, "gd_cmask6", [128, 14, 6, 128], BF16)
        h.cp("pool", cmask6[:], cmask[:].unsqueeze(2).to_broadcast([128, 14, 6, 128]), [cmask], [cmask6])
        rmask = P.sb(es, "gd_rmask", [128, 2], F32)
        h.memset("pool", rmask[:], 0.0, [rmask])
        h.memset("pool", rmask[0:64, 0:1], 1.0, [rmask])
        h.memset("pool", rmask[64:128, 1:2], 1.0, [rmask])

        def headmm(Wps, lh, rh, R, Wh):
            w = w4(Wps)
            h.mm([(w[:, hd // 3, hd % 3, :], lh[:, hd, :], rh[:, hd, :], True, True) for hd in range(6)], R, list(Wh))

        stop = k.flags.get("gdn_stop", 99)
        for mt in range(NMT):
            cols = slice(mt * MT, (mt + 1) * MT)
            i2 = mt % 2
            rp = [k.r_pre[mt]]
            h.dma("sp", qnT[i2][:], k.qn_d.t[:, cols].rearrange("(j p) t -> p j t", p=128), rp, [qnT[i2]], qnT[i2])
            h.dma("sp", knT[i2][:], k.kn_d.t[:, cols].rearrange("(j p) t -> p j t", p=128), rp, [knT[i2]], knT[i2])
            h.dma("sp", vT[i2][:], k.v_d.t[:, cols].rearrange("(j p) t -> p j t", p=128), rp, [vT[i2]], vT[i2])
            h.dma("sp", pj[i2][:], k.pt_d.t[mt * MT:(mt + 1) * MT, :].rearrange("(a p) n -> p a n", p=128), rp, [pj[i2]], pj[i2])
            qn_, kn_, v_, pj_, yo_ = qnT[i2], knT[i2], vT[i2], pj[i2], yo[i2]
            for s_ in range(2):
                h.ts("dve", kz[s_][:], kn_[:], rmask[:, s_:s_ + 1], None, ALU.mult, None, [kn_, rmask], [kz[s_]])
            for ti in range(4):
                tc = slice(ti * 128, (ti + 1) * 128)
                tix = mt * 4 + ti
                sp12 = R_sp12.at(tix); gda = R_gda.at(tix); cs12 = R_cs12.at(tix); cl12 = R_cl12.at(tix); beta = R_beta.at(tix); nbeta = R_nbeta.at(tix); egc = R_egc.at(tix); t6 = R_t6.at(tix); dkk = R_dkk.at(tix); gtot = R_gtot.at(tix); gtc = R_gtc.at(tix); dg = R_dg.at(tix); arg = R_arg.at(tix); E = R_E.at(tix); EU = R_EU.at(tix); ELn = R_ELn.at(tix); attnT = R_attnT.at(tix); Xb = R_Xb.at(tix); kdec = R_kdec.at(tix); v_tm = R_v_tm.at(tix); rr_ = R_rr_.at(tix); rb = R_rb.at(tix); vnb = R_vnb.at(tix); oa = R_oa.at(tix); o = R_o.at(tix); sq = R_sq.at(tix); ss6 = R_ss6.at(tix); rs6 = R_rs6.at(tix); zg = R_zg.at(tix); yb = R_yb.at(tix); tmpS = R_tmpS.at(tix); Tb = R_Tb.at(tix); M1b = R_M1b.at(tix); M1pb = R_M1pb.at(tix)
                Pm = [r.at(tix) for r in R_Pm]; Qm = [r.at(tix) for r in R_Qm]; sp_fn = sp_fns[tix % 2]; M1bh = M1bh_s[tix % 2]; M1pbh = M1pbh_s[tix % 2]; Tb2 = R_Tb2.at(tix); Xb2 = R_Xb2.at(tix); ez = R_ez.at(tix)
                gate_tile(k, sp_fn, pj_[:, ti, 768:780], pj_, b12, na12, masks, sp12, gda, cs12, cl12, pA)
                gc = cs12[:, 0:6]
                h.act(beta[:], pj_[:, ti, 780:786], AF.Exp, [pj_], [beta], scale=-1.0)
                h.ts("dve", beta[:], beta[:], 1.0, None, ALU.add, None, [beta], [beta])
                h.recip(beta[:], beta[:], [beta], [beta])
                h.ts("dve", nbeta[:], beta[:], -1.0, None, ALU.mult, None, [beta], [nbeta])
                h.act(egc[:], gc, AF.Exp, [cs12], [egc])
                h.tt("dve", t6[:], cl12[:, 0:6], gc, ALU.subtract, [cl12, cs12], [t6])
                h.act(dkk[:], t6[:], AF.Exp, [t6], [dkk])
                h.act(gtot[:], cl12[:, 0:6], AF.Exp, [cl12], [gtot])
                g2 = gtot[:].rearrange("p (j s) -> p j s", s=2)
                h.cp("dve", gtc[0:64, :], g2[0:64, :, 0], [gtot], [gtc])
                h.cp("dve", gtc[64:128, :], g2[64:128, :, 1], [gtot], [gtc])
                if stop < 1:
                    h.memset("pool", yo_[:, :, tc], 0.0, [yo_])
                    continue
                h.tt("pool", dg[:], b3(identf[:], 6, 128), s3(gc, 6, 128), ALU.mult, [identf, cs12], [dg])
                h.mm([(W0[:, 0, 0:384], masks["ones"][:], dg[:, 0:3, :].rearrange("p a l -> p (a l)"), True, True),
                      (W0[:, 1, 0:384], masks["ones"][:], dg[:, 3:6, :].rearrange("p a l -> p (a l)"), True, True)],
                     [masks["ones"], dg], [W0])
                if stop < 1.2:
                    h.memset("pool", yo_[:, :, tc], 0.0, [yo_])
                    continue
                h.tt("dve", v4(arg[:]), w4(W0), s4(gc), ALU.subtract, [W0, cs12], [arg])
                if stop < 1.4:
                    h.memset("pool", yo_[:, :, tc], 0.0, [yo_])
                    continue
                h.act(arg[:], arg[:], AF.Abs, [arg], [arg])
                h.act(E[:], arg[:], AF.Exp, [arg], [E], scale=-1.0)
                if stop < 1.6:
                    h.memset("pool", yo_[:, :, tc], 0.0, [yo_])
                    continue
                h.tt("pool", EU[:], E[:], b3(masks["U"][:], 6, 128), ALU.mult, [E, masks["U"]], [EU])
                if stop < 1.8:
                    h.memset("pool", yo_[:, :, tc], 0.0, [yo_])
                    continue
                h.tt("pool", ELn[:], E[:], b3(masks["Ls"][:], 6, 128), ALU.mult, [E, masks["Ls"]], [ELn])
                if stop < 1.9:
                    h.memset("pool", yo_[:, :, tc], 0.0, [yo_])
                    continue
                h.tt("pool", ELn[:], ELn[:], s3(nbeta[:], 6, 128), ALU.mult, [ELn, nbeta], [ELn])
                if stop < 2:
                    h.memset("pool", yo_[:, :, tc], 0.0, [yo_])
                    continue
                w1 = w4(W1)
                w2 = w4(W2)
                h.mm([(w1[:, hd // 3, hd % 3, :], kz[hd % 2][:, hd // 2, tc], kn_[:, hd // 2, tc], True, True) for hd in range(6)],
                     [kz[0], kz[1], kn_], [W1a, W1b])
                h.mm([(w2[:, hd // 3, hd % 3, :], kz[hd % 2][:, hd // 2, tc], qn_[:, hd // 2, tc], True, True) for hd in range(6)],
                     [kz[0], kz[1], qn_], [W2a, W2b])
                h.tt("dve", v4(Pm[0][:]), w1, v4(ELn[:]), ALU.mult, [W1a, W1b, ELn], [Pm[0]])
                h.tt("dve", v4(attnT[:]), w2, v4(EU[:]), ALU.mult, [W2a, W2b, EU], [attnT])
                if stop < 3:
                    h.memset("pool", yo_[:, :, tc], 0.0, [yo_])
                    continue
                h.tr([(PB[:, hd * 128:(hd + 1) * 128], Pm[0][:, hd, :]) for hd in range(6)], identb[:], [Pm[0], identb], [PB])
                h.cp("act", Qm[0][:], PB[:, 0:768].rearrange("p (a l) -> p a l", a=6), [PB], [Qm[0]])
                Nn, NT_ = Pm[0], Qm[0]
                h.tt("pool", Pm[1][:], Nn[:], b3(cmask[:, 0, :], 6, 128), ALU.mult, [Nn, cmask], [Pm[1]])
                h.tt("pool", Qm[1][:], NT_[:], b3(cmask[:, 7, :], 6, 128), ALU.mult, [NT_, cmask], [Qm[1]])
                h.tt("dve", Tb[:], Pm[1][:], b3(identf[:], 6, 128), ALU.add, [Pm[1], identf], [Tb])
                h.tt("dve", Xb[:], Qm[1][:], b3(identf[:], 6, 128), ALU.add, [Qm[1], identf], [Xb])
                Tc, Xc = Tb, Xb
                Tn, Xn = Tb2, Xb2
                for lv in range(1, 7):
                    w1_ = w4(W1)
                    w2_ = w4(W2)
                    for hf_ in range(2):
                        hs = range(3 * hf_, 3 * hf_ + 3)
                        h.mm([(w1_[:, hf_, hd % 3, :], NT_[:, hd, :], Tc[:, hd, :], True, True) for hd in hs], [NT_, Tc], [W1h[hf_]])
                        h.mm([(w2_[:, hf_, hd % 3, :], Nn[:, hd, :], Xc[:, hd, :], True, True) for hd in hs], [Nn, Xc], [W2h[hf_]])
                    for hf_ in range(2):
                        sl = slice(3 * hf_, 3 * hf_ + 3)
                        h.tt("dve", M1b[:, sl, :], w1_[:, hf_, :, :], cmask6[:, lv, sl, :], ALU.mult, [W1h[hf_], cmask6], [M1bh[hf_]])
                        h.tt("dve", M1pb[:, sl, :], w2_[:, hf_, :, :], cmask6[:, 7 + lv, sl, :], ALU.mult, [W2h[hf_], cmask6], [M1pbh[hf_]])
                    for hf_ in range(2):
                        hs = range(3 * hf_, 3 * hf_ + 3)
                        sl = slice(3 * hf_, 3 * hf_ + 3)
                        h.mm([(W1[:, hf_, 0:384], identb[:], Tc[:, sl, :].rearrange("p a l -> p (a l)"), True, False)]
                             + [(w1_[:, hf_, hd % 3, :], Xc[:, hd, :], M1b[:, hd, :], False, True) for hd in hs],
                             [identb, Tc, Xc, M1bh[hf_]], [W1h[hf_]])
                        h.mm([(W2[:, hf_, 0:384], identb[:], Xc[:, sl, :].rearrange("p a l -> p (a l)"), True, False)]
                             + [(w2_[:, hf_, hd % 3, :], Tc[:, hd, :], M1pb[:, hd, :], False, True) for hd in hs],
                             [identb, Tc, Xc, M1pbh[hf_]], [W2h[hf_]])
                    for hf_ in range(2):
                        sl = slice(3 * hf_, 3 * hf_ + 3)
                        h.cp("act", Tn[:, sl, :], w1_[:, hf_, :, :], [W1h[hf_]], [Tn])
                        h.cp("act", Xn[:, sl, :], w2_[:, hf_, :, :], [W2h[hf_]], [Xn])
                    Tc, Xc, Tn, Xn = Tn, Xn, Tc, Xc
                Xb = Xc
                if stop < 4:
                    h.memset("pool", yo_[:, :, tc], 0.0, [yo_])
                    continue
                h.tr([(PB[:, j * 128:(j + 1) * 128], kn_[:, j, tc]) for j in range(3)], identb[:], [kn_, identb], [PB])
                h.tt("dve", x3(kdec[:]), x3(PB[:, 0:384]), s3(dkk[:], 6, 64), ALU.mult, [PB, dkk], [kdec])
                h.tr([(W0[:, 0, j * 128:(j + 1) * 128], v_[:, j, tc]) for j in range(3)], identf[:], [v_, identf], [W0])
                h.cp("act", v_tm[:], W0[:, 0, 0:384], [W0], [v_tm])
                if stop < 5:
                    h.memset("pool", yo_[:, :, tc], 0.0, [yo_])
                    continue
                h.mm([(W1[:, 0, j * 128:(j + 1) * 128], kn_[:, j, tc], Sb[:, j, :], True, True) for j in range(3)], [kn_, Sb], [W1a, W1b])
                h.tt("dve", x3(rr_[:]), x3(W1[:, 0, 0:384]), s3(egc[:], 6, 64), ALU.mult, [W1a, W1b, egc], [rr_])
                h.tt("pool", rr_[:], rr_[:], v_tm[:], ALU.subtract, [rr_, v_tm], [rr_])
                h.tt("pool", x3(rb[:]), x3(rr_[:]), s3(nbeta[:], 6, 64), ALU.mult, [rr_, nbeta], [rb])
                h.mm([(W2[:, 0, hd * 64:(hd + 1) * 64], Xb[:, hd, :], rb[:, hd * 64:(hd + 1) * 64], True, True) for hd in range(6)], [Xb, rb], [W2a, W2b])
                h.cp("act", vnb[:], W2[:, 0, 0:384], [W2a, W2b], [vnb])
                h.mm([(W1[:, 0, j * 128:(j + 1) * 128], qn_[:, j, tc], Sb[:, j, :], True, True) for j in range(3)], [qn_, Sb], [W1a, W1b])
                h.tt("dve", x3(oa[:]), x3(W1[:, 0, 0:384]), s3(egc[:], 6, 64), ALU.mult, [W1a, W1b, egc], [oa])
                h.mm([(W2[:, 0, hd * 64:(hd + 1) * 64], attnT[:, hd, :], vnb[:, hd * 64:(hd + 1) * 64], True, True) for hd in range(6)], [attnT, vnb], [W2a, W2b])
                h.tt("dve", o[:], W2[:, 0, 0:384], oa[:], ALU.add, [W2a, W2b, oa], [o])
                h.mm([(W0[:, 0, j * 128:(j + 1) * 128], kdec[:, j * 128:(j + 1) * 128], vnb[:, j * 128:(j + 1) * 128], True, True) for j in range(3)],
                     [kdec, vnb], [W0])
                h.tt("dve", tmpS[:], W0[:, 0, 0:384].rearrange("p (j c) -> p j c", j=3), b3(masks["bd"][:], 3, 128), ALU.mult, [W0, masks["bd"]], [tmpS])
                h.tt("pool", Sf[:], Sf[:], s3(gtc[:], 3, 128), ALU.mult, [Sf, gtc], [Sf])
                h.tt("pool", Sf[:], Sf[:], tmpS[:], ALU.add, [Sf, tmpS], [Sf])
                h.cp("act", Sb[:], Sf[:], [Sf], [Sb])
                if stop < 6:
                    h.memset("pool", yo_[:, :, tc], 0.0, [yo_])
                    continue
                h.tt("pool", sq[:], o[:], o[:], ALU.mult, [o], [sq])
                h.reduce(ss6[:], x3(sq[:]), ALU.add, [sq], [ss6])
                h.act(rs6[:], ss6[:], AF.Ln, [ss6, epsb], [rs6], bias=epsb[:, 0:1], scale=1.0 / 64)
                h.act(rs6[:], rs6[:], AF.Exp, [rs6], [rs6], scale=-0.5)
                h.tt("dve", x3(o[:]), x3(o[:]), s3(rs6[:], 6, 64), ALU.mult, [o, rs6], [o])
                h.tt("pool", x3(o[:]), x3(o[:]), b3(gnw[:], 6, 64), ALU.mult, [o, gnw], [o])
                h.act(ez[:], pj_[:, ti, 0:384], AF.Exp, [pj_], [ez], scale=-1.0)
                h.ts("pool", ez[:], ez[:], 1.0, None, ALU.add, None, [ez], [ez])
                h.tt("pool", zg[:], o[:], pj_[:, ti, 0:384], ALU.mult, [o, pj_], [zg])
                h.recip(ez[:], ez[:], [ez], [ez])
                h.tt("pool", yb[:], zg[:], ez[:], ALU.mult, [zg, ez], [yb])
                h.tr([(PB[:, j * 128:(j + 1) * 128], yb[:, j * 128:(j + 1) * 128]) for j in range(3)], identb[:], [yb, identb], [PB])
                h.cp("act", yo_[:, :, tc], PB[:, 0:384].rearrange("p (j t) -> p j t", j=3), [PB], [yo_])
            h.dma("sp", k.y_d.t[256:640, cols].rearrange("(j p) t -> p j t", p=128), yo_[:], [yo_], [k.r_y[1][mt]], yo_)
        P.end_phase()


class K:
    def __init__(self, T, L, flags):
        self.T = T
        self.L = L
        self.NT = T // 128
        self.flags = flags


def bcast(ap, shape):
    return ap.to_broadcast(list(shape))


def build(T, L, flags=None):
    flags = flags or {}
    nc = bass.Bass("TRN2", target_bir_lowering=False)
    k = K(T, L, flags)
    NT = T // 128
    with contextlib.ExitStack() as es0:
        P = Prog(nc, es0)
        P.verbose = bool(flags.get("verbose"))
        k.P = P
        inp = {}

        def ein(name, shape):
            inp[name] = P.dram(name, shape, F32, "ExternalInput")
            return inp[name]

        x_in = ein("x", [T, D])
        c_in = ein("c", [1, D])
        w_ada = ein("w_ada", [L, D, 6 * D])
        b_ada = ein("b_ada", [L, 6 * D])
        norm_mix = ein("norm_mix", [L, D])
        norm_ffn = ein("norm_ffn", [L, D])
        norm_final = ein("norm_final", [1, D])
        w_rt = ein("w_rt", [L, D, 36])
        b_rt = ein("b_rt", [L, 36])
        w_gate = ein("moe_w_gate", [L, NEXP, D, DEXP])
        w_up = ein("moe_w_up", [L, NEXP, D, DEXP])
        w_down = ein("moe_w_down", [L, NEXP, DEXP, D])
        ein("w_in_f", [L, D, 2304])
        ein("w_in_t", [L, D, 786])
        ein("w_out", [L, D, D])
        ein("conv_w", [L, 128, 16, 4])
        ein("conv_b", [L, 128, 16])
        ein("bias12", [L, 12])
        ein("alog12", [L, 12])
        ein("ssd_d", [L, 6])
        ein("ssd_norm", [L, 384])
        ein("gdn_norm", [L, 64])
        ein("gdn_cmask", [14, 128, 128])
        ein("s5_are", [L, 128, 8])
        ein("s5_aim", [L, 128, 8])
        ein("s5_ldt", [L, 128, 8])
        ein("s5_dcol", [L, 128, 2])
        ein("s5_ncol", [L, 128, 2])
        ein("s5_bT_re", [L, 8, 128, 128])
        ein("s5_bT_im", [L, 8, 128, 128])
        ein("s5_cT_re", [L, 8, 128, 128])
        ein("s5_cT_im", [L, 8, 128, 128])
        ein("s5_w_glu", [L, 256, 256])
        out = P.dram("out", [T, D], F32, "ExternalOutput")
        k.u_d = P.dram("u_d", [256, T], F32)
        k.qn_d = P.dram("qn_d", [384, T], BF16)
        k.kn_d = P.dram("kn_d", [384, T], BF16)
        k.v_d = P.dram("v_d", [384, T], F32)
        k.xs_d = P.dram("xs_d", [384, T], F32)
        k.B_d = P.dram("B_d", [256, T], BF16)
        k.C_d = P.dram("C_d", [256, T], BF16)
        k.pt_d = P.dram("pt_d", [T, 786], F32)
        k.y_d = P.dram("y_d", [D, T], BF16)
        k.r_pre = [P.region(f"pre_{i}") for i in range(max(1, T // 512))]
        k.r_y = [[P.region(f"y{j}_{i}") for i in range(max(1, T // 512))] for j in range(3)]
        k.h = H(P)
        h = k.h
        modv = P.dram("modv", [L, 6 * D], F32)
        dbg = flags.get("dbg", False)
        if dbg:
            dbg_coef = P.dram("dbg_coef", [T, 32], F32, "ExternalOutput")
            dbg_y = P.dram("dbg_y", [T, D], F32, "ExternalOutput")
            dbg_h = P.dram("dbg_h", [T, D], F32, "ExternalOutput")
            r_dbg = P.region("dbg")
        scr = [P.dram("xs0", [T, D], F32), P.dram("xs1", [T, D], F32)]
        k.inp = inp

        def regs(name):
            return [P.region(f"{name}_{i}") for i in range(NT)]
        r_x = regs("x")
        r_scr = [regs("xs0"), regs("xs1")]
        r_out = regs("out")
        r_modv = P.region("modv")

        identf = P.sb(es0, "identf", [128, 128], F32)
        identb = P.sb(es0, "identb", [128, 128], BF16)
        P.op("pool", lambda e: e.memset(identf[:], 0.0), writes=[identf])
        P.op("pool", lambda e: e.affine_select(out=identf[:], in_=identf[:], pattern=[[-1, 128]],
                                               compare_op=ALU.not_equal, fill=1.0, base=0,
                                               channel_multiplier=1),
             reads=[identf], writes=[identf])
        P.op("dve", lambda e: e.tensor_copy(out=identb[:], in_=identf[:]), reads=[identf], writes=[identb])
        k.identf, k.identb = identf, identb

        with contextlib.ExitStack() as es:
            ccol = P.sb(es, "ccol", [128, 8], F32)
            cb = P.sb(es, "cb", [128, 8, 128], BF16)
            wa = [P.sb(es, f"wa{i}", [128, 8, 512], BF16) for i in range(2)]
            pm = [P.ps(es, f"pm{i}", [128, 512], F32) for i in range(2)]
            brow = [P.sb(es, f"brow{i}", [1, 512], F32) for i in range(2)]
            mrow = [P.sb(es, f"mrow{i}", [1, 512], F32) for i in range(2)]
            P.op("sp", lambda e: e.dma_start(out=ccol[:], in_=c_in.t.rearrange("o (k p) -> p (o k)", p=128),
                                             allow_slow_non_contiguous=True),
                 writes=[ccol], dma=ccol)
            P.op("act", lambda e: e.activation(out=ccol[:], in_=ccol[:], func=AF.Silu), reads=[ccol], writes=[ccol])
            P.op("dve", lambda e: e.tensor_copy(out=cb[:], in_=bcast(ccol[:].unsqueeze(2), [128, 8, 128])),
                 reads=[ccol], writes=[cb])
            it = 0
            for l in range(L):
                for n in range(12):
                    i = it % 2
                    it += 1
                    P.op("pool", lambda e, l=l, n=n, i=i: e.dma_start(
                        out=wa[i][:], in_=w_ada.t[l, :, n * 512:(n + 1) * 512].rearrange("(k p) n -> p k n", p=128)),
                        writes=[wa[i]], dma=wa[i])
                    P.op("sp", lambda e, l=l, n=n, i=i: e.dma_start(
                        out=brow[i][:], in_=b_ada.t[l:l + 1, n * 512:(n + 1) * 512]),
                        writes=[brow[i]], dma=brow[i])

                    def mm(e, i=i):
                        r = None
                        for kk in range(8):
                            r = e.matmul(pm[i][:], lhsT=cb[:, kk, :], rhs=wa[i][:, kk, :], start=(kk == 0), stop=(kk == 7))
                        return r
                    P.op("pe", mm, reads=[cb, wa[i]], writes=[pm[i]])
                    P.op("dve", lambda e, i=i: e.tensor_tensor(out=mrow[i][:], in0=pm[i][0:1, :], in1=brow[i][:], op=ALU.add),
                         reads=[pm[i], brow[i]], writes=[mrow[i]])
                    P.op("sp", lambda e, l=l, n=n, i=i: e.dma_start(
                        out=modv.t[l:l + 1, n * 512:(n + 1) * 512], in_=mrow[i][:]),
                        reads=[mrow[i]], writes=[r_modv], dma=mrow[i])
            P.end_phase()

        def load_row(dst, src_ap, extra_reads=()):
            P.op("sp", lambda e: e.dma_start(out=dst[:], in_=src_ap.partition_broadcast(128)),
                 reads=list(extra_reads), writes=[dst], dma=dst)

        def norm_consts(es, l, nw, i_scale, i_shift, tag):
            A = P.sb(es, f"A{tag}", [128, D], F32)
            B = P.sb(es, f"B{tag}", [128, D], F32)
            W = P.sb(es, f"W{tag}", [128, D], F32)
            load_row(A, modv.t[l:l + 1, i_scale * D:(i_scale + 1) * D], [r_modv])
            load_row(B, modv.t[l:l + 1, i_shift * D:(i_shift + 1) * D], [r_modv])
            load_row(W, nw)
            P.op("dve", lambda e: e.scalar_tensor_tensor(out=A[:], in0=A[:], scalar=1.0, in1=W[:], op0=ALU.add, op1=ALU.mult),
                 reads=[A, W], writes=[A])
            return A, B

        def rms_mod(xt, A, B, hout, ssq, rstd, junk, tmp):
            P.op("act", lambda e: e.activation(out=junk[:], in_=xt[:], func=AF.Square, accum_out=ssq[:]),
                 reads=[xt], writes=[junk, ssq], est=0.9)
            P.op("dve", lambda e: e.tensor_scalar(out=rstd[:], in0=ssq[:], scalar1=1.0 / D, scalar2=EPS, op0=ALU.mult, op1=ALU.add),
                 reads=[ssq], writes=[rstd])
            P.op("act", lambda e: e.activation(out=rstd[:], in_=rstd[:], func=AF.Sqrt), reads=[rstd], writes=[rstd])
            P.op("dve", lambda e: e.reciprocal(out=rstd[:], in_=rstd[:]), reads=[rstd], writes=[rstd])
            if B is None:
                P.op("dve", lambda e: e.scalar_tensor_tensor(out=hout[:], in0=xt[:], scalar=rstd[:, 0:1], in1=A[:], op0=ALU.mult, op1=ALU.mult),
                     reads=[xt, rstd, A], writes=[hout], est=1.15)
            else:
                P.op("dve", lambda e: e.scalar_tensor_tensor(out=tmp[:], in0=xt[:], scalar=rstd[:, 0:1], in1=A[:], op0=ALU.mult, op1=ALU.mult),
                     reads=[xt, rstd, A], writes=[tmp], est=1.15)
                P.op("pool", lambda e: e.tensor_tensor(out=hout[:], in0=tmp[:], in1=B[:], op=ALU.add),
                     reads=[tmp, B], writes=[hout], est=1.3)

        k.modv, k.r_modv = modv, r_modv
        k.load_row, k.norm_consts, k.rms_mod = load_row, norm_consts, rms_mod
        cur = (x_in, r_x)
        nxt_i = 0

        def next_dst():
            nonlocal nxt_i
            d = (scr[nxt_i], r_scr[nxt_i])
            nxt_i ^= 1
            return d

        for l in range(L):
            if flags.get("mixer", True):
                dstt = next_dst()
                mixer_layer(k, l, cur, dstt)
                cur = dstt
            if flags.get("moe", True):
                src, rsrc = cur
                dst, rdst = next_dst()
                SBT = min(16, NT)
                with contextlib.ExitStack() as es:
                    A, B = norm_consts(es, l, norm_ffn.t[l:l + 1, :], 4, 3, "f")
                    G = P.sb(es, "Gf", [128, D], F32)
                    load_row(G, modv.t[l:l + 1, 5 * D:6 * D], [r_modv])
                    brt = P.sb(es, "brt", [128, 36], F32)
                    load_row(brt, b_rt.t[l:l + 1, :])
                    wrt = P.sb(es, "wrt", [128, 8, 36], F32)
                    P.op("sp", lambda e: e.dma_start(out=wrt[:], in_=w_rt.t[l].rearrange("(k p) n -> p k n", p=128)),
                         writes=[wrt], dma=wrt)
                    hT = P.sb(es, "hT", [128, 8, SBT * 128], BF16)
                    yacc = [P.sb(es, f"yacc{i}", [128, D], F32) for i in range(SBT)]
                    coef = P.sb(es, "coef", [128, SBT, 32], F32)
                    xt = [P.sb(es, f"xt{i}", [128, D], F32) for i in range(2)]
                    R_hf = Rot(P, es, "hf", [128, D], F32)
                    R_tmp = Rot(P, es, "tmpf", [128, D], F32)
                    R_junk = Rot(P, es, "junkf", [128, D], F32)
                    R_hTf = Rot(P, es, "hTf", [128, 8, 128], F32)
                    R_ssq = Rot(P, es, "ssq", [128, 1], F32)
                    R_rstd = Rot(P, es, "rstd", [128, 1], F32)
                    R_lg = Rot(P, es, "lg", [128, 36], F32)
                    R_sm = Rot(P, es, "sm", [128, 16], F32)
                    R_gm = Rot(P, es, "gm", [128, 4], F32)
                    R_gex = Rot(P, es, "gex", [128, 4], F32)
                    R_le4 = Rot(P, es, "le4", [128, 4, 8], F32)
                    R_les = Rot(P, es, "les", [128, 8], F32)
                    R_le2 = Rot(P, es, "le2", [128, 8], F32)
                    R_mk1 = Rot(P, es, "mk1", [128, 8], F32)
                    R_mk2 = Rot(P, es, "mk2", [128, 8], F32)
                    R_csel = Rot(P, es, "csel", [128, 8], F32)
                    wg = [P.sb(es, f"wg{i}", [128, 8, DEXP], BF16) for i in range(2)]
                    wu = [P.sb(es, f"wu{i}", [128, 8, DEXP], BF16) for i in range(2)]
                    wd = [P.sb(es, f"wd{i}", [128, 2, D], BF16) for i in range(2)]
                    sg = [P.sb(es, f"sg{i}", [128, 512], F32) for i in range(2)]
                    hid = [P.sb(es, f"hid{i}", [128, 2, 512], BF16) for i in range(2)]
                    ptr = P.ps(es, "ptr", [128, 8, 128], F32)
                    pg = [P.ps(es, f"pg{i}", [128, 512], F32) for i in range(2)]
                    pu = [P.ps(es, f"pu{i}", [128, 512], F32) for i in range(2)]
                    py = [P.ps(es, f"py{i}", [128, 512], F32) for i in range(2)]
                    py.append(Buf(ptr.t[:, 0:4, :].rearrange("p a b -> p (a b)"), "py2"))
                    py.append(Buf(ptr.t[:, 4:8, :].rearrange("p a b -> p (a b)"), "py3"))
                    ptrA, ptrB = py[2], py[3]

                    wcnt = 0
                    for sb0 in range(0, NT, SBT):
                        def router_tile(t, ti, xb, hf, tmp, junk, hTf, ssq, rstd, lg, sm, gm, gex, le4, les, le2, mk1, mk2, csel):
                                P.op("sp", lambda e, t=t, xb=xb: e.dma_start(out=xb[:], in_=src.t[t * 128:(t + 1) * 128, :]),
                                     reads=[rsrc[t]], writes=[xb], dma=xb)
                                rms_mod(xb, A, B, hf, ssq, rstd, junk, tmp)

                                if dbg and l == 0:
                                    P.op("sp", lambda e, t=t: e.dma_start(out=dbg_h.t[t * 128:(t + 1) * 128, :], in_=hf[:]),
                                         reads=[hf], writes=[r_dbg], dma=hf)

                                def trf(e):
                                    r = None
                                    for kk in range(8):
                                        r = e.transpose(out=ptr[:, kk, :], in_=hf[:, kk * 128:(kk + 1) * 128], identity=identf[:])
                                    return r
                                P.op("pe", trf, reads=[hf, identf], writes=[ptrA, ptrB])
                                P.op("act", lambda e: e.copy(out=hTf[:], in_=ptr[:]), reads=[ptrA, ptrB], writes=[hTf])
                                P.op("dve", lambda e, ti=ti: e.tensor_copy(out=hT[:, :, ti * 128:(ti + 1) * 128], in_=hTf[:]),
                                     reads=[hTf], writes=[hT])

                                def mrt(e):
                                    r = None
                                    for kk in range(8):
                                        r = e.matmul(ptr[:, 0, 0:36], lhsT=hTf[:, kk, :], rhs=wrt[:, kk, :], start=(kk == 0), stop=(kk == 7))
                                    return r
                                P.op("pe", mrt, reads=[hTf, wrt], writes=[ptrA, ptrB])
                                P.op("dve", lambda e: e.tensor_tensor(out=lg[:], in0=ptr[:, 0, 0:36], in1=brt[:], op=ALU.add),
                                     reads=[ptrA, ptrB, brt], writes=[lg])
                                P.op("dve", lambda e: e.tensor_reduce(out=sm[:, 0:1], in_=lg[:, 0:4], axis=AX.X, op=ALU.max),
                                     reads=[lg], writes=[sm])
                                P.op("dve", lambda e: e.tensor_scalar(out=gm[:], in0=lg[:, 0:4], scalar1=sm[:, 0:1], scalar2=None, op0=ALU.is_equal),
                                     reads=[lg, sm], writes=[gm])
                                P.op("dve", lambda e: e.tensor_scalar(out=sm[:, 1:2], in0=sm[:, 0:1], scalar1=-1.0, scalar2=None, op0=ALU.mult),
                                     reads=[sm], writes=[sm])
                                P.op("act", lambda e: e.activation(out=gex[:], in_=lg[:, 0:4], func=AF.Exp, bias=sm[:, 1:2], accum_out=sm[:, 2:3]),
                                     reads=[lg, sm], writes=[gex, sm])
                                P.op("dve", lambda e: e.reciprocal(out=sm[:, 3:4], in_=sm[:, 2:3]), reads=[sm], writes=[sm])
                                P.op("dve", lambda e: e.tensor_tensor(out=le4[:], in0=lg[:, 4:36].rearrange("p (g e) -> p g e", g=4),
                                                                      in1=bcast(gm[:].unsqueeze(2), [128, 4, 8]), op=ALU.mult),
                                     reads=[lg, gm], writes=[le4])
                                P.op("dve", lambda e: e.tensor_reduce(out=les[:], in_=le4[:].rearrange("p g e -> p e g"), axis=AX.X, op=ALU.add),
                                     reads=[le4], writes=[les])
                                P.op("dve", lambda e: e.tensor_reduce(out=sm[:, 4:5], in_=les[:], axis=AX.X, op=ALU.max), reads=[les], writes=[sm])
                                P.op("dve", lambda e: e.tensor_scalar(out=mk1[:], in0=les[:], scalar1=sm[:, 4:5], scalar2=None, op0=ALU.is_equal),
                                     reads=[les, sm], writes=[mk1])
                                P.op("dve", lambda e: e.scalar_tensor_tensor(out=le2[:], in0=mk1[:], scalar=-1e30, in1=les[:], op0=ALU.mult, op1=ALU.add),
                                     reads=[mk1, les], writes=[le2])
                                P.op("dve", lambda e: e.tensor_reduce(out=sm[:, 5:6], in_=le2[:], axis=AX.X, op=ALU.max), reads=[le2], writes=[sm])
                                P.op("dve", lambda e: e.tensor_scalar(out=mk2[:], in0=le2[:], scalar1=sm[:, 5:6], scalar2=None, op0=ALU.is_equal),
                                     reads=[le2, sm], writes=[mk2])
                                P.op("dve", lambda e: e.tensor_tensor(out=sm[:, 6:7], in0=sm[:, 4:5], in1=sm[:, 5:6], op=ALU.subtract),
                                     reads=[sm], writes=[sm])
                                P.op("act", lambda e: e.activation(out=sm[:, 7:8], in_=sm[:, 6:7], func=AF.Sigmoid), reads=[sm], writes=[sm])
                                P.op("act", lambda e: e.activation(out=sm[:, 8:9], in_=sm[:, 6:7], func=AF.Sigmoid, scale=-1.0), reads=[sm], writes=[sm])
                                P.op("dve", lambda e: e.tensor_scalar(out=sm[:, 7:9], in0=sm[:, 7:9], scalar1=sm[:, 3:4], scalar2=None, op0=ALU.mult),
                                     reads=[sm], writes=[sm])
                                P.op("dve", lambda e: e.tensor_scalar(out=csel[:], in0=mk1[:], scalar1=sm[:, 7:8], scalar2=None, op0=ALU.mult),
                                     reads=[mk1, sm], writes=[csel])
                                P.op("dve", lambda e: e.scalar_tensor_tensor(out=csel[:], in0=mk2[:], scalar=sm[:, 8:9], in1=csel[:], op0=ALU.mult, op1=ALU.add),
                                     reads=[mk2, sm, csel], writes=[csel])
                                P.op("dve", lambda e, ti=ti: e.tensor_tensor(out=coef[:, ti, :].rearrange("p (g e) -> p g e", g=4),
                                                                             in0=bcast(gm[:].unsqueeze(2), [128, 4, 8]),
                                                                             in1=bcast(csel[:].unsqueeze(1), [128, 4, 8]), op=ALU.mult),
                                     reads=[gm, csel], writes=[coef])

                        for ti in range(SBT):
                            t = sb0 + ti
                            router_tile(t, ti, xt[t % 2], R_hf.at(t), R_tmp.at(t), R_junk.at(t), R_hTf.at(t), R_ssq.at(t), R_rstd.at(t), R_lg.at(t), R_sm.at(t), R_gm.at(t), R_gex.at(t), R_le4.at(t), R_les.at(t), R_le2.at(t), R_mk1.at(t), R_mk2.at(t), R_csel.at(t))
                        nblk = (SBT * 128 + 511) // 512
                        seq = [(ex, blk) for ex in range(NEXP) for blk in range(nblk)]

                        def load_w(ex):
                            wi = ex % 2
                            h.dma("pool", wg[wi][:], w_gate.t[l, ex].rearrange("(k p) n -> p k n", p=128), [], [wg[wi]], wg[wi])
                            h.dma("pool", wu[wi][:], w_up.t[l, ex].rearrange("(k p) n -> p k n", p=128), [], [wu[wi]], wu[wi])
                            h.dma("pool", wd[wi][:], w_down.t[l, ex].rearrange("(k p) n -> p k n", p=128), [], [wd[wi]], wd[wi])

                        def GU(i):
                            ex, blk = seq[i]
                            wi, bi = ex % 2, i % 2
                            c0 = blk * 512
                            cw = min(512, SBT * 128 - c0)
                            for fc in range(2):
                                fs = slice(fc * 128, (fc + 1) * 128)
                                h.mm([(pg[fc][:, 0:cw], wg[wi][:, kk, fs], hT[:, kk, c0:c0 + cw], kk == 0, kk == 7) for kk in range(8)],
                                     [wg[wi], hT], [pg[fc]])
                                h.act(sg[fc][:, 0:cw], pg[fc][:, 0:cw], AF.Silu, [pg[fc]], [sg[fc]])
                                yield
                                h.mm([(pu[fc][:, 0:cw], wu[wi][:, kk, fs], hT[:, kk, c0:c0 + cw], kk == 0, kk == 7) for kk in range(8)],
                                     [wu[wi], hT], [pu[fc]])
                                h.tt("dve", hid[bi][:, fc, 0:cw], sg[fc][:, 0:cw], pu[fc][:, 0:cw], ALU.mult, [sg[fc], pu[fc]], [hid[bi]])
                                yield

                        def DN(i):
                            ex, blk = seq[i]
                            wi, bi = ex % 2, i % 2
                            c0 = blk * 512
                            cw = min(512, SBT * 128 - c0)
                            for st in range(cw // 128):
                                ti = blk * 4 + st
                                for n2 in range(2):
                                    pi = (st * 2 + n2) % 4
                                    ns = slice(n2 * 512, (n2 + 1) * 512)
                                    h.mm([(py[pi][:], hid[bi][:, fc, st * 128:(st + 1) * 128], wd[wi][:, fc, ns], fc == 0, fc == 1) for fc in range(2)],
                                         [hid[bi], wd[wi]], [py[pi]])
                                    if ex == 0:
                                        h.ts("dve", yacc[ti][:, ns], py[pi][:], coef[:, ti, ex:ex + 1], None, ALU.mult, None, [py[pi], coef], [yacc[ti]])
                                    else:
                                        h.stt("dve", yacc[ti][:, ns], py[pi][:], coef[:, ti, ex:ex + 1], yacc[ti][:, ns], ALU.mult, ALU.add,
                                              [py[pi], coef, yacc[ti]], [yacc[ti]])
                                    yield
                            if blk == nblk - 1 and ex + 2 < NEXP:
                                load_w(ex + 2)

                        def drain(g):
                            for _ in g:
                                pass

                        def step(g, n):
                            for _ in range(n):
                                if next(g, "END") == "END":
                                    return

                        load_w(0)
                        load_w(1)
                        drain(GU(0))
                        for i in range(len(seq)):
                            gd = DN(i)
                            if i + 1 < len(seq):
                                gg = GU(i + 1)
                                for _ in range(4):
                                    step(gg, 1)
                                    step(gd, 2)
                                drain(gg)
                            drain(gd)
                        for ti in range(SBT):
                            t = sb0 + ti
                            if dbg and l == 0:
                                P.op("sp", lambda e, t=t, ti=ti: e.dma_start(out=dbg_y.t[t * 128:(t + 1) * 128, :], in_=yacc[ti][:]),
                                     reads=[yacc[ti]], writes=[r_dbg], dma=yacc[ti])
                                P.op("sp", lambda e, t=t, ti=ti: e.dma_start(out=dbg_coef.t[t * 128:(t + 1) * 128, :], in_=coef[:, ti, :]),
                                     reads=[coef], writes=[r_dbg], dma=coef)
                            xb = xt[t % 2]
                            P.op("sp", lambda e, t=t, xb=xb: e.dma_start(out=xb[:], in_=src.t[t * 128:(t + 1) * 128, :]),
                                 reads=[rsrc[t]], writes=[xb], dma=xb)
                            P.op("pool", lambda e, ti=ti: e.tensor_tensor(out=yacc[ti][:], in0=yacc[ti][:], in1=G[:], op=ALU.mult),
                                 reads=[yacc[ti], G], writes=[yacc[ti]])
                            P.op("dve", lambda e, ti=ti, xb=xb: e.tensor_tensor(out=yacc[ti][:], in0=yacc[ti][:], in1=xb[:], op=ALU.add),
                                 reads=[yacc[ti], xb], writes=[yacc[ti]])
                            P.op("sp", lambda e, t=t, ti=ti: e.dma_start(out=dst.t[t * 128:(t + 1) * 128, :], in_=yacc[ti][:]),
                                 reads=[yacc[ti]], writes=[rdst[t]], dma=yacc[ti])
                    P.end_phase()
                cur = (dst, rdst)

        src, rsrc = cur
        with contextlib.ExitStack() as es:
            Wn = P.sb(es, "Wn", [128, D], F32)
            load_row(Wn, norm_final.t[0:1, :])
            xt = [P.sb(es, f"xtn{i}", [128, D], F32) for i in range(2)]
            ho = [P.sb(es, f"hon{i}", [128, D], F32) for i in range(2)]
            junk = P.sb(es, "junkn", [128, D], F32)
            ssq = P.sb(es, "ssqn", [128, 1], F32)
            rstd = P.sb(es, "rstdn", [128, 1], F32)
            for t in range(NT):
                xb = xt[t % 2]
                hb = ho[t % 2]
                P.op("sp", lambda e, t=t, xb=xb: e.dma_start(out=xb[:], in_=src.t[t * 128:(t + 1) * 128, :]),
                     reads=[rsrc[t]], writes=[xb], dma=xb)
                rms_mod(xb, Wn, None, hb, ssq, rstd, junk, None)
                P.op("sp", lambda e, t=t, hb=hb: e.dma_start(out=out.t[t * 128:(t + 1) * 128, :], in_=hb[:]),
                     reads=[hb], writes=[r_out[t]], dma=hb)
            P.final_wait("sp", r_out)
            P.end_phase()
    return nc


def host_inputs(inputs, L, T):
    f = lambda a: np.ascontiguousarray(np.asarray(a, dtype=np.float32))
    w_rt = f(np.concatenate([inputs["moe_w_grp"][:L], inputs["moe_w_rt"][:L]], axis=-1))
    b_rt = f(np.concatenate([inputs["moe_b_grp"][:L], inputs["moe_b_rt"][:L]], axis=-1))
    w_in = np.asarray(inputs["w_in"][:L], dtype=np.float32)
    w_in_f = np.concatenate([w_in[:, :, 0:1408], w_in[:, :, 2188:3084]], axis=-1)
    w_in_t = np.concatenate([w_in[:, :, 1408:1792], w_in[:, :, 1804:2188], w_in[:, :, 1792:1798],
                             w_in[:, :, 3084:3090], w_in[:, :, 1798:1804]], axis=-1)
    gcw = np.asarray(inputs["gdn_conv_w"][:L], dtype=np.float32)
    scw = np.asarray(inputs["ssd_conv_w"][:L], dtype=np.float32)
    cw = np.concatenate([gcw, scw], axis=-1)
    conv_w = cw.reshape(L, 4, 16, 128).transpose(0, 3, 2, 1)
    cb = np.concatenate([np.zeros((L, 1152), np.float32), np.asarray(inputs["ssd_conv_b"][:L], dtype=np.float32)], axis=-1)
    conv_b = cb.reshape(L, 16, 128).transpose(0, 2, 1)
    bias12 = np.concatenate([inputs["gdn_dt_bias"][:L], inputs["ssd_dt_bias"][:L]], axis=-1)
    alog12 = np.concatenate([inputs["gdn_a_log"][:L], inputs["ssd_a_log"][:L]], axis=-1)
    def st_layout(a):
        a = np.asarray(a[:L], dtype=np.float32)
        return a.reshape(L, 8, 2, 64).transpose(0, 2, 3, 1).reshape(L, 128, 8)
    ldt = np.repeat(np.asarray(inputs["s5_log_dt"][:L], dtype=np.float32)[:, :, None], 64, axis=2)
    def bT_layout(b):
        b = np.asarray(b[:L], dtype=np.float32)
        o = np.zeros((L, 8, 128, 128), np.float32)
        for sc in range(8):
            for gl in range(2):
                r0 = 32 * (sc % 4) + 16 * gl
                o[:, sc, r0:r0 + 16, gl * 64:(gl + 1) * 64] = b[:, 2 * sc + gl].transpose(0, 2, 1)
        return o
    def cT_layout(c):
        c = np.asarray(c[:L], dtype=np.float32)
        o = np.zeros((L, 8, 128, 128), np.float32)
        for sc in range(8):
            for gl in range(2):
                r0 = 32 * (sc % 4) + 16 * gl
                o[:, sc, gl * 64:(gl + 1) * 64, r0:r0 + 16] = c[:, 2 * sc + gl].transpose(0, 2, 1)
        return o
    ii = np.arange(128)[:, None]
    jj = np.arange(128)[None, :]
    cm = []
    for lv in range(7):
        s_ = 1 << lv
        cm.append(((ii // (2 * s_) == jj // (2 * s_)) & (ii % (2 * s_) >= s_) & (jj % (2 * s_) < s_)).astype(np.float32))
    cmask = np.stack(cm + [m.T for m in cm], axis=0)
    col2 = lambda a: np.asarray(a[:L], dtype=np.float32).reshape(L, 2, 128).transpose(0, 2, 1)
    shared = {
        "gdn_cmask": f(cmask),
        "s5_are": f(st_layout(inputs["s5_a_re"])), "s5_aim": f(st_layout(inputs["s5_a_im"])), "s5_ldt": f(st_layout(ldt)),
        "s5_dcol": f(col2(inputs["s5_d"])), "s5_ncol": f(col2(inputs["s5_norm"])),
        "s5_bT_re": f(bT_layout(inputs["s5_b_re"])), "s5_bT_im": f(bT_layout(inputs["s5_b_im"])),
        "s5_cT_re": f(cT_layout(inputs["s5_c_re"])), "s5_cT_im": f(cT_layout(inputs["s5_c_im"])),
        "s5_w_glu": f(inputs["s5_w_glu"][:L]),
        "w_in_f": f(w_in_f), "w_in_t": f(w_in_t), "w_out": f(inputs["w_out"][:L]),
        "conv_w": f(conv_w), "conv_b": f(conv_b), "bias12": f(bias12), "alog12": f(alog12),
        "ssd_d": f(inputs["ssd_d"][:L]), "ssd_norm": f(inputs["ssd_norm"][:L]), "gdn_norm": f(inputs["gdn_norm"][:L]),
        "w_ada": f(inputs["w_ada"][:L]), "b_ada": f(inputs["b_ada"][:L]),
        "norm_mix": f(inputs["norm_mix"][:L]), "norm_ffn": f(inputs["norm_ffn"][:L]),
        "norm_final": f(inputs["norm_final"]).reshape(1, D),
        "w_rt": w_rt, "b_rt": b_rt,
        "moe_w_gate": f(inputs["moe_w_gate"][:L]), "moe_w_up": f(inputs["moe_w_up"][:L]),
        "moe_w_down": f(inputs["moe_w_down"][:L]),
    }
    maps = []
    B = inputs["x"].shape[0]
    for b in range(B):
        m = dict(shared)
        m["x"] = f(inputs["x"][b, :T])
        m["c"] = f(inputs["c"][b]).reshape(1, D)
        maps.append(m)
    return maps


def run(inputs, L, T, flags=None, trace=False):
    nc = build(T, L, flags)
    maps = host_inputs(inputs, L, T)
    res = run_bass_kernel_spmd(nc, maps, core_ids=list(range(len(maps))))
    if flags and flags.get("dbg"):
        return res.results
    return np.stack([r["out"] for r in res.results], axis=0)


def kernel(**inputs):
    return run(inputs, 4, 4096).astype(np.float32)
```
